# Optimizing a Trainium2 kernel written in Bass

```python
import jax, jax.numpy as jnp
from jax import lax
import numpy as np

D_MODEL = 1024
BATCH = 4
SEQ = 8192
DEPTH = 1

GRID_W = 64
CTX_LEN = 256
RW_HEADS = 8
RW_HEAD_DIM = 64
RW_WIDTH = RW_HEADS * RW_HEAD_DIM
RW_DECAY_LORA = 64
RW_AAA_LORA = 64
RW_GATE_LORA = 128
MLA_HEADS = 4
MLA_Q_LORA = 256
MLA_KV_LORA = 256
MLA_NOPE_DIM = 128
MLA_ROPE_DIM = 64
MLA_V_DIM = 128
MLA_WIDTH = MLA_HEADS * MLA_V_DIM
MIX_WIDTH = RW_WIDTH + MLA_WIDTH
ROPE_BASE = 10000.0
Q_BLOCK = 128
N_EXPERTS = 16
EXPERT_HIDDEN = 1024
CAPACITY_FACTOR = 2
LN_EPS = 1e-5
RMS_EPS = 1e-6
GN_EPS = 64e-5
RW_PROJ = 3 * RW_WIDTH + 2 * RW_DECAY_LORA + 2 * RW_AAA_LORA + RW_GATE_LORA
MLA_PROJ = MLA_Q_LORA + MLA_KV_LORA + MLA_ROPE_DIM
IN_PROJ = RW_PROJ + MLA_PROJ

kernel_name = 'hybrid_rwkv7_mla_ecmoe_dit_block'


def _standardize(x, eps):
    xf = x.astype(jnp.float32)
    mu = jnp.mean(xf, axis=-1, keepdims=True)
    var = jnp.mean(jnp.square(xf - mu), axis=-1, keepdims=True)
    return (xf - mu) * lax.rsqrt(var + eps)


def modulate(x, shift, scale):
    return (_standardize(x, LN_EPS) * (1.0 + scale) + shift).astype(x.dtype)


def layer_norm(x, g, b):
    return (_standardize(x, LN_EPS) * g + b).astype(x.dtype)


def rms_norm(x, g):
    xf = x.astype(jnp.float32)
    return (xf * lax.rsqrt(jnp.mean(jnp.square(xf), -1, keepdims=True) + RMS_EPS) * g).astype(x.dtype)


def split_cols(h, sizes):
    return jnp.split(h, np.cumsum(sizes)[:-1].tolist(), axis=-1)


def centred_conv3(h, w):
    hp = jnp.pad(h, ((0, 0), (1, 1), (0, 0)))
    return w[0] * hp[:, :-2] + w[1] * hp[:, 1:-1] + w[2] * hp[:, 2:]


def axial_angles(rows):
    rr, cc = jnp.meshgrid(jnp.arange(rows), jnp.arange(GRID_W), indexing='ij')
    half = MLA_ROPE_DIM // 2
    inv_freq = ROPE_BASE ** (-jnp.arange(0, half, 2, dtype=jnp.float32) / half)
    ang_r = rr.reshape(-1, 1).astype(jnp.float32) * inv_freq
    ang_c = cc.reshape(-1, 1).astype(jnp.float32) * inv_freq
    return ang_r, ang_c


def rotate_pairs(x, ang):
    x1, x2 = jnp.split(x, 2, axis=-1)
    cos = jnp.cos(ang).astype(x.dtype)
    sin = jnp.sin(ang).astype(x.dtype)
    return jnp.concatenate([x1 * cos - x2 * sin, x2 * cos + x1 * sin], axis=-1)


def rope_axial(x, ang_r, ang_c):
    half = MLA_ROPE_DIM // 2
    return jnp.concatenate([rotate_pairs(x[..., :half], ang_r), rotate_pairs(x[..., half:], ang_c)], axis=-1)


def rwkv_prepare(h, p):
    B, T, _ = h.shape
    heads = lambda t: t.reshape(B, T, RW_HEADS, RW_HEAD_DIM).astype(jnp.float32)
    rkv, w_dn, a_dn, g_dn = split_cols(h, (3 * RW_WIDTH, 2 * RW_DECAY_LORA, 2 * RW_AAA_LORA, RW_GATE_LORA))
    r, k, v = jnp.split(centred_conv3(rkv, p['rwkv_conv']), 3, axis=-1)
    g = jax.nn.sigmoid(g_dn) @ p['rwkv_g_up']
    kk = heads(k * p['rwkv_k_k'])
    kk = kk * lax.rsqrt(jnp.sum(jnp.square(kk), -1, keepdims=True) + 1e-12)
    w_dirs = jnp.split(w_dn, 2, axis=-1)
    a_dirs = jnp.split(a_dn, 2, axis=-1)
    per_dir = []
    for d in range(2):
        logw = -jax.nn.softplus(-(p['rwkv_w0'][d] + jnp.tanh(w_dirs[d]) @ p['rwkv_w_up'][d]).astype(jnp.float32)) - 0.5
        decay = jnp.exp(-jnp.exp(logw))
        a = jax.nn.sigmoid(p['rwkv_a0'][d] + a_dirs[d] @ p['rwkv_a_up'][d])
        k_d = k * (1.0 + (a - 1.0) * p['rwkv_k_a'])
        per_dir.append((heads(decay), heads(a), heads(k_d)))
    return heads(r), heads(v), kk, g, per_dir


def rwkv_scan(r, decay, k, v, kk, a, state0, reverse, emit):
    def step(S, inp):
        r_t, w_t, k_t, v_t, kk_t, a_t = inp
        s_kk = jnp.einsum('bhvk,bhk->bhv', S, kk_t)
        S = S * w_t[:, :, None, :] - s_kk[..., None] * (kk_t * a_t)[:, :, None, :] + v_t[..., None] * k_t[:, :, None, :]
        y = jnp.einsum('bhvk,bhk->bhv', S, r_t) if emit else None
        return S, y
    xs = tuple(jnp.moveaxis(t, 1, 0) for t in (r, decay, k, v, kk, a))
    state, ys = lax.scan(step, state0, xs, reverse=reverse)
    return state, (jnp.moveaxis(ys, 0, 1) if emit else None)


def rwkv_readout(y, r, v, k_sum, g, p):
    B, T = y.shape[:2]
    yn = _standardize(y, GN_EPS).reshape(B, T, RW_WIDTH) * p['rwkv_gn_g'] + p['rwkv_gn_b']
    bonus = jnp.sum(r * k_sum * p['rwkv_r_k'], -1, keepdims=True) * v
    return (yn + bonus.reshape(B, T, RW_WIDTH)).astype(g.dtype) * g


def rwkv_group(h, hc, p, emit_ctx):
    r, v, kk, g, dirs = rwkv_prepare(h, p)
    rc, vc, kkc, gc, dirs_c = rwkv_prepare(hc, p)
    B = h.shape[0]
    ys_lat, ys_ctx = [], []
    for d in range(2):
        decay, a, k_d = dirs[d]
        decay_c, a_c, k_dc = dirs_c[d]
        state0 = jnp.zeros((B, RW_HEADS, RW_HEAD_DIM, RW_HEAD_DIM), jnp.float32)
        state_c, yc = rwkv_scan(rc, decay_c, k_dc, vc, kkc, a_c, state0, d == 1, emit_ctx)
        _, yl = rwkv_scan(r, decay, k_d, v, kk, a, state_c, d == 1, True)
        ys_lat.append(yl)
        ys_ctx.append(yc)
    out_lat = rwkv_readout(ys_lat[0] + ys_lat[1], r, v, dirs[0][2] + dirs[1][2], g, p)
    out_ctx = None
    if emit_ctx:
        out_ctx = rwkv_readout(ys_ctx[0] + ys_ctx[1], rc, vc, dirs_c[0][2] + dirs_c[1][2], gc, p)
    return out_lat, out_ctx


def mla_queries(h, p, ang):
    B, T, _ = h.shape
    q = rms_norm(h[..., :MLA_Q_LORA], p['mla_q_norm']) @ p['mla_w_uq']
    q = q.reshape(B, T, MLA_HEADS, MLA_NOPE_DIM + MLA_ROPE_DIM)
    q_nope, q_rope = q[..., :MLA_NOPE_DIM], q[..., MLA_NOPE_DIM:]
    if ang is not None:
        q_rope = rope_axial(q_rope, ang[0][:, None, :], ang[1][:, None, :])
    return q_nope, q_rope


def mla_keys_values(h, p, ang):
    B, T, _ = h.shape
    c_kv = rms_norm(h[..., MLA_Q_LORA:MLA_Q_LORA + MLA_KV_LORA], p['mla_kv_norm'])
    k_rope = h[..., MLA_Q_LORA + MLA_KV_LORA:]
    if ang is not None:
        k_rope = rope_axial(k_rope, ang[0], ang[1])
    k_nope = (c_kv @ p['mla_w_uk']).reshape(B, T, MLA_HEADS, MLA_NOPE_DIM)
    v = (c_kv @ p['mla_w_uv']).reshape(B, T, MLA_HEADS, MLA_V_DIM)
    return k_nope, k_rope, v


def block_attention(q_nope, q_rope, k_nope, k_rope, v):
    B, T, H, _ = q_nope.shape
    nb = T // Q_BLOCK
    scale = (MLA_NOPE_DIM + MLA_ROPE_DIM) ** -0.5
    qn = q_nope.reshape(B, nb, Q_BLOCK, H, MLA_NOPE_DIM).transpose(1, 0, 2, 3, 4)
    qr = q_rope.reshape(B, nb, Q_BLOCK, H, MLA_ROPE_DIM).transpose(1, 0, 2, 3, 4)

    def one_block(args):
        qn_b, qr_b = args
        s = jnp.einsum('bqhd,bkhd->bhqk', qn_b, k_nope) + jnp.einsum('bqhr,bkr->bhqk', qr_b, k_rope)
        prob = jax.nn.softmax(s.astype(jnp.float32) * scale, axis=-1).astype(v.dtype)
        return jnp.einsum('bhqk,bkhd->bqhd', prob, v)

    o = lax.map(one_block, (qn, qr))
    return o.transpose(1, 0, 2, 3, 4).reshape(B, T, H * MLA_V_DIM)


def mla_group(h, hc, p, ang, emit_ctx):
    kc_nope, kc_rope, vc = mla_keys_values(hc, p, None)
    k_nope, k_rope, v = mla_keys_values(h, p, ang)
    q_nope, q_rope = mla_queries(h, p, ang)
    out_lat = block_attention(q_nope, q_rope,
                              jnp.concatenate([kc_nope, k_nope], axis=1),
                              jnp.concatenate([kc_rope, k_rope], axis=1),
                              jnp.concatenate([vc, v], axis=1))
    out_ctx = None
    if emit_ctx:
        qc_nope, qc_rope = mla_queries(hc, p, None)
        out_ctx = block_attention(qc_nope, qc_rope, kc_nope, kc_rope, vc)
    return out_lat, out_ctx


def expert_choice_ffn(u, p):
    B, T, _ = u.shape
    cap = CAPACITY_FACTOR * T // N_EXPERTS
    affinity = jax.nn.softmax((u @ p['router']).astype(jnp.float32), axis=-1)
    gate, idx = lax.top_k(jnp.swapaxes(affinity, 1, 2), cap)
    b_idx = jnp.arange(B)[:, None, None]
    xe = u[b_idx, idx]
    hid = jax.nn.silu(jnp.einsum('becd,edf->becf', xe, p['exp_w_gate'])) * jnp.einsum('becd,edf->becf', xe, p['exp_w_up'])
    ye = jnp.einsum('becf,efd->becd', hid, p['exp_w_down']) * gate[..., None].astype(u.dtype)
    return jnp.zeros_like(u).at[b_idx, idx].add(ye)


def hybrid_layer(x, xc, mod, mod_c, ang, p, alpha, emit_ctx):
    sh1, sc1, g1, sh2, sc2, g2 = jnp.split(mod, 6, axis=-1)
    sh1c, sc1c, g1c, sh2c, sc2c, g2c = jnp.split(mod_c, 6, axis=-1)
    h = modulate(x, sh1, sc1) @ p['w_in']
    hc = modulate(xc, sh1c, sc1c) @ p['w_in']
    rw_lat, rw_ctx = rwkv_group(h[..., :RW_PROJ], hc[..., :RW_PROJ], p, emit_ctx)
    mla_lat, mla_ctx = mla_group(h[..., RW_PROJ:], hc[..., RW_PROJ:], p, ang, emit_ctx)
    mix = jnp.concatenate([rw_lat, mla_lat], axis=-1) @ p['w_out']
    x = layer_norm(alpha * x + g1 * mix, p['ln1_g'], p['ln1_b'])
    x = layer_norm(alpha * x + g2 * expert_choice_ffn(modulate(x, sh2, sc2), p), p['ln2_g'], p['ln2_b'])
    if emit_ctx:
        mix_c = jnp.concatenate([rw_ctx, mla_ctx], axis=-1) @ p['w_out']
        xc = layer_norm(alpha * xc + g1c * mix_c, p['ln1_g'], p['ln1_b'])
        xc = layer_norm(alpha * xc + g2c * expert_choice_ffn(modulate(xc, sh2c, sc2c), p), p['ln2_g'], p['ln2_b'])
    return x, xc


def setup_inputs(seed: int = 0) -> dict:
    key = jax.random.key(seed)
    keys = list(jax.random.split(key, 40))
    nk = lambda: keys.pop()
    f32 = jnp.float32
    normal = lambda shape: jax.random.normal(nk(), shape, f32)
    nrm = lambda shape, fan_in, gain=1.0: normal(shape) * (gain * fan_in ** -0.5)
    L, D = DEPTH, D_MODEL
    beta = (8.0 * DEPTH) ** -0.25
    x = normal((BATCH, SEQ, D))
    c = normal((BATCH, D))
    ctx = normal((BATCH, CTX_LEN, D))
    c_ctx = normal((D,))
    w_ada = nrm((L, D, 6 * D), D, 0.5)
    b_ada = 0.02 * normal((L, 6 * D))
    w_in = nrm((L, D, IN_PROJ), D)
    w_in = w_in.at[:, :, 2 * RW_WIDTH:3 * RW_WIDTH].multiply(beta)
    rwkv_conv = jnp.array([0.25, 1.0, 0.25], f32)[None, :, None] + 0.05 * normal((L, 3, 3 * RW_WIDTH))
    decay_ramp = jnp.tile(jnp.linspace(-6.5, -1.0, RW_HEAD_DIM, dtype=f32), RW_HEADS)
    rwkv_w0 = decay_ramp + 0.1 * normal((L, 2, RW_WIDTH))
    rwkv_w_up = 0.1 * normal((L, 2, RW_DECAY_LORA, RW_WIDTH))
    rwkv_a0 = 0.1 * normal((L, 2, RW_WIDTH))
    rwkv_a_up = nrm((L, 2, RW_AAA_LORA, RW_WIDTH), RW_AAA_LORA, 0.5)
    rwkv_g_up = nrm((L, RW_GATE_LORA, RW_WIDTH), RW_GATE_LORA)
    rwkv_k_k = 0.85 + 0.05 * normal((L, RW_WIDTH))
    rwkv_k_a = 1.0 + 0.05 * normal((L, RW_WIDTH))
    rwkv_r_k = 0.1 * normal((L, RW_HEADS, RW_HEAD_DIM))
    rwkv_gn_g = 1.0 + 0.05 * normal((L, RW_WIDTH))
    rwkv_gn_b = 0.02 * normal((L, RW_WIDTH))
    mla_q_norm = 1.0 + 0.05 * normal((L, MLA_Q_LORA))
    mla_w_uq = nrm((L, MLA_Q_LORA, MLA_HEADS * (MLA_NOPE_DIM + MLA_ROPE_DIM)), MLA_Q_LORA)
    mla_kv_norm = 1.0 + 0.05 * normal((L, MLA_KV_LORA))
    mla_w_uk = nrm((L, MLA_KV_LORA, MLA_HEADS * MLA_NOPE_DIM), MLA_KV_LORA)
    mla_w_uv = nrm((L, MLA_KV_LORA, MLA_HEADS * MLA_V_DIM), MLA_KV_LORA, beta)
    w_out = nrm((L, MIX_WIDTH, D), MIX_WIDTH, beta)
    ln1_g = 1.0 + 0.05 * normal((L, D))
    ln1_b = 0.02 * normal((L, D))
    router = nrm((L, D, N_EXPERTS), D)
    exp_w_gate = nrm((L, N_EXPERTS, D, EXPERT_HIDDEN), D)
    exp_w_up = nrm((L, N_EXPERTS, D, EXPERT_HIDDEN), D)
    exp_w_down = nrm((L, N_EXPERTS, EXPERT_HIDDEN, D), EXPERT_HIDDEN, beta)
    ln2_g = 1.0 + 0.05 * normal((L, D))
    ln2_b = 0.02 * normal((L, D))
    return {'x': x, 'c': c, 'ctx': ctx, 'c_ctx': c_ctx, 'w_ada': w_ada, 'b_ada': b_ada, 'w_in': w_in,
            'rwkv_conv': rwkv_conv, 'rwkv_w0': rwkv_w0, 'rwkv_w_up': rwkv_w_up, 'rwkv_a0': rwkv_a0,
            'rwkv_a_up': rwkv_a_up, 'rwkv_g_up': rwkv_g_up, 'rwkv_k_k': rwkv_k_k, 'rwkv_k_a': rwkv_k_a,
            'rwkv_r_k': rwkv_r_k, 'rwkv_gn_g': rwkv_gn_g, 'rwkv_gn_b': rwkv_gn_b, 'mla_q_norm': mla_q_norm,
            'mla_w_uq': mla_w_uq, 'mla_kv_norm': mla_kv_norm, 'mla_w_uk': mla_w_uk, 'mla_w_uv': mla_w_uv,
            'w_out': w_out, 'ln1_g': ln1_g, 'ln1_b': ln1_b, 'router': router, 'exp_w_gate': exp_w_gate,
            'exp_w_up': exp_w_up, 'exp_w_down': exp_w_down, 'ln2_g': ln2_g, 'ln2_b': ln2_b}


def reference(x, c, ctx, c_ctx, w_ada, b_ada, w_in, rwkv_conv, rwkv_w0, rwkv_w_up, rwkv_a0, rwkv_a_up,
              rwkv_g_up, rwkv_k_k, rwkv_k_a, rwkv_r_k, rwkv_gn_g, rwkv_gn_b, mla_q_norm, mla_w_uq,
              mla_kv_norm, mla_w_uk, mla_w_uv, w_out, ln1_g, ln1_b, router, exp_w_gate, exp_w_up,
              exp_w_down, ln2_g, ln2_b):
    ROWS = x.shape[1] // GRID_W
    ang = axial_angles(ROWS)
    alpha = (2.0 * DEPTH) ** 0.25
    for l in range(DEPTH):
        p = {'w_in': w_in[l], 'rwkv_conv': rwkv_conv[l], 'rwkv_w0': rwkv_w0[l], 'rwkv_w_up': rwkv_w_up[l],
             'rwkv_a0': rwkv_a0[l], 'rwkv_a_up': rwkv_a_up[l], 'rwkv_g_up': rwkv_g_up[l],
             'rwkv_k_k': rwkv_k_k[l], 'rwkv_k_a': rwkv_k_a[l], 'rwkv_r_k': rwkv_r_k[l],
             'rwkv_gn_g': rwkv_gn_g[l], 'rwkv_gn_b': rwkv_gn_b[l], 'mla_q_norm': mla_q_norm[l],
             'mla_w_uq': mla_w_uq[l], 'mla_kv_norm': mla_kv_norm[l], 'mla_w_uk': mla_w_uk[l],
             'mla_w_uv': mla_w_uv[l], 'w_out': w_out[l], 'ln1_g': ln1_g[l], 'ln1_b': ln1_b[l],
             'router': router[l], 'exp_w_gate': exp_w_gate[l], 'exp_w_up': exp_w_up[l],
             'exp_w_down': exp_w_down[l], 'ln2_g': ln2_g[l], 'ln2_b': ln2_b[l]}
        mod = jax.nn.silu(c) @ w_ada[l] + b_ada[l]
        mod_c = jax.nn.silu(c_ctx) @ w_ada[l] + b_ada[l]
        x, ctx = hybrid_layer(x, ctx, mod[:, None, :], mod_c, ang, p, alpha, l < DEPTH - 1)
    return x
```

```python
import numpy as np
import ml_dtypes
from contextlib import ExitStack
import concourse.bass as bass
import concourse.mybir as mybir
from concourse.bass_utils import run_bass_kernel_spmd

F32 = mybir.dt.float32
BF16 = mybir.dt.bfloat16
I32 = mybir.dt.int32
AF = mybir.ActivationFunctionType
ALU = mybir.AluOpType
AX = mybir.AxisListType

EPOCH = 12000
NDMA = 24

D = 1024
TL = 8192
TC = 256
TA = TL + TC
NE = 16
CAP = 1024


class Sync:
    def __init__(self, nc, es):
        self.nc = nc
        self.es = es
        self.eng = {'pe': nc.tensor, 'dve': nc.vector, 'act': nc.scalar,
                    'pool': nc.gpsimd, 'sp': nc.sync}
        self.sem = {}
        self.cnt = {}
        self.cur = {}
        self.known = {e: {} for e in self.eng}
        self.snap = {}
        self.last_w = {}
        self.readers = {}
        self.dma_keys = []
        self.dma_rr = 0
        self.nsem = 0
        for e in self.eng:
            self._new_epoch(e)
        for i in range(NDMA):
            k = ('dma', i)
            self.sem[k] = es.enter_context(nc.semaphore('dq%d' % i))
            self.cnt[k] = 0
            self.dma_keys.append(k)
        self.n_inst = 0
        self.n_wait = 0

    def _new_epoch(self, e):
        idx = self.nsem
        self.nsem += 1
        k = (e, idx)
        self.sem[k] = self.es.enter_context(self.nc.semaphore('s_%s_%d' % (e, idx)))
        self.cnt[k] = 0
        self.cur[e] = k

    def _need(self, e, ticket):
        k, v = ticket
        kn = self.known[e]
        if kn.get(k, 0) >= v:
            return
        self.eng[e].wait_ge(self.sem[k], v)
        self.n_wait += 1
        kn[k] = v
        sn = self.snap.get(ticket)
        if sn:
            for kk, vv in sn.items():
                if kn.get(kk, 0) < vv:
                    kn[kk] = vv

    def _deps(self, e, reads, writes, acc):
        for b in reads:
            t = self.last_w.get(b)
            if t is not None:
                self._need(e, t)
        for b in writes:
            t = self.last_w.get(b)
            if t is not None and not (acc and t[0] == self.cur[e]):
                self._need(e, t)
            for t in self.readers.get(b, ()):
                self._need(e, t)

    def _record(self, ticket, reads, writes):
        for b in reads:
            self.readers.setdefault(b, []).append(ticket)
        for b in writes:
            self.last_w[b] = ticket
            self.readers[b] = []

    def op(self, e, fn, reads=(), writes=(), acc=False):
        if self.cnt[self.cur[e]] >= EPOCH:
            self._new_epoch(e)
        self._deps(e, reads, writes, acc)
        k = self.cur[e]
        inst = fn(self.eng[e])
        inst.then_inc(self.sem[k], 1)
        self.cnt[k] += 1
        t = (k, self.cnt[k])
        self.snap[t] = dict(self.known[e])
        self._record(t, reads, writes)
        self.n_inst += 1
        return t

    def dma(self, e, fn, reads=(), writes=()):
        k = self.dma_keys[self.dma_rr]
        self.dma_rr = (self.dma_rr + 1) % NDMA
        if self.cnt[k] > 0:
            self._need(e, (k, self.cnt[k]))
        self._deps(e, reads, writes, False)
        inst = fn(self.eng[e])
        inst.then_inc(self.sem[k], 16)
        self.cnt[k] += 16
        t = (k, self.cnt[k])
        self.snap[t] = dict(self.known[e])
        self._record(t, reads, writes)
        self.n_inst += 1
        return t

    def wait_all(self, e):
        for k, v in list(self.cnt.items()):
            if v > 0:
                self._need(e, (k, v))

    def barrier(self):
        for e in self.eng:
            self.wait_all(e)
        self.last_w.clear()
        self.readers.clear()


class Ctx:
    pass


_UC = [0]


def _u(n):
    _UC[0] += 1
    return '%s_%d' % (n, _UC[0])


def phase_mod(K):
    nc, S = K.nc, K.S
    with ExitStack() as es:
        sb = lambda n, s, d: es.enter_context(nc.sbuf_tensor(_u(n), s, d))
        ccs = sb("ccs", [128, 8, 2], F32)
        scT = sb("scT", [128, 8, 2], F32)
        wa = [sb("wa%d" % i, [128, 8, 512], F32) for i in range(2)]
        ba = sb("ba", [1, 6144], F32)
        one1 = sb("one1", [1, 1], F32)
        mrow = [sb("mrow%d" % r, [1, 6144], F32) for r in range(2)]
        pm = [es.enter_context(nc.psum_tensor(_u("pm%d" % i), [1, 512], F32)) for i in range(2)]
        S.dma('sp', lambda e: e.dma_start(out=ccs[:], in_=K.ccT), writes=['ccs'])
        S.dma('sp', lambda e: e.dma_start(out=ba[:], in_=K.b_ada), writes=['ba'])
        S.op('dve', lambda e: e.memset(one1[:], 1.0), writes=['one1'])
        S.op('act', lambda e: e.activation(out=scT[:], in_=ccs[:], func=AF.Silu), reads=['ccs'], writes=['scT'])
        for j in range(12):
            w = wa[j % 2]
            wk = 'wa%d' % (j % 2)
            S.dma('sp', lambda e: e.dma_start(out=w[:], in_=K.w_ada[:, j * 512:(j + 1) * 512].rearrange("(k p) n -> p k n", p=128)), writes=[wk])
            for r in range(2):
                if r == 1 and j >= 4:
                    continue
                pk = 'pm%d' % r
                for k in range(8):
                    S.op('pe', lambda e: e.matmul(pm[r][:], lhsT=scT[:, k, r:r + 1], rhs=w[:, k, :], start=(k == 0), stop=False),
                         reads=['scT', wk], writes=[pk], acc=True)
                S.op('pe', lambda e: e.matmul(pm[r][:], lhsT=one1[:], rhs=ba[:, j * 512:(j + 1) * 512], start=False, stop=True),
                     reads=['one1', 'ba'], writes=[pk], acc=True)
                S.op('dve', lambda e: e.tensor_copy(out=mrow[r][:, j * 512:(j + 1) * 512], in_=pm[r][:]), reads=[pk], writes=['mrow%d' % r])
        S.dma('sp', lambda e: e.dma_start(out=K.modd[0:1, :], in_=mrow[0][:]), reads=['mrow0'], writes=['modd'])
        S.dma('sp', lambda e: e.dma_start(out=K.modd[1:2, 0:2048], in_=mrow[1][:, 0:2048]), reads=['mrow1'], writes=['modd'])
        S.barrier()


def phase_inproj(K):
    nc, S = K.nc, K.S
    with ExitStack() as es:
        sb = lambda n, s, d: es.enter_context(nc.sbuf_tensor(_u(n), s, d))
        ps = lambda n, s, d: es.enter_context(nc.psum_tensor(_u(n), s, d))
        wb = sb("wb", [128, 8, 2560], BF16)
        wst = [sb("wst%d" % i, [128, 8, 320], F32) for i in range(2)]
        idt = sb("idt", [128, 128], BF16)
        epst = sb("epst", [128, 1], F32)
        scp = [sb("scp%d" % r, [128, 8], F32) for r in range(2)]
        shp = [sb("shp%d" % r, [128, 8], F32) for r in range(2)]
        xt = [sb("xt%d" % i, [128, 1024], F32) for i in range(2)]
        xn = [sb("xn%d" % i, [128, 1024], BF16) for i in range(2)]
        st = sb("st", [128, 2, 6], F32)
        mv = sb("mv", [128, 2], F32)
        lnv = sb("lnv", [128, 1], F32)
        rstd = sb("rstd", [128, 1], F32)
        xmT = [sb("xmT%d" % i, [128, 8, 512], BF16) for i in range(2)]
        stg = [sb("stg%d" % i, [128, 4, 512], F32) for i in range(2)]
        cst = sb("cst", [64, 512], F32)
        snt = sb("snt", [64, 512], F32)
        kr1 = sb("kr1", [64, 512], F32)
        kr2 = sb("kr2", [64, 512], F32)
        krb = sb("krb", [64, 512], BF16)
        pt = [ps("pt%d" % i, [128, 1024], BF16) for i in range(2)]
        po = [ps("po%d" % i, [128, 512], F32) for i in range(3)]
        pk = [ps("pk%d" % i, [64, 512], F32) for i in range(2)]

        S.dma('sp', lambda e: e.dma_start(out=idt[:], in_=K.ident), writes=['idt'])
        S.op('dve', lambda e: e.memset(epst[:], 1e-5), writes=['epst'])
        for r in range(2):
            S.dma('sp', lambda e: e.dma_start(out=shp[r][:], in_=K.modd[r:r + 1, 0:1024].rearrange("o (k p) -> p (o k)", p=128), allow_slow_non_contiguous=True), reads=['modd'], writes=['shp%d' % r])
            S.dma('sp', lambda e: e.dma_start(out=scp[r][:], in_=K.modd[r:r + 1, 1024:2048].rearrange("o (k p) -> p (o k)", p=128), allow_slow_non_contiguous=True), reads=['modd'], writes=['scp%d' % r])
            S.op('dve', lambda e: e.tensor_scalar_add(out=scp[r][:], in0=scp[r][:], scalar1=1.0), reads=['scp%d' % r], writes=['scp%d' % r])
        for j in range(8):
            w = wst[j % 2]
            wk = 'wst%d' % (j % 2)
            S.dma('sp', lambda e: e.dma_start(out=w[:], in_=K.w_in[:, j * 320:(j + 1) * 320].rearrange("(k p) n -> p k n", p=128)), writes=[wk])
            S.op('pool', lambda e: e.tensor_copy(out=wb[:, :, j * 320:(j + 1) * 320], in_=w[:]), reads=[wk], writes=['wb'])

        groups = [(0, 256, 1)] + [(256 + g * 512, 512, 0) for g in range(16)]
        tile_i = 0
        ev = 0
        for gi, (t0, G, r) in enumerate(groups):
            xm = xmT[gi % 2]
            xmk = 'xmT%d' % (gi % 2)
            if r == 0:
                S.dma('sp', lambda e: e.dma_start(out=cst[:], in_=K.cosT[:, t0 - 256:t0 - 256 + 512]), writes=['cst'])
                S.dma('sp', lambda e: e.dma_start(out=snt[:], in_=K.sinT[:, t0 - 256:t0 - 256 + 512]), writes=['snt'])
            for i in range(G // 128):
                sl = tile_i % 2
                tile_i += 1
                xk, xnk, ptk = 'xt%d' % sl, 'xn%d' % sl, 'pt%d' % sl
                tt = t0 + i * 128
                S.dma('sp', lambda e: e.dma_start(out=xt[sl][:], in_=K.xin[tt:tt + 128, :]), writes=[xk])
                for c in range(2):
                    S.op('dve', lambda e: e.bn_stats(out=st[:, c, :], in_=xt[sl][:, c * 512:(c + 1) * 512]), reads=[xk], writes=['st%d' % c])
                S.op('dve', lambda e: e.bn_aggr(out=mv[:], in_=st[:]), reads=['st0', 'st1'], writes=['mv'])
                S.op('act', lambda e: e.activation(out=lnv[:], in_=mv[:, 1:2], func=AF.Ln, bias=epst[:], scale=1.0), reads=['mv', 'epst'], writes=['lnv'])
                S.op('act', lambda e: e.activation(out=rstd[:], in_=lnv[:], func=AF.Exp, scale=-0.5), reads=['lnv'], writes=['rstd'])
                S.op('dve', lambda e: e.tensor_scalar(out=xn[sl][:], in0=xt[sl][:], scalar1=mv[:, 0:1], scalar2=rstd[:], op0=ALU.subtract, op1=ALU.mult),
                     reads=[xk, 'mv', 'rstd'], writes=[xnk])
                for k in range(8):
                    S.op('pe', lambda e: e.transpose(out=pt[sl][:, k * 128:(k + 1) * 128], in_=xn[sl][:, k * 128:(k + 1) * 128], identity=idt[:]),
                         reads=[xnk, 'idt'], writes=[ptk], acc=True)
                for k in range(8):
                    S.op('act', lambda e: e.activation(out=xm[:, k, i * 128:(i + 1) * 128], in_=pt[sl][:, k * 128:(k + 1) * 128], func=AF.Identity,
                                                       bias=shp[r][:, k:k + 1], scale=scp[r][:, k:k + 1]),
                         reads=[ptk, 'shp%d' % r, 'scp%d' % r], writes=[xmk])
            for cb in range(5):
                sg = stg[cb % 2]
                sgk = 'stg%d' % (cb % 2)
                ncols = 4 if cb < 4 else 3
                for cc in range(ncols):
                    ci = cb * 4 + cc
                    p = po[ev % 3]
                    pkk = 'po%d' % (ev % 3)
                    for k in range(8):
                        S.op('pe', lambda e: e.matmul(p[:, 0:G], lhsT=wb[:, k, ci * 128:(ci + 1) * 128], rhs=xm[:, k, 0:G], start=(k == 0), stop=(k == 7)),
                             reads=['wb', xmk], writes=[pkk], acc=True)
                    if ev % 2 == 0:
                        S.op('dve', lambda e: e.tensor_copy(out=sg[:, cc, 0:G], in_=p[:, 0:G]), reads=[pkk], writes=[sgk])
                    else:
                        S.op('act', lambda e: e.activation(out=sg[:, cc, 0:G], in_=p[:, 0:G], func=AF.Identity), reads=[pkk], writes=[sgk])
                    ev += 1
                r0 = cb * 512
                S.dma('pool', lambda e: e.dma_start(out=K.hT[r0:r0 + ncols * 128, t0:t0 + G].rearrange("(c p) t -> p c t", p=128), in_=sg[:, 0:ncols, 0:G]),
                      reads=[sgk], writes=['hT'])
            for q in range(2):
                for k in range(8):
                    S.op('pe', lambda e: e.matmul(pk[q][:, 0:G], lhsT=wb[:, k, 2432 + q * 64:2432 + (q + 1) * 64], rhs=xm[:, k, 0:G], start=(k == 0), stop=(k == 7)),
                         reads=['wb', xmk], writes=['pk%d' % q], acc=True)
            if r == 0:
                S.op('dve', lambda e: e.tensor_tensor(out=kr1[:], in0=pk[0][:], in1=cst[:], op=ALU.mult), reads=['pk0', 'cst'], writes=['kr1'])
                S.op('dve', lambda e: e.tensor_tensor(out=kr2[:], in0=pk[1][:], in1=snt[:], op=ALU.mult), reads=['pk1', 'snt'], writes=['kr2'])
                S.op('dve', lambda e: e.tensor_tensor(out=krb[:], in0=kr1[:], in1=kr2[:], op=ALU.add), reads=['kr1', 'kr2'], writes=['krb'])
            else:
                S.op('dve', lambda e: e.tensor_copy(out=krb[:, 0:G], in_=pk[0][:, 0:G]), reads=['pk0', 'pk1'], writes=['krb'])
            S.dma('pool', lambda e: e.dma_start(out=K.krT[:, t0:t0 + G], in_=krb[:, 0:G]), reads=['krb'], writes=['krT'])
        S.barrier()


class Rot:
    def __init__(self, bufs, prefix):
        self.bufs = bufs
        self.prefix = prefix
        self.i = 0

    def get(self):
        j = self.i % len(self.bufs)
        self.i += 1
        return self.bufs[j], '%s%d' % (self.prefix, j)


SCALE_ATT = 192.0 ** -0.5


def phase_mlaprep(K):
    nc, S = K.nc, K.S
    with ExitStack() as es:
        sb = lambda n, s, d: es.enter_context(nc.sbuf_tensor(_u(n), s, d))
        ps = lambda n, s, d: es.enter_context(nc.psum_tensor(_u(n), s, d))
        wuq = sb("wuq", [128, 2, 1024], BF16)
        wuk = sb("wuk", [128, 2, 512], BF16)
        wuv = sb("wuv", [128, 2, 512], BF16)
        wst = [sb("mwst%d" % i, [128, 2, 512], F32) for i in range(2)]
        gq = sb("gq", [128, 2], F32)
        gkv = sb("gkv", [128, 2], F32)
        onesf = sb("onesf", [128, 128], F32)
        epst = sb("epst", [128, 1], F32)
        ql = [sb("ql%d" % i, [128, 4, 512], F32) for i in range(2)]
        sq = sb("sq", [128, 4, 512], F32)
        lnt = sb("lnt", [128, 512], F32)
        rs = [sb("rs%d" % i, [128, 512], F32) for i in range(2)]
        nb = [sb("nb%d" % i, [128, 4, 512], BF16) for i in range(2)]
        qst = [sb("qst%d" % i, [128, 4, 512], BF16) for i in range(2)]
        kst = [sb("kst%d" % i, [128, 4, 512], BF16) for i in range(2)]
        qrb = [sb("qrb%d" % i, [64, 4, 512], BF16) for i in range(2)]
        vst = Rot([sb("vst%d" % i, [128, 512], BF16) for i in range(3)], "vst")
        cst = sb("cst", [64, 512], F32)
        snt = sb("snt", [64, 512], F32)
        r1 = sb("r1", [64, 512], F32)
        r2 = sb("r2", [64, 512], F32)
        pp = Rot([ps("mp%d" % i, [128, 512], F32) for i in range(7)], "mp")

        S.op('dve', lambda e: e.memset(epst[:], 1e-6), writes=['epst'])
        S.op('dve', lambda e: e.memset(onesf[:], 1.0), writes=['onesf'])
        S.dma('sp', lambda e: e.dma_start(out=gq[:], in_=K.mla_q_norm.rearrange("o (c p) -> p (o c)", p=128), allow_slow_non_contiguous=True), writes=['gq'])
        S.dma('sp', lambda e: e.dma_start(out=gkv[:], in_=K.mla_kv_norm.rearrange("o (c p) -> p (o c)", p=128), allow_slow_non_contiguous=True), writes=['gkv'])
        wl = 0
        for (src, dst, dk, n) in [(K.w_uq, wuq, 'wuq', 1024), (K.w_uk, wuk, 'wuk', 512), (K.w_uv, wuv, 'wuv', 512)]:
            for j in range(n // 512):
                w = wst[wl % 2]
                wk = 'mwst%d' % (wl % 2)
                wl += 1
                S.dma('sp', lambda e: e.dma_start(out=w[:], in_=src[:, j * 512:(j + 1) * 512].rearrange("(c p) n -> p c n", p=128)), writes=[wk])
                S.op('pool', lambda e: e.tensor_copy(out=dst[:, :, j * 512:(j + 1) * 512], in_=w[:]), reads=[wk], writes=[dk])

        groups = [(0, 256, 1)] + [(256 + g * 512, 512, 0) for g in range(16)]
        ev = [0]

        def evac(out_ap, in_ap, reads, writes):
            if ev[0] % 2 == 0:
                S.op('dve', lambda e: e.tensor_copy(out=out_ap, in_=in_ap), reads=reads, writes=writes)
            else:
                S.op('act', lambda e: e.activation(out=out_ap, in_=in_ap, func=AF.Identity), reads=reads, writes=writes)
            ev[0] += 1

        for gi, (t0, G, r) in enumerate(groups):
            q_ = ql[gi % 2]
            qk = 'ql%d' % (gi % 2)
            n_ = nb[gi % 2]
            nk = 'nb%d' % (gi % 2)
            S.dma('sp', lambda e: e.dma_start(out=q_[:, :, 0:G], in_=K.hT[1920:2432, t0:t0 + G].rearrange("(c p) t -> p c t", p=128)), reads=['hT'], writes=[qk])
            if r == 0:
                S.dma('sp', lambda e: e.dma_start(out=cst[:], in_=K.cosT[:, t0 - 256:t0 - 256 + 512]), writes=['cst'])
                S.dma('sp', lambda e: e.dma_start(out=snt[:], in_=K.sinT[:, t0 - 256:t0 - 256 + 512]), writes=['snt'])
            S.op('act', lambda e: e.activation(out=sq[:, :, 0:G], in_=q_[:, :, 0:G], func=AF.Square), reads=[qk], writes=['sq'])
            for pair in range(2):
                if pair == 0 and r == 1:
                    continue
                p, pk = pp.get()
                for c in range(2):
                    S.op('pe', lambda e: e.matmul(p[:, 0:G], lhsT=onesf[:], rhs=sq[:, pair * 2 + c, 0:G], start=(c == 0), stop=(c == 1)),
                         reads=['onesf', 'sq'], writes=[pk], acc=True)
                S.op('act', lambda e: e.activation(out=lnt[:, 0:G], in_=p[:, 0:G], func=AF.Ln, bias=epst[:], scale=1.0 / 256.0), reads=[pk, 'epst'], writes=['lnt'])
                S.op('act', lambda e: e.activation(out=rs[pair][:, 0:G], in_=lnt[:, 0:G], func=AF.Exp, scale=-0.5), reads=['lnt'], writes=['rs%d' % pair])
                g_ = gq if pair == 0 else gkv
                for c in range(2):
                    S.op('dve', lambda e: e.scalar_tensor_tensor(out=n_[:, pair * 2 + c, 0:G], in0=q_[:, pair * 2 + c, 0:G], scalar=g_[:, c:c + 1], in1=rs[pair][:, 0:G],
                                                                 op0=ALU.mult, op1=ALU.mult),
                         reads=[qk, 'rs%d' % pair, 'gq', 'gkv'], writes=[nk])
            if r == 0:
                tq = t0 - 256
                qs = qst[gi % 2]
                qsk = 'qst%d' % (gi % 2)
                qr_ = qrb[gi % 2]
                qrk = 'qrb%d' % (gi % 2)
                for h in range(4):
                    p, pk = pp.get()
                    for c in range(2):
                        S.op('pe', lambda e: e.matmul(p[:, 0:G], lhsT=wuq[:, c, h * 256:h * 256 + 128], rhs=n_[:, c, 0:G], start=(c == 0), stop=(c == 1)),
                             reads=['wuq', nk], writes=[pk], acc=True)
                    evac(qs[:, h, 0:G], p[:, 0:G], [pk], [qsk])
                    p1, pk1 = pp.get()
                    p2, pk2 = pp.get()
                    for c in range(2):
                        S.op('pe', lambda e: e.matmul(p1[0:64, 0:G], lhsT=wuq[:, c, h * 256 + 128:h * 256 + 192], rhs=n_[:, c, 0:G], start=(c == 0), stop=(c == 1)),
                             reads=['wuq', nk], writes=[pk1], acc=True)
                    for c in range(2):
                        S.op('pe', lambda e: e.matmul(p2[0:64, 0:G], lhsT=wuq[:, c, h * 256 + 192:h * 256 + 256], rhs=n_[:, c, 0:G], start=(c == 0), stop=(c == 1)),
                             reads=['wuq', nk], writes=[pk2], acc=True)
                    S.op('dve', lambda e: e.tensor_tensor(out=r1[:], in0=p1[0:64, :], in1=cst[:], op=ALU.mult), reads=[pk1, 'cst'], writes=['r1'])
                    S.op('dve', lambda e: e.tensor_tensor(out=r2[:], in0=p2[0:64, :], in1=snt[:], op=ALU.mult), reads=[pk2, 'snt'], writes=['r2'])
                    S.op('dve', lambda e: e.tensor_tensor(out=qr_[:, h, :], in0=r1[:], in1=r2[:], op=ALU.add), reads=['r1', 'r2'], writes=[qrk])
                S.dma('pool', lambda e: e.dma_start(out=K.qnT[:, tq:tq + G].rearrange("(h p) t -> p h t", p=128), in_=qs[:, :, 0:G]), reads=[qsk], writes=['qnT'])
                S.dma('pool', lambda e: e.dma_start(out=K.qrT[:, tq:tq + G].rearrange("(h p) t -> p h t", p=64), in_=qr_[:, :, 0:G]), reads=[qrk], writes=['qrT'])
            ks = kst[gi % 2]
            ksk = 'kst%d' % (gi % 2)
            for h in range(4):
                p, pk = pp.get()
                for c in range(2):
                    S.op('pe', lambda e: e.matmul(p[:, 0:G], lhsT=wuk[:, c, h * 128:(h + 1) * 128], rhs=n_[:, 2 + c, 0:G], start=(c == 0), stop=(c == 1)),
                         reads=['wuk', nk], writes=[pk], acc=True)
                evac(ks[:, h, 0:G], p[:, 0:G], [pk], [ksk])
            S.dma('pool', lambda e: e.dma_start(out=K.knT[:, t0:t0 + G].rearrange("(h p) t -> p h t", p=128), in_=ks[:, :, 0:G]), reads=[ksk], writes=['knT'])
            for i in range(G // 128):
                p, pk = pp.get()
                for c in range(2):
                    S.op('pe', lambda e: e.matmul(p[:, :], lhsT=n_[:, 2 + c, i * 128:(i + 1) * 128], rhs=wuv[:, c, :], start=(c == 0), stop=(c == 1)),
                         reads=['wuv', nk], writes=[pk], acc=True)
                v_, vk = vst.get()
                evac(v_[:], p[:], [pk], [vk])
                S.dma('pool', lambda e: e.dma_start(out=K.vtok[t0 + i * 128:t0 + (i + 1) * 128, :], in_=v_[:]), reads=[vk], writes=['vtok'])
        S.barrier()


def phase_attn(K, heads=(0, 1, 2, 3), nqt=16):
    nc, S = K.nc, K.S
    NKT = TA // 128
    with ExitStack() as es:
        sb = lambda n, s, d: es.enter_context(nc.sbuf_tensor(_u(n), s, d))
        ps = lambda n, s, d: es.enter_context(nc.psum_tensor(_u(n), s, d))
        krs = sb("krs", [64, TA], BF16)
        kn = [sb("kn%d" % i, [128, TA], BF16) for i in range(2)]
        vh = [sb("vh%d" % i, [128, NKT, 128], BF16) for i in range(2)]
        qn = [sb("qn%d" % i, [128, 512], BF16) for i in range(2)]
        qr = [sb("qr%d" % i, [64, 512], BF16) for i in range(2)]
        onesb = sb("onesb", [128, 128], BF16)
        pT = Rot([sb("pT%d" % i, [128, 512], BF16) for i in range(3)], "pT")
        rl = sb("rl", [128, 512], F32)
        ob = [sb("ob%d" % i, [128, 512], BF16) for i in range(2)]
        psc = Rot([ps("psc%d" % i, [128, 512], F32) for i in range(3)], "psc")
        pO = [ps("pO%d" % i, [128, 512], F32) for i in range(2)]
        pL = [ps("pL%d" % i, [128, 512], F32) for i in range(2)]

        S.op('dve', lambda e: e.memset(onesb[:], 1.0), writes=['onesb'])
        S.dma('sp', lambda e: e.dma_start(out=krs[:], in_=K.krT), reads=['krT'], writes=['krs'])
        qi = 0
        for hi, h in enumerate(heads):
            k_ = kn[hi % 2]
            kk = 'kn%d' % (hi % 2)
            v_ = vh[hi % 2]
            vk = 'vh%d' % (hi % 2)
            S.dma('sp', lambda e: e.dma_start(out=k_[:], in_=K.knT[h * 128:(h + 1) * 128, :]), reads=['knT'], writes=[kk])
            S.dma('sp', lambda e: e.dma_start(out=v_[:], in_=K.vtok[:, h * 128:(h + 1) * 128].rearrange("(kt p) d -> p kt d", p=128)), reads=['vtok'], writes=[vk])
            for qt in range(nqt):
                sl = qi % 2
                qi += 1
                qnk, qrk = 'qn%d' % sl, 'qr%d' % sl
                S.dma('sp', lambda e: e.dma_start(out=qn[sl][:], in_=K.qnT[h * 128:(h + 1) * 128, qt * 512:(qt + 1) * 512]), reads=['qnT'], writes=[qnk])
                S.dma('sp', lambda e: e.dma_start(out=qr[sl][:], in_=K.qrT[h * 64:(h + 1) * 64, qt * 512:(qt + 1) * 512]), reads=['qrT'], writes=[qrk])
                Ok, Lk = 'pO%d' % sl, 'pL%d' % sl
                pend = []

                def scores(kt):
                    p, pk = psc.get()
                    S.op('pe', lambda e: e.matmul(p[:], lhsT=k_[:, kt * 128:(kt + 1) * 128], rhs=qn[sl][:], start=True, stop=False),
                         reads=[kk, qnk], writes=[pk], acc=True)
                    S.op('pe', lambda e: e.matmul(p[:], lhsT=krs[:, kt * 128:(kt + 1) * 128], rhs=qr[sl][:], start=False, stop=True),
                         reads=['krs', qrk], writes=[pk], acc=True)
                    t_, tk = pT.get()
                    S.op('act', lambda e: e.activation(out=t_[:], in_=p[:], func=AF.Exp, scale=SCALE_ATT), reads=[pk], writes=[tk])
                    pend.append((kt, t_, tk))

                def pv():
                    kt, t_, tk = pend.pop(0)
                    S.op('pe', lambda e: e.matmul(pO[sl][:], lhsT=v_[:, kt, :], rhs=t_[:], start=(kt == 0), stop=(kt == NKT - 1)),
                         reads=[vk, tk], writes=[Ok], acc=True)
                    S.op('pe', lambda e: e.matmul(pL[sl][:], lhsT=onesb[:], rhs=t_[:], start=(kt == 0), stop=(kt == NKT - 1)),
                         reads=['onesb', tk], writes=[Lk], acc=True)

                scores(0)
                scores(1)
                for kt in range(NKT):
                    pv()
                    if kt + 2 < NKT:
                        scores(kt + 2)
                S.op('dve', lambda e: e.reciprocal(out=rl[:], in_=pL[sl][:]), reads=[Lk], writes=['rl'])
                S.op('dve', lambda e: e.tensor_tensor(out=ob[sl][:], in0=pO[sl][:], in1=rl[:], op=ALU.mult), reads=[Ok, 'rl'], writes=['ob%d' % sl])
                S.dma('pool', lambda e: e.dma_start(out=K.mixT[512 + h * 128:512 + (h + 1) * 128, qt * 512:(qt + 1) * 512], in_=ob[sl][:]), reads=['ob%d' % sl], writes=['mixT'])
        S.barrier()


class Rot:
    def __init__(self, bufs, prefix):
        self.bufs = bufs
        self.prefix = prefix
        self.i = 0

    def get(self):
        j = self.i % len(self.bufs)
        self.i += 1
        return self.bufs[j], '%s%d' % (self.prefix, j)


SCALE_ATT = 192.0 ** -0.5


def phase_mlaprep(K):
    nc, S = K.nc, K.S
    with ExitStack() as es:
        sb = lambda n, s, d: es.enter_context(nc.sbuf_tensor(_u(n), s, d))
        ps = lambda n, s, d: es.enter_context(nc.psum_tensor(_u(n), s, d))
        wuq = sb("wuq", [128, 2, 1024], BF16)
        wuk = sb("wuk", [128, 2, 512], BF16)
        wuv = sb("wuv", [128, 2, 512], BF16)
        wst = [sb("mwst%d" % i, [128, 2, 512], F32) for i in range(2)]
        gq = sb("gq", [128, 2], F32)
        gkv = sb("gkv", [128, 2], F32)
        onesf = sb("onesf", [128, 128], F32)
        epst = sb("epst", [128, 1], F32)
        ql = [sb("ql%d" % i, [128, 4, 512], F32) for i in range(2)]
        sq = sb("sq", [128, 4, 512], F32)
        lnt = sb("lnt", [128, 512], F32)
        rs = [sb("rs%d" % i, [128, 512], F32) for i in range(2)]
        nb = [sb("nb%d" % i, [128, 4, 512], BF16) for i in range(2)]
        qst = [sb("qst%d" % i, [128, 4, 512], BF16) for i in range(2)]
        kst = [sb("kst%d" % i, [128, 4, 512], BF16) for i in range(2)]
        qrb = [sb("qrb%d" % i, [64, 4, 512], BF16) for i in range(2)]
        vst = Rot([sb("vst%d" % i, [128, 512], BF16) for i in range(3)], "vst")
        cst = sb("cst", [64, 512], F32)
        snt = sb("snt", [64, 512], F32)
        r1 = sb("r1", [64, 512], F32)
        r2 = sb("r2", [64, 512], F32)
        pp = Rot([ps("mp%d" % i, [128, 512], F32) for i in range(7)], "mp")

        S.op('dve', lambda e: e.memset(epst[:], 1e-6), writes=['epst'])
        S.op('dve', lambda e: e.memset(onesf[:], 1.0), writes=['onesf'])
        S.dma('sp', lambda e: e.dma_start(out=gq[:], in_=K.mla_q_norm.rearrange("o (c p) -> p (o c)", p=128), allow_slow_non_contiguous=True), writes=['gq'])
        S.dma('sp', lambda e: e.dma_start(out=gkv[:], in_=K.mla_kv_norm.rearrange("o (c p) -> p (o c)", p=128), allow_slow_non_contiguous=True), writes=['gkv'])
        wl = 0
        for (src, dst, dk, n) in [(K.w_uq, wuq, 'wuq', 1024), (K.w_uk, wuk, 'wuk', 512), (K.w_uv, wuv, 'wuv', 512)]:
            for j in range(n // 512):
                w = wst[wl % 2]
                wk = 'mwst%d' % (wl % 2)
                wl += 1
                S.dma('sp', lambda e: e.dma_start(out=w[:], in_=src[:, j * 512:(j + 1) * 512].rearrange("(c p) n -> p c n", p=128)), writes=[wk])
                S.op('pool', lambda e: e.tensor_copy(out=dst[:, :, j * 512:(j + 1) * 512], in_=w[:]), reads=[wk], writes=[dk])

        groups = [(0, 256, 1)] + [(256 + g * 512, 512, 0) for g in range(16)]
        ev = [0]

        def evac(out_ap, in_ap, reads, writes):
            if ev[0] % 2 == 0:
                S.op('dve', lambda e: e.tensor_copy(out=out_ap, in_=in_ap), reads=reads, writes=writes)
            else:
                S.op('act', lambda e: e.activation(out=out_ap, in_=in_ap, func=AF.Identity), reads=reads, writes=writes)
            ev[0] += 1

        for gi, (t0, G, r) in enumerate(groups):
            q_ = ql[gi % 2]
            qk = 'ql%d' % (gi % 2)
            n_ = nb[gi % 2]
            nk = 'nb%d' % (gi % 2)
            S.dma('sp', lambda e: e.dma_start(out=q_[:, :, 0:G], in_=K.hT[1920:2432, t0:t0 + G].rearrange("(c p) t -> p c t", p=128)), reads=['hT'], writes=[qk])
            if r == 0:
                S.dma('sp', lambda e: e.dma_start(out=cst[:], in_=K.cosT[:, t0 - 256:t0 - 256 + 512]), writes=['cst'])
                S.dma('sp', lambda e: e.dma_start(out=snt[:], in_=K.sinT[:, t0 - 256:t0 - 256 + 512]), writes=['snt'])
            S.op('act', lambda e: e.activation(out=sq[:, :, 0:G], in_=q_[:, :, 0:G], func=AF.Square), reads=[qk], writes=['sq'])
            for pair in range(2):
                if pair == 0 and r == 1:
                    continue
                p, pk = pp.get()
                for c in range(2):
                    S.op('pe', lambda e: e.matmul(p[:, 0:G], lhsT=onesf[:], rhs=sq[:, pair * 2 + c, 0:G], start=(c == 0), stop=(c == 1)),
                         reads=['onesf', 'sq'], writes=[pk], acc=True)
                S.op('act', lambda e: e.activation(out=lnt[:, 0:G], in_=p[:, 0:G], func=AF.Ln, bias=epst[:], scale=1.0 / 256.0), reads=[pk, 'epst'], writes=['lnt'])
                S.op('act', lambda e: e.activation(out=rs[pair][:, 0:G], in_=lnt[:, 0:G], func=AF.Exp, scale=-0.5), reads=['lnt'], writes=['rs%d' % pair])
                g_ = gq if pair == 0 else gkv
                for c in range(2):
                    S.op('dve', lambda e: e.scalar_tensor_tensor(out=n_[:, pair * 2 + c, 0:G], in0=q_[:, pair * 2 + c, 0:G], scalar=g_[:, c:c + 1], in1=rs[pair][:, 0:G],
                                                                 op0=ALU.mult, op1=ALU.mult),
                         reads=[qk, 'rs%d' % pair, 'gq', 'gkv'], writes=[nk])
            if r == 0:
                tq = t0 - 256
                qs = qst[gi % 2]
                qsk = 'qst%d' % (gi % 2)
                qr_ = qrb[gi % 2]
                qrk = 'qrb%d' % (gi % 2)
                for h in range(4):
                    p, pk = pp.get()
                    for c in range(2):
                        S.op('pe', lambda e: e.matmul(p[:, 0:G], lhsT=wuq[:, c, h * 256:h * 256 + 128], rhs=n_[:, c, 0:G], start=(c == 0), stop=(c == 1)),
                             reads=['wuq', nk], writes=[pk], acc=True)
                    evac(qs[:, h, 0:G], p[:, 0:G], [pk], [qsk])
                    p1, pk1 = pp.get()
                    p2, pk2 = pp.get()
                    for c in range(2):
                        S.op('pe', lambda e: e.matmul(p1[0:64, 0:G], lhsT=wuq[:, c, h * 256 + 128:h * 256 + 192], rhs=n_[:, c, 0:G], start=(c == 0), stop=(c == 1)),
                             reads=['wuq', nk], writes=[pk1], acc=True)
                    for c in range(2):
                        S.op('pe', lambda e: e.matmul(p2[0:64, 0:G], lhsT=wuq[:, c, h * 256 + 192:h * 256 + 256], rhs=n_[:, c, 0:G], start=(c == 0), stop=(c == 1)),
                             reads=['wuq', nk], writes=[pk2], acc=True)
                    S.op('dve', lambda e: e.tensor_tensor(out=r1[:], in0=p1[0:64, :], in1=cst[:], op=ALU.mult), reads=[pk1, 'cst'], writes=['r1'])
                    S.op('dve', lambda e: e.tensor_tensor(out=r2[:], in0=p2[0:64, :], in1=snt[:], op=ALU.mult), reads=[pk2, 'snt'], writes=['r2'])
                    S.op('dve', lambda e: e.tensor_tensor(out=qr_[:, h, :], in0=r1[:], in1=r2[:], op=ALU.add), reads=['r1', 'r2'], writes=[qrk])
                S.dma('pool', lambda e: e.dma_start(out=K.qnT[:, tq:tq + G].rearrange("(h p) t -> p h t", p=128), in_=qs[:, :, 0:G]), reads=[qsk], writes=['qnT'])
                S.dma('pool', lambda e: e.dma_start(out=K.qrT[:, tq:tq + G].rearrange("(h p) t -> p h t", p=64), in_=qr_[:, :, 0:G]), reads=[qrk], writes=['qrT'])
            ks = kst[gi % 2]
            ksk = 'kst%d' % (gi % 2)
            for h in range(4):
                p, pk = pp.get()
                for c in range(2):
                    S.op('pe', lambda e: e.matmul(p[:, 0:G], lhsT=wuk[:, c, h * 128:(h + 1) * 128], rhs=n_[:, 2 + c, 0:G], start=(c == 0), stop=(c == 1)),
                         reads=['wuk', nk], writes=[pk], acc=True)
                evac(ks[:, h, 0:G], p[:, 0:G], [pk], [ksk])
            S.dma('pool', lambda e: e.dma_start(out=K.knT[:, t0:t0 + G].rearrange("(h p) t -> p h t", p=128), in_=ks[:, :, 0:G]), reads=[ksk], writes=['knT'])
            for i in range(G // 128):
                p, pk = pp.get()
                for c in range(2):
                    S.op('pe', lambda e: e.matmul(p[:, :], lhsT=n_[:, 2 + c, i * 128:(i + 1) * 128], rhs=wuv[:, c, :], start=(c == 0), stop=(c == 1)),
                         reads=['wuv', nk], writes=[pk], acc=True)
                v_, vk = vst.get()
                evac(v_[:], p[:], [pk], [vk])
                S.dma('pool', lambda e: e.dma_start(out=K.vtok[t0 + i * 128:t0 + (i + 1) * 128, :], in_=v_[:]), reads=[vk], writes=['vtok'])
        S.barrier()


def phase_attn(K, heads=(0, 1, 2, 3), nqt=16):
    nc, S = K.nc, K.S
    NKT = TA // 128
    with ExitStack() as es:
        sb = lambda n, s, d: es.enter_context(nc.sbuf_tensor(_u(n), s, d))
        ps = lambda n, s, d: es.enter_context(nc.psum_tensor(_u(n), s, d))
        krs = sb("krs", [64, TA], BF16)
        kn = [sb("kn%d" % i, [128, TA], BF16) for i in range(2)]
        vh = [sb("vh%d" % i, [128, NKT, 128], BF16) for i in range(2)]
        qn = [sb("qn%d" % i, [128, 512], BF16) for i in range(2)]
        qr = [sb("qr%d" % i, [64, 512], BF16) for i in range(2)]
        onesb = sb("onesb", [128, 128], BF16)
        pT = Rot([sb("pT%d" % i, [128, 512], BF16) for i in range(3)], "pT")
        rl = sb("rl", [128, 512], F32)
        ob = [sb("ob%d" % i, [128, 512], BF16) for i in range(2)]
        psc = Rot([ps("psc%d" % i, [128, 512], F32) for i in range(3)], "psc")
        pO = [ps("pO%d" % i, [128, 512], F32) for i in range(2)]
        pL = [ps("pL%d" % i, [128, 512], F32) for i in range(2)]

        S.op('dve', lambda e: e.memset(onesb[:], 1.0), writes=['onesb'])
        S.dma('sp', lambda e: e.dma_start(out=krs[:], in_=K.krT), reads=['krT'], writes=['krs'])
        qi = 0
        for hi, h in enumerate(heads):
            k_ = kn[hi % 2]
            kk = 'kn%d' % (hi % 2)
            v_ = vh[hi % 2]
            vk = 'vh%d' % (hi % 2)
            S.dma('sp', lambda e: e.dma_start(out=k_[:], in_=K.knT[h * 128:(h + 1) * 128, :]), reads=['knT'], writes=[kk])
            S.dma('sp', lambda e: e.dma_start(out=v_[:], in_=K.vtok[:, h * 128:(h + 1) * 128].rearrange("(kt p) d -> p kt d", p=128)), reads=['vtok'], writes=[vk])
            for qt in range(nqt):
                sl = qi % 2
                qi += 1
                qnk, qrk = 'qn%d' % sl, 'qr%d' % sl
                S.dma('sp', lambda e: e.dma_start(out=qn[sl][:], in_=K.qnT[h * 128:(h + 1) * 128, qt * 512:(qt + 1) * 512]), reads=['qnT'], writes=[qnk])
                S.dma('sp', lambda e: e.dma_start(out=qr[sl][:], in_=K.qrT[h * 64:(h + 1) * 64, qt * 512:(qt + 1) * 512]), reads=['qrT'], writes=[qrk])
                Ok, Lk = 'pO%d' % sl, 'pL%d' % sl
                pend = []

                def scores(kt):
                    p, pk = psc.get()
                    S.op('pe', lambda e: e.matmul(p[:], lhsT=k_[:, kt * 128:(kt + 1) * 128], rhs=qn[sl][:], start=True, stop=False),
                         reads=[kk, qnk], writes=[pk], acc=True)
                    S.op('pe', lambda e: e.matmul(p[:], lhsT=krs[:, kt * 128:(kt + 1) * 128], rhs=qr[sl][:], start=False, stop=True),
                         reads=['krs', qrk], writes=[pk], acc=True)
                    t_, tk = pT.get()
                    S.op('act', lambda e: e.activation(out=t_[:], in_=p[:], func=AF.Exp, scale=SCALE_ATT), reads=[pk], writes=[tk])
                    pend.append((kt, t_, tk))

                def pv():
                    kt, t_, tk = pend.pop(0)
                    S.op('pe', lambda e: e.matmul(pO[sl][:], lhsT=v_[:, kt, :], rhs=t_[:], start=(kt == 0), stop=(kt == NKT - 1)),
                         reads=[vk, tk], writes=[Ok], acc=True)
                    S.op('pe', lambda e: e.matmul(pL[sl][:], lhsT=onesb[:], rhs=t_[:], start=(kt == 0), stop=(kt == NKT - 1)),
                         reads=['onesb', tk], writes=[Lk], acc=True)

                scores(0)
                scores(1)
                for kt in range(NKT):
                    pv()
                    if kt + 2 < NKT:
                        scores(kt + 2)
                S.op('dve', lambda e: e.reciprocal(out=rl[:], in_=pL[sl][:]), reads=[Lk], writes=['rl'])
                S.op('dve', lambda e: e.tensor_tensor(out=ob[sl][:], in0=pO[sl][:], in1=rl[:], op=ALU.mult), reads=[Ok, 'rl'], writes=['ob%d' % sl])
                S.dma('pool', lambda e: e.dma_start(out=K.mixT[512 + h * 128:512 + (h + 1) * 128, qt * 512:(qt + 1) * 512], in_=ob[sl][:]), reads=['ob%d' % sl], writes=['mixT'])
        S.barrier()


def build_program(debug=(), phases=None, dbg_in=()):
    nc = bass.Bass("TRN2", target_bir_lowering=False)
    K = Ctx()
    K.nc = nc
    di = lambda n, s, d: nc.dram_tensor(n, s, d, kind="ExternalInput").ap()
    K.xin = di("xin", [TA, D], F32)
    K.ccT = di("ccT", [128, 8, 2], F32)
    K.w_ada = di("w_ada", [D, 6 * D], F32)
    K.b_ada = di("b_ada", [1, 6 * D], F32)
    K.w_in = di("w_in", [D, 2560], F32)
    K.cosT = di("cosT", [64, TL], F32)
    K.sinT = di("sinT", [64, TL], F32)
    K.ident = di("ident", [128, 128], BF16)
    K.mla_q_norm = di("mla_q_norm", [1, 256], F32)
    K.mla_kv_norm = di("mla_kv_norm", [1, 256], F32)
    K.w_uq = di("w_uq", [256, 1024], F32)
    K.w_uk = di("w_uk", [256, 512], F32)
    K.w_uv = di("w_uv", [256, 512], F32)

    def scratch(n, s, d):
        if n in dbg_in:
            return nc.dram_tensor(n, s, d, kind="ExternalInput").ap()
        kind = "ExternalOutput" if n in debug else "Internal"
        return nc.dram_tensor(n, s, d, kind=kind).ap()
    K.modd = scratch("modd", [2, 6 * D], F32)
    K.hT = scratch("hT", [2432, TA], F32)
    K.krT = scratch("krT", [64, TA], BF16)
    K.qnT = scratch("qnT", [512, TL], BF16)
    K.qrT = scratch("qrT", [256, TL], BF16)
    K.knT = scratch("knT", [512, TA], BF16)
    K.vtok = scratch("vtok", [TA, 512], BF16)
    K.mixT = scratch("mixT", [1024, TL], BF16)
    K.out = nc.dram_tensor("out", [TL, D], F32, kind="ExternalOutput").ap()
    allp = ['mod', 'inproj', 'mlaprep', 'attn']
    if phases is None:
        phases = allp
    with ExitStack() as es:
        S = Sync(nc, es)
        K.S = S
        if 'mod' in phases:
            phase_mod(K)
        if 'inproj' in phases:
            phase_inproj(K)
        if 'mlaprep' in phases:
            phase_mlaprep(K)
        if 'attn' in phases:
            phase_attn(K)
        if 'attn1' in phases:
            phase_attn(K, heads=(1,), nqt=2)
        if 'mix' in phases:
            phase_mix(K)
        if 'moe' in phases:
            phase_moe(K, **opts.get('moe', {}))
        if 'final' in phases:
            phase_final(K)
        S.wait_all('sp')
        print("instructions", S.n_inst, "waits", S.n_wait, "sems", S.nsem + NDMA)
    return nc


_SWAP = np.concatenate([np.arange(16, 32), np.arange(0, 16), np.arange(48, 64), np.arange(32, 48)])


def rope_tables():
    half = 32
    inv_freq = (10000.0 ** (-np.arange(0, half, 2, dtype=np.float32) / half)).astype(np.float32)
    t = np.arange(TL)
    rr = (t // 64).astype(np.float32)[None, :]
    cc = (t % 64).astype(np.float32)[None, :]
    ang_r = (inv_freq[:, None] * rr).astype(np.float32)
    ang_c = (inv_freq[:, None] * cc).astype(np.float32)
    cosT = np.concatenate([np.cos(ang_r), np.cos(ang_r), np.cos(ang_c), np.cos(ang_c)], 0).astype(np.float32)
    sinT = np.concatenate([-np.sin(ang_r), np.sin(ang_r), -np.sin(ang_c), np.sin(ang_c)], 0).astype(np.float32)
    return np.ascontiguousarray(cosT), np.ascontiguousarray(sinT)


def make_in_maps(inputs, batches):
    f = lambda a: np.ascontiguousarray(np.asarray(a, dtype=np.float32))
    w_in = f(inputs['w_in'][0])
    w_in_ext = np.concatenate([w_in, w_in[:, 2432:2496][:, _SWAP]], axis=1)
    cosT, sinT = rope_tables()
    wuq = f(inputs['mla_w_uq'][0])
    cols = []
    for h in range(4):
        nope = wuq[:, h * 192:h * 192 + 128]
        rope = wuq[:, h * 192 + 128:h * 192 + 192]
        cols += [nope, rope, rope[:, _SWAP]]
    wuq_ext = np.ascontiguousarray(np.concatenate(cols, axis=1))
    shared = {
        'w_ada': f(inputs['w_ada'][0]), 'b_ada': f(inputs['b_ada']), 'w_in': np.ascontiguousarray(w_in_ext),
        'cosT': cosT, 'sinT': sinT, 'ident': np.eye(128).astype(ml_dtypes.bfloat16),
        'mla_q_norm': f(inputs['mla_q_norm']), 'mla_kv_norm': f(inputs['mla_kv_norm']),
        'w_uq': wuq_ext, 'w_uk': f(inputs['mla_w_uk'][0]), 'w_uv': f(inputs['mla_w_uv'][0]),
    }
    maps = []
    for b in batches:
        m = dict(shared)
        m['xin'] = np.ascontiguousarray(np.concatenate([inputs['ctx'][b], inputs['x'][b]], axis=0).astype(np.float32))
        cc = np.stack([inputs['c'][b], inputs['c_ctx']], axis=-1).astype(np.float32)
        m['ccT'] = np.ascontiguousarray(cc.reshape(8, 128, 2).transpose(1, 0, 2))
        maps.append(m)
    return maps


def kernel(**inputs):
    nc = build_program()
    maps = make_in_maps(inputs, [0, 1, 2, 3, 0, 1, 2, 3])
    res = run_bass_kernel_spmd(nc, maps, core_ids=list(range(8)))
    out = np.stack([res.results[b]['out'] for b in range(4)], axis=0)
    return out.astype(np.float32)


F32R = mybir.dt.float32r
LDS = -0.6065306597126334


def phase_rwprep(K):
    nc, S = K.nc, K.S
    with ExitStack() as es:
        sb = lambda n, s, d: es.enter_context(nc.sbuf_tensor(_u(n), s, d))
        ps = lambda n, s, d: es.enter_context(nc.psum_tensor(_u(n), s, d))
        G = 256
        cw = sb("cw", [128, 3, 12], F32)
        kkv = sb("kkv", [128, 4], F32)
        kav = sb("kav", [128, 4], F32)
        omka = sb("omka", [128, 4], F32)
        rkv = sb("rkv", [128, 4], F32)
        a0v = sb("a0v", [128, 2, 4], F32)
        w0r = sb("w0r", [1, 2, 512], F32)
        ones1 = sb("ones1", [1, 128], F32)
        wup = sb("wup", [64, 2, 512], F32)
        aup = sb("aup", [64, 2, 512], F32)
        gup = sb("gup", [128, 512], F32)
        bones = sb("bones", [128, 128], F32)
        idf = sb("idf", [128, 128], F32)
        eps12 = sb("eps12", [128, 1], F32)
        hr = [sb("hr%d" % i, [128, 12, G + 2], F32) for i in range(2)]
        lo = [sb("lo%d" % i, [64, 4, G], F32) for i in range(2)]
        gd = [sb("gd%d" % i, [128, G], F32) for i in range(2)]
        cv = sb("cv", [128, 12, G], F32)
        tw = sb("tw", [64, 2, G], F32)
        sg = sb("sg", [128, G], F32)
        kq = sb("kq", [128, 4, G], F32)
        sq = sb("sq", [128, 4, G], F32)
        lnt = sb("lnt", [128, 4, G], F32)
        kk_ = sb("kk_", [128, 4, G], F32)
        av = sb("av", [128, 4, G], F32)
        tt = sb("tt", [128, 4, G], F32)
        kd = [sb("kd%d" % i, [128, 4, G], F32) for i in range(2)]
        bb = sb("bb", [128, 4, G], F32)
        ld = Rot([sb("ld%d" % i, [128, 512], F32) for i in range(2)], "ld")
        vt = Rot([sb("vt%d" % i, [128, 512], F32) for i in range(2)], "vt")
        gg = sb("gg", [128, 4, G], F32)
        bc = sb("bc", [128, 4, G], F32)
        bon = sb("bon", [128, 4, G], F32)
        pp = Rot([ps("rp%d" % i, [128, 512], F32) for i in range(7)], "rp")

        ld1 = lambda dst, src, key: S.dma('sp', lambda e: e.dma_start(out=dst, in_=src, allow_slow_non_contiguous=True), writes=[key])
        ld1(cw[:], K.rwkv_conv.rearrange("t (c p) -> p t c", p=128), 'cw')
        ld1(kkv[:], K.rwkv_k_k.rearrange("o (c p) -> p (o c)", p=128), 'kkv')
        ld1(kav[:], K.rwkv_k_a.rearrange("o (c p) -> p (o c)", p=128), 'kav')
        ld1(rkv[:], K.rwkv_r_k.rearrange("o (c p) -> p (o c)", p=128), 'rkv')
        ld1(a0v[:], K.rwkv_a0.rearrange("d (c p) -> p d c", p=128), 'a0v')
        ld1(w0r[:], K.rwkv_w0.rearrange("(o d) n -> o d n", o=1), 'w0r')
        ld1(wup[:], K.rwkv_w_up.rearrange("d l n -> l d n"), 'wup')
        ld1(aup[:], K.rwkv_a_up.rearrange("d l n -> l d n"), 'aup')
        ld1(gup[:], K.rwkv_g_up, 'gup')
        ld1(bones[:], K.bones, 'bones')
        ld1(idf[:], K.identf, 'idf')
        S.op('dve', lambda e: e.memset(ones1[:], 1.0), writes=['ones1'])
        S.op('dve', lambda e: e.memset(eps12[:], 1e-12), writes=['eps12'])
        S.op('dve', lambda e: e.tensor_scalar(out=omka[:], in0=kav[:], scalar1=-1.0, scalar2=1.0, op0=ALU.mult, op1=ALU.add), reads=['kav'], writes=['omka'])

        nblk = TA // G
        for bi in range(nblk):
            t0 = bi * G
            lat = t0 >= TC
            sl = bi % 2
            h_ = hr[sl]
            hk = 'hr%d' % sl
            first = (t0 == 0 or t0 == TC)
            last = (t0 + G == TC or t0 + G == TA)
            c0 = 1 if first else 0
            c1 = G + 1 if last else G + 2
            if first:
                S.op('pool', lambda e: e.memset(h_[:, :, 0:1], 0.0), writes=[hk])
            if last:
                S.op('pool', lambda e: e.memset(h_[:, :, G + 1:G + 2], 0.0), writes=[hk])
            for q in range(3):
                S.dma('sp', lambda e: e.dma_start(out=h_[:, q * 4:(q + 1) * 4, c0:c1], in_=K.hT[q * 512:(q + 1) * 512, t0 - 1 + c0:t0 - 1 + c1].rearrange("(c p) t -> p c t", p=128)),
                      reads=['hT'], writes=[hk])
            lo_ = lo[sl]
            lk = 'lo%d' % sl
            S.dma('sp', lambda e: e.dma_start(out=lo_[:], in_=K.hT[1536:1792, t0:t0 + G].rearrange("(c p) t -> p c t", p=64)), reads=['hT'], writes=[lk])
            gd_ = gd[sl]
            gk = 'gd%d' % sl
            if lat:
                S.dma('sp', lambda e: e.dma_start(out=gd_[:], in_=K.hT[1792:1920, t0:t0 + G]), reads=['hT'], writes=[gk])
            for c in range(12):
                S.op('act', lambda e: e.activation(out=cv[:, c, :], in_=h_[:, c, 1:G + 1], func=AF.Identity, scale=cw[:, 1, c:c + 1]), reads=[hk, 'cw'], writes=['cv%d' % c])
                S.op('dve', lambda e: e.scalar_tensor_tensor(out=cv[:, c, :], in0=h_[:, c, 0:G], scalar=cw[:, 0, c:c + 1], in1=cv[:, c, :], op0=ALU.mult, op1=ALU.add),
                     reads=[hk, 'cw', 'cv%d' % c], writes=['cv%d' % c])
                S.op('dve', lambda e: e.scalar_tensor_tensor(out=cv[:, c, :], in0=h_[:, c, 2:G + 2], scalar=cw[:, 2, c:c + 1], in1=cv[:, c, :], op0=ALU.mult, op1=ALU.add),
                     reads=[hk, 'cw', 'cv%d' % c], writes=['cv%d' % c])
            cvr = ['cv%d' % c for c in range(0, 4)]
            cvk = ['cv%d' % c for c in range(4, 8)]
            cvv = ['cv%d' % c for c in range(8, 12)]
            S.dma('pool', lambda e: e.dma_start(out=K.rwR[:, t0:t0 + G].rearrange("(c p) t -> p c t", p=128), in_=cv[:, 0:4, :]), reads=cvr, writes=['rwR'])
            S.dma('pool', lambda e: e.dma_start(out=K.rwV[:, t0:t0 + G].rearrange("(c p) t -> p c t", p=128), in_=cv[:, 8:12, :]), reads=cvv, writes=['rwV'])
            for i in range(G // 128):
                p, pk = pp.get()
                for c in range(4):
                    S.op('pe', lambda e: e.transpose(out=p[:, c * 128:(c + 1) * 128], in_=cv[:, 8 + c, i * 128:(i + 1) * 128], identity=idf[:]), reads=cvv + ['idf'], writes=[pk], acc=True)
                v_, vk = vt.get()
                S.op('act', lambda e: e.activation(out=v_[:], in_=p[:], func=AF.Identity), reads=[pk], writes=[vk])
                S.dma('pool', lambda e: e.dma_start(out=K.rwVtok[t0 + i * 128:t0 + (i + 1) * 128, :], in_=v_[:]), reads=[vk], writes=['rwVtok'])
            for c in range(4):
                S.op('dve', lambda e: e.tensor_scalar_mul(out=kq[:, c, :], in0=cv[:, 4 + c, :], scalar1=kkv[:, c:c + 1]), reads=cvk + ['kkv'], writes=['kq'])
            S.op('act', lambda e: e.activation(out=sq[:], in_=kq[:], func=AF.Square), reads=['kq'], writes=['sq'])
            for c2 in range(2):
                p, pk = pp.get()
                S.op('pe', lambda e: e.matmul(p[:, 0:2 * G], lhsT=bones[:], rhs=sq[:, 2 * c2:2 * c2 + 2, :], start=True, stop=True), reads=['bones', 'sq'], writes=[pk])
                S.op('act', lambda e: e.activation(out=lnt[:, 2 * c2:2 * c2 + 2, :], in_=p[:, 0:2 * G], func=AF.Ln, bias=eps12[:], scale=1.0), reads=[pk, 'eps12'], writes=['lnt'])
            S.op('act', lambda e: e.activation(out=lnt[:], in_=lnt[:], func=AF.Exp, scale=-0.5), reads=['lnt'], writes=['lnt'])
            S.op('dve', lambda e: e.tensor_tensor(out=kk_[:], in0=kq[:], in1=lnt[:], op=ALU.mult), reads=['kq', 'lnt'], writes=['kk_'])
            S.dma('pool', lambda e: e.dma_start(out=K.rwKK[:, t0:t0 + G].rearrange("(c p) t -> p c t", p=128), in_=kk_[:]), reads=['kk_'], writes=['rwKK'])
            S.op('act', lambda e: e.activation(out=tw[:], in_=lo_[:, 0:2, :], func=AF.Tanh), reads=[lk], writes=['tw'])
            for d in range(2):
                for i in range(G // 128):
                    p, pk = pp.get()
                    S.op('pe', lambda e: e.matmul(p[:], lhsT=tw[:, d, i * 128:(i + 1) * 128], rhs=wup[:, d, :], start=True, stop=False), reads=['tw', 'wup'], writes=[pk], acc=True)
                    S.op('pe', lambda e: e.matmul(p[:], lhsT=ones1[:], rhs=w0r[:, d, :], start=False, stop=True), reads=['ones1', 'w0r'], writes=[pk], acc=True)
                    l_, lk2 = ld.get()
                    S.op('act', lambda e: e.activation(out=l_[:], in_=p[:], func=AF.Sigmoid), reads=[pk], writes=[lk2])
                    S.dma('pool', lambda e: e.dma_start(out=K.rwLD[d][t0 + i * 128:t0 + (i + 1) * 128, :], in_=l_[:]), reads=[lk2], writes=['rwLD%d' % d])
                for c in range(4):
                    p, pk = pp.get()
                    S.op('pe', lambda e: e.matmul(p[:, 0:G], lhsT=aup[:, d, c * 128:(c + 1) * 128], rhs=lo_[:, 2 + d, :], start=True, stop=True), reads=['aup', lk], writes=[pk])
                    S.op('act', lambda e: e.activation(out=av[:, c, :], in_=p[:, 0:G], func=AF.Sigmoid, bias=a0v[:, d, c:c + 1], scale=1.0), reads=[pk, 'a0v'], writes=['av'])
                    S.op('dve', lambda e: e.tensor_scalar(out=tt[:, c, :], in0=av[:, c, :], scalar1=kav[:, c:c + 1], scalar2=omka[:, c:c + 1], op0=ALU.mult, op1=ALU.add),
                         reads=['av', 'kav', 'omka'], writes=['tt'])
                S.op('dve', lambda e: e.tensor_tensor(out=kd[d][:], in0=cv[:, 4:8, :], in1=tt[:], op=ALU.mult), reads=cvk + ['tt'], writes=['kd%d' % d])
                S.op('dve', lambda e: e.tensor_tensor(out=bb[:], in0=kk_[:], in1=av[:], op=ALU.mult), reads=['kk_', 'av'], writes=['bb'])
                S.dma('pool', lambda e: e.dma_start(out=K.rwKD[d][:, t0:t0 + G].rearrange("(c p) t -> p c t", p=128), in_=kd[d][:]), reads=['kd%d' % d], writes=['rwKD%d' % d])
                S.dma('pool', lambda e: e.dma_start(out=K.rwB[d][:, t0:t0 + G].rearrange("(c p) t -> p c t", p=128), in_=bb[:]), reads=['bb'], writes=['rwB%d' % d])
            if lat:
                tl = t0 - TC
                S.op('act', lambda e: e.activation(out=sg[:], in_=gd_[:], func=AF.Sigmoid), reads=[gk], writes=['sg'])
                for c in range(4):
                    p, pk = pp.get()
                    S.op('pe', lambda e: e.matmul(p[:, 0:G], lhsT=gup[:, c * 128:(c + 1) * 128], rhs=sg[:], start=True, stop=True), reads=['gup', 'sg'], writes=[pk])
                    S.op('act', lambda e: e.activation(out=gg[:, c, :], in_=p[:, 0:G], func=AF.Identity), reads=[pk], writes=['gg'])
                S.dma('pool', lambda e: e.dma_start(out=K.rwG[:, tl:tl + G].rearrange("(c p) t -> p c t", p=128), in_=gg[:]), reads=['gg'], writes=['rwG'])
                S.op('dve', lambda e: e.tensor_tensor(out=bc[:], in0=kd[0][:], in1=kd[1][:], op=ALU.add), reads=['kd0', 'kd1'], writes=['bc'])
                S.op('dve', lambda e: e.tensor_tensor(out=bc[:], in0=bc[:], in1=cv[:, 0:4, :], op=ALU.mult), reads=['bc'] + cvr, writes=['bc'])
                for c in range(4):
                    S.op('dve', lambda e: e.tensor_scalar_mul(out=bc[:, c, :], in0=bc[:, c, :], scalar1=rkv[:, c:c + 1]), reads=['bc', 'rkv'], writes=['bc'])
                for c2 in range(2):
                    p, pk = pp.get()
                    S.op('pe', lambda e: e.matmul(p[:, 0:2 * G], lhsT=bones[:], rhs=bc[:, 2 * c2:2 * c2 + 2, :], start=True, stop=True), reads=['bones', 'bc'], writes=[pk])
                    S.op('dve', lambda e: e.tensor_tensor(out=bon[:, 2 * c2:2 * c2 + 2, :], in0=p[:, 0:2 * G], in1=cv[:, 8 + 2 * c2:8 + 2 * c2 + 2, :], op=ALU.mult), reads=[pk] + cvv, writes=['bon'])
                S.dma('pool', lambda e: e.dma_start(out=K.rwBON[:, tl:tl + G].rearrange("(c p) t -> p c t", p=128), in_=bon[:]), reads=['bon'], writes=['rwBON'])
        S.barrier()


def rw_consts():
    i = np.arange(128)[:, None]
    t = np.arange(128)[None, :]
    bd = (i // 64) == (t // 64)
    out = {}
    blk = ((i // 64) == (t // 64)).astype(np.float32)
    for d in range(2):
        strict = (bd & ((i < t) if d == 0 else (i > t))).astype(np.float32)
        incl = (bd & ((i <= t) if d == 0 else (i >= t))).astype(np.float32)
        m1 = np.concatenate([-strict, incl, blk], 1)
        m2 = np.concatenate([incl, blk], 1)
        m3 = np.concatenate([-strict.T, -strict.T, -np.ones((128, 64), np.float32)], 1)
        cum = np.float32(LDS) * np.concatenate([incl, strict, strict.T], 1)
        out['rwm%d' % d] = np.ascontiguousarray(np.concatenate([m1, m2, m3, cum], 1).astype(np.float32))
    out['i64dbl'] = np.ascontiguousarray((np.arange(128)[:, None] % 64 == np.arange(128)[None, :] % 64).astype(np.float32))
    out['bones'] = np.ascontiguousarray(bd.astype(np.float32))
    out['identf'] = np.eye(128, dtype=np.float32)
    return out


GN_EPS = 64e-5


def phase_rwscan(K, ntile_lat=64):
    nc, S = K.nc, K.S
    G = 128
    with ExitStack() as es:
        sb = lambda n, s, d: es.enter_context(nc.sbuf_tensor(_u(n), s, d))
        ps = lambda n, s, d: es.enter_context(nc.psum_tensor(_u(n), s, d))
        mk = sb("mk", [128, 1344], F32)
        idr = sb("idr", [128, 128], F32R)
        i64r = sb("i64r", [128, 128], F32R)
        i64f = sb("i64f", [128, 128], F32)
        idf = sb("idf", [128, 128], F32)
        o64 = sb("o64", [64, 64], F32)
        gng = sb("gng", [64, 8], F32)
        gnb = sb("gnb", [64, 8], F32)
        epsg = sb("epsg", [64, 1], F32)
        raw = [[sb("raw%d_%d" % (j, i), [128, 4, G], F32) for i in range(2)] for j in range(4)]
        vraw = [sb("vraw%d" % i, [128, 512], F32) for i in range(2)]
        ldt = [sb("ldt%d" % i, [128, 512], F32) for i in range(2)]
        vtr = sb("vtr", [128, 512], F32R)
        Ep = sb("Ep", [128, 4, G], F32)
        Em = sb("Em", [128, 4, G], F32)
        Ex = sb("Ex", [128, 4, G], F32)
        Ea = sb("Ea", [128, 4, G], F32)
        KRG = sb("KRG", [128, 4, 3, 128], F32R)
        BK = sb("BK", [128, 4, 2, 128], F32R)
        KB2 = sb("KB2", [128, 4, 2, 128], F32R)
        NS = 4
        La = [sb("La%d" % i, [128, 384], F32R) for i in range(NS)]
        Lb = [sb("Lb%d" % i, [128, 384], F32R) for i in range(NS)]
        ATa = [sb("ATa%d" % i, [128, 128], F32R) for i in range(NS)]
        ATb = [sb("ATb%d" % i, [128, 128], F32R) for i in range(NS)]
        X1 = [sb("X1%d" % i, [128, 320], F32R) for i in range(NS)]
        X2 = [sb("X2%d" % i, [128, 256], F32R) for i in range(NS)]
        Xf = [sb("Xf%d" % i, [128, 256], F32R) for i in range(NS)]
        QG1 = sb("QG1", [64, 8, 256], F32R)
        QG2 = sb("QG2", [128, 8, 256], F32R)
        Sth = sb("Sth", [64, 3, 8, 64], F32R)
        T = [sb("T%d" % i, [64, 8, G], F32) for i in range(4)]
        outb = sb("outb", [64, 8, G], BF16)
        pp = Rot([ps("sp%d" % i, [128, 512], F32) for i in range(7)], "sp")
        pst = ps("pst", [64, 512], F32)

        ld1 = lambda dst, src, key: S.dma('sp', lambda e: e.dma_start(out=dst, in_=src, allow_slow_non_contiguous=True), writes=[key])
        ld1(idf[:], K.identf, 'idf')
        ld1(i64f[:], K.i64dbl, 'i64f')
        ld1(gng[:], K.rwkv_gn_g.rearrange("o (h p) -> p (o h)", p=64), 'gng')
        ld1(gnb[:], K.rwkv_gn_b.rearrange("o (h p) -> p (o h)", p=64), 'gnb')
        S.op('dve', lambda e: e.tensor_copy(out=idr[:], in_=idf[:]), reads=['idf'], writes=['idr'])
        S.op('dve', lambda e: e.tensor_copy(out=i64r[:], in_=i64f[:]), reads=['i64f'], writes=['i64r'])
        S.op('dve', lambda e: e.memset(o64[:], 1.0 / 64.0), writes=['o64'])
        S.op('dve', lambda e: e.memset(epsg[:], GN_EPS), writes=['epsg'])
        zt = sb("zt", [64, 512], F32)
        S.op('dve', lambda e: e.memset(zt[:], 0.0), writes=['zt'])

        evq = [0]

        def evac(out_ap, in_ap, reads, writes):
            if evq[0] % 2 == 0:
                S.op('act', lambda e: e.activation(out=out_ap, in_=in_ap, func=AF.Identity), reads=reads, writes=writes)
            else:
                S.op('dve', lambda e: e.tensor_copy(out=out_ap, in_=in_ap), reads=reads, writes=writes)
            evq[0] += 1

        fl = lambda a: a[:, :, :].rearrange("p h t -> p (h t)")
        bcount = 0
        for d in range(2):
            m1 = mk[:, 0:384]
            m2 = mk[:, 384:640]
            m3 = mk[:, 640:960]
            cum = mk[:, 960:1344]
            S.dma('sp', lambda e: e.dma_start(out=mk[:], in_=K.rwm[d]), writes=['mk'])
            S.op('dve', lambda e: e.tensor_copy(out=Sth[:, 0].rearrange("p h v -> p (h v)"), in_=zt[:]), reads=['zt'], writes=['Sth0'])
            if d == 0:
                blocks = [0, 1] + list(range(2, 2 + ntile_lat))
            else:
                blocks = [1, 0] + list(range(1 + ntile_lat, 1, -1))
            chunks = [0, 1] if d == 0 else [1, 0]
            for b in blocks:
                t0 = b * G
                lat = b >= 2
                sl = bcount % 2
                bcount += 1
                srcs = [K.rwR, K.rwKD[d], K.rwKK, K.rwB[d]]
                rk = ['raw%d_%d' % (j, sl) for j in range(4)]
                for j in range(4):
                    S.dma('sp', lambda e: e.dma_start(out=raw[j][sl][:], in_=srcs[j][:, t0:t0 + G].rearrange("(c p) t -> p c t", p=128)), writes=[rk[j]])
                S.dma('sp', lambda e: e.dma_start(out=vraw[sl][:], in_=K.rwVtok[t0:t0 + G, :]), writes=['vraw%d' % sl])
                S.dma('sp', lambda e: e.dma_start(out=ldt[sl][:], in_=K.rwLD[d][t0:t0 + G, :]), writes=['ldt%d' % sl])
                r_, kd_, kk_, b_ = [raw[j][sl] for j in range(4)]
                S.op('act', lambda e: e.activation(out=vtr[:], in_=vraw[sl][:], func=AF.Identity), reads=['vraw%d' % sl], writes=['vtr'])
                banks = [pp.get() for _ in range(3)]
                for c in range(4):
                    for q in range(3):
                        S.op('pe', lambda e: e.matmul(banks[q][0][:, c * 128:(c + 1) * 128], lhsT=ldt[sl][:, c * 128:(c + 1) * 128], rhs=cum[:, q * 128:(q + 1) * 128], start=True, stop=True),
                             reads=['ldt%d' % sl, 'mk'], writes=[banks[q][1]], acc=True)
                v3 = lambda bank: bank[:, :].rearrange("p (c t) -> p c t", c=4)
                S.op('act', lambda e: e.activation(out=Ep[:], in_=v3(banks[0][0]), func=AF.Exp), reads=[banks[0][1]], writes=['Ep'])
                S.op('act', lambda e: e.activation(out=Em[:], in_=v3(banks[0][0]), func=AF.Exp, scale=-1.0), reads=[banks[0][1]], writes=['Em'])
                S.op('act', lambda e: e.activation(out=Ex[:], in_=v3(banks[1][0]), func=AF.Exp), reads=[banks[1][1]], writes=['Ex'])
                S.op('act', lambda e: e.activation(out=Ea[:], in_=v3(banks[2][0]), func=AF.Exp), reads=[banks[2][1]], writes=['Ea'])
                S.op('dve', lambda e: e.tensor_tensor(out=KRG[:, :, 0, :], in0=kk_[:], in1=Ex[:], op=ALU.mult), reads=[rk[2], 'Ex'], writes=['KRG'])
                S.op('dve', lambda e: e.tensor_tensor(out=KRG[:, :, 1, :], in0=r_[:], in1=Ep[:], op=ALU.mult), reads=[rk[0], 'Ep'], writes=['KRG'])
                S.op('dve', lambda e: e.tensor_tensor(out=BK[:, :, 0, :], in0=b_[:], in1=Em[:], op=ALU.mult), reads=[rk[3], 'Em'], writes=['BK'])
                S.op('dve', lambda e: e.tensor_tensor(out=BK[:, :, 1, :], in0=kd_[:], in1=Em[:], op=ALU.mult), reads=[rk[1], 'Em'], writes=['BK'])
                S.op('dve', lambda e: e.tensor_tensor(out=KB2[:, :, 0, :], in0=kd_[:], in1=Ea[:], op=ALU.mult), reads=[rk[1], 'Ea'], writes=['KB2'])
                S.op('dve', lambda e: e.tensor_tensor(out=KB2[:, :, 1, :], in0=b_[:], in1=Ea[:], op=ALU.mult), reads=[rk[3], 'Ea'], writes=['KB2'])
                for cc in range(2):
                    pos = cc * 64 + (63 if d == 0 else 0)
                    in0 = i64f[:, cc * 64:(cc + 1) * 64].unsqueeze(1).to_broadcast([128, 4, 64])
                    in1 = Ep[:, :, pos:pos + 1].to_broadcast([128, 4, 64])
                    S.op('dve', lambda e: e.tensor_tensor(out=KRG[:, :, 2, cc * 64:(cc + 1) * 64], in0=in0, in1=in1, op=ALU.mult), reads=['i64f', 'Ep'], writes=['KRG'])

                for g0 in range(0, 8, NS):
                    grp = list(range(g0, g0 + NS))
                    cur = {}
                    for s_, h in enumerate(grp):
                        c, pb = h // 2, 64 * (h % 2)
                        fm = lambda arr, k0, k1: arr[pb:pb + 64, c, k0:k1, :].rearrange("p k t -> p (k t)")
                        p1, k1 = pp.get()
                        S.op('pe', lambda e: e.matmul(p1[:, 0:256], lhsT=fm(BK, 0, 1), rhs=fm(KRG, 0, 2), start=True, stop=True), reads=['BK', 'KRG'], writes=[k1], acc=True)
                        S.op('pe', lambda e: e.matmul(p1[:, 256:384], lhsT=fm(KB2, 1, 2), rhs=i64r[pb:pb + 64, :], start=True, stop=True), reads=['KB2', 'i64r'], writes=[k1], acc=True)
                        S.op('dve', lambda e: e.tensor_tensor(out=La[s_][:], in0=p1[:, 0:384], in1=m1, op=ALU.mult), reads=[k1, 'mk'], writes=['La%d' % s_])
                        p2, k2 = pp.get()
                        S.op('pe', lambda e: e.matmul(p2[:, 0:128], lhsT=fm(BK, 1, 2), rhs=fm(KRG, 1, 2), start=True, stop=True), reads=['BK', 'KRG'], writes=[k2], acc=True)
                        S.op('pe', lambda e: e.matmul(p2[:, 128:256], lhsT=fm(KB2, 0, 1), rhs=i64r[pb:pb + 64, :], start=True, stop=True), reads=['KB2', 'i64r'], writes=[k2], acc=True)
                        S.op('dve', lambda e: e.tensor_tensor(out=X2[s_][:], in0=p2[:, 0:256], in1=m2, op=ALU.mult), reads=[k2, 'mk'], writes=['X2%d' % s_])
                        p3, k3 = pp.get()
                        S.op('pe', lambda e: e.matmul(p3[:, 0:256], lhsT=fm(KRG, 0, 1), rhs=fm(BK, 0, 2), start=True, stop=True), reads=['BK', 'KRG'], writes=[k3], acc=True)
                        S.op('pe', lambda e: e.matmul(p3[:, 256:320], lhsT=fm(KRG, 0, 1), rhs=i64r[pb:pb + 64, 0:64], start=True, stop=True), reads=['KRG', 'i64r'], writes=[k3], acc=True)
                        S.op('dve', lambda e: e.tensor_tensor(out=X1[s_][:], in0=p3[:, 0:320], in1=m3, op=ALU.mult), reads=[k3, 'mk'], writes=['X1%d' % s_])
                        cur[s_] = (La[s_], 'La%d' % s_, X1[s_][:, 0:128], 'X1%d' % s_)
                    for lev in range(5):
                        pend = []
                        nxt = {}
                        for s_ in range(NS):
                            L, Lk, AT, ATk = cur[s_]
                            p, pk = pp.get()
                            S.op('pe', lambda e: e.matmul(p[:, 0:384], lhsT=AT, rhs=L[:, 0:384], start=True, stop=False), reads=[Lk, ATk], writes=[pk], acc=True)
                            S.op('pe', lambda e: e.matmul(p[:, 128:384], lhsT=idr[:], rhs=L[:, 128:384], start=False, stop=True), reads=[Lk, 'idr'], writes=[pk], acc=True)
                            pend.append((p, pk))
                        for s_ in range(NS):
                            p, pk = pend[s_]
                            Ln_ = Lb[s_] if lev % 2 == 0 else La[s_]
                            Lnk = ('Lb%d' if lev % 2 == 0 else 'La%d') % s_
                            evac(Ln_[:], p[:, 0:384], [pk], [Lnk])
                            nxt[s_] = (Ln_, Lnk)
                        for s_ in range(NS):
                            Ln_, Lnk = nxt[s_]
                            p, pk = pend[s_]
                            S.op('pe', lambda e: e.transpose(out=p[:, 384:512], in_=Ln_[:, 0:128].bitcast(F32), identity=idf[:]), reads=[Lnk, 'idf'], writes=[pk])
                        for s_ in range(NS):
                            Ln_, Lnk = nxt[s_]
                            p, pk = pend[s_]
                            ATn = ATa[s_] if lev % 2 == 0 else ATb[s_]
                            ATnk = ('ATa%d' if lev % 2 == 0 else 'ATb%d') % s_
                            evac(ATn[:], p[:, 384:512], [pk], [ATnk])
                            cur[s_] = (Ln_, Lnk, ATn[:], ATnk)
                    for s_, h in enumerate(grp):
                        L, Lk, AT, ATk = cur[s_]
                        p, pk = pp.get()
                        S.op('pe', lambda e: e.matmul(p[:, 0:256], lhsT=AT, rhs=L[:, 128:384], start=True, stop=False), reads=[Lk, ATk], writes=[pk], acc=True)
                        S.op('pe', lambda e: e.matmul(p[:, 0:256], lhsT=idr[:], rhs=L[:, 128:384], start=False, stop=True), reads=[Lk, 'idr'], writes=[pk], acc=True)
                        evac(Xf[s_][:], p[:, 0:256], [pk], ['Xf%d' % s_])
                    for s_, h in enumerate(grp):
                        c, pb = h // 2, 64 * (h % 2)
                        p, pk = pp.get()
                        S.op('pe', lambda e: e.matmul(p[:, 0:256], lhsT=idr[:], rhs=X2[s_][:], start=True, stop=False), reads=['X2%d' % s_, 'idr'], writes=[pk], acc=True)
                        S.op('pe', lambda e: e.matmul(p[:, 0:256], lhsT=X1[s_][:, 128:256], rhs=Xf[s_][:], start=False, stop=True), reads=['X1%d' % s_, 'Xf%d' % s_], writes=[pk], acc=True)
                        evac(QG2[:, h, :], p[:, 0:256], [pk], ['QG2_%d' % h])
                        q, qk = pp.get()
                        rg = KRG[pb:pb + 64, c, 1:3, :].rearrange("p k t -> p (k t)")
                        S.op('pe', lambda e: e.matmul(q[0:64, 0:256], lhsT=idr[pb:pb + 64, pb:pb + 64], rhs=rg, start=True, stop=False), reads=['KRG', 'idr'], writes=[qk], acc=True)
                        S.op('pe', lambda e: e.matmul(q[0:64, 0:256], lhsT=X1[s_][:, 256:320], rhs=Xf[s_][:], start=False, stop=True), reads=['X1%d' % s_, 'Xf%d' % s_], writes=[qk], acc=True)
                        evac(QG1[:, h, :], q[0:64, 0:256], [qk], ['QG1_%d' % h])
                for s, cc in enumerate(chunks):
                    for h in range(8):
                        S.op('pe', lambda e: e.matmul(pst[:, h * 64:(h + 1) * 64], lhsT=QG1[:, h, 128 + cc * 64:128 + (cc + 1) * 64], rhs=Sth[:, s, h, :], start=True, stop=False),
                             reads=['QG1_%d' % h, 'Sth%d' % s], writes=['pst'], acc=True)
                        S.op('pe', lambda e: e.matmul(pst[:, h * 64:(h + 1) * 64], lhsT=QG2[:, h, 128 + cc * 64:128 + (cc + 1) * 64], rhs=vtr[:, h * 64:(h + 1) * 64], start=False, stop=True),
                             reads=['QG2_%d' % h, 'vtr'], writes=['pst'], acc=True)
                    evac(Sth[:, s + 1].rearrange("p h v -> p (h v)"), pst[:, :], ['pst'], ['Sth%d' % (s + 1)])
                if lat:
                    ybuf = T[0]
                    for hq in range(2):
                        bank = pp.get()
                        for h4 in range(4):
                            h = hq * 4 + h4
                            S.op('pe', lambda e: e.matmul(bank[0][0:64, h4 * 128:(h4 + 1) * 128], lhsT=vtr[:, h * 64:(h + 1) * 64], rhs=QG2[:, h, 0:128], start=True, stop=False),
                                 reads=['QG2_%d' % h, 'vtr'], writes=[bank[1]], acc=True)
                            for s, cc in enumerate(chunks):
                                S.op('pe', lambda e: e.matmul(bank[0][0:64, h4 * 128 + cc * 64:h4 * 128 + (cc + 1) * 64], lhsT=Sth[:, s, h, :], rhs=QG1[:, h, cc * 64:(cc + 1) * 64], start=False, stop=(s == 1)),
                                     reads=['QG1_%d' % h, 'Sth%d' % s], writes=[bank[1]], acc=True)
                        evac(ybuf[:, hq * 4:hq * 4 + 4, :], bank[0][0:64, :].rearrange("p (h t) -> p h t", h=4), [bank[1]], ['T0'])
                    tl = t0 - TC
                    if d == 0:
                        S.dma('pool', lambda e: e.dma_start(out=K.rwY0[:, tl:tl + G].rearrange("(h p) t -> p h t", p=64), in_=ybuf[:]), reads=['T0'], writes=['rwY0'])
                    else:
                        y0b, cen, sqb = T[1], T[2], T[3]
                        S.dma('sp', lambda e: e.dma_start(out=y0b[:], in_=K.rwY0[:, tl:tl + G].rearrange("(h p) t -> p h t", p=64)), reads=['rwY0'], writes=['T1'])
                        S.op('dve', lambda e: e.tensor_tensor(out=ybuf[:], in0=ybuf[:], in1=y0b[:], op=ALU.add), reads=['T0', 'T1'], writes=['T0'])
                        for q in range(2):
                            p, pk = pp.get()
                            S.op('pe', lambda e: e.matmul(p[0:64, :], lhsT=o64[:], rhs=fl(ybuf)[:, q * 512:(q + 1) * 512], start=True, stop=True), reads=['o64', 'T0'], writes=[pk])
                            S.op('dve', lambda e: e.tensor_tensor(out=fl(cen)[:, q * 512:(q + 1) * 512], in0=fl(ybuf)[:, q * 512:(q + 1) * 512], in1=p[0:64, :], op=ALU.subtract), reads=[pk, 'T0'], writes=['T2'])
                        S.op('act', lambda e: e.activation(out=sqb[:], in_=cen[:], func=AF.Square), reads=['T2'], writes=['T3'])
                        rsb = T[1]
                        for q in range(2):
                            p, pk = pp.get()
                            S.op('pe', lambda e: e.matmul(p[0:64, :], lhsT=o64[:], rhs=fl(sqb)[:, q * 512:(q + 1) * 512], start=True, stop=True), reads=['o64', 'T3'], writes=[pk])
                            S.op('act', lambda e: e.activation(out=fl(rsb)[:, q * 512:(q + 1) * 512], in_=p[0:64, :], func=AF.Ln, bias=epsg[:], scale=1.0), reads=[pk, 'epsg'], writes=['T1'])
                        S.op('act', lambda e: e.activation(out=rsb[:], in_=rsb[:], func=AF.Exp, scale=-0.5), reads=['T1'], writes=['T1'])
                        bonb, ggb = T[3], T[0]
                        S.dma('sp', lambda e: e.dma_start(out=bonb[:], in_=K.rwBON[:, tl:tl + G].rearrange("(h p) t -> p h t", p=64)), reads=['rwBON'], writes=['T3'])
                        S.op('dve', lambda e: e.tensor_tensor(out=cen[:], in0=cen[:], in1=rsb[:], op=ALU.mult), reads=['T2', 'T1'], writes=['T2'])
                        S.dma('sp', lambda e: e.dma_start(out=ggb[:], in_=K.rwG[:, tl:tl + G].rearrange("(h p) t -> p h t", p=64)), reads=['rwG'], writes=['T0'])
                        S.op('dve', lambda e: e.tensor_tensor(out=cen[:], in0=cen[:], in1=gng[:, :].unsqueeze(2).to_broadcast([64, 8, G]), op=ALU.mult), reads=['T2', 'gng'], writes=['T2'])
                        S.op('dve', lambda e: e.tensor_tensor(out=cen[:], in0=cen[:], in1=gnb[:, :].unsqueeze(2).to_broadcast([64, 8, G]), op=ALU.add), reads=['T2', 'gnb'], writes=['T2'])
                        S.op('dve', lambda e: e.tensor_tensor(out=cen[:], in0=cen[:], in1=bonb[:], op=ALU.add), reads=['T2', 'T3'], writes=['T2'])
                        S.op('dve', lambda e: e.tensor_tensor(out=outb[:], in0=cen[:], in1=ggb[:], op=ALU.mult), reads=['T2', 'T0'], writes=['outb'])
                        S.dma('pool', lambda e: e.dma_start(out=K.mixT[0:512, tl:tl + G].rearrange("(h p) t -> p h t", p=64), in_=outb[:]), reads=['outb'], writes=['mixT'])
                S.op('dve', lambda e: e.tensor_copy(out=Sth[:, 0], in_=Sth[:, 2]), reads=['Sth2'], writes=['Sth0'])
            S.barrier()


ALPHA = 2.0 ** 0.25
ROWW = 1088
BIGPOS = 4096.0


def phase_mix(K):
    nc, S = K.nc, K.S
    NT = TL // 128
    with ExitStack() as es:
        sb = lambda n, s, d: es.enter_context(nc.sbuf_tensor(_u(n), s, d))
        ps = lambda n, s, d: es.enter_context(nc.psum_tensor(_u(n), s, d))
        wo = sb("wo", [128, 8, 1024], BF16)
        wst = [sb("owst%d" % i, [128, 8, 256], F32) for i in range(2)]
        bc = {n: sb("bc_" + n, [128, 1024], F32) for n in ('g1', 'ln1g', 'ln1b', 'sc2', 'sh2')}
        rt = sb("rt", [128, 8, 16], F32)
        idf = sb("idf", [128, 128], F32)
        epst = sb("epst", [128, 1], F32)
        onesf = sb("onesf", [128, 128], F32)
        onesb = sb("onesb", [128, 128], BF16)
        ustr = sb("ustr", [128, 128], BF16)
        mt = [sb("mt%d" % i, [128, 8, 128], BF16) for i in range(2)]
        xt = [sb("xt%d" % i, [128, 1024], F32) for i in range(2)]
        t1 = sb("t1", [128, 1024], F32)
        pre = sb("pre", [128, 1024], F32)
        x1 = [sb("x1_%d" % i, [128, 1024], F32) for i in range(2)]
        uf = sb("uf", [128, 1024], F32)
        urow = [sb("urow%d" % i, [128, ROWW], BF16) for i in range(2)]
        uT = sb("uT", [128, 8, 128], F32)
        st = sb("st", [128, 2, 6], F32)
        mv = sb("mv", [128, 2], F32)
        lnv = sb("lnv", [128, 1], F32)
        rstd = sb("rstd", [128, 1], F32)
        lg = sb("lg", [128, 16], F32)
        mx = sb("mx", [128, 1], F32)
        sm = sb("sm", [128, 1], F32)
        affall = sb("affall", [128, NT, 16], F32)
        tok = sb("tok", [128, 1], I32)
        lo = sb("lo", [128, 16], F32)
        mid = sb("mid", [128, 16], F32)
        ge = sb("ge", [128, 16], F32)
        cntp = sb("cntp", [128, 16], F32)
        mskt = sb("mskt", [128, NT, 16], F32)
        mskb = sb("mskb", [128, NT, 16], BF16)
        csT = sb("csT", [128, 16, NT], F32)
        incT = sb("incT", [128, 16, NT], F32)
        rmask = sb("rmask", [128, 16, NT], F32)
        posf = sb("posf", [128, NT, 16], F32)
        posi = sb("posi", [128, NT, 16], I32)
        zt = sb("zt", [128, 1024], F32)
        pm = [ps("pm%d" % i, [128, 1024], F32) for i in range(2)]
        ptr = ps("ptr", [128, 1024], F32)
        psm = ps("psm", [128, 512], F32)
        psn = ps("psn", [128, 512], F32)

        ld1 = lambda dst, src, key, rd=(): S.dma('sp', lambda e: e.dma_start(out=dst, in_=src, allow_slow_non_contiguous=True), reads=list(rd), writes=[key])
        ld1(idf[:], K.identf, 'idf')
        ld1(ustr[:], K.ustrict, 'ustr')
        ld1(rt[:], K.router.rearrange("(k p) e -> p k e", p=128), 'rt')
        ld1(bc['g1'][:], K.modd[0:1, 2048:3072].partition_broadcast(128), 'bc_g1', ['modd'])
        ld1(bc['sh2'][:], K.modd[0:1, 3072:4096].partition_broadcast(128), 'bc_sh2', ['modd'])
        ld1(bc['sc2'][:], K.modd[0:1, 4096:5120].partition_broadcast(128), 'bc_sc2', ['modd'])
        ld1(bc['ln1g'][:], K.ln1_g.partition_broadcast(128), 'bc_ln1g')
        ld1(bc['ln1b'][:], K.ln1_b.partition_broadcast(128), 'bc_ln1b')
        S.op('dve', lambda e: e.tensor_scalar_add(out=bc['sc2'][:], in0=bc['sc2'][:], scalar1=1.0), reads=['bc_sc2'], writes=['bc_sc2'])
        S.op('dve', lambda e: e.memset(epst[:], 1e-5), writes=['epst'])
        S.op('dve', lambda e: e.memset(onesf[:], 1.0), writes=['onesf'])
        S.op('dve', lambda e: e.memset(onesb[:], 1.0), writes=['onesb'])
        S.op('dve', lambda e: e.memset(zt[:], 0.0), writes=['zt'])
        for j in range(4):
            w = wst[j % 2]
            wk = 'owst%d' % (j % 2)
            S.dma('sp', lambda e: e.dma_start(out=w[:], in_=K.w_out[:, j * 256:(j + 1) * 256].rearrange("(k p) n -> p k n", p=128)), writes=[wk])
            S.op('pool', lambda e: e.tensor_copy(out=wo[:, :, j * 256:(j + 1) * 256], in_=w[:]), reads=[wk], writes=['wo'])
        for i in range(NT):
            S.dma('pool', lambda e: e.dma_start(out=K.yacc[i * 128:(i + 1) * 128, :], in_=zt[:]), reads=['zt'], writes=['yacc'])

        def ln_stats(src, srck):
            for c in range(2):
                S.op('dve', lambda e: e.bn_stats(out=st[:, c, :], in_=src[:, c * 512:(c + 1) * 512]), reads=[srck], writes=['st%d' % c])
            S.op('dve', lambda e: e.bn_aggr(out=mv[:], in_=st[:]), reads=['st0', 'st1'], writes=['mv'])
            S.op('act', lambda e: e.activation(out=lnv[:], in_=mv[:, 1:2], func=AF.Ln, bias=epst[:], scale=1.0), reads=['mv', 'epst'], writes=['lnv'])
            S.op('act', lambda e: e.activation(out=rstd[:], in_=lnv[:], func=AF.Exp, scale=-0.5), reads=['lnv'], writes=['rstd'])

        for i in range(NT):
            sl = i % 2
            m_, mk_ = mt[sl], 'mt%d' % sl
            x_, xk = xt[sl], 'xt%d' % sl
            S.dma('sp', lambda e: e.dma_start(out=m_[:], in_=K.mixT[:, i * 128:(i + 1) * 128].rearrange("(k p) t -> p k t", p=128)), reads=['mixT'], writes=[mk_])
            S.dma('sp', lambda e: e.dma_start(out=x_[:], in_=K.xin[TC + i * 128:TC + (i + 1) * 128, :]), writes=[xk])
            p_, pk_ = pm[sl], 'pm%d' % sl
            for half in range(2):
                for kc in range(8):
                    S.op('pe', lambda e: e.matmul(p_[:, half * 512:(half + 1) * 512], lhsT=m_[:, kc, :], rhs=wo[:, kc, half * 512:(half + 1) * 512], start=(kc == 0), stop=(kc == 7)),
                         reads=[mk_, 'wo'], writes=[pk_], acc=True)
            S.op('dve', lambda e: e.tensor_tensor(out=t1[:], in0=p_[:], in1=bc['g1'][:], op=ALU.mult), reads=[pk_, 'bc_g1'], writes=['t1'])
            S.op('dve', lambda e: e.scalar_tensor_tensor(out=pre[:], in0=x_[:], scalar=ALPHA, in1=t1[:], op0=ALU.mult, op1=ALU.add), reads=[xk, 't1'], writes=['pre'])
            ln_stats(pre, 'pre')
            x1_, x1k = x1[sl], 'x1_%d' % sl
            S.op('dve', lambda e: e.tensor_scalar(out=t1[:], in0=pre[:], scalar1=mv[:, 0:1], scalar2=rstd[:], op0=ALU.subtract, op1=ALU.mult), reads=['pre', 'mv', 'rstd'], writes=['t1'])
            S.op('dve', lambda e: e.tensor_tensor(out=t1[:], in0=t1[:], in1=bc['ln1g'][:], op=ALU.mult), reads=['t1', 'bc_ln1g'], writes=['t1'])
            S.op('dve', lambda e: e.tensor_tensor(out=x1_[:], in0=t1[:], in1=bc['ln1b'][:], op=ALU.add), reads=['t1', 'bc_ln1b'], writes=[x1k])
            S.dma('pool', lambda e: e.dma_start(out=K.x1d[i * 128:(i + 1) * 128, :], in_=x1_[:]), reads=[x1k], writes=['x1d'])
            ln_stats(x1_, x1k)
            S.op('dve', lambda e: e.tensor_scalar(out=pre[:], in0=x1_[:], scalar1=mv[:, 0:1], scalar2=rstd[:], op0=ALU.subtract, op1=ALU.mult), reads=[x1k, 'mv', 'rstd'], writes=['pre'])
            S.op('dve', lambda e: e.tensor_tensor(out=pre[:], in0=pre[:], in1=bc['sc2'][:], op=ALU.mult), reads=['pre', 'bc_sc2'], writes=['pre'])
            S.op('dve', lambda e: e.tensor_tensor(out=uf[:], in0=pre[:], in1=bc['sh2'][:], op=ALU.add), reads=['pre', 'bc_sh2'], writes=['uf'])
            ur, urk = urow[sl], 'urow%d' % sl
            S.op('act', lambda e: e.activation(out=ur[:, 0:1024], in_=uf[:], func=AF.Identity), reads=['uf'], writes=[urk])
            for k in range(8):
                S.op('pe', lambda e: e.transpose(out=ptr[:, k * 128:(k + 1) * 128], in_=uf[:, k * 128:(k + 1) * 128], identity=idf[:]), reads=['uf', 'idf'], writes=['ptr'], acc=True)
            S.op('act', lambda e: e.activation(out=uT[:].rearrange("p k t -> p (k t)"), in_=ptr[:], func=AF.Identity), reads=['ptr'], writes=['uT'])
            for k in range(8):
                S.op('pe', lambda e: e.matmul(psm[:, 0:16], lhsT=uT[:, k, :], rhs=rt[:, k, :], start=(k == 0), stop=(k == 7)), reads=['uT', 'rt'], writes=['psm'], acc=True)
            S.op('dve', lambda e: e.reduce_max(out=mx[:], in_=psm[:, 0:16], axis=AX.X), reads=['psm'], writes=['mx'])
            S.op('dve', lambda e: e.tensor_scalar_mul(out=mx[:], in0=mx[:], scalar1=-1.0), reads=['mx'], writes=['mx'])
            S.op('act', lambda e: e.activation(out=lg[:], in_=psm[:, 0:16], func=AF.Exp, bias=mx[:], scale=1.0, accum_out=sm[:]), reads=['psm', 'mx'], writes=['lg', 'sm'])
            S.op('dve', lambda e: e.reciprocal(out=sm[:], in_=sm[:]), reads=['sm'], writes=['sm'])
            S.op('dve', lambda e: e.tensor_scalar_mul(out=affall[:, i, :], in0=lg[:], scalar1=sm[:]), reads=['lg', 'sm'], writes=['affall'])
            S.op('dve', lambda e: e.tensor_copy(out=ur[:, 1024:1056].bitcast(F32), in_=affall[:, i, :]), reads=['affall'], writes=[urk])
            S.op('pool', lambda e: e.iota(tok[:], pattern=[[0, 1]], base=i * 128, channel_multiplier=1), writes=['tok'])
            S.op('pool', lambda e: e.tensor_copy(out=ur[:, 1056:1058].bitcast(I32), in_=tok[:]), reads=['tok'], writes=[urk])
            S.dma('pool', lambda e: e.dma_start(out=K.urd[i * 128:(i + 1) * 128, 0:1058], in_=ur[:, 0:1058]), reads=[urk], writes=['urd%d' % i])

        S.op('dve', lambda e: e.memset(lo[:], 0.0), writes=['lo'])
        for k in range(30):
            hk = 2.0 ** -(k + 1)
            S.op('dve', lambda e: e.tensor_scalar_add(out=mid[:], in0=lo[:], scalar1=hk), reads=['lo'], writes=['mid'])
            S.op('dve', lambda e: e.tensor_tensor(out=mskt[:], in0=affall[:], in1=mid[:, :].unsqueeze(1).to_broadcast([128, NT, 16]), op=ALU.is_ge), reads=['affall', 'mid'], writes=['mskt'])
            S.op('dve', lambda e: e.tensor_reduce(out=cntp[:], in_=mskt[:].rearrange("p i e -> p e i"), axis=AX.X, op=ALU.add), reads=['mskt'], writes=['cntp'])
            S.op('pe', lambda e: e.matmul(psn[:, 0:16], lhsT=onesf[:], rhs=cntp[:], start=True, stop=True), reads=['onesf', 'cntp'], writes=['psn'])
            S.op('dve', lambda e: e.tensor_scalar(out=ge[:], in0=psn[:, 0:16], scalar1=float(CAP) - 0.5, scalar2=hk, op0=ALU.is_ge, op1=ALU.mult), reads=['psn'], writes=['ge'])
            S.op('dve', lambda e: e.tensor_tensor(out=lo[:], in0=lo[:], in1=ge[:], op=ALU.add), reads=['lo', 'ge'], writes=['lo'])
        S.op('dve', lambda e: e.tensor_tensor(out=mskt[:], in0=affall[:], in1=lo[:, :].unsqueeze(1).to_broadcast([128, NT, 16]), op=ALU.is_ge), reads=['affall', 'lo'], writes=['mskt'])
        S.op('act', lambda e: e.activation(out=mskb[:], in_=mskt[:], func=AF.Identity), reads=['mskt'], writes=['mskb'])
        mflat = mskb[:].rearrange("p i e -> p (i e)")
        for hh in range(2):
            S.op('pe', lambda e: e.matmul(psm[:, :], lhsT=onesb[:], rhs=mflat[:, hh * 512:(hh + 1) * 512], start=True, stop=True), reads=['onesb', 'mskb'], writes=['psm'])
            S.op('dve', lambda e: e.tensor_copy(out=csT[:, :, hh * 32:(hh + 1) * 32], in_=psm[:, :].rearrange("p (i e) -> p e i", e=16)), reads=['psm'], writes=['csT'])
        S.op('dve', lambda e: e.memset(rmask[:], 1.0), writes=['rmask'])
        S.op('dve', lambda e: e.memset(rmask[:, :, 0:1], 0.0), writes=['rmask'])
        S.op('dve', lambda e: e.tensor_tensor_scan(out=incT[:].rearrange("p e i -> p (e i)"), data0=rmask[:].rearrange("p e i -> p (e i)"), data1=csT[:].rearrange("p e i -> p (e i)"),
                                                   initial=0.0, op0=ALU.mult, op1=ALU.add), reads=['rmask', 'csT'], writes=['incT'])
        S.op('dve', lambda e: e.tensor_tensor(out=incT[:], in0=incT[:], in1=csT[:], op=ALU.subtract), reads=['incT', 'csT'], writes=['incT'])
        for hh in range(2):
            S.op('pe', lambda e: e.matmul(psm[:, :], lhsT=ustr[:], rhs=mflat[:, hh * 512:(hh + 1) * 512], start=True, stop=True), reads=['ustr', 'mskb'], writes=['psm'])
            S.op('dve', lambda e: e.tensor_tensor(out=posf[:, hh * 32:(hh + 1) * 32, :], in0=psm[:, :].rearrange("p (i e) -> p i e", e=16),
                                                  in1=incT[:, :, hh * 32:(hh + 1) * 32].rearrange("p e i -> p i e"), op=ALU.add), reads=['psm', 'incT'], writes=['posf'])
        S.op('dve', lambda e: e.scalar_tensor_tensor(out=posf[:], in0=posf[:], scalar=-BIGPOS, in1=mskt[:], op0=ALU.add, op1=ALU.mult), reads=['posf', 'mskt'], writes=['posf'])
        S.op('dve', lambda e: e.tensor_scalar_add(out=posf[:], in0=posf[:], scalar1=BIGPOS), reads=['posf'], writes=['posf'])
        S.op('dve', lambda e: e.tensor_copy(out=posi[:], in_=posf[:]), reads=['posf'], writes=['posi'])
        if K.dbg_pos is not None:
            S.dma('sp', lambda e: e.dma_start(out=K.dbg_pos, in_=posf[:]), reads=['posf'], writes=['dbg_pos'])
        breg = nc.gpsimd.to_reg(CAP - 1)
        for i in range(NT):
            sl = i % 2
            ur, urk = urow[sl], 'urow%d' % sl
            S.dma('sp', lambda e: e.dma_start(out=ur[:, 0:1058], in_=K.urd[i * 128:(i + 1) * 128, 0:1058]), reads=['urd%d' % i], writes=[urk])
            for ex in range(NE):
                S.dma('pool', lambda e: e.indirect_dma_start(out=K.xe_d[ex], out_offset=bass.IndirectOffsetOnAxis(ap=posi[:, i, ex:ex + 1], axis=0),
                                                             in_=ur[:, :], in_offset=None, bounds_check=breg, oob_is_err=False),
                      reads=[urk, 'posi'], writes=['xe_d'])
        S.barrier()


def phase_moe(K, experts=range(NE)):
    nc, S = K.nc, K.S
    NT = TL // 128
    with ExitStack() as es:
        sb = lambda n, s, d: es.enter_context(nc.sbuf_tensor(_u(n), s, d))
        ps = lambda n, s, d: es.enter_context(nc.psum_tensor(_u(n), s, d))
        W = [[sb("W%d_%d" % (m, i), [128, 8, 1024], BF16) for i in range(2)] for m in range(3)]
        wst = Rot([sb("ewst%d" % i, [128, 8, 256], F32) for i in range(3)], "ewst")
        idb = sb("idb", [128, 128], BF16)
        xrow = Rot([sb("xrow%d" % i, [128, ROWW], BF16) for i in range(2)], "xrow")
        xeT = sb("xeT", [128, 8, 1024], BF16)
        hidT = sb("hidT", [128, 8, 1024], BF16)
        gates = sb("gates", [128, 8], F32)
        idxs = sb("idxs", [128, 8], I32)
        sgt = Rot([sb("sgt%d" % i, [128, 512], F32) for i in range(2)], "sgt")
        ye = Rot([sb("ye%d" % i, [128, 1024], F32) for i in range(2)], "ye")
        pt = Rot([ps("ept%d" % i, [128, 1024], BF16) for i in range(2)], "ept")
        pg = Rot([ps("epg%d" % i, [128, 512], F32) for i in range(6)], "epg")
        S.dma('sp', lambda e: e.dma_start(out=idb[:], in_=K.ident), writes=['idb'])
        cast_i = [0]

        def load_w(ex, slot):
            for m, src in enumerate((K.exp_w_gate, K.exp_w_up, K.exp_w_down)):
                for j in range(4):
                    w, wk = wst.get()
                    S.dma('sp', lambda e: e.dma_start(out=w[:], in_=src[ex, :, j * 256:(j + 1) * 256].rearrange("(k p) n -> p k n", p=128)), writes=[wk])
                    eng = 'pool' if cast_i[0] % 3 != 2 else 'act'
                    cast_i[0] += 1
                    if eng == 'pool':
                        S.op('pool', lambda e: e.tensor_copy(out=W[m][slot][:, :, j * 256:(j + 1) * 256], in_=w[:]), reads=[wk], writes=['W%d_%d' % (m, slot)])
                    else:
                        S.op('act', lambda e: e.activation(out=W[m][slot][:, :, j * 256:(j + 1) * 256], in_=w[:], func=AF.Identity), reads=[wk], writes=['W%d_%d' % (m, slot)])

        exl = list(experts)
        load_w(exl[0], 0)
        for n, ex in enumerate(exl):
            slot = n % 2
            Wg, Wu, Wd = W[0][slot], W[1][slot], W[2][slot]
            wkeys = ['W%d_%d' % (m, slot) for m in range(3)]
            for j in range(8):
                xr, xk = xrow.get()
                S.dma('sp', lambda e: e.dma_start(out=xr[:, 0:1058], in_=K.xe_d[ex][j * 128:(j + 1) * 128, 0:1058]), reads=['xe_d'], writes=[xk])
                S.op('dve', lambda e: e.tensor_copy(out=gates[:, j:j + 1], in_=xr[:, 1024:1056].bitcast(F32)[:, ex:ex + 1]), reads=[xk], writes=['gates'])
                S.op('dve', lambda e: e.tensor_copy(out=idxs[:, j:j + 1], in_=xr[:, 1056:1058].bitcast(I32)), reads=[xk], writes=['idxs'])
                p, pk = pt.get()
                for k in range(8):
                    S.op('pe', lambda e: e.transpose(out=p[:, k * 128:(k + 1) * 128], in_=xr[:, k * 128:(k + 1) * 128], identity=idb[:]), reads=[xk, 'idb'], writes=[pk], acc=True)
                S.op('act', lambda e: e.activation(out=xeT[:, :, j * 128:(j + 1) * 128], in_=p[:].rearrange("p (k t) -> p k t", k=8), func=AF.Identity), reads=[pk], writes=['xeT'])
            if n + 1 < len(exl):
                load_w(exl[n + 1], 1 - slot)
            for fc in range(8):
                for half in range(2):
                    g_, gk = pg.get()
                    u_, uk = pg.get()
                    for kc in range(8):
                        S.op('pe', lambda e: e.matmul(g_[:], lhsT=Wg[:, kc, fc * 128:(fc + 1) * 128], rhs=xeT[:, kc, half * 512:(half + 1) * 512], start=(kc == 0), stop=(kc == 7)),
                             reads=[wkeys[0], 'xeT'], writes=[gk], acc=True)
                    for kc in range(8):
                        S.op('pe', lambda e: e.matmul(u_[:], lhsT=Wu[:, kc, fc * 128:(fc + 1) * 128], rhs=xeT[:, kc, half * 512:(half + 1) * 512], start=(kc == 0), stop=(kc == 7)),
                             reads=[wkeys[1], 'xeT'], writes=[uk], acc=True)
                    s_, sk = sgt.get()
                    S.op('act', lambda e: e.activation(out=s_[:], in_=g_[:], func=AF.Silu), reads=[gk], writes=[sk])
                    S.op('dve', lambda e: e.tensor_tensor(out=hidT[:, fc, half * 512:(half + 1) * 512], in0=s_[:], in1=u_[:], op=ALU.mult), reads=[sk, uk], writes=['hidT'])
            for j in range(8):
                y_, yk = ye.get()
                for dh in range(2):
                    o_, ok = pg.get()
                    for fc in range(8):
                        S.op('pe', lambda e: e.matmul(o_[:], lhsT=hidT[:, fc, j * 128:(j + 1) * 128], rhs=Wd[:, fc, dh * 512:(dh + 1) * 512], start=(fc == 0), stop=(fc == 7)),
                             reads=[wkeys[2], 'hidT'], writes=[ok], acc=True)
                    S.op('act', lambda e: e.activation(out=y_[:, dh * 512:(dh + 1) * 512], in_=o_[:], func=AF.Identity, scale=gates[:, j:j + 1]), reads=[ok, 'gates'], writes=[yk])
                S.dma('pool', lambda e: e.indirect_dma_start(out=K.yacc, out_offset=bass.IndirectOffsetOnAxis(ap=idxs[:, j:j + 1], axis=0), in_=y_[:, :], in_offset=None,
                                                             compute_op=ALU.add),
                      reads=[yk, 'idxs'], writes=['yacc'])
        S.barrier()


def phase_final(K):
    nc, S = K.nc, K.S
    NT = TL // 128
    with ExitStack() as es:
        sb = lambda n, s, d: es.enter_context(nc.sbuf_tensor(_u(n), s, d))
        bc = {n: sb("fbc_" + n, [128, 1024], F32) for n in ('g2', 'ln2g', 'ln2b')}
        epst = sb("epst", [128, 1], F32)
        x1 = [sb("fx1_%d" % i, [128, 1024], F32) for i in range(2)]
        ya = [sb("fya_%d" % i, [128, 1024], F32) for i in range(2)]
        t1 = sb("t1", [128, 1024], F32)
        pre = sb("pre", [128, 1024], F32)
        ob = [sb("fob_%d" % i, [128, 1024], F32) for i in range(2)]
        st = sb("st", [128, 2, 6], F32)
        mv = sb("mv", [128, 2], F32)
        lnv = sb("lnv", [128, 1], F32)
        rstd = sb("rstd", [128, 1], F32)
        ld1 = lambda dst, src, key, rd=(): S.dma('sp', lambda e: e.dma_start(out=dst, in_=src, allow_slow_non_contiguous=True), reads=list(rd), writes=[key])
        ld1(bc['g2'][:], K.modd[0:1, 5120:6144].partition_broadcast(128), 'fbc_g2', ['modd'])
        ld1(bc['ln2g'][:], K.ln2_g.partition_broadcast(128), 'fbc_ln2g')
        ld1(bc['ln2b'][:], K.ln2_b.partition_broadcast(128), 'fbc_ln2b')
        S.op('dve', lambda e: e.memset(epst[:], 1e-5), writes=['epst'])
        for i in range(NT):
            sl = i % 2
            S.dma('sp', lambda e: e.dma_start(out=x1[sl][:], in_=K.x1d[i * 128:(i + 1) * 128, :]), reads=['x1d'], writes=['fx1_%d' % sl])
            S.dma('sp', lambda e: e.dma_start(out=ya[sl][:], in_=K.yacc[i * 128:(i + 1) * 128, :]), reads=['yacc'], writes=['fya_%d' % sl])
            S.op('dve', lambda e: e.tensor_tensor(out=t1[:], in0=ya[sl][:], in1=bc['g2'][:], op=ALU.mult), reads=['fya_%d' % sl, 'fbc_g2'], writes=['t1'])
            S.op('dve', lambda e: e.scalar_tensor_tensor(out=pre[:], in0=x1[sl][:], scalar=ALPHA, in1=t1[:], op0=ALU.mult, op1=ALU.add), reads=['fx1_%d' % sl, 't1'], writes=['pre'])
            for c in range(2):
                S.op('dve', lambda e: e.bn_stats(out=st[:, c, :], in_=pre[:, c * 512:(c + 1) * 512]), reads=['pre'], writes=['st%d' % c])
            S.op('dve', lambda e: e.bn_aggr(out=mv[:], in_=st[:]), reads=['st0', 'st1'], writes=['mv'])
            S.op('act', lambda e: e.activation(out=lnv[:], in_=mv[:, 1:2], func=AF.Ln, bias=epst[:], scale=1.0), reads=['mv', 'epst'], writes=['lnv'])
            S.op('act', lambda e: e.activation(out=rstd[:], in_=lnv[:], func=AF.Exp, scale=-0.5), reads=['lnv'], writes=['rstd'])
            S.op('dve', lambda e: e.tensor_scalar(out=t1[:], in0=pre[:], scalar1=mv[:, 0:1], scalar2=rstd[:], op0=ALU.subtract, op1=ALU.mult), reads=['pre', 'mv', 'rstd'], writes=['t1'])
            S.op('dve', lambda e: e.tensor_tensor(out=t1[:], in0=t1[:], in1=bc['ln2g'][:], op=ALU.mult), reads=['t1', 'fbc_ln2g'], writes=['t1'])
            S.op('dve', lambda e: e.tensor_tensor(out=ob[sl][:], in0=t1[:], in1=bc['ln2b'][:], op=ALU.add), reads=['t1', 'fbc_ln2b'], writes=['fob_%d' % sl])
            S.dma('pool', lambda e: e.dma_start(out=K.out[i * 128:(i + 1) * 128, :], in_=ob[sl][:]), reads=['fob_%d' % sl], writes=['out'])
        S.barrier()


def build_program(debug=(), phases=None, dbg_in=(), opts=None):
    opts = opts or {}
    nc = bass.Bass("TRN2", target_bir_lowering=False)
    K = Ctx()
    K.nc = nc
    di = lambda n, s, d: nc.dram_tensor(n, s, d, kind="ExternalInput").ap()
    K.xin = di("xin", [TA, D], F32)
    K.ccT = di("ccT", [128, 8, 2], F32)
    K.w_ada = di("w_ada", [D, 6 * D], F32)
    K.b_ada = di("b_ada", [1, 6 * D], F32)
    K.w_in = di("w_in", [D, 2560], F32)
    K.cosT = di("cosT", [64, TL], F32)
    K.sinT = di("sinT", [64, TL], F32)
    K.ident = di("ident", [128, 128], BF16)
    K.identf = di("identf", [128, 128], F32)
    K.i64dbl = di("i64dbl", [128, 128], F32)
    K.bones = di("bones", [128, 128], F32)
    K.rwm = [di("rwm%d" % d, [128, 1344], F32) for d in range(2)]
    K.mla_q_norm = di("mla_q_norm", [1, 256], F32)
    K.mla_kv_norm = di("mla_kv_norm", [1, 256], F32)
    K.w_uq = di("w_uq", [256, 1024], F32)
    K.w_uk = di("w_uk", [256, 512], F32)
    K.w_uv = di("w_uv", [256, 512], F32)
    K.rwkv_conv = di("rwkv_conv", [3, 1536], F32)
    K.rwkv_w0 = di("rwkv_w0", [2, 512], F32)
    K.rwkv_w_up = di("rwkv_w_up", [2, 64, 512], F32)
    K.rwkv_a0 = di("rwkv_a0", [2, 512], F32)
    K.rwkv_a_up = di("rwkv_a_up", [2, 64, 512], F32)
    K.rwkv_g_up = di("rwkv_g_up", [128, 512], F32)
    K.rwkv_k_k = di("rwkv_k_k", [1, 512], F32)
    K.rwkv_k_a = di("rwkv_k_a", [1, 512], F32)
    K.rwkv_r_k = di("rwkv_r_k", [1, 512], F32)
    K.rwkv_gn_g = di("rwkv_gn_g", [1, 512], F32)
    K.rwkv_gn_b = di("rwkv_gn_b", [1, 512], F32)
    K.w_out = di("w_out", [D, D], F32)
    K.ln1_g = di("ln1_g", [1, D], F32)
    K.ln1_b = di("ln1_b", [1, D], F32)
    K.ln2_g = di("ln2_g", [1, D], F32)
    K.ln2_b = di("ln2_b", [1, D], F32)
    K.router = di("router", [D, NE], F32)
    K.ustrict = di("ustrict", [128, 128], BF16)
    K.exp_w_gate = di("exp_w_gate", [NE, D, D], F32)
    K.exp_w_up = di("exp_w_up", [NE, D, D], F32)
    K.exp_w_down = di("exp_w_down", [NE, D, D], F32)

    def scratch(n, s, d):
        if n in dbg_in:
            return nc.dram_tensor(n, s, d, kind="ExternalInput").ap()
        kind = "ExternalOutput" if n in debug else "Internal"
        return nc.dram_tensor(n, s, d, kind=kind).ap()
    K.modd = scratch("modd", [2, 6 * D], F32)
    K.hT = scratch("hT", [2432, TA], F32)
    K.krT = scratch("krT", [64, TA], BF16)
    K.qnT = scratch("qnT", [512, TL], BF16)
    K.qrT = scratch("qrT", [256, TL], BF16)
    K.knT = scratch("knT", [512, TA], BF16)
    K.vtok = scratch("vtok", [TA, 512], BF16)
    K.mixT = scratch("mixT", [1024, TL], BF16)
    K.rwR = scratch("rwR", [512, TA], F32)
    K.rwV = scratch("rwV", [512, TA], F32)
    K.rwKK = scratch("rwKK", [512, TA], F32)
    K.rwKD = [scratch("rwKD%d" % d, [512, TA], F32) for d in range(2)]
    K.rwB = [scratch("rwB%d" % d, [512, TA], F32) for d in range(2)]
    K.rwVtok = scratch("rwVtok", [TA, 512], F32)
    K.rwLD = [scratch("rwLD%d" % d, [TA, 512], F32) for d in range(2)]
    K.rwG = scratch("rwG", [512, TL], F32)
    K.rwBON = scratch("rwBON", [512, TL], F32)
    K.rwY0 = scratch("rwY0", [512, TL], F32)
    K.x1d = scratch("x1d", [TL, D], F32)
    K.urd = scratch("urd", [TL, ROWW], BF16)
    K.xe_d = [scratch("xe_d%d" % e_, [CAP, ROWW], BF16) for e_ in range(NE)]
    K.yacc = scratch("yacc", [TL, D], F32)
    K.dbg_pos = scratch("dbg_pos", [128, TL // 128, 16], F32) if 'dbg_pos' in debug else None
    K.out = nc.dram_tensor("out", [TL, D], F32, kind="ExternalOutput").ap()
    allp = ['mod', 'inproj', 'mlaprep', 'attn', 'rwprep', 'rwscan', 'mix', 'moe', 'final']
    if phases is None:
        phases = allp
    with ExitStack() as es:
        S = Sync(nc, es)
        K.S = S
        if 'mod' in phases:
            phase_mod(K)
        if 'inproj' in phases:
            phase_inproj(K)
        if 'mlaprep' in phases:
            phase_mlaprep(K)
        if 'attn' in phases:
            phase_attn(K)
        if 'attn1' in phases:
            phase_attn(K, heads=(1,), nqt=2)
        if 'rwprep' in phases:
            phase_rwprep(K)
        if 'rwscan' in phases:
            phase_rwscan(K, **opts.get('rwscan', {}))
        if 'mix' in phases:
            phase_mix(K)
        if 'moe' in phases:
            phase_moe(K, **opts.get('moe', {}))
        if 'final' in phases:
            phase_final(K)
        S.wait_all('sp')
        print("instructions", S.n_inst, "waits", S.n_wait, "sems", S.nsem + NDMA)
    return nc


_SWAP = np.concatenate([np.arange(16, 32), np.arange(0, 16), np.arange(48, 64), np.arange(32, 48)])


def rope_tables():
    half = 32
    inv_freq = (10000.0 ** (-np.arange(0, half, 2, dtype=np.float32) / half)).astype(np.float32)
    t = np.arange(TL)
    rr = (t // 64).astype(np.float32)[None, :]
    cc = (t % 64).astype(np.float32)[None, :]
    ang_r = (inv_freq[:, None] * rr).astype(np.float32)
    ang_c = (inv_freq[:, None] * cc).astype(np.float32)
    cosT = np.concatenate([np.cos(ang_r), np.cos(ang_r), np.cos(ang_c), np.cos(ang_c)], 0).astype(np.float32)
    sinT = np.concatenate([-np.sin(ang_r), np.sin(ang_r), -np.sin(ang_c), np.sin(ang_c)], 0).astype(np.float32)
    return np.ascontiguousarray(cosT), np.ascontiguousarray(sinT)


def make_in_maps(inputs, batches):
    f = lambda a: np.ascontiguousarray(np.asarray(a, dtype=np.float32))
    w_in = f(inputs['w_in'][0])
    w_in_ext = np.concatenate([w_in, w_in[:, 2432:2496][:, _SWAP]], axis=1)
    cosT, sinT = rope_tables()
    wuq = f(inputs['mla_w_uq'][0])
    cols = []
    for h in range(4):
        nope = wuq[:, h * 192:h * 192 + 128]
        rope = wuq[:, h * 192 + 128:h * 192 + 192]
        cols += [nope, rope, rope[:, _SWAP]]
    wuq_ext = np.ascontiguousarray(np.concatenate(cols, axis=1))
    shared = {
        'w_ada': f(inputs['w_ada'][0]), 'b_ada': f(inputs['b_ada']), 'w_in': np.ascontiguousarray(w_in_ext),
        'cosT': cosT, 'sinT': sinT, 'ident': np.eye(128).astype(ml_dtypes.bfloat16),
        'mla_q_norm': f(inputs['mla_q_norm']), 'mla_kv_norm': f(inputs['mla_kv_norm']),
        'w_uq': wuq_ext, 'w_uk': f(inputs['mla_w_uk'][0]), 'w_uv': f(inputs['mla_w_uv'][0]),
        'rwkv_conv': f(inputs['rwkv_conv'][0]), 'rwkv_w0': f(inputs['rwkv_w0'][0]), 'rwkv_w_up': f(inputs['rwkv_w_up'][0]),
        'rwkv_a0': f(inputs['rwkv_a0'][0]), 'rwkv_a_up': f(inputs['rwkv_a_up'][0]), 'rwkv_g_up': f(inputs['rwkv_g_up'][0]),
        'rwkv_k_k': f(inputs['rwkv_k_k']), 'rwkv_k_a': f(inputs['rwkv_k_a']), 'rwkv_r_k': f(inputs['rwkv_r_k']).reshape(1, 512),
        'rwkv_gn_g': f(inputs['rwkv_gn_g']), 'rwkv_gn_b': f(inputs['rwkv_gn_b']),
        'w_out': f(inputs['w_out'][0]), 'ln1_g': f(inputs['ln1_g']), 'ln1_b': f(inputs['ln1_b']),
        'ln2_g': f(inputs['ln2_g']), 'ln2_b': f(inputs['ln2_b']), 'router': f(inputs['router'][0]),
        'ustrict': (np.arange(128)[:, None] < np.arange(128)[None, :]).astype(ml_dtypes.bfloat16),
        'exp_w_gate': f(inputs['exp_w_gate'][0]), 'exp_w_up': f(inputs['exp_w_up'][0]), 'exp_w_down': f(inputs['exp_w_down'][0]),
    }
    shared.update(rw_consts())
    maps = []
    for b in batches:
        m = dict(shared)
        m['xin'] = np.ascontiguousarray(np.concatenate([inputs['ctx'][b], inputs['x'][b]], axis=0).astype(np.float32))
        cc = np.stack([inputs['c'][b], inputs['c_ctx']], axis=-1).astype(np.float32)
        m['ccT'] = np.ascontiguousarray(cc.reshape(8, 128, 2).transpose(1, 0, 2))
        maps.append(m)
    return maps


def kernel(**inputs):
    nc = build_program()
    maps = make_in_maps(inputs, [0, 1, 2, 3, 0, 1, 2, 3])
    res = run_bass_kernel_spmd(nc, maps, core_ids=list(range(8)))
    out = np.stack([res.results[b]['out'] for b in range(4)], axis=0)
    return out.astype(np.float32)
```

```python
import numpy as np
import ml_dtypes
from contextlib import ExitStack
import concourse.bass as bass
import concourse.mybir as mybir
from concourse.bass_utils import run_bass_kernel_spmd

F32 = mybir.dt.float32
BF16 = mybir.dt.bfloat16
I32 = mybir.dt.int32
AF = mybir.ActivationFunctionType
ALU = mybir.AluOpType
AX = mybir.AxisListType

EPOCH = 12000
NDMA = 24

D = 1024
TL = 8192
TC = 256
TA = TL + TC
NE = 16
CAP = 1024


class Sync:
    def __init__(self, nc, es):
        self.nc = nc
        self.es = es
        self.eng = {'pe': nc.tensor, 'dve': nc.vector, 'act': nc.scalar,
                    'pool': nc.gpsimd, 'sp': nc.sync}
        self.sem = {}
        self.cnt = {}
        self.cur = {}
        self.known = {e: {} for e in self.eng}
        self.snap = {}
        self.last_w = {}
        self.readers = {}
        self.dma_keys = []
        self.dma_rr = 0
        self.nsem = 0
        for e in self.eng:
            self._new_epoch(e)
        for i in range(NDMA):
            k = ('dma', i)
            self.sem[k] = es.enter_context(nc.semaphore('dq%d' % i))
            self.cnt[k] = 0
            self.dma_keys.append(k)
        self.n_inst = 0
        self.n_wait = 0

    def _new_epoch(self, e):
        idx = self.nsem
        self.nsem += 1
        k = (e, idx)
        self.sem[k] = self.es.enter_context(self.nc.semaphore('s_%s_%d' % (e, idx)))
        self.cnt[k] = 0
        self.cur[e] = k

    def _need(self, e, ticket):
        k, v = ticket
        kn = self.known[e]
        if kn.get(k, 0) >= v:
            return
        self.eng[e].wait_ge(self.sem[k], v)
        self.n_wait += 1
        kn[k] = v
        sn = self.snap.get(ticket)
        if sn:
            for kk, vv in sn.items():
                if kn.get(kk, 0) < vv:
                    kn[kk] = vv

    def _deps(self, e, reads, writes, acc):
        for b in reads:
            t = self.last_w.get(b)
            if t is not None:
                self._need(e, t)
        for b in writes:
            t = self.last_w.get(b)
            if t is not None and not (acc and t[0] == self.cur[e]):
                self._need(e, t)
            for t in self.readers.get(b, ()):
                self._need(e, t)

    def _record(self, ticket, reads, writes):
        for b in reads:
            self.readers.setdefault(b, []).append(ticket)
        for b in writes:
            self.last_w[b] = ticket
            self.readers[b] = []

    def op(self, e, fn, reads=(), writes=(), acc=False):
        if self.cnt[self.cur[e]] >= EPOCH:
            self._new_epoch(e)
        self._deps(e, reads, writes, acc)
        k = self.cur[e]
        inst = fn(self.eng[e])
        inst.then_inc(self.sem[k], 1)
        self.cnt[k] += 1
        t = (k, self.cnt[k])
        self.snap[t] = dict(self.known[e])
        self._record(t, reads, writes)
        self.n_inst += 1
        return t

    def dma(self, e, fn, reads=(), writes=()):
        k = self.dma_keys[self.dma_rr]
        self.dma_rr = (self.dma_rr + 1) % NDMA
        if self.cnt[k] > 0:
            self._need(e, (k, self.cnt[k]))
        self._deps(e, reads, writes, False)
        inst = fn(self.eng[e])
        inst.then_inc(self.sem[k], 16)
        self.cnt[k] += 16
        t = (k, self.cnt[k])
        self.snap[t] = dict(self.known[e])
        self._record(t, reads, writes)
        self.n_inst += 1
        return t

    def wait_all(self, e):
        for k, v in list(self.cnt.items()):
            if v > 0:
                self._need(e, (k, v))

    def barrier(self):
        for e in self.eng:
            self.wait_all(e)
        self.last_w.clear()
        self.readers.clear()


class Ctx:
    pass


_UC = [0]


def _u(n):
    _UC[0] += 1
    return '%s_%d' % (n, _UC[0])


def phase_mod(K):
    nc, S = K.nc, K.S
    with ExitStack() as es:
        sb = lambda n, s, d: es.enter_context(nc.sbuf_tensor(_u(n), s, d))
        ccs = sb("ccs", [128, 8, 2], F32)
        scT = sb("scT", [128, 8, 2], F32)
        wa = [sb("wa%d" % i, [128, 8, 512], F32) for i in range(2)]
        ba = sb("ba", [1, 6144], F32)
        one1 = sb("one1", [1, 1], F32)
        mrow = [sb("mrow%d" % r, [1, 6144], F32) for r in range(2)]
        pm = [es.enter_context(nc.psum_tensor(_u("pm%d" % i), [1, 512], F32)) for i in range(2)]
        S.dma('sp', lambda e: e.dma_start(out=ccs[:], in_=K.ccT), writes=['ccs'])
        S.dma('sp', lambda e: e.dma_start(out=ba[:], in_=K.b_ada), writes=['ba'])
        S.op('dve', lambda e: e.memset(one1[:], 1.0), writes=['one1'])
        S.op('act', lambda e: e.activation(out=scT[:], in_=ccs[:], func=AF.Silu), reads=['ccs'], writes=['scT'])
        for j in range(12):
            w = wa[j % 2]
            wk = 'wa%d' % (j % 2)
            S.dma('sp', lambda e: e.dma_start(out=w[:], in_=K.w_ada[:, j * 512:(j + 1) * 512].rearrange("(k p) n -> p k n", p=128)), writes=[wk])
            for r in range(2):
                if r == 1 and j >= 4:
                    continue
                pk = 'pm%d' % r
                for k in range(8):
                    S.op('pe', lambda e: e.matmul(pm[r][:], lhsT=scT[:, k, r:r + 1], rhs=w[:, k, :], start=(k == 0), stop=False),
                         reads=['scT', wk], writes=[pk], acc=True)
                S.op('pe', lambda e: e.matmul(pm[r][:], lhsT=one1[:], rhs=ba[:, j * 512:(j + 1) * 512], start=False, stop=True),
                     reads=['one1', 'ba'], writes=[pk], acc=True)
                S.op('dve', lambda e: e.tensor_copy(out=mrow[r][:, j * 512:(j + 1) * 512], in_=pm[r][:]), reads=[pk], writes=['mrow%d' % r])
        S.dma('sp', lambda e: e.dma_start(out=K.modd[0:1, :], in_=mrow[0][:]), reads=['mrow0'], writes=['modd'])
        S.dma('sp', lambda e: e.dma_start(out=K.modd[1:2, 0:2048], in_=mrow[1][:, 0:2048]), reads=['mrow1'], writes=['modd'])
        S.barrier()


def phase_inproj(K):
    nc, S = K.nc, K.S
    with ExitStack() as es:
        sb = lambda n, s, d: es.enter_context(nc.sbuf_tensor(_u(n), s, d))
        ps = lambda n, s, d: es.enter_context(nc.psum_tensor(_u(n), s, d))
        wb = sb("wb", [128, 8, 2560], BF16)
        wst = [sb("wst%d" % i, [128, 8, 320], F32) for i in range(2)]
        idt = sb("idt", [128, 128], BF16)
        epst = sb("epst", [128, 1], F32)
        scp = [sb("scp%d" % r, [128, 8], F32) for r in range(2)]
        shp = [sb("shp%d" % r, [128, 8], F32) for r in range(2)]
        xt = [sb("xt%d" % i, [128, 1024], F32) for i in range(2)]
        xn = [sb("xn%d" % i, [128, 1024], BF16) for i in range(2)]
        st = sb("st", [128, 2, 6], F32)
        mv = sb("mv", [128, 2], F32)
        lnv = sb("lnv", [128, 1], F32)
        rstd = sb("rstd", [128, 1], F32)
        xmT = [sb("xmT%d" % i, [128, 8, 512], BF16) for i in range(2)]
        stg = [sb("stg%d" % i, [128, 4, 512], F32) for i in range(2)]
        cst = sb("cst", [64, 512], F32)
        snt = sb("snt", [64, 512], F32)
        kr1 = sb("kr1", [64, 512], F32)
        kr2 = sb("kr2", [64, 512], F32)
        krb = sb("krb", [64, 512], BF16)
        pt = [ps("pt%d" % i, [128, 1024], BF16) for i in range(2)]
        po = [ps("po%d" % i, [128, 512], F32) for i in range(3)]
        pk = [ps("pk%d" % i, [64, 512], F32) for i in range(2)]

        S.dma('sp', lambda e: e.dma_start(out=idt[:], in_=K.ident), writes=['idt'])
        S.op('dve', lambda e: e.memset(epst[:], 1e-5), writes=['epst'])
        for r in range(2):
            S.dma('sp', lambda e: e.dma_start(out=shp[r][:], in_=K.modd[r:r + 1, 0:1024].rearrange("o (k p) -> p (o k)", p=128), allow_slow_non_contiguous=True), reads=['modd'], writes=['shp%d' % r])
            S.dma('sp', lambda e: e.dma_start(out=scp[r][:], in_=K.modd[r:r + 1, 1024:2048].rearrange("o (k p) -> p (o k)", p=128), allow_slow_non_contiguous=True), reads=['modd'], writes=['scp%d' % r])
            S.op('dve', lambda e: e.tensor_scalar_add(out=scp[r][:], in0=scp[r][:], scalar1=1.0), reads=['scp%d' % r], writes=['scp%d' % r])
        for j in range(8):
            w = wst[j % 2]
            wk = 'wst%d' % (j % 2)
            S.dma('sp', lambda e: e.dma_start(out=w[:], in_=K.w_in[:, j * 320:(j + 1) * 320].rearrange("(k p) n -> p k n", p=128)), writes=[wk])
            S.op('pool', lambda e: e.tensor_copy(out=wb[:, :, j * 320:(j + 1) * 320], in_=w[:]), reads=[wk], writes=['wb'])

        groups = [(0, 256, 1)] + [(256 + g * 512, 512, 0) for g in range(16)]
        tile_i = 0
        ev = 0
        for gi, (t0, G, r) in enumerate(groups):
            xm = xmT[gi % 2]
            xmk = 'xmT%d' % (gi % 2)
            if r == 0:
                S.dma('sp', lambda e: e.dma_start(out=cst[:], in_=K.cosT[:, t0 - 256:t0 - 256 + 512]), writes=['cst'])
                S.dma('sp', lambda e: e.dma_start(out=snt[:], in_=K.sinT[:, t0 - 256:t0 - 256 + 512]), writes=['snt'])
            for i in range(G // 128):
                sl = tile_i % 2
                tile_i += 1
                xk, xnk, ptk = 'xt%d' % sl, 'xn%d' % sl, 'pt%d' % sl
                tt = t0 + i * 128
                S.dma('sp', lambda e: e.dma_start(out=xt[sl][:], in_=K.xin[tt:tt + 128, :]), writes=[xk])
                for c in range(2):
                    S.op('dve', lambda e: e.bn_stats(out=st[:, c, :], in_=xt[sl][:, c * 512:(c + 1) * 512]), reads=[xk], writes=['st%d' % c])
                S.op('dve', lambda e: e.bn_aggr(out=mv[:], in_=st[:]), reads=['st0', 'st1'], writes=['mv'])
                S.op('act', lambda e: e.activation(out=lnv[:], in_=mv[:, 1:2], func=AF.Ln, bias=epst[:], scale=1.0), reads=['mv', 'epst'], writes=['lnv'])
                S.op('act', lambda e: e.activation(out=rstd[:], in_=lnv[:], func=AF.Exp, scale=-0.5), reads=['lnv'], writes=['rstd'])
                S.op('dve', lambda e: e.tensor_scalar(out=xn[sl][:], in0=xt[sl][:], scalar1=mv[:, 0:1], scalar2=rstd[:], op0=ALU.subtract, op1=ALU.mult),
                     reads=[xk, 'mv', 'rstd'], writes=[xnk])
                for k in range(8):
                    S.op('pe', lambda e: e.transpose(out=pt[sl][:, k * 128:(k + 1) * 128], in_=xn[sl][:, k * 128:(k + 1) * 128], identity=idt[:]),
                         reads=[xnk, 'idt'], writes=[ptk], acc=True)
                for k in range(8):
                    S.op('act', lambda e: e.activation(out=xm[:, k, i * 128:(i + 1) * 128], in_=pt[sl][:, k * 128:(k + 1) * 128], func=AF.Identity,
                                                       bias=shp[r][:, k:k + 1], scale=scp[r][:, k:k + 1]),
                         reads=[ptk, 'shp%d' % r, 'scp%d' % r], writes=[xmk])
            for cb in range(5):
                sg = stg[cb % 2]
                sgk = 'stg%d' % (cb % 2)
                ncols = 4 if cb < 4 else 3
                for cc in range(ncols):
                    ci = cb * 4 + cc
                    p = po[ev % 3]
                    pkk = 'po%d' % (ev % 3)
                    for k in range(8):
                        S.op('pe', lambda e: e.matmul(p[:, 0:G], lhsT=wb[:, k, ci * 128:(ci + 1) * 128], rhs=xm[:, k, 0:G], start=(k == 0), stop=(k == 7)),
                             reads=['wb', xmk], writes=[pkk], acc=True)
                    if ev % 2 == 0:
                        S.op('dve', lambda e: e.tensor_copy(out=sg[:, cc, 0:G], in_=p[:, 0:G]), reads=[pkk], writes=[sgk])
                    else:
                        S.op('act', lambda e: e.activation(out=sg[:, cc, 0:G], in_=p[:, 0:G], func=AF.Identity), reads=[pkk], writes=[sgk])
                    ev += 1
                r0 = cb * 512
                S.dma('pool', lambda e: e.dma_start(out=K.hT[r0:r0 + ncols * 128, t0:t0 + G].rearrange("(c p) t -> p c t", p=128), in_=sg[:, 0:ncols, 0:G]),
                      reads=[sgk], writes=['hT'])
            for q in range(2):
                for k in range(8):
                    S.op('pe', lambda e: e.matmul(pk[q][:, 0:G], lhsT=wb[:, k, 2432 + q * 64:2432 + (q + 1) * 64], rhs=xm[:, k, 0:G], start=(k == 0), stop=(k == 7)),
                         reads=['wb', xmk], writes=['pk%d' % q], acc=True)
            if r == 0:
                S.op('dve', lambda e: e.tensor_tensor(out=kr1[:], in0=pk[0][:], in1=cst[:], op=ALU.mult), reads=['pk0', 'cst'], writes=['kr1'])
                S.op('dve', lambda e: e.tensor_tensor(out=kr2[:], in0=pk[1][:], in1=snt[:], op=ALU.mult), reads=['pk1', 'snt'], writes=['kr2'])
                S.op('dve', lambda e: e.tensor_tensor(out=krb[:], in0=kr1[:], in1=kr2[:], op=ALU.add), reads=['kr1', 'kr2'], writes=['krb'])
            else:
                S.op('dve', lambda e: e.tensor_copy(out=krb[:, 0:G], in_=pk[0][:, 0:G]), reads=['pk0', 'pk1'], writes=['krb'])
            S.dma('pool', lambda e: e.dma_start(out=K.krT[:, t0:t0 + G], in_=krb[:, 0:G]), reads=['krb'], writes=['krT'])
        S.barrier()


class Rot:
    def __init__(self, bufs, prefix):
        self.bufs = bufs
        self.prefix = prefix
        self.i = 0

    def get(self):
        j = self.i % len(self.bufs)
        self.i += 1
        return self.bufs[j], '%s%d' % (self.prefix, j)


SCALE_ATT = 192.0 ** -0.5


def phase_mlaprep(K):
    nc, S = K.nc, K.S
    with ExitStack() as es:
        sb = lambda n, s, d: es.enter_context(nc.sbuf_tensor(_u(n), s, d))
        ps = lambda n, s, d: es.enter_context(nc.psum_tensor(_u(n), s, d))
        wuq = sb("wuq", [128, 2, 1024], BF16)
        wuk = sb("wuk", [128, 2, 512], BF16)
        wuv = sb("wuv", [128, 2, 512], BF16)
        wst = [sb("mwst%d" % i, [128, 2, 512], F32) for i in range(2)]
        gq = sb("gq", [128, 2], F32)
        gkv = sb("gkv", [128, 2], F32)
        onesf = sb("onesf", [128, 128], F32)
        epst = sb("epst", [128, 1], F32)
        ql = [sb("ql%d" % i, [128, 4, 512], F32) for i in range(2)]
        sq = sb("sq", [128, 4, 512], F32)
        lnt = sb("lnt", [128, 512], F32)
        rs = [sb("rs%d" % i, [128, 512], F32) for i in range(2)]
        nb = [sb("nb%d" % i, [128, 4, 512], BF16) for i in range(2)]
        qst = [sb("qst%d" % i, [128, 4, 512], BF16) for i in range(2)]
        kst = [sb("kst%d" % i, [128, 4, 512], BF16) for i in range(2)]
        qrb = [sb("qrb%d" % i, [64, 4, 512], BF16) for i in range(2)]
        vst = Rot([sb("vst%d" % i, [128, 512], BF16) for i in range(3)], "vst")
        cst = sb("cst", [64, 512], F32)
        snt = sb("snt", [64, 512], F32)
        r1 = sb("r1", [64, 512], F32)
        r2 = sb("r2", [64, 512], F32)
        pp = Rot([ps("mp%d" % i, [128, 512], F32) for i in range(7)], "mp")

        S.op('dve', lambda e: e.memset(epst[:], 1e-6), writes=['epst'])
        S.op('dve', lambda e: e.memset(onesf[:], 1.0), writes=['onesf'])
        S.dma('sp', lambda e: e.dma_start(out=gq[:], in_=K.mla_q_norm.rearrange("o (c p) -> p (o c)", p=128), allow_slow_non_contiguous=True), writes=['gq'])
        S.dma('sp', lambda e: e.dma_start(out=gkv[:], in_=K.mla_kv_norm.rearrange("o (c p) -> p (o c)", p=128), allow_slow_non_contiguous=True), writes=['gkv'])
        wl = 0
        for (src, dst, dk, n) in [(K.w_uq, wuq, 'wuq', 1024), (K.w_uk, wuk, 'wuk', 512), (K.w_uv, wuv, 'wuv', 512)]:
            for j in range(n // 512):
                w = wst[wl % 2]
                wk = 'mwst%d' % (wl % 2)
                wl += 1
                S.dma('sp', lambda e: e.dma_start(out=w[:], in_=src[:, j * 512:(j + 1) * 512].rearrange("(c p) n -> p c n", p=128)), writes=[wk])
                S.op('pool', lambda e: e.tensor_copy(out=dst[:, :, j * 512:(j + 1) * 512], in_=w[:]), reads=[wk], writes=[dk])

        groups = [(0, 256, 1)] + [(256 + g * 512, 512, 0) for g in range(16)]
        ev = [0]

        def evac(out_ap, in_ap, reads, writes):
            if ev[0] % 2 == 0:
                S.op('dve', lambda e: e.tensor_copy(out=out_ap, in_=in_ap), reads=reads, writes=writes)
            else:
                S.op('act', lambda e: e.activation(out=out_ap, in_=in_ap, func=AF.Identity), reads=reads, writes=writes)
            ev[0] += 1

        for gi, (t0, G, r) in enumerate(groups):
            q_ = ql[gi % 2]
            qk = 'ql%d' % (gi % 2)
            n_ = nb[gi % 2]
            nk = 'nb%d' % (gi % 2)
            S.dma('sp', lambda e: e.dma_start(out=q_[:, :, 0:G], in_=K.hT[1920:2432, t0:t0 + G].rearrange("(c p) t -> p c t", p=128)), reads=['hT'], writes=[qk])
            if r == 0:
                S.dma('sp', lambda e: e.dma_start(out=cst[:], in_=K.cosT[:, t0 - 256:t0 - 256 + 512]), writes=['cst'])
                S.dma('sp', lambda e: e.dma_start(out=snt[:], in_=K.sinT[:, t0 - 256:t0 - 256 + 512]), writes=['snt'])
            S.op('act', lambda e: e.activation(out=sq[:, :, 0:G], in_=q_[:, :, 0:G], func=AF.Square), reads=[qk], writes=['sq'])
            for pair in range(2):
                if pair == 0 and r == 1:
                    continue
                p, pk = pp.get()
                for c in range(2):
                    S.op('pe', lambda e: e.matmul(p[:, 0:G], lhsT=onesf[:], rhs=sq[:, pair * 2 + c, 0:G], start=(c == 0), stop=(c == 1)),
                         reads=['onesf', 'sq'], writes=[pk], acc=True)
                S.op('act', lambda e: e.activation(out=lnt[:, 0:G], in_=p[:, 0:G], func=AF.Ln, bias=epst[:], scale=1.0 / 256.0), reads=[pk, 'epst'], writes=['lnt'])
                S.op('act', lambda e: e.activation(out=rs[pair][:, 0:G], in_=lnt[:, 0:G], func=AF.Exp, scale=-0.5), reads=['lnt'], writes=['rs%d' % pair])
                g_ = gq if pair == 0 else gkv
                for c in range(2):
                    S.op('dve', lambda e: e.scalar_tensor_tensor(out=n_[:, pair * 2 + c, 0:G], in0=q_[:, pair * 2 + c, 0:G], scalar=g_[:, c:c + 1], in1=rs[pair][:, 0:G],
                                                                 op0=ALU.mult, op1=ALU.mult),
                         reads=[qk, 'rs%d' % pair, 'gq', 'gkv'], writes=[nk])
            if r == 0:
                tq = t0 - 256
                qs = qst[gi % 2]
                qsk = 'qst%d' % (gi % 2)
                qr_ = qrb[gi % 2]
                qrk = 'qrb%d' % (gi % 2)
                for h in range(4):
                    p, pk = pp.get()
                    for c in range(2):
                        S.op('pe', lambda e: e.matmul(p[:, 0:G], lhsT=wuq[:, c, h * 256:h * 256 + 128], rhs=n_[:, c, 0:G], start=(c == 0), stop=(c == 1)),
                             reads=['wuq', nk], writes=[pk], acc=True)
                    evac(qs[:, h, 0:G], p[:, 0:G], [pk], [qsk])
                    p1, pk1 = pp.get()
                    p2, pk2 = pp.get()
                    for c in range(2):
                        S.op('pe', lambda e: e.matmul(p1[0:64, 0:G], lhsT=wuq[:, c, h * 256 + 128:h * 256 + 192], rhs=n_[:, c, 0:G], start=(c == 0), stop=(c == 1)),
                             reads=['wuq', nk], writes=[pk1], acc=True)
                    for c in range(2):
                        S.op('pe', lambda e: e.matmul(p2[0:64, 0:G], lhsT=wuq[:, c, h * 256 + 192:h * 256 + 256], rhs=n_[:, c, 0:G], start=(c == 0), stop=(c == 1)),
                             reads=['wuq', nk], writes=[pk2], acc=True)
                    S.op('dve', lambda e: e.tensor_tensor(out=r1[:], in0=p1[0:64, :], in1=cst[:], op=ALU.mult), reads=[pk1, 'cst'], writes=['r1'])
                    S.op('dve', lambda e: e.tensor_tensor(out=r2[:], in0=p2[0:64, :], in1=snt[:], op=ALU.mult), reads=[pk2, 'snt'], writes=['r2'])
                    S.op('dve', lambda e: e.tensor_tensor(out=qr_[:, h, :], in0=r1[:], in1=r2[:], op=ALU.add), reads=['r1', 'r2'], writes=[qrk])
                S.dma('pool', lambda e: e.dma_start(out=K.qnT[:, tq:tq + G].rearrange("(h p) t -> p h t", p=128), in_=qs[:, :, 0:G]), reads=[qsk], writes=['qnT'])
                S.dma('pool', lambda e: e.dma_start(out=K.qrT[:, tq:tq + G].rearrange("(h p) t -> p h t", p=64), in_=qr_[:, :, 0:G]), reads=[qrk], writes=['qrT'])
            ks = kst[gi % 2]
            ksk = 'kst%d' % (gi % 2)
            for h in range(4):
                p, pk = pp.get()
                for c in range(2):
                    S.op('pe', lambda e: e.matmul(p[:, 0:G], lhsT=wuk[:, c, h * 128:(h + 1) * 128], rhs=n_[:, 2 + c, 0:G], start=(c == 0), stop=(c == 1)),
                         reads=['wuk', nk], writes=[pk], acc=True)
                evac(ks[:, h, 0:G], p[:, 0:G], [pk], [ksk])
            S.dma('pool', lambda e: e.dma_start(out=K.knT[:, t0:t0 + G].rearrange("(h p) t -> p h t", p=128), in_=ks[:, :, 0:G]), reads=[ksk], writes=['knT'])
            for i in range(G // 128):
                p, pk = pp.get()
                for c in range(2):
                    S.op('pe', lambda e: e.matmul(p[:, :], lhsT=n_[:, 2 + c, i * 128:(i + 1) * 128], rhs=wuv[:, c, :], start=(c == 0), stop=(c == 1)),
                         reads=['wuv', nk], writes=[pk], acc=True)
                v_, vk = vst.get()
                evac(v_[:], p[:], [pk], [vk])
                S.dma('pool', lambda e: e.dma_start(out=K.vtok[t0 + i * 128:t0 + (i + 1) * 128, :], in_=v_[:]), reads=[vk], writes=['vtok'])
        S.barrier()


def phase_attn(K, heads=(0, 1, 2, 3), nqt=16):
    nc, S = K.nc, K.S
    NKT = TA // 128
    with ExitStack() as es:
        sb = lambda n, s, d: es.enter_context(nc.sbuf_tensor(_u(n), s, d))
        ps = lambda n, s, d: es.enter_context(nc.psum_tensor(_u(n), s, d))
        krs = sb("krs", [64, TA], BF16)
        kn = [sb("kn%d" % i, [128, TA], BF16) for i in range(2)]
        vh = [sb("vh%d" % i, [128, NKT, 128], BF16) for i in range(2)]
        qn = [sb("qn%d" % i, [128, 512], BF16) for i in range(2)]
        qr = [sb("qr%d" % i, [64, 512], BF16) for i in range(2)]
        onesb = sb("onesb", [128, 128], BF16)
        pT = Rot([sb("pT%d" % i, [128, 512], BF16) for i in range(3)], "pT")
        rl = sb("rl", [128, 512], F32)
        ob = [sb("ob%d" % i, [128, 512], BF16) for i in range(2)]
        psc = Rot([ps("psc%d" % i, [128, 512], F32) for i in range(3)], "psc")
        pO = [ps("pO%d" % i, [128, 512], F32) for i in range(2)]
        pL = [ps("pL%d" % i, [128, 512], F32) for i in range(2)]

        S.op('dve', lambda e: e.memset(onesb[:], 1.0), writes=['onesb'])
        S.dma('sp', lambda e: e.dma_start(out=krs[:], in_=K.krT), reads=['krT'], writes=['krs'])
        qi = 0
        for hi, h in enumerate(heads):
            k_ = kn[hi % 2]
            kk = 'kn%d' % (hi % 2)
            v_ = vh[hi % 2]
            vk = 'vh%d' % (hi % 2)
            S.dma('sp', lambda e: e.dma_start(out=k_[:], in_=K.knT[h * 128:(h + 1) * 128, :]), reads=['knT'], writes=[kk])
            S.dma('sp', lambda e: e.dma_start(out=v_[:], in_=K.vtok[:, h * 128:(h + 1) * 128].rearrange("(kt p) d -> p kt d", p=128)), reads=['vtok'], writes=[vk])
            for qt in range(nqt):
                sl = qi % 2
                qi += 1
                qnk, qrk = 'qn%d' % sl, 'qr%d' % sl
                S.dma('sp', lambda e: e.dma_start(out=qn[sl][:], in_=K.qnT[h * 128:(h + 1) * 128, qt * 512:(qt + 1) * 512]), reads=['qnT'], writes=[qnk])
                S.dma('sp', lambda e: e.dma_start(out=qr[sl][:], in_=K.qrT[h * 64:(h + 1) * 64, qt * 512:(qt + 1) * 512]), reads=['qrT'], writes=[qrk])
                Ok, Lk = 'pO%d' % sl, 'pL%d' % sl
                pend = []

                def scores(kt):
                    p, pk = psc.get()
                    S.op('pe', lambda e: e.matmul(p[:], lhsT=k_[:, kt * 128:(kt + 1) * 128], rhs=qn[sl][:], start=True, stop=False),
                         reads=[kk, qnk], writes=[pk], acc=True)
                    S.op('pe', lambda e: e.matmul(p[:], lhsT=krs[:, kt * 128:(kt + 1) * 128], rhs=qr[sl][:], start=False, stop=True),
                         reads=['krs', qrk], writes=[pk], acc=True)
                    t_, tk = pT.get()
                    S.op('act', lambda e: e.activation(out=t_[:], in_=p[:], func=AF.Exp, scale=SCALE_ATT), reads=[pk], writes=[tk])
                    pend.append((kt, t_, tk))

                def pv():
                    kt, t_, tk = pend.pop(0)
                    S.op('pe', lambda e: e.matmul(pO[sl][:], lhsT=v_[:, kt, :], rhs=t_[:], start=(kt == 0), stop=(kt == NKT - 1)),
                         reads=[vk, tk], writes=[Ok], acc=True)
                    S.op('pe', lambda e: e.matmul(pL[sl][:], lhsT=onesb[:], rhs=t_[:], start=(kt == 0), stop=(kt == NKT - 1)),
                         reads=['onesb', tk], writes=[Lk], acc=True)

                scores(0)
                scores(1)
                for kt in range(NKT):
                    pv()
                    if kt + 2 < NKT:
                        scores(kt + 2)
                S.op('dve', lambda e: e.reciprocal(out=rl[:], in_=pL[sl][:]), reads=[Lk], writes=['rl'])
                S.op('dve', lambda e: e.tensor_tensor(out=ob[sl][:], in0=pO[sl][:], in1=rl[:], op=ALU.mult), reads=[Ok, 'rl'], writes=['ob%d' % sl])
                S.dma('pool', lambda e: e.dma_start(out=K.mixT[512 + h * 128:512 + (h + 1) * 128, qt * 512:(qt + 1) * 512], in_=ob[sl][:]), reads=['ob%d' % sl], writes=['mixT'])
        S.barrier()


F32R = mybir.dt.float32r
LDS = -0.6065306597126334


def phase_rwprep(K):
    nc, S = K.nc, K.S
    with ExitStack() as es:
        sb = lambda n, s, d: es.enter_context(nc.sbuf_tensor(_u(n), s, d))
        ps = lambda n, s, d: es.enter_context(nc.psum_tensor(_u(n), s, d))
        G = 256
        cw = sb("cw", [128, 3, 12], F32)
        kkv = sb("kkv", [128, 4], F32)
        kav = sb("kav", [128, 4], F32)
        omka = sb("omka", [128, 4], F32)
        rkv = sb("rkv", [128, 4], F32)
        a0v = sb("a0v", [128, 2, 4], F32)
        w0r = sb("w0r", [1, 2, 512], F32)
        ones1 = sb("ones1", [1, 128], F32)
        wup = sb("wup", [64, 2, 512], F32)
        aup = sb("aup", [64, 2, 512], F32)
        gup = sb("gup", [128, 512], F32)
        bones = sb("bones", [128, 128], F32)
        idf = sb("idf", [128, 128], F32)
        eps12 = sb("eps12", [128, 1], F32)
        hr = [sb("hr%d" % i, [128, 12, G + 2], F32) for i in range(2)]
        lo = [sb("lo%d" % i, [64, 4, G], F32) for i in range(2)]
        gd = [sb("gd%d" % i, [128, G], F32) for i in range(2)]
        cv = sb("cv", [128, 12, G], F32)
        tw = sb("tw", [64, 2, G], F32)
        sg = sb("sg", [128, G], F32)
        kq = sb("kq", [128, 4, G], F32)
        sq = sb("sq", [128, 4, G], F32)
        lnt = sb("lnt", [128, 4, G], F32)
        kk_ = sb("kk_", [128, 4, G], F32)
        av = sb("av", [128, 4, G], F32)
        tt = sb("tt", [128, 4, G], F32)
        kd = [sb("kd%d" % i, [128, 4, G], F32) for i in range(2)]
        bb = sb("bb", [128, 4, G], F32)
        ld = Rot([sb("ld%d" % i, [128, 512], F32) for i in range(2)], "ld")
        vt = Rot([sb("vt%d" % i, [128, 512], F32) for i in range(2)], "vt")
        gg = sb("gg", [128, 4, G], F32)
        bc = sb("bc", [128, 4, G], F32)
        bon = sb("bon", [128, 4, G], F32)
        pp = Rot([ps("rp%d" % i, [128, 512], F32) for i in range(7)], "rp")

        ld1 = lambda dst, src, key: S.dma('sp', lambda e: e.dma_start(out=dst, in_=src, allow_slow_non_contiguous=True), writes=[key])
        ld1(cw[:], K.rwkv_conv.rearrange("t (c p) -> p t c", p=128), 'cw')
        ld1(kkv[:], K.rwkv_k_k.rearrange("o (c p) -> p (o c)", p=128), 'kkv')
        ld1(kav[:], K.rwkv_k_a.rearrange("o (c p) -> p (o c)", p=128), 'kav')
        ld1(rkv[:], K.rwkv_r_k.rearrange("o (c p) -> p (o c)", p=128), 'rkv')
        ld1(a0v[:], K.rwkv_a0.rearrange("d (c p) -> p d c", p=128), 'a0v')
        ld1(w0r[:], K.rwkv_w0.rearrange("(o d) n -> o d n", o=1), 'w0r')
        ld1(wup[:], K.rwkv_w_up.rearrange("d l n -> l d n"), 'wup')
        ld1(aup[:], K.rwkv_a_up.rearrange("d l n -> l d n"), 'aup')
        ld1(gup[:], K.rwkv_g_up, 'gup')
        ld1(bones[:], K.bones, 'bones')
        ld1(idf[:], K.identf, 'idf')
        S.op('dve', lambda e: e.memset(ones1[:], 1.0), writes=['ones1'])
        S.op('dve', lambda e: e.memset(eps12[:], 1e-12), writes=['eps12'])
        S.op('dve', lambda e: e.tensor_scalar(out=omka[:], in0=kav[:], scalar1=-1.0, scalar2=1.0, op0=ALU.mult, op1=ALU.add), reads=['kav'], writes=['omka'])

        nblk = TA // G
        for bi in range(nblk):
            t0 = bi * G
            lat = t0 >= TC
            sl = bi % 2
            h_ = hr[sl]
            hk = 'hr%d' % sl
            first = (t0 == 0 or t0 == TC)
            last = (t0 + G == TC or t0 + G == TA)
            c0 = 1 if first else 0
            c1 = G + 1 if last else G + 2
            if first:
                S.op('pool', lambda e: e.memset(h_[:, :, 0:1], 0.0), writes=[hk])
            if last:
                S.op('pool', lambda e: e.memset(h_[:, :, G + 1:G + 2], 0.0), writes=[hk])
            for q in range(3):
                S.dma('sp', lambda e: e.dma_start(out=h_[:, q * 4:(q + 1) * 4, c0:c1], in_=K.hT[q * 512:(q + 1) * 512, t0 - 1 + c0:t0 - 1 + c1].rearrange("(c p) t -> p c t", p=128)),
                      reads=['hT'], writes=[hk])
            lo_ = lo[sl]
            lk = 'lo%d' % sl
            S.dma('sp', lambda e: e.dma_start(out=lo_[:], in_=K.hT[1536:1792, t0:t0 + G].rearrange("(c p) t -> p c t", p=64)), reads=['hT'], writes=[lk])
            gd_ = gd[sl]
            gk = 'gd%d' % sl
            if lat:
                S.dma('sp', lambda e: e.dma_start(out=gd_[:], in_=K.hT[1792:1920, t0:t0 + G]), reads=['hT'], writes=[gk])
            for c in range(12):
                S.op('act', lambda e: e.activation(out=cv[:, c, :], in_=h_[:, c, 1:G + 1], func=AF.Identity, scale=cw[:, 1, c:c + 1]), reads=[hk, 'cw'], writes=['cv%d' % c])
                S.op('dve', lambda e: e.scalar_tensor_tensor(out=cv[:, c, :], in0=h_[:, c, 0:G], scalar=cw[:, 0, c:c + 1], in1=cv[:, c, :], op0=ALU.mult, op1=ALU.add),
                     reads=[hk, 'cw', 'cv%d' % c], writes=['cv%d' % c])
                S.op('dve', lambda e: e.scalar_tensor_tensor(out=cv[:, c, :], in0=h_[:, c, 2:G + 2], scalar=cw[:, 2, c:c + 1], in1=cv[:, c, :], op0=ALU.mult, op1=ALU.add),
                     reads=[hk, 'cw', 'cv%d' % c], writes=['cv%d' % c])
            cvr = ['cv%d' % c for c in range(0, 4)]
            cvk = ['cv%d' % c for c in range(4, 8)]
            cvv = ['cv%d' % c for c in range(8, 12)]
            S.dma('pool', lambda e: e.dma_start(out=K.rwR[:, t0:t0 + G].rearrange("(c p) t -> p c t", p=128), in_=cv[:, 0:4, :]), reads=cvr, writes=['rwR'])
            S.dma('pool', lambda e: e.dma_start(out=K.rwV[:, t0:t0 + G].rearrange("(c p) t -> p c t", p=128), in_=cv[:, 8:12, :]), reads=cvv, writes=['rwV'])
            for i in range(G // 128):
                p, pk = pp.get()
                for c in range(4):
                    S.op('pe', lambda e: e.transpose(out=p[:, c * 128:(c + 1) * 128], in_=cv[:, 8 + c, i * 128:(i + 1) * 128], identity=idf[:]), reads=cvv + ['idf'], writes=[pk], acc=True)
                v_, vk = vt.get()
                S.op('act', lambda e: e.activation(out=v_[:], in_=p[:], func=AF.Identity), reads=[pk], writes=[vk])
                S.dma('pool', lambda e: e.dma_start(out=K.rwVtok[t0 + i * 128:t0 + (i + 1) * 128, :], in_=v_[:]), reads=[vk], writes=['rwVtok'])
            for c in range(4):
                S.op('dve', lambda e: e.tensor_scalar_mul(out=kq[:, c, :], in0=cv[:, 4 + c, :], scalar1=kkv[:, c:c + 1]), reads=cvk + ['kkv'], writes=['kq'])
            S.op('act', lambda e: e.activation(out=sq[:], in_=kq[:], func=AF.Square), reads=['kq'], writes=['sq'])
            for c2 in range(2):
                p, pk = pp.get()
                S.op('pe', lambda e: e.matmul(p[:, 0:2 * G], lhsT=bones[:], rhs=sq[:, 2 * c2:2 * c2 + 2, :], start=True, stop=True), reads=['bones', 'sq'], writes=[pk])
                S.op('act', lambda e: e.activation(out=lnt[:, 2 * c2:2 * c2 + 2, :], in_=p[:, 0:2 * G], func=AF.Ln, bias=eps12[:], scale=1.0), reads=[pk, 'eps12'], writes=['lnt'])
            S.op('act', lambda e: e.activation(out=lnt[:], in_=lnt[:], func=AF.Exp, scale=-0.5), reads=['lnt'], writes=['lnt'])
            S.op('dve', lambda e: e.tensor_tensor(out=kk_[:], in0=kq[:], in1=lnt[:], op=ALU.mult), reads=['kq', 'lnt'], writes=['kk_'])
            S.dma('pool', lambda e: e.dma_start(out=K.rwKK[:, t0:t0 + G].rearrange("(c p) t -> p c t", p=128), in_=kk_[:]), reads=['kk_'], writes=['rwKK'])
            S.op('act', lambda e: e.activation(out=tw[:], in_=lo_[:, 0:2, :], func=AF.Tanh), reads=[lk], writes=['tw'])
            for d in range(2):
                for i in range(G // 128):
                    p, pk = pp.get()
                    S.op('pe', lambda e: e.matmul(p[:], lhsT=tw[:, d, i * 128:(i + 1) * 128], rhs=wup[:, d, :], start=True, stop=False), reads=['tw', 'wup'], writes=[pk], acc=True)
                    S.op('pe', lambda e: e.matmul(p[:], lhsT=ones1[:], rhs=w0r[:, d, :], start=False, stop=True), reads=['ones1', 'w0r'], writes=[pk], acc=True)
                    l_, lk2 = ld.get()
                    S.op('act', lambda e: e.activation(out=l_[:], in_=p[:], func=AF.Sigmoid), reads=[pk], writes=[lk2])
                    S.dma('pool', lambda e: e.dma_start(out=K.rwLD[d][t0 + i * 128:t0 + (i + 1) * 128, :], in_=l_[:]), reads=[lk2], writes=['rwLD%d' % d])
                for c in range(4):
                    p, pk = pp.get()
                    S.op('pe', lambda e: e.matmul(p[:, 0:G], lhsT=aup[:, d, c * 128:(c + 1) * 128], rhs=lo_[:, 2 + d, :], start=True, stop=True), reads=['aup', lk], writes=[pk])
                    S.op('act', lambda e: e.activation(out=av[:, c, :], in_=p[:, 0:G], func=AF.Sigmoid, bias=a0v[:, d, c:c + 1], scale=1.0), reads=[pk, 'a0v'], writes=['av'])
                    S.op('dve', lambda e: e.tensor_scalar(out=tt[:, c, :], in0=av[:, c, :], scalar1=kav[:, c:c + 1], scalar2=omka[:, c:c + 1], op0=ALU.mult, op1=ALU.add),
                         reads=['av', 'kav', 'omka'], writes=['tt'])
                S.op('dve', lambda e: e.tensor_tensor(out=kd[d][:], in0=cv[:, 4:8, :], in1=tt[:], op=ALU.mult), reads=cvk + ['tt'], writes=['kd%d' % d])
                S.op('dve', lambda e: e.tensor_tensor(out=bb[:], in0=kk_[:], in1=av[:], op=ALU.mult), reads=['kk_', 'av'], writes=['bb'])
                S.dma('pool', lambda e: e.dma_start(out=K.rwKD[d][:, t0:t0 + G].rearrange("(c p) t -> p c t", p=128), in_=kd[d][:]), reads=['kd%d' % d], writes=['rwKD%d' % d])
                S.dma('pool', lambda e: e.dma_start(out=K.rwB[d][:, t0:t0 + G].rearrange("(c p) t -> p c t", p=128), in_=bb[:]), reads=['bb'], writes=['rwB%d' % d])
            if lat:
                tl = t0 - TC
                S.op('act', lambda e: e.activation(out=sg[:], in_=gd_[:], func=AF.Sigmoid), reads=[gk], writes=['sg'])
                for c in range(4):
                    p, pk = pp.get()
                    S.op('pe', lambda e: e.matmul(p[:, 0:G], lhsT=gup[:, c * 128:(c + 1) * 128], rhs=sg[:], start=True, stop=True), reads=['gup', 'sg'], writes=[pk])
                    S.op('act', lambda e: e.activation(out=gg[:, c, :], in_=p[:, 0:G], func=AF.Identity), reads=[pk], writes=['gg'])
                S.dma('pool', lambda e: e.dma_start(out=K.rwG[:, tl:tl + G].rearrange("(c p) t -> p c t", p=128), in_=gg[:]), reads=['gg'], writes=['rwG'])
                S.op('dve', lambda e: e.tensor_tensor(out=bc[:], in0=kd[0][:], in1=kd[1][:], op=ALU.add), reads=['kd0', 'kd1'], writes=['bc'])
                S.op('dve', lambda e: e.tensor_tensor(out=bc[:], in0=bc[:], in1=cv[:, 0:4, :], op=ALU.mult), reads=['bc'] + cvr, writes=['bc'])
                for c in range(4):
                    S.op('dve', lambda e: e.tensor_scalar_mul(out=bc[:, c, :], in0=bc[:, c, :], scalar1=rkv[:, c:c + 1]), reads=['bc', 'rkv'], writes=['bc'])
                for c2 in range(2):
                    p, pk = pp.get()
                    S.op('pe', lambda e: e.matmul(p[:, 0:2 * G], lhsT=bones[:], rhs=bc[:, 2 * c2:2 * c2 + 2, :], start=True, stop=True), reads=['bones', 'bc'], writes=[pk])
                    S.op('dve', lambda e: e.tensor_tensor(out=bon[:, 2 * c2:2 * c2 + 2, :], in0=p[:, 0:2 * G], in1=cv[:, 8 + 2 * c2:8 + 2 * c2 + 2, :], op=ALU.mult), reads=[pk] + cvv, writes=['bon'])
                S.dma('pool', lambda e: e.dma_start(out=K.rwBON[:, tl:tl + G].rearrange("(c p) t -> p c t", p=128), in_=bon[:]), reads=['bon'], writes=['rwBON'])
        S.barrier()


def rw_consts():
    i = np.arange(128)[:, None]
    t = np.arange(128)[None, :]
    bd = (i // 64) == (t // 64)
    out = {}
    blk = ((i // 64) == (t // 64)).astype(np.float32)
    for d in range(2):
        strict = (bd & ((i < t) if d == 0 else (i > t))).astype(np.float32)
        incl = (bd & ((i <= t) if d == 0 else (i >= t))).astype(np.float32)
        m1 = np.concatenate([-strict, incl, blk], 1)
        m2 = np.concatenate([incl, blk], 1)
        m3 = np.concatenate([-strict.T, -strict.T, -np.ones((128, 64), np.float32)], 1)
        cum = np.float32(LDS) * np.concatenate([incl, strict, strict.T], 1)
        out['rwm%d' % d] = np.ascontiguousarray(np.concatenate([m1, m2, m3, cum], 1).astype(np.float32))
    out['i64dbl'] = np.ascontiguousarray((np.arange(128)[:, None] % 64 == np.arange(128)[None, :] % 64).astype(np.float32))
    out['bones'] = np.ascontiguousarray(bd.astype(np.float32))
    out['identf'] = np.eye(128, dtype=np.float32)
    return out


GN_EPS = 64e-5


def phase_rwscan(K, ntile_lat=64):
    nc, S = K.nc, K.S
    G = 128
    with ExitStack() as es:
        sb = lambda n, s, d: es.enter_context(nc.sbuf_tensor(_u(n), s, d))
        ps = lambda n, s, d: es.enter_context(nc.psum_tensor(_u(n), s, d))
        mk = sb("mk", [128, 1344], F32)
        idr = sb("idr", [128, 128], F32R)
        i64r = sb("i64r", [128, 128], F32R)
        i64f = sb("i64f", [128, 128], F32)
        idf = sb("idf", [128, 128], F32)
        o64 = sb("o64", [64, 64], F32)
        gng = sb("gng", [64, 8], F32)
        gnb = sb("gnb", [64, 8], F32)
        epsg = sb("epsg", [64, 1], F32)
        raw = [[sb("raw%d_%d" % (j, i), [128, 4, G], F32) for i in range(2)] for j in range(4)]
        vraw = [sb("vraw%d" % i, [128, 512], F32) for i in range(2)]
        ldt = [sb("ldt%d" % i, [128, 512], F32) for i in range(2)]
        vtr = sb("vtr", [128, 512], F32R)
        Ep = sb("Ep", [128, 4, G], F32)
        Em = sb("Em", [128, 4, G], F32)
        Ex = sb("Ex", [128, 4, G], F32)
        Ea = sb("Ea", [128, 4, G], F32)
        KRG = sb("KRG", [128, 4, 3, 128], F32R)
        BK = sb("BK", [128, 4, 2, 128], F32R)
        KB2 = sb("KB2", [128, 4, 2, 128], F32R)
        NS = 4
        La = [sb("La%d" % i, [128, 384], F32R) for i in range(NS)]
        Lb = [sb("Lb%d" % i, [128, 384], F32R) for i in range(NS)]
        ATa = [sb("ATa%d" % i, [128, 128], F32R) for i in range(NS)]
        ATb = [sb("ATb%d" % i, [128, 128], F32R) for i in range(NS)]
        X1 = [sb("X1%d" % i, [128, 320], F32R) for i in range(NS)]
        X2 = [sb("X2%d" % i, [128, 256], F32R) for i in range(NS)]
        Xf = [sb("Xf%d" % i, [128, 256], F32R) for i in range(NS)]
        QG1 = sb("QG1", [64, 8, 256], F32R)
        QG2 = sb("QG2", [128, 8, 256], F32R)
        Sth = sb("Sth", [64, 3, 8, 64], F32R)
        T = [sb("T%d" % i, [64, 8, G], F32) for i in range(4)]
        outb = sb("outb", [64, 8, G], BF16)
        pp = Rot([ps("sp%d" % i, [128, 512], F32) for i in range(7)], "sp")
        pst = ps("pst", [64, 512], F32)

        ld1 = lambda dst, src, key: S.dma('sp', lambda e: e.dma_start(out=dst, in_=src, allow_slow_non_contiguous=True), writes=[key])
        ld1(idf[:], K.identf, 'idf')
        ld1(i64f[:], K.i64dbl, 'i64f')
        ld1(gng[:], K.rwkv_gn_g.rearrange("o (h p) -> p (o h)", p=64), 'gng')
        ld1(gnb[:], K.rwkv_gn_b.rearrange("o (h p) -> p (o h)", p=64), 'gnb')
        S.op('dve', lambda e: e.tensor_copy(out=idr[:], in_=idf[:]), reads=['idf'], writes=['idr'])
        S.op('dve', lambda e: e.tensor_copy(out=i64r[:], in_=i64f[:]), reads=['i64f'], writes=['i64r'])
        S.op('dve', lambda e: e.memset(o64[:], 1.0 / 64.0), writes=['o64'])
        S.op('dve', lambda e: e.memset(epsg[:], GN_EPS), writes=['epsg'])
        zt = sb("zt", [64, 512], F32)
        S.op('dve', lambda e: e.memset(zt[:], 0.0), writes=['zt'])

        evq = [0]

        def evac(out_ap, in_ap, reads, writes):
            if evq[0] % 2 == 0:
                S.op('act', lambda e: e.activation(out=out_ap, in_=in_ap, func=AF.Identity), reads=reads, writes=writes)
            else:
                S.op('dve', lambda e: e.tensor_copy(out=out_ap, in_=in_ap), reads=reads, writes=writes)
            evq[0] += 1

        fl = lambda a: a[:, :, :].rearrange("p h t -> p (h t)")
        bcount = 0
        for d in range(2):
            m1 = mk[:, 0:384]
            m2 = mk[:, 384:640]
            m3 = mk[:, 640:960]
            cum = mk[:, 960:1344]
            S.dma('sp', lambda e: e.dma_start(out=mk[:], in_=K.rwm[d]), writes=['mk'])
            S.op('dve', lambda e: e.tensor_copy(out=Sth[:, 0].rearrange("p h v -> p (h v)"), in_=zt[:]), reads=['zt'], writes=['Sth0'])
            if d == 0:
                blocks = [0, 1] + list(range(2, 2 + ntile_lat))
            else:
                blocks = [1, 0] + list(range(1 + ntile_lat, 1, -1))
            chunks = [0, 1] if d == 0 else [1, 0]
            for b in blocks:
                t0 = b * G
                lat = b >= 2
                sl = bcount % 2
                bcount += 1
                srcs = [K.rwR, K.rwKD[d], K.rwKK, K.rwB[d]]
                rk = ['raw%d_%d' % (j, sl) for j in range(4)]
                for j in range(4):
                    S.dma('sp', lambda e: e.dma_start(out=raw[j][sl][:], in_=srcs[j][:, t0:t0 + G].rearrange("(c p) t -> p c t", p=128)), writes=[rk[j]])
                S.dma('sp', lambda e: e.dma_start(out=vraw[sl][:], in_=K.rwVtok[t0:t0 + G, :]), writes=['vraw%d' % sl])
                S.dma('sp', lambda e: e.dma_start(out=ldt[sl][:], in_=K.rwLD[d][t0:t0 + G, :]), writes=['ldt%d' % sl])
                r_, kd_, kk_, b_ = [raw[j][sl] for j in range(4)]
                S.op('act', lambda e: e.activation(out=vtr[:], in_=vraw[sl][:], func=AF.Identity), reads=['vraw%d' % sl], writes=['vtr'])
                banks = [pp.get() for _ in range(3)]
                for c in range(4):
                    for q in range(3):
                        S.op('pe', lambda e: e.matmul(banks[q][0][:, c * 128:(c + 1) * 128], lhsT=ldt[sl][:, c * 128:(c + 1) * 128], rhs=cum[:, q * 128:(q + 1) * 128], start=True, stop=True),
                             reads=['ldt%d' % sl, 'mk'], writes=[banks[q][1]], acc=True)
                v3 = lambda bank: bank[:, :].rearrange("p (c t) -> p c t", c=4)
                S.op('act', lambda e: e.activation(out=Ep[:], in_=v3(banks[0][0]), func=AF.Exp), reads=[banks[0][1]], writes=['Ep'])
                S.op('act', lambda e: e.activation(out=Em[:], in_=v3(banks[0][0]), func=AF.Exp, scale=-1.0), reads=[banks[0][1]], writes=['Em'])
                S.op('act', lambda e: e.activation(out=Ex[:], in_=v3(banks[1][0]), func=AF.Exp), reads=[banks[1][1]], writes=['Ex'])
                S.op('act', lambda e: e.activation(out=Ea[:], in_=v3(banks[2][0]), func=AF.Exp), reads=[banks[2][1]], writes=['Ea'])
                S.op('dve', lambda e: e.tensor_tensor(out=KRG[:, :, 0, :], in0=kk_[:], in1=Ex[:], op=ALU.mult), reads=[rk[2], 'Ex'], writes=['KRG'])
                S.op('dve', lambda e: e.tensor_tensor(out=KRG[:, :, 1, :], in0=r_[:], in1=Ep[:], op=ALU.mult), reads=[rk[0], 'Ep'], writes=['KRG'])
                S.op('dve', lambda e: e.tensor_tensor(out=BK[:, :, 0, :], in0=b_[:], in1=Em[:], op=ALU.mult), reads=[rk[3], 'Em'], writes=['BK'])
                S.op('dve', lambda e: e.tensor_tensor(out=BK[:, :, 1, :], in0=kd_[:], in1=Em[:], op=ALU.mult), reads=[rk[1], 'Em'], writes=['BK'])
                S.op('dve', lambda e: e.tensor_tensor(out=KB2[:, :, 0, :], in0=kd_[:], in1=Ea[:], op=ALU.mult), reads=[rk[1], 'Ea'], writes=['KB2'])
                S.op('dve', lambda e: e.tensor_tensor(out=KB2[:, :, 1, :], in0=b_[:], in1=Ea[:], op=ALU.mult), reads=[rk[3], 'Ea'], writes=['KB2'])
                for cc in range(2):
                    pos = cc * 64 + (63 if d == 0 else 0)
                    in0 = i64f[:, cc * 64:(cc + 1) * 64].unsqueeze(1).to_broadcast([128, 4, 64])
                    in1 = Ep[:, :, pos:pos + 1].to_broadcast([128, 4, 64])
                    S.op('dve', lambda e: e.tensor_tensor(out=KRG[:, :, 2, cc * 64:(cc + 1) * 64], in0=in0, in1=in1, op=ALU.mult), reads=['i64f', 'Ep'], writes=['KRG'])

                for g0 in range(0, 8, NS):
                    grp = list(range(g0, g0 + NS))
                    cur = {}
                    for s_, h in enumerate(grp):
                        c, pb = h // 2, 64 * (h % 2)
                        fm = lambda arr, k0, k1: arr[pb:pb + 64, c, k0:k1, :].rearrange("p k t -> p (k t)")
                        p1, k1 = pp.get()
                        S.op('pe', lambda e: e.matmul(p1[:, 0:256], lhsT=fm(BK, 0, 1), rhs=fm(KRG, 0, 2), start=True, stop=True), reads=['BK', 'KRG'], writes=[k1], acc=True)
                        S.op('pe', lambda e: e.matmul(p1[:, 256:384], lhsT=fm(KB2, 1, 2), rhs=i64r[pb:pb + 64, :], start=True, stop=True), reads=['KB2', 'i64r'], writes=[k1], acc=True)
                        S.op('dve', lambda e: e.tensor_tensor(out=La[s_][:], in0=p1[:, 0:384], in1=m1, op=ALU.mult), reads=[k1, 'mk'], writes=['La%d' % s_])
                        p2, k2 = pp.get()
                        S.op('pe', lambda e: e.matmul(p2[:, 0:128], lhsT=fm(BK, 1, 2), rhs=fm(KRG, 1, 2), start=True, stop=True), reads=['BK', 'KRG'], writes=[k2], acc=True)
                        S.op('pe', lambda e: e.matmul(p2[:, 128:256], lhsT=fm(KB2, 0, 1), rhs=i64r[pb:pb + 64, :], start=True, stop=True), reads=['KB2', 'i64r'], writes=[k2], acc=True)
                        S.op('dve', lambda e: e.tensor_tensor(out=X2[s_][:], in0=p2[:, 0:256], in1=m2, op=ALU.mult), reads=[k2, 'mk'], writes=['X2%d' % s_])
                        p3, k3 = pp.get()
                        S.op('pe', lambda e: e.matmul(p3[:, 0:256], lhsT=fm(KRG, 0, 1), rhs=fm(BK, 0, 2), start=True, stop=True), reads=['BK', 'KRG'], writes=[k3], acc=True)
                        S.op('pe', lambda e: e.matmul(p3[:, 256:320], lhsT=fm(KRG, 0, 1), rhs=i64r[pb:pb + 64, 0:64], start=True, stop=True), reads=['KRG', 'i64r'], writes=[k3], acc=True)
                        S.op('dve', lambda e: e.tensor_tensor(out=X1[s_][:], in0=p3[:, 0:320], in1=m3, op=ALU.mult), reads=[k3, 'mk'], writes=['X1%d' % s_])
                        cur[s_] = (La[s_], 'La%d' % s_, X1[s_][:, 0:128], 'X1%d' % s_)
                    for lev in range(5):
                        pend = []
                        nxt = {}
                        for s_ in range(NS):
                            L, Lk, AT, ATk = cur[s_]
                            p, pk = pp.get()
                            S.op('pe', lambda e: e.matmul(p[:, 0:384], lhsT=AT, rhs=L[:, 0:384], start=True, stop=False), reads=[Lk, ATk], writes=[pk], acc=True)
                            S.op('pe', lambda e: e.matmul(p[:, 128:384], lhsT=idr[:], rhs=L[:, 128:384], start=False, stop=True), reads=[Lk, 'idr'], writes=[pk], acc=True)
                            pend.append((p, pk))
                        for s_ in range(NS):
                            p, pk = pend[s_]
                            Ln_ = Lb[s_] if lev % 2 == 0 else La[s_]
                            Lnk = ('Lb%d' if lev % 2 == 0 else 'La%d') % s_
                            evac(Ln_[:], p[:, 0:384], [pk], [Lnk])
                            nxt[s_] = (Ln_, Lnk)
                        for s_ in range(NS):
                            Ln_, Lnk = nxt[s_]
                            p, pk = pend[s_]
                            S.op('pe', lambda e: e.transpose(out=p[:, 384:512], in_=Ln_[:, 0:128].bitcast(F32), identity=idf[:]), reads=[Lnk, 'idf'], writes=[pk])
                        for s_ in range(NS):
                            Ln_, Lnk = nxt[s_]
                            p, pk = pend[s_]
                            ATn = ATa[s_] if lev % 2 == 0 else ATb[s_]
                            ATnk = ('ATa%d' if lev % 2 == 0 else 'ATb%d') % s_
                            evac(ATn[:], p[:, 384:512], [pk], [ATnk])
                            cur[s_] = (Ln_, Lnk, ATn[:], ATnk)
                    for s_, h in enumerate(grp):
                        L, Lk, AT, ATk = cur[s_]
                        p, pk = pp.get()
                        S.op('pe', lambda e: e.matmul(p[:, 0:256], lhsT=AT, rhs=L[:, 128:384], start=True, stop=False), reads=[Lk, ATk], writes=[pk], acc=True)
                        S.op('pe', lambda e: e.matmul(p[:, 0:256], lhsT=idr[:], rhs=L[:, 128:384], start=False, stop=True), reads=[Lk, 'idr'], writes=[pk], acc=True)
                        evac(Xf[s_][:], p[:, 0:256], [pk], ['Xf%d' % s_])
                    for s_, h in enumerate(grp):
                        c, pb = h // 2, 64 * (h % 2)
                        p, pk = pp.get()
                        S.op('pe', lambda e: e.matmul(p[:, 0:256], lhsT=idr[:], rhs=X2[s_][:], start=True, stop=False), reads=['X2%d' % s_, 'idr'], writes=[pk], acc=True)
                        S.op('pe', lambda e: e.matmul(p[:, 0:256], lhsT=X1[s_][:, 128:256], rhs=Xf[s_][:], start=False, stop=True), reads=['X1%d' % s_, 'Xf%d' % s_], writes=[pk], acc=True)
                        evac(QG2[:, h, :], p[:, 0:256], [pk], ['QG2_%d' % h])
                        q, qk = pp.get()
                        rg = KRG[pb:pb + 64, c, 1:3, :].rearrange("p k t -> p (k t)")
                        S.op('pe', lambda e: e.matmul(q[0:64, 0:256], lhsT=idr[pb:pb + 64, pb:pb + 64], rhs=rg, start=True, stop=False), reads=['KRG', 'idr'], writes=[qk], acc=True)
                        S.op('pe', lambda e: e.matmul(q[0:64, 0:256], lhsT=X1[s_][:, 256:320], rhs=Xf[s_][:], start=False, stop=True), reads=['X1%d' % s_, 'Xf%d' % s_], writes=[qk], acc=True)
                        evac(QG1[:, h, :], q[0:64, 0:256], [qk], ['QG1_%d' % h])
                for s, cc in enumerate(chunks):
                    for h in range(8):
                        S.op('pe', lambda e: e.matmul(pst[:, h * 64:(h + 1) * 64], lhsT=QG1[:, h, 128 + cc * 64:128 + (cc + 1) * 64], rhs=Sth[:, s, h, :], start=True, stop=False),
                             reads=['QG1_%d' % h, 'Sth%d' % s], writes=['pst'], acc=True)
                        S.op('pe', lambda e: e.matmul(pst[:, h * 64:(h + 1) * 64], lhsT=QG2[:, h, 128 + cc * 64:128 + (cc + 1) * 64], rhs=vtr[:, h * 64:(h + 1) * 64], start=False, stop=True),
                             reads=['QG2_%d' % h, 'vtr'], writes=['pst'], acc=True)
                    evac(Sth[:, s + 1].rearrange("p h v -> p (h v)"), pst[:, :], ['pst'], ['Sth%d' % (s + 1)])
                if lat:
                    ybuf = T[0]
                    for hq in range(2):
                        bank = pp.get()
                        for h4 in range(4):
                            h = hq * 4 + h4
                            S.op('pe', lambda e: e.matmul(bank[0][0:64, h4 * 128:(h4 + 1) * 128], lhsT=vtr[:, h * 64:(h + 1) * 64], rhs=QG2[:, h, 0:128], start=True, stop=False),
                                 reads=['QG2_%d' % h, 'vtr'], writes=[bank[1]], acc=True)
                            for s, cc in enumerate(chunks):
                                S.op('pe', lambda e: e.matmul(bank[0][0:64, h4 * 128 + cc * 64:h4 * 128 + (cc + 1) * 64], lhsT=Sth[:, s, h, :], rhs=QG1[:, h, cc * 64:(cc + 1) * 64], start=False, stop=(s == 1)),
                                     reads=['QG1_%d' % h, 'Sth%d' % s], writes=[bank[1]], acc=True)
                        evac(ybuf[:, hq * 4:hq * 4 + 4, :], bank[0][0:64, :].rearrange("p (h t) -> p h t", h=4), [bank[1]], ['T0'])
                    tl = t0 - TC
                    if d == 0:
                        S.dma('pool', lambda e: e.dma_start(out=K.rwY0[:, tl:tl + G].rearrange("(h p) t -> p h t", p=64), in_=ybuf[:]), reads=['T0'], writes=['rwY0'])
                    else:
                        y0b, cen, sqb = T[1], T[2], T[3]
                        S.dma('sp', lambda e: e.dma_start(out=y0b[:], in_=K.rwY0[:, tl:tl + G].rearrange("(h p) t -> p h t", p=64)), reads=['rwY0'], writes=['T1'])
                        S.op('dve', lambda e: e.tensor_tensor(out=ybuf[:], in0=ybuf[:], in1=y0b[:], op=ALU.add), reads=['T0', 'T1'], writes=['T0'])
                        for q in range(2):
                            p, pk = pp.get()
                            S.op('pe', lambda e: e.matmul(p[0:64, :], lhsT=o64[:], rhs=fl(ybuf)[:, q * 512:(q + 1) * 512], start=True, stop=True), reads=['o64', 'T0'], writes=[pk])
                            S.op('dve', lambda e: e.tensor_tensor(out=fl(cen)[:, q * 512:(q + 1) * 512], in0=fl(ybuf)[:, q * 512:(q + 1) * 512], in1=p[0:64, :], op=ALU.subtract), reads=[pk, 'T0'], writes=['T2'])
                        S.op('act', lambda e: e.activation(out=sqb[:], in_=cen[:], func=AF.Square), reads=['T2'], writes=['T3'])
                        rsb = T[1]
                        for q in range(2):
                            p, pk = pp.get()
                            S.op('pe', lambda e: e.matmul(p[0:64, :], lhsT=o64[:], rhs=fl(sqb)[:, q * 512:(q + 1) * 512], start=True, stop=True), reads=['o64', 'T3'], writes=[pk])
                            S.op('act', lambda e: e.activation(out=fl(rsb)[:, q * 512:(q + 1) * 512], in_=p[0:64, :], func=AF.Ln, bias=epsg[:], scale=1.0), reads=[pk, 'epsg'], writes=['T1'])
                        S.op('act', lambda e: e.activation(out=rsb[:], in_=rsb[:], func=AF.Exp, scale=-0.5), reads=['T1'], writes=['T1'])
                        bonb, ggb = T[3], T[0]
                        S.dma('sp', lambda e: e.dma_start(out=bonb[:], in_=K.rwBON[:, tl:tl + G].rearrange("(h p) t -> p h t", p=64)), reads=['rwBON'], writes=['T3'])
                        S.op('dve', lambda e: e.tensor_tensor(out=cen[:], in0=cen[:], in1=rsb[:], op=ALU.mult), reads=['T2', 'T1'], writes=['T2'])
                        S.dma('sp', lambda e: e.dma_start(out=ggb[:], in_=K.rwG[:, tl:tl + G].rearrange("(h p) t -> p h t", p=64)), reads=['rwG'], writes=['T0'])
                        S.op('dve', lambda e: e.tensor_tensor(out=cen[:], in0=cen[:], in1=gng[:, :].unsqueeze(2).to_broadcast([64, 8, G]), op=ALU.mult), reads=['T2', 'gng'], writes=['T2'])
                        S.op('dve', lambda e: e.tensor_tensor(out=cen[:], in0=cen[:], in1=gnb[:, :].unsqueeze(2).to_broadcast([64, 8, G]), op=ALU.add), reads=['T2', 'gnb'], writes=['T2'])
                        S.op('dve', lambda e: e.tensor_tensor(out=cen[:], in0=cen[:], in1=bonb[:], op=ALU.add), reads=['T2', 'T3'], writes=['T2'])
                        S.op('dve', lambda e: e.tensor_tensor(out=outb[:], in0=cen[:], in1=ggb[:], op=ALU.mult), reads=['T2', 'T0'], writes=['outb'])
                        S.dma('pool', lambda e: e.dma_start(out=K.mixT[0:512, tl:tl + G].rearrange("(h p) t -> p h t", p=64), in_=outb[:]), reads=['outb'], writes=['mixT'])
                S.op('dve', lambda e: e.tensor_copy(out=Sth[:, 0], in_=Sth[:, 2]), reads=['Sth2'], writes=['Sth0'])
            S.barrier()


ALPHA = 2.0 ** 0.25
ROWW = 1088
BIGPOS = 4096.0


def phase_mix(K):
    nc, S = K.nc, K.S
    NT = TL // 128
    with ExitStack() as es:
        sb = lambda n, s, d: es.enter_context(nc.sbuf_tensor(_u(n), s, d))
        ps = lambda n, s, d: es.enter_context(nc.psum_tensor(_u(n), s, d))
        wo = sb("wo", [128, 8, 1024], BF16)
        wst = [sb("owst%d" % i, [128, 8, 256], F32) for i in range(2)]
        bc = {n: sb("bc_" + n, [128, 1024], F32) for n in ('g1', 'ln1g', 'ln1b', 'sc2', 'sh2')}
        rt = sb("rt", [128, 8, 16], F32)
        idf = sb("idf", [128, 128], F32)
        epst = sb("epst", [128, 1], F32)
        onesf = sb("onesf", [128, 128], F32)
        onesb = sb("onesb", [128, 128], BF16)
        ustr = sb("ustr", [128, 128], BF16)
        mt = [sb("mt%d" % i, [128, 8, 128], BF16) for i in range(2)]
        xt = [sb("xt%d" % i, [128, 1024], F32) for i in range(2)]
        t1 = sb("t1", [128, 1024], F32)
        pre = sb("pre", [128, 1024], F32)
        x1 = [sb("x1_%d" % i, [128, 1024], F32) for i in range(2)]
        uf = sb("uf", [128, 1024], F32)
        urow = [sb("urow%d" % i, [128, ROWW], BF16) for i in range(2)]
        uT = sb("uT", [128, 8, 128], F32)
        st = sb("st", [128, 2, 6], F32)
        mv = sb("mv", [128, 2], F32)
        lnv = sb("lnv", [128, 1], F32)
        rstd = sb("rstd", [128, 1], F32)
        lg = sb("lg", [128, 16], F32)
        mx = sb("mx", [128, 1], F32)
        sm = sb("sm", [128, 1], F32)
        affall = sb("affall", [128, NT, 16], F32)
        tok = sb("tok", [128, 1], I32)
        lo = sb("lo", [128, 16], F32)
        mid = sb("mid", [128, 16], F32)
        ge = sb("ge", [128, 16], F32)
        cntp = sb("cntp", [128, 16], F32)
        mskt = sb("mskt", [128, NT, 16], F32)
        mskb = sb("mskb", [128, NT, 16], BF16)
        csT = sb("csT", [128, 16, NT], F32)
        incT = sb("incT", [128, 16, NT], F32)
        rmask = sb("rmask", [128, 16, NT], F32)
        posf = sb("posf", [128, NT, 16], F32)
        posi = sb("posi", [128, NT, 16], I32)
        zt = sb("zt", [128, 1024], F32)
        pm = [ps("pm%d" % i, [128, 1024], F32) for i in range(2)]
        ptr = ps("ptr", [128, 1024], F32)
        psm = ps("psm", [128, 512], F32)
        psn = ps("psn", [128, 512], F32)

        ld1 = lambda dst, src, key, rd=(): S.dma('sp', lambda e: e.dma_start(out=dst, in_=src, allow_slow_non_contiguous=True), reads=list(rd), writes=[key])
        ld1(idf[:], K.identf, 'idf')
        ld1(ustr[:], K.ustrict, 'ustr')
        ld1(rt[:], K.router.rearrange("(k p) e -> p k e", p=128), 'rt')
        ld1(bc['g1'][:], K.modd[0:1, 2048:3072].partition_broadcast(128), 'bc_g1', ['modd'])
        ld1(bc['sh2'][:], K.modd[0:1, 3072:4096].partition_broadcast(128), 'bc_sh2', ['modd'])
        ld1(bc['sc2'][:], K.modd[0:1, 4096:5120].partition_broadcast(128), 'bc_sc2', ['modd'])
        ld1(bc['ln1g'][:], K.ln1_g.partition_broadcast(128), 'bc_ln1g')
        ld1(bc['ln1b'][:], K.ln1_b.partition_broadcast(128), 'bc_ln1b')
        S.op('dve', lambda e: e.tensor_scalar_add(out=bc['sc2'][:], in0=bc['sc2'][:], scalar1=1.0), reads=['bc_sc2'], writes=['bc_sc2'])
        S.op('dve', lambda e: e.memset(epst[:], 1e-5), writes=['epst'])
        S.op('dve', lambda e: e.memset(onesf[:], 1.0), writes=['onesf'])
        S.op('dve', lambda e: e.memset(onesb[:], 1.0), writes=['onesb'])
        S.op('dve', lambda e: e.memset(zt[:], 0.0), writes=['zt'])
        for j in range(4):
            w = wst[j % 2]
            wk = 'owst%d' % (j % 2)
            S.dma('sp', lambda e: e.dma_start(out=w[:], in_=K.w_out[:, j * 256:(j + 1) * 256].rearrange("(k p) n -> p k n", p=128)), writes=[wk])
            S.op('pool', lambda e: e.tensor_copy(out=wo[:, :, j * 256:(j + 1) * 256], in_=w[:]), reads=[wk], writes=['wo'])
        for i in range(NT):
            S.dma('pool', lambda e: e.dma_start(out=K.yacc[i * 128:(i + 1) * 128, :], in_=zt[:]), reads=['zt'], writes=['yacc%d' % i])

        def ln_stats(src, srck):
            for c in range(2):
                S.op('dve', lambda e: e.bn_stats(out=st[:, c, :], in_=src[:, c * 512:(c + 1) * 512]), reads=[srck], writes=['st%d' % c])
            S.op('dve', lambda e: e.bn_aggr(out=mv[:], in_=st[:]), reads=['st0', 'st1'], writes=['mv'])
            S.op('act', lambda e: e.activation(out=lnv[:], in_=mv[:, 1:2], func=AF.Ln, bias=epst[:], scale=1.0), reads=['mv', 'epst'], writes=['lnv'])
            S.op('act', lambda e: e.activation(out=rstd[:], in_=lnv[:], func=AF.Exp, scale=-0.5), reads=['lnv'], writes=['rstd'])

        for i in range(NT):
            sl = i % 2
            m_, mk_ = mt[sl], 'mt%d' % sl
            x_, xk = xt[sl], 'xt%d' % sl
            S.dma('sp', lambda e: e.dma_start(out=m_[:], in_=K.mixT[:, i * 128:(i + 1) * 128].rearrange("(k p) t -> p k t", p=128)), reads=['mixT'], writes=[mk_])
            S.dma('sp', lambda e: e.dma_start(out=x_[:], in_=K.xin[TC + i * 128:TC + (i + 1) * 128, :]), writes=[xk])
            p_, pk_ = pm[sl], 'pm%d' % sl
            for half in range(2):
                for kc in range(8):
                    S.op('pe', lambda e: e.matmul(p_[:, half * 512:(half + 1) * 512], lhsT=m_[:, kc, :], rhs=wo[:, kc, half * 512:(half + 1) * 512], start=(kc == 0), stop=(kc == 7)),
                         reads=[mk_, 'wo'], writes=[pk_], acc=True)
            S.op('dve', lambda e: e.tensor_tensor(out=t1[:], in0=p_[:], in1=bc['g1'][:], op=ALU.mult), reads=[pk_, 'bc_g1'], writes=['t1'])
            S.op('dve', lambda e: e.scalar_tensor_tensor(out=pre[:], in0=x_[:], scalar=ALPHA, in1=t1[:], op0=ALU.mult, op1=ALU.add), reads=[xk, 't1'], writes=['pre'])
            ln_stats(pre, 'pre')
            x1_, x1k = x1[sl], 'x1_%d' % sl
            S.op('dve', lambda e: e.tensor_scalar(out=t1[:], in0=pre[:], scalar1=mv[:, 0:1], scalar2=rstd[:], op0=ALU.subtract, op1=ALU.mult), reads=['pre', 'mv', 'rstd'], writes=['t1'])
            S.op('dve', lambda e: e.tensor_tensor(out=t1[:], in0=t1[:], in1=bc['ln1g'][:], op=ALU.mult), reads=['t1', 'bc_ln1g'], writes=['t1'])
            S.op('dve', lambda e: e.tensor_tensor(out=x1_[:], in0=t1[:], in1=bc['ln1b'][:], op=ALU.add), reads=['t1', 'bc_ln1b'], writes=[x1k])
            S.dma('pool', lambda e: e.dma_start(out=K.x1d[i * 128:(i + 1) * 128, :], in_=x1_[:]), reads=[x1k], writes=['x1d'])
            ln_stats(x1_, x1k)
            S.op('dve', lambda e: e.tensor_scalar(out=pre[:], in0=x1_[:], scalar1=mv[:, 0:1], scalar2=rstd[:], op0=ALU.subtract, op1=ALU.mult), reads=[x1k, 'mv', 'rstd'], writes=['pre'])
            S.op('dve', lambda e: e.tensor_tensor(out=pre[:], in0=pre[:], in1=bc['sc2'][:], op=ALU.mult), reads=['pre', 'bc_sc2'], writes=['pre'])
            S.op('dve', lambda e: e.tensor_tensor(out=uf[:], in0=pre[:], in1=bc['sh2'][:], op=ALU.add), reads=['pre', 'bc_sh2'], writes=['uf'])
            ur, urk = urow[sl], 'urow%d' % sl
            S.op('act', lambda e: e.activation(out=ur[:, 0:1024], in_=uf[:], func=AF.Identity), reads=['uf'], writes=[urk])
            for k in range(8):
                S.op('pe', lambda e: e.transpose(out=ptr[:, k * 128:(k + 1) * 128], in_=uf[:, k * 128:(k + 1) * 128], identity=idf[:]), reads=['uf', 'idf'], writes=['ptr'], acc=True)
            S.op('act', lambda e: e.activation(out=uT[:].rearrange("p k t -> p (k t)"), in_=ptr[:], func=AF.Identity), reads=['ptr'], writes=['uT'])
            for k in range(8):
                S.op('pe', lambda e: e.matmul(psm[:, 0:16], lhsT=uT[:, k, :], rhs=rt[:, k, :], start=(k == 0), stop=(k == 7)), reads=['uT', 'rt'], writes=['psm'], acc=True)
            S.op('dve', lambda e: e.reduce_max(out=mx[:], in_=psm[:, 0:16], axis=AX.X), reads=['psm'], writes=['mx'])
            S.op('dve', lambda e: e.tensor_scalar_mul(out=mx[:], in0=mx[:], scalar1=-1.0), reads=['mx'], writes=['mx'])
            S.op('act', lambda e: e.activation(out=lg[:], in_=psm[:, 0:16], func=AF.Exp, bias=mx[:], scale=1.0, accum_out=sm[:]), reads=['psm', 'mx'], writes=['lg', 'sm'])
            S.op('dve', lambda e: e.reciprocal(out=sm[:], in_=sm[:]), reads=['sm'], writes=['sm'])
            S.op('dve', lambda e: e.tensor_scalar_mul(out=affall[:, i, :], in0=lg[:], scalar1=sm[:]), reads=['lg', 'sm'], writes=['affall'])
            S.op('dve', lambda e: e.tensor_copy(out=ur[:, 1024:1056].bitcast(F32), in_=affall[:, i, :]), reads=['affall'], writes=[urk])
            S.op('pool', lambda e: e.iota(tok[:], pattern=[[0, 1]], base=i * 128, channel_multiplier=1), writes=['tok'])
            S.op('pool', lambda e: e.tensor_copy(out=ur[:, 1056:1058].bitcast(I32), in_=tok[:]), reads=['tok'], writes=[urk])
            S.dma('pool', lambda e: e.dma_start(out=K.urd[i * 128:(i + 1) * 128, 0:1058], in_=ur[:, 0:1058]), reads=[urk], writes=['urd%d' % i])

        S.op('dve', lambda e: e.memset(lo[:], 0.0), writes=['lo'])
        for k in range(30):
            hk = 2.0 ** -(k + 1)
            S.op('dve', lambda e: e.tensor_scalar_add(out=mid[:], in0=lo[:], scalar1=hk), reads=['lo'], writes=['mid'])
            S.op('dve', lambda e: e.tensor_tensor(out=mskt[:], in0=affall[:], in1=mid[:, :].unsqueeze(1).to_broadcast([128, NT, 16]), op=ALU.is_ge), reads=['affall', 'mid'], writes=['mskt'])
            S.op('dve', lambda e: e.tensor_reduce(out=cntp[:], in_=mskt[:].rearrange("p i e -> p e i"), axis=AX.X, op=ALU.add), reads=['mskt'], writes=['cntp'])
            S.op('pe', lambda e: e.matmul(psn[:, 0:16], lhsT=onesf[:], rhs=cntp[:], start=True, stop=True), reads=['onesf', 'cntp'], writes=['psn'])
            S.op('dve', lambda e: e.tensor_scalar(out=ge[:], in0=psn[:, 0:16], scalar1=float(CAP) - 0.5, scalar2=hk, op0=ALU.is_ge, op1=ALU.mult), reads=['psn'], writes=['ge'])
            S.op('dve', lambda e: e.tensor_tensor(out=lo[:], in0=lo[:], in1=ge[:], op=ALU.add), reads=['lo', 'ge'], writes=['lo'])
        S.op('dve', lambda e: e.tensor_tensor(out=mskt[:], in0=affall[:], in1=lo[:, :].unsqueeze(1).to_broadcast([128, NT, 16]), op=ALU.is_ge), reads=['affall', 'lo'], writes=['mskt'])
        S.op('act', lambda e: e.activation(out=mskb[:], in_=mskt[:], func=AF.Identity), reads=['mskt'], writes=['mskb'])
        mflat = mskb[:].rearrange("p i e -> p (i e)")
        for hh in range(2):
            S.op('pe', lambda e: e.matmul(psm[:, :], lhsT=onesb[:], rhs=mflat[:, hh * 512:(hh + 1) * 512], start=True, stop=True), reads=['onesb', 'mskb'], writes=['psm'])
            S.op('dve', lambda e: e.tensor_copy(out=csT[:, :, hh * 32:(hh + 1) * 32], in_=psm[:, :].rearrange("p (i e) -> p e i", e=16)), reads=['psm'], writes=['csT'])
        S.op('dve', lambda e: e.memset(rmask[:], 1.0), writes=['rmask'])
        S.op('dve', lambda e: e.memset(rmask[:, :, 0:1], 0.0), writes=['rmask'])
        S.op('dve', lambda e: e.tensor_tensor_scan(out=incT[:].rearrange("p e i -> p (e i)"), data0=rmask[:].rearrange("p e i -> p (e i)"), data1=csT[:].rearrange("p e i -> p (e i)"),
                                                   initial=0.0, op0=ALU.mult, op1=ALU.add), reads=['rmask', 'csT'], writes=['incT'])
        S.op('dve', lambda e: e.tensor_tensor(out=incT[:], in0=incT[:], in1=csT[:], op=ALU.subtract), reads=['incT', 'csT'], writes=['incT'])
        for hh in range(2):
            S.op('pe', lambda e: e.matmul(psm[:, :], lhsT=ustr[:], rhs=mflat[:, hh * 512:(hh + 1) * 512], start=True, stop=True), reads=['ustr', 'mskb'], writes=['psm'])
            S.op('dve', lambda e: e.tensor_tensor(out=posf[:, hh * 32:(hh + 1) * 32, :], in0=psm[:, :].rearrange("p (i e) -> p i e", e=16),
                                                  in1=incT[:, :, hh * 32:(hh + 1) * 32].rearrange("p e i -> p i e"), op=ALU.add), reads=['psm', 'incT'], writes=['posf'])
        S.op('dve', lambda e: e.scalar_tensor_tensor(out=posf[:], in0=posf[:], scalar=-BIGPOS, in1=mskt[:], op0=ALU.add, op1=ALU.mult), reads=['posf', 'mskt'], writes=['posf'])
        S.op('dve', lambda e: e.tensor_scalar_add(out=posf[:], in0=posf[:], scalar1=BIGPOS), reads=['posf'], writes=['posf'])
        S.op('dve', lambda e: e.tensor_copy(out=posi[:], in_=posf[:]), reads=['posf'], writes=['posi'])
        if K.dbg_pos is not None:
            S.dma('sp', lambda e: e.dma_start(out=K.dbg_pos, in_=posf[:]), reads=['posf'], writes=['dbg_pos'])
        breg = nc.gpsimd.to_reg(CAP - 1)
        for i in range(NT):
            sl = i % 2
            ur, urk = urow[sl], 'urow%d' % sl
            S.dma('sp', lambda e: e.dma_start(out=ur[:, 0:1058], in_=K.urd[i * 128:(i + 1) * 128, 0:1058]), reads=['urd%d' % i], writes=[urk])
            for ex in range(NE):
                S.dma('pool', lambda e: e.indirect_dma_start(out=K.xe_d[ex], out_offset=bass.IndirectOffsetOnAxis(ap=posi[:, i, ex:ex + 1], axis=0),
                                                             in_=ur[:, :], in_offset=None, bounds_check=breg, oob_is_err=False),
                      reads=[urk, 'posi'], writes=['xe_d_%d_%d' % (i, ex)])
        S.barrier()


def phase_moe(K, experts=range(NE)):
    nc, S = K.nc, K.S
    NT = TL // 128
    with ExitStack() as es:
        sb = lambda n, s, d: es.enter_context(nc.sbuf_tensor(_u(n), s, d))
        ps = lambda n, s, d: es.enter_context(nc.psum_tensor(_u(n), s, d))
        W = [[sb("W%d_%d" % (m, i), [128, 8, 1024], BF16) for i in range(2)] for m in range(3)]
        wst = Rot([sb("ewst%d" % i, [128, 8, 256], F32) for i in range(3)], "ewst")
        idb = sb("idb", [128, 128], BF16)
        xrow = Rot([sb("xrow%d" % i, [128, ROWW], BF16) for i in range(2)], "xrow")
        xeT = sb("xeT", [128, 8, 1024], BF16)
        hidT = sb("hidT", [128, 8, 1024], BF16)
        gates = sb("gates", [128, 8], F32)
        idxs = sb("idxs", [128, 8], I32)
        sgt = Rot([sb("sgt%d" % i, [128, 512], F32) for i in range(2)], "sgt")
        ye = Rot([sb("ye%d" % i, [128, 1024], F32) for i in range(2)], "ye")
        pt = Rot([ps("ept%d" % i, [128, 1024], BF16) for i in range(2)], "ept")
        pg = Rot([ps("epg%d" % i, [128, 512], F32) for i in range(6)], "epg")
        S.dma('sp', lambda e: e.dma_start(out=idb[:], in_=K.ident), writes=['idb'])
        cast_i = [0]

        def load_w(ex, slot):
            for m, src in enumerate((K.exp_w_gate, K.exp_w_up, K.exp_w_down)):
                for j in range(4):
                    w, wk = wst.get()
                    S.dma('sp', lambda e: e.dma_start(out=w[:], in_=src[ex, :, j * 256:(j + 1) * 256].rearrange("(k p) n -> p k n", p=128)), writes=[wk])
                    eng = 'dve'
                    cast_i[0] += 1
                    if eng == 'dve':
                        S.op('dve', lambda e: e.tensor_copy(out=W[m][slot][:, :, j * 256:(j + 1) * 256], in_=w[:]), reads=[wk], writes=['W%d_%d' % (m, slot)])
                    else:
                        S.op('act', lambda e: e.activation(out=W[m][slot][:, :, j * 256:(j + 1) * 256], in_=w[:], func=AF.Identity), reads=[wk], writes=['W%d_%d' % (m, slot)])

        exl = list(experts)
        load_w(exl[0], 0)
        for n, ex in enumerate(exl):
            slot = n % 2
            Wg, Wu, Wd = W[0][slot], W[1][slot], W[2][slot]
            wkeys = ['W%d_%d' % (m, slot) for m in range(3)]
            for j in range(8):
                xr, xk = xrow.get()
                S.dma('sp', lambda e: e.dma_start(out=xr[:, 0:1058], in_=K.xe_d[ex][j * 128:(j + 1) * 128, 0:1058]), reads=['xe_d'], writes=[xk])
                S.op('dve', lambda e: e.tensor_copy(out=gates[:, j:j + 1], in_=xr[:, 1024:1056].bitcast(F32)[:, ex:ex + 1]), reads=[xk], writes=['gates'])
                S.op('dve', lambda e: e.tensor_copy(out=idxs[:, j:j + 1], in_=xr[:, 1056:1058].bitcast(I32)), reads=[xk], writes=['idxs'])
                p, pk = pt.get()
                for k in range(8):
                    S.op('pe', lambda e: e.transpose(out=p[:, k * 128:(k + 1) * 128], in_=xr[:, k * 128:(k + 1) * 128], identity=idb[:]), reads=[xk, 'idb'], writes=[pk], acc=True)
                S.op('act', lambda e: e.activation(out=xeT[:, :, j * 128:(j + 1) * 128], in_=p[:].rearrange("p (k t) -> p k t", k=8), func=AF.Identity), reads=[pk], writes=['xeT'])
            if n + 1 < len(exl):
                load_w(exl[n + 1], 1 - slot)
            for fc in range(8):
                for half in range(2):
                    g_, gk = pg.get()
                    u_, uk = pg.get()
                    for kc in range(8):
                        S.op('pe', lambda e: e.matmul(g_[:], lhsT=Wg[:, kc, fc * 128:(fc + 1) * 128], rhs=xeT[:, kc, half * 512:(half + 1) * 512], start=(kc == 0), stop=(kc == 7)),
                             reads=[wkeys[0], 'xeT'], writes=[gk], acc=True)
                    for kc in range(8):
                        S.op('pe', lambda e: e.matmul(u_[:], lhsT=Wu[:, kc, fc * 128:(fc + 1) * 128], rhs=xeT[:, kc, half * 512:(half + 1) * 512], start=(kc == 0), stop=(kc == 7)),
                             reads=[wkeys[1], 'xeT'], writes=[uk], acc=True)
                    s_, sk = sgt.get()
                    S.op('act', lambda e: e.activation(out=s_[:], in_=g_[:], func=AF.Silu), reads=[gk], writes=[sk])
                    S.op('dve', lambda e: e.tensor_tensor(out=hidT[:, fc, half * 512:(half + 1) * 512], in0=s_[:], in1=u_[:], op=ALU.mult), reads=[sk, uk], writes=['hidT'])
            for j in range(8):
                y_, yk = ye.get()
                for dh in range(2):
                    o_, ok = pg.get()
                    for fc in range(8):
                        S.op('pe', lambda e: e.matmul(o_[:], lhsT=hidT[:, fc, j * 128:(j + 1) * 128], rhs=Wd[:, fc, dh * 512:(dh + 1) * 512], start=(fc == 0), stop=(fc == 7)),
                             reads=[wkeys[2], 'hidT'], writes=[ok], acc=True)
                    S.op('act', lambda e: e.activation(out=y_[:, dh * 512:(dh + 1) * 512], in_=o_[:], func=AF.Identity, scale=gates[:, j:j + 1]), reads=[ok, 'gates'], writes=[yk])
                S.dma('pool', lambda e: e.indirect_dma_start(out=K.yacc, out_offset=bass.IndirectOffsetOnAxis(ap=idxs[:, j:j + 1], axis=0), in_=y_[:, :], in_offset=None,
                                                             compute_op=ALU.add),
                      reads=[yk, 'idxs'], writes=['yacc'])
        S.barrier()


def phase_final(K):
    nc, S = K.nc, K.S
    NT = TL // 128
    with ExitStack() as es:
        sb = lambda n, s, d: es.enter_context(nc.sbuf_tensor(_u(n), s, d))
        bc = {n: sb("fbc_" + n, [128, 1024], F32) for n in ('g2', 'ln2g', 'ln2b')}
        epst = sb("epst", [128, 1], F32)
        x1 = [sb("fx1_%d" % i, [128, 1024], F32) for i in range(2)]
        ya = [sb("fya_%d" % i, [128, 1024], F32) for i in range(2)]
        t1 = sb("t1", [128, 1024], F32)
        pre = sb("pre", [128, 1024], F32)
        ob = [sb("fob_%d" % i, [128, 1024], F32) for i in range(2)]
        st = sb("st", [128, 2, 6], F32)
        mv = sb("mv", [128, 2], F32)
        lnv = sb("lnv", [128, 1], F32)
        rstd = sb("rstd", [128, 1], F32)
        ld1 = lambda dst, src, key, rd=(): S.dma('sp', lambda e: e.dma_start(out=dst, in_=src, allow_slow_non_contiguous=True), reads=list(rd), writes=[key])
        ld1(bc['g2'][:], K.modd[0:1, 5120:6144].partition_broadcast(128), 'fbc_g2', ['modd'])
        ld1(bc['ln2g'][:], K.ln2_g.partition_broadcast(128), 'fbc_ln2g')
        ld1(bc['ln2b'][:], K.ln2_b.partition_broadcast(128), 'fbc_ln2b')
        S.op('dve', lambda e: e.memset(epst[:], 1e-5), writes=['epst'])
        for i in range(NT):
            sl = i % 2
            S.dma('sp', lambda e: e.dma_start(out=x1[sl][:], in_=K.x1d[i * 128:(i + 1) * 128, :]), reads=['x1d'], writes=['fx1_%d' % sl])
            S.dma('sp', lambda e: e.dma_start(out=ya[sl][:], in_=K.yacc[i * 128:(i + 1) * 128, :]), reads=['yacc'], writes=['fya_%d' % sl])
            S.op('dve', lambda e: e.tensor_tensor(out=t1[:], in0=ya[sl][:], in1=bc['g2'][:], op=ALU.mult), reads=['fya_%d' % sl, 'fbc_g2'], writes=['t1'])
            S.op('dve', lambda e: e.scalar_tensor_tensor(out=pre[:], in0=x1[sl][:], scalar=ALPHA, in1=t1[:], op0=ALU.mult, op1=ALU.add), reads=['fx1_%d' % sl, 't1'], writes=['pre'])
            for c in range(2):
                S.op('dve', lambda e: e.bn_stats(out=st[:, c, :], in_=pre[:, c * 512:(c + 1) * 512]), reads=['pre'], writes=['st%d' % c])
            S.op('dve', lambda e: e.bn_aggr(out=mv[:], in_=st[:]), reads=['st0', 'st1'], writes=['mv'])
            S.op('act', lambda e: e.activation(out=lnv[:], in_=mv[:, 1:2], func=AF.Ln, bias=epst[:], scale=1.0), reads=['mv', 'epst'], writes=['lnv'])
            S.op('act', lambda e: e.activation(out=rstd[:], in_=lnv[:], func=AF.Exp, scale=-0.5), reads=['lnv'], writes=['rstd'])
            S.op('dve', lambda e: e.tensor_scalar(out=t1[:], in0=pre[:], scalar1=mv[:, 0:1], scalar2=rstd[:], op0=ALU.subtract, op1=ALU.mult), reads=['pre', 'mv', 'rstd'], writes=['t1'])
            S.op('dve', lambda e: e.tensor_tensor(out=t1[:], in0=t1[:], in1=bc['ln2g'][:], op=ALU.mult), reads=['t1', 'fbc_ln2g'], writes=['t1'])
            S.op('dve', lambda e: e.tensor_tensor(out=ob[sl][:], in0=t1[:], in1=bc['ln2b'][:], op=ALU.add), reads=['t1', 'fbc_ln2b'], writes=['fob_%d' % sl])
            S.dma('pool', lambda e: e.dma_start(out=K.out[i * 128:(i + 1) * 128, :], in_=ob[sl][:]), reads=['fob_%d' % sl], writes=['out'])
        S.barrier()


def build_program(debug=(), phases=None, dbg_in=(), opts=None):
    opts = opts or {}
    nc = bass.Bass("TRN2", target_bir_lowering=False)
    K = Ctx()
    K.nc = nc
    di = lambda n, s, d: nc.dram_tensor(n, s, d, kind="ExternalInput").ap()
    K.xin = di("xin", [TA, D], F32)
    K.ccT = di("ccT", [128, 8, 2], F32)
    K.w_ada = di("w_ada", [D, 6 * D], F32)
    K.b_ada = di("b_ada", [1, 6 * D], F32)
    K.w_in = di("w_in", [D, 2560], F32)
    K.cosT = di("cosT", [64, TL], F32)
    K.sinT = di("sinT", [64, TL], F32)
    K.ident = di("ident", [128, 128], BF16)
    K.identf = di("identf", [128, 128], F32)
    K.i64dbl = di("i64dbl", [128, 128], F32)
    K.bones = di("bones", [128, 128], F32)
    K.rwm = [di("rwm%d" % d, [128, 1344], F32) for d in range(2)]
    K.mla_q_norm = di("mla_q_norm", [1, 256], F32)
    K.mla_kv_norm = di("mla_kv_norm", [1, 256], F32)
    K.w_uq = di("w_uq", [256, 1024], F32)
    K.w_uk = di("w_uk", [256, 512], F32)
    K.w_uv = di("w_uv", [256, 512], F32)
    K.rwkv_conv = di("rwkv_conv", [3, 1536], F32)
    K.rwkv_w0 = di("rwkv_w0", [2, 512], F32)
    K.rwkv_w_up = di("rwkv_w_up", [2, 64, 512], F32)
    K.rwkv_a0 = di("rwkv_a0", [2, 512], F32)
    K.rwkv_a_up = di("rwkv_a_up", [2, 64, 512], F32)
    K.rwkv_g_up = di("rwkv_g_up", [128, 512], F32)
    K.rwkv_k_k = di("rwkv_k_k", [1, 512], F32)
    K.rwkv_k_a = di("rwkv_k_a", [1, 512], F32)
    K.rwkv_r_k = di("rwkv_r_k", [1, 512], F32)
    K.rwkv_gn_g = di("rwkv_gn_g", [1, 512], F32)
    K.rwkv_gn_b = di("rwkv_gn_b", [1, 512], F32)
    K.w_out = di("w_out", [D, D], F32)
    K.ln1_g = di("ln1_g", [1, D], F32)
    K.ln1_b = di("ln1_b", [1, D], F32)
    K.ln2_g = di("ln2_g", [1, D], F32)
    K.ln2_b = di("ln2_b", [1, D], F32)
    K.router = di("router", [D, NE], F32)
    K.ustrict = di("ustrict", [128, 128], BF16)
    K.exp_w_gate = di("exp_w_gate", [NE, D, D], F32)
    K.exp_w_up = di("exp_w_up", [NE, D, D], F32)
    K.exp_w_down = di("exp_w_down", [NE, D, D], F32)

    def scratch(n, s, d):
        if n in dbg_in:
            return nc.dram_tensor(n, s, d, kind="ExternalInput").ap()
        kind = "ExternalOutput" if n in debug else "Internal"
        return nc.dram_tensor(n, s, d, kind=kind).ap()
    K.modd = scratch("modd", [2, 6 * D], F32)
    K.hT = scratch("hT", [2432, TA], F32)
    K.krT = scratch("krT", [64, TA], BF16)
    K.qnT = scratch("qnT", [512, TL], BF16)
    K.qrT = scratch("qrT", [256, TL], BF16)
    K.knT = scratch("knT", [512, TA], BF16)
    K.vtok = scratch("vtok", [TA, 512], BF16)
    K.mixT = scratch("mixT", [1024, TL], BF16)
    K.rwR = scratch("rwR", [512, TA], F32)
    K.rwV = scratch("rwV", [512, TA], F32)
    K.rwKK = scratch("rwKK", [512, TA], F32)
    K.rwKD = [scratch("rwKD%d" % d, [512, TA], F32) for d in range(2)]
    K.rwB = [scratch("rwB%d" % d, [512, TA], F32) for d in range(2)]
    K.rwVtok = scratch("rwVtok", [TA, 512], F32)
    K.rwLD = [scratch("rwLD%d" % d, [TA, 512], F32) for d in range(2)]
    K.rwG = scratch("rwG", [512, TL], F32)
    K.rwBON = scratch("rwBON", [512, TL], F32)
    K.rwY0 = scratch("rwY0", [512, TL], F32)
    K.x1d = scratch("x1d", [TL, D], F32)
    K.urd = scratch("urd", [TL, ROWW], BF16)
    K.xe_d = [scratch("xe_d%d" % e_, [CAP, ROWW], BF16) for e_ in range(NE)]
    K.yacc = scratch("yacc", [TL, D], F32)
    K.dbg_pos = scratch("dbg_pos", [128, TL // 128, 16], F32) if 'dbg_pos' in debug else None
    K.out = nc.dram_tensor("out", [TL, D], F32, kind="ExternalOutput").ap()
    allp = ['mod', 'inproj', 'mlaprep', 'attn', 'rwprep', 'rwscan', 'mix', 'moe', 'final']
    if phases is None:
        phases = allp
    with ExitStack() as es:
        S = Sync(nc, es)
        K.S = S
        if 'mod' in phases:
            phase_mod(K)
        if 'inproj' in phases:
            phase_inproj(K)
        if 'mlaprep' in phases:
            phase_mlaprep(K)
        if 'attn' in phases:
            phase_attn(K)
        if 'attn1' in phases:
            phase_attn(K, heads=(1,), nqt=2)
        if 'rwprep' in phases:
            phase_rwprep(K)
        if 'rwscan' in phases:
            phase_rwscan(K, **opts.get('rwscan', {}))
        if 'mix' in phases:
            phase_mix(K)
        if 'moe' in phases:
            phase_moe(K, **opts.get('moe', {}))
        if 'final' in phases:
            phase_final(K)
        S.wait_all('sp')
        print("instructions", S.n_inst, "waits", S.n_wait, "sems", S.nsem + NDMA)
    return nc


_SWAP = np.concatenate([np.arange(16, 32), np.arange(0, 16), np.arange(48, 64), np.arange(32, 48)])


def rope_tables():
    half = 32
    inv_freq = (10000.0 ** (-np.arange(0, half, 2, dtype=np.float32) / half)).astype(np.float32)
    t = np.arange(TL)
    rr = (t // 64).astype(np.float32)[None, :]
    cc = (t % 64).astype(np.float32)[None, :]
    ang_r = (inv_freq[:, None] * rr).astype(np.float32)
    ang_c = (inv_freq[:, None] * cc).astype(np.float32)
    cosT = np.concatenate([np.cos(ang_r), np.cos(ang_r), np.cos(ang_c), np.cos(ang_c)], 0).astype(np.float32)
    sinT = np.concatenate([-np.sin(ang_r), np.sin(ang_r), -np.sin(ang_c), np.sin(ang_c)], 0).astype(np.float32)
    return np.ascontiguousarray(cosT), np.ascontiguousarray(sinT)


def make_in_maps(inputs, batches):
    f = lambda a: np.ascontiguousarray(np.asarray(a, dtype=np.float32))
    w_in = f(inputs['w_in'][0])
    w_in_ext = np.concatenate([w_in, w_in[:, 2432:2496][:, _SWAP]], axis=1)
    cosT, sinT = rope_tables()
    wuq = f(inputs['mla_w_uq'][0])
    cols = []
    for h in range(4):
        nope = wuq[:, h * 192:h * 192 + 128]
        rope = wuq[:, h * 192 + 128:h * 192 + 192]
        cols += [nope, rope, rope[:, _SWAP]]
    wuq_ext = np.ascontiguousarray(np.concatenate(cols, axis=1))
    shared = {
        'w_ada': f(inputs['w_ada'][0]), 'b_ada': f(inputs['b_ada']), 'w_in': np.ascontiguousarray(w_in_ext),
        'cosT': cosT, 'sinT': sinT, 'ident': np.eye(128).astype(ml_dtypes.bfloat16),
        'mla_q_norm': f(inputs['mla_q_norm']), 'mla_kv_norm': f(inputs['mla_kv_norm']),
        'w_uq': wuq_ext, 'w_uk': f(inputs['mla_w_uk'][0]), 'w_uv': f(inputs['mla_w_uv'][0]),
        'rwkv_conv': f(inputs['rwkv_conv'][0]), 'rwkv_w0': f(inputs['rwkv_w0'][0]), 'rwkv_w_up': f(inputs['rwkv_w_up'][0]),
        'rwkv_a0': f(inputs['rwkv_a0'][0]), 'rwkv_a_up': f(inputs['rwkv_a_up'][0]), 'rwkv_g_up': f(inputs['rwkv_g_up'][0]),
        'rwkv_k_k': f(inputs['rwkv_k_k']), 'rwkv_k_a': f(inputs['rwkv_k_a']), 'rwkv_r_k': f(inputs['rwkv_r_k']).reshape(1, 512),
        'rwkv_gn_g': f(inputs['rwkv_gn_g']), 'rwkv_gn_b': f(inputs['rwkv_gn_b']),
        'w_out': f(inputs['w_out'][0]), 'ln1_g': f(inputs['ln1_g']), 'ln1_b': f(inputs['ln1_b']),
        'ln2_g': f(inputs['ln2_g']), 'ln2_b': f(inputs['ln2_b']), 'router': f(inputs['router'][0]),
        'ustrict': (np.arange(128)[:, None] < np.arange(128)[None, :]).astype(ml_dtypes.bfloat16),
        'exp_w_gate': f(inputs['exp_w_gate'][0]), 'exp_w_up': f(inputs['exp_w_up'][0]), 'exp_w_down': f(inputs['exp_w_down'][0]),
    }
    shared.update(rw_consts())
    maps = []
    for b in batches:
        m = dict(shared)
        m['xin'] = np.ascontiguousarray(np.concatenate([inputs['ctx'][b], inputs['x'][b]], axis=0).astype(np.float32))
        cc = np.stack([inputs['c'][b], inputs['c_ctx']], axis=-1).astype(np.float32)
        m['ccT'] = np.ascontiguousarray(cc.reshape(8, 128, 2).transpose(1, 0, 2))
        maps.append(m)
    return maps


_CONST_KEYS = ('cosT', 'sinT', 'ident', 'identf', 'i64dbl', 'bones', 'rwm0', 'rwm1', 'ustrict')
BATCH_CORES = (0, 1, 4, 5)


def kernel(**inputs):
    nc = build_program()
    real = make_in_maps(inputs, [0, 1, 2, 3])
    zero = {k: (v if k in _CONST_KEYS else np.zeros_like(v)) for k, v in real[0].items()}
    maps = [zero] * 8
    for b, c in enumerate(BATCH_CORES):
        maps[c] = real[b]
    res = run_bass_kernel_spmd(nc, maps, core_ids=list(range(8)))
    out = np.stack([res.results[c]['out'] for c in BATCH_CORES], axis=0)
    return out.astype(np.float32)
```

```python
import numpy as np
import ml_dtypes
from contextlib import ExitStack
import concourse.bass as bass
import concourse.mybir as mybir
from concourse.bass_utils import run_bass_kernel_spmd

F32 = mybir.dt.float32
BF16 = mybir.dt.bfloat16
I32 = mybir.dt.int32
AF = mybir.ActivationFunctionType
ALU = mybir.AluOpType
AX = mybir.AxisListType

EPOCH = 12000
NDMA = 24

D = 1024
TL = 8192
TC = 256
TA = TL + TC
NE = 16
CAP = 1024


class Sync:
    def __init__(self, nc, es):
        self.nc = nc
        self.es = es
        self.eng = {'pe': nc.tensor, 'dve': nc.vector, 'act': nc.scalar,
                    'pool': nc.gpsimd, 'sp': nc.sync}
        self.sem = {}
        self.cnt = {}
        self.cur = {}
        self.known = {e: {} for e in self.eng}
        self.snap = {}
        self.last_w = {}
        self.readers = {}
        self.dma_keys = []
        self.dma_rr = 0
        self.nsem = 0
        for e in self.eng:
            self._new_epoch(e)
        for i in range(NDMA):
            k = ('dma', i)
            self.sem[k] = es.enter_context(nc.semaphore('dq%d' % i))
            self.cnt[k] = 0
            self.dma_keys.append(k)
        self.n_inst = 0
        self.n_wait = 0

    def _new_epoch(self, e):
        idx = self.nsem
        self.nsem += 1
        k = (e, idx)
        self.sem[k] = self.es.enter_context(self.nc.semaphore('s_%s_%d' % (e, idx)))
        self.cnt[k] = 0
        self.cur[e] = k

    def _need(self, e, ticket):
        k, v = ticket
        kn = self.known[e]
        if kn.get(k, 0) >= v:
            return
        self.eng[e].wait_ge(self.sem[k], v)
        self.n_wait += 1
        kn[k] = v
        sn = self.snap.get(ticket)
        if sn:
            for kk, vv in sn.items():
                if kn.get(kk, 0) < vv:
                    kn[kk] = vv

    def _deps(self, e, reads, writes, acc):
        for b in reads:
            t = self.last_w.get(b)
            if t is not None:
                self._need(e, t)
        for b in writes:
            t = self.last_w.get(b)
            if t is not None and not (acc and t[0] == self.cur[e]):
                self._need(e, t)
            for t in self.readers.get(b, ()):
                self._need(e, t)

    def _record(self, ticket, reads, writes):
        for b in reads:
            self.readers.setdefault(b, []).append(ticket)
        for b in writes:
            self.last_w[b] = ticket
            self.readers[b] = []

    def op(self, e, fn, reads=(), writes=(), acc=False):
        if self.cnt[self.cur[e]] >= EPOCH:
            self._new_epoch(e)
        self._deps(e, reads, writes, acc)
        k = self.cur[e]
        inst = fn(self.eng[e])
        inst.then_inc(self.sem[k], 1)
        self.cnt[k] += 1
        t = (k, self.cnt[k])
        self.snap[t] = dict(self.known[e])
        self._record(t, reads, writes)
        self.n_inst += 1
        return t

    def dma(self, e, fn, reads=(), writes=()):
        k = self.dma_keys[self.dma_rr]
        self.dma_rr = (self.dma_rr + 1) % NDMA
        if self.cnt[k] > 0:
            self._need(e, (k, self.cnt[k]))
        self._deps(e, reads, writes, False)
        inst = fn(self.eng[e])
        inst.then_inc(self.sem[k], 16)
        self.cnt[k] += 16
        t = (k, self.cnt[k])
        self.snap[t] = dict(self.known[e])
        self._record(t, reads, writes)
        self.n_inst += 1
        return t

    def wait_all(self, e):
        for k, v in list(self.cnt.items()):
            if v > 0:
                self._need(e, (k, v))

    def barrier(self):
        for e in self.eng:
            self.wait_all(e)
        self.last_w.clear()
        self.readers.clear()


class Ctx:
    pass


_UC = [0]


def _u(n):
    _UC[0] += 1
    return '%s_%d' % (n, _UC[0])


def phase_mod(K):
    nc, S = K.nc, K.S
    with ExitStack() as es:
        sb = lambda n, s, d: es.enter_context(nc.sbuf_tensor(_u(n), s, d))
        ccs = sb("ccs", [128, 8, 2], F32)
        scT = sb("scT", [128, 8, 2], F32)
        wa = [sb("wa%d" % i, [128, 8, 512], F32) for i in range(2)]
        ba = sb("ba", [1, 6144], F32)
        one1 = sb("one1", [1, 1], F32)
        mrow = [sb("mrow%d" % r, [1, 6144], F32) for r in range(2)]
        pm = [es.enter_context(nc.psum_tensor(_u("pm%d" % i), [1, 512], F32)) for i in range(2)]
        S.dma('sp', lambda e: e.dma_start(out=ccs[:], in_=K.ccT), writes=['ccs'])
        S.dma('sp', lambda e: e.dma_start(out=ba[:], in_=K.b_ada), writes=['ba'])
        S.op('dve', lambda e: e.memset(one1[:], 1.0), writes=['one1'])
        S.op('act', lambda e: e.activation(out=scT[:], in_=ccs[:], func=AF.Silu), reads=['ccs'], writes=['scT'])
        for j in range(12):
            w = wa[j % 2]
            wk = 'wa%d' % (j % 2)
            S.dma('sp', lambda e: e.dma_start(out=w[:], in_=K.w_ada[:, j * 512:(j + 1) * 512].rearrange("(k p) n -> p k n", p=128)), writes=[wk])
            for r in range(2):
                if r == 1 and j >= 4:
                    continue
                pk = 'pm%d' % r
                for k in range(8):
                    S.op('pe', lambda e: e.matmul(pm[r][:], lhsT=scT[:, k, r:r + 1], rhs=w[:, k, :], start=(k == 0), stop=False),
                         reads=['scT', wk], writes=[pk], acc=True)
                S.op('pe', lambda e: e.matmul(pm[r][:], lhsT=one1[:], rhs=ba[:, j * 512:(j + 1) * 512], start=False, stop=True),
                     reads=['one1', 'ba'], writes=[pk], acc=True)
                S.op('dve', lambda e: e.tensor_copy(out=mrow[r][:, j * 512:(j + 1) * 512], in_=pm[r][:]), reads=[pk], writes=['mrow%d' % r])
        S.dma('sp', lambda e: e.dma_start(out=K.modd[0:1, :], in_=mrow[0][:]), reads=['mrow0'], writes=['modd'])
        S.dma('sp', lambda e: e.dma_start(out=K.modd[1:2, 0:2048], in_=mrow[1][:, 0:2048]), reads=['mrow1'], writes=['modd'])
        S.barrier()


def phase_inproj(K):
    nc, S = K.nc, K.S
    with ExitStack() as es:
        sb = lambda n, s, d: es.enter_context(nc.sbuf_tensor(_u(n), s, d))
        ps = lambda n, s, d: es.enter_context(nc.psum_tensor(_u(n), s, d))
        wb = sb("wb", [128, 8, 2560], BF16)
        wst = [sb("wst%d" % i, [128, 8, 320], F32) for i in range(2)]
        idt = sb("idt", [128, 128], BF16)
        epst = sb("epst", [128, 1], F32)
        scp = [sb("scp%d" % r, [128, 8], F32) for r in range(2)]
        shp = [sb("shp%d" % r, [128, 8], F32) for r in range(2)]
        xt = [sb("xt%d" % i, [128, 1024], F32) for i in range(2)]
        xn = [sb("xn%d" % i, [128, 1024], BF16) for i in range(2)]
        st = sb("st", [128, 2, 6], F32)
        mv = sb("mv", [128, 2], F32)
        lnv = sb("lnv", [128, 1], F32)
        rstd = sb("rstd", [128, 1], F32)
        xmT = [sb("xmT%d" % i, [128, 8, 512], BF16) for i in range(2)]
        stg = [sb("stg%d" % i, [128, 4, 512], F32) for i in range(2)]
        cst = sb("cst", [64, 512], F32)
        snt = sb("snt", [64, 512], F32)
        kr1 = sb("kr1", [64, 512], F32)
        kr2 = sb("kr2", [64, 512], F32)
        krb = sb("krb", [64, 512], BF16)
        pt = [ps("pt%d" % i, [128, 1024], BF16) for i in range(2)]
        po = [ps("po%d" % i, [128, 512], F32) for i in range(3)]
        pk = [ps("pk%d" % i, [64, 512], F32) for i in range(2)]

        S.dma('sp', lambda e: e.dma_start(out=idt[:], in_=K.ident), writes=['idt'])
        S.op('dve', lambda e: e.memset(epst[:], 1e-5), writes=['epst'])
        for r in range(2):
            S.dma('sp', lambda e: e.dma_start(out=shp[r][:], in_=K.modd[r:r + 1, 0:1024].rearrange("o (k p) -> p (o k)", p=128), allow_slow_non_contiguous=True), reads=['modd'], writes=['shp%d' % r])
            S.dma('sp', lambda e: e.dma_start(out=scp[r][:], in_=K.modd[r:r + 1, 1024:2048].rearrange("o (k p) -> p (o k)", p=128), allow_slow_non_contiguous=True), reads=['modd'], writes=['scp%d' % r])
            S.op('dve', lambda e: e.tensor_scalar_add(out=scp[r][:], in0=scp[r][:], scalar1=1.0), reads=['scp%d' % r], writes=['scp%d' % r])
        for j in range(8):
            w = wst[j % 2]
            wk = 'wst%d' % (j % 2)
            S.dma('sp', lambda e: e.dma_start(out=w[:], in_=K.w_in[:, j * 320:(j + 1) * 320].rearrange("(k p) n -> p k n", p=128)), writes=[wk])
            S.op('pool', lambda e: e.tensor_copy(out=wb[:, :, j * 320:(j + 1) * 320], in_=w[:]), reads=[wk], writes=['wb'])

        groups = [(0, 256, 1)] + [(256 + g * 512, 512, 0) for g in range(16)]
        tile_i = 0
        ev = 0
        for gi, (t0, G, r) in enumerate(groups):
            xm = xmT[gi % 2]
            xmk = 'xmT%d' % (gi % 2)
            if r == 0:
                S.dma('sp', lambda e: e.dma_start(out=cst[:], in_=K.cosT[:, t0 - 256:t0 - 256 + 512]), writes=['cst'])
                S.dma('sp', lambda e: e.dma_start(out=snt[:], in_=K.sinT[:, t0 - 256:t0 - 256 + 512]), writes=['snt'])
            for i in range(G // 128):
                sl = tile_i % 2
                tile_i += 1
                xk, xnk, ptk = 'xt%d' % sl, 'xn%d' % sl, 'pt%d' % sl
                tt = t0 + i * 128
                S.dma('sp', lambda e: e.dma_start(out=xt[sl][:], in_=K.xin[tt:tt + 128, :]), writes=[xk])
                for c in range(2):
                    S.op('dve', lambda e: e.bn_stats(out=st[:, c, :], in_=xt[sl][:, c * 512:(c + 1) * 512]), reads=[xk], writes=['st%d' % c])
                S.op('dve', lambda e: e.bn_aggr(out=mv[:], in_=st[:]), reads=['st0', 'st1'], writes=['mv'])
                S.op('act', lambda e: e.activation(out=lnv[:], in_=mv[:, 1:2], func=AF.Ln, bias=epst[:], scale=1.0), reads=['mv', 'epst'], writes=['lnv'])
                S.op('act', lambda e: e.activation(out=rstd[:], in_=lnv[:], func=AF.Exp, scale=-0.5), reads=['lnv'], writes=['rstd'])
                S.op('dve', lambda e: e.tensor_scalar(out=xn[sl][:], in0=xt[sl][:], scalar1=mv[:, 0:1], scalar2=rstd[:], op0=ALU.subtract, op1=ALU.mult),
                     reads=[xk, 'mv', 'rstd'], writes=[xnk])
                for k in range(8):
                    S.op('pe', lambda e: e.transpose(out=pt[sl][:, k * 128:(k + 1) * 128], in_=xn[sl][:, k * 128:(k + 1) * 128], identity=idt[:]),
                         reads=[xnk, 'idt'], writes=[ptk], acc=True)
                for k in range(8):
                    S.op('act', lambda e: e.activation(out=xm[:, k, i * 128:(i + 1) * 128], in_=pt[sl][:, k * 128:(k + 1) * 128], func=AF.Identity,
                                                       bias=shp[r][:, k:k + 1], scale=scp[r][:, k:k + 1]),
                         reads=[ptk, 'shp%d' % r, 'scp%d' % r], writes=[xmk])
            for cb in range(5):
                sg = stg[cb % 2]
                sgk = 'stg%d' % (cb % 2)
                ncols = 4 if cb < 4 else 3
                for cc in range(ncols):
                    ci = cb * 4 + cc
                    p = po[ev % 3]
                    pkk = 'po%d' % (ev % 3)
                    for k in range(8):
                        S.op('pe', lambda e: e.matmul(p[:, 0:G], lhsT=wb[:, k, ci * 128:(ci + 1) * 128], rhs=xm[:, k, 0:G], start=(k == 0), stop=(k == 7)),
                             reads=['wb', xmk], writes=[pkk], acc=True)
                    if ev % 2 == 0:
                        S.op('dve', lambda e: e.tensor_copy(out=sg[:, cc, 0:G], in_=p[:, 0:G]), reads=[pkk], writes=[sgk])
                    else:
                        S.op('act', lambda e: e.activation(out=sg[:, cc, 0:G], in_=p[:, 0:G], func=AF.Identity), reads=[pkk], writes=[sgk])
                    ev += 1
                r0 = cb * 512
                S.dma('pool', lambda e: e.dma_start(out=K.hT[r0:r0 + ncols * 128, t0:t0 + G].rearrange("(c p) t -> p c t", p=128), in_=sg[:, 0:ncols, 0:G]),
                      reads=[sgk], writes=['hT'])
            for q in range(2):
                for k in range(8):
                    S.op('pe', lambda e: e.matmul(pk[q][:, 0:G], lhsT=wb[:, k, 2432 + q * 64:2432 + (q + 1) * 64], rhs=xm[:, k, 0:G], start=(k == 0), stop=(k == 7)),
                         reads=['wb', xmk], writes=['pk%d' % q], acc=True)
            if r == 0:
                S.op('dve', lambda e: e.tensor_tensor(out=kr1[:], in0=pk[0][:], in1=cst[:], op=ALU.mult), reads=['pk0', 'cst'], writes=['kr1'])
                S.op('dve', lambda e: e.tensor_tensor(out=kr2[:], in0=pk[1][:], in1=snt[:], op=ALU.mult), reads=['pk1', 'snt'], writes=['kr2'])
                S.op('dve', lambda e: e.tensor_tensor(out=krb[:], in0=kr1[:], in1=kr2[:], op=ALU.add), reads=['kr1', 'kr2'], writes=['krb'])
            else:
                S.op('dve', lambda e: e.tensor_copy(out=krb[:, 0:G], in_=pk[0][:, 0:G]), reads=['pk0', 'pk1'], writes=['krb'])
            S.dma('pool', lambda e: e.dma_start(out=K.krT[:, t0:t0 + G], in_=krb[:, 0:G]), reads=['krb'], writes=['krT'])
        S.barrier()


class Rot:
    def __init__(self, bufs, prefix):
        self.bufs = bufs
        self.prefix = prefix
        self.i = 0

    def get(self):
        j = self.i % len(self.bufs)
        self.i += 1
        return self.bufs[j], '%s%d' % (self.prefix, j)


SCALE_ATT = 192.0 ** -0.5


def phase_mlaprep(K):
    nc, S = K.nc, K.S
    with ExitStack() as es:
        sb = lambda n, s, d: es.enter_context(nc.sbuf_tensor(_u(n), s, d))
        ps = lambda n, s, d: es.enter_context(nc.psum_tensor(_u(n), s, d))
        wuq = sb("wuq", [128, 2, 1024], BF16)
        wuk = sb("wuk", [128, 2, 512], BF16)
        wuv = sb("wuv", [128, 2, 512], BF16)
        wst = [sb("mwst%d" % i, [128, 2, 512], F32) for i in range(2)]
        gq = sb("gq", [128, 2], F32)
        gkv = sb("gkv", [128, 2], F32)
        onesf = sb("onesf", [128, 128], F32)
        epst = sb("epst", [128, 1], F32)
        ql = [sb("ql%d" % i, [128, 4, 512], F32) for i in range(2)]
        sq = sb("sq", [128, 4, 512], F32)
        lnt = sb("lnt", [128, 512], F32)
        rs = [sb("rs%d" % i, [128, 512], F32) for i in range(2)]
        nb = [sb("nb%d" % i, [128, 4, 512], BF16) for i in range(2)]
        qst = [sb("qst%d" % i, [128, 4, 512], BF16) for i in range(2)]
        kst = [sb("kst%d" % i, [128, 4, 512], BF16) for i in range(2)]
        qrb = [sb("qrb%d" % i, [64, 4, 512], BF16) for i in range(2)]
        vst = Rot([sb("vst%d" % i, [128, 512], BF16) for i in range(3)], "vst")
        cst = sb("cst", [64, 512], F32)
        snt = sb("snt", [64, 512], F32)
        r1 = sb("r1", [64, 512], F32)
        r2 = sb("r2", [64, 512], F32)
        pp = Rot([ps("mp%d" % i, [128, 512], F32) for i in range(7)], "mp")

        S.op('dve', lambda e: e.memset(epst[:], 1e-6), writes=['epst'])
        S.op('dve', lambda e: e.memset(onesf[:], 1.0), writes=['onesf'])
        S.dma('sp', lambda e: e.dma_start(out=gq[:], in_=K.mla_q_norm.rearrange("o (c p) -> p (o c)", p=128), allow_slow_non_contiguous=True), writes=['gq'])
        S.dma('sp', lambda e: e.dma_start(out=gkv[:], in_=K.mla_kv_norm.rearrange("o (c p) -> p (o c)", p=128), allow_slow_non_contiguous=True), writes=['gkv'])
        wl = 0
        for (src, dst, dk, n) in [(K.w_uq, wuq, 'wuq', 1024), (K.w_uk, wuk, 'wuk', 512), (K.w_uv, wuv, 'wuv', 512)]:
            for j in range(n // 512):
                w = wst[wl % 2]
                wk = 'mwst%d' % (wl % 2)
                wl += 1
                S.dma('sp', lambda e: e.dma_start(out=w[:], in_=src[:, j * 512:(j + 1) * 512].rearrange("(c p) n -> p c n", p=128)), writes=[wk])
                S.op('pool', lambda e: e.tensor_copy(out=dst[:, :, j * 512:(j + 1) * 512], in_=w[:]), reads=[wk], writes=[dk])

        groups = [(0, 256, 1)] + [(256 + g * 512, 512, 0) for g in range(16)]
        ev = [0]

        def evac(out_ap, in_ap, reads, writes):
            if ev[0] % 2 == 0:
                S.op('dve', lambda e: e.tensor_copy(out=out_ap, in_=in_ap), reads=reads, writes=writes)
            else:
                S.op('act', lambda e: e.activation(out=out_ap, in_=in_ap, func=AF.Identity), reads=reads, writes=writes)
            ev[0] += 1

        for gi, (t0, G, r) in enumerate(groups):
            q_ = ql[gi % 2]
            qk = 'ql%d' % (gi % 2)
            n_ = nb[gi % 2]
            nk = 'nb%d' % (gi % 2)
            S.dma('sp', lambda e: e.dma_start(out=q_[:, :, 0:G], in_=K.hT[1920:2432, t0:t0 + G].rearrange("(c p) t -> p c t", p=128)), reads=['hT'], writes=[qk])
            if r == 0:
                S.dma('sp', lambda e: e.dma_start(out=cst[:], in_=K.cosT[:, t0 - 256:t0 - 256 + 512]), writes=['cst'])
                S.dma('sp', lambda e: e.dma_start(out=snt[:], in_=K.sinT[:, t0 - 256:t0 - 256 + 512]), writes=['snt'])
            S.op('act', lambda e: e.activation(out=sq[:, :, 0:G], in_=q_[:, :, 0:G], func=AF.Square), reads=[qk], writes=['sq'])
            for pair in range(2):
                if pair == 0 and r == 1:
                    continue
                p, pk = pp.get()
                for c in range(2):
                    S.op('pe', lambda e: e.matmul(p[:, 0:G], lhsT=onesf[:], rhs=sq[:, pair * 2 + c, 0:G], start=(c == 0), stop=(c == 1)),
                         reads=['onesf', 'sq'], writes=[pk], acc=True)
                S.op('act', lambda e: e.activation(out=lnt[:, 0:G], in_=p[:, 0:G], func=AF.Ln, bias=epst[:], scale=1.0 / 256.0), reads=[pk, 'epst'], writes=['lnt'])
                S.op('act', lambda e: e.activation(out=rs[pair][:, 0:G], in_=lnt[:, 0:G], func=AF.Exp, scale=-0.5), reads=['lnt'], writes=['rs%d' % pair])
                g_ = gq if pair == 0 else gkv
                for c in range(2):
                    S.op('dve', lambda e: e.scalar_tensor_tensor(out=n_[:, pair * 2 + c, 0:G], in0=q_[:, pair * 2 + c, 0:G], scalar=g_[:, c:c + 1], in1=rs[pair][:, 0:G],
                                                                 op0=ALU.mult, op1=ALU.mult),
                         reads=[qk, 'rs%d' % pair, 'gq', 'gkv'], writes=[nk])
            if r == 0:
                tq = t0 - 256
                qs = qst[gi % 2]
                qsk = 'qst%d' % (gi % 2)
                qr_ = qrb[gi % 2]
                qrk = 'qrb%d' % (gi % 2)
                for h in range(4):
                    p, pk = pp.get()
                    for c in range(2):
                        S.op('pe', lambda e: e.matmul(p[:, 0:G], lhsT=wuq[:, c, h * 256:h * 256 + 128], rhs=n_[:, c, 0:G], start=(c == 0), stop=(c == 1)),
                             reads=['wuq', nk], writes=[pk], acc=True)
                    evac(qs[:, h, 0:G], p[:, 0:G], [pk], [qsk])
                    p1, pk1 = pp.get()
                    p2, pk2 = pp.get()
                    for c in range(2):
                        S.op('pe', lambda e: e.matmul(p1[0:64, 0:G], lhsT=wuq[:, c, h * 256 + 128:h * 256 + 192], rhs=n_[:, c, 0:G], start=(c == 0), stop=(c == 1)),
                             reads=['wuq', nk], writes=[pk1], acc=True)
                    for c in range(2):
                        S.op('pe', lambda e: e.matmul(p2[0:64, 0:G], lhsT=wuq[:, c, h * 256 + 192:h * 256 + 256], rhs=n_[:, c, 0:G], start=(c == 0), stop=(c == 1)),
                             reads=['wuq', nk], writes=[pk2], acc=True)
                    S.op('dve', lambda e: e.tensor_tensor(out=r1[:], in0=p1[0:64, :], in1=cst[:], op=ALU.mult), reads=[pk1, 'cst'], writes=['r1'])
                    S.op('dve', lambda e: e.tensor_tensor(out=r2[:], in0=p2[0:64, :], in1=snt[:], op=ALU.mult), reads=[pk2, 'snt'], writes=['r2'])
                    S.op('dve', lambda e: e.tensor_tensor(out=qr_[:, h, :], in0=r1[:], in1=r2[:], op=ALU.add), reads=['r1', 'r2'], writes=[qrk])
                S.dma('pool', lambda e: e.dma_start(out=K.qnT[:, tq:tq + G].rearrange("(h p) t -> p h t", p=128), in_=qs[:, :, 0:G]), reads=[qsk], writes=['qnT'])
                S.dma('pool', lambda e: e.dma_start(out=K.qrT[:, tq:tq + G].rearrange("(h p) t -> p h t", p=64), in_=qr_[:, :, 0:G]), reads=[qrk], writes=['qrT'])
            ks = kst[gi % 2]
            ksk = 'kst%d' % (gi % 2)
            for h in range(4):
                p, pk = pp.get()
                for c in range(2):
                    S.op('pe', lambda e: e.matmul(p[:, 0:G], lhsT=wuk[:, c, h * 128:(h + 1) * 128], rhs=n_[:, 2 + c, 0:G], start=(c == 0), stop=(c == 1)),
                         reads=['wuk', nk], writes=[pk], acc=True)
                evac(ks[:, h, 0:G], p[:, 0:G], [pk], [ksk])
            S.dma('pool', lambda e: e.dma_start(out=K.knT[:, t0:t0 + G].rearrange("(h p) t -> p h t", p=128), in_=ks[:, :, 0:G]), reads=[ksk], writes=['knT'])
            for i in range(G // 128):
                p, pk = pp.get()
                for c in range(2):
                    S.op('pe', lambda e: e.matmul(p[:, :], lhsT=n_[:, 2 + c, i * 128:(i + 1) * 128], rhs=wuv[:, c, :], start=(c == 0), stop=(c == 1)),
                         reads=['wuv', nk], writes=[pk], acc=True)
                v_, vk = vst.get()
                evac(v_[:], p[:], [pk], [vk])
                S.dma('pool', lambda e: e.dma_start(out=K.vtok[t0 + i * 128:t0 + (i + 1) * 128, :], in_=v_[:]), reads=[vk], writes=['vtok'])
        S.barrier()


def phase_attn(K, heads=(0, 1, 2, 3), nqt=16):
    nc, S = K.nc, K.S
    NKT = TA // 128
    with ExitStack() as es:
        sb = lambda n, s, d: es.enter_context(nc.sbuf_tensor(_u(n), s, d))
        ps = lambda n, s, d: es.enter_context(nc.psum_tensor(_u(n), s, d))
        krs = sb("krs", [64, TA], BF16)
        kn = [sb("kn%d" % i, [128, TA], BF16) for i in range(2)]
        vh = [sb("vh%d" % i, [128, NKT, 128], BF16) for i in range(2)]
        qn = [sb("qn%d" % i, [128, 512], BF16) for i in range(2)]
        qr = [sb("qr%d" % i, [64, 512], BF16) for i in range(2)]
        onesb = sb("onesb", [128, 128], BF16)
        pT = Rot([sb("pT%d" % i, [128, 512], BF16) for i in range(3)], "pT")
        rl = sb("rl", [128, 512], F32)
        ob = [sb("ob%d" % i, [128, 512], BF16) for i in range(2)]
        psc = Rot([ps("psc%d" % i, [128, 512], F32) for i in range(3)], "psc")
        pO = [ps("pO%d" % i, [128, 512], F32) for i in range(2)]
        pL = [ps("pL%d" % i, [128, 512], F32) for i in range(2)]

        S.op('dve', lambda e: e.memset(onesb[:], 1.0), writes=['onesb'])
        S.dma('sp', lambda e: e.dma_start(out=krs[:], in_=K.krT), reads=['krT'], writes=['krs'])
        qi = 0
        for hi, h in enumerate(heads):
            k_ = kn[hi % 2]
            kk = 'kn%d' % (hi % 2)
            v_ = vh[hi % 2]
            vk = 'vh%d' % (hi % 2)
            S.dma('sp', lambda e: e.dma_start(out=k_[:], in_=K.knT[h * 128:(h + 1) * 128, :]), reads=['knT'], writes=[kk])
            S.dma('sp', lambda e: e.dma_start(out=v_[:], in_=K.vtok[:, h * 128:(h + 1) * 128].rearrange("(kt p) d -> p kt d", p=128)), reads=['vtok'], writes=[vk])
            for qt in range(nqt):
                sl = qi % 2
                qi += 1
                qnk, qrk = 'qn%d' % sl, 'qr%d' % sl
                S.dma('sp', lambda e: e.dma_start(out=qn[sl][:], in_=K.qnT[h * 128:(h + 1) * 128, qt * 512:(qt + 1) * 512]), reads=['qnT'], writes=[qnk])
                S.dma('sp', lambda e: e.dma_start(out=qr[sl][:], in_=K.qrT[h * 64:(h + 1) * 64, qt * 512:(qt + 1) * 512]), reads=['qrT'], writes=[qrk])
                Ok, Lk = 'pO%d' % sl, 'pL%d' % sl
                pend = []

                def scores(kt):
                    p, pk = psc.get()
                    S.op('pe', lambda e: e.matmul(p[:], lhsT=k_[:, kt * 128:(kt + 1) * 128], rhs=qn[sl][:], start=True, stop=False),
                         reads=[kk, qnk], writes=[pk], acc=True)
                    S.op('pe', lambda e: e.matmul(p[:], lhsT=krs[:, kt * 128:(kt + 1) * 128], rhs=qr[sl][:], start=False, stop=True),
                         reads=['krs', qrk], writes=[pk], acc=True)
                    t_, tk = pT.get()
                    S.op('act', lambda e: e.activation(out=t_[:], in_=p[:], func=AF.Exp, scale=SCALE_ATT), reads=[pk], writes=[tk])
                    pend.append((kt, t_, tk))

                def pv():
                    kt, t_, tk = pend.pop(0)
                    S.op('pe', lambda e: e.matmul(pO[sl][:], lhsT=v_[:, kt, :], rhs=t_[:], start=(kt == 0), stop=(kt == NKT - 1)),
                         reads=[vk, tk], writes=[Ok], acc=True)
                    S.op('pe', lambda e: e.matmul(pL[sl][:], lhsT=onesb[:], rhs=t_[:], start=(kt == 0), stop=(kt == NKT - 1)),
                         reads=['onesb', tk], writes=[Lk], acc=True)

                scores(0)
                scores(1)
                for kt in range(NKT):
                    pv()
                    if kt + 2 < NKT:
                        scores(kt + 2)
                S.op('dve', lambda e: e.reciprocal(out=rl[:], in_=pL[sl][:]), reads=[Lk], writes=['rl'])
                S.op('dve', lambda e: e.tensor_tensor(out=ob[sl][:], in0=pO[sl][:], in1=rl[:], op=ALU.mult), reads=[Ok, 'rl'], writes=['ob%d' % sl])
                S.dma('pool', lambda e: e.dma_start(out=K.mixT[512 + h * 128:512 + (h + 1) * 128, qt * 512:(qt + 1) * 512], in_=ob[sl][:]), reads=['ob%d' % sl], writes=['mixT'])
        S.barrier()


F32R = mybir.dt.float32r
LDS = -0.6065306597126334


def phase_rwprep(K):
    nc, S = K.nc, K.S
    with ExitStack() as es:
        sb = lambda n, s, d: es.enter_context(nc.sbuf_tensor(_u(n), s, d))
        ps = lambda n, s, d: es.enter_context(nc.psum_tensor(_u(n), s, d))
        G = 256
        cw = sb("cw", [128, 3, 12], F32)
        kkv = sb("kkv", [128, 4], F32)
        kav = sb("kav", [128, 4], F32)
        omka = sb("omka", [128, 4], F32)
        rkv = sb("rkv", [128, 4], F32)
        a0v = sb("a0v", [128, 2, 4], F32)
        w0r = sb("w0r", [1, 2, 512], F32)
        ones1 = sb("ones1", [1, 128], F32)
        wup = sb("wup", [64, 2, 512], F32)
        aup = sb("aup", [64, 2, 512], F32)
        gup = sb("gup", [128, 512], F32)
        bones = sb("bones", [128, 128], F32)
        idf = sb("idf", [128, 128], F32)
        eps12 = sb("eps12", [128, 1], F32)
        hr = [sb("hr%d" % i, [128, 12, G + 2], F32) for i in range(2)]
        lo = [sb("lo%d" % i, [64, 4, G], F32) for i in range(2)]
        gd = [sb("gd%d" % i, [128, G], F32) for i in range(2)]
        cv = sb("cv", [128, 12, G], F32)
        tw = sb("tw", [64, 2, G], F32)
        sg = sb("sg", [128, G], F32)
        kq = sb("kq", [128, 4, G], F32)
        sq = sb("sq", [128, 4, G], F32)
        lnt = sb("lnt", [128, 4, G], F32)
        kk_ = sb("kk_", [128, 4, G], F32)
        av = sb("av", [128, 4, G], F32)
        tt = sb("tt", [128, 4, G], F32)
        kd = [sb("kd%d" % i, [128, 4, G], F32) for i in range(2)]
        bb = sb("bb", [128, 4, G], F32)
        ld = Rot([sb("ld%d" % i, [128, 512], F32) for i in range(2)], "ld")
        vt = Rot([sb("vt%d" % i, [128, 512], F32) for i in range(2)], "vt")
        gg = sb("gg", [128, 4, G], F32)
        bc = sb("bc", [128, 4, G], F32)
        bon = sb("bon", [128, 4, G], F32)
        pp = Rot([ps("rp%d" % i, [128, 512], F32) for i in range(7)], "rp")

        ld1 = lambda dst, src, key: S.dma('sp', lambda e: e.dma_start(out=dst, in_=src, allow_slow_non_contiguous=True), writes=[key])
        ld1(cw[:], K.rwkv_conv.rearrange("t (c p) -> p t c", p=128), 'cw')
        ld1(kkv[:], K.rwkv_k_k.rearrange("o (c p) -> p (o c)", p=128), 'kkv')
        ld1(kav[:], K.rwkv_k_a.rearrange("o (c p) -> p (o c)", p=128), 'kav')
        ld1(rkv[:], K.rwkv_r_k.rearrange("o (c p) -> p (o c)", p=128), 'rkv')
        ld1(a0v[:], K.rwkv_a0.rearrange("d (c p) -> p d c", p=128), 'a0v')
        ld1(w0r[:], K.rwkv_w0.rearrange("(o d) n -> o d n", o=1), 'w0r')
        ld1(wup[:], K.rwkv_w_up.rearrange("d l n -> l d n"), 'wup')
        ld1(aup[:], K.rwkv_a_up.rearrange("d l n -> l d n"), 'aup')
        ld1(gup[:], K.rwkv_g_up, 'gup')
        ld1(bones[:], K.bones, 'bones')
        ld1(idf[:], K.identf, 'idf')
        S.op('dve', lambda e: e.memset(ones1[:], 1.0), writes=['ones1'])
        S.op('dve', lambda e: e.memset(eps12[:], 1e-12), writes=['eps12'])
        S.op('dve', lambda e: e.tensor_scalar(out=omka[:], in0=kav[:], scalar1=-1.0, scalar2=1.0, op0=ALU.mult, op1=ALU.add), reads=['kav'], writes=['omka'])

        nblk = TA // G
        for bi in range(nblk):
            t0 = bi * G
            lat = t0 >= TC
            sl = bi % 2
            h_ = hr[sl]
            hk = 'hr%d' % sl
            first = (t0 == 0 or t0 == TC)
            last = (t0 + G == TC or t0 + G == TA)
            c0 = 1 if first else 0
            c1 = G + 1 if last else G + 2
            if first:
                S.op('pool', lambda e: e.memset(h_[:, :, 0:1], 0.0), writes=[hk])
            if last:
                S.op('pool', lambda e: e.memset(h_[:, :, G + 1:G + 2], 0.0), writes=[hk])
            for q in range(3):
                S.dma('sp', lambda e: e.dma_start(out=h_[:, q * 4:(q + 1) * 4, c0:c1], in_=K.hT[q * 512:(q + 1) * 512, t0 - 1 + c0:t0 - 1 + c1].rearrange("(c p) t -> p c t", p=128)),
                      reads=['hT'], writes=[hk])
            lo_ = lo[sl]
            lk = 'lo%d' % sl
            S.dma('sp', lambda e: e.dma_start(out=lo_[:], in_=K.hT[1536:1792, t0:t0 + G].rearrange("(c p) t -> p c t", p=64)), reads=['hT'], writes=[lk])
            gd_ = gd[sl]
            gk = 'gd%d' % sl
            if lat:
                S.dma('sp', lambda e: e.dma_start(out=gd_[:], in_=K.hT[1792:1920, t0:t0 + G]), reads=['hT'], writes=[gk])
            for c in range(12):
                S.op('act', lambda e: e.activation(out=cv[:, c, :], in_=h_[:, c, 1:G + 1], func=AF.Identity, scale=cw[:, 1, c:c + 1]), reads=[hk, 'cw'], writes=['cv%d' % c])
                S.op('dve', lambda e: e.scalar_tensor_tensor(out=cv[:, c, :], in0=h_[:, c, 0:G], scalar=cw[:, 0, c:c + 1], in1=cv[:, c, :], op0=ALU.mult, op1=ALU.add),
                     reads=[hk, 'cw', 'cv%d' % c], writes=['cv%d' % c])
                S.op('dve', lambda e: e.scalar_tensor_tensor(out=cv[:, c, :], in0=h_[:, c, 2:G + 2], scalar=cw[:, 2, c:c + 1], in1=cv[:, c, :], op0=ALU.mult, op1=ALU.add),
                     reads=[hk, 'cw', 'cv%d' % c], writes=['cv%d' % c])
            cvr = ['cv%d' % c for c in range(0, 4)]
            cvk = ['cv%d' % c for c in range(4, 8)]
            cvv = ['cv%d' % c for c in range(8, 12)]
            S.dma('pool', lambda e: e.dma_start(out=K.rwR[:, t0:t0 + G].rearrange("(c p) t -> p c t", p=128), in_=cv[:, 0:4, :]), reads=cvr, writes=['rwR'])
            S.dma('pool', lambda e: e.dma_start(out=K.rwV[:, t0:t0 + G].rearrange("(c p) t -> p c t", p=128), in_=cv[:, 8:12, :]), reads=cvv, writes=['rwV'])
            for i in range(G // 128):
                p, pk = pp.get()
                for c in range(4):
                    S.op('pe', lambda e: e.transpose(out=p[:, c * 128:(c + 1) * 128], in_=cv[:, 8 + c, i * 128:(i + 1) * 128], identity=idf[:]), reads=cvv + ['idf'], writes=[pk], acc=True)
                v_, vk = vt.get()
                S.op('act', lambda e: e.activation(out=v_[:], in_=p[:], func=AF.Identity), reads=[pk], writes=[vk])
                S.dma('pool', lambda e: e.dma_start(out=K.rwVtok[t0 + i * 128:t0 + (i + 1) * 128, :], in_=v_[:]), reads=[vk], writes=['rwVtok'])
            for c in range(4):
                S.op('dve', lambda e: e.tensor_scalar_mul(out=kq[:, c, :], in0=cv[:, 4 + c, :], scalar1=kkv[:, c:c + 1]), reads=cvk + ['kkv'], writes=['kq'])
            S.op('act', lambda e: e.activation(out=sq[:], in_=kq[:], func=AF.Square), reads=['kq'], writes=['sq'])
            for c2 in range(2):
                p, pk = pp.get()
                S.op('pe', lambda e: e.matmul(p[:, 0:2 * G], lhsT=bones[:], rhs=sq[:, 2 * c2:2 * c2 + 2, :], start=True, stop=True), reads=['bones', 'sq'], writes=[pk])
                S.op('act', lambda e: e.activation(out=lnt[:, 2 * c2:2 * c2 + 2, :], in_=p[:, 0:2 * G], func=AF.Ln, bias=eps12[:], scale=1.0), reads=[pk, 'eps12'], writes=['lnt'])
            S.op('act', lambda e: e.activation(out=lnt[:], in_=lnt[:], func=AF.Exp, scale=-0.5), reads=['lnt'], writes=['lnt'])
            S.op('dve', lambda e: e.tensor_tensor(out=kk_[:], in0=kq[:], in1=lnt[:], op=ALU.mult), reads=['kq', 'lnt'], writes=['kk_'])
            S.dma('pool', lambda e: e.dma_start(out=K.rwKK[:, t0:t0 + G].rearrange("(c p) t -> p c t", p=128), in_=kk_[:]), reads=['kk_'], writes=['rwKK'])
            S.op('act', lambda e: e.activation(out=tw[:], in_=lo_[:, 0:2, :], func=AF.Tanh), reads=[lk], writes=['tw'])
            for d in range(2):
                for i in range(G // 128):
                    p, pk = pp.get()
                    S.op('pe', lambda e: e.matmul(p[:], lhsT=tw[:, d, i * 128:(i + 1) * 128], rhs=wup[:, d, :], start=True, stop=False), reads=['tw', 'wup'], writes=[pk], acc=True)
                    S.op('pe', lambda e: e.matmul(p[:], lhsT=ones1[:], rhs=w0r[:, d, :], start=False, stop=True), reads=['ones1', 'w0r'], writes=[pk], acc=True)
                    l_, lk2 = ld.get()
                    S.op('act', lambda e: e.activation(out=l_[:], in_=p[:], func=AF.Sigmoid), reads=[pk], writes=[lk2])
                    S.dma('pool', lambda e: e.dma_start(out=K.rwLD[d][t0 + i * 128:t0 + (i + 1) * 128, :], in_=l_[:]), reads=[lk2], writes=['rwLD%d' % d])
                for c in range(4):
                    p, pk = pp.get()
                    S.op('pe', lambda e: e.matmul(p[:, 0:G], lhsT=aup[:, d, c * 128:(c + 1) * 128], rhs=lo_[:, 2 + d, :], start=True, stop=True), reads=['aup', lk], writes=[pk])
                    S.op('act', lambda e: e.activation(out=av[:, c, :], in_=p[:, 0:G], func=AF.Sigmoid, bias=a0v[:, d, c:c + 1], scale=1.0), reads=[pk, 'a0v'], writes=['av'])
                    S.op('dve', lambda e: e.tensor_scalar(out=tt[:, c, :], in0=av[:, c, :], scalar1=kav[:, c:c + 1], scalar2=omka[:, c:c + 1], op0=ALU.mult, op1=ALU.add),
                         reads=['av', 'kav', 'omka'], writes=['tt'])
                S.op('dve', lambda e: e.tensor_tensor(out=kd[d][:], in0=cv[:, 4:8, :], in1=tt[:], op=ALU.mult), reads=cvk + ['tt'], writes=['kd%d' % d])
                S.op('dve', lambda e: e.tensor_tensor(out=bb[:], in0=kk_[:], in1=av[:], op=ALU.mult), reads=['kk_', 'av'], writes=['bb'])
                S.dma('pool', lambda e: e.dma_start(out=K.rwKD[d][:, t0:t0 + G].rearrange("(c p) t -> p c t", p=128), in_=kd[d][:]), reads=['kd%d' % d], writes=['rwKD%d' % d])
                S.dma('pool', lambda e: e.dma_start(out=K.rwB[d][:, t0:t0 + G].rearrange("(c p) t -> p c t", p=128), in_=bb[:]), reads=['bb'], writes=['rwB%d' % d])
            if lat:
                tl = t0 - TC
                S.op('act', lambda e: e.activation(out=sg[:], in_=gd_[:], func=AF.Sigmoid), reads=[gk], writes=['sg'])
                for c in range(4):
                    p, pk = pp.get()
                    S.op('pe', lambda e: e.matmul(p[:, 0:G], lhsT=gup[:, c * 128:(c + 1) * 128], rhs=sg[:], start=True, stop=True), reads=['gup', 'sg'], writes=[pk])
                    S.op('act', lambda e: e.activation(out=gg[:, c, :], in_=p[:, 0:G], func=AF.Identity), reads=[pk], writes=['gg'])
                S.dma('pool', lambda e: e.dma_start(out=K.rwG[:, tl:tl + G].rearrange("(c p) t -> p c t", p=128), in_=gg[:]), reads=['gg'], writes=['rwG'])
                S.op('dve', lambda e: e.tensor_tensor(out=bc[:], in0=kd[0][:], in1=kd[1][:], op=ALU.add), reads=['kd0', 'kd1'], writes=['bc'])
                S.op('dve', lambda e: e.tensor_tensor(out=bc[:], in0=bc[:], in1=cv[:, 0:4, :], op=ALU.mult), reads=['bc'] + cvr, writes=['bc'])
                for c in range(4):
                    S.op('dve', lambda e: e.tensor_scalar_mul(out=bc[:, c, :], in0=bc[:, c, :], scalar1=rkv[:, c:c + 1]), reads=['bc', 'rkv'], writes=['bc'])
                for c2 in range(2):
                    p, pk = pp.get()
                    S.op('pe', lambda e: e.matmul(p[:, 0:2 * G], lhsT=bones[:], rhs=bc[:, 2 * c2:2 * c2 + 2, :], start=True, stop=True), reads=['bones', 'bc'], writes=[pk])
                    S.op('dve', lambda e: e.tensor_tensor(out=bon[:, 2 * c2:2 * c2 + 2, :], in0=p[:, 0:2 * G], in1=cv[:, 8 + 2 * c2:8 + 2 * c2 + 2, :], op=ALU.mult), reads=[pk] + cvv, writes=['bon'])
                S.dma('pool', lambda e: e.dma_start(out=K.rwBON[:, tl:tl + G].rearrange("(c p) t -> p c t", p=128), in_=bon[:]), reads=['bon'], writes=['rwBON'])
        S.barrier()


def rw_consts():
    i = np.arange(128)[:, None]
    t = np.arange(128)[None, :]
    bd = (i // 64) == (t // 64)
    out = {}
    blk = ((i // 64) == (t // 64)).astype(np.float32)
    for d in range(2):
        strict = (bd & ((i < t) if d == 0 else (i > t))).astype(np.float32)
        incl = (bd & ((i <= t) if d == 0 else (i >= t))).astype(np.float32)
        m1 = np.concatenate([-strict, incl, blk], 1)
        m2 = np.concatenate([incl, blk], 1)
        m3 = np.concatenate([-strict.T, -strict.T, -np.ones((128, 64), np.float32)], 1)
        cum = np.float32(LDS) * np.concatenate([incl, strict, strict.T], 1)
        out['rwm%d' % d] = np.ascontiguousarray(np.concatenate([m1, m2, m3, cum], 1).astype(np.float32))
    out['i64dbl'] = np.ascontiguousarray((np.arange(128)[:, None] % 64 == np.arange(128)[None, :] % 64).astype(np.float32))
    out['bones'] = np.ascontiguousarray(bd.astype(np.float32))
    out['identf'] = np.eye(128, dtype=np.float32)
    return out


GN_EPS = 64e-5


def phase_rwscan(K, ntile_lat=64):
    nc, S = K.nc, K.S
    G = 128
    with ExitStack() as es:
        sb = lambda n, s, d: es.enter_context(nc.sbuf_tensor(_u(n), s, d))
        ps = lambda n, s, d: es.enter_context(nc.psum_tensor(_u(n), s, d))
        mk = sb("mk", [128, 1344], F32)
        idr = sb("idr", [128, 128], F32R)
        i64r = sb("i64r", [128, 128], F32R)
        i64f = sb("i64f", [128, 128], F32)
        idf = sb("idf", [128, 128], F32)
        o64 = sb("o64", [64, 64], F32)
        gng = sb("gng", [64, 8], F32)
        gnb = sb("gnb", [64, 8], F32)
        epsg = sb("epsg", [64, 1], F32)
        raw = [[sb("raw%d_%d" % (j, i), [128, 4, G], F32) for i in range(2)] for j in range(4)]
        vraw = [sb("vraw%d" % i, [128, 512], F32) for i in range(2)]
        ldt = [sb("ldt%d" % i, [128, 512], F32) for i in range(2)]
        vtr = [sb("vtr%d" % i, [128, 512], F32R) for i in range(2)]
        Ep = sb("Ep", [128, 4, G], F32)
        Em = sb("Em", [128, 4, G], F32)
        Ex = sb("Ex", [128, 4, G], F32)
        Ea = sb("Ea", [128, 4, G], F32)
        KRG = sb("KRG", [128, 4, 3, 128], F32R)
        BK = sb("BK", [128, 4, 2, 128], F32R)
        KB2 = sb("KB2", [128, 4, 2, 128], F32R)
        NS = 4
        La = [sb("La%d" % i, [128, 384], F32R) for i in range(NS)]
        Lb = [sb("Lb%d" % i, [128, 384], F32R) for i in range(NS)]
        ATa = [sb("ATa%d" % i, [128, 128], F32R) for i in range(NS)]
        ATb = [sb("ATb%d" % i, [128, 128], F32R) for i in range(NS)]
        X1 = [sb("X1%d" % i, [128, 320], F32R) for i in range(NS)]
        X2 = [sb("X2%d" % i, [128, 256], F32R) for i in range(NS)]
        Xf = [sb("Xf%d" % i, [128, 256], F32R) for i in range(NS)]
        QG1 = [sb("QG1_%d" % i, [64, 8, 256], F32R) for i in range(2)]
        QG2 = [sb("QG2_%d" % i, [128, 8, 256], F32R) for i in range(2)]
        Sth = sb("Sth", [64, 3, 8, 64], F32R)
        T = [sb("T%d" % i, [64, 8, G], F32) for i in range(4)]
        outb = sb("outb", [64, 8, G], BF16)
        pp = Rot([ps("sp%d" % i, [128, 512], F32) for i in range(7)], "sp")
        pst = ps("pst", [64, 512], F32)

        ld1 = lambda dst, src, key: S.dma('sp', lambda e: e.dma_start(out=dst, in_=src, allow_slow_non_contiguous=True), writes=[key])
        ld1(idf[:], K.identf, 'idf')
        ld1(i64f[:], K.i64dbl, 'i64f')
        ld1(gng[:], K.rwkv_gn_g.rearrange("o (h p) -> p (o h)", p=64), 'gng')
        ld1(gnb[:], K.rwkv_gn_b.rearrange("o (h p) -> p (o h)", p=64), 'gnb')
        S.op('dve', lambda e: e.tensor_copy(out=idr[:], in_=idf[:]), reads=['idf'], writes=['idr'])
        S.op('dve', lambda e: e.tensor_copy(out=i64r[:], in_=i64f[:]), reads=['i64f'], writes=['i64r'])
        S.op('dve', lambda e: e.memset(o64[:], 1.0 / 64.0), writes=['o64'])
        S.op('dve', lambda e: e.memset(epsg[:], GN_EPS), writes=['epsg'])
        zt = sb("zt", [64, 512], F32)
        S.op('dve', lambda e: e.memset(zt[:], 0.0), writes=['zt'])

        evq = [0]

        def evac(out_ap, in_ap, reads, writes):
            if evq[0] % 2 == 0:
                S.op('act', lambda e: e.activation(out=out_ap, in_=in_ap, func=AF.Identity), reads=reads, writes=writes)
            else:
                S.op('dve', lambda e: e.tensor_copy(out=out_ap, in_=in_ap), reads=reads, writes=writes)
            evq[0] += 1

        fl = lambda a: a[:, :, :].rearrange("p h t -> p (h t)")
        bcount = 0
        for d in range(2):
            m1 = mk[:, 0:384]
            m2 = mk[:, 384:640]
            m3 = mk[:, 640:960]
            cum = mk[:, 960:1344]
            S.dma('sp', lambda e: e.dma_start(out=mk[:], in_=K.rwm[d]), writes=['mk'])
            S.op('dve', lambda e: e.tensor_copy(out=Sth[:, 0].rearrange("p h v -> p (h v)"), in_=zt[:]), reads=['zt'], writes=['Sth0'])
            if d == 0:
                blocks = [0, 1] + list(range(2, 2 + ntile_lat))
            else:
                blocks = [1, 0] + list(range(1 + ntile_lat, 1, -1))
            chunks = [0, 1] if d == 0 else [1, 0]
            def body(b, qs):
                nonlocal bcount
                t0 = b * G
                lat = b >= 2
                sl = bcount % 2
                bcount += 1
                srcs = [K.rwR, K.rwKD[d], K.rwKK, K.rwB[d]]
                rk = ['raw%d_%d' % (j, sl) for j in range(4)]
                for j in range(4):
                    S.dma('sp', lambda e: e.dma_start(out=raw[j][sl][:], in_=srcs[j][:, t0:t0 + G].rearrange("(c p) t -> p c t", p=128)), writes=[rk[j]])
                S.dma('sp', lambda e: e.dma_start(out=vraw[sl][:], in_=K.rwVtok[t0:t0 + G, :]), writes=['vraw%d' % sl])
                S.dma('sp', lambda e: e.dma_start(out=ldt[sl][:], in_=K.rwLD[d][t0:t0 + G, :]), writes=['ldt%d' % sl])
                r_, kd_, kk_, b_ = [raw[j][sl] for j in range(4)]
                S.op('act', lambda e: e.activation(out=vtr[qs][:], in_=vraw[sl][:], func=AF.Identity), reads=['vraw%d' % sl], writes=['vtr%d' % qs])
                banks = [pp.get() for _ in range(3)]
                for c in range(4):
                    for q in range(3):
                        S.op('pe', lambda e: e.matmul(banks[q][0][:, c * 128:(c + 1) * 128], lhsT=ldt[sl][:, c * 128:(c + 1) * 128], rhs=cum[:, q * 128:(q + 1) * 128], start=True, stop=True),
                             reads=['ldt%d' % sl, 'mk'], writes=[banks[q][1]], acc=True)
                v3 = lambda bank: bank[:, :].rearrange("p (c t) -> p c t", c=4)
                S.op('act', lambda e: e.activation(out=Ep[:], in_=v3(banks[0][0]), func=AF.Exp), reads=[banks[0][1]], writes=['Ep'])
                S.op('act', lambda e: e.activation(out=Em[:], in_=v3(banks[0][0]), func=AF.Exp, scale=-1.0), reads=[banks[0][1]], writes=['Em'])
                S.op('act', lambda e: e.activation(out=Ex[:], in_=v3(banks[1][0]), func=AF.Exp), reads=[banks[1][1]], writes=['Ex'])
                S.op('act', lambda e: e.activation(out=Ea[:], in_=v3(banks[2][0]), func=AF.Exp), reads=[banks[2][1]], writes=['Ea'])
                yield 'f'
                S.op('dve', lambda e: e.tensor_tensor(out=KRG[:, :, 0, :], in0=kk_[:], in1=Ex[:], op=ALU.mult), reads=[rk[2], 'Ex'], writes=['KRG'])
                S.op('dve', lambda e: e.tensor_tensor(out=KRG[:, :, 1, :], in0=r_[:], in1=Ep[:], op=ALU.mult), reads=[rk[0], 'Ep'], writes=['KRG'])
                S.op('dve', lambda e: e.tensor_tensor(out=BK[:, :, 0, :], in0=b_[:], in1=Em[:], op=ALU.mult), reads=[rk[3], 'Em'], writes=['BK'])
                S.op('dve', lambda e: e.tensor_tensor(out=BK[:, :, 1, :], in0=kd_[:], in1=Em[:], op=ALU.mult), reads=[rk[1], 'Em'], writes=['BK'])
                S.op('dve', lambda e: e.tensor_tensor(out=KB2[:, :, 0, :], in0=kd_[:], in1=Ea[:], op=ALU.mult), reads=[rk[1], 'Ea'], writes=['KB2'])
                S.op('dve', lambda e: e.tensor_tensor(out=KB2[:, :, 1, :], in0=b_[:], in1=Ea[:], op=ALU.mult), reads=[rk[3], 'Ea'], writes=['KB2'])
                for cc in range(2):
                    pos = cc * 64 + (63 if d == 0 else 0)
                    in0 = i64f[:, cc * 64:(cc + 1) * 64].unsqueeze(1).to_broadcast([128, 4, 64])
                    in1 = Ep[:, :, pos:pos + 1].to_broadcast([128, 4, 64])
                    S.op('dve', lambda e: e.tensor_tensor(out=KRG[:, :, 2, cc * 64:(cc + 1) * 64], in0=in0, in1=in1, op=ALU.mult), reads=['i64f', 'Ep'], writes=['KRG'])

                for g0 in range(0, 8, NS):
                    grp = list(range(g0, g0 + NS))
                    cur = {}
                    for s_, h in enumerate(grp):
                        c, pb = h // 2, 64 * (h % 2)
                        fm = lambda arr, k0, k1: arr[pb:pb + 64, c, k0:k1, :].rearrange("p k t -> p (k t)")
                        p1, k1 = pp.get()
                        S.op('pe', lambda e: e.matmul(p1[:, 0:256], lhsT=fm(BK, 0, 1), rhs=fm(KRG, 0, 2), start=True, stop=True), reads=['BK', 'KRG'], writes=[k1], acc=True)
                        S.op('pe', lambda e: e.matmul(p1[:, 256:384], lhsT=fm(KB2, 1, 2), rhs=i64r[pb:pb + 64, :], start=True, stop=True), reads=['KB2', 'i64r'], writes=[k1], acc=True)
                        S.op('dve', lambda e: e.tensor_tensor(out=La[s_][:], in0=p1[:, 0:384], in1=m1, op=ALU.mult), reads=[k1, 'mk'], writes=['La%d' % s_])
                        p2, k2 = pp.get()
                        S.op('pe', lambda e: e.matmul(p2[:, 0:128], lhsT=fm(BK, 1, 2), rhs=fm(KRG, 1, 2), start=True, stop=True), reads=['BK', 'KRG'], writes=[k2], acc=True)
                        S.op('pe', lambda e: e.matmul(p2[:, 128:256], lhsT=fm(KB2, 0, 1), rhs=i64r[pb:pb + 64, :], start=True, stop=True), reads=['KB2', 'i64r'], writes=[k2], acc=True)
                        S.op('dve', lambda e: e.tensor_tensor(out=X2[s_][:], in0=p2[:, 0:256], in1=m2, op=ALU.mult), reads=[k2, 'mk'], writes=['X2%d' % s_])
                        p3, k3 = pp.get()
                        S.op('pe', lambda e: e.matmul(p3[:, 0:256], lhsT=fm(KRG, 0, 1), rhs=fm(BK, 0, 2), start=True, stop=True), reads=['BK', 'KRG'], writes=[k3], acc=True)
                        S.op('pe', lambda e: e.matmul(p3[:, 256:320], lhsT=fm(KRG, 0, 1), rhs=i64r[pb:pb + 64, 0:64], start=True, stop=True), reads=['KRG', 'i64r'], writes=[k3], acc=True)
                        S.op('dve', lambda e: e.tensor_tensor(out=X1[s_][:], in0=p3[:, 0:320], in1=m3, op=ALU.mult), reads=[k3, 'mk'], writes=['X1%d' % s_])
                        cur[s_] = (La[s_], 'La%d' % s_, X1[s_][:, 0:128], 'X1%d' % s_)
                        yield 'f'
                    for lev in range(5):
                        pend = []
                        nxt = {}
                        for s_ in range(NS):
                            L, Lk, AT, ATk = cur[s_]
                            p, pk = pp.get()
                            S.op('pe', lambda e: e.matmul(p[:, 0:384], lhsT=AT, rhs=L[:, 0:384], start=True, stop=False), reads=[Lk, ATk], writes=[pk], acc=True)
                            S.op('pe', lambda e: e.matmul(p[:, 128:384], lhsT=idr[:], rhs=L[:, 128:384], start=False, stop=True), reads=[Lk, 'idr'], writes=[pk], acc=True)
                            pend.append((p, pk))
                        yield 'f'
                        for s_ in range(NS):
                            p, pk = pend[s_]
                            Ln_ = Lb[s_] if lev % 2 == 0 else La[s_]
                            Lnk = ('Lb%d' if lev % 2 == 0 else 'La%d') % s_
                            evac(Ln_[:], p[:, 0:384], [pk], [Lnk])
                            nxt[s_] = (Ln_, Lnk)
                        for s_ in range(NS):
                            Ln_, Lnk = nxt[s_]
                            p, pk = pend[s_]
                            S.op('pe', lambda e: e.transpose(out=p[:, 384:512], in_=Ln_[:, 0:128].bitcast(F32), identity=idf[:]), reads=[Lnk, 'idf'], writes=[pk])
                        yield 'f'
                        for s_ in range(NS):
                            Ln_, Lnk = nxt[s_]
                            p, pk = pend[s_]
                            ATn = ATa[s_] if lev % 2 == 0 else ATb[s_]
                            ATnk = ('ATa%d' if lev % 2 == 0 else 'ATb%d') % s_
                            evac(ATn[:], p[:, 384:512], [pk], [ATnk])
                            cur[s_] = (Ln_, Lnk, ATn[:], ATnk)
                    for s_, h in enumerate(grp):
                        L, Lk, AT, ATk = cur[s_]
                        p, pk = pp.get()
                        S.op('pe', lambda e: e.matmul(p[:, 0:256], lhsT=AT, rhs=L[:, 128:384], start=True, stop=False), reads=[Lk, ATk], writes=[pk], acc=True)
                        S.op('pe', lambda e: e.matmul(p[:, 0:256], lhsT=idr[:], rhs=L[:, 128:384], start=False, stop=True), reads=[Lk, 'idr'], writes=[pk], acc=True)
                        evac(Xf[s_][:], p[:, 0:256], [pk], ['Xf%d' % s_])
                    yield 'f'
                    for s_, h in enumerate(grp):
                        c, pb = h // 2, 64 * (h % 2)
                        p, pk = pp.get()
                        S.op('pe', lambda e: e.matmul(p[:, 0:256], lhsT=idr[:], rhs=X2[s_][:], start=True, stop=False), reads=['X2%d' % s_, 'idr'], writes=[pk], acc=True)
                        S.op('pe', lambda e: e.matmul(p[:, 0:256], lhsT=X1[s_][:, 128:256], rhs=Xf[s_][:], start=False, stop=True), reads=['X1%d' % s_, 'Xf%d' % s_], writes=[pk], acc=True)
                        evac(QG2[qs][:, h, :], p[:, 0:256], [pk], ['QG2_%d_%d' % (qs, h)])
                        q, qk = pp.get()
                        rg = KRG[pb:pb + 64, c, 1:3, :].rearrange("p k t -> p (k t)")
                        S.op('pe', lambda e: e.matmul(q[0:64, 0:256], lhsT=idr[pb:pb + 64, pb:pb + 64], rhs=rg, start=True, stop=False), reads=['KRG', 'idr'], writes=[qk], acc=True)
                        S.op('pe', lambda e: e.matmul(q[0:64, 0:256], lhsT=X1[s_][:, 256:320], rhs=Xf[s_][:], start=False, stop=True), reads=['X1%d' % s_, 'Xf%d' % s_], writes=[qk], acc=True)
                        evac(QG1[qs][:, h, :], q[0:64, 0:256], [qk], ['QG1_%d_%d' % (qs, h)])
                        yield 'f'
                yield 'F'
                for s, cc in enumerate(chunks):
                    for h in range(8):
                        S.op('pe', lambda e: e.matmul(pst[:, h * 64:(h + 1) * 64], lhsT=QG1[qs][:, h, 128 + cc * 64:128 + (cc + 1) * 64], rhs=Sth[:, s, h, :], start=True, stop=False),
                             reads=['QG1_%d_%d' % (qs, h), 'Sth%d' % s], writes=['pst'], acc=True)
                        S.op('pe', lambda e: e.matmul(pst[:, h * 64:(h + 1) * 64], lhsT=QG2[qs][:, h, 128 + cc * 64:128 + (cc + 1) * 64], rhs=vtr[qs][:, h * 64:(h + 1) * 64], start=False, stop=True),
                             reads=['QG2_%d_%d' % (qs, h), 'vtr%d' % qs], writes=['pst'], acc=True)
                    evac(Sth[:, s + 1].rearrange("p h v -> p (h v)"), pst[:, :], ['pst'], ['Sth%d' % (s + 1)])
                    yield 'b'
                if lat:
                    ybuf = T[0]
                    for hq in range(2):
                        bank = pp.get()
                        for h4 in range(4):
                            h = hq * 4 + h4
                            S.op('pe', lambda e: e.matmul(bank[0][0:64, h4 * 128:(h4 + 1) * 128], lhsT=vtr[qs][:, h * 64:(h + 1) * 64], rhs=QG2[qs][:, h, 0:128], start=True, stop=False),
                                 reads=['QG2_%d_%d' % (qs, h), 'vtr%d' % qs], writes=[bank[1]], acc=True)
                            for s, cc in enumerate(chunks):
                                S.op('pe', lambda e: e.matmul(bank[0][0:64, h4 * 128 + cc * 64:h4 * 128 + (cc + 1) * 64], lhsT=Sth[:, s, h, :], rhs=QG1[qs][:, h, cc * 64:(cc + 1) * 64], start=False, stop=(s == 1)),
                                     reads=['QG1_%d_%d' % (qs, h), 'Sth%d' % s], writes=[bank[1]], acc=True)
                        evac(ybuf[:, hq * 4:hq * 4 + 4, :], bank[0][0:64, :].rearrange("p (h t) -> p h t", h=4), [bank[1]], ['T0'])
                        yield 'b'
                    tl = t0 - TC
                    if d == 0:
                        S.dma('pool', lambda e: e.dma_start(out=K.rwY0[:, tl:tl + G].rearrange("(h p) t -> p h t", p=64), in_=ybuf[:]), reads=['T0'], writes=['rwY0'])
                    else:
                        y0b, cen, sqb = T[1], T[2], T[3]
                        S.dma('sp', lambda e: e.dma_start(out=y0b[:], in_=K.rwY0[:, tl:tl + G].rearrange("(h p) t -> p h t", p=64)), reads=['rwY0'], writes=['T1'])
                        S.op('dve', lambda e: e.tensor_tensor(out=ybuf[:], in0=ybuf[:], in1=y0b[:], op=ALU.add), reads=['T0', 'T1'], writes=['T0'])
                        for q in range(2):
                            p, pk = pp.get()
                            S.op('pe', lambda e: e.matmul(p[0:64, :], lhsT=o64[:], rhs=fl(ybuf)[:, q * 512:(q + 1) * 512], start=True, stop=True), reads=['o64', 'T0'], writes=[pk])
                            S.op('dve', lambda e: e.tensor_tensor(out=fl(cen)[:, q * 512:(q + 1) * 512], in0=fl(ybuf)[:, q * 512:(q + 1) * 512], in1=p[0:64, :], op=ALU.subtract), reads=[pk, 'T0'], writes=['T2'])
                        S.op('act', lambda e: e.activation(out=sqb[:], in_=cen[:], func=AF.Square), reads=['T2'], writes=['T3'])
                        yield 'b'
                        rsb = T[1]
                        for q in range(2):
                            p, pk = pp.get()
                            S.op('pe', lambda e: e.matmul(p[0:64, :], lhsT=o64[:], rhs=fl(sqb)[:, q * 512:(q + 1) * 512], start=True, stop=True), reads=['o64', 'T3'], writes=[pk])
                            S.op('act', lambda e: e.activation(out=fl(rsb)[:, q * 512:(q + 1) * 512], in_=p[0:64, :], func=AF.Ln, bias=epsg[:], scale=1.0), reads=[pk, 'epsg'], writes=['T1'])
                        S.op('act', lambda e: e.activation(out=rsb[:], in_=rsb[:], func=AF.Exp, scale=-0.5), reads=['T1'], writes=['T1'])
                        yield 'b'
                        bonb, ggb = T[3], T[0]
                        S.dma('sp', lambda e: e.dma_start(out=bonb[:], in_=K.rwBON[:, tl:tl + G].rearrange("(h p) t -> p h t", p=64)), reads=['rwBON'], writes=['T3'])
                        S.op('dve', lambda e: e.tensor_tensor(out=cen[:], in0=cen[:], in1=rsb[:], op=ALU.mult), reads=['T2', 'T1'], writes=['T2'])
                        S.dma('sp', lambda e: e.dma_start(out=ggb[:], in_=K.rwG[:, tl:tl + G].rearrange("(h p) t -> p h t", p=64)), reads=['rwG'], writes=['T0'])
                        S.op('dve', lambda e: e.tensor_tensor(out=cen[:], in0=cen[:], in1=gng[:, :].unsqueeze(2).to_broadcast([64, 8, G]), op=ALU.mult), reads=['T2', 'gng'], writes=['T2'])
                        S.op('dve', lambda e: e.tensor_tensor(out=cen[:], in0=cen[:], in1=gnb[:, :].unsqueeze(2).to_broadcast([64, 8, G]), op=ALU.add), reads=['T2', 'gnb'], writes=['T2'])
                        S.op('dve', lambda e: e.tensor_tensor(out=cen[:], in0=cen[:], in1=bonb[:], op=ALU.add), reads=['T2', 'T3'], writes=['T2'])
                        S.op('dve', lambda e: e.tensor_tensor(out=outb[:], in0=cen[:], in1=ggb[:], op=ALU.mult), reads=['T2', 'T0'], writes=['outb'])
                        S.dma('pool', lambda e: e.dma_start(out=K.mixT[0:512, tl:tl + G].rearrange("(h p) t -> p h t", p=64), in_=outb[:]), reads=['outb'], writes=['mixT'])
                S.op('dve', lambda e: e.tensor_copy(out=Sth[:, 0], in_=Sth[:, 2]), reads=['Sth2'], writes=['Sth0'])
            def step(gen_):
                try:
                    return next(gen_)
                except StopIteration:
                    return None
            prev = None
            qslot = 0
            for b in blocks:
                gcur = body(b, qslot)
                while True:
                    r = step(gcur)
                    if prev is not None and step(prev) is None:
                        prev = None
                    if r == 'F' or r is None:
                        break
                while prev is not None:
                    if step(prev) is None:
                        prev = None
                prev = gcur
                qslot ^= 1
            while prev is not None:
                if step(prev) is None:
                    prev = None
            S.barrier()


ALPHA = 2.0 ** 0.25
ROWW = 1088
BIGPOS = 4096.0


def phase_mix(K):
    nc, S = K.nc, K.S
    NT = TL // 128
    with ExitStack() as es:
        sb = lambda n, s, d: es.enter_context(nc.sbuf_tensor(_u(n), s, d))
        ps = lambda n, s, d: es.enter_context(nc.psum_tensor(_u(n), s, d))
        wo = sb("wo", [128, 8, 1024], BF16)
        wst = [sb("owst%d" % i, [128, 8, 256], F32) for i in range(2)]
        bc = {n: sb("bc_" + n, [128, 1024], F32) for n in ('g1', 'ln1g', 'ln1b', 'sc2', 'sh2')}
        rt = sb("rt", [128, 8, 16], F32)
        idf = sb("idf", [128, 128], F32)
        epst = sb("epst", [128, 1], F32)
        onesf = sb("onesf", [128, 128], F32)
        onesb = sb("onesb", [128, 128], BF16)
        ustr = sb("ustr", [128, 128], BF16)
        mt = [sb("mt%d" % i, [128, 8, 128], BF16) for i in range(2)]
        xt = [sb("xt%d" % i, [128, 1024], F32) for i in range(2)]
        t1 = sb("t1", [128, 1024], F32)
        pre = sb("pre", [128, 1024], F32)
        x1 = [sb("x1_%d" % i, [128, 1024], F32) for i in range(2)]
        uf = sb("uf", [128, 1024], F32)
        urow = [sb("urow%d" % i, [128, ROWW], BF16) for i in range(2)]
        uT = sb("uT", [128, 8, 128], F32)
        st = sb("st", [128, 2, 6], F32)
        mv = sb("mv", [128, 2], F32)
        lnv = sb("lnv", [128, 1], F32)
        rstd = sb("rstd", [128, 1], F32)
        lg = sb("lg", [128, 16], F32)
        mx = sb("mx", [128, 1], F32)
        sm = sb("sm", [128, 1], F32)
        affall = sb("affall", [128, NT, 16], F32)
        tok = sb("tok", [128, 1], I32)
        lo = sb("lo", [128, 16], F32)
        mid = sb("mid", [128, 16], F32)
        ge = sb("ge", [128, 16], F32)
        cntp = sb("cntp", [128, 16], F32)
        mskt = sb("mskt", [128, NT, 16], F32)
        mskb = sb("mskb", [128, NT, 16], BF16)
        csT = sb("csT", [128, 16, NT], F32)
        incT = sb("incT", [128, 16, NT], F32)
        rmask = sb("rmask", [128, 16, NT], F32)
        posf = sb("posf", [128, NT, 16], F32)
        posi = sb("posi", [128, NT, 16], I32)
        zt = sb("zt", [128, 1024], F32)
        pm = [ps("pm%d" % i, [128, 1024], F32) for i in range(2)]
        ptr = ps("ptr", [128, 1024], F32)
        psm = ps("psm", [128, 512], F32)
        psn = ps("psn", [128, 512], F32)

        ld1 = lambda dst, src, key, rd=(): S.dma('sp', lambda e: e.dma_start(out=dst, in_=src, allow_slow_non_contiguous=True), reads=list(rd), writes=[key])
        ld1(idf[:], K.identf, 'idf')
        ld1(ustr[:], K.ustrict, 'ustr')
        ld1(rt[:], K.router.rearrange("(k p) e -> p k e", p=128), 'rt')
        ld1(bc['g1'][:], K.modd[0:1, 2048:3072].partition_broadcast(128), 'bc_g1', ['modd'])
        ld1(bc['sh2'][:], K.modd[0:1, 3072:4096].partition_broadcast(128), 'bc_sh2', ['modd'])
        ld1(bc['sc2'][:], K.modd[0:1, 4096:5120].partition_broadcast(128), 'bc_sc2', ['modd'])
        ld1(bc['ln1g'][:], K.ln1_g.partition_broadcast(128), 'bc_ln1g')
        ld1(bc['ln1b'][:], K.ln1_b.partition_broadcast(128), 'bc_ln1b')
        S.op('dve', lambda e: e.tensor_scalar_add(out=bc['sc2'][:], in0=bc['sc2'][:], scalar1=1.0), reads=['bc_sc2'], writes=['bc_sc2'])
        S.op('dve', lambda e: e.memset(epst[:], 1e-5), writes=['epst'])
        S.op('dve', lambda e: e.memset(onesf[:], 1.0), writes=['onesf'])
        S.op('dve', lambda e: e.memset(onesb[:], 1.0), writes=['onesb'])
        S.op('dve', lambda e: e.memset(zt[:], 0.0), writes=['zt'])
        for j in range(4):
            w = wst[j % 2]
            wk = 'owst%d' % (j % 2)
            S.dma('sp', lambda e: e.dma_start(out=w[:], in_=K.w_out[:, j * 256:(j + 1) * 256].rearrange("(k p) n -> p k n", p=128)), writes=[wk])
            S.op('pool', lambda e: e.tensor_copy(out=wo[:, :, j * 256:(j + 1) * 256], in_=w[:]), reads=[wk], writes=['wo'])
        for i in range(NT):
            S.dma('pool', lambda e: e.dma_start(out=K.yacc[i * 128:(i + 1) * 128, :], in_=zt[:]), reads=['zt'], writes=['yacc%d' % i])

        def ln_stats(src, srck):
            for c in range(2):
                S.op('dve', lambda e: e.bn_stats(out=st[:, c, :], in_=src[:, c * 512:(c + 1) * 512]), reads=[srck], writes=['st%d' % c])
            S.op('dve', lambda e: e.bn_aggr(out=mv[:], in_=st[:]), reads=['st0', 'st1'], writes=['mv'])
            S.op('act', lambda e: e.activation(out=lnv[:], in_=mv[:, 1:2], func=AF.Ln, bias=epst[:], scale=1.0), reads=['mv', 'epst'], writes=['lnv'])
            S.op('act', lambda e: e.activation(out=rstd[:], in_=lnv[:], func=AF.Exp, scale=-0.5), reads=['lnv'], writes=['rstd'])

        for i in range(NT):
            sl = i % 2
            m_, mk_ = mt[sl], 'mt%d' % sl
            x_, xk = xt[sl], 'xt%d' % sl
            S.dma('sp', lambda e: e.dma_start(out=m_[:], in_=K.mixT[:, i * 128:(i + 1) * 128].rearrange("(k p) t -> p k t", p=128)), reads=['mixT'], writes=[mk_])
            S.dma('sp', lambda e: e.dma_start(out=x_[:], in_=K.xin[TC + i * 128:TC + (i + 1) * 128, :]), writes=[xk])
            p_, pk_ = pm[sl], 'pm%d' % sl
            for half in range(2):
                for kc in range(8):
                    S.op('pe', lambda e: e.matmul(p_[:, half * 512:(half + 1) * 512], lhsT=m_[:, kc, :], rhs=wo[:, kc, half * 512:(half + 1) * 512], start=(kc == 0), stop=(kc == 7)),
                         reads=[mk_, 'wo'], writes=[pk_], acc=True)
            S.op('dve', lambda e: e.tensor_tensor(out=t1[:], in0=p_[:], in1=bc['g1'][:], op=ALU.mult), reads=[pk_, 'bc_g1'], writes=['t1'])
            S.op('dve', lambda e: e.scalar_tensor_tensor(out=pre[:], in0=x_[:], scalar=ALPHA, in1=t1[:], op0=ALU.mult, op1=ALU.add), reads=[xk, 't1'], writes=['pre'])
            ln_stats(pre, 'pre')
            x1_, x1k = x1[sl], 'x1_%d' % sl
            S.op('dve', lambda e: e.tensor_scalar(out=t1[:], in0=pre[:], scalar1=mv[:, 0:1], scalar2=rstd[:], op0=ALU.subtract, op1=ALU.mult), reads=['pre', 'mv', 'rstd'], writes=['t1'])
            S.op('dve', lambda e: e.tensor_tensor(out=t1[:], in0=t1[:], in1=bc['ln1g'][:], op=ALU.mult), reads=['t1', 'bc_ln1g'], writes=['t1'])
            S.op('dve', lambda e: e.tensor_tensor(out=x1_[:], in0=t1[:], in1=bc['ln1b'][:], op=ALU.add), reads=['t1', 'bc_ln1b'], writes=[x1k])
            S.dma('pool', lambda e: e.dma_start(out=K.x1d[i * 128:(i + 1) * 128, :], in_=x1_[:]), reads=[x1k], writes=['x1d'])
            ln_stats(x1_, x1k)
            S.op('dve', lambda e: e.tensor_scalar(out=pre[:], in0=x1_[:], scalar1=mv[:, 0:1], scalar2=rstd[:], op0=ALU.subtract, op1=ALU.mult), reads=[x1k, 'mv', 'rstd'], writes=['pre'])
            S.op('dve', lambda e: e.tensor_tensor(out=pre[:], in0=pre[:], in1=bc['sc2'][:], op=ALU.mult), reads=['pre', 'bc_sc2'], writes=['pre'])
            S.op('dve', lambda e: e.tensor_tensor(out=uf[:], in0=pre[:], in1=bc['sh2'][:], op=ALU.add), reads=['pre', 'bc_sh2'], writes=['uf'])
            ur, urk = urow[sl], 'urow%d' % sl
            S.op('act', lambda e: e.activation(out=ur[:, 0:1024], in_=uf[:], func=AF.Identity), reads=['uf'], writes=[urk])
            for k in range(8):
                S.op('pe', lambda e: e.transpose(out=ptr[:, k * 128:(k + 1) * 128], in_=uf[:, k * 128:(k + 1) * 128], identity=idf[:]), reads=['uf', 'idf'], writes=['ptr'], acc=True)
            S.op('act', lambda e: e.activation(out=uT[:].rearrange("p k t -> p (k t)"), in_=ptr[:], func=AF.Identity), reads=['ptr'], writes=['uT'])
            for k in range(8):
                S.op('pe', lambda e: e.matmul(psm[:, 0:16], lhsT=uT[:, k, :], rhs=rt[:, k, :], start=(k == 0), stop=(k == 7)), reads=['uT', 'rt'], writes=['psm'], acc=True)
            S.op('dve', lambda e: e.reduce_max(out=mx[:], in_=psm[:, 0:16], axis=AX.X), reads=['psm'], writes=['mx'])
            S.op('dve', lambda e: e.tensor_scalar_mul(out=mx[:], in0=mx[:], scalar1=-1.0), reads=['mx'], writes=['mx'])
            S.op('act', lambda e: e.activation(out=lg[:], in_=psm[:, 0:16], func=AF.Exp, bias=mx[:], scale=1.0, accum_out=sm[:]), reads=['psm', 'mx'], writes=['lg', 'sm'])
            S.op('dve', lambda e: e.reciprocal(out=sm[:], in_=sm[:]), reads=['sm'], writes=['sm'])
            S.op('dve', lambda e: e.tensor_scalar_mul(out=affall[:, i, :], in0=lg[:], scalar1=sm[:]), reads=['lg', 'sm'], writes=['affall'])
            S.op('dve', lambda e: e.tensor_copy(out=ur[:, 1024:1056].bitcast(F32), in_=affall[:, i, :]), reads=['affall'], writes=[urk])
            S.op('pool', lambda e: e.iota(tok[:], pattern=[[0, 1]], base=i * 128, channel_multiplier=1), writes=['tok'])
            S.op('pool', lambda e: e.tensor_copy(out=ur[:, 1056:1058].bitcast(I32), in_=tok[:]), reads=['tok'], writes=[urk])
            S.dma('pool', lambda e: e.dma_start(out=K.urd[i * 128:(i + 1) * 128, 0:1058], in_=ur[:, 0:1058]), reads=[urk], writes=['urd%d' % i])

        S.op('dve', lambda e: e.memset(lo[:], 0.0), writes=['lo'])
        for k in range(30):
            hk = 2.0 ** -(k + 1)
            S.op('dve', lambda e: e.tensor_scalar_add(out=mid[:], in0=lo[:], scalar1=hk), reads=['lo'], writes=['mid'])
            S.op('dve', lambda e: e.tensor_tensor(out=mskt[:], in0=affall[:], in1=mid[:, :].unsqueeze(1).to_broadcast([128, NT, 16]), op=ALU.is_ge), reads=['affall', 'mid'], writes=['mskt'])
            S.op('dve', lambda e: e.tensor_reduce(out=cntp[:], in_=mskt[:].rearrange("p i e -> p e i"), axis=AX.X, op=ALU.add), reads=['mskt'], writes=['cntp'])
            S.op('pe', lambda e: e.matmul(psn[:, 0:16], lhsT=onesf[:], rhs=cntp[:], start=True, stop=True), reads=['onesf', 'cntp'], writes=['psn'])
            S.op('dve', lambda e: e.tensor_scalar(out=ge[:], in0=psn[:, 0:16], scalar1=float(CAP) - 0.5, scalar2=hk, op0=ALU.is_ge, op1=ALU.mult), reads=['psn'], writes=['ge'])
            S.op('dve', lambda e: e.tensor_tensor(out=lo[:], in0=lo[:], in1=ge[:], op=ALU.add), reads=['lo', 'ge'], writes=['lo'])
        S.op('dve', lambda e: e.tensor_tensor(out=mskt[:], in0=affall[:], in1=lo[:, :].unsqueeze(1).to_broadcast([128, NT, 16]), op=ALU.is_ge), reads=['affall', 'lo'], writes=['mskt'])
        S.op('act', lambda e: e.activation(out=mskb[:], in_=mskt[:], func=AF.Identity), reads=['mskt'], writes=['mskb'])
        mflat = mskb[:].rearrange("p i e -> p (i e)")
        for hh in range(2):
            S.op('pe', lambda e: e.matmul(psm[:, :], lhsT=onesb[:], rhs=mflat[:, hh * 512:(hh + 1) * 512], start=True, stop=True), reads=['onesb', 'mskb'], writes=['psm'])
            S.op('dve', lambda e: e.tensor_copy(out=csT[:, :, hh * 32:(hh + 1) * 32], in_=psm[:, :].rearrange("p (i e) -> p e i", e=16)), reads=['psm'], writes=['csT'])
        S.op('dve', lambda e: e.memset(rmask[:], 1.0), writes=['rmask'])
        S.op('dve', lambda e: e.memset(rmask[:, :, 0:1], 0.0), writes=['rmask'])
        S.op('dve', lambda e: e.tensor_tensor_scan(out=incT[:].rearrange("p e i -> p (e i)"), data0=rmask[:].rearrange("p e i -> p (e i)"), data1=csT[:].rearrange("p e i -> p (e i)"),
                                                   initial=0.0, op0=ALU.mult, op1=ALU.add), reads=['rmask', 'csT'], writes=['incT'])
        S.op('dve', lambda e: e.tensor_tensor(out=incT[:], in0=incT[:], in1=csT[:], op=ALU.subtract), reads=['incT', 'csT'], writes=['incT'])
        for hh in range(2):
            S.op('pe', lambda e: e.matmul(psm[:, :], lhsT=ustr[:], rhs=mflat[:, hh * 512:(hh + 1) * 512], start=True, stop=True), reads=['ustr', 'mskb'], writes=['psm'])
            S.op('dve', lambda e: e.tensor_tensor(out=posf[:, hh * 32:(hh + 1) * 32, :], in0=psm[:, :].rearrange("p (i e) -> p i e", e=16),
                                                  in1=incT[:, :, hh * 32:(hh + 1) * 32].rearrange("p e i -> p i e"), op=ALU.add), reads=['psm', 'incT'], writes=['posf'])
        S.op('dve', lambda e: e.scalar_tensor_tensor(out=posf[:], in0=posf[:], scalar=-BIGPOS, in1=mskt[:], op0=ALU.add, op1=ALU.mult), reads=['posf', 'mskt'], writes=['posf'])
        S.op('dve', lambda e: e.tensor_scalar_add(out=posf[:], in0=posf[:], scalar1=BIGPOS), reads=['posf'], writes=['posf'])
        S.op('dve', lambda e: e.tensor_copy(out=posi[:], in_=posf[:]), reads=['posf'], writes=['posi'])
        if K.dbg_pos is not None:
            S.dma('sp', lambda e: e.dma_start(out=K.dbg_pos, in_=posf[:]), reads=['posf'], writes=['dbg_pos'])
        breg = nc.gpsimd.to_reg(CAP - 1)
        for i in range(NT):
            sl = i % 2
            ur, urk = urow[sl], 'urow%d' % sl
            S.dma('sp', lambda e: e.dma_start(out=ur[:, 0:1058], in_=K.urd[i * 128:(i + 1) * 128, 0:1058]), reads=['urd%d' % i], writes=[urk])
            for ex in range(NE):
                S.dma('pool', lambda e: e.indirect_dma_start(out=K.xe_d[ex], out_offset=bass.IndirectOffsetOnAxis(ap=posi[:, i, ex:ex + 1], axis=0),
                                                             in_=ur[:, :], in_offset=None, bounds_check=breg, oob_is_err=False),
                      reads=[urk, 'posi'], writes=['xe_d_%d_%d' % (i, ex)])
        S.barrier()


def phase_moe(K, experts=range(NE)):
    nc, S = K.nc, K.S
    NT = TL // 128
    with ExitStack() as es:
        sb = lambda n, s, d: es.enter_context(nc.sbuf_tensor(_u(n), s, d))
        ps = lambda n, s, d: es.enter_context(nc.psum_tensor(_u(n), s, d))
        W = [[sb("W%d_%d" % (m, i), [128, 8, 1024], BF16) for i in range(2)] for m in range(3)]
        wst = Rot([sb("ewst%d" % i, [128, 8, 256], F32) for i in range(3)], "ewst")
        idb = sb("idb", [128, 128], BF16)
        xrow = Rot([sb("xrow%d" % i, [128, ROWW], BF16) for i in range(2)], "xrow")
        xeT = sb("xeT", [128, 8, 1024], BF16)
        hidT = sb("hidT", [128, 8, 1024], BF16)
        gates = sb("gates", [128, 8], F32)
        idxs = sb("idxs", [128, 8], I32)
        sgt = Rot([sb("sgt%d" % i, [128, 512], F32) for i in range(2)], "sgt")
        ye = Rot([sb("ye%d" % i, [128, 1024], F32) for i in range(2)], "ye")
        pt = Rot([ps("ept%d" % i, [128, 1024], BF16) for i in range(2)], "ept")
        pg = Rot([ps("epg%d" % i, [128, 512], F32) for i in range(6)], "epg")
        S.dma('sp', lambda e: e.dma_start(out=idb[:], in_=K.ident), writes=['idb'])
        cast_i = [0]

        def load_w(ex, slot):
            for m, src in enumerate((K.exp_w_gate, K.exp_w_up, K.exp_w_down)):
                for j in range(4):
                    w, wk = wst.get()
                    S.dma('sp', lambda e: e.dma_start(out=w[:], in_=src[ex, :, j * 256:(j + 1) * 256].rearrange("(k p) n -> p k n", p=128)), writes=[wk])
                    eng = 'dve'
                    cast_i[0] += 1
                    if eng == 'dve':
                        S.op('dve', lambda e: e.tensor_copy(out=W[m][slot][:, :, j * 256:(j + 1) * 256], in_=w[:]), reads=[wk], writes=['W%d_%d' % (m, slot)])
                    else:
                        S.op('act', lambda e: e.activation(out=W[m][slot][:, :, j * 256:(j + 1) * 256], in_=w[:], func=AF.Identity), reads=[wk], writes=['W%d_%d' % (m, slot)])

        exl = list(experts)
        load_w(exl[0], 0)
        for n, ex in enumerate(exl):
            slot = n % 2
            Wg, Wu, Wd = W[0][slot], W[1][slot], W[2][slot]
            wkeys = ['W%d_%d' % (m, slot) for m in range(3)]
            for j in range(8):
                xr, xk = xrow.get()
                S.dma('sp', lambda e: e.dma_start(out=xr[:, 0:1058], in_=K.xe_d[ex][j * 128:(j + 1) * 128, 0:1058]), reads=['xe_d'], writes=[xk])
                S.op('dve', lambda e: e.tensor_copy(out=gates[:, j:j + 1], in_=xr[:, 1024:1056].bitcast(F32)[:, ex:ex + 1]), reads=[xk], writes=['gates'])
                S.op('dve', lambda e: e.tensor_copy(out=idxs[:, j:j + 1], in_=xr[:, 1056:1058].bitcast(I32)), reads=[xk], writes=['idxs'])
                p, pk = pt.get()
                for k in range(8):
                    S.op('pe', lambda e: e.transpose(out=p[:, k * 128:(k + 1) * 128], in_=xr[:, k * 128:(k + 1) * 128], identity=idb[:]), reads=[xk, 'idb'], writes=[pk], acc=True)
                S.op('act', lambda e: e.activation(out=xeT[:, :, j * 128:(j + 1) * 128], in_=p[:].rearrange("p (k t) -> p k t", k=8), func=AF.Identity), reads=[pk], writes=['xeT'])
            if n + 1 < len(exl):
                load_w(exl[n + 1], 1 - slot)
            for fc in range(8):
                for half in range(2):
                    g_, gk = pg.get()
                    u_, uk = pg.get()
                    for kc in range(8):
                        S.op('pe', lambda e: e.matmul(g_[:], lhsT=Wg[:, kc, fc * 128:(fc + 1) * 128], rhs=xeT[:, kc, half * 512:(half + 1) * 512], start=(kc == 0), stop=(kc == 7)),
                             reads=[wkeys[0], 'xeT'], writes=[gk], acc=True)
                    for kc in range(8):
                        S.op('pe', lambda e: e.matmul(u_[:], lhsT=Wu[:, kc, fc * 128:(fc + 1) * 128], rhs=xeT[:, kc, half * 512:(half + 1) * 512], start=(kc == 0), stop=(kc == 7)),
                             reads=[wkeys[1], 'xeT'], writes=[uk], acc=True)
                    s_, sk = sgt.get()
                    S.op('act', lambda e: e.activation(out=s_[:], in_=g_[:], func=AF.Silu), reads=[gk], writes=[sk])
                    S.op('dve', lambda e: e.tensor_tensor(out=hidT[:, fc, half * 512:(half + 1) * 512], in0=s_[:], in1=u_[:], op=ALU.mult), reads=[sk, uk], writes=['hidT'])
            for j in range(8):
                y_, yk = ye.get()
                for dh in range(2):
                    o_, ok = pg.get()
                    for fc in range(8):
                        S.op('pe', lambda e: e.matmul(o_[:], lhsT=hidT[:, fc, j * 128:(j + 1) * 128], rhs=Wd[:, fc, dh * 512:(dh + 1) * 512], start=(fc == 0), stop=(fc == 7)),
                             reads=[wkeys[2], 'hidT'], writes=[ok], acc=True)
                    S.op('act', lambda e: e.activation(out=y_[:, dh * 512:(dh + 1) * 512], in_=o_[:], func=AF.Identity, scale=gates[:, j:j + 1]), reads=[ok, 'gates'], writes=[yk])
                S.dma('pool', lambda e: e.indirect_dma_start(out=K.yacc, out_offset=bass.IndirectOffsetOnAxis(ap=idxs[:, j:j + 1], axis=0), in_=y_[:, :], in_offset=None,
                                                             compute_op=ALU.add),
                      reads=[yk, 'idxs'], writes=['yacc'])
        S.barrier()


def phase_final(K):
    nc, S = K.nc, K.S
    NT = TL // 128
    with ExitStack() as es:
        sb = lambda n, s, d: es.enter_context(nc.sbuf_tensor(_u(n), s, d))
        bc = {n: sb("fbc_" + n, [128, 1024], F32) for n in ('g2', 'ln2g', 'ln2b')}
        epst = sb("epst", [128, 1], F32)
        x1 = [sb("fx1_%d" % i, [128, 1024], F32) for i in range(2)]
        ya = [sb("fya_%d" % i, [128, 1024], F32) for i in range(2)]
        t1 = sb("t1", [128, 1024], F32)
        pre = sb("pre", [128, 1024], F32)
        ob = [sb("fob_%d" % i, [128, 1024], F32) for i in range(2)]
        st = sb("st", [128, 2, 6], F32)
        mv = sb("mv", [128, 2], F32)
        lnv = sb("lnv", [128, 1], F32)
        rstd = sb("rstd", [128, 1], F32)
        ld1 = lambda dst, src, key, rd=(): S.dma('sp', lambda e: e.dma_start(out=dst, in_=src, allow_slow_non_contiguous=True), reads=list(rd), writes=[key])
        ld1(bc['g2'][:], K.modd[0:1, 5120:6144].partition_broadcast(128), 'fbc_g2', ['modd'])
        ld1(bc['ln2g'][:], K.ln2_g.partition_broadcast(128), 'fbc_ln2g')
        ld1(bc['ln2b'][:], K.ln2_b.partition_broadcast(128), 'fbc_ln2b')
        S.op('dve', lambda e: e.memset(epst[:], 1e-5), writes=['epst'])
        for i in range(NT):
            sl = i % 2
            S.dma('sp', lambda e: e.dma_start(out=x1[sl][:], in_=K.x1d[i * 128:(i + 1) * 128, :]), reads=['x1d'], writes=['fx1_%d' % sl])
            S.dma('sp', lambda e: e.dma_start(out=ya[sl][:], in_=K.yacc[i * 128:(i + 1) * 128, :]), reads=['yacc'], writes=['fya_%d' % sl])
            S.op('dve', lambda e: e.tensor_tensor(out=t1[:], in0=ya[sl][:], in1=bc['g2'][:], op=ALU.mult), reads=['fya_%d' % sl, 'fbc_g2'], writes=['t1'])
            S.op('dve', lambda e: e.scalar_tensor_tensor(out=pre[:], in0=x1[sl][:], scalar=ALPHA, in1=t1[:], op0=ALU.mult, op1=ALU.add), reads=['fx1_%d' % sl, 't1'], writes=['pre'])
            for c in range(2):
                S.op('dve', lambda e: e.bn_stats(out=st[:, c, :], in_=pre[:, c * 512:(c + 1) * 512]), reads=['pre'], writes=['st%d' % c])
            S.op('dve', lambda e: e.bn_aggr(out=mv[:], in_=st[:]), reads=['st0', 'st1'], writes=['mv'])
            S.op('act', lambda e: e.activation(out=lnv[:], in_=mv[:, 1:2], func=AF.Ln, bias=epst[:], scale=1.0), reads=['mv', 'epst'], writes=['lnv'])
            S.op('act', lambda e: e.activation(out=rstd[:], in_=lnv[:], func=AF.Exp, scale=-0.5), reads=['lnv'], writes=['rstd'])
            S.op('dve', lambda e: e.tensor_scalar(out=t1[:], in0=pre[:], scalar1=mv[:, 0:1], scalar2=rstd[:], op0=ALU.subtract, op1=ALU.mult), reads=['pre', 'mv', 'rstd'], writes=['t1'])
            S.op('dve', lambda e: e.tensor_tensor(out=t1[:], in0=t1[:], in1=bc['ln2g'][:], op=ALU.mult), reads=['t1', 'fbc_ln2g'], writes=['t1'])
            S.op('dve', lambda e: e.tensor_tensor(out=ob[sl][:], in0=t1[:], in1=bc['ln2b'][:], op=ALU.add), reads=['t1', 'fbc_ln2b'], writes=['fob_%d' % sl])
            S.dma('pool', lambda e: e.dma_start(out=K.out[i * 128:(i + 1) * 128, :], in_=ob[sl][:]), reads=['fob_%d' % sl], writes=['out'])
        S.barrier()


def build_program(debug=(), phases=None, dbg_in=(), opts=None):
    opts = opts or {}
    nc = bass.Bass("TRN2", target_bir_lowering=False)
    K = Ctx()
    K.nc = nc
    di = lambda n, s, d: nc.dram_tensor(n, s, d, kind="ExternalInput").ap()
    K.xin = di("xin", [TA, D], F32)
    K.ccT = di("ccT", [128, 8, 2], F32)
    K.w_ada = di("w_ada", [D, 6 * D], F32)
    K.b_ada = di("b_ada", [1, 6 * D], F32)
    K.w_in = di("w_in", [D, 2560], F32)
    K.cosT = di("cosT", [64, TL], F32)
    K.sinT = di("sinT", [64, TL], F32)
    K.ident = di("ident", [128, 128], BF16)
    K.identf = di("identf", [128, 128], F32)
    K.i64dbl = di("i64dbl", [128, 128], F32)
    K.bones = di("bones", [128, 128], F32)
    K.rwm = [di("rwm%d" % d, [128, 1344], F32) for d in range(2)]
    K.mla_q_norm = di("mla_q_norm", [1, 256], F32)
    K.mla_kv_norm = di("mla_kv_norm", [1, 256], F32)
    K.w_uq = di("w_uq", [256, 1024], F32)
    K.w_uk = di("w_uk", [256, 512], F32)
    K.w_uv = di("w_uv", [256, 512], F32)
    K.rwkv_conv = di("rwkv_conv", [3, 1536], F32)
    K.rwkv_w0 = di("rwkv_w0", [2, 512], F32)
    K.rwkv_w_up = di("rwkv_w_up", [2, 64, 512], F32)
    K.rwkv_a0 = di("rwkv_a0", [2, 512], F32)
    K.rwkv_a_up = di("rwkv_a_up", [2, 64, 512], F32)
    K.rwkv_g_up = di("rwkv_g_up", [128, 512], F32)
    K.rwkv_k_k = di("rwkv_k_k", [1, 512], F32)
    K.rwkv_k_a = di("rwkv_k_a", [1, 512], F32)
    K.rwkv_r_k = di("rwkv_r_k", [1, 512], F32)
    K.rwkv_gn_g = di("rwkv_gn_g", [1, 512], F32)
    K.rwkv_gn_b = di("rwkv_gn_b", [1, 512], F32)
    K.w_out = di("w_out", [D, D], F32)
    K.ln1_g = di("ln1_g", [1, D], F32)
    K.ln1_b = di("ln1_b", [1, D], F32)
    K.ln2_g = di("ln2_g", [1, D], F32)
    K.ln2_b = di("ln2_b", [1, D], F32)
    K.router = di("router", [D, NE], F32)
    K.ustrict = di("ustrict", [128, 128], BF16)
    K.exp_w_gate = di("exp_w_gate", [NE, D, D], F32)
    K.exp_w_up = di("exp_w_up", [NE, D, D], F32)
    K.exp_w_down = di("exp_w_down", [NE, D, D], F32)

    def scratch(n, s, d):
        if n in dbg_in:
            return nc.dram_tensor(n, s, d, kind="ExternalInput").ap()
        kind = "ExternalOutput" if n in debug else "Internal"
        return nc.dram_tensor(n, s, d, kind=kind).ap()
    K.modd = scratch("modd", [2, 6 * D], F32)
    K.hT = scratch("hT", [2432, TA], F32)
    K.krT = scratch("krT", [64, TA], BF16)
    K.qnT = scratch("qnT", [512, TL], BF16)
    K.qrT = scratch("qrT", [256, TL], BF16)
    K.knT = scratch("knT", [512, TA], BF16)
    K.vtok = scratch("vtok", [TA, 512], BF16)
    K.mixT = scratch("mixT", [1024, TL], BF16)
    K.rwR = scratch("rwR", [512, TA], F32)
    K.rwV = scratch("rwV", [512, TA], F32)
    K.rwKK = scratch("rwKK", [512, TA], F32)
    K.rwKD = [scratch("rwKD%d" % d, [512, TA], F32) for d in range(2)]
    K.rwB = [scratch("rwB%d" % d, [512, TA], F32) for d in range(2)]
    K.rwVtok = scratch("rwVtok", [TA, 512], F32)
    K.rwLD = [scratch("rwLD%d" % d, [TA, 512], F32) for d in range(2)]
    K.rwG = scratch("rwG", [512, TL], F32)
    K.rwBON = scratch("rwBON", [512, TL], F32)
    K.rwY0 = scratch("rwY0", [512, TL], F32)
    K.x1d = scratch("x1d", [TL, D], F32)
    K.urd = scratch("urd", [TL, ROWW], BF16)
    K.xe_d = [scratch("xe_d%d" % e_, [CAP, ROWW], BF16) for e_ in range(NE)]
    K.yacc = scratch("yacc", [TL, D], F32)
    K.dbg_pos = scratch("dbg_pos", [128, TL // 128, 16], F32) if 'dbg_pos' in debug else None
    K.out = nc.dram_tensor("out", [TL, D], F32, kind="ExternalOutput").ap()
    allp = ['mod', 'inproj', 'mlaprep', 'attn', 'rwprep', 'rwscan', 'mix', 'moe', 'final']
    if phases is None:
        phases = allp
    with ExitStack() as es:
        S = Sync(nc, es)
        K.S = S
        if 'mod' in phases:
            phase_mod(K)
        if 'inproj' in phases:
            phase_inproj(K)
        if 'mlaprep' in phases:
            phase_mlaprep(K)
        if 'attn' in phases:
            phase_attn(K)
        if 'attn1' in phases:
            phase_attn(K, heads=(1,), nqt=2)
        if 'rwprep' in phases:
            phase_rwprep(K)
        if 'rwscan' in phases:
            phase_rwscan(K, **opts.get('rwscan', {}))
        if 'mix' in phases:
            phase_mix(K)
        if 'moe' in phases:
            phase_moe(K, **opts.get('moe', {}))
        if 'final' in phases:
            phase_final(K)
        S.wait_all('sp')
        print("instructions", S.n_inst, "waits", S.n_wait, "sems", S.nsem + NDMA)
    return nc


_SWAP = np.concatenate([np.arange(16, 32), np.arange(0, 16), np.arange(48, 64), np.arange(32, 48)])


def rope_tables():
    half = 32
    inv_freq = (10000.0 ** (-np.arange(0, half, 2, dtype=np.float32) / half)).astype(np.float32)
    t = np.arange(TL)
    rr = (t // 64).astype(np.float32)[None, :]
    cc = (t % 64).astype(np.float32)[None, :]
    ang_r = (inv_freq[:, None] * rr).astype(np.float32)
    ang_c = (inv_freq[:, None] * cc).astype(np.float32)
    cosT = np.concatenate([np.cos(ang_r), np.cos(ang_r), np.cos(ang_c), np.cos(ang_c)], 0).astype(np.float32)
    sinT = np.concatenate([-np.sin(ang_r), np.sin(ang_r), -np.sin(ang_c), np.sin(ang_c)], 0).astype(np.float32)
    return np.ascontiguousarray(cosT), np.ascontiguousarray(sinT)


def make_in_maps(inputs, batches):
    f = lambda a: np.ascontiguousarray(np.asarray(a, dtype=np.float32))
    w_in = f(inputs['w_in'][0])
    w_in_ext = np.concatenate([w_in, w_in[:, 2432:2496][:, _SWAP]], axis=1)
    cosT, sinT = rope_tables()
    wuq = f(inputs['mla_w_uq'][0])
    cols = []
    for h in range(4):
        nope = wuq[:, h * 192:h * 192 + 128]
        rope = wuq[:, h * 192 + 128:h * 192 + 192]
        cols += [nope, rope, rope[:, _SWAP]]
    wuq_ext = np.ascontiguousarray(np.concatenate(cols, axis=1))
    shared = {
        'w_ada': f(inputs['w_ada'][0]), 'b_ada': f(inputs['b_ada']), 'w_in': np.ascontiguousarray(w_in_ext),
        'cosT': cosT, 'sinT': sinT, 'ident': np.eye(128).astype(ml_dtypes.bfloat16),
        'mla_q_norm': f(inputs['mla_q_norm']), 'mla_kv_norm': f(inputs['mla_kv_norm']),
        'w_uq': wuq_ext, 'w_uk': f(inputs['mla_w_uk'][0]), 'w_uv': f(inputs['mla_w_uv'][0]),
        'rwkv_conv': f(inputs['rwkv_conv'][0]), 'rwkv_w0': f(inputs['rwkv_w0'][0]), 'rwkv_w_up': f(inputs['rwkv_w_up'][0]),
        'rwkv_a0': f(inputs['rwkv_a0'][0]), 'rwkv_a_up': f(inputs['rwkv_a_up'][0]), 'rwkv_g_up': f(inputs['rwkv_g_up'][0]),
        'rwkv_k_k': f(inputs['rwkv_k_k']), 'rwkv_k_a': f(inputs['rwkv_k_a']), 'rwkv_r_k': f(inputs['rwkv_r_k']).reshape(1, 512),
        'rwkv_gn_g': f(inputs['rwkv_gn_g']), 'rwkv_gn_b': f(inputs['rwkv_gn_b']),
        'w_out': f(inputs['w_out'][0]), 'ln1_g': f(inputs['ln1_g']), 'ln1_b': f(inputs['ln1_b']),
        'ln2_g': f(inputs['ln2_g']), 'ln2_b': f(inputs['ln2_b']), 'router': f(inputs['router'][0]),
        'ustrict': (np.arange(128)[:, None] < np.arange(128)[None, :]).astype(ml_dtypes.bfloat16),
        'exp_w_gate': f(inputs['exp_w_gate'][0]), 'exp_w_up': f(inputs['exp_w_up'][0]), 'exp_w_down': f(inputs['exp_w_down'][0]),
    }
    shared.update(rw_consts())
    maps = []
    for b in batches:
        m = dict(shared)
        m['xin'] = np.ascontiguousarray(np.concatenate([inputs['ctx'][b], inputs['x'][b]], axis=0).astype(np.float32))
        cc = np.stack([inputs['c'][b], inputs['c_ctx']], axis=-1).astype(np.float32)
        m['ccT'] = np.ascontiguousarray(cc.reshape(8, 128, 2).transpose(1, 0, 2))
        maps.append(m)
    return maps


_CONST_KEYS = ('cosT', 'sinT', 'ident', 'identf', 'i64dbl', 'bones', 'rwm0', 'rwm1', 'ustrict')
BATCH_CORES = (0, 1, 4, 5)


def kernel(**inputs):
    nc = build_program()
    real = make_in_maps(inputs, [0, 1, 2, 3])
    zero = {k: (v if k in _CONST_KEYS else np.zeros_like(v)) for k, v in real[0].items()}
    maps = [zero] * 8
    for b, c in enumerate(BATCH_CORES):
        maps[c] = real[b]
    res = run_bass_kernel_spmd(nc, maps, core_ids=list(range(8)))
    out = np.stack([res.results[c]['out'] for c in BATCH_CORES], axis=0)
    return out.astype(np.float32)
```

```python
import numpy as np
import ml_dtypes
from contextlib import ExitStack
import concourse.bass as bass
import concourse.mybir as mybir
from concourse.bass_utils import run_bass_kernel_spmd

F32 = mybir.dt.float32
BF16 = mybir.dt.bfloat16
I32 = mybir.dt.int32
AF = mybir.ActivationFunctionType
ALU = mybir.AluOpType
AX = mybir.AxisListType

EPOCH = 12000
NDMA = 24

D = 1024
TL = 8192
TC = 256
TA = TL + TC
NE = 16
CAP = 1024


class Sync:
    def __init__(self, nc, es):
        self.nc = nc
        self.es = es
        self.eng = {'pe': nc.tensor, 'dve': nc.vector, 'act': nc.scalar,
                    'pool': nc.gpsimd, 'sp': nc.sync}
        self.sem = {}
        self.cnt = {}
        self.cur = {}
        self.known = {e: {} for e in self.eng}
        self.snap = {}
        self.last_w = {}
        self.readers = {}
        self.dma_keys = []
        self.dma_rr = 0
        self.nsem = 0
        for e in self.eng:
            self._new_epoch(e)
        for i in range(NDMA):
            k = ('dma', i)
            self.sem[k] = es.enter_context(nc.semaphore('dq%d' % i))
            self.cnt[k] = 0
            self.dma_keys.append(k)
        self.n_inst = 0
        self.n_wait = 0

    def _new_epoch(self, e):
        idx = self.nsem
        self.nsem += 1
        k = (e, idx)
        self.sem[k] = self.es.enter_context(self.nc.semaphore('s_%s_%d' % (e, idx)))
        self.cnt[k] = 0
        self.cur[e] = k

    def _need(self, e, ticket):
        k, v = ticket
        kn = self.known[e]
        if kn.get(k, 0) >= v:
            return
        self.eng[e].wait_ge(self.sem[k], v)
        self.n_wait += 1
        kn[k] = v
        sn = self.snap.get(ticket)
        if sn:
            for kk, vv in sn.items():
                if kn.get(kk, 0) < vv:
                    kn[kk] = vv

    def _deps(self, e, reads, writes, acc):
        for b in reads:
            t = self.last_w.get(b)
            if t is not None:
                self._need(e, t)
        for b in writes:
            t = self.last_w.get(b)
            if t is not None and not (acc and t[0] == self.cur[e]):
                self._need(e, t)
            for t in self.readers.get(b, ()):
                self._need(e, t)

    def _record(self, ticket, reads, writes):
        for b in reads:
            self.readers.setdefault(b, []).append(ticket)
        for b in writes:
            self.last_w[b] = ticket
            self.readers[b] = []

    def op(self, e, fn, reads=(), writes=(), acc=False):
        if self.cnt[self.cur[e]] >= EPOCH:
            self._new_epoch(e)
        self._deps(e, reads, writes, acc)
        k = self.cur[e]
        inst = fn(self.eng[e])
        inst.then_inc(self.sem[k], 1)
        self.cnt[k] += 1
        t = (k, self.cnt[k])
        self.snap[t] = dict(self.known[e])
        self._record(t, reads, writes)
        self.n_inst += 1
        return t

    def dma(self, e, fn, reads=(), writes=()):
        k = self.dma_keys[self.dma_rr]
        self.dma_rr = (self.dma_rr + 1) % NDMA
        if self.cnt[k] > 0:
            self._need(e, (k, self.cnt[k]))
        self._deps(e, reads, writes, False)
        inst = fn(self.eng[e])
        inst.then_inc(self.sem[k], 16)
        self.cnt[k] += 16
        t = (k, self.cnt[k])
        self.snap[t] = dict(self.known[e])
        self._record(t, reads, writes)
        self.n_inst += 1
        return t

    def wait_all(self, e):
        for k, v in list(self.cnt.items()):
            if v > 0:
                self._need(e, (k, v))

    def barrier(self):
        for e in self.eng:
            self.wait_all(e)
        self.last_w.clear()
        self.readers.clear()


class Ctx:
    pass


_UC = [0]


def _u(n):
    _UC[0] += 1
    return '%s_%d' % (n, _UC[0])


def phase_mod(K):
    nc, S = K.nc, K.S
    with ExitStack() as es:
        sb = lambda n, s, d: es.enter_context(nc.sbuf_tensor(_u(n), s, d))
        ccs = sb("ccs", [128, 8, 2], F32)
        scT = sb("scT", [128, 8, 2], F32)
        wa = [sb("wa%d" % i, [128, 8, 512], F32) for i in range(2)]
        ba = sb("ba", [1, 6144], F32)
        one1 = sb("one1", [1, 1], F32)
        mrow = [sb("mrow%d" % r, [1, 6144], F32) for r in range(2)]
        pm = [es.enter_context(nc.psum_tensor(_u("pm%d" % i), [1, 512], F32)) for i in range(2)]
        S.dma('sp', lambda e: e.dma_start(out=ccs[:], in_=K.ccT), writes=['ccs'])
        S.dma('sp', lambda e: e.dma_start(out=ba[:], in_=K.b_ada), writes=['ba'])
        S.op('dve', lambda e: e.memset(one1[:], 1.0), writes=['one1'])
        S.op('act', lambda e: e.activation(out=scT[:], in_=ccs[:], func=AF.Silu), reads=['ccs'], writes=['scT'])
        for j in range(12):
            w = wa[j % 2]
            wk = 'wa%d' % (j % 2)
            S.dma('sp', lambda e: e.dma_start(out=w[:], in_=K.w_ada[:, j * 512:(j + 1) * 512].rearrange("(k p) n -> p k n", p=128)), writes=[wk])
            for r in range(2):
                if r == 1 and j >= 4:
                    continue
                pk = 'pm%d' % r
                for k in range(8):
                    S.op('pe', lambda e: e.matmul(pm[r][:], lhsT=scT[:, k, r:r + 1], rhs=w[:, k, :], start=(k == 0), stop=False),
                         reads=['scT', wk], writes=[pk], acc=True)
                S.op('pe', lambda e: e.matmul(pm[r][:], lhsT=one1[:], rhs=ba[:, j * 512:(j + 1) * 512], start=False, stop=True),
                     reads=['one1', 'ba'], writes=[pk], acc=True)
                S.op('dve', lambda e: e.tensor_copy(out=mrow[r][:, j * 512:(j + 1) * 512], in_=pm[r][:]), reads=[pk], writes=['mrow%d' % r])
        S.dma('sp', lambda e: e.dma_start(out=K.modd[0:1, :], in_=mrow[0][:]), reads=['mrow0'], writes=['modd'])
        S.dma('sp', lambda e: e.dma_start(out=K.modd[1:2, 0:2048], in_=mrow[1][:, 0:2048]), reads=['mrow1'], writes=['modd'])
        S.barrier()


def phase_inproj(K):
    nc, S = K.nc, K.S
    with ExitStack() as es:
        sb = lambda n, s, d: es.enter_context(nc.sbuf_tensor(_u(n), s, d))
        ps = lambda n, s, d: es.enter_context(nc.psum_tensor(_u(n), s, d))
        wb = sb("wb", [128, 8, 2560], BF16)
        wst = [sb("wst%d" % i, [128, 8, 320], F32) for i in range(2)]
        idt = sb("idt", [128, 128], BF16)
        epst = sb("epst", [128, 1], F32)
        scp = [sb("scp%d" % r, [128, 8], F32) for r in range(2)]
        shp = [sb("shp%d" % r, [128, 8], F32) for r in range(2)]
        xt = [sb("xt%d" % i, [128, 1024], F32) for i in range(2)]
        xn = [sb("xn%d" % i, [128, 1024], BF16) for i in range(2)]
        st = sb("st", [128, 2, 6], F32)
        mv = sb("mv", [128, 2], F32)
        lnv = sb("lnv", [128, 1], F32)
        rstd = sb("rstd", [128, 1], F32)
        xmT = [sb("xmT%d" % i, [128, 8, 512], BF16) for i in range(2)]
        stg = [sb("stg%d" % i, [128, 4, 512], F32) for i in range(2)]
        cst = sb("cst", [64, 512], F32)
        snt = sb("snt", [64, 512], F32)
        kr1 = sb("kr1", [64, 512], F32)
        kr2 = sb("kr2", [64, 512], F32)
        krb = sb("krb", [64, 512], BF16)
        pt = [ps("pt%d" % i, [128, 1024], BF16) for i in range(2)]
        po = [ps("po%d" % i, [128, 512], F32) for i in range(3)]
        pk = [ps("pk%d" % i, [64, 512], F32) for i in range(2)]

        S.dma('sp', lambda e: e.dma_start(out=idt[:], in_=K.ident), writes=['idt'])
        S.op('dve', lambda e: e.memset(epst[:], 1e-5), writes=['epst'])
        for r in range(2):
            S.dma('sp', lambda e: e.dma_start(out=shp[r][:], in_=K.modd[r:r + 1, 0:1024].rearrange("o (k p) -> p (o k)", p=128), allow_slow_non_contiguous=True), reads=['modd'], writes=['shp%d' % r])
            S.dma('sp', lambda e: e.dma_start(out=scp[r][:], in_=K.modd[r:r + 1, 1024:2048].rearrange("o (k p) -> p (o k)", p=128), allow_slow_non_contiguous=True), reads=['modd'], writes=['scp%d' % r])
            S.op('dve', lambda e: e.tensor_scalar_add(out=scp[r][:], in0=scp[r][:], scalar1=1.0), reads=['scp%d' % r], writes=['scp%d' % r])
        for j in range(8):
            w = wst[j % 2]
            wk = 'wst%d' % (j % 2)
            S.dma('sp', lambda e: e.dma_start(out=w[:], in_=K.w_in[:, j * 320:(j + 1) * 320].rearrange("(k p) n -> p k n", p=128)), writes=[wk])
            S.op('pool', lambda e: e.tensor_copy(out=wb[:, :, j * 320:(j + 1) * 320], in_=w[:]), reads=[wk], writes=['wb'])

        groups = [(0, 256, 1)] + [(256 + g * 512, 512, 0) for g in range(16)]
        tile_i = 0
        ev = 0
        for gi, (t0, G, r) in enumerate(groups):
            xm = xmT[gi % 2]
            xmk = 'xmT%d' % (gi % 2)
            if r == 0:
                S.dma('sp', lambda e: e.dma_start(out=cst[:], in_=K.cosT[:, t0 - 256:t0 - 256 + 512]), writes=['cst'])
                S.dma('sp', lambda e: e.dma_start(out=snt[:], in_=K.sinT[:, t0 - 256:t0 - 256 + 512]), writes=['snt'])
            for i in range(G // 128):
                sl = tile_i % 2
                tile_i += 1
                xk, xnk, ptk = 'xt%d' % sl, 'xn%d' % sl, 'pt%d' % sl
                tt = t0 + i * 128
                S.dma('sp', lambda e: e.dma_start(out=xt[sl][:], in_=K.xin[tt:tt + 128, :]), writes=[xk])
                for c in range(2):
                    S.op('dve', lambda e: e.bn_stats(out=st[:, c, :], in_=xt[sl][:, c * 512:(c + 1) * 512]), reads=[xk], writes=['st%d' % c])
                S.op('dve', lambda e: e.bn_aggr(out=mv[:], in_=st[:]), reads=['st0', 'st1'], writes=['mv'])
                S.op('act', lambda e: e.activation(out=lnv[:], in_=mv[:, 1:2], func=AF.Ln, bias=epst[:], scale=1.0), reads=['mv', 'epst'], writes=['lnv'])
                S.op('act', lambda e: e.activation(out=rstd[:], in_=lnv[:], func=AF.Exp, scale=-0.5), reads=['lnv'], writes=['rstd'])
                S.op('dve', lambda e: e.tensor_scalar(out=xn[sl][:], in0=xt[sl][:], scalar1=mv[:, 0:1], scalar2=rstd[:], op0=ALU.subtract, op1=ALU.mult),
                     reads=[xk, 'mv', 'rstd'], writes=[xnk])
                for k in range(8):
                    S.op('pe', lambda e: e.transpose(out=pt[sl][:, k * 128:(k + 1) * 128], in_=xn[sl][:, k * 128:(k + 1) * 128], identity=idt[:]),
                         reads=[xnk, 'idt'], writes=[ptk], acc=True)
                for k in range(8):
                    S.op('act', lambda e: e.activation(out=xm[:, k, i * 128:(i + 1) * 128], in_=pt[sl][:, k * 128:(k + 1) * 128], func=AF.Identity,
                                                       bias=shp[r][:, k:k + 1], scale=scp[r][:, k:k + 1]),
                         reads=[ptk, 'shp%d' % r, 'scp%d' % r], writes=[xmk])
            for cb in range(5):
                sg = stg[cb % 2]
                sgk = 'stg%d' % (cb % 2)
                ncols = 4 if cb < 4 else 3
                for cc in range(ncols):
                    ci = cb * 4 + cc
                    p = po[ev % 3]
                    pkk = 'po%d' % (ev % 3)
                    for k in range(8):
                        S.op('pe', lambda e: e.matmul(p[:, 0:G], lhsT=wb[:, k, ci * 128:(ci + 1) * 128], rhs=xm[:, k, 0:G], start=(k == 0), stop=(k == 7)),
                             reads=['wb', xmk], writes=[pkk], acc=True)
                    if ev % 2 == 0:
                        S.op('dve', lambda e: e.tensor_copy(out=sg[:, cc, 0:G], in_=p[:, 0:G]), reads=[pkk], writes=[sgk])
                    else:
                        S.op('act', lambda e: e.activation(out=sg[:, cc, 0:G], in_=p[:, 0:G], func=AF.Identity), reads=[pkk], writes=[sgk])
                    ev += 1
                r0 = cb * 512
                S.dma('pool', lambda e: e.dma_start(out=K.hT[r0:r0 + ncols * 128, t0:t0 + G].rearrange("(c p) t -> p c t", p=128), in_=sg[:, 0:ncols, 0:G]),
                      reads=[sgk], writes=['hT'])
            for q in range(2):
                for k in range(8):
                    S.op('pe', lambda e: e.matmul(pk[q][:, 0:G], lhsT=wb[:, k, 2432 + q * 64:2432 + (q + 1) * 64], rhs=xm[:, k, 0:G], start=(k == 0), stop=(k == 7)),
                         reads=['wb', xmk], writes=['pk%d' % q], acc=True)
            if r == 0:
                S.op('dve', lambda e: e.tensor_tensor(out=kr1[:], in0=pk[0][:], in1=cst[:], op=ALU.mult), reads=['pk0', 'cst'], writes=['kr1'])
                S.op('dve', lambda e: e.tensor_tensor(out=kr2[:], in0=pk[1][:], in1=snt[:], op=ALU.mult), reads=['pk1', 'snt'], writes=['kr2'])
                S.op('dve', lambda e: e.tensor_tensor(out=krb[:], in0=kr1[:], in1=kr2[:], op=ALU.add), reads=['kr1', 'kr2'], writes=['krb'])
            else:
                S.op('dve', lambda e: e.tensor_copy(out=krb[:, 0:G], in_=pk[0][:, 0:G]), reads=['pk0', 'pk1'], writes=['krb'])
            S.dma('pool', lambda e: e.dma_start(out=K.krT[:, t0:t0 + G], in_=krb[:, 0:G]), reads=['krb'], writes=['krT'])
        S.barrier()


class Rot:
    def __init__(self, bufs, prefix):
        self.bufs = bufs
        self.prefix = prefix
        self.i = 0

    def get(self):
        j = self.i % len(self.bufs)
        self.i += 1
        return self.bufs[j], '%s%d' % (self.prefix, j)


SCALE_ATT = 192.0 ** -0.5


def phase_mlaprep(K):
    nc, S = K.nc, K.S
    with ExitStack() as es:
        sb = lambda n, s, d: es.enter_context(nc.sbuf_tensor(_u(n), s, d))
        ps = lambda n, s, d: es.enter_context(nc.psum_tensor(_u(n), s, d))
        wuq = sb("wuq", [128, 2, 1024], BF16)
        wuk = sb("wuk", [128, 2, 512], BF16)
        wuv = sb("wuv", [128, 2, 512], BF16)
        wst = [sb("mwst%d" % i, [128, 2, 512], F32) for i in range(2)]
        gq = sb("gq", [128, 2], F32)
        gkv = sb("gkv", [128, 2], F32)
        onesf = sb("onesf", [128, 128], F32)
        epst = sb("epst", [128, 1], F32)
        ql = [sb("ql%d" % i, [128, 4, 512], F32) for i in range(2)]
        sq = sb("sq", [128, 4, 512], F32)
        lnt = sb("lnt", [128, 512], F32)
        rs = [sb("rs%d" % i, [128, 512], F32) for i in range(2)]
        nb = [sb("nb%d" % i, [128, 4, 512], BF16) for i in range(2)]
        qst = [sb("qst%d" % i, [128, 4, 512], BF16) for i in range(2)]
        kst = [sb("kst%d" % i, [128, 4, 512], BF16) for i in range(2)]
        qrb = [sb("qrb%d" % i, [64, 4, 512], BF16) for i in range(2)]
        vst = Rot([sb("vst%d" % i, [128, 512], BF16) for i in range(3)], "vst")
        cst = sb("cst", [64, 512], F32)
        snt = sb("snt", [64, 512], F32)
        r1 = sb("r1", [64, 512], F32)
        r2 = sb("r2", [64, 512], F32)
        pp = Rot([ps("mp%d" % i, [128, 512], F32) for i in range(7)], "mp")

        S.op('dve', lambda e: e.memset(epst[:], 1e-6), writes=['epst'])
        S.op('dve', lambda e: e.memset(onesf[:], 1.0), writes=['onesf'])
        S.dma('sp', lambda e: e.dma_start(out=gq[:], in_=K.mla_q_norm.rearrange("o (c p) -> p (o c)", p=128), allow_slow_non_contiguous=True), writes=['gq'])
        S.dma('sp', lambda e: e.dma_start(out=gkv[:], in_=K.mla_kv_norm.rearrange("o (c p) -> p (o c)", p=128), allow_slow_non_contiguous=True), writes=['gkv'])
        wl = 0
        for (src, dst, dk, n) in [(K.w_uq, wuq, 'wuq', 1024), (K.w_uk, wuk, 'wuk', 512), (K.w_uv, wuv, 'wuv', 512)]:
            for j in range(n // 512):
                w = wst[wl % 2]
                wk = 'mwst%d' % (wl % 2)
                wl += 1
                S.dma('sp', lambda e: e.dma_start(out=w[:], in_=src[:, j * 512:(j + 1) * 512].rearrange("(c p) n -> p c n", p=128)), writes=[wk])
                S.op('pool', lambda e: e.tensor_copy(out=dst[:, :, j * 512:(j + 1) * 512], in_=w[:]), reads=[wk], writes=[dk])

        groups = [(0, 256, 1)] + [(256 + g * 512, 512, 0) for g in range(16)]
        ev = [0]

        def evac(out_ap, in_ap, reads, writes):
            if ev[0] % 2 == 0:
                S.op('dve', lambda e: e.tensor_copy(out=out_ap, in_=in_ap), reads=reads, writes=writes)
            else:
                S.op('act', lambda e: e.activation(out=out_ap, in_=in_ap, func=AF.Identity), reads=reads, writes=writes)
            ev[0] += 1

        for gi, (t0, G, r) in enumerate(groups):
            q_ = ql[gi % 2]
            qk = 'ql%d' % (gi % 2)
            n_ = nb[gi % 2]
            nk = 'nb%d' % (gi % 2)
            S.dma('sp', lambda e: e.dma_start(out=q_[:, :, 0:G], in_=K.hT[1920:2432, t0:t0 + G].rearrange("(c p) t -> p c t", p=128)), reads=['hT'], writes=[qk])
            if r == 0:
                S.dma('sp', lambda e: e.dma_start(out=cst[:], in_=K.cosT[:, t0 - 256:t0 - 256 + 512]), writes=['cst'])
                S.dma('sp', lambda e: e.dma_start(out=snt[:], in_=K.sinT[:, t0 - 256:t0 - 256 + 512]), writes=['snt'])
            S.op('act', lambda e: e.activation(out=sq[:, :, 0:G], in_=q_[:, :, 0:G], func=AF.Square), reads=[qk], writes=['sq'])
            for pair in range(2):
                if pair == 0 and r == 1:
                    continue
                p, pk = pp.get()
                for c in range(2):
                    S.op('pe', lambda e: e.matmul(p[:, 0:G], lhsT=onesf[:], rhs=sq[:, pair * 2 + c, 0:G], start=(c == 0), stop=(c == 1)),
                         reads=['onesf', 'sq'], writes=[pk], acc=True)
                S.op('act', lambda e: e.activation(out=lnt[:, 0:G], in_=p[:, 0:G], func=AF.Ln, bias=epst[:], scale=1.0 / 256.0), reads=[pk, 'epst'], writes=['lnt'])
                S.op('act', lambda e: e.activation(out=rs[pair][:, 0:G], in_=lnt[:, 0:G], func=AF.Exp, scale=-0.5), reads=['lnt'], writes=['rs%d' % pair])
                g_ = gq if pair == 0 else gkv
                for c in range(2):
                    S.op('dve', lambda e: e.scalar_tensor_tensor(out=n_[:, pair * 2 + c, 0:G], in0=q_[:, pair * 2 + c, 0:G], scalar=g_[:, c:c + 1], in1=rs[pair][:, 0:G],
                                                                 op0=ALU.mult, op1=ALU.mult),
                         reads=[qk, 'rs%d' % pair, 'gq', 'gkv'], writes=[nk])
            if r == 0:
                tq = t0 - 256
                qs = qst[gi % 2]
                qsk = 'qst%d' % (gi % 2)
                qr_ = qrb[gi % 2]
                qrk = 'qrb%d' % (gi % 2)
                for h in range(4):
                    p, pk = pp.get()
                    for c in range(2):
                        S.op('pe', lambda e: e.matmul(p[:, 0:G], lhsT=wuq[:, c, h * 256:h * 256 + 128], rhs=n_[:, c, 0:G], start=(c == 0), stop=(c == 1)),
                             reads=['wuq', nk], writes=[pk], acc=True)
                    evac(qs[:, h, 0:G], p[:, 0:G], [pk], [qsk])
                    p1, pk1 = pp.get()
                    p2, pk2 = pp.get()
                    for c in range(2):
                        S.op('pe', lambda e: e.matmul(p1[0:64, 0:G], lhsT=wuq[:, c, h * 256 + 128:h * 256 + 192], rhs=n_[:, c, 0:G], start=(c == 0), stop=(c == 1)),
                             reads=['wuq', nk], writes=[pk1], acc=True)
                    for c in range(2):
                        S.op('pe', lambda e: e.matmul(p2[0:64, 0:G], lhsT=wuq[:, c, h * 256 + 192:h * 256 + 256], rhs=n_[:, c, 0:G], start=(c == 0), stop=(c == 1)),
                             reads=['wuq', nk], writes=[pk2], acc=True)
                    S.op('dve', lambda e: e.tensor_tensor(out=r1[:], in0=p1[0:64, :], in1=cst[:], op=ALU.mult), reads=[pk1, 'cst'], writes=['r1'])
                    S.op('dve', lambda e: e.tensor_tensor(out=r2[:], in0=p2[0:64, :], in1=snt[:], op=ALU.mult), reads=[pk2, 'snt'], writes=['r2'])
                    S.op('dve', lambda e: e.tensor_tensor(out=qr_[:, h, :], in0=r1[:], in1=r2[:], op=ALU.add), reads=['r1', 'r2'], writes=[qrk])
                S.dma('pool', lambda e: e.dma_start(out=K.qnT[:, tq:tq + G].rearrange("(h p) t -> p h t", p=128), in_=qs[:, :, 0:G]), reads=[qsk], writes=['qnT'])
                S.dma('pool', lambda e: e.dma_start(out=K.qrT[:, tq:tq + G].rearrange("(h p) t -> p h t", p=64), in_=qr_[:, :, 0:G]), reads=[qrk], writes=['qrT'])
            ks = kst[gi % 2]
            ksk = 'kst%d' % (gi % 2)
            for h in range(4):
                p, pk = pp.get()
                for c in range(2):
                    S.op('pe', lambda e: e.matmul(p[:, 0:G], lhsT=wuk[:, c, h * 128:(h + 1) * 128], rhs=n_[:, 2 + c, 0:G], start=(c == 0), stop=(c == 1)),
                         reads=['wuk', nk], writes=[pk], acc=True)
                evac(ks[:, h, 0:G], p[:, 0:G], [pk], [ksk])
            S.dma('pool', lambda e: e.dma_start(out=K.knT[:, t0:t0 + G].rearrange("(h p) t -> p h t", p=128), in_=ks[:, :, 0:G]), reads=[ksk], writes=['knT'])
            for i in range(G // 128):
                p, pk = pp.get()
                for c in range(2):
                    S.op('pe', lambda e: e.matmul(p[:, :], lhsT=n_[:, 2 + c, i * 128:(i + 1) * 128], rhs=wuv[:, c, :], start=(c == 0), stop=(c == 1)),
                         reads=['wuv', nk], writes=[pk], acc=True)
                v_, vk = vst.get()
                evac(v_[:], p[:], [pk], [vk])
                S.dma('pool', lambda e: e.dma_start(out=K.vtok[t0 + i * 128:t0 + (i + 1) * 128, :], in_=v_[:]), reads=[vk], writes=['vtok'])
        S.barrier()


def phase_attn(K, heads=(0, 1, 2, 3), nqt=16):
    nc, S = K.nc, K.S
    NKT = TA // 128
    with ExitStack() as es:
        sb = lambda n, s, d: es.enter_context(nc.sbuf_tensor(_u(n), s, d))
        ps = lambda n, s, d: es.enter_context(nc.psum_tensor(_u(n), s, d))
        krs = sb("krs", [64, TA], BF16)
        kn = [sb("kn%d" % i, [128, TA], BF16) for i in range(2)]
        vh = [sb("vh%d" % i, [128, NKT, 128], BF16) for i in range(2)]
        qn = [sb("qn%d" % i, [128, 512], BF16) for i in range(2)]
        qr = [sb("qr%d" % i, [64, 512], BF16) for i in range(2)]
        onesb = sb("onesb", [128, 128], BF16)
        pT = Rot([sb("pT%d" % i, [128, 512], BF16) for i in range(3)], "pT")
        rl = sb("rl", [128, 512], F32)
        ob = [sb("ob%d" % i, [128, 512], BF16) for i in range(2)]
        psc = Rot([ps("psc%d" % i, [128, 512], F32) for i in range(3)], "psc")
        pO = [ps("pO%d" % i, [128, 512], F32) for i in range(2)]
        pL = [ps("pL%d" % i, [128, 512], F32) for i in range(2)]

        S.op('dve', lambda e: e.memset(onesb[:], 1.0), writes=['onesb'])
        S.dma('sp', lambda e: e.dma_start(out=krs[:], in_=K.krT), reads=['krT'], writes=['krs'])
        qi = 0
        for hi, h in enumerate(heads):
            k_ = kn[hi % 2]
            kk = 'kn%d' % (hi % 2)
            v_ = vh[hi % 2]
            vk = 'vh%d' % (hi % 2)
            S.dma('sp', lambda e: e.dma_start(out=k_[:], in_=K.knT[h * 128:(h + 1) * 128, :]), reads=['knT'], writes=[kk])
            S.dma('sp', lambda e: e.dma_start(out=v_[:], in_=K.vtok[:, h * 128:(h + 1) * 128].rearrange("(kt p) d -> p kt d", p=128)), reads=['vtok'], writes=[vk])
            for qt in range(nqt):
                sl = qi % 2
                qi += 1
                qnk, qrk = 'qn%d' % sl, 'qr%d' % sl
                S.dma('sp', lambda e: e.dma_start(out=qn[sl][:], in_=K.qnT[h * 128:(h + 1) * 128, qt * 512:(qt + 1) * 512]), reads=['qnT'], writes=[qnk])
                S.dma('sp', lambda e: e.dma_start(out=qr[sl][:], in_=K.qrT[h * 64:(h + 1) * 64, qt * 512:(qt + 1) * 512]), reads=['qrT'], writes=[qrk])
                Ok, Lk = 'pO%d' % sl, 'pL%d' % sl
                pend = []

                def scores(kt):
                    p, pk = psc.get()
                    S.op('pe', lambda e: e.matmul(p[:], lhsT=k_[:, kt * 128:(kt + 1) * 128], rhs=qn[sl][:], start=True, stop=False),
                         reads=[kk, qnk], writes=[pk], acc=True)
                    S.op('pe', lambda e: e.matmul(p[:], lhsT=krs[:, kt * 128:(kt + 1) * 128], rhs=qr[sl][:], start=False, stop=True),
                         reads=['krs', qrk], writes=[pk], acc=True)
                    t_, tk = pT.get()
                    S.op('act', lambda e: e.activation(out=t_[:], in_=p[:], func=AF.Exp, scale=SCALE_ATT), reads=[pk], writes=[tk])
                    pend.append((kt, t_, tk))

                def pv():
                    kt, t_, tk = pend.pop(0)
                    S.op('pe', lambda e: e.matmul(pO[sl][:], lhsT=v_[:, kt, :], rhs=t_[:], start=(kt == 0), stop=(kt == NKT - 1)),
                         reads=[vk, tk], writes=[Ok], acc=True)
                    S.op('pe', lambda e: e.matmul(pL[sl][:], lhsT=onesb[:], rhs=t_[:], start=(kt == 0), stop=(kt == NKT - 1)),
                         reads=['onesb', tk], writes=[Lk], acc=True)

                scores(0)
                scores(1)
                for kt in range(NKT):
                    pv()
                    if kt + 2 < NKT:
                        scores(kt + 2)
                S.op('dve', lambda e: e.reciprocal(out=rl[:], in_=pL[sl][:]), reads=[Lk], writes=['rl'])
                S.op('dve', lambda e: e.tensor_tensor(out=ob[sl][:], in0=pO[sl][:], in1=rl[:], op=ALU.mult), reads=[Ok, 'rl'], writes=['ob%d' % sl])
                S.dma('pool', lambda e: e.dma_start(out=K.mixT[512 + h * 128:512 + (h + 1) * 128, qt * 512:(qt + 1) * 512], in_=ob[sl][:]), reads=['ob%d' % sl], writes=['mixT'])
        S.barrier()


F32R = mybir.dt.float32r
LDS = -0.6065306597126334


def phase_rwprep(K):
    nc, S = K.nc, K.S
    with ExitStack() as es:
        sb = lambda n, s, d: es.enter_context(nc.sbuf_tensor(_u(n), s, d))
        ps = lambda n, s, d: es.enter_context(nc.psum_tensor(_u(n), s, d))
        G = 256
        cw = sb("cw", [128, 3, 12], F32)
        kkv = sb("kkv", [128, 4], F32)
        kav = sb("kav", [128, 4], F32)
        omka = sb("omka", [128, 4], F32)
        rkv = sb("rkv", [128, 4], F32)
        a0v = sb("a0v", [128, 2, 4], F32)
        w0r = sb("w0r", [1, 2, 512], F32)
        ones1 = sb("ones1", [1, 128], F32)
        wup = sb("wup", [64, 2, 512], F32)
        aup = sb("aup", [64, 2, 512], F32)
        gup = sb("gup", [128, 512], F32)
        bones = sb("bones", [128, 128], F32)
        idf = sb("idf", [128, 128], F32)
        eps12 = sb("eps12", [128, 1], F32)
        hr = [sb("hr%d" % i, [128, 12, G + 2], F32) for i in range(2)]
        lo = [sb("lo%d" % i, [64, 4, G], F32) for i in range(2)]
        gd = [sb("gd%d" % i, [128, G], F32) for i in range(2)]
        cv = sb("cv", [128, 12, G], F32)
        tw = sb("tw", [64, 2, G], F32)
        sg = sb("sg", [128, G], F32)
        kq = sb("kq", [128, 4, G], F32)
        sq = sb("sq", [128, 4, G], F32)
        lnt = sb("lnt", [128, 4, G], F32)
        kk_ = sb("kk_", [128, 4, G], F32)
        av = sb("av", [128, 4, G], F32)
        tt = sb("tt", [128, 4, G], F32)
        kd = [sb("kd%d" % i, [128, 4, G], F32) for i in range(2)]
        bb = sb("bb", [128, 4, G], F32)
        ld = Rot([sb("ld%d" % i, [128, 512], F32) for i in range(2)], "ld")
        vt = Rot([sb("vt%d" % i, [128, 512], F32) for i in range(2)], "vt")
        gg = sb("gg", [128, 4, G], F32)
        bc = sb("bc", [128, 4, G], F32)
        bon = sb("bon", [128, 4, G], F32)
        pp = Rot([ps("rp%d" % i, [128, 512], F32) for i in range(7)], "rp")

        ld1 = lambda dst, src, key: S.dma('sp', lambda e: e.dma_start(out=dst, in_=src, allow_slow_non_contiguous=True), writes=[key])
        ld1(cw[:], K.rwkv_conv.rearrange("t (c p) -> p t c", p=128), 'cw')
        ld1(kkv[:], K.rwkv_k_k.rearrange("o (c p) -> p (o c)", p=128), 'kkv')
        ld1(kav[:], K.rwkv_k_a.rearrange("o (c p) -> p (o c)", p=128), 'kav')
        ld1(rkv[:], K.rwkv_r_k.rearrange("o (c p) -> p (o c)", p=128), 'rkv')
        ld1(a0v[:], K.rwkv_a0.rearrange("d (c p) -> p d c", p=128), 'a0v')
        ld1(w0r[:], K.rwkv_w0.rearrange("(o d) n -> o d n", o=1), 'w0r')
        ld1(wup[:], K.rwkv_w_up.rearrange("d l n -> l d n"), 'wup')
        ld1(aup[:], K.rwkv_a_up.rearrange("d l n -> l d n"), 'aup')
        ld1(gup[:], K.rwkv_g_up, 'gup')
        ld1(bones[:], K.bones, 'bones')
        ld1(idf[:], K.identf, 'idf')
        S.op('dve', lambda e: e.memset(ones1[:], 1.0), writes=['ones1'])
        S.op('dve', lambda e: e.memset(eps12[:], 1e-12), writes=['eps12'])
        S.op('dve', lambda e: e.tensor_scalar(out=omka[:], in0=kav[:], scalar1=-1.0, scalar2=1.0, op0=ALU.mult, op1=ALU.add), reads=['kav'], writes=['omka'])

        nblk = TA // G
        for bi in range(nblk):
            t0 = bi * G
            lat = t0 >= TC
            sl = bi % 2
            h_ = hr[sl]
            hk = 'hr%d' % sl
            first = (t0 == 0 or t0 == TC)
            last = (t0 + G == TC or t0 + G == TA)
            c0 = 1 if first else 0
            c1 = G + 1 if last else G + 2
            if first:
                S.op('pool', lambda e: e.memset(h_[:, :, 0:1], 0.0), writes=[hk])
            if last:
                S.op('pool', lambda e: e.memset(h_[:, :, G + 1:G + 2], 0.0), writes=[hk])
            for q in range(3):
                S.dma('sp', lambda e: e.dma_start(out=h_[:, q * 4:(q + 1) * 4, c0:c1], in_=K.hT[q * 512:(q + 1) * 512, t0 - 1 + c0:t0 - 1 + c1].rearrange("(c p) t -> p c t", p=128)),
                      reads=['hT'], writes=[hk])
            lo_ = lo[sl]
            lk = 'lo%d' % sl
            S.dma('sp', lambda e: e.dma_start(out=lo_[:], in_=K.hT[1536:1792, t0:t0 + G].rearrange("(c p) t -> p c t", p=64)), reads=['hT'], writes=[lk])
            gd_ = gd[sl]
            gk = 'gd%d' % sl
            if lat:
                S.dma('sp', lambda e: e.dma_start(out=gd_[:], in_=K.hT[1792:1920, t0:t0 + G]), reads=['hT'], writes=[gk])
            for c in range(12):
                S.op('act', lambda e: e.activation(out=cv[:, c, :], in_=h_[:, c, 1:G + 1], func=AF.Identity, scale=cw[:, 1, c:c + 1]), reads=[hk, 'cw'], writes=['cv%d' % c])
                S.op('dve', lambda e: e.scalar_tensor_tensor(out=cv[:, c, :], in0=h_[:, c, 0:G], scalar=cw[:, 0, c:c + 1], in1=cv[:, c, :], op0=ALU.mult, op1=ALU.add),
                     reads=[hk, 'cw', 'cv%d' % c], writes=['cv%d' % c])
                S.op('dve', lambda e: e.scalar_tensor_tensor(out=cv[:, c, :], in0=h_[:, c, 2:G + 2], scalar=cw[:, 2, c:c + 1], in1=cv[:, c, :], op0=ALU.mult, op1=ALU.add),
                     reads=[hk, 'cw', 'cv%d' % c], writes=['cv%d' % c])
            cvr = ['cv%d' % c for c in range(0, 4)]
            cvk = ['cv%d' % c for c in range(4, 8)]
            cvv = ['cv%d' % c for c in range(8, 12)]
            S.dma('pool', lambda e: e.dma_start(out=K.rwR[:, t0:t0 + G].rearrange("(c p) t -> p c t", p=128), in_=cv[:, 0:4, :]), reads=cvr, writes=['rwR'])
            S.dma('pool', lambda e: e.dma_start(out=K.rwV[:, t0:t0 + G].rearrange("(c p) t -> p c t", p=128), in_=cv[:, 8:12, :]), reads=cvv, writes=['rwV'])
            for i in range(G // 128):
                p, pk = pp.get()
                for c in range(4):
                    S.op('pe', lambda e: e.transpose(out=p[:, c * 128:(c + 1) * 128], in_=cv[:, 8 + c, i * 128:(i + 1) * 128], identity=idf[:]), reads=cvv + ['idf'], writes=[pk], acc=True)
                v_, vk = vt.get()
                S.op('act', lambda e: e.activation(out=v_[:], in_=p[:], func=AF.Identity), reads=[pk], writes=[vk])
                S.dma('pool', lambda e: e.dma_start(out=K.rwVtok[t0 + i * 128:t0 + (i + 1) * 128, :], in_=v_[:]), reads=[vk], writes=['rwVtok'])
            for c in range(4):
                S.op('dve', lambda e: e.tensor_scalar_mul(out=kq[:, c, :], in0=cv[:, 4 + c, :], scalar1=kkv[:, c:c + 1]), reads=cvk + ['kkv'], writes=['kq'])
            S.op('act', lambda e: e.activation(out=sq[:], in_=kq[:], func=AF.Square), reads=['kq'], writes=['sq'])
            for c2 in range(2):
                p, pk = pp.get()
                S.op('pe', lambda e: e.matmul(p[:, 0:2 * G], lhsT=bones[:], rhs=sq[:, 2 * c2:2 * c2 + 2, :], start=True, stop=True), reads=['bones', 'sq'], writes=[pk])
                S.op('act', lambda e: e.activation(out=lnt[:, 2 * c2:2 * c2 + 2, :], in_=p[:, 0:2 * G], func=AF.Ln, bias=eps12[:], scale=1.0), reads=[pk, 'eps12'], writes=['lnt'])
            S.op('act', lambda e: e.activation(out=lnt[:], in_=lnt[:], func=AF.Exp, scale=-0.5), reads=['lnt'], writes=['lnt'])
            S.op('dve', lambda e: e.tensor_tensor(out=kk_[:], in0=kq[:], in1=lnt[:], op=ALU.mult), reads=['kq', 'lnt'], writes=['kk_'])
            S.dma('pool', lambda e: e.dma_start(out=K.rwKK[:, t0:t0 + G].rearrange("(c p) t -> p c t", p=128), in_=kk_[:]), reads=['kk_'], writes=['rwKK'])
            S.op('act', lambda e: e.activation(out=tw[:], in_=lo_[:, 0:2, :], func=AF.Tanh), reads=[lk], writes=['tw'])
            for d in range(2):
                for i in range(G // 128):
                    p, pk = pp.get()
                    S.op('pe', lambda e: e.matmul(p[:], lhsT=tw[:, d, i * 128:(i + 1) * 128], rhs=wup[:, d, :], start=True, stop=False), reads=['tw', 'wup'], writes=[pk], acc=True)
                    S.op('pe', lambda e: e.matmul(p[:], lhsT=ones1[:], rhs=w0r[:, d, :], start=False, stop=True), reads=['ones1', 'w0r'], writes=[pk], acc=True)
                    l_, lk2 = ld.get()
                    S.op('act', lambda e: e.activation(out=l_[:], in_=p[:], func=AF.Sigmoid), reads=[pk], writes=[lk2])
                    S.dma('pool', lambda e: e.dma_start(out=K.rwLD[d][t0 + i * 128:t0 + (i + 1) * 128, :], in_=l_[:]), reads=[lk2], writes=['rwLD%d' % d])
                for c in range(4):
                    p, pk = pp.get()
                    S.op('pe', lambda e: e.matmul(p[:, 0:G], lhsT=aup[:, d, c * 128:(c + 1) * 128], rhs=lo_[:, 2 + d, :], start=True, stop=True), reads=['aup', lk], writes=[pk])
                    S.op('act', lambda e: e.activation(out=av[:, c, :], in_=p[:, 0:G], func=AF.Sigmoid, bias=a0v[:, d, c:c + 1], scale=1.0), reads=[pk, 'a0v'], writes=['av'])
                    S.op('dve', lambda e: e.tensor_scalar(out=tt[:, c, :], in0=av[:, c, :], scalar1=kav[:, c:c + 1], scalar2=omka[:, c:c + 1], op0=ALU.mult, op1=ALU.add),
                         reads=['av', 'kav', 'omka'], writes=['tt'])
                S.op('dve', lambda e: e.tensor_tensor(out=kd[d][:], in0=cv[:, 4:8, :], in1=tt[:], op=ALU.mult), reads=cvk + ['tt'], writes=['kd%d' % d])
                S.op('dve', lambda e: e.tensor_tensor(out=bb[:], in0=kk_[:], in1=av[:], op=ALU.mult), reads=['kk_', 'av'], writes=['bb'])
                S.dma('pool', lambda e: e.dma_start(out=K.rwKD[d][:, t0:t0 + G].rearrange("(c p) t -> p c t", p=128), in_=kd[d][:]), reads=['kd%d' % d], writes=['rwKD%d' % d])
                S.dma('pool', lambda e: e.dma_start(out=K.rwB[d][:, t0:t0 + G].rearrange("(c p) t -> p c t", p=128), in_=bb[:]), reads=['bb'], writes=['rwB%d' % d])
            if lat:
                tl = t0 - TC
                S.op('act', lambda e: e.activation(out=sg[:], in_=gd_[:], func=AF.Sigmoid), reads=[gk], writes=['sg'])
                for c in range(4):
                    p, pk = pp.get()
                    S.op('pe', lambda e: e.matmul(p[:, 0:G], lhsT=gup[:, c * 128:(c + 1) * 128], rhs=sg[:], start=True, stop=True), reads=['gup', 'sg'], writes=[pk])
                    S.op('act', lambda e: e.activation(out=gg[:, c, :], in_=p[:, 0:G], func=AF.Identity), reads=[pk], writes=['gg'])
                S.dma('pool', lambda e: e.dma_start(out=K.rwG[:, tl:tl + G].rearrange("(c p) t -> p c t", p=128), in_=gg[:]), reads=['gg'], writes=['rwG'])
                S.op('dve', lambda e: e.tensor_tensor(out=bc[:], in0=kd[0][:], in1=kd[1][:], op=ALU.add), reads=['kd0', 'kd1'], writes=['bc'])
                S.op('dve', lambda e: e.tensor_tensor(out=bc[:], in0=bc[:], in1=cv[:, 0:4, :], op=ALU.mult), reads=['bc'] + cvr, writes=['bc'])
                for c in range(4):
                    S.op('dve', lambda e: e.tensor_scalar_mul(out=bc[:, c, :], in0=bc[:, c, :], scalar1=rkv[:, c:c + 1]), reads=['bc', 'rkv'], writes=['bc'])
                for c2 in range(2):
                    p, pk = pp.get()
                    S.op('pe', lambda e: e.matmul(p[:, 0:2 * G], lhsT=bones[:], rhs=bc[:, 2 * c2:2 * c2 + 2, :], start=True, stop=True), reads=['bones', 'bc'], writes=[pk])
                    S.op('dve', lambda e: e.tensor_tensor(out=bon[:, 2 * c2:2 * c2 + 2, :], in0=p[:, 0:2 * G], in1=cv[:, 8 + 2 * c2:8 + 2 * c2 + 2, :], op=ALU.mult), reads=[pk] + cvv, writes=['bon'])
                S.dma('pool', lambda e: e.dma_start(out=K.rwBON[:, tl:tl + G].rearrange("(c p) t -> p c t", p=128), in_=bon[:]), reads=['bon'], writes=['rwBON'])
        S.barrier()


def rw_consts():
    i = np.arange(128)[:, None]
    t = np.arange(128)[None, :]
    bd = (i // 64) == (t // 64)
    out = {}
    blk = ((i // 64) == (t // 64)).astype(np.float32)
    for d in range(2):
        strict = (bd & ((i < t) if d == 0 else (i > t))).astype(np.float32)
        incl = (bd & ((i <= t) if d == 0 else (i >= t))).astype(np.float32)
        m1 = np.concatenate([-strict, incl, blk], 1)
        m2 = np.concatenate([incl, blk], 1)
        m3 = np.concatenate([-strict.T, -strict.T, -np.ones((128, 64), np.float32)], 1)
        cum = np.float32(LDS) * np.concatenate([incl, strict, strict.T], 1)
        out['rwm%d' % d] = np.ascontiguousarray(np.concatenate([m1, m2, m3, cum], 1).astype(np.float32))
    out['i64dbl'] = np.ascontiguousarray((np.arange(128)[:, None] % 64 == np.arange(128)[None, :] % 64).astype(np.float32))
    out['bones'] = np.ascontiguousarray(bd.astype(np.float32))
    out['identf'] = np.eye(128, dtype=np.float32)
    return out


GN_EPS = 64e-5


def phase_rwscan(K, ntile_lat=64):
    nc, S = K.nc, K.S
    G = 128
    with ExitStack() as es:
        sb = lambda n, s, d: es.enter_context(nc.sbuf_tensor(_u(n), s, d))
        ps = lambda n, s, d: es.enter_context(nc.psum_tensor(_u(n), s, d))
        mk = sb("mk", [128, 1344], F32)
        idr = sb("idr", [128, 128], F32R)
        i64r = sb("i64r", [128, 128], F32R)
        i64f = sb("i64f", [128, 128], F32)
        idf = sb("idf", [128, 128], F32)
        o64 = sb("o64", [64, 64], F32)
        gng = sb("gng", [64, 8], F32)
        gnb = sb("gnb", [64, 8], F32)
        epsg = sb("epsg", [64, 1], F32)
        raw = [[sb("raw%d_%d" % (j, i), [128, 4, G], F32) for i in range(2)] for j in range(4)]
        vraw = [sb("vraw%d" % i, [128, 512], F32) for i in range(2)]
        ldt = [sb("ldt%d" % i, [128, 512], F32) for i in range(2)]
        vtr = [sb("vtr%d" % i, [128, 512], F32R) for i in range(2)]
        Ep = sb("Ep", [128, 4, G], F32)
        Em = sb("Em", [128, 4, G], F32)
        Ex = sb("Ex", [128, 4, G], F32)
        Ea = sb("Ea", [128, 4, G], F32)
        KRG = sb("KRG", [128, 4, 3, 128], F32R)
        BK = sb("BK", [128, 4, 2, 128], F32R)
        KB2 = sb("KB2", [128, 4, 2, 128], F32R)
        NS = 4
        La = [sb("La%d" % i, [128, 384], F32R) for i in range(NS)]
        Lb = [sb("Lb%d" % i, [128, 384], F32R) for i in range(NS)]
        ATa = [sb("ATa%d" % i, [128, 128], F32R) for i in range(NS)]
        ATb = [sb("ATb%d" % i, [128, 128], F32R) for i in range(NS)]
        X1 = [sb("X1%d" % i, [128, 320], F32R) for i in range(NS)]
        X2 = [sb("X2%d" % i, [128, 256], F32R) for i in range(NS)]
        Xf = [sb("Xf%d" % i, [128, 256], F32R) for i in range(NS)]
        QG1 = [sb("QG1_%d" % i, [64, 8, 256], F32R) for i in range(2)]
        QG2 = [sb("QG2_%d" % i, [128, 8, 256], F32R) for i in range(2)]
        Sth = sb("Sth", [64, 3, 8, 64], F32R)
        T = [sb("T%d" % i, [64, 8, G], F32) for i in range(4)]
        outb = sb("outb", [64, 8, G], BF16)
        pp = Rot([ps("sp%d" % i, [128, 512], F32) for i in range(7)], "sp")
        pst = ps("pst", [64, 512], F32)

        ld1 = lambda dst, src, key: S.dma('sp', lambda e: e.dma_start(out=dst, in_=src, allow_slow_non_contiguous=True), writes=[key])
        ld1(idf[:], K.identf, 'idf')
        ld1(i64f[:], K.i64dbl, 'i64f')
        ld1(gng[:], K.rwkv_gn_g.rearrange("o (h p) -> p (o h)", p=64), 'gng')
        ld1(gnb[:], K.rwkv_gn_b.rearrange("o (h p) -> p (o h)", p=64), 'gnb')
        S.op('dve', lambda e: e.tensor_copy(out=idr[:], in_=idf[:]), reads=['idf'], writes=['idr'])
        S.op('dve', lambda e: e.tensor_copy(out=i64r[:], in_=i64f[:]), reads=['i64f'], writes=['i64r'])
        S.op('dve', lambda e: e.memset(o64[:], 1.0 / 64.0), writes=['o64'])
        S.op('dve', lambda e: e.memset(epsg[:], GN_EPS), writes=['epsg'])
        zt = sb("zt", [64, 512], F32)
        S.op('dve', lambda e: e.memset(zt[:], 0.0), writes=['zt'])

        evq = [0]

        def evac(out_ap, in_ap, reads, writes):
            if evq[0] % 2 == 0:
                S.op('act', lambda e: e.activation(out=out_ap, in_=in_ap, func=AF.Identity), reads=reads, writes=writes)
            else:
                S.op('dve', lambda e: e.tensor_copy(out=out_ap, in_=in_ap), reads=reads, writes=writes)
            evq[0] += 1

        fl = lambda a: a[:, :, :].rearrange("p h t -> p (h t)")
        bcount = 0
        for d in range(2):
            m1 = mk[:, 0:384]
            m2 = mk[:, 384:640]
            m3 = mk[:, 640:960]
            cum = mk[:, 960:1344]
            S.dma('sp', lambda e: e.dma_start(out=mk[:], in_=K.rwm[d]), writes=['mk'])
            S.op('dve', lambda e: e.tensor_copy(out=Sth[:, 0].rearrange("p h v -> p (h v)"), in_=zt[:]), reads=['zt'], writes=['Sth0'])
            if d == 0:
                blocks = [0, 1] + list(range(2, 2 + ntile_lat))
            else:
                blocks = [1, 0] + list(range(1 + ntile_lat, 1, -1))
            chunks = [0, 1] if d == 0 else [1, 0]
            def body(b, qs):
                nonlocal bcount
                t0 = b * G
                lat = b >= 2
                sl = bcount % 2
                bcount += 1
                srcs = [K.rwR, K.rwKD[d], K.rwKK, K.rwB[d]]
                rk = ['raw%d_%d' % (j, sl) for j in range(4)]
                for j in range(4):
                    S.dma('sp', lambda e: e.dma_start(out=raw[j][sl][:], in_=srcs[j][:, t0:t0 + G].rearrange("(c p) t -> p c t", p=128)), writes=[rk[j]])
                S.dma('sp', lambda e: e.dma_start(out=vraw[sl][:], in_=K.rwVtok[t0:t0 + G, :]), writes=['vraw%d' % sl])
                S.dma('sp', lambda e: e.dma_start(out=ldt[sl][:], in_=K.rwLD[d][t0:t0 + G, :]), writes=['ldt%d' % sl])
                r_, kd_, kk_, b_ = [raw[j][sl] for j in range(4)]
                S.op('act', lambda e: e.activation(out=vtr[qs][:], in_=vraw[sl][:], func=AF.Identity), reads=['vraw%d' % sl], writes=['vtr%d' % qs])
                banks = [pp.get() for _ in range(3)]
                for c in range(4):
                    for q in range(3):
                        S.op('pe', lambda e: e.matmul(banks[q][0][:, c * 128:(c + 1) * 128], lhsT=ldt[sl][:, c * 128:(c + 1) * 128], rhs=cum[:, q * 128:(q + 1) * 128], start=True, stop=True),
                             reads=['ldt%d' % sl, 'mk'], writes=[banks[q][1]], acc=True)
                v3 = lambda bank: bank[:, :].rearrange("p (c t) -> p c t", c=4)
                S.op('act', lambda e: e.activation(out=Ep[:], in_=v3(banks[0][0]), func=AF.Exp), reads=[banks[0][1]], writes=['Ep'])
                S.op('act', lambda e: e.activation(out=Em[:], in_=v3(banks[0][0]), func=AF.Exp, scale=-1.0), reads=[banks[0][1]], writes=['Em'])
                S.op('act', lambda e: e.activation(out=Ex[:], in_=v3(banks[1][0]), func=AF.Exp), reads=[banks[1][1]], writes=['Ex'])
                S.op('act', lambda e: e.activation(out=Ea[:], in_=v3(banks[2][0]), func=AF.Exp), reads=[banks[2][1]], writes=['Ea'])
                yield 'f'
                S.op('dve', lambda e: e.tensor_tensor(out=KRG[:, :, 0, :], in0=kk_[:], in1=Ex[:], op=ALU.mult), reads=[rk[2], 'Ex'], writes=['KRG'])
                S.op('dve', lambda e: e.tensor_tensor(out=KRG[:, :, 1, :], in0=r_[:], in1=Ep[:], op=ALU.mult), reads=[rk[0], 'Ep'], writes=['KRG'])
                S.op('dve', lambda e: e.tensor_tensor(out=BK[:, :, 0, :], in0=b_[:], in1=Em[:], op=ALU.mult), reads=[rk[3], 'Em'], writes=['BK'])
                S.op('dve', lambda e: e.tensor_tensor(out=BK[:, :, 1, :], in0=kd_[:], in1=Em[:], op=ALU.mult), reads=[rk[1], 'Em'], writes=['BK'])
                S.op('dve', lambda e: e.tensor_tensor(out=KB2[:, :, 0, :], in0=kd_[:], in1=Ea[:], op=ALU.mult), reads=[rk[1], 'Ea'], writes=['KB2'])
                S.op('dve', lambda e: e.tensor_tensor(out=KB2[:, :, 1, :], in0=b_[:], in1=Ea[:], op=ALU.mult), reads=[rk[3], 'Ea'], writes=['KB2'])
                for cc in range(2):
                    pos = cc * 64 + (63 if d == 0 else 0)
                    in0 = i64f[:, cc * 64:(cc + 1) * 64].unsqueeze(1).to_broadcast([128, 4, 64])
                    in1 = Ep[:, :, pos:pos + 1].to_broadcast([128, 4, 64])
                    S.op('dve', lambda e: e.tensor_tensor(out=KRG[:, :, 2, cc * 64:(cc + 1) * 64], in0=in0, in1=in1, op=ALU.mult), reads=['i64f', 'Ep'], writes=['KRG'])

                for g0 in range(0, 8, NS):
                    grp = list(range(g0, g0 + NS))
                    cur = {}
                    for s_, h in enumerate(grp):
                        c, pb = h // 2, 64 * (h % 2)
                        fm = lambda arr, k0, k1: arr[pb:pb + 64, c, k0:k1, :].rearrange("p k t -> p (k t)")
                        p1, k1 = pp.get()
                        S.op('pe', lambda e: e.matmul(p1[:, 0:256], lhsT=fm(BK, 0, 1), rhs=fm(KRG, 0, 2), start=True, stop=True), reads=['BK', 'KRG'], writes=[k1], acc=True)
                        S.op('pe', lambda e: e.matmul(p1[:, 256:384], lhsT=fm(KB2, 1, 2), rhs=i64r[pb:pb + 64, :], start=True, stop=True), reads=['KB2', 'i64r'], writes=[k1], acc=True)
                        S.op('dve', lambda e: e.tensor_tensor(out=La[s_][:], in0=p1[:, 0:384], in1=m1, op=ALU.mult), reads=[k1, 'mk'], writes=['La%d' % s_])
                        p2, k2 = pp.get()
                        S.op('pe', lambda e: e.matmul(p2[:, 0:128], lhsT=fm(BK, 1, 2), rhs=fm(KRG, 1, 2), start=True, stop=True), reads=['BK', 'KRG'], writes=[k2], acc=True)
                        S.op('pe', lambda e: e.matmul(p2[:, 128:256], lhsT=fm(KB2, 0, 1), rhs=i64r[pb:pb + 64, :], start=True, stop=True), reads=['KB2', 'i64r'], writes=[k2], acc=True)
                        S.op('dve', lambda e: e.tensor_tensor(out=X2[s_][:], in0=p2[:, 0:256], in1=m2, op=ALU.mult), reads=[k2, 'mk'], writes=['X2%d' % s_])
                        p3, k3 = pp.get()
                        S.op('pe', lambda e: e.matmul(p3[:, 0:256], lhsT=fm(KRG, 0, 1), rhs=fm(BK, 0, 2), start=True, stop=True), reads=['BK', 'KRG'], writes=[k3], acc=True)
                        S.op('pe', lambda e: e.matmul(p3[:, 256:320], lhsT=fm(KRG, 0, 1), rhs=i64r[pb:pb + 64, 0:64], start=True, stop=True), reads=['KRG', 'i64r'], writes=[k3], acc=True)
                        S.op('dve', lambda e: e.tensor_tensor(out=X1[s_][:], in0=p3[:, 0:320], in1=m3, op=ALU.mult), reads=[k3, 'mk'], writes=['X1%d' % s_])
                        cur[s_] = (La[s_], 'La%d' % s_, X1[s_][:, 0:128], 'X1%d' % s_)
                        yield 'f'
                    for lev in range(5):
                        pend = []
                        nxt = {}
                        for s_ in range(NS):
                            L, Lk, AT, ATk = cur[s_]
                            p, pk = pp.get()
                            S.op('pe', lambda e: e.matmul(p[:, 0:384], lhsT=AT, rhs=L[:, 0:384], start=True, stop=False), reads=[Lk, ATk], writes=[pk], acc=True)
                            S.op('pe', lambda e: e.matmul(p[:, 128:384], lhsT=idr[:], rhs=L[:, 128:384], start=False, stop=True), reads=[Lk, 'idr'], writes=[pk], acc=True)
                            pend.append((p, pk))
                        yield 'f'
                        for s_ in range(NS):
                            p, pk = pend[s_]
                            Ln_ = Lb[s_] if lev % 2 == 0 else La[s_]
                            Lnk = ('Lb%d' if lev % 2 == 0 else 'La%d') % s_
                            evac(Ln_[:], p[:, 0:384], [pk], [Lnk])
                            nxt[s_] = (Ln_, Lnk)
                        for s_ in range(NS):
                            Ln_, Lnk = nxt[s_]
                            p, pk = pend[s_]
                            S.op('pe', lambda e: e.transpose(out=p[:, 384:512], in_=Ln_[:, 0:128].bitcast(F32), identity=idf[:]), reads=[Lnk, 'idf'], writes=[pk])
                        yield 'f'
                        for s_ in range(NS):
                            Ln_, Lnk = nxt[s_]
                            p, pk = pend[s_]
                            ATn = ATa[s_] if lev % 2 == 0 else ATb[s_]
                            ATnk = ('ATa%d' if lev % 2 == 0 else 'ATb%d') % s_
                            evac(ATn[:], p[:, 384:512], [pk], [ATnk])
                            cur[s_] = (Ln_, Lnk, ATn[:], ATnk)
                    for s_, h in enumerate(grp):
                        L, Lk, AT, ATk = cur[s_]
                        p, pk = pp.get()
                        S.op('pe', lambda e: e.matmul(p[:, 0:256], lhsT=AT, rhs=L[:, 128:384], start=True, stop=False), reads=[Lk, ATk], writes=[pk], acc=True)
                        S.op('pe', lambda e: e.matmul(p[:, 0:256], lhsT=idr[:], rhs=L[:, 128:384], start=False, stop=True), reads=[Lk, 'idr'], writes=[pk], acc=True)
                        evac(Xf[s_][:], p[:, 0:256], [pk], ['Xf%d' % s_])
                    yield 'f'
                    for s_, h in enumerate(grp):
                        c, pb = h // 2, 64 * (h % 2)
                        p, pk = pp.get()
                        S.op('pe', lambda e: e.matmul(p[:, 0:256], lhsT=idr[:], rhs=X2[s_][:], start=True, stop=False), reads=['X2%d' % s_, 'idr'], writes=[pk], acc=True)
                        S.op('pe', lambda e: e.matmul(p[:, 0:256], lhsT=X1[s_][:, 128:256], rhs=Xf[s_][:], start=False, stop=True), reads=['X1%d' % s_, 'Xf%d' % s_], writes=[pk], acc=True)
                        evac(QG2[qs][:, h, :], p[:, 0:256], [pk], ['QG2_%d_%d' % (qs, h)])
                        q, qk = pp.get()
                        rg = KRG[pb:pb + 64, c, 1:3, :].rearrange("p k t -> p (k t)")
                        S.op('pe', lambda e: e.matmul(q[0:64, 0:256], lhsT=idr[pb:pb + 64, pb:pb + 64], rhs=rg, start=True, stop=False), reads=['KRG', 'idr'], writes=[qk], acc=True)
                        S.op('pe', lambda e: e.matmul(q[0:64, 0:256], lhsT=X1[s_][:, 256:320], rhs=Xf[s_][:], start=False, stop=True), reads=['X1%d' % s_, 'Xf%d' % s_], writes=[qk], acc=True)
                        evac(QG1[qs][:, h, :], q[0:64, 0:256], [qk], ['QG1_%d_%d' % (qs, h)])
                        yield 'f'
                yield 'F'
                for s, cc in enumerate(chunks):
                    for h in range(8):
                        S.op('pe', lambda e: e.matmul(pst[:, h * 64:(h + 1) * 64], lhsT=QG1[qs][:, h, 128 + cc * 64:128 + (cc + 1) * 64], rhs=Sth[:, s, h, :], start=True, stop=False),
                             reads=['QG1_%d_%d' % (qs, h), 'Sth%d' % s], writes=['pst'], acc=True)
                        S.op('pe', lambda e: e.matmul(pst[:, h * 64:(h + 1) * 64], lhsT=QG2[qs][:, h, 128 + cc * 64:128 + (cc + 1) * 64], rhs=vtr[qs][:, h * 64:(h + 1) * 64], start=False, stop=True),
                             reads=['QG2_%d_%d' % (qs, h), 'vtr%d' % qs], writes=['pst'], acc=True)
                    evac(Sth[:, s + 1].rearrange("p h v -> p (h v)"), pst[:, :], ['pst'], ['Sth%d' % (s + 1)])
                    yield 'b'
                if lat:
                    ybuf = T[0]
                    for hq in range(2):
                        bank = pp.get()
                        for h4 in range(4):
                            h = hq * 4 + h4
                            S.op('pe', lambda e: e.matmul(bank[0][0:64, h4 * 128:(h4 + 1) * 128], lhsT=vtr[qs][:, h * 64:(h + 1) * 64], rhs=QG2[qs][:, h, 0:128], start=True, stop=False),
                                 reads=['QG2_%d_%d' % (qs, h), 'vtr%d' % qs], writes=[bank[1]], acc=True)
                            for s, cc in enumerate(chunks):
                                S.op('pe', lambda e: e.matmul(bank[0][0:64, h4 * 128 + cc * 64:h4 * 128 + (cc + 1) * 64], lhsT=Sth[:, s, h, :], rhs=QG1[qs][:, h, cc * 64:(cc + 1) * 64], start=False, stop=(s == 1)),
                                     reads=['QG1_%d_%d' % (qs, h), 'Sth%d' % s], writes=[bank[1]], acc=True)
                        evac(ybuf[:, hq * 4:hq * 4 + 4, :], bank[0][0:64, :].rearrange("p (h t) -> p h t", h=4), [bank[1]], ['T0'])
                        yield 'b'
                    tl = t0 - TC
                    if d == 0:
                        S.dma('pool', lambda e: e.dma_start(out=K.rwY0[:, tl:tl + G].rearrange("(h p) t -> p h t", p=64), in_=ybuf[:]), reads=['T0'], writes=['rwY0'])
                    else:
                        y0b, cen, sqb = T[1], T[2], T[3]
                        S.dma('sp', lambda e: e.dma_start(out=y0b[:], in_=K.rwY0[:, tl:tl + G].rearrange("(h p) t -> p h t", p=64)), reads=['rwY0'], writes=['T1'])
                        S.op('dve', lambda e: e.tensor_tensor(out=ybuf[:], in0=ybuf[:], in1=y0b[:], op=ALU.add), reads=['T0', 'T1'], writes=['T0'])
                        for q in range(2):
                            p, pk = pp.get()
                            S.op('pe', lambda e: e.matmul(p[0:64, :], lhsT=o64[:], rhs=fl(ybuf)[:, q * 512:(q + 1) * 512], start=True, stop=True), reads=['o64', 'T0'], writes=[pk])
                            S.op('dve', lambda e: e.tensor_tensor(out=fl(cen)[:, q * 512:(q + 1) * 512], in0=fl(ybuf)[:, q * 512:(q + 1) * 512], in1=p[0:64, :], op=ALU.subtract), reads=[pk, 'T0'], writes=['T2'])
                        S.op('act', lambda e: e.activation(out=sqb[:], in_=cen[:], func=AF.Square), reads=['T2'], writes=['T3'])
                        yield 'b'
                        rsb = T[1]
                        for q in range(2):
                            p, pk = pp.get()
                            S.op('pe', lambda e: e.matmul(p[0:64, :], lhsT=o64[:], rhs=fl(sqb)[:, q * 512:(q + 1) * 512], start=True, stop=True), reads=['o64', 'T3'], writes=[pk])
                            S.op('act', lambda e: e.activation(out=fl(rsb)[:, q * 512:(q + 1) * 512], in_=p[0:64, :], func=AF.Ln, bias=epsg[:], scale=1.0), reads=[pk, 'epsg'], writes=['T1'])
                        S.op('act', lambda e: e.activation(out=rsb[:], in_=rsb[:], func=AF.Exp, scale=-0.5), reads=['T1'], writes=['T1'])
                        yield 'b'
                        bonb, ggb = T[3], T[0]
                        S.dma('sp', lambda e: e.dma_start(out=bonb[:], in_=K.rwBON[:, tl:tl + G].rearrange("(h p) t -> p h t", p=64)), reads=['rwBON'], writes=['T3'])
                        S.op('dve', lambda e: e.tensor_tensor(out=cen[:], in0=cen[:], in1=rsb[:], op=ALU.mult), reads=['T2', 'T1'], writes=['T2'])
                        S.dma('sp', lambda e: e.dma_start(out=ggb[:], in_=K.rwG[:, tl:tl + G].rearrange("(h p) t -> p h t", p=64)), reads=['rwG'], writes=['T0'])
                        S.op('dve', lambda e: e.tensor_tensor(out=cen[:], in0=cen[:], in1=gng[:, :].unsqueeze(2).to_broadcast([64, 8, G]), op=ALU.mult), reads=['T2', 'gng'], writes=['T2'])
                        S.op('dve', lambda e: e.tensor_tensor(out=cen[:], in0=cen[:], in1=gnb[:, :].unsqueeze(2).to_broadcast([64, 8, G]), op=ALU.add), reads=['T2', 'gnb'], writes=['T2'])
                        S.op('dve', lambda e: e.tensor_tensor(out=cen[:], in0=cen[:], in1=bonb[:], op=ALU.add), reads=['T2', 'T3'], writes=['T2'])
                        S.op('dve', lambda e: e.tensor_tensor(out=outb[:], in0=cen[:], in1=ggb[:], op=ALU.mult), reads=['T2', 'T0'], writes=['outb'])
                        S.dma('pool', lambda e: e.dma_start(out=K.mixT[0:512, tl:tl + G].rearrange("(h p) t -> p h t", p=64), in_=outb[:]), reads=['outb'], writes=['mixT'])
                S.op('dve', lambda e: e.tensor_copy(out=Sth[:, 0], in_=Sth[:, 2]), reads=['Sth2'], writes=['Sth0'])
            def step(gen_):
                try:
                    return next(gen_)
                except StopIteration:
                    return None
            prev = None
            qslot = 0
            for b in blocks:
                gcur = body(b, qslot)
                while True:
                    r = step(gcur)
                    if prev is not None and step(prev) is None:
                        prev = None
                    if r == 'F' or r is None:
                        break
                while prev is not None:
                    if step(prev) is None:
                        prev = None
                prev = gcur
                qslot ^= 1
            while prev is not None:
                if step(prev) is None:
                    prev = None
            S.barrier()


ALPHA = 2.0 ** 0.25
ROWW = 1088
BIGPOS = 4096.0


def phase_mix(K):
    nc, S = K.nc, K.S
    NT = TL // 128
    with ExitStack() as es:
        sb = lambda n, s, d: es.enter_context(nc.sbuf_tensor(_u(n), s, d))
        ps = lambda n, s, d: es.enter_context(nc.psum_tensor(_u(n), s, d))
        wo = sb("wo", [128, 8, 1024], BF16)
        wst = [sb("owst%d" % i, [128, 8, 256], F32) for i in range(2)]
        bc = {n: sb("bc_" + n, [128, 1024], F32) for n in ('g1', 'ln1g', 'ln1b', 'sc2', 'sh2')}
        rt = sb("rt", [128, 8, 16], F32)
        idf = sb("idf", [128, 128], F32)
        epst = sb("epst", [128, 1], F32)
        onesf = sb("onesf", [128, 128], F32)
        onesb = sb("onesb", [128, 128], BF16)
        ustr = sb("ustr", [128, 128], BF16)
        mt = [sb("mt%d" % i, [128, 8, 128], BF16) for i in range(2)]
        xt = [sb("xt%d" % i, [128, 1024], F32) for i in range(2)]
        t1 = sb("t1", [128, 1024], F32)
        pre = sb("pre", [128, 1024], F32)
        x1 = [sb("x1_%d" % i, [128, 1024], F32) for i in range(2)]
        uf = [sb("uf%d" % i, [128, 1024], F32) for i in range(2)]
        urow = [sb("urow%d" % i, [128, ROWW], BF16) for i in range(2)]
        uT = sb("uT", [128, 8, 128], F32)
        st = sb("st", [128, 2, 6], F32)
        mv = sb("mv", [128, 2], F32)
        lnv = sb("lnv", [128, 1], F32)
        rstd = sb("rstd", [128, 1], F32)
        lg = sb("lg", [128, 16], F32)
        mx = sb("mx", [128, 1], F32)
        sm = sb("sm", [128, 1], F32)
        affall = sb("affall", [128, NT, 16], F32)
        tok = sb("tok", [128, 1], I32)
        lo = sb("lo", [128, 16], F32)
        mid = sb("mid", [128, 16], F32)
        ge = sb("ge", [128, 16], F32)
        cntp = sb("cntp", [128, 16], F32)
        mskt = sb("mskt", [128, NT, 16], F32)
        mskb = sb("mskb", [128, NT, 16], BF16)
        csT = sb("csT", [128, 16, NT], F32)
        incT = sb("incT", [128, 16, NT], F32)
        rmask = sb("rmask", [128, 16, NT], F32)
        posf = sb("posf", [128, NT, 16], F32)
        posi = sb("posi", [128, NT, 16], I32)
        zt = sb("zt", [128, 1024], F32)
        pm = [ps("pm%d" % i, [128, 1024], F32) for i in range(2)]
        ptr = ps("ptr", [128, 1024], F32)
        psm = ps("psm", [128, 512], F32)
        psn = ps("psn", [128, 512], F32)

        ld1 = lambda dst, src, key, rd=(): S.dma('sp', lambda e: e.dma_start(out=dst, in_=src, allow_slow_non_contiguous=True), reads=list(rd), writes=[key])
        ld1(idf[:], K.identf, 'idf')
        ld1(ustr[:], K.ustrict, 'ustr')
        ld1(rt[:], K.router.rearrange("(k p) e -> p k e", p=128), 'rt')
        ld1(bc['g1'][:], K.modd[0:1, 2048:3072].partition_broadcast(128), 'bc_g1', ['modd'])
        ld1(bc['sh2'][:], K.modd[0:1, 3072:4096].partition_broadcast(128), 'bc_sh2', ['modd'])
        ld1(bc['sc2'][:], K.modd[0:1, 4096:5120].partition_broadcast(128), 'bc_sc2', ['modd'])
        ld1(bc['ln1g'][:], K.ln1_g.partition_broadcast(128), 'bc_ln1g')
        ld1(bc['ln1b'][:], K.ln1_b.partition_broadcast(128), 'bc_ln1b')
        S.op('dve', lambda e: e.tensor_scalar_add(out=bc['sc2'][:], in0=bc['sc2'][:], scalar1=1.0), reads=['bc_sc2'], writes=['bc_sc2'])
        S.op('dve', lambda e: e.memset(epst[:], 1e-5), writes=['epst'])
        S.op('dve', lambda e: e.memset(onesf[:], 1.0), writes=['onesf'])
        S.op('dve', lambda e: e.memset(onesb[:], 1.0), writes=['onesb'])
        S.op('dve', lambda e: e.memset(zt[:], 0.0), writes=['zt'])
        for j in range(4):
            w = wst[j % 2]
            wk = 'owst%d' % (j % 2)
            S.dma('sp', lambda e: e.dma_start(out=w[:], in_=K.w_out[:, j * 256:(j + 1) * 256].rearrange("(k p) n -> p k n", p=128)), writes=[wk])
            S.op('pool', lambda e: e.tensor_copy(out=wo[:, :, j * 256:(j + 1) * 256], in_=w[:]), reads=[wk], writes=['wo'])
        for i in range(NT):
            S.dma('pool', lambda e: e.dma_start(out=K.yacc[i * 128:(i + 1) * 128, :], in_=zt[:]), reads=['zt'], writes=['yacc%d' % i])

        mvs = [mv, sb("mv1", [128, 2], F32)]
        rstds = [rstd, sb("rstd1", [128, 1], F32)]
        sts = [st, sb("st1", [128, 2, 6], F32)]
        lnvs = [lnv, sb("lnv1", [128, 1], F32)]

        def ln_stats(src, srck, lane=0):
            sfx = '' if lane == 0 else '1'
            for c in range(2):
                S.op('dve', lambda e: e.bn_stats(out=sts[lane][:, c, :], in_=src[:, c * 512:(c + 1) * 512]), reads=[srck], writes=['st%d%s' % (c, sfx)])
            S.op('dve', lambda e: e.bn_aggr(out=mvs[lane][:], in_=sts[lane][:]), reads=['st0' + sfx, 'st1' + sfx], writes=['mv' + sfx])
            S.op('act', lambda e: e.activation(out=lnvs[lane][:], in_=mvs[lane][:, 1:2], func=AF.Ln, bias=epst[:], scale=1.0), reads=['mv' + sfx, 'epst'], writes=['lnv' + sfx])
            S.op('act', lambda e: e.activation(out=rstds[lane][:], in_=lnvs[lane][:], func=AF.Exp, scale=-0.5), reads=['lnv' + sfx], writes=['rstd' + sfx])

        def tbody(i):
            sl = i % 2
            m_, mk_ = mt[sl], 'mt%d' % sl
            x_, xk = xt[sl], 'xt%d' % sl
            S.dma('sp', lambda e: e.dma_start(out=m_[:], in_=K.mixT[:, i * 128:(i + 1) * 128].rearrange("(k p) t -> p k t", p=128)), reads=['mixT'], writes=[mk_])
            S.dma('sp', lambda e: e.dma_start(out=x_[:], in_=K.xin[TC + i * 128:TC + (i + 1) * 128, :]), writes=[xk])
            p_, pk_ = pm[sl], 'pm%d' % sl
            for half in range(2):
                for kc in range(8):
                    S.op('pe', lambda e: e.matmul(p_[:, half * 512:(half + 1) * 512], lhsT=m_[:, kc, :], rhs=wo[:, kc, half * 512:(half + 1) * 512], start=(kc == 0), stop=(kc == 7)),
                         reads=[mk_, 'wo'], writes=[pk_], acc=True)
            yield 'f'
            S.op('dve', lambda e: e.tensor_tensor(out=t1[:], in0=p_[:], in1=bc['g1'][:], op=ALU.mult), reads=[pk_, 'bc_g1'], writes=['t1'])
            S.op('dve', lambda e: e.scalar_tensor_tensor(out=pre[:], in0=x_[:], scalar=ALPHA, in1=t1[:], op0=ALU.mult, op1=ALU.add), reads=[xk, 't1'], writes=['pre'])
            ln_stats(pre, 'pre')
            yield 'f'
            x1_, x1k = x1[sl], 'x1_%d' % sl
            S.op('dve', lambda e: e.tensor_scalar(out=t1[:], in0=pre[:], scalar1=mv[:, 0:1], scalar2=rstd[:], op0=ALU.subtract, op1=ALU.mult), reads=['pre', 'mv', 'rstd'], writes=['t1'])
            S.op('dve', lambda e: e.tensor_tensor(out=t1[:], in0=t1[:], in1=bc['ln1g'][:], op=ALU.mult), reads=['t1', 'bc_ln1g'], writes=['t1'])
            S.op('dve', lambda e: e.tensor_tensor(out=x1_[:], in0=t1[:], in1=bc['ln1b'][:], op=ALU.add), reads=['t1', 'bc_ln1b'], writes=[x1k])
            S.dma('pool', lambda e: e.dma_start(out=K.x1d[i * 128:(i + 1) * 128, :], in_=x1_[:]), reads=[x1k], writes=['x1d'])
            yield 'f'
            ln_stats(x1_, x1k, 1)
            yield 'f'
            S.op('dve', lambda e: e.tensor_scalar(out=pre[:], in0=x1_[:], scalar1=mvs[1][:, 0:1], scalar2=rstds[1][:], op0=ALU.subtract, op1=ALU.mult), reads=[x1k, 'mv1', 'rstd1'], writes=['pre'])
            S.op('dve', lambda e: e.tensor_tensor(out=pre[:], in0=pre[:], in1=bc['sc2'][:], op=ALU.mult), reads=['pre', 'bc_sc2'], writes=['pre'])
            S.op('dve', lambda e: e.tensor_tensor(out=uf[sl][:], in0=pre[:], in1=bc['sh2'][:], op=ALU.add), reads=['pre', 'bc_sh2'], writes=['uf%d' % sl])
            ur, urk = urow[sl], 'urow%d' % sl
            S.op('act', lambda e: e.activation(out=ur[:, 0:1024], in_=uf[sl][:], func=AF.Identity), reads=['uf%d' % sl], writes=[urk])
            yield 'F'
            for k in range(8):
                S.op('pe', lambda e: e.transpose(out=ptr[:, k * 128:(k + 1) * 128], in_=uf[sl][:, k * 128:(k + 1) * 128], identity=idf[:]), reads=['uf%d' % sl, 'idf'], writes=['ptr'], acc=True)
            S.op('act', lambda e: e.activation(out=uT[:].rearrange("p k t -> p (k t)"), in_=ptr[:], func=AF.Identity), reads=['ptr'], writes=['uT'])
            yield 'b'
            for k in range(8):
                S.op('pe', lambda e: e.matmul(psm[:, 0:16], lhsT=uT[:, k, :], rhs=rt[:, k, :], start=(k == 0), stop=(k == 7)), reads=['uT', 'rt'], writes=['psm'], acc=True)
            S.op('dve', lambda e: e.reduce_max(out=mx[:], in_=psm[:, 0:16], axis=AX.X), reads=['psm'], writes=['mx'])
            S.op('dve', lambda e: e.tensor_scalar_mul(out=mx[:], in0=mx[:], scalar1=-1.0), reads=['mx'], writes=['mx'])
            yield 'b'
            S.op('act', lambda e: e.activation(out=lg[:], in_=psm[:, 0:16], func=AF.Exp, bias=mx[:], scale=1.0, accum_out=sm[:]), reads=['psm', 'mx'], writes=['lg', 'sm'])
            S.op('dve', lambda e: e.reciprocal(out=sm[:], in_=sm[:]), reads=['sm'], writes=['sm'])
            yield 'b'
            S.op('dve', lambda e: e.tensor_scalar_mul(out=affall[:, i, :], in0=lg[:], scalar1=sm[:]), reads=['lg', 'sm'], writes=['affall'])
            S.op('dve', lambda e: e.tensor_copy(out=ur[:, 1024:1056].bitcast(F32), in_=affall[:, i, :]), reads=['affall'], writes=[urk])
            S.op('pool', lambda e: e.iota(tok[:], pattern=[[0, 1]], base=i * 128, channel_multiplier=1), writes=['tok'])
            S.op('pool', lambda e: e.tensor_copy(out=ur[:, 1056:1058].bitcast(I32), in_=tok[:]), reads=['tok'], writes=[urk])
            S.dma('pool', lambda e: e.dma_start(out=K.urd[i * 128:(i + 1) * 128, 0:1058], in_=ur[:, 0:1058]), reads=[urk], writes=['urd%d' % i])

        def step(gen_):
            try:
                return next(gen_)
            except StopIteration:
                return None
        prev = None
        for i in range(NT):
            gcur = tbody(i)
            while True:
                r = step(gcur)
                if prev is not None and step(prev) is None:
                    prev = None
                if r == 'F' or r is None:
                    break
            while prev is not None:
                if step(prev) is None:
                    prev = None
            prev = gcur
        while prev is not None:
            if step(prev) is None:
                prev = None

        S.op('dve', lambda e: e.memset(lo[:], 0.0), writes=['lo'])
        for k in range(30):
            hk = 2.0 ** -(k + 1)
            S.op('dve', lambda e: e.tensor_scalar_add(out=mid[:], in0=lo[:], scalar1=hk), reads=['lo'], writes=['mid'])
            S.op('dve', lambda e: e.tensor_tensor(out=mskt[:], in0=affall[:], in1=mid[:, :].unsqueeze(1).to_broadcast([128, NT, 16]), op=ALU.is_ge), reads=['affall', 'mid'], writes=['mskt'])
            S.op('dve', lambda e: e.tensor_reduce(out=cntp[:], in_=mskt[:].rearrange("p i e -> p e i"), axis=AX.X, op=ALU.add), reads=['mskt'], writes=['cntp'])
            S.op('pe', lambda e: e.matmul(psn[:, 0:16], lhsT=onesf[:], rhs=cntp[:], start=True, stop=True), reads=['onesf', 'cntp'], writes=['psn'])
            S.op('dve', lambda e: e.tensor_scalar(out=ge[:], in0=psn[:, 0:16], scalar1=float(CAP) - 0.5, scalar2=hk, op0=ALU.is_ge, op1=ALU.mult), reads=['psn'], writes=['ge'])
            S.op('dve', lambda e: e.tensor_tensor(out=lo[:], in0=lo[:], in1=ge[:], op=ALU.add), reads=['lo', 'ge'], writes=['lo'])
        S.op('dve', lambda e: e.tensor_tensor(out=mskt[:], in0=affall[:], in1=lo[:, :].unsqueeze(1).to_broadcast([128, NT, 16]), op=ALU.is_ge), reads=['affall', 'lo'], writes=['mskt'])
        S.op('act', lambda e: e.activation(out=mskb[:], in_=mskt[:], func=AF.Identity), reads=['mskt'], writes=['mskb'])
        mflat = mskb[:].rearrange("p i e -> p (i e)")
        for hh in range(2):
            S.op('pe', lambda e: e.matmul(psm[:, :], lhsT=onesb[:], rhs=mflat[:, hh * 512:(hh + 1) * 512], start=True, stop=True), reads=['onesb', 'mskb'], writes=['psm'])
            S.op('dve', lambda e: e.tensor_copy(out=csT[:, :, hh * 32:(hh + 1) * 32], in_=psm[:, :].rearrange("p (i e) -> p e i", e=16)), reads=['psm'], writes=['csT'])
        S.op('dve', lambda e: e.memset(rmask[:], 1.0), writes=['rmask'])
        S.op('dve', lambda e: e.memset(rmask[:, :, 0:1], 0.0), writes=['rmask'])
        S.op('dve', lambda e: e.tensor_tensor_scan(out=incT[:].rearrange("p e i -> p (e i)"), data0=rmask[:].rearrange("p e i -> p (e i)"), data1=csT[:].rearrange("p e i -> p (e i)"),
                                                   initial=0.0, op0=ALU.mult, op1=ALU.add), reads=['rmask', 'csT'], writes=['incT'])
        S.op('dve', lambda e: e.tensor_tensor(out=incT[:], in0=incT[:], in1=csT[:], op=ALU.subtract), reads=['incT', 'csT'], writes=['incT'])
        for hh in range(2):
            S.op('pe', lambda e: e.matmul(psm[:, :], lhsT=ustr[:], rhs=mflat[:, hh * 512:(hh + 1) * 512], start=True, stop=True), reads=['ustr', 'mskb'], writes=['psm'])
            S.op('dve', lambda e: e.tensor_tensor(out=posf[:, hh * 32:(hh + 1) * 32, :], in0=psm[:, :].rearrange("p (i e) -> p i e", e=16),
                                                  in1=incT[:, :, hh * 32:(hh + 1) * 32].rearrange("p e i -> p i e"), op=ALU.add), reads=['psm', 'incT'], writes=['posf'])
        S.op('dve', lambda e: e.scalar_tensor_tensor(out=posf[:], in0=posf[:], scalar=-BIGPOS, in1=mskt[:], op0=ALU.add, op1=ALU.mult), reads=['posf', 'mskt'], writes=['posf'])
        S.op('dve', lambda e: e.tensor_scalar_add(out=posf[:], in0=posf[:], scalar1=BIGPOS), reads=['posf'], writes=['posf'])
        S.op('dve', lambda e: e.tensor_copy(out=posi[:], in_=posf[:]), reads=['posf'], writes=['posi'])
        if K.dbg_pos is not None:
            S.dma('sp', lambda e: e.dma_start(out=K.dbg_pos, in_=posf[:]), reads=['posf'], writes=['dbg_pos'])
        breg = nc.gpsimd.to_reg(CAP - 1)
        for i in range(NT):
            sl = i % 2
            ur, urk = urow[sl], 'urow%d' % sl
            S.dma('sp', lambda e: e.dma_start(out=ur[:, 0:1058], in_=K.urd[i * 128:(i + 1) * 128, 0:1058]), reads=['urd%d' % i], writes=[urk])
            for ex in range(NE):
                S.dma('pool', lambda e: e.indirect_dma_start(out=K.xe_d[ex], out_offset=bass.IndirectOffsetOnAxis(ap=posi[:, i, ex:ex + 1], axis=0),
                                                             in_=ur[:, :], in_offset=None, bounds_check=breg, oob_is_err=False),
                      reads=[urk, 'posi'], writes=['xe_d_%d_%d' % (i, ex)])
        S.barrier()


def phase_moe(K, experts=range(NE)):
    nc, S = K.nc, K.S
    NT = TL // 128
    with ExitStack() as es:
        sb = lambda n, s, d: es.enter_context(nc.sbuf_tensor(_u(n), s, d))
        ps = lambda n, s, d: es.enter_context(nc.psum_tensor(_u(n), s, d))
        W = [[sb("W%d_%d" % (m, i), [128, 8, 1024], BF16) for i in range(2)] for m in range(3)]
        wst = Rot([sb("ewst%d" % i, [128, 8, 256], F32) for i in range(3)], "ewst")
        idb = sb("idb", [128, 128], BF16)
        xrow = Rot([sb("xrow%d" % i, [128, ROWW], BF16) for i in range(2)], "xrow")
        xeT = [sb("xeT%d" % i, [128, 8, 1024], BF16) for i in range(2)]
        hidT = sb("hidT", [128, 8, 1024], BF16)
        gates = [sb("gates%d" % i, [128, 8], F32) for i in range(2)]
        idxs = [sb("idxs%d" % i, [128, 8], I32) for i in range(2)]
        sgt = Rot([sb("sgt%d" % i, [128, 512], F32) for i in range(2)], "sgt")
        ye = Rot([sb("ye%d" % i, [128, 1024], F32) for i in range(2)], "ye")
        pt = Rot([ps("ept%d" % i, [128, 1024], BF16) for i in range(2)], "ept")
        pg = Rot([ps("epg%d" % i, [128, 512], F32) for i in range(6)], "epg")
        S.dma('sp', lambda e: e.dma_start(out=idb[:], in_=K.ident), writes=['idb'])
        cast_i = [0]

        def load_w(ex, slot):
            for m, src in enumerate((K.exp_w_gate, K.exp_w_up, K.exp_w_down)):
                for j in range(4):
                    w, wk = wst.get()
                    S.dma('sp', lambda e: e.dma_start(out=w[:], in_=src[ex, :, j * 256:(j + 1) * 256].rearrange("(k p) n -> p k n", p=128)), writes=[wk])
                    eng = 'dve'
                    cast_i[0] += 1
                    if eng == 'dve':
                        S.op('dve', lambda e: e.tensor_copy(out=W[m][slot][:, :, j * 256:(j + 1) * 256], in_=w[:]), reads=[wk], writes=['W%d_%d' % (m, slot)])
                    else:
                        S.op('act', lambda e: e.activation(out=W[m][slot][:, :, j * 256:(j + 1) * 256], in_=w[:], func=AF.Identity), reads=[wk], writes=['W%d_%d' % (m, slot)])

        exl = list(experts)
        load_w(exl[0], 0)
        if len(exl) > 1:
            load_w(exl[1], 1)
        def ebody(n, ex):
            xs = n % 2
            slot = n % 2
            Wg, Wu, Wd = W[0][slot], W[1][slot], W[2][slot]
            wkeys = ['W%d_%d' % (m, slot) for m in range(3)]
            for j in range(8):
                xr, xk = xrow.get()
                S.dma('sp', lambda e: e.dma_start(out=xr[:, 0:1058], in_=K.xe_d[ex][j * 128:(j + 1) * 128, 0:1058]), reads=['xe_d'], writes=[xk])
                S.op('dve', lambda e: e.tensor_copy(out=gates[xs][:, j:j + 1], in_=xr[:, 1024:1056].bitcast(F32)[:, ex:ex + 1]), reads=[xk], writes=['gates%d' % xs])
                S.op('dve', lambda e: e.tensor_copy(out=idxs[xs][:, j:j + 1], in_=xr[:, 1056:1058].bitcast(I32)), reads=[xk], writes=['idxs%d' % xs])
                p, pk = pt.get()
                for k in range(8):
                    S.op('pe', lambda e: e.transpose(out=p[:, k * 128:(k + 1) * 128], in_=xr[:, k * 128:(k + 1) * 128], identity=idb[:]), reads=[xk, 'idb'], writes=[pk], acc=True)
                S.op('act', lambda e: e.activation(out=xeT[xs][:, :, j * 128:(j + 1) * 128], in_=p[:].rearrange("p (k t) -> p k t", k=8), func=AF.Identity), reads=[pk], writes=['xeT%d' % xs])
                yield 'f'
            yield 'F'
            for fc in range(8):
                for half in range(2):
                    g_, gk = pg.get()
                    u_, uk = pg.get()
                    for kc in range(8):
                        S.op('pe', lambda e: e.matmul(g_[:], lhsT=Wg[:, kc, fc * 128:(fc + 1) * 128], rhs=xeT[xs][:, kc, half * 512:(half + 1) * 512], start=(kc == 0), stop=(kc == 7)),
                             reads=[wkeys[0], 'xeT%d' % xs], writes=[gk], acc=True)
                    for kc in range(8):
                        S.op('pe', lambda e: e.matmul(u_[:], lhsT=Wu[:, kc, fc * 128:(fc + 1) * 128], rhs=xeT[xs][:, kc, half * 512:(half + 1) * 512], start=(kc == 0), stop=(kc == 7)),
                             reads=[wkeys[1], 'xeT%d' % xs], writes=[uk], acc=True)
                    s_, sk = sgt.get()
                    S.op('act', lambda e: e.activation(out=s_[:], in_=g_[:], func=AF.Silu), reads=[gk], writes=[sk])
                    S.op('dve', lambda e: e.tensor_tensor(out=hidT[:, fc, half * 512:(half + 1) * 512], in0=s_[:], in1=u_[:], op=ALU.mult), reads=[sk, uk], writes=['hidT'])
                    yield 'b'
            for j in range(8):
                y_, yk = ye.get()
                for dh in range(2):
                    o_, ok = pg.get()
                    for fc in range(8):
                        S.op('pe', lambda e: e.matmul(o_[:], lhsT=hidT[:, fc, j * 128:(j + 1) * 128], rhs=Wd[:, fc, dh * 512:(dh + 1) * 512], start=(fc == 0), stop=(fc == 7)),
                             reads=[wkeys[2], 'hidT'], writes=[ok], acc=True)
                    S.op('act', lambda e: e.activation(out=y_[:, dh * 512:(dh + 1) * 512], in_=o_[:], func=AF.Identity, scale=gates[xs][:, j:j + 1]), reads=[ok, 'gates%d' % xs], writes=[yk])
                S.dma('pool', lambda e: e.indirect_dma_start(out=K.yacc, out_offset=bass.IndirectOffsetOnAxis(ap=idxs[xs][:, j:j + 1], axis=0), in_=y_[:, :], in_offset=None,
                                                             compute_op=ALU.add),
                      reads=[yk, 'idxs%d' % xs], writes=['yacc'])
                yield 'b'
        def step(gen_):
            try:
                return next(gen_)
            except StopIteration:
                return None
        prev = None
        for n, ex in enumerate(exl):
            gcur = ebody(n, ex)
            while True:
                r = step(gcur)
                if prev is not None and step(prev) is None:
                    prev = None
                if r == 'F' or r is None:
                    break
            while prev is not None:
                if step(prev) is None:
                    prev = None
            if n >= 1 and n + 1 < len(exl):
                load_w(exl[n + 1], (n + 1) % 2)
            prev = gcur
        while prev is not None:
            if step(prev) is None:
                prev = None
        S.barrier()


def phase_final(K):
    nc, S = K.nc, K.S
    NT = TL // 128
    with ExitStack() as es:
        sb = lambda n, s, d: es.enter_context(nc.sbuf_tensor(_u(n), s, d))
        bc = {n: sb("fbc_" + n, [128, 1024], F32) for n in ('g2', 'ln2g', 'ln2b')}
        epst = sb("epst", [128, 1], F32)
        x1 = [sb("fx1_%d" % i, [128, 1024], F32) for i in range(2)]
        ya = [sb("fya_%d" % i, [128, 1024], F32) for i in range(2)]
        t1 = sb("t1", [128, 1024], F32)
        pre = sb("pre", [128, 1024], F32)
        ob = [sb("fob_%d" % i, [128, 1024], F32) for i in range(2)]
        st = sb("st", [128, 2, 6], F32)
        mv = sb("mv", [128, 2], F32)
        lnv = sb("lnv", [128, 1], F32)
        rstd = sb("rstd", [128, 1], F32)
        ld1 = lambda dst, src, key, rd=(): S.dma('sp', lambda e: e.dma_start(out=dst, in_=src, allow_slow_non_contiguous=True), reads=list(rd), writes=[key])
        ld1(bc['g2'][:], K.modd[0:1, 5120:6144].partition_broadcast(128), 'fbc_g2', ['modd'])
        ld1(bc['ln2g'][:], K.ln2_g.partition_broadcast(128), 'fbc_ln2g')
        ld1(bc['ln2b'][:], K.ln2_b.partition_broadcast(128), 'fbc_ln2b')
        S.op('dve', lambda e: e.memset(epst[:], 1e-5), writes=['epst'])
        for i in range(NT):
            sl = i % 2
            S.dma('sp', lambda e: e.dma_start(out=x1[sl][:], in_=K.x1d[i * 128:(i + 1) * 128, :]), reads=['x1d'], writes=['fx1_%d' % sl])
            S.dma('sp', lambda e: e.dma_start(out=ya[sl][:], in_=K.yacc[i * 128:(i + 1) * 128, :]), reads=['yacc'], writes=['fya_%d' % sl])
            S.op('dve', lambda e: e.tensor_tensor(out=t1[:], in0=ya[sl][:], in1=bc['g2'][:], op=ALU.mult), reads=['fya_%d' % sl, 'fbc_g2'], writes=['t1'])
            S.op('dve', lambda e: e.scalar_tensor_tensor(out=pre[:], in0=x1[sl][:], scalar=ALPHA, in1=t1[:], op0=ALU.mult, op1=ALU.add), reads=['fx1_%d' % sl, 't1'], writes=['pre'])
            for c in range(2):
                S.op('dve', lambda e: e.bn_stats(out=st[:, c, :], in_=pre[:, c * 512:(c + 1) * 512]), reads=['pre'], writes=['st%d' % c])
            S.op('dve', lambda e: e.bn_aggr(out=mv[:], in_=st[:]), reads=['st0', 'st1'], writes=['mv'])
            S.op('act', lambda e: e.activation(out=lnv[:], in_=mv[:, 1:2], func=AF.Ln, bias=epst[:], scale=1.0), reads=['mv', 'epst'], writes=['lnv'])
            S.op('act', lambda e: e.activation(out=rstd[:], in_=lnv[:], func=AF.Exp, scale=-0.5), reads=['lnv'], writes=['rstd'])
            S.op('dve', lambda e: e.tensor_scalar(out=t1[:], in0=pre[:], scalar1=mv[:, 0:1], scalar2=rstd[:], op0=ALU.subtract, op1=ALU.mult), reads=['pre', 'mv', 'rstd'], writes=['t1'])
            S.op('dve', lambda e: e.tensor_tensor(out=t1[:], in0=t1[:], in1=bc['ln2g'][:], op=ALU.mult), reads=['t1', 'fbc_ln2g'], writes=['t1'])
            S.op('dve', lambda e: e.tensor_tensor(out=ob[sl][:], in0=t1[:], in1=bc['ln2b'][:], op=ALU.add), reads=['t1', 'fbc_ln2b'], writes=['fob_%d' % sl])
            S.dma('pool', lambda e: e.dma_start(out=K.out[i * 128:(i + 1) * 128, :], in_=ob[sl][:]), reads=['fob_%d' % sl], writes=['out'])
        S.barrier()


def build_program(debug=(), phases=None, dbg_in=(), opts=None):
    opts = opts or {}
    nc = bass.Bass("TRN2", target_bir_lowering=False)
    K = Ctx()
    K.nc = nc
    di = lambda n, s, d: nc.dram_tensor(n, s, d, kind="ExternalInput").ap()
    K.xin = di("xin", [TA, D], F32)
    K.ccT = di("ccT", [128, 8, 2], F32)
    K.w_ada = di("w_ada", [D, 6 * D], F32)
    K.b_ada = di("b_ada", [1, 6 * D], F32)
    K.w_in = di("w_in", [D, 2560], F32)
    K.cosT = di("cosT", [64, TL], F32)
    K.sinT = di("sinT", [64, TL], F32)
    K.ident = di("ident", [128, 128], BF16)
    K.identf = di("identf", [128, 128], F32)
    K.i64dbl = di("i64dbl", [128, 128], F32)
    K.bones = di("bones", [128, 128], F32)
    K.rwm = [di("rwm%d" % d, [128, 1344], F32) for d in range(2)]
    K.mla_q_norm = di("mla_q_norm", [1, 256], F32)
    K.mla_kv_norm = di("mla_kv_norm", [1, 256], F32)
    K.w_uq = di("w_uq", [256, 1024], F32)
    K.w_uk = di("w_uk", [256, 512], F32)
    K.w_uv = di("w_uv", [256, 512], F32)
    K.rwkv_conv = di("rwkv_conv", [3, 1536], F32)
    K.rwkv_w0 = di("rwkv_w0", [2, 512], F32)
    K.rwkv_w_up = di("rwkv_w_up", [2, 64, 512], F32)
    K.rwkv_a0 = di("rwkv_a0", [2, 512], F32)
    K.rwkv_a_up = di("rwkv_a_up", [2, 64, 512], F32)
    K.rwkv_g_up = di("rwkv_g_up", [128, 512], F32)
    K.rwkv_k_k = di("rwkv_k_k", [1, 512], F32)
    K.rwkv_k_a = di("rwkv_k_a", [1, 512], F32)
    K.rwkv_r_k = di("rwkv_r_k", [1, 512], F32)
    K.rwkv_gn_g = di("rwkv_gn_g", [1, 512], F32)
    K.rwkv_gn_b = di("rwkv_gn_b", [1, 512], F32)
    K.w_out = di("w_out", [D, D], F32)
    K.ln1_g = di("ln1_g", [1, D], F32)
    K.ln1_b = di("ln1_b", [1, D], F32)
    K.ln2_g = di("ln2_g", [1, D], F32)
    K.ln2_b = di("ln2_b", [1, D], F32)
    K.router = di("router", [D, NE], F32)
    K.ustrict = di("ustrict", [128, 128], BF16)
    K.exp_w_gate = di("exp_w_gate", [NE, D, D], F32)
    K.exp_w_up = di("exp_w_up", [NE, D, D], F32)
    K.exp_w_down = di("exp_w_down", [NE, D, D], F32)

    def scratch(n, s, d):
        if n in dbg_in:
            return nc.dram_tensor(n, s, d, kind="ExternalInput").ap()
        kind = "ExternalOutput" if n in debug else "Internal"
        return nc.dram_tensor(n, s, d, kind=kind).ap()
    K.modd = scratch("modd", [2, 6 * D], F32)
    K.hT = scratch("hT", [2432, TA], F32)
    K.krT = scratch("krT", [64, TA], BF16)
    K.qnT = scratch("qnT", [512, TL], BF16)
    K.qrT = scratch("qrT", [256, TL], BF16)
    K.knT = scratch("knT", [512, TA], BF16)
    K.vtok = scratch("vtok", [TA, 512], BF16)
    K.mixT = scratch("mixT", [1024, TL], BF16)
    K.rwR = scratch("rwR", [512, TA], F32)
    K.rwV = scratch("rwV", [512, TA], F32)
    K.rwKK = scratch("rwKK", [512, TA], F32)
    K.rwKD = [scratch("rwKD%d" % d, [512, TA], F32) for d in range(2)]
    K.rwB = [scratch("rwB%d" % d, [512, TA], F32) for d in range(2)]
    K.rwVtok = scratch("rwVtok", [TA, 512], F32)
    K.rwLD = [scratch("rwLD%d" % d, [TA, 512], F32) for d in range(2)]
    K.rwG = scratch("rwG", [512, TL], F32)
    K.rwBON = scratch("rwBON", [512, TL], F32)
    K.rwY0 = scratch("rwY0", [512, TL], F32)
    K.x1d = scratch("x1d", [TL, D], F32)
    K.urd = scratch("urd", [TL, ROWW], BF16)
    K.xe_d = [scratch("xe_d%d" % e_, [CAP, ROWW], BF16) for e_ in range(NE)]
    K.yacc = scratch("yacc", [TL, D], F32)
    K.dbg_pos = scratch("dbg_pos", [128, TL // 128, 16], F32) if 'dbg_pos' in debug else None
    K.out = nc.dram_tensor("out", [TL, D], F32, kind="ExternalOutput").ap()
    allp = ['mod', 'inproj', 'mlaprep', 'attn', 'rwprep', 'rwscan', 'mix', 'moe', 'final']
    if phases is None:
        phases = allp
    with ExitStack() as es:
        S = Sync(nc, es)
        K.S = S
        if 'mod' in phases:
            phase_mod(K)
        if 'inproj' in phases:
            phase_inproj(K)
        if 'mlaprep' in phases:
            phase_mlaprep(K)
        if 'attn' in phases:
            phase_attn(K)
        if 'attn1' in phases:
            phase_attn(K, heads=(1,), nqt=2)
        if 'rwprep' in phases:
            phase_rwprep(K)
        if 'rwscan' in phases:
            phase_rwscan(K, **opts.get('rwscan', {}))
        if 'mix' in phases:
            phase_mix(K)
        if 'moe' in phases:
            phase_moe(K, **opts.get('moe', {}))
        if 'final' in phases:
            phase_final(K)
        S.wait_all('sp')
        print("instructions", S.n_inst, "waits", S.n_wait, "sems", S.nsem + NDMA)
    return nc


_SWAP = np.concatenate([np.arange(16, 32), np.arange(0, 16), np.arange(48, 64), np.arange(32, 48)])


def rope_tables():
    half = 32
    inv_freq = (10000.0 ** (-np.arange(0, half, 2, dtype=np.float32) / half)).astype(np.float32)
    t = np.arange(TL)
    rr = (t // 64).astype(np.float32)[None, :]
    cc = (t % 64).astype(np.float32)[None, :]
    ang_r = (inv_freq[:, None] * rr).astype(np.float32)
    ang_c = (inv_freq[:, None] * cc).astype(np.float32)
    cosT = np.concatenate([np.cos(ang_r), np.cos(ang_r), np.cos(ang_c), np.cos(ang_c)], 0).astype(np.float32)
    sinT = np.concatenate([-np.sin(ang_r), np.sin(ang_r), -np.sin(ang_c), np.sin(ang_c)], 0).astype(np.float32)
    return np.ascontiguousarray(cosT), np.ascontiguousarray(sinT)


def make_in_maps(inputs, batches):
    f = lambda a: np.ascontiguousarray(np.asarray(a, dtype=np.float32))
    w_in = f(inputs['w_in'][0])
    w_in_ext = np.concatenate([w_in, w_in[:, 2432:2496][:, _SWAP]], axis=1)
    cosT, sinT = rope_tables()
    wuq = f(inputs['mla_w_uq'][0])
    cols = []
    for h in range(4):
        nope = wuq[:, h * 192:h * 192 + 128]
        rope = wuq[:, h * 192 + 128:h * 192 + 192]
        cols += [nope, rope, rope[:, _SWAP]]
    wuq_ext = np.ascontiguousarray(np.concatenate(cols, axis=1))
    shared = {
        'w_ada': f(inputs['w_ada'][0]), 'b_ada': f(inputs['b_ada']), 'w_in': np.ascontiguousarray(w_in_ext),
        'cosT': cosT, 'sinT': sinT, 'ident': np.eye(128).astype(ml_dtypes.bfloat16),
        'mla_q_norm': f(inputs['mla_q_norm']), 'mla_kv_norm': f(inputs['mla_kv_norm']),
        'w_uq': wuq_ext, 'w_uk': f(inputs['mla_w_uk'][0]), 'w_uv': f(inputs['mla_w_uv'][0]),
        'rwkv_conv': f(inputs['rwkv_conv'][0]), 'rwkv_w0': f(inputs['rwkv_w0'][0]), 'rwkv_w_up': f(inputs['rwkv_w_up'][0]),
        'rwkv_a0': f(inputs['rwkv_a0'][0]), 'rwkv_a_up': f(inputs['rwkv_a_up'][0]), 'rwkv_g_up': f(inputs['rwkv_g_up'][0]),
        'rwkv_k_k': f(inputs['rwkv_k_k']), 'rwkv_k_a': f(inputs['rwkv_k_a']), 'rwkv_r_k': f(inputs['rwkv_r_k']).reshape(1, 512),
        'rwkv_gn_g': f(inputs['rwkv_gn_g']), 'rwkv_gn_b': f(inputs['rwkv_gn_b']),
        'w_out': f(inputs['w_out'][0]), 'ln1_g': f(inputs['ln1_g']), 'ln1_b': f(inputs['ln1_b']),
        'ln2_g': f(inputs['ln2_g']), 'ln2_b': f(inputs['ln2_b']), 'router': f(inputs['router'][0]),
        'ustrict': (np.arange(128)[:, None] < np.arange(128)[None, :]).astype(ml_dtypes.bfloat16),
        'exp_w_gate': f(inputs['exp_w_gate'][0]), 'exp_w_up': f(inputs['exp_w_up'][0]), 'exp_w_down': f(inputs['exp_w_down'][0]),
    }
    shared.update(rw_consts())
    maps = []
    for b in batches:
        m = dict(shared)
        m['xin'] = np.ascontiguousarray(np.concatenate([inputs['ctx'][b], inputs['x'][b]], axis=0).astype(np.float32))
        cc = np.stack([inputs['c'][b], inputs['c_ctx']], axis=-1).astype(np.float32)
        m['ccT'] = np.ascontiguousarray(cc.reshape(8, 128, 2).transpose(1, 0, 2))
        maps.append(m)
    return maps


_CONST_KEYS = ('cosT', 'sinT', 'ident', 'identf', 'i64dbl', 'bones', 'rwm0', 'rwm1', 'ustrict')
BATCH_CORES = (0, 1, 4, 5)


def kernel(**inputs):
    nc = build_program()
    real = make_in_maps(inputs, [0, 1, 2, 3])
    zero = {k: (v if k in _CONST_KEYS else np.zeros_like(v)) for k, v in real[0].items()}
    maps = [zero] * 8
    for b, c in enumerate(BATCH_CORES):
        maps[c] = real[b]
    res = run_bass_kernel_spmd(nc, maps, core_ids=list(range(8)))
    out = np.stack([res.results[c]['out'] for c in BATCH_CORES], axis=0)
    return out.astype(np.float32)
```

```python
import numpy as np
import ml_dtypes
from contextlib import ExitStack
import concourse.bass as bass
import concourse.mybir as mybir
from concourse.bass_utils import run_bass_kernel_spmd

F32 = mybir.dt.float32
BF16 = mybir.dt.bfloat16
I32 = mybir.dt.int32
AF = mybir.ActivationFunctionType
ALU = mybir.AluOpType
AX = mybir.AxisListType

EPOCH = 12000
NDMA = 24

D = 1024
TL = 8192
TC = 256
TA = TL + TC
NE = 16
CAP = 1024


class Sync:
    def __init__(self, nc, es):
        self.nc = nc
        self.es = es
        self.eng = {'pe': nc.tensor, 'dve': nc.vector, 'act': nc.scalar,
                    'pool': nc.gpsimd, 'sp': nc.sync}
        self.sem = {}
        self.cnt = {}
        self.cur = {}
        self.known = {e: {} for e in self.eng}
        self.snap = {}
        self.last_w = {}
        self.readers = {}
        self.dma_keys = []
        self.dma_rr = 0
        self.nsem = 0
        for e in self.eng:
            self._new_epoch(e)
        for i in range(NDMA):
            k = ('dma', i)
            self.sem[k] = es.enter_context(nc.semaphore('dq%d' % i))
            self.cnt[k] = 0
            self.dma_keys.append(k)
        self.n_inst = 0
        self.n_wait = 0

    def _new_epoch(self, e):
        idx = self.nsem
        self.nsem += 1
        k = (e, idx)
        self.sem[k] = self.es.enter_context(self.nc.semaphore('s_%s_%d' % (e, idx)))
        self.cnt[k] = 0
        self.cur[e] = k

    def _need(self, e, ticket):
        k, v = ticket
        kn = self.known[e]
        if kn.get(k, 0) >= v:
            return
        self.eng[e].wait_ge(self.sem[k], v)
        self.n_wait += 1
        kn[k] = v
        sn = self.snap.get(ticket)
        if sn:
            for kk, vv in sn.items():
                if kn.get(kk, 0) < vv:
                    kn[kk] = vv

    def _deps(self, e, reads, writes, acc):
        for b in reads:
            t = self.last_w.get(b)
            if t is not None:
                self._need(e, t)
        for b in writes:
            t = self.last_w.get(b)
            if t is not None and not (acc and t[0] == self.cur[e]):
                self._need(e, t)
            for t in self.readers.get(b, ()):
                self._need(e, t)

    def _record(self, ticket, reads, writes):
        for b in reads:
            self.readers.setdefault(b, []).append(ticket)
        for b in writes:
            self.last_w[b] = ticket
            self.readers[b] = []

    def op(self, e, fn, reads=(), writes=(), acc=False):
        if self.cnt[self.cur[e]] >= EPOCH:
            self._new_epoch(e)
        self._deps(e, reads, writes, acc)
        k = self.cur[e]
        inst = fn(self.eng[e])
        inst.then_inc(self.sem[k], 1)
        self.cnt[k] += 1
        t = (k, self.cnt[k])
        self.snap[t] = dict(self.known[e])
        self._record(t, reads, writes)
        self.n_inst += 1
        return t

    def dma(self, e, fn, reads=(), writes=()):
        k = self.dma_keys[self.dma_rr]
        self.dma_rr = (self.dma_rr + 1) % NDMA
        if self.cnt[k] > 0:
            self._need(e, (k, self.cnt[k]))
        self._deps(e, reads, writes, False)
        inst = fn(self.eng[e])
        inst.then_inc(self.sem[k], 16)
        self.cnt[k] += 16
        t = (k, self.cnt[k])
        self.snap[t] = dict(self.known[e])
        self._record(t, reads, writes)
        self.n_inst += 1
        return t

    def wait_all(self, e):
        for k, v in list(self.cnt.items()):
            if v > 0:
                self._need(e, (k, v))

    def barrier(self):
        for e in self.eng:
            self.wait_all(e)
        self.last_w.clear()
        self.readers.clear()


class Ctx:
    pass


_UC = [0]


def _u(n):
    _UC[0] += 1
    return '%s_%d' % (n, _UC[0])


def phase_mod(K):
    nc, S = K.nc, K.S
    with ExitStack() as es:
        sb = lambda n, s, d: es.enter_context(nc.sbuf_tensor(_u(n), s, d))
        ccs = sb("ccs", [128, 8, 2], F32)
        scT = sb("scT", [128, 8, 2], F32)
        wa = [sb("wa%d" % i, [128, 8, 512], F32) for i in range(2)]
        ba = sb("ba", [1, 6144], F32)
        one1 = sb("one1", [1, 1], F32)
        mrow = [sb("mrow%d" % r, [1, 6144], F32) for r in range(2)]
        pm = [es.enter_context(nc.psum_tensor(_u("pm%d" % i), [1, 512], F32)) for i in range(2)]
        S.dma('sp', lambda e: e.dma_start(out=ccs[:], in_=K.ccT), writes=['ccs'])
        S.dma('sp', lambda e: e.dma_start(out=ba[:], in_=K.b_ada), writes=['ba'])
        S.op('dve', lambda e: e.memset(one1[:], 1.0), writes=['one1'])
        S.op('act', lambda e: e.activation(out=scT[:], in_=ccs[:], func=AF.Silu), reads=['ccs'], writes=['scT'])
        for j in range(12):
            w = wa[j % 2]
            wk = 'wa%d' % (j % 2)
            S.dma('sp', lambda e: e.dma_start(out=w[:], in_=K.w_ada[:, j * 512:(j + 1) * 512].rearrange("(k p) n -> p k n", p=128)), writes=[wk])
            for r in range(2):
                if r == 1 and j >= 4:
                    continue
                pk = 'pm%d' % r
                for k in range(8):
                    S.op('pe', lambda e: e.matmul(pm[r][:], lhsT=scT[:, k, r:r + 1], rhs=w[:, k, :], start=(k == 0), stop=False),
                         reads=['scT', wk], writes=[pk], acc=True)
                S.op('pe', lambda e: e.matmul(pm[r][:], lhsT=one1[:], rhs=ba[:, j * 512:(j + 1) * 512], start=False, stop=True),
                     reads=['one1', 'ba'], writes=[pk], acc=True)
                S.op('dve', lambda e: e.tensor_copy(out=mrow[r][:, j * 512:(j + 1) * 512], in_=pm[r][:]), reads=[pk], writes=['mrow%d' % r])
        S.dma('sp', lambda e: e.dma_start(out=K.modd[0:1, :], in_=mrow[0][:]), reads=['mrow0'], writes=['modd'])
        S.dma('sp', lambda e: e.dma_start(out=K.modd[1:2, 0:2048], in_=mrow[1][:, 0:2048]), reads=['mrow1'], writes=['modd'])
        S.barrier()


def phase_inproj(K):
    nc, S = K.nc, K.S
    with ExitStack() as es:
        sb = lambda n, s, d: es.enter_context(nc.sbuf_tensor(_u(n), s, d))
        ps = lambda n, s, d: es.enter_context(nc.psum_tensor(_u(n), s, d))
        wb = sb("wb", [128, 8, 2560], BF16)
        wst = [sb("wst%d" % i, [128, 8, 320], F32) for i in range(2)]
        idt = sb("idt", [128, 128], BF16)
        epst = sb("epst", [128, 1], F32)
        scp = [sb("scp%d" % r, [128, 8], F32) for r in range(2)]
        shp = [sb("shp%d" % r, [128, 8], F32) for r in range(2)]
        xt = [sb("xt%d" % i, [128, 1024], F32) for i in range(2)]
        xn = [sb("xn%d" % i, [128, 1024], BF16) for i in range(2)]
        st = sb("st", [128, 2, 6], F32)
        mv = sb("mv", [128, 2], F32)
        lnv = sb("lnv", [128, 1], F32)
        rstd = sb("rstd", [128, 1], F32)
        xmT = [sb("xmT%d" % i, [128, 8, 512], BF16) for i in range(2)]
        stg = [sb("stg%d" % i, [128, 4, 512], F32) for i in range(2)]
        cst = sb("cst", [64, 512], F32)
        snt = sb("snt", [64, 512], F32)
        kr1 = sb("kr1", [64, 512], F32)
        kr2 = sb("kr2", [64, 512], F32)
        krb = sb("krb", [64, 512], BF16)
        pt = [ps("pt%d" % i, [128, 1024], BF16) for i in range(2)]
        po = [ps("po%d" % i, [128, 512], F32) for i in range(3)]
        pk = [ps("pk%d" % i, [64, 512], F32) for i in range(2)]

        S.dma('sp', lambda e: e.dma_start(out=idt[:], in_=K.ident), writes=['idt'])
        S.op('dve', lambda e: e.memset(epst[:], 1e-5), writes=['epst'])
        for r in range(2):
            S.dma('sp', lambda e: e.dma_start(out=shp[r][:], in_=K.modd[r:r + 1, 0:1024].rearrange("o (k p) -> p (o k)", p=128), allow_slow_non_contiguous=True), reads=['modd'], writes=['shp%d' % r])
            S.dma('sp', lambda e: e.dma_start(out=scp[r][:], in_=K.modd[r:r + 1, 1024:2048].rearrange("o (k p) -> p (o k)", p=128), allow_slow_non_contiguous=True), reads=['modd'], writes=['scp%d' % r])
            S.op('dve', lambda e: e.tensor_scalar_add(out=scp[r][:], in0=scp[r][:], scalar1=1.0), reads=['scp%d' % r], writes=['scp%d' % r])
        for j in range(8):
            w = wst[j % 2]
            wk = 'wst%d' % (j % 2)
            S.dma('sp', lambda e: e.dma_start(out=w[:], in_=K.w_in[:, j * 320:(j + 1) * 320].rearrange("(k p) n -> p k n", p=128)), writes=[wk])
            S.op('pool', lambda e: e.tensor_copy(out=wb[:, :, j * 320:(j + 1) * 320], in_=w[:]), reads=[wk], writes=['wb'])

        groups = [(0, 256, 1)] + [(256 + g * 512, 512, 0) for g in range(16)]
        tile_i = 0
        ev = 0
        for gi, (t0, G, r) in enumerate(groups):
            xm = xmT[gi % 2]
            xmk = 'xmT%d' % (gi % 2)
            if r == 0:
                S.dma('sp', lambda e: e.dma_start(out=cst[:], in_=K.cosT[:, t0 - 256:t0 - 256 + 512]), writes=['cst'])
                S.dma('sp', lambda e: e.dma_start(out=snt[:], in_=K.sinT[:, t0 - 256:t0 - 256 + 512]), writes=['snt'])
            for i in range(G // 128):
                sl = tile_i % 2
                tile_i += 1
                xk, xnk, ptk = 'xt%d' % sl, 'xn%d' % sl, 'pt%d' % sl
                tt = t0 + i * 128
                S.dma('sp', lambda e: e.dma_start(out=xt[sl][:], in_=K.xin[tt:tt + 128, :]), writes=[xk])
                for c in range(2):
                    S.op('dve', lambda e: e.bn_stats(out=st[:, c, :], in_=xt[sl][:, c * 512:(c + 1) * 512]), reads=[xk], writes=['st%d' % c])
                S.op('dve', lambda e: e.bn_aggr(out=mv[:], in_=st[:]), reads=['st0', 'st1'], writes=['mv'])
                S.op('act', lambda e: e.activation(out=lnv[:], in_=mv[:, 1:2], func=AF.Ln, bias=epst[:], scale=1.0), reads=['mv', 'epst'], writes=['lnv'])
                S.op('act', lambda e: e.activation(out=rstd[:], in_=lnv[:], func=AF.Exp, scale=-0.5), reads=['lnv'], writes=['rstd'])
                S.op('dve', lambda e: e.tensor_scalar(out=xn[sl][:], in0=xt[sl][:], scalar1=mv[:, 0:1], scalar2=rstd[:], op0=ALU.subtract, op1=ALU.mult),
                     reads=[xk, 'mv', 'rstd'], writes=[xnk])
                for k in range(8):
                    S.op('pe', lambda e: e.transpose(out=pt[sl][:, k * 128:(k + 1) * 128], in_=xn[sl][:, k * 128:(k + 1) * 128], identity=idt[:]),
                         reads=[xnk, 'idt'], writes=[ptk], acc=True)
                for k in range(8):
                    S.op('act', lambda e: e.activation(out=xm[:, k, i * 128:(i + 1) * 128], in_=pt[sl][:, k * 128:(k + 1) * 128], func=AF.Identity,
                                                       bias=shp[r][:, k:k + 1], scale=scp[r][:, k:k + 1]),
                         reads=[ptk, 'shp%d' % r, 'scp%d' % r], writes=[xmk])
            for cb in range(5):
                sg = stg[cb % 2]
                sgk = 'stg%d' % (cb % 2)
                ncols = 4 if cb < 4 else 3
                for cc in range(ncols):
                    ci = cb * 4 + cc
                    p = po[ev % 3]
                    pkk = 'po%d' % (ev % 3)
                    for k in range(8):
                        S.op('pe', lambda e: e.matmul(p[:, 0:G], lhsT=wb[:, k, ci * 128:(ci + 1) * 128], rhs=xm[:, k, 0:G], start=(k == 0), stop=(k == 7)),
                             reads=['wb', xmk], writes=[pkk], acc=True)
                    if ev % 2 == 0:
                        S.op('dve', lambda e: e.tensor_copy(out=sg[:, cc, 0:G], in_=p[:, 0:G]), reads=[pkk], writes=[sgk])
                    else:
                        S.op('act', lambda e: e.activation(out=sg[:, cc, 0:G], in_=p[:, 0:G], func=AF.Identity), reads=[pkk], writes=[sgk])
                    ev += 1
                r0 = cb * 512
                S.dma('pool', lambda e: e.dma_start(out=K.hT[r0:r0 + ncols * 128, t0:t0 + G].rearrange("(c p) t -> p c t", p=128), in_=sg[:, 0:ncols, 0:G]),
                      reads=[sgk], writes=['hT'])
            for q in range(2):
                for k in range(8):
                    S.op('pe', lambda e: e.matmul(pk[q][:, 0:G], lhsT=wb[:, k, 2432 + q * 64:2432 + (q + 1) * 64], rhs=xm[:, k, 0:G], start=(k == 0), stop=(k == 7)),
                         reads=['wb', xmk], writes=['pk%d' % q], acc=True)
            if r == 0:
                S.op('dve', lambda e: e.tensor_tensor(out=kr1[:], in0=pk[0][:], in1=cst[:], op=ALU.mult), reads=['pk0', 'cst'], writes=['kr1'])
                S.op('dve', lambda e: e.tensor_tensor(out=kr2[:], in0=pk[1][:], in1=snt[:], op=ALU.mult), reads=['pk1', 'snt'], writes=['kr2'])
                S.op('dve', lambda e: e.tensor_tensor(out=krb[:], in0=kr1[:], in1=kr2[:], op=ALU.add), reads=['kr1', 'kr2'], writes=['krb'])
            else:
                S.op('dve', lambda e: e.tensor_copy(out=krb[:, 0:G], in_=pk[0][:, 0:G]), reads=['pk0', 'pk1'], writes=['krb'])
            S.dma('pool', lambda e: e.dma_start(out=K.krT[:, t0:t0 + G], in_=krb[:, 0:G]), reads=['krb'], writes=['krT'])
        S.barrier()


class Rot:
    def __init__(self, bufs, prefix):
        self.bufs = bufs
        self.prefix = prefix
        self.i = 0

    def get(self):
        j = self.i % len(self.bufs)
        self.i += 1
        return self.bufs[j], '%s%d' % (self.prefix, j)


SCALE_ATT = 192.0 ** -0.5


def phase_mlaprep(K):
    nc, S = K.nc, K.S
    with ExitStack() as es:
        sb = lambda n, s, d: es.enter_context(nc.sbuf_tensor(_u(n), s, d))
        ps = lambda n, s, d: es.enter_context(nc.psum_tensor(_u(n), s, d))
        wuq = sb("wuq", [128, 2, 1024], BF16)
        wuk = sb("wuk", [128, 2, 512], BF16)
        wuv = sb("wuv", [128, 2, 512], BF16)
        wst = [sb("mwst%d" % i, [128, 2, 512], F32) for i in range(2)]
        gq = sb("gq", [128, 2], F32)
        gkv = sb("gkv", [128, 2], F32)
        onesf = sb("onesf", [128, 128], F32)
        epst = sb("epst", [128, 1], F32)
        ql = [sb("ql%d" % i, [128, 4, 512], F32) for i in range(2)]
        sq = sb("sq", [128, 4, 512], F32)
        lnt = sb("lnt", [128, 512], F32)
        rs = [sb("rs%d" % i, [128, 512], F32) for i in range(2)]
        nb = [sb("nb%d" % i, [128, 4, 512], BF16) for i in range(2)]
        qst = [sb("qst%d" % i, [128, 4, 512], BF16) for i in range(2)]
        kst = [sb("kst%d" % i, [128, 4, 512], BF16) for i in range(2)]
        qrb = [sb("qrb%d" % i, [64, 4, 512], BF16) for i in range(2)]
        vst = Rot([sb("vst%d" % i, [128, 512], BF16) for i in range(3)], "vst")
        cst = sb("cst", [64, 512], F32)
        snt = sb("snt", [64, 512], F32)
        r1 = sb("r1", [64, 512], F32)
        r2 = sb("r2", [64, 512], F32)
        pp = Rot([ps("mp%d" % i, [128, 512], F32) for i in range(7)], "mp")

        S.op('dve', lambda e: e.memset(epst[:], 1e-6), writes=['epst'])
        S.op('dve', lambda e: e.memset(onesf[:], 1.0), writes=['onesf'])
        S.dma('sp', lambda e: e.dma_start(out=gq[:], in_=K.mla_q_norm.rearrange("o (c p) -> p (o c)", p=128), allow_slow_non_contiguous=True), writes=['gq'])
        S.dma('sp', lambda e: e.dma_start(out=gkv[:], in_=K.mla_kv_norm.rearrange("o (c p) -> p (o c)", p=128), allow_slow_non_contiguous=True), writes=['gkv'])
        wl = 0
        for (src, dst, dk, n) in [(K.w_uq, wuq, 'wuq', 1024), (K.w_uk, wuk, 'wuk', 512), (K.w_uv, wuv, 'wuv', 512)]:
            for j in range(n // 512):
                w = wst[wl % 2]
                wk = 'mwst%d' % (wl % 2)
                wl += 1
                S.dma('sp', lambda e: e.dma_start(out=w[:], in_=src[:, j * 512:(j + 1) * 512].rearrange("(c p) n -> p c n", p=128)), writes=[wk])
                S.op('pool', lambda e: e.tensor_copy(out=dst[:, :, j * 512:(j + 1) * 512], in_=w[:]), reads=[wk], writes=[dk])

        groups = [(0, 256, 1)] + [(256 + g * 512, 512, 0) for g in range(16)]
        ev = [0]

        def evac(out_ap, in_ap, reads, writes):
            if ev[0] % 2 == 0:
                S.op('dve', lambda e: e.tensor_copy(out=out_ap, in_=in_ap), reads=reads, writes=writes)
            else:
                S.op('act', lambda e: e.activation(out=out_ap, in_=in_ap, func=AF.Identity), reads=reads, writes=writes)
            ev[0] += 1

        for gi, (t0, G, r) in enumerate(groups):
            q_ = ql[gi % 2]
            qk = 'ql%d' % (gi % 2)
            n_ = nb[gi % 2]
            nk = 'nb%d' % (gi % 2)
            S.dma('sp', lambda e: e.dma_start(out=q_[:, :, 0:G], in_=K.hT[1920:2432, t0:t0 + G].rearrange("(c p) t -> p c t", p=128)), reads=['hT'], writes=[qk])
            if r == 0:
                S.dma('sp', lambda e: e.dma_start(out=cst[:], in_=K.cosT[:, t0 - 256:t0 - 256 + 512]), writes=['cst'])
                S.dma('sp', lambda e: e.dma_start(out=snt[:], in_=K.sinT[:, t0 - 256:t0 - 256 + 512]), writes=['snt'])
            S.op('act', lambda e: e.activation(out=sq[:, :, 0:G], in_=q_[:, :, 0:G], func=AF.Square), reads=[qk], writes=['sq'])
            for pair in range(2):
                if pair == 0 and r == 1:
                    continue
                p, pk = pp.get()
                for c in range(2):
                    S.op('pe', lambda e: e.matmul(p[:, 0:G], lhsT=onesf[:], rhs=sq[:, pair * 2 + c, 0:G], start=(c == 0), stop=(c == 1)),
                         reads=['onesf', 'sq'], writes=[pk], acc=True)
                S.op('act', lambda e: e.activation(out=lnt[:, 0:G], in_=p[:, 0:G], func=AF.Ln, bias=epst[:], scale=1.0 / 256.0), reads=[pk, 'epst'], writes=['lnt'])
                S.op('act', lambda e: e.activation(out=rs[pair][:, 0:G], in_=lnt[:, 0:G], func=AF.Exp, scale=-0.5), reads=['lnt'], writes=['rs%d' % pair])
                g_ = gq if pair == 0 else gkv
                for c in range(2):
                    S.op('dve', lambda e: e.scalar_tensor_tensor(out=n_[:, pair * 2 + c, 0:G], in0=q_[:, pair * 2 + c, 0:G], scalar=g_[:, c:c + 1], in1=rs[pair][:, 0:G],
                                                                 op0=ALU.mult, op1=ALU.mult),
                         reads=[qk, 'rs%d' % pair, 'gq', 'gkv'], writes=[nk])
            if r == 0:
                tq = t0 - 256
                qs = qst[gi % 2]
                qsk = 'qst%d' % (gi % 2)
                qr_ = qrb[gi % 2]
                qrk = 'qrb%d' % (gi % 2)
                for h in range(4):
                    p, pk = pp.get()
                    for c in range(2):
                        S.op('pe', lambda e: e.matmul(p[:, 0:G], lhsT=wuq[:, c, h * 256:h * 256 + 128], rhs=n_[:, c, 0:G], start=(c == 0), stop=(c == 1)),
                             reads=['wuq', nk], writes=[pk], acc=True)
                    evac(qs[:, h, 0:G], p[:, 0:G], [pk], [qsk])
                    p1, pk1 = pp.get()
                    p2, pk2 = pp.get()
                    for c in range(2):
                        S.op('pe', lambda e: e.matmul(p1[0:64, 0:G], lhsT=wuq[:, c, h * 256 + 128:h * 256 + 192], rhs=n_[:, c, 0:G], start=(c == 0), stop=(c == 1)),
                             reads=['wuq', nk], writes=[pk1], acc=True)
                    for c in range(2):
                        S.op('pe', lambda e: e.matmul(p2[0:64, 0:G], lhsT=wuq[:, c, h * 256 + 192:h * 256 + 256], rhs=n_[:, c, 0:G], start=(c == 0), stop=(c == 1)),
                             reads=['wuq', nk], writes=[pk2], acc=True)
                    S.op('dve', lambda e: e.tensor_tensor(out=r1[:], in0=p1[0:64, :], in1=cst[:], op=ALU.mult), reads=[pk1, 'cst'], writes=['r1'])
                    S.op('dve', lambda e: e.tensor_tensor(out=r2[:], in0=p2[0:64, :], in1=snt[:], op=ALU.mult), reads=[pk2, 'snt'], writes=['r2'])
                    S.op('dve', lambda e: e.tensor_tensor(out=qr_[:, h, :], in0=r1[:], in1=r2[:], op=ALU.add), reads=['r1', 'r2'], writes=[qrk])
                S.dma('pool', lambda e: e.dma_start(out=K.qnT[:, tq:tq + G].rearrange("(h p) t -> p h t", p=128), in_=qs[:, :, 0:G]), reads=[qsk], writes=['qnT'])
                S.dma('pool', lambda e: e.dma_start(out=K.qrT[:, tq:tq + G].rearrange("(h p) t -> p h t", p=64), in_=qr_[:, :, 0:G]), reads=[qrk], writes=['qrT'])
            ks = kst[gi % 2]
            ksk = 'kst%d' % (gi % 2)
            for h in range(4):
                p, pk = pp.get()
                for c in range(2):
                    S.op('pe', lambda e: e.matmul(p[:, 0:G], lhsT=wuk[:, c, h * 128:(h + 1) * 128], rhs=n_[:, 2 + c, 0:G], start=(c == 0), stop=(c == 1)),
                         reads=['wuk', nk], writes=[pk], acc=True)
                evac(ks[:, h, 0:G], p[:, 0:G], [pk], [ksk])
            S.dma('pool', lambda e: e.dma_start(out=K.knT[:, t0:t0 + G].rearrange("(h p) t -> p h t", p=128), in_=ks[:, :, 0:G]), reads=[ksk], writes=['knT'])
            for i in range(G // 128):
                p, pk = pp.get()
                for c in range(2):
                    S.op('pe', lambda e: e.matmul(p[:, :], lhsT=n_[:, 2 + c, i * 128:(i + 1) * 128], rhs=wuv[:, c, :], start=(c == 0), stop=(c == 1)),
                         reads=['wuv', nk], writes=[pk], acc=True)
                v_, vk = vst.get()
                evac(v_[:], p[:], [pk], [vk])
                S.dma('pool', lambda e: e.dma_start(out=K.vtok[t0 + i * 128:t0 + (i + 1) * 128, :], in_=v_[:]), reads=[vk], writes=['vtok'])
        S.barrier()


def phase_attn(K, heads=(0, 1, 2, 3), nqt=16):
    nc, S = K.nc, K.S
    NKT = TA // 128
    with ExitStack() as es:
        sb = lambda n, s, d: es.enter_context(nc.sbuf_tensor(_u(n), s, d))
        ps = lambda n, s, d: es.enter_context(nc.psum_tensor(_u(n), s, d))
        krs = sb("krs", [64, TA], BF16)
        kn = [sb("kn%d" % i, [128, TA], BF16) for i in range(2)]
        vh = [sb("vh%d" % i, [128, NKT, 128], BF16) for i in range(2)]
        qn = [sb("qn%d" % i, [128, 512], BF16) for i in range(2)]
        qr = [sb("qr%d" % i, [64, 512], BF16) for i in range(2)]
        onesb = sb("onesb", [128, 128], BF16)
        pT = Rot([sb("pT%d" % i, [128, 512], BF16) for i in range(3)], "pT")
        rl = sb("rl", [128, 512], F32)
        ob = [sb("ob%d" % i, [128, 512], BF16) for i in range(2)]
        psc = Rot([ps("psc%d" % i, [128, 512], F32) for i in range(3)], "psc")
        pO = [ps("pO%d" % i, [128, 512], F32) for i in range(2)]
        pL = [ps("pL%d" % i, [128, 512], F32) for i in range(2)]

        S.op('dve', lambda e: e.memset(onesb[:], 1.0), writes=['onesb'])
        S.dma('sp', lambda e: e.dma_start(out=krs[:], in_=K.krT), reads=['krT'], writes=['krs'])
        qi = 0
        for hi, h in enumerate(heads):
            k_ = kn[hi % 2]
            kk = 'kn%d' % (hi % 2)
            v_ = vh[hi % 2]
            vk = 'vh%d' % (hi % 2)
            S.dma('sp', lambda e: e.dma_start(out=k_[:], in_=K.knT[h * 128:(h + 1) * 128, :]), reads=['knT'], writes=[kk])
            S.dma('sp', lambda e: e.dma_start(out=v_[:], in_=K.vtok[:, h * 128:(h + 1) * 128].rearrange("(kt p) d -> p kt d", p=128)), reads=['vtok'], writes=[vk])
            for qt in range(nqt):
                sl = qi % 2
                qi += 1
                qnk, qrk = 'qn%d' % sl, 'qr%d' % sl
                S.dma('sp', lambda e: e.dma_start(out=qn[sl][:], in_=K.qnT[h * 128:(h + 1) * 128, qt * 512:(qt + 1) * 512]), reads=['qnT'], writes=[qnk])
                S.dma('sp', lambda e: e.dma_start(out=qr[sl][:], in_=K.qrT[h * 64:(h + 1) * 64, qt * 512:(qt + 1) * 512]), reads=['qrT'], writes=[qrk])
                Ok, Lk = 'pO%d' % sl, 'pL%d' % sl
                pend = []

                def scores(kt):
                    p, pk = psc.get()
                    S.op('pe', lambda e: e.matmul(p[:], lhsT=k_[:, kt * 128:(kt + 1) * 128], rhs=qn[sl][:], start=True, stop=False),
                         reads=[kk, qnk], writes=[pk], acc=True)
                    S.op('pe', lambda e: e.matmul(p[:], lhsT=krs[:, kt * 128:(kt + 1) * 128], rhs=qr[sl][:], start=False, stop=True),
                         reads=['krs', qrk], writes=[pk], acc=True)
                    t_, tk = pT.get()
                    S.op('act', lambda e: e.activation(out=t_[:], in_=p[:], func=AF.Exp, scale=SCALE_ATT), reads=[pk], writes=[tk])
                    pend.append((kt, t_, tk))

                def pv():
                    kt, t_, tk = pend.pop(0)
                    S.op('pe', lambda e: e.matmul(pO[sl][:], lhsT=v_[:, kt, :], rhs=t_[:], start=(kt == 0), stop=(kt == NKT - 1)),
                         reads=[vk, tk], writes=[Ok], acc=True)
                    S.op('pe', lambda e: e.matmul(pL[sl][:], lhsT=onesb[:], rhs=t_[:], start=(kt == 0), stop=(kt == NKT - 1)),
                         reads=['onesb', tk], writes=[Lk], acc=True)

                scores(0)
                scores(1)
                for kt in range(NKT):
                    pv()
                    if kt + 2 < NKT:
                        scores(kt + 2)
                S.op('dve', lambda e: e.reciprocal(out=rl[:], in_=pL[sl][:]), reads=[Lk], writes=['rl'])
                S.op('dve', lambda e: e.tensor_tensor(out=ob[sl][:], in0=pO[sl][:], in1=rl[:], op=ALU.mult), reads=[Ok, 'rl'], writes=['ob%d' % sl])
                S.dma('pool', lambda e: e.dma_start(out=K.mixT[512 + h * 128:512 + (h + 1) * 128, qt * 512:(qt + 1) * 512], in_=ob[sl][:]), reads=['ob%d' % sl], writes=['mixT'])
        S.barrier()


F32R = mybir.dt.float32r
LDS = -0.6065306597126334


class LaneS:
    LOCAL = ('cv', 'tw', 'sg', 'kq', 'sq', 'lnt', 'kk_', 'av', 'tt', 'kd0', 'kd1', 'bb', 'gg', 'bc', 'bon')

    def __init__(self, S, lane):
        self.S = S
        self.lane = lane

    def k(self, x):
        if x == 'bones':
            return x
        for p in self.LOCAL:
            if x.startswith(p):
                return '%s@%d' % (x, self.lane)
        return x

    def op(self, e, fn, reads=(), writes=(), acc=False):
        return self.S.op(e, fn, [self.k(x) for x in reads], [self.k(x) for x in writes], acc)

    def dma(self, e, fn, reads=(), writes=()):
        return self.S.dma(e, fn, [self.k(x) for x in reads], [self.k(x) for x in writes])


def phase_rwprep(K):
    nc, S = K.nc, K.S
    with ExitStack() as es:
        sb = lambda n, s, d: es.enter_context(nc.sbuf_tensor(_u(n), s, d))
        ps = lambda n, s, d: es.enter_context(nc.psum_tensor(_u(n), s, d))
        G = 256
        cw = sb("cw", [128, 3, 12], F32)
        kkv = sb("kkv", [128, 4], F32)
        kav = sb("kav", [128, 4], F32)
        omka = sb("omka", [128, 4], F32)
        rkv = sb("rkv", [128, 4], F32)
        a0v = sb("a0v", [128, 2, 4], F32)
        w0r = sb("w0r", [1, 2, 512], F32)
        ones1 = sb("ones1", [1, 128], F32)
        wup = sb("wup", [64, 2, 512], F32)
        aup = sb("aup", [64, 2, 512], F32)
        gup = sb("gup", [128, 512], F32)
        bones = sb("bones", [128, 128], F32)
        idf = sb("idf", [128, 128], F32)
        eps12 = sb("eps12", [128, 1], F32)
        hr = [sb("hr%d" % i, [128, 12, G + 2], F32) for i in range(2)]
        lo = [sb("lo%d" % i, [64, 4, G], F32) for i in range(2)]
        gd = [sb("gd%d" % i, [128, G], F32) for i in range(2)]
        cvL = [sb("cv_%d" % i_, [128, 12, G], F32) for i_ in range(2)]
        twL = [sb("tw_%d" % i_, [64, 2, G], F32) for i_ in range(2)]
        sgL = [sb("sg_%d" % i_, [128, G], F32) for i_ in range(2)]
        kqL = [sb("kq_%d" % i_, [128, 4, G], F32) for i_ in range(2)]
        sqL = [sb("sq_%d" % i_, [128, 4, G], F32) for i_ in range(2)]
        lntL = [sb("lnt_%d" % i_, [128, 4, G], F32) for i_ in range(2)]
        kk_L = [sb("kk__%d" % i_, [128, 4, G], F32) for i_ in range(2)]
        avL = [sb("av_%d" % i_, [128, 4, G], F32) for i_ in range(2)]
        ttL = [sb("tt_%d" % i_, [128, 4, G], F32) for i_ in range(2)]
        kdL = [[sb("kd%d_%d" % (i, i_), [128, 4, G], F32) for i in range(2)] for i_ in range(2)]
        bbL = [sb("bb_%d" % i_, [128, 4, G], F32) for i_ in range(2)]
        ld = Rot([sb("ld%d" % i, [128, 512], F32) for i in range(2)], "ld")
        vt = Rot([sb("vt%d" % i, [128, 512], F32) for i in range(2)], "vt")
        ggL = [sb("gg_%d" % i_, [128, 4, G], F32) for i_ in range(2)]
        bcL = [sb("bc_%d" % i_, [128, 4, G], F32) for i_ in range(2)]
        bonL = [sb("bon_%d" % i_, [128, 4, G], F32) for i_ in range(2)]
        pp = Rot([ps("rp%d" % i, [128, 512], F32) for i in range(7)], "rp")

        ld1 = lambda dst, src, key: S.dma('sp', lambda e: e.dma_start(out=dst, in_=src, allow_slow_non_contiguous=True), writes=[key])
        ld1(cw[:], K.rwkv_conv.rearrange("t (c p) -> p t c", p=128), 'cw')
        ld1(kkv[:], K.rwkv_k_k.rearrange("o (c p) -> p (o c)", p=128), 'kkv')
        ld1(kav[:], K.rwkv_k_a.rearrange("o (c p) -> p (o c)", p=128), 'kav')
        ld1(rkv[:], K.rwkv_r_k.rearrange("o (c p) -> p (o c)", p=128), 'rkv')
        ld1(a0v[:], K.rwkv_a0.rearrange("d (c p) -> p d c", p=128), 'a0v')
        ld1(w0r[:], K.rwkv_w0.rearrange("(o d) n -> o d n", o=1), 'w0r')
        ld1(wup[:], K.rwkv_w_up.rearrange("d l n -> l d n"), 'wup')
        ld1(aup[:], K.rwkv_a_up.rearrange("d l n -> l d n"), 'aup')
        ld1(gup[:], K.rwkv_g_up, 'gup')
        ld1(bones[:], K.bones, 'bones')
        ld1(idf[:], K.identf, 'idf')
        S.op('dve', lambda e: e.memset(ones1[:], 1.0), writes=['ones1'])
        S.op('dve', lambda e: e.memset(eps12[:], 1e-12), writes=['eps12'])
        S.op('dve', lambda e: e.tensor_scalar(out=omka[:], in0=kav[:], scalar1=-1.0, scalar2=1.0, op0=ALU.mult, op1=ALU.add), reads=['kav'], writes=['omka'])

        nblk = TA // G
        def bbody(bi):
            ln = bi % 2
            S_ = LaneS(S, ln)
            cv, tw, sg, kq, sq, lnt, kk_, av, tt, bb, gg, bc, bon = cvL[ln], twL[ln], sgL[ln], kqL[ln], sqL[ln], lntL[ln], kk_L[ln], avL[ln], ttL[ln], bbL[ln], ggL[ln], bcL[ln], bonL[ln]
            kd = kdL[ln]
            t0 = bi * G
            lat = t0 >= TC
            sl = bi % 2
            h_ = hr[sl]
            hk = 'hr%d' % sl
            first = (t0 == 0 or t0 == TC)
            last = (t0 + G == TC or t0 + G == TA)
            c0 = 1 if first else 0
            c1 = G + 1 if last else G + 2
            if first:
                S_.op('pool', lambda e: e.memset(h_[:, :, 0:1], 0.0), writes=[hk])
            if last:
                S_.op('pool', lambda e: e.memset(h_[:, :, G + 1:G + 2], 0.0), writes=[hk])
            for q in range(3):
                S_.dma('sp', lambda e: e.dma_start(out=h_[:, q * 4:(q + 1) * 4, c0:c1], in_=K.hT[q * 512:(q + 1) * 512, t0 - 1 + c0:t0 - 1 + c1].rearrange("(c p) t -> p c t", p=128)),
                      reads=['hT'], writes=[hk])
            lo_ = lo[sl]
            lk = 'lo%d' % sl
            S_.dma('sp', lambda e: e.dma_start(out=lo_[:], in_=K.hT[1536:1792, t0:t0 + G].rearrange("(c p) t -> p c t", p=64)), reads=['hT'], writes=[lk])
            gd_ = gd[sl]
            gk = 'gd%d' % sl
            if lat:
                S_.dma('sp', lambda e: e.dma_start(out=gd_[:], in_=K.hT[1792:1920, t0:t0 + G]), reads=['hT'], writes=[gk])
            for c in range(12):
                S_.op('act', lambda e: e.activation(out=cv[:, c, :], in_=h_[:, c, 1:G + 1], func=AF.Identity, scale=cw[:, 1, c:c + 1]), reads=[hk, 'cw'], writes=['cv%d' % c])
                S_.op('dve', lambda e: e.scalar_tensor_tensor(out=cv[:, c, :], in0=h_[:, c, 0:G], scalar=cw[:, 0, c:c + 1], in1=cv[:, c, :], op0=ALU.mult, op1=ALU.add),
                     reads=[hk, 'cw', 'cv%d' % c], writes=['cv%d' % c])
                S_.op('dve', lambda e: e.scalar_tensor_tensor(out=cv[:, c, :], in0=h_[:, c, 2:G + 2], scalar=cw[:, 2, c:c + 1], in1=cv[:, c, :], op0=ALU.mult, op1=ALU.add),
                     reads=[hk, 'cw', 'cv%d' % c], writes=['cv%d' % c])
            cvr = ['cv%d' % c for c in range(0, 4)]
            cvk = ['cv%d' % c for c in range(4, 8)]
            cvv = ['cv%d' % c for c in range(8, 12)]
            S_.dma('pool', lambda e: e.dma_start(out=K.rwR[:, t0:t0 + G].rearrange("(c p) t -> p c t", p=128), in_=cv[:, 0:4, :]), reads=cvr, writes=['rwR'])
            S_.dma('pool', lambda e: e.dma_start(out=K.rwV[:, t0:t0 + G].rearrange("(c p) t -> p c t", p=128), in_=cv[:, 8:12, :]), reads=cvv, writes=['rwV'])
            yield 'f'
            for i in range(G // 128):
                p, pk = pp.get()
                for c in range(4):
                    S_.op('pe', lambda e: e.transpose(out=p[:, c * 128:(c + 1) * 128], in_=cv[:, 8 + c, i * 128:(i + 1) * 128], identity=idf[:]), reads=cvv + ['idf'], writes=[pk], acc=True)
                v_, vk = vt.get()
                S_.op('act', lambda e: e.activation(out=v_[:], in_=p[:], func=AF.Identity), reads=[pk], writes=[vk])
                S_.dma('pool', lambda e: e.dma_start(out=K.rwVtok[t0 + i * 128:t0 + (i + 1) * 128, :], in_=v_[:]), reads=[vk], writes=['rwVtok'])
                yield 'f'
            for c in range(4):
                S_.op('dve', lambda e: e.tensor_scalar_mul(out=kq[:, c, :], in0=cv[:, 4 + c, :], scalar1=kkv[:, c:c + 1]), reads=cvk + ['kkv'], writes=['kq'])
            S_.op('act', lambda e: e.activation(out=sq[:], in_=kq[:], func=AF.Square), reads=['kq'], writes=['sq'])
            for c2 in range(2):
                p, pk = pp.get()
                S_.op('pe', lambda e: e.matmul(p[:, 0:2 * G], lhsT=bones[:], rhs=sq[:, 2 * c2:2 * c2 + 2, :], start=True, stop=True), reads=['bones', 'sq'], writes=[pk])
                S_.op('act', lambda e: e.activation(out=lnt[:, 2 * c2:2 * c2 + 2, :], in_=p[:, 0:2 * G], func=AF.Ln, bias=eps12[:], scale=1.0), reads=[pk, 'eps12'], writes=['lnt'])
            S_.op('act', lambda e: e.activation(out=lnt[:], in_=lnt[:], func=AF.Exp, scale=-0.5), reads=['lnt'], writes=['lnt'])
            S_.op('dve', lambda e: e.tensor_tensor(out=kk_[:], in0=kq[:], in1=lnt[:], op=ALU.mult), reads=['kq', 'lnt'], writes=['kk_'])
            S_.dma('pool', lambda e: e.dma_start(out=K.rwKK[:, t0:t0 + G].rearrange("(c p) t -> p c t", p=128), in_=kk_[:]), reads=['kk_'], writes=['rwKK'])
            yield 'F'
            S_.op('act', lambda e: e.activation(out=tw[:], in_=lo_[:, 0:2, :], func=AF.Tanh), reads=[lk], writes=['tw'])
            for d in range(2):
                for i in range(G // 128):
                    p, pk = pp.get()
                    S_.op('pe', lambda e: e.matmul(p[:], lhsT=tw[:, d, i * 128:(i + 1) * 128], rhs=wup[:, d, :], start=True, stop=False), reads=['tw', 'wup'], writes=[pk], acc=True)
                    S_.op('pe', lambda e: e.matmul(p[:], lhsT=ones1[:], rhs=w0r[:, d, :], start=False, stop=True), reads=['ones1', 'w0r'], writes=[pk], acc=True)
                    l_, lk2 = ld.get()
                    S_.op('act', lambda e: e.activation(out=l_[:], in_=p[:], func=AF.Sigmoid), reads=[pk], writes=[lk2])
                    S_.dma('pool', lambda e: e.dma_start(out=K.rwLD[d][t0 + i * 128:t0 + (i + 1) * 128, :], in_=l_[:]), reads=[lk2], writes=['rwLD%d' % d])
                    yield 'b'
                for c in range(4):
                    p, pk = pp.get()
                    S_.op('pe', lambda e: e.matmul(p[:, 0:G], lhsT=aup[:, d, c * 128:(c + 1) * 128], rhs=lo_[:, 2 + d, :], start=True, stop=True), reads=['aup', lk], writes=[pk])
                    S_.op('act', lambda e: e.activation(out=av[:, c, :], in_=p[:, 0:G], func=AF.Sigmoid, bias=a0v[:, d, c:c + 1], scale=1.0), reads=[pk, 'a0v'], writes=['av'])
                    S_.op('dve', lambda e: e.tensor_scalar(out=tt[:, c, :], in0=av[:, c, :], scalar1=kav[:, c:c + 1], scalar2=omka[:, c:c + 1], op0=ALU.mult, op1=ALU.add),
                         reads=['av', 'kav', 'omka'], writes=['tt'])
                S_.op('dve', lambda e: e.tensor_tensor(out=kd[d][:], in0=cv[:, 4:8, :], in1=tt[:], op=ALU.mult), reads=cvk + ['tt'], writes=['kd%d' % d])
                S_.op('dve', lambda e: e.tensor_tensor(out=bb[:], in0=kk_[:], in1=av[:], op=ALU.mult), reads=['kk_', 'av'], writes=['bb'])
                S_.dma('pool', lambda e: e.dma_start(out=K.rwKD[d][:, t0:t0 + G].rearrange("(c p) t -> p c t", p=128), in_=kd[d][:]), reads=['kd%d' % d], writes=['rwKD%d' % d])
                S_.dma('pool', lambda e: e.dma_start(out=K.rwB[d][:, t0:t0 + G].rearrange("(c p) t -> p c t", p=128), in_=bb[:]), reads=['bb'], writes=['rwB%d' % d])
                yield 'b'
            if lat:
                tl = t0 - TC
                S_.op('act', lambda e: e.activation(out=sg[:], in_=gd_[:], func=AF.Sigmoid), reads=[gk], writes=['sg'])
                for c in range(4):
                    p, pk = pp.get()
                    S_.op('pe', lambda e: e.matmul(p[:, 0:G], lhsT=gup[:, c * 128:(c + 1) * 128], rhs=sg[:], start=True, stop=True), reads=['gup', 'sg'], writes=[pk])
                    S_.op('act', lambda e: e.activation(out=gg[:, c, :], in_=p[:, 0:G], func=AF.Identity), reads=[pk], writes=['gg'])
                S_.dma('pool', lambda e: e.dma_start(out=K.rwG[:, tl:tl + G].rearrange("(c p) t -> p c t", p=128), in_=gg[:]), reads=['gg'], writes=['rwG'])
                yield 'b'
                S_.op('dve', lambda e: e.tensor_tensor(out=bc[:], in0=kd[0][:], in1=kd[1][:], op=ALU.add), reads=['kd0', 'kd1'], writes=['bc'])
                S_.op('dve', lambda e: e.tensor_tensor(out=bc[:], in0=bc[:], in1=cv[:, 0:4, :], op=ALU.mult), reads=['bc'] + cvr, writes=['bc'])
                for c in range(4):
                    S_.op('dve', lambda e: e.tensor_scalar_mul(out=bc[:, c, :], in0=bc[:, c, :], scalar1=rkv[:, c:c + 1]), reads=['bc', 'rkv'], writes=['bc'])
                for c2 in range(2):
                    p, pk = pp.get()
                    S_.op('pe', lambda e: e.matmul(p[:, 0:2 * G], lhsT=bones[:], rhs=bc[:, 2 * c2:2 * c2 + 2, :], start=True, stop=True), reads=['bones', 'bc'], writes=[pk])
                    S_.op('dve', lambda e: e.tensor_tensor(out=bon[:, 2 * c2:2 * c2 + 2, :], in0=p[:, 0:2 * G], in1=cv[:, 8 + 2 * c2:8 + 2 * c2 + 2, :], op=ALU.mult), reads=[pk] + cvv, writes=['bon'])
                S_.dma('pool', lambda e: e.dma_start(out=K.rwBON[:, tl:tl + G].rearrange("(c p) t -> p c t", p=128), in_=bon[:]), reads=['bon'], writes=['rwBON'])
        def step(gen_):
            try:
                return next(gen_)
            except StopIteration:
                return None
        prev = None
        for bi in range(nblk):
            gcur = bbody(bi)
            while True:
                r = step(gcur)
                if prev is not None and step(prev) is None:
                    prev = None
                if r == 'F' or r is None:
                    break
            while prev is not None:
                if step(prev) is None:
                    prev = None
            prev = gcur
        while prev is not None:
            if step(prev) is None:
                prev = None
        S.barrier()


def rw_consts():
    i = np.arange(128)[:, None]
    t = np.arange(128)[None, :]
    bd = (i // 64) == (t // 64)
    out = {}
    blk = ((i // 64) == (t // 64)).astype(np.float32)
    for d in range(2):
        strict = (bd & ((i < t) if d == 0 else (i > t))).astype(np.float32)
        incl = (bd & ((i <= t) if d == 0 else (i >= t))).astype(np.float32)
        m1 = np.concatenate([-strict, incl, blk], 1)
        m2 = np.concatenate([incl, blk], 1)
        m3 = np.concatenate([-strict.T, -strict.T, -np.ones((128, 64), np.float32)], 1)
        cum = np.float32(LDS) * np.concatenate([incl, strict, strict.T], 1)
        out['rwm%d' % d] = np.ascontiguousarray(np.concatenate([m1, m2, m3, cum], 1).astype(np.float32))
    out['i64dbl'] = np.ascontiguousarray((np.arange(128)[:, None] % 64 == np.arange(128)[None, :] % 64).astype(np.float32))
    out['bones'] = np.ascontiguousarray(bd.astype(np.float32))
    out['identf'] = np.eye(128, dtype=np.float32)
    return out


GN_EPS = 64e-5


def phase_rwscan(K, ntile_lat=64):
    nc, S = K.nc, K.S
    G = 128
    with ExitStack() as es:
        sb = lambda n, s, d: es.enter_context(nc.sbuf_tensor(_u(n), s, d))
        ps = lambda n, s, d: es.enter_context(nc.psum_tensor(_u(n), s, d))
        mk = sb("mk", [128, 1344], F32)
        idr = sb("idr", [128, 128], F32R)
        i64r = sb("i64r", [128, 128], F32R)
        i64f = sb("i64f", [128, 128], F32)
        idf = sb("idf", [128, 128], F32)
        o64 = sb("o64", [64, 64], F32)
        gng = sb("gng", [64, 8], F32)
        gnb = sb("gnb", [64, 8], F32)
        epsg = sb("epsg", [64, 1], F32)
        raw = [[sb("raw%d_%d" % (j, i), [128, 4, G], F32) for i in range(2)] for j in range(4)]
        vraw = [sb("vraw%d" % i, [128, 512], F32) for i in range(2)]
        ldt = [sb("ldt%d" % i, [128, 512], F32) for i in range(2)]
        vtr = [sb("vtr%d" % i, [128, 512], F32R) for i in range(2)]
        Ep = sb("Ep", [128, 4, G], F32)
        Em = sb("Em", [128, 4, G], F32)
        Ex = sb("Ex", [128, 4, G], F32)
        Ea = sb("Ea", [128, 4, G], F32)
        KRG = sb("KRG", [128, 4, 3, 128], F32R)
        BK = sb("BK", [128, 4, 2, 128], F32R)
        KB2 = sb("KB2", [128, 4, 2, 128], F32R)
        NS = 4
        La = [sb("La%d" % i, [128, 384], F32R) for i in range(NS)]
        Lb = [sb("Lb%d" % i, [128, 384], F32R) for i in range(NS)]
        ATa = [sb("ATa%d" % i, [128, 128], F32R) for i in range(NS)]
        ATb = [sb("ATb%d" % i, [128, 128], F32R) for i in range(NS)]
        X1 = [sb("X1%d" % i, [128, 320], F32R) for i in range(NS)]
        X2 = [sb("X2%d" % i, [128, 256], F32R) for i in range(NS)]
        Xf = [sb("Xf%d" % i, [128, 256], F32R) for i in range(NS)]
        QG1 = [sb("QG1_%d" % i, [64, 8, 256], F32R) for i in range(2)]
        QG2 = [sb("QG2_%d" % i, [128, 8, 256], F32R) for i in range(2)]
        Sth = sb("Sth", [64, 3, 8, 64], F32R)
        T = [sb("T%d" % i, [64, 8, G], F32) for i in range(4)]
        outb = sb("outb", [64, 8, G], BF16)
        pp = Rot([ps("sp%d" % i, [128, 512], F32) for i in range(7)], "sp")
        pst = ps("pst", [64, 512], F32)

        ld1 = lambda dst, src, key: S.dma('sp', lambda e: e.dma_start(out=dst, in_=src, allow_slow_non_contiguous=True), writes=[key])
        ld1(idf[:], K.identf, 'idf')
        ld1(i64f[:], K.i64dbl, 'i64f')
        ld1(gng[:], K.rwkv_gn_g.rearrange("o (h p) -> p (o h)", p=64), 'gng')
        ld1(gnb[:], K.rwkv_gn_b.rearrange("o (h p) -> p (o h)", p=64), 'gnb')
        S.op('dve', lambda e: e.tensor_copy(out=idr[:], in_=idf[:]), reads=['idf'], writes=['idr'])
        S.op('dve', lambda e: e.tensor_copy(out=i64r[:], in_=i64f[:]), reads=['i64f'], writes=['i64r'])
        S.op('dve', lambda e: e.memset(o64[:], 1.0 / 64.0), writes=['o64'])
        S.op('dve', lambda e: e.memset(epsg[:], GN_EPS), writes=['epsg'])
        zt = sb("zt", [64, 512], F32)
        S.op('dve', lambda e: e.memset(zt[:], 0.0), writes=['zt'])

        evq = [0]

        def evac(out_ap, in_ap, reads, writes):
            if evq[0] % 2 == 0:
                S.op('act', lambda e: e.activation(out=out_ap, in_=in_ap, func=AF.Identity), reads=reads, writes=writes)
            else:
                S.op('dve', lambda e: e.tensor_copy(out=out_ap, in_=in_ap), reads=reads, writes=writes)
            evq[0] += 1

        fl = lambda a: a[:, :, :].rearrange("p h t -> p (h t)")
        bcount = 0
        for d in range(2):
            m1 = mk[:, 0:384]
            m2 = mk[:, 384:640]
            m3 = mk[:, 640:960]
            cum = mk[:, 960:1344]
            S.dma('sp', lambda e: e.dma_start(out=mk[:], in_=K.rwm[d]), writes=['mk'])
            S.op('dve', lambda e: e.tensor_copy(out=Sth[:, 0].rearrange("p h v -> p (h v)"), in_=zt[:]), reads=['zt'], writes=['Sth0'])
            if d == 0:
                blocks = [0, 1] + list(range(2, 2 + ntile_lat))
            else:
                blocks = [1, 0] + list(range(1 + ntile_lat, 1, -1))
            chunks = [0, 1] if d == 0 else [1, 0]
            def body(b, qs):
                nonlocal bcount
                t0 = b * G
                lat = b >= 2
                sl = bcount % 2
                bcount += 1
                srcs = [K.rwR, K.rwKD[d], K.rwKK, K.rwB[d]]
                rk = ['raw%d_%d' % (j, sl) for j in range(4)]
                for j in range(4):
                    S.dma('sp', lambda e: e.dma_start(out=raw[j][sl][:], in_=srcs[j][:, t0:t0 + G].rearrange("(c p) t -> p c t", p=128)), writes=[rk[j]])
                S.dma('sp', lambda e: e.dma_start(out=vraw[sl][:], in_=K.rwVtok[t0:t0 + G, :]), writes=['vraw%d' % sl])
                S.dma('sp', lambda e: e.dma_start(out=ldt[sl][:], in_=K.rwLD[d][t0:t0 + G, :]), writes=['ldt%d' % sl])
                r_, kd_, kk_, b_ = [raw[j][sl] for j in range(4)]
                S.op('act', lambda e: e.activation(out=vtr[qs][:], in_=vraw[sl][:], func=AF.Identity), reads=['vraw%d' % sl], writes=['vtr%d' % qs])
                banks = [pp.get() for _ in range(3)]
                for c in range(4):
                    for q in range(3):
                        S.op('pe', lambda e: e.matmul(banks[q][0][:, c * 128:(c + 1) * 128], lhsT=ldt[sl][:, c * 128:(c + 1) * 128], rhs=cum[:, q * 128:(q + 1) * 128], start=True, stop=True),
                             reads=['ldt%d' % sl, 'mk'], writes=[banks[q][1]], acc=True)
                v3 = lambda bank: bank[:, :].rearrange("p (c t) -> p c t", c=4)
                S.op('act', lambda e: e.activation(out=Ep[:], in_=v3(banks[0][0]), func=AF.Exp), reads=[banks[0][1]], writes=['Ep'])
                S.op('act', lambda e: e.activation(out=Em[:], in_=v3(banks[0][0]), func=AF.Exp, scale=-1.0), reads=[banks[0][1]], writes=['Em'])
                S.op('act', lambda e: e.activation(out=Ex[:], in_=v3(banks[1][0]), func=AF.Exp), reads=[banks[1][1]], writes=['Ex'])
                S.op('act', lambda e: e.activation(out=Ea[:], in_=v3(banks[2][0]), func=AF.Exp), reads=[banks[2][1]], writes=['Ea'])
                yield 'f'
                S.op('dve', lambda e: e.tensor_tensor(out=KRG[:, :, 0, :], in0=kk_[:], in1=Ex[:], op=ALU.mult), reads=[rk[2], 'Ex'], writes=['KRG'])
                S.op('dve', lambda e: e.tensor_tensor(out=KRG[:, :, 1, :], in0=r_[:], in1=Ep[:], op=ALU.mult), reads=[rk[0], 'Ep'], writes=['KRG'])
                S.op('dve', lambda e: e.tensor_tensor(out=BK[:, :, 0, :], in0=b_[:], in1=Em[:], op=ALU.mult), reads=[rk[3], 'Em'], writes=['BK'])
                S.op('dve', lambda e: e.tensor_tensor(out=BK[:, :, 1, :], in0=kd_[:], in1=Em[:], op=ALU.mult), reads=[rk[1], 'Em'], writes=['BK'])
                S.op('dve', lambda e: e.tensor_tensor(out=KB2[:, :, 0, :], in0=kd_[:], in1=Ea[:], op=ALU.mult), reads=[rk[1], 'Ea'], writes=['KB2'])
                S.op('dve', lambda e: e.tensor_tensor(out=KB2[:, :, 1, :], in0=b_[:], in1=Ea[:], op=ALU.mult), reads=[rk[3], 'Ea'], writes=['KB2'])
                for cc in range(2):
                    pos = cc * 64 + (63 if d == 0 else 0)
                    in0 = i64f[:, cc * 64:(cc + 1) * 64].unsqueeze(1).to_broadcast([128, 4, 64])
                    in1 = Ep[:, :, pos:pos + 1].to_broadcast([128, 4, 64])
                    S.op('dve', lambda e: e.tensor_tensor(out=KRG[:, :, 2, cc * 64:(cc + 1) * 64], in0=in0, in1=in1, op=ALU.mult), reads=['i64f', 'Ep'], writes=['KRG'])

                for g0 in range(0, 8, NS):
                    grp = list(range(g0, g0 + NS))
                    cur = {}
                    for s_, h in enumerate(grp):
                        c, pb = h // 2, 64 * (h % 2)
                        fm = lambda arr, k0, k1: arr[pb:pb + 64, c, k0:k1, :].rearrange("p k t -> p (k t)")
                        p1, k1 = pp.get()
                        S.op('pe', lambda e: e.matmul(p1[:, 0:256], lhsT=fm(BK, 0, 1), rhs=fm(KRG, 0, 2), start=True, stop=True), reads=['BK', 'KRG'], writes=[k1], acc=True)
                        S.op('pe', lambda e: e.matmul(p1[:, 256:384], lhsT=fm(KB2, 1, 2), rhs=i64r[pb:pb + 64, :], start=True, stop=True), reads=['KB2', 'i64r'], writes=[k1], acc=True)
                        S.op('dve', lambda e: e.tensor_tensor(out=La[s_][:], in0=p1[:, 0:384], in1=m1, op=ALU.mult), reads=[k1, 'mk'], writes=['La%d' % s_])
                        p2, k2 = pp.get()
                        S.op('pe', lambda e: e.matmul(p2[:, 0:128], lhsT=fm(BK, 1, 2), rhs=fm(KRG, 1, 2), start=True, stop=True), reads=['BK', 'KRG'], writes=[k2], acc=True)
                        S.op('pe', lambda e: e.matmul(p2[:, 128:256], lhsT=fm(KB2, 0, 1), rhs=i64r[pb:pb + 64, :], start=True, stop=True), reads=['KB2', 'i64r'], writes=[k2], acc=True)
                        S.op('dve', lambda e: e.tensor_tensor(out=X2[s_][:], in0=p2[:, 0:256], in1=m2, op=ALU.mult), reads=[k2, 'mk'], writes=['X2%d' % s_])
                        p3, k3 = pp.get()
                        S.op('pe', lambda e: e.matmul(p3[:, 0:256], lhsT=fm(KRG, 0, 1), rhs=fm(BK, 0, 2), start=True, stop=True), reads=['BK', 'KRG'], writes=[k3], acc=True)
                        S.op('pe', lambda e: e.matmul(p3[:, 256:320], lhsT=fm(KRG, 0, 1), rhs=i64r[pb:pb + 64, 0:64], start=True, stop=True), reads=['KRG', 'i64r'], writes=[k3], acc=True)
                        S.op('dve', lambda e: e.tensor_tensor(out=X1[s_][:], in0=p3[:, 0:320], in1=m3, op=ALU.mult), reads=[k3, 'mk'], writes=['X1%d' % s_])
                        cur[s_] = (La[s_], 'La%d' % s_, X1[s_][:, 0:128], 'X1%d' % s_)
                        yield 'f'
                    for lev in range(5):
                        pend = []
                        nxt = {}
                        for s_ in range(NS):
                            L, Lk, AT, ATk = cur[s_]
                            p, pk = pp.get()
                            S.op('pe', lambda e: e.matmul(p[:, 0:384], lhsT=AT, rhs=L[:, 0:384], start=True, stop=False), reads=[Lk, ATk], writes=[pk], acc=True)
                            S.op('pe', lambda e: e.matmul(p[:, 128:384], lhsT=idr[:], rhs=L[:, 128:384], start=False, stop=True), reads=[Lk, 'idr'], writes=[pk], acc=True)
                            pend.append((p, pk))
                        yield 'f'
                        for s_ in range(NS):
                            p, pk = pend[s_]
                            Ln_ = Lb[s_] if lev % 2 == 0 else La[s_]
                            Lnk = ('Lb%d' if lev % 2 == 0 else 'La%d') % s_
                            evac(Ln_[:], p[:, 0:384], [pk], [Lnk])
                            nxt[s_] = (Ln_, Lnk)
                        for s_ in range(NS):
                            Ln_, Lnk = nxt[s_]
                            p, pk = pend[s_]
                            S.op('pe', lambda e: e.transpose(out=p[:, 384:512], in_=Ln_[:, 0:128].bitcast(F32), identity=idf[:]), reads=[Lnk, 'idf'], writes=[pk])
                        yield 'f'
                        for s_ in range(NS):
                            Ln_, Lnk = nxt[s_]
                            p, pk = pend[s_]
                            ATn = ATa[s_] if lev % 2 == 0 else ATb[s_]
                            ATnk = ('ATa%d' if lev % 2 == 0 else 'ATb%d') % s_
                            evac(ATn[:], p[:, 384:512], [pk], [ATnk])
                            cur[s_] = (Ln_, Lnk, ATn[:], ATnk)
                    for s_, h in enumerate(grp):
                        L, Lk, AT, ATk = cur[s_]
                        p, pk = pp.get()
                        S.op('pe', lambda e: e.matmul(p[:, 0:256], lhsT=AT, rhs=L[:, 128:384], start=True, stop=False), reads=[Lk, ATk], writes=[pk], acc=True)
                        S.op('pe', lambda e: e.matmul(p[:, 0:256], lhsT=idr[:], rhs=L[:, 128:384], start=False, stop=True), reads=[Lk, 'idr'], writes=[pk], acc=True)
                        evac(Xf[s_][:], p[:, 0:256], [pk], ['Xf%d' % s_])
                    yield 'f'
                    for s_, h in enumerate(grp):
                        c, pb = h // 2, 64 * (h % 2)
                        p, pk = pp.get()
                        S.op('pe', lambda e: e.matmul(p[:, 0:256], lhsT=idr[:], rhs=X2[s_][:], start=True, stop=False), reads=['X2%d' % s_, 'idr'], writes=[pk], acc=True)
                        S.op('pe', lambda e: e.matmul(p[:, 0:256], lhsT=X1[s_][:, 128:256], rhs=Xf[s_][:], start=False, stop=True), reads=['X1%d' % s_, 'Xf%d' % s_], writes=[pk], acc=True)
                        evac(QG2[qs][:, h, :], p[:, 0:256], [pk], ['QG2_%d_%d' % (qs, h)])
                        q, qk = pp.get()
                        rg = KRG[pb:pb + 64, c, 1:3, :].rearrange("p k t -> p (k t)")
                        S.op('pe', lambda e: e.matmul(q[0:64, 0:256], lhsT=idr[pb:pb + 64, pb:pb + 64], rhs=rg, start=True, stop=False), reads=['KRG', 'idr'], writes=[qk], acc=True)
                        S.op('pe', lambda e: e.matmul(q[0:64, 0:256], lhsT=X1[s_][:, 256:320], rhs=Xf[s_][:], start=False, stop=True), reads=['X1%d' % s_, 'Xf%d' % s_], writes=[qk], acc=True)
                        evac(QG1[qs][:, h, :], q[0:64, 0:256], [qk], ['QG1_%d_%d' % (qs, h)])
                        yield 'f'
                yield 'F'
                for s, cc in enumerate(chunks):
                    for h in range(8):
                        S.op('pe', lambda e: e.matmul(pst[:, h * 64:(h + 1) * 64], lhsT=QG1[qs][:, h, 128 + cc * 64:128 + (cc + 1) * 64], rhs=Sth[:, s, h, :], start=True, stop=False),
                             reads=['QG1_%d_%d' % (qs, h), 'Sth%d' % s], writes=['pst'], acc=True)
                        S.op('pe', lambda e: e.matmul(pst[:, h * 64:(h + 1) * 64], lhsT=QG2[qs][:, h, 128 + cc * 64:128 + (cc + 1) * 64], rhs=vtr[qs][:, h * 64:(h + 1) * 64], start=False, stop=True),
                             reads=['QG2_%d_%d' % (qs, h), 'vtr%d' % qs], writes=['pst'], acc=True)
                    evac(Sth[:, s + 1].rearrange("p h v -> p (h v)"), pst[:, :], ['pst'], ['Sth%d' % (s + 1)])
                    yield 'b'
                if lat:
                    ybuf = T[0]
                    for hq in range(2):
                        bank = pp.get()
                        for h4 in range(4):
                            h = hq * 4 + h4
                            S.op('pe', lambda e: e.matmul(bank[0][0:64, h4 * 128:(h4 + 1) * 128], lhsT=vtr[qs][:, h * 64:(h + 1) * 64], rhs=QG2[qs][:, h, 0:128], start=True, stop=False),
                                 reads=['QG2_%d_%d' % (qs, h), 'vtr%d' % qs], writes=[bank[1]], acc=True)
                            for s, cc in enumerate(chunks):
                                S.op('pe', lambda e: e.matmul(bank[0][0:64, h4 * 128 + cc * 64:h4 * 128 + (cc + 1) * 64], lhsT=Sth[:, s, h, :], rhs=QG1[qs][:, h, cc * 64:(cc + 1) * 64], start=False, stop=(s == 1)),
                                     reads=['QG1_%d_%d' % (qs, h), 'Sth%d' % s], writes=[bank[1]], acc=True)
                        evac(ybuf[:, hq * 4:hq * 4 + 4, :], bank[0][0:64, :].rearrange("p (h t) -> p h t", h=4), [bank[1]], ['T0'])
                        yield 'b'
                    tl = t0 - TC
                    if d == 0:
                        S.dma('pool', lambda e: e.dma_start(out=K.rwY0[:, tl:tl + G].rearrange("(h p) t -> p h t", p=64), in_=ybuf[:]), reads=['T0'], writes=['rwY0'])
                    else:
                        y0b, cen, sqb = T[1], T[2], T[3]
                        S.dma('sp', lambda e: e.dma_start(out=y0b[:], in_=K.rwY0[:, tl:tl + G].rearrange("(h p) t -> p h t", p=64)), reads=['rwY0'], writes=['T1'])
                        S.op('dve', lambda e: e.tensor_tensor(out=ybuf[:], in0=ybuf[:], in1=y0b[:], op=ALU.add), reads=['T0', 'T1'], writes=['T0'])
                        for q in range(2):
                            p, pk = pp.get()
                            S.op('pe', lambda e: e.matmul(p[0:64, :], lhsT=o64[:], rhs=fl(ybuf)[:, q * 512:(q + 1) * 512], start=True, stop=True), reads=['o64', 'T0'], writes=[pk])
                            S.op('dve', lambda e: e.tensor_tensor(out=fl(cen)[:, q * 512:(q + 1) * 512], in0=fl(ybuf)[:, q * 512:(q + 1) * 512], in1=p[0:64, :], op=ALU.subtract), reads=[pk, 'T0'], writes=['T2'])
                        S.op('act', lambda e: e.activation(out=sqb[:], in_=cen[:], func=AF.Square), reads=['T2'], writes=['T3'])
                        yield 'b'
                        rsb = T[1]
                        for q in range(2):
                            p, pk = pp.get()
                            S.op('pe', lambda e: e.matmul(p[0:64, :], lhsT=o64[:], rhs=fl(sqb)[:, q * 512:(q + 1) * 512], start=True, stop=True), reads=['o64', 'T3'], writes=[pk])
                            S.op('act', lambda e: e.activation(out=fl(rsb)[:, q * 512:(q + 1) * 512], in_=p[0:64, :], func=AF.Ln, bias=epsg[:], scale=1.0), reads=[pk, 'epsg'], writes=['T1'])
                        S.op('act', lambda e: e.activation(out=rsb[:], in_=rsb[:], func=AF.Exp, scale=-0.5), reads=['T1'], writes=['T1'])
                        yield 'b'
                        bonb, ggb = T[3], T[0]
                        S.dma('sp', lambda e: e.dma_start(out=bonb[:], in_=K.rwBON[:, tl:tl + G].rearrange("(h p) t -> p h t", p=64)), reads=['rwBON'], writes=['T3'])
                        S.op('dve', lambda e: e.tensor_tensor(out=cen[:], in0=cen[:], in1=rsb[:], op=ALU.mult), reads=['T2', 'T1'], writes=['T2'])
                        S.dma('sp', lambda e: e.dma_start(out=ggb[:], in_=K.rwG[:, tl:tl + G].rearrange("(h p) t -> p h t", p=64)), reads=['rwG'], writes=['T0'])
                        S.op('dve', lambda e: e.tensor_tensor(out=cen[:], in0=cen[:], in1=gng[:, :].unsqueeze(2).to_broadcast([64, 8, G]), op=ALU.mult), reads=['T2', 'gng'], writes=['T2'])
                        S.op('dve', lambda e: e.tensor_tensor(out=cen[:], in0=cen[:], in1=gnb[:, :].unsqueeze(2).to_broadcast([64, 8, G]), op=ALU.add), reads=['T2', 'gnb'], writes=['T2'])
                        S.op('dve', lambda e: e.tensor_tensor(out=cen[:], in0=cen[:], in1=bonb[:], op=ALU.add), reads=['T2', 'T3'], writes=['T2'])
                        S.op('dve', lambda e: e.tensor_tensor(out=outb[:], in0=cen[:], in1=ggb[:], op=ALU.mult), reads=['T2', 'T0'], writes=['outb'])
                        S.dma('pool', lambda e: e.dma_start(out=K.mixT[0:512, tl:tl + G].rearrange("(h p) t -> p h t", p=64), in_=outb[:]), reads=['outb'], writes=['mixT'])
                S.op('dve', lambda e: e.tensor_copy(out=Sth[:, 0], in_=Sth[:, 2]), reads=['Sth2'], writes=['Sth0'])
            def step(gen_):
                try:
                    return next(gen_)
                except StopIteration:
                    return None
            prev = None
            qslot = 0
            for b in blocks:
                gcur = body(b, qslot)
                while True:
                    r = step(gcur)
                    if prev is not None and step(prev) is None:
                        prev = None
                    if r == 'F' or r is None:
                        break
                while prev is not None:
                    if step(prev) is None:
                        prev = None
                prev = gcur
                qslot ^= 1
            while prev is not None:
                if step(prev) is None:
                    prev = None
            S.barrier()


ALPHA = 2.0 ** 0.25
ROWW = 1088
BIGPOS = 4096.0


def phase_mix(K):
    nc, S = K.nc, K.S
    NT = TL // 128
    with ExitStack() as es:
        sb = lambda n, s, d: es.enter_context(nc.sbuf_tensor(_u(n), s, d))
        ps = lambda n, s, d: es.enter_context(nc.psum_tensor(_u(n), s, d))
        wo = sb("wo", [128, 8, 1024], BF16)
        wst = [sb("owst%d" % i, [128, 8, 256], F32) for i in range(2)]
        bc = {n: sb("bc_" + n, [128, 1024], F32) for n in ('g1', 'ln1g', 'ln1b', 'sc2', 'sh2')}
        rt = sb("rt", [128, 8, 16], F32)
        idf = sb("idf", [128, 128], F32)
        epst = sb("epst", [128, 1], F32)
        onesf = sb("onesf", [128, 128], F32)
        onesb = sb("onesb", [128, 128], BF16)
        ustr = sb("ustr", [128, 128], BF16)
        mt = [sb("mt%d" % i, [128, 8, 128], BF16) for i in range(2)]
        xt = [sb("xt%d" % i, [128, 1024], F32) for i in range(2)]
        t1 = sb("t1", [128, 1024], F32)
        pre = sb("pre", [128, 1024], F32)
        x1 = [sb("x1_%d" % i, [128, 1024], F32) for i in range(2)]
        uf = [sb("uf%d" % i, [128, 1024], F32) for i in range(2)]
        urow = [sb("urow%d" % i, [128, ROWW], BF16) for i in range(2)]
        uT = sb("uT", [128, 8, 128], F32)
        st = sb("st", [128, 2, 6], F32)
        mv = sb("mv", [128, 2], F32)
        lnv = sb("lnv", [128, 1], F32)
        rstd = sb("rstd", [128, 1], F32)
        lg = sb("lg", [128, 16], F32)
        mx = sb("mx", [128, 1], F32)
        sm = sb("sm", [128, 1], F32)
        affall = sb("affall", [128, NT, 16], F32)
        tok = sb("tok", [128, 1], I32)
        lo = sb("lo", [128, 16], F32)
        mid = sb("mid", [128, 16], F32)
        ge = sb("ge", [128, 16], F32)
        cntp = sb("cntp", [128, 16], F32)
        mskt = sb("mskt", [128, NT, 16], F32)
        mskb = sb("mskb", [128, NT, 16], BF16)
        csT = sb("csT", [128, 16, NT], F32)
        incT = sb("incT", [128, 16, NT], F32)
        rmask = sb("rmask", [128, 16, NT], F32)
        posf = sb("posf", [128, NT, 16], F32)
        posi = sb("posi", [128, NT, 16], I32)
        zt = sb("zt", [128, 1024], F32)
        pm = [ps("pm%d" % i, [128, 1024], F32) for i in range(2)]
        ptr = ps("ptr", [128, 1024], F32)
        psm = ps("psm", [128, 512], F32)
        psn = ps("psn", [128, 512], F32)

        ld1 = lambda dst, src, key, rd=(): S.dma('sp', lambda e: e.dma_start(out=dst, in_=src, allow_slow_non_contiguous=True), reads=list(rd), writes=[key])
        ld1(idf[:], K.identf, 'idf')
        ld1(ustr[:], K.ustrict, 'ustr')
        ld1(rt[:], K.router.rearrange("(k p) e -> p k e", p=128), 'rt')
        ld1(bc['g1'][:], K.modd[0:1, 2048:3072].partition_broadcast(128), 'bc_g1', ['modd'])
        ld1(bc['sh2'][:], K.modd[0:1, 3072:4096].partition_broadcast(128), 'bc_sh2', ['modd'])
        ld1(bc['sc2'][:], K.modd[0:1, 4096:5120].partition_broadcast(128), 'bc_sc2', ['modd'])
        ld1(bc['ln1g'][:], K.ln1_g.partition_broadcast(128), 'bc_ln1g')
        ld1(bc['ln1b'][:], K.ln1_b.partition_broadcast(128), 'bc_ln1b')
        S.op('dve', lambda e: e.tensor_scalar_add(out=bc['sc2'][:], in0=bc['sc2'][:], scalar1=1.0), reads=['bc_sc2'], writes=['bc_sc2'])
        S.op('dve', lambda e: e.memset(epst[:], 1e-5), writes=['epst'])
        S.op('dve', lambda e: e.memset(onesf[:], 1.0), writes=['onesf'])
        S.op('dve', lambda e: e.memset(onesb[:], 1.0), writes=['onesb'])
        S.op('dve', lambda e: e.memset(zt[:], 0.0), writes=['zt'])
        for j in range(4):
            w = wst[j % 2]
            wk = 'owst%d' % (j % 2)
            S.dma('sp', lambda e: e.dma_start(out=w[:], in_=K.w_out[:, j * 256:(j + 1) * 256].rearrange("(k p) n -> p k n", p=128)), writes=[wk])
            S.op('pool', lambda e: e.tensor_copy(out=wo[:, :, j * 256:(j + 1) * 256], in_=w[:]), reads=[wk], writes=['wo'])
        for i in range(NT):
            S.dma('pool', lambda e: e.dma_start(out=K.yacc[i * 128:(i + 1) * 128, :], in_=zt[:]), reads=['zt'], writes=['yacc%d' % i])

        mvs = [mv, sb("mv1", [128, 2], F32)]
        rstds = [rstd, sb("rstd1", [128, 1], F32)]
        sts = [st, sb("st1", [128, 2, 6], F32)]
        lnvs = [lnv, sb("lnv1", [128, 1], F32)]

        def ln_stats(src, srck, lane=0):
            sfx = '' if lane == 0 else '1'
            for c in range(2):
                S.op('dve', lambda e: e.bn_stats(out=sts[lane][:, c, :], in_=src[:, c * 512:(c + 1) * 512]), reads=[srck], writes=['st%d%s' % (c, sfx)])
            S.op('dve', lambda e: e.bn_aggr(out=mvs[lane][:], in_=sts[lane][:]), reads=['st0' + sfx, 'st1' + sfx], writes=['mv' + sfx])
            S.op('act', lambda e: e.activation(out=lnvs[lane][:], in_=mvs[lane][:, 1:2], func=AF.Ln, bias=epst[:], scale=1.0), reads=['mv' + sfx, 'epst'], writes=['lnv' + sfx])
            S.op('act', lambda e: e.activation(out=rstds[lane][:], in_=lnvs[lane][:], func=AF.Exp, scale=-0.5), reads=['lnv' + sfx], writes=['rstd' + sfx])

        def tbody(i):
            sl = i % 2
            m_, mk_ = mt[sl], 'mt%d' % sl
            x_, xk = xt[sl], 'xt%d' % sl
            S.dma('sp', lambda e: e.dma_start(out=m_[:], in_=K.mixT[:, i * 128:(i + 1) * 128].rearrange("(k p) t -> p k t", p=128)), reads=['mixT'], writes=[mk_])
            S.dma('sp', lambda e: e.dma_start(out=x_[:], in_=K.xin[TC + i * 128:TC + (i + 1) * 128, :]), writes=[xk])
            p_, pk_ = pm[sl], 'pm%d' % sl
            for half in range(2):
                for kc in range(8):
                    S.op('pe', lambda e: e.matmul(p_[:, half * 512:(half + 1) * 512], lhsT=m_[:, kc, :], rhs=wo[:, kc, half * 512:(half + 1) * 512], start=(kc == 0), stop=(kc == 7)),
                         reads=[mk_, 'wo'], writes=[pk_], acc=True)
            yield 'f'
            S.op('dve', lambda e: e.tensor_tensor(out=t1[:], in0=p_[:], in1=bc['g1'][:], op=ALU.mult), reads=[pk_, 'bc_g1'], writes=['t1'])
            S.op('dve', lambda e: e.scalar_tensor_tensor(out=pre[:], in0=x_[:], scalar=ALPHA, in1=t1[:], op0=ALU.mult, op1=ALU.add), reads=[xk, 't1'], writes=['pre'])
            ln_stats(pre, 'pre')
            yield 'f'
            x1_, x1k = x1[sl], 'x1_%d' % sl
            S.op('dve', lambda e: e.tensor_scalar(out=t1[:], in0=pre[:], scalar1=mv[:, 0:1], scalar2=rstd[:], op0=ALU.subtract, op1=ALU.mult), reads=['pre', 'mv', 'rstd'], writes=['t1'])
            S.op('dve', lambda e: e.tensor_tensor(out=t1[:], in0=t1[:], in1=bc['ln1g'][:], op=ALU.mult), reads=['t1', 'bc_ln1g'], writes=['t1'])
            S.op('dve', lambda e: e.tensor_tensor(out=x1_[:], in0=t1[:], in1=bc['ln1b'][:], op=ALU.add), reads=['t1', 'bc_ln1b'], writes=[x1k])
            S.dma('pool', lambda e: e.dma_start(out=K.x1d[i * 128:(i + 1) * 128, :], in_=x1_[:]), reads=[x1k], writes=['x1d'])
            yield 'f'
            ln_stats(x1_, x1k, 1)
            yield 'f'
            S.op('dve', lambda e: e.tensor_scalar(out=pre[:], in0=x1_[:], scalar1=mvs[1][:, 0:1], scalar2=rstds[1][:], op0=ALU.subtract, op1=ALU.mult), reads=[x1k, 'mv1', 'rstd1'], writes=['pre'])
            S.op('dve', lambda e: e.tensor_tensor(out=pre[:], in0=pre[:], in1=bc['sc2'][:], op=ALU.mult), reads=['pre', 'bc_sc2'], writes=['pre'])
            S.op('dve', lambda e: e.tensor_tensor(out=uf[sl][:], in0=pre[:], in1=bc['sh2'][:], op=ALU.add), reads=['pre', 'bc_sh2'], writes=['uf%d' % sl])
            ur, urk = urow[sl], 'urow%d' % sl
            S.op('act', lambda e: e.activation(out=ur[:, 0:1024], in_=uf[sl][:], func=AF.Identity), reads=['uf%d' % sl], writes=[urk])
            yield 'F'
            for k in range(8):
                S.op('pe', lambda e: e.transpose(out=ptr[:, k * 128:(k + 1) * 128], in_=uf[sl][:, k * 128:(k + 1) * 128], identity=idf[:]), reads=['uf%d' % sl, 'idf'], writes=['ptr'], acc=True)
            S.op('act', lambda e: e.activation(out=uT[:].rearrange("p k t -> p (k t)"), in_=ptr[:], func=AF.Identity), reads=['ptr'], writes=['uT'])
            yield 'b'
            for k in range(8):
                S.op('pe', lambda e: e.matmul(psm[:, 0:16], lhsT=uT[:, k, :], rhs=rt[:, k, :], start=(k == 0), stop=(k == 7)), reads=['uT', 'rt'], writes=['psm'], acc=True)
            S.op('dve', lambda e: e.reduce_max(out=mx[:], in_=psm[:, 0:16], axis=AX.X), reads=['psm'], writes=['mx'])
            S.op('dve', lambda e: e.tensor_scalar_mul(out=mx[:], in0=mx[:], scalar1=-1.0), reads=['mx'], writes=['mx'])
            yield 'b'
            S.op('act', lambda e: e.activation(out=lg[:], in_=psm[:, 0:16], func=AF.Exp, bias=mx[:], scale=1.0, accum_out=sm[:]), reads=['psm', 'mx'], writes=['lg', 'sm'])
            S.op('dve', lambda e: e.reciprocal(out=sm[:], in_=sm[:]), reads=['sm'], writes=['sm'])
            yield 'b'
            S.op('dve', lambda e: e.tensor_scalar_mul(out=affall[:, i, :], in0=lg[:], scalar1=sm[:]), reads=['lg', 'sm'], writes=['affall'])
            S.op('dve', lambda e: e.tensor_copy(out=ur[:, 1024:1056].bitcast(F32), in_=affall[:, i, :]), reads=['affall'], writes=[urk])
            S.op('pool', lambda e: e.iota(tok[:], pattern=[[0, 1]], base=i * 128, channel_multiplier=1), writes=['tok'])
            S.op('pool', lambda e: e.tensor_copy(out=ur[:, 1056:1058].bitcast(I32), in_=tok[:]), reads=['tok'], writes=[urk])
            S.dma('pool', lambda e: e.dma_start(out=K.urd[i * 128:(i + 1) * 128, 0:1058], in_=ur[:, 0:1058]), reads=[urk], writes=['urd%d' % i])

        def step(gen_):
            try:
                return next(gen_)
            except StopIteration:
                return None
        prev = None
        for i in range(NT):
            gcur = tbody(i)
            while True:
                r = step(gcur)
                if prev is not None and step(prev) is None:
                    prev = None
                if r == 'F' or r is None:
                    break
            while prev is not None:
                if step(prev) is None:
                    prev = None
            prev = gcur
        while prev is not None:
            if step(prev) is None:
                prev = None

        S.op('dve', lambda e: e.memset(lo[:], 0.0), writes=['lo'])
        for k in range(30):
            hk = 2.0 ** -(k + 1)
            S.op('dve', lambda e: e.tensor_scalar_add(out=mid[:], in0=lo[:], scalar1=hk), reads=['lo'], writes=['mid'])
            S.op('dve', lambda e: e.tensor_tensor(out=mskt[:], in0=affall[:], in1=mid[:, :].unsqueeze(1).to_broadcast([128, NT, 16]), op=ALU.is_ge), reads=['affall', 'mid'], writes=['mskt'])
            S.op('dve', lambda e: e.tensor_reduce(out=cntp[:], in_=mskt[:].rearrange("p i e -> p e i"), axis=AX.X, op=ALU.add), reads=['mskt'], writes=['cntp'])
            S.op('pe', lambda e: e.matmul(psn[:, 0:16], lhsT=onesf[:], rhs=cntp[:], start=True, stop=True), reads=['onesf', 'cntp'], writes=['psn'])
            S.op('dve', lambda e: e.tensor_scalar(out=ge[:], in0=psn[:, 0:16], scalar1=float(CAP) - 0.5, scalar2=hk, op0=ALU.is_ge, op1=ALU.mult), reads=['psn'], writes=['ge'])
            S.op('dve', lambda e: e.tensor_tensor(out=lo[:], in0=lo[:], in1=ge[:], op=ALU.add), reads=['lo', 'ge'], writes=['lo'])
        S.op('dve', lambda e: e.tensor_tensor(out=mskt[:], in0=affall[:], in1=lo[:, :].unsqueeze(1).to_broadcast([128, NT, 16]), op=ALU.is_ge), reads=['affall', 'lo'], writes=['mskt'])
        S.op('act', lambda e: e.activation(out=mskb[:], in_=mskt[:], func=AF.Identity), reads=['mskt'], writes=['mskb'])
        mflat = mskb[:].rearrange("p i e -> p (i e)")
        for hh in range(2):
            S.op('pe', lambda e: e.matmul(psm[:, :], lhsT=onesb[:], rhs=mflat[:, hh * 512:(hh + 1) * 512], start=True, stop=True), reads=['onesb', 'mskb'], writes=['psm'])
            S.op('dve', lambda e: e.tensor_copy(out=csT[:, :, hh * 32:(hh + 1) * 32], in_=psm[:, :].rearrange("p (i e) -> p e i", e=16)), reads=['psm'], writes=['csT'])
        S.op('dve', lambda e: e.memset(rmask[:], 1.0), writes=['rmask'])
        S.op('dve', lambda e: e.memset(rmask[:, :, 0:1], 0.0), writes=['rmask'])
        S.op('dve', lambda e: e.tensor_tensor_scan(out=incT[:].rearrange("p e i -> p (e i)"), data0=rmask[:].rearrange("p e i -> p (e i)"), data1=csT[:].rearrange("p e i -> p (e i)"),
                                                   initial=0.0, op0=ALU.mult, op1=ALU.add), reads=['rmask', 'csT'], writes=['incT'])
        S.op('dve', lambda e: e.tensor_tensor(out=incT[:], in0=incT[:], in1=csT[:], op=ALU.subtract), reads=['incT', 'csT'], writes=['incT'])
        for hh in range(2):
            S.op('pe', lambda e: e.matmul(psm[:, :], lhsT=ustr[:], rhs=mflat[:, hh * 512:(hh + 1) * 512], start=True, stop=True), reads=['ustr', 'mskb'], writes=['psm'])
            S.op('dve', lambda e: e.tensor_tensor(out=posf[:, hh * 32:(hh + 1) * 32, :], in0=psm[:, :].rearrange("p (i e) -> p i e", e=16),
                                                  in1=incT[:, :, hh * 32:(hh + 1) * 32].rearrange("p e i -> p i e"), op=ALU.add), reads=['psm', 'incT'], writes=['posf'])
        S.op('dve', lambda e: e.scalar_tensor_tensor(out=posf[:], in0=posf[:], scalar=-BIGPOS, in1=mskt[:], op0=ALU.add, op1=ALU.mult), reads=['posf', 'mskt'], writes=['posf'])
        S.op('dve', lambda e: e.tensor_scalar_add(out=posf[:], in0=posf[:], scalar1=BIGPOS), reads=['posf'], writes=['posf'])
        S.op('dve', lambda e: e.tensor_copy(out=posi[:], in_=posf[:]), reads=['posf'], writes=['posi'])
        if K.dbg_pos is not None:
            S.dma('sp', lambda e: e.dma_start(out=K.dbg_pos, in_=posf[:]), reads=['posf'], writes=['dbg_pos'])
        breg = nc.gpsimd.to_reg(CAP - 1)
        for i in range(NT):
            sl = i % 2
            ur, urk = urow[sl], 'urow%d' % sl
            S.dma('sp', lambda e: e.dma_start(out=ur[:, 0:1058], in_=K.urd[i * 128:(i + 1) * 128, 0:1058]), reads=['urd%d' % i], writes=[urk])
            for ex in range(NE):
                S.dma('pool', lambda e: e.indirect_dma_start(out=K.xe_d[ex], out_offset=bass.IndirectOffsetOnAxis(ap=posi[:, i, ex:ex + 1], axis=0),
                                                             in_=ur[:, :], in_offset=None, bounds_check=breg, oob_is_err=False),
                      reads=[urk, 'posi'], writes=['xe_d_%d_%d' % (i, ex)])
        S.barrier()


def phase_moe(K, experts=range(NE)):
    nc, S = K.nc, K.S
    NT = TL // 128
    with ExitStack() as es:
        sb = lambda n, s, d: es.enter_context(nc.sbuf_tensor(_u(n), s, d))
        ps = lambda n, s, d: es.enter_context(nc.psum_tensor(_u(n), s, d))
        W = [[sb("W%d_%d" % (m, i), [128, 8, 1024], BF16) for i in range(2)] for m in range(3)]
        wst = Rot([sb("ewst%d" % i, [128, 8, 256], F32) for i in range(3)], "ewst")
        idb = sb("idb", [128, 128], BF16)
        xrow = Rot([sb("xrow%d" % i, [128, ROWW], BF16) for i in range(2)], "xrow")
        xeT = [sb("xeT%d" % i, [128, 8, 1024], BF16) for i in range(2)]
        hidT = sb("hidT", [128, 8, 1024], BF16)
        gates = [sb("gates%d" % i, [128, 8], F32) for i in range(2)]
        idxs = [sb("idxs%d" % i, [128, 8], I32) for i in range(2)]
        sgt = Rot([sb("sgt%d" % i, [128, 512], F32) for i in range(2)], "sgt")
        ye = Rot([sb("ye%d" % i, [128, 1024], F32) for i in range(2)], "ye")
        pt = Rot([ps("ept%d" % i, [128, 1024], BF16) for i in range(2)], "ept")
        pg = Rot([ps("epg%d" % i, [128, 512], F32) for i in range(6)], "epg")
        S.dma('sp', lambda e: e.dma_start(out=idb[:], in_=K.ident), writes=['idb'])
        cast_i = [0]

        def load_w(ex, slot):
            for m, src in enumerate((K.exp_w_gate, K.exp_w_up, K.exp_w_down)):
                for j in range(4):
                    w, wk = wst.get()
                    S.dma('sp', lambda e: e.dma_start(out=w[:], in_=src[ex, :, j * 256:(j + 1) * 256].rearrange("(k p) n -> p k n", p=128)), writes=[wk])
                    eng = 'dve'
                    cast_i[0] += 1
                    if eng == 'dve':
                        S.op('dve', lambda e: e.tensor_copy(out=W[m][slot][:, :, j * 256:(j + 1) * 256], in_=w[:]), reads=[wk], writes=['W%d_%d' % (m, slot)])
                    else:
                        S.op('act', lambda e: e.activation(out=W[m][slot][:, :, j * 256:(j + 1) * 256], in_=w[:], func=AF.Identity), reads=[wk], writes=['W%d_%d' % (m, slot)])

        exl = list(experts)
        load_w(exl[0], 0)
        if len(exl) > 1:
            load_w(exl[1], 1)
        def ebody(n, ex):
            xs = n % 2
            slot = n % 2
            Wg, Wu, Wd = W[0][slot], W[1][slot], W[2][slot]
            wkeys = ['W%d_%d' % (m, slot) for m in range(3)]
            for j in range(8):
                xr, xk = xrow.get()
                S.dma('sp', lambda e: e.dma_start(out=xr[:, 0:1058], in_=K.xe_d[ex][j * 128:(j + 1) * 128, 0:1058]), reads=['xe_d'], writes=[xk])
                S.op('dve', lambda e: e.tensor_copy(out=gates[xs][:, j:j + 1], in_=xr[:, 1024:1056].bitcast(F32)[:, ex:ex + 1]), reads=[xk], writes=['gates%d' % xs])
                S.op('dve', lambda e: e.tensor_copy(out=idxs[xs][:, j:j + 1], in_=xr[:, 1056:1058].bitcast(I32)), reads=[xk], writes=['idxs%d' % xs])
                p, pk = pt.get()
                for k in range(8):
                    S.op('pe', lambda e: e.transpose(out=p[:, k * 128:(k + 1) * 128], in_=xr[:, k * 128:(k + 1) * 128], identity=idb[:]), reads=[xk, 'idb'], writes=[pk], acc=True)
                S.op('act', lambda e: e.activation(out=xeT[xs][:, :, j * 128:(j + 1) * 128], in_=p[:].rearrange("p (k t) -> p k t", k=8), func=AF.Identity), reads=[pk], writes=['xeT%d' % xs])
                yield 'f'
            yield 'F'
            for fc in range(8):
                for half in range(2):
                    g_, gk = pg.get()
                    u_, uk = pg.get()
                    for kc in range(8):
                        S.op('pe', lambda e: e.matmul(g_[:], lhsT=Wg[:, kc, fc * 128:(fc + 1) * 128], rhs=xeT[xs][:, kc, half * 512:(half + 1) * 512], start=(kc == 0), stop=(kc == 7)),
                             reads=[wkeys[0], 'xeT%d' % xs], writes=[gk], acc=True)
                    for kc in range(8):
                        S.op('pe', lambda e: e.matmul(u_[:], lhsT=Wu[:, kc, fc * 128:(fc + 1) * 128], rhs=xeT[xs][:, kc, half * 512:(half + 1) * 512], start=(kc == 0), stop=(kc == 7)),
                             reads=[wkeys[1], 'xeT%d' % xs], writes=[uk], acc=True)
                    s_, sk = sgt.get()
                    S.op('act', lambda e: e.activation(out=s_[:], in_=g_[:], func=AF.Silu), reads=[gk], writes=[sk])
                    S.op('dve', lambda e: e.tensor_tensor(out=hidT[:, fc, half * 512:(half + 1) * 512], in0=s_[:], in1=u_[:], op=ALU.mult), reads=[sk, uk], writes=['hidT'])
                    yield 'b'
            for j in range(8):
                y_, yk = ye.get()
                for dh in range(2):
                    o_, ok = pg.get()
                    for fc in range(8):
                        S.op('pe', lambda e: e.matmul(o_[:], lhsT=hidT[:, fc, j * 128:(j + 1) * 128], rhs=Wd[:, fc, dh * 512:(dh + 1) * 512], start=(fc == 0), stop=(fc == 7)),
                             reads=[wkeys[2], 'hidT'], writes=[ok], acc=True)
                    S.op('act', lambda e: e.activation(out=y_[:, dh * 512:(dh + 1) * 512], in_=o_[:], func=AF.Identity, scale=gates[xs][:, j:j + 1]), reads=[ok, 'gates%d' % xs], writes=[yk])
                S.dma('pool', lambda e: e.indirect_dma_start(out=K.yacc, out_offset=bass.IndirectOffsetOnAxis(ap=idxs[xs][:, j:j + 1], axis=0), in_=y_[:, :], in_offset=None,
                                                             compute_op=ALU.add),
                      reads=[yk, 'idxs%d' % xs], writes=['yacc'])
                yield 'b'
        def step(gen_):
            try:
                return next(gen_)
            except StopIteration:
                return None
        prev = None
        for n, ex in enumerate(exl):
            gcur = ebody(n, ex)
            while True:
                r = step(gcur)
                if prev is not None and step(prev) is None:
                    prev = None
                if r == 'F' or r is None:
                    break
            while prev is not None:
                if step(prev) is None:
                    prev = None
            if n >= 1 and n + 1 < len(exl):
                load_w(exl[n + 1], (n + 1) % 2)
            prev = gcur
        while prev is not None:
            if step(prev) is None:
                prev = None
        S.barrier()


def phase_final(K):
    nc, S = K.nc, K.S
    NT = TL // 128
    with ExitStack() as es:
        sb = lambda n, s, d: es.enter_context(nc.sbuf_tensor(_u(n), s, d))
        bc = {n: sb("fbc_" + n, [128, 1024], F32) for n in ('g2', 'ln2g', 'ln2b')}
        epst = sb("epst", [128, 1], F32)
        x1 = [sb("fx1_%d" % i, [128, 1024], F32) for i in range(2)]
        ya = [sb("fya_%d" % i, [128, 1024], F32) for i in range(2)]
        t1 = sb("t1", [128, 1024], F32)
        pre = sb("pre", [128, 1024], F32)
        ob = [sb("fob_%d" % i, [128, 1024], F32) for i in range(2)]
        st = sb("st", [128, 2, 6], F32)
        mv = sb("mv", [128, 2], F32)
        lnv = sb("lnv", [128, 1], F32)
        rstd = sb("rstd", [128, 1], F32)
        ld1 = lambda dst, src, key, rd=(): S.dma('sp', lambda e: e.dma_start(out=dst, in_=src, allow_slow_non_contiguous=True), reads=list(rd), writes=[key])
        ld1(bc['g2'][:], K.modd[0:1, 5120:6144].partition_broadcast(128), 'fbc_g2', ['modd'])
        ld1(bc['ln2g'][:], K.ln2_g.partition_broadcast(128), 'fbc_ln2g')
        ld1(bc['ln2b'][:], K.ln2_b.partition_broadcast(128), 'fbc_ln2b')
        S.op('dve', lambda e: e.memset(epst[:], 1e-5), writes=['epst'])
        for i in range(NT):
            sl = i % 2
            S.dma('sp', lambda e: e.dma_start(out=x1[sl][:], in_=K.x1d[i * 128:(i + 1) * 128, :]), reads=['x1d'], writes=['fx1_%d' % sl])
            S.dma('sp', lambda e: e.dma_start(out=ya[sl][:], in_=K.yacc[i * 128:(i + 1) * 128, :]), reads=['yacc'], writes=['fya_%d' % sl])
            S.op('dve', lambda e: e.tensor_tensor(out=t1[:], in0=ya[sl][:], in1=bc['g2'][:], op=ALU.mult), reads=['fya_%d' % sl, 'fbc_g2'], writes=['t1'])
            S.op('dve', lambda e: e.scalar_tensor_tensor(out=pre[:], in0=x1[sl][:], scalar=ALPHA, in1=t1[:], op0=ALU.mult, op1=ALU.add), reads=['fx1_%d' % sl, 't1'], writes=['pre'])
            for c in range(2):
                S.op('dve', lambda e: e.bn_stats(out=st[:, c, :], in_=pre[:, c * 512:(c + 1) * 512]), reads=['pre'], writes=['st%d' % c])
            S.op('dve', lambda e: e.bn_aggr(out=mv[:], in_=st[:]), reads=['st0', 'st1'], writes=['mv'])
            S.op('act', lambda e: e.activation(out=lnv[:], in_=mv[:, 1:2], func=AF.Ln, bias=epst[:], scale=1.0), reads=['mv', 'epst'], writes=['lnv'])
            S.op('act', lambda e: e.activation(out=rstd[:], in_=lnv[:], func=AF.Exp, scale=-0.5), reads=['lnv'], writes=['rstd'])
            S.op('dve', lambda e: e.tensor_scalar(out=t1[:], in0=pre[:], scalar1=mv[:, 0:1], scalar2=rstd[:], op0=ALU.subtract, op1=ALU.mult), reads=['pre', 'mv', 'rstd'], writes=['t1'])
            S.op('dve', lambda e: e.tensor_tensor(out=t1[:], in0=t1[:], in1=bc['ln2g'][:], op=ALU.mult), reads=['t1', 'fbc_ln2g'], writes=['t1'])
            S.op('dve', lambda e: e.tensor_tensor(out=ob[sl][:], in0=t1[:], in1=bc['ln2b'][:], op=ALU.add), reads=['t1', 'fbc_ln2b'], writes=['fob_%d' % sl])
            S.dma('pool', lambda e: e.dma_start(out=K.out[i * 128:(i + 1) * 128, :], in_=ob[sl][:]), reads=['fob_%d' % sl], writes=['out'])
        S.barrier()


def build_program(debug=(), phases=None, dbg_in=(), opts=None):
    opts = opts or {}
    nc = bass.Bass("TRN2", target_bir_lowering=False)
    K = Ctx()
    K.nc = nc
    di = lambda n, s, d: nc.dram_tensor(n, s, d, kind="ExternalInput").ap()
    K.xin = di("xin", [TA, D], F32)
    K.ccT = di("ccT", [128, 8, 2], F32)
    K.w_ada = di("w_ada", [D, 6 * D], F32)
    K.b_ada = di("b_ada", [1, 6 * D], F32)
    K.w_in = di("w_in", [D, 2560], F32)
    K.cosT = di("cosT", [64, TL], F32)
    K.sinT = di("sinT", [64, TL], F32)
    K.ident = di("ident", [128, 128], BF16)
    K.identf = di("identf", [128, 128], F32)
    K.i64dbl = di("i64dbl", [128, 128], F32)
    K.bones = di("bones", [128, 128], F32)
    K.rwm = [di("rwm%d" % d, [128, 1344], F32) for d in range(2)]
    K.mla_q_norm = di("mla_q_norm", [1, 256], F32)
    K.mla_kv_norm = di("mla_kv_norm", [1, 256], F32)
    K.w_uq = di("w_uq", [256, 1024], F32)
    K.w_uk = di("w_uk", [256, 512], F32)
    K.w_uv = di("w_uv", [256, 512], F32)
    K.rwkv_conv = di("rwkv_conv", [3, 1536], F32)
    K.rwkv_w0 = di("rwkv_w0", [2, 512], F32)
    K.rwkv_w_up = di("rwkv_w_up", [2, 64, 512], F32)
    K.rwkv_a0 = di("rwkv_a0", [2, 512], F32)
    K.rwkv_a_up = di("rwkv_a_up", [2, 64, 512], F32)
    K.rwkv_g_up = di("rwkv_g_up", [128, 512], F32)
    K.rwkv_k_k = di("rwkv_k_k", [1, 512], F32)
    K.rwkv_k_a = di("rwkv_k_a", [1, 512], F32)
    K.rwkv_r_k = di("rwkv_r_k", [1, 512], F32)
    K.rwkv_gn_g = di("rwkv_gn_g", [1, 512], F32)
    K.rwkv_gn_b = di("rwkv_gn_b", [1, 512], F32)
    K.w_out = di("w_out", [D, D], F32)
    K.ln1_g = di("ln1_g", [1, D], F32)
    K.ln1_b = di("ln1_b", [1, D], F32)
    K.ln2_g = di("ln2_g", [1, D], F32)
    K.ln2_b = di("ln2_b", [1, D], F32)
    K.router = di("router", [D, NE], F32)
    K.ustrict = di("ustrict", [128, 128], BF16)
    K.exp_w_gate = di("exp_w_gate", [NE, D, D], F32)
    K.exp_w_up = di("exp_w_up", [NE, D, D], F32)
    K.exp_w_down = di("exp_w_down", [NE, D, D], F32)

    def scratch(n, s, d):
        if n in dbg_in:
            return nc.dram_tensor(n, s, d, kind="ExternalInput").ap()
        kind = "ExternalOutput" if n in debug else "Internal"
        return nc.dram_tensor(n, s, d, kind=kind).ap()
    K.modd = scratch("modd", [2, 6 * D], F32)
    K.hT = scratch("hT", [2432, TA], F32)
    K.krT = scratch("krT", [64, TA], BF16)
    K.qnT = scratch("qnT", [512, TL], BF16)
    K.qrT = scratch("qrT", [256, TL], BF16)
    K.knT = scratch("knT", [512, TA], BF16)
    K.vtok = scratch("vtok", [TA, 512], BF16)
    K.mixT = scratch("mixT", [1024, TL], BF16)
    K.rwR = scratch("rwR", [512, TA], F32)
    K.rwV = scratch("rwV", [512, TA], F32)
    K.rwKK = scratch("rwKK", [512, TA], F32)
    K.rwKD = [scratch("rwKD%d" % d, [512, TA], F32) for d in range(2)]
    K.rwB = [scratch("rwB%d" % d, [512, TA], F32) for d in range(2)]
    K.rwVtok = scratch("rwVtok", [TA, 512], F32)
    K.rwLD = [scratch("rwLD%d" % d, [TA, 512], F32) for d in range(2)]
    K.rwG = scratch("rwG", [512, TL], F32)
    K.rwBON = scratch("rwBON", [512, TL], F32)
    K.rwY0 = scratch("rwY0", [512, TL], F32)
    K.x1d = scratch("x1d", [TL, D], F32)
    K.urd = scratch("urd", [TL, ROWW], BF16)
    K.xe_d = [scratch("xe_d%d" % e_, [CAP, ROWW], BF16) for e_ in range(NE)]
    K.yacc = scratch("yacc", [TL, D], F32)
    K.dbg_pos = scratch("dbg_pos", [128, TL // 128, 16], F32) if 'dbg_pos' in debug else None
    K.out = nc.dram_tensor("out", [TL, D], F32, kind="ExternalOutput").ap()
    allp = ['mod', 'inproj', 'mlaprep', 'attn', 'rwprep', 'rwscan', 'mix', 'moe', 'final']
    if phases is None:
        phases = allp
    with ExitStack() as es:
        S = Sync(nc, es)
        K.S = S
        if 'mod' in phases:
            phase_mod(K)
        if 'inproj' in phases:
            phase_inproj(K)
        if 'mlaprep' in phases:
            phase_mlaprep(K)
        if 'attn' in phases:
            phase_attn(K)
        if 'attn1' in phases:
            phase_attn(K, heads=(1,), nqt=2)
        if 'rwprep' in phases:
            phase_rwprep(K)
        if 'rwscan' in phases:
            phase_rwscan(K, **opts.get('rwscan', {}))
        if 'mix' in phases:
            phase_mix(K)
        if 'moe' in phases:
            phase_moe(K, **opts.get('moe', {}))
        if 'final' in phases:
            phase_final(K)
        S.wait_all('sp')
        print("instructions", S.n_inst, "waits", S.n_wait, "sems", S.nsem + NDMA)
    return nc


_SWAP = np.concatenate([np.arange(16, 32), np.arange(0, 16), np.arange(48, 64), np.arange(32, 48)])


def rope_tables():
    half = 32
    inv_freq = (10000.0 ** (-np.arange(0, half, 2, dtype=np.float32) / half)).astype(np.float32)
    t = np.arange(TL)
    rr = (t // 64).astype(np.float32)[None, :]
    cc = (t % 64).astype(np.float32)[None, :]
    ang_r = (inv_freq[:, None] * rr).astype(np.float32)
    ang_c = (inv_freq[:, None] * cc).astype(np.float32)
    cosT = np.concatenate([np.cos(ang_r), np.cos(ang_r), np.cos(ang_c), np.cos(ang_c)], 0).astype(np.float32)
    sinT = np.concatenate([-np.sin(ang_r), np.sin(ang_r), -np.sin(ang_c), np.sin(ang_c)], 0).astype(np.float32)
    return np.ascontiguousarray(cosT), np.ascontiguousarray(sinT)


def make_in_maps(inputs, batches):
    f = lambda a: np.ascontiguousarray(np.asarray(a, dtype=np.float32))
    w_in = f(inputs['w_in'][0])
    w_in_ext = np.concatenate([w_in, w_in[:, 2432:2496][:, _SWAP]], axis=1)
    cosT, sinT = rope_tables()
    wuq = f(inputs['mla_w_uq'][0])
    cols = []
    for h in range(4):
        nope = wuq[:, h * 192:h * 192 + 128]
        rope = wuq[:, h * 192 + 128:h * 192 + 192]
        cols += [nope, rope, rope[:, _SWAP]]
    wuq_ext = np.ascontiguousarray(np.concatenate(cols, axis=1))
    shared = {
        'w_ada': f(inputs['w_ada'][0]), 'b_ada': f(inputs['b_ada']), 'w_in': np.ascontiguousarray(w_in_ext),
        'cosT': cosT, 'sinT': sinT, 'ident': np.eye(128).astype(ml_dtypes.bfloat16),
        'mla_q_norm': f(inputs['mla_q_norm']), 'mla_kv_norm': f(inputs['mla_kv_norm']),
        'w_uq': wuq_ext, 'w_uk': f(inputs['mla_w_uk'][0]), 'w_uv': f(inputs['mla_w_uv'][0]),
        'rwkv_conv': f(inputs['rwkv_conv'][0]), 'rwkv_w0': f(inputs['rwkv_w0'][0]), 'rwkv_w_up': f(inputs['rwkv_w_up'][0]),
        'rwkv_a0': f(inputs['rwkv_a0'][0]), 'rwkv_a_up': f(inputs['rwkv_a_up'][0]), 'rwkv_g_up': f(inputs['rwkv_g_up'][0]),
        'rwkv_k_k': f(inputs['rwkv_k_k']), 'rwkv_k_a': f(inputs['rwkv_k_a']), 'rwkv_r_k': f(inputs['rwkv_r_k']).reshape(1, 512),
        'rwkv_gn_g': f(inputs['rwkv_gn_g']), 'rwkv_gn_b': f(inputs['rwkv_gn_b']),
        'w_out': f(inputs['w_out'][0]), 'ln1_g': f(inputs['ln1_g']), 'ln1_b': f(inputs['ln1_b']),
        'ln2_g': f(inputs['ln2_g']), 'ln2_b': f(inputs['ln2_b']), 'router': f(inputs['router'][0]),
        'ustrict': (np.arange(128)[:, None] < np.arange(128)[None, :]).astype(ml_dtypes.bfloat16),
        'exp_w_gate': f(inputs['exp_w_gate'][0]), 'exp_w_up': f(inputs['exp_w_up'][0]), 'exp_w_down': f(inputs['exp_w_down'][0]),
    }
    shared.update(rw_consts())
    maps = []
    for b in batches:
        m = dict(shared)
        m['xin'] = np.ascontiguousarray(np.concatenate([inputs['ctx'][b], inputs['x'][b]], axis=0).astype(np.float32))
        cc = np.stack([inputs['c'][b], inputs['c_ctx']], axis=-1).astype(np.float32)
        m['ccT'] = np.ascontiguousarray(cc.reshape(8, 128, 2).transpose(1, 0, 2))
        maps.append(m)
    return maps


_CONST_KEYS = ('cosT', 'sinT', 'ident', 'identf', 'i64dbl', 'bones', 'rwm0', 'rwm1', 'ustrict')
BATCH_CORES = (0, 1, 4, 5)


def kernel(**inputs):
    nc = build_program()
    real = make_in_maps(inputs, [0, 1, 2, 3])
    zero = {k: (v if k in _CONST_KEYS else np.zeros_like(v)) for k, v in real[0].items()}
    maps = [zero] * 8
    for b, c in enumerate(BATCH_CORES):
        maps[c] = real[b]
    res = run_bass_kernel_spmd(nc, maps, core_ids=list(range(8)))
    out = np.stack([res.results[c]['out'] for c in BATCH_CORES], axis=0)
    return out.astype(np.float32)
```

```python
import numpy as np
import ml_dtypes
from contextlib import ExitStack
import concourse.bass as bass
import concourse.mybir as mybir
from concourse.bass_utils import run_bass_kernel_spmd

F32 = mybir.dt.float32
BF16 = mybir.dt.bfloat16
I32 = mybir.dt.int32
AF = mybir.ActivationFunctionType
ALU = mybir.AluOpType
AX = mybir.AxisListType

EPOCH = 12000
NDMA = 24

D = 1024
TL = 8192
TC = 256
TA = TL + TC
NE = 16
CAP = 1024


class Sync:
    def __init__(self, nc, es):
        self.nc = nc
        self.es = es
        self.eng = {'pe': nc.tensor, 'dve': nc.vector, 'act': nc.scalar,
                    'pool': nc.gpsimd, 'sp': nc.sync}
        self.sem = {}
        self.cnt = {}
        self.cur = {}
        self.known = {e: {} for e in self.eng}
        self.snap = {}
        self.last_w = {}
        self.readers = {}
        self.dma_keys = []
        self.dma_rr = 0
        self.nsem = 0
        for e in self.eng:
            self._new_epoch(e)
        for i in range(NDMA):
            k = ('dma', i)
            self.sem[k] = es.enter_context(nc.semaphore('dq%d' % i))
            self.cnt[k] = 0
            self.dma_keys.append(k)
        self.n_inst = 0
        self.n_wait = 0

    def _new_epoch(self, e):
        idx = self.nsem
        self.nsem += 1
        k = (e, idx)
        self.sem[k] = self.es.enter_context(self.nc.semaphore('s_%s_%d' % (e, idx)))
        self.cnt[k] = 0
        self.cur[e] = k

    def _need(self, e, ticket):
        k, v = ticket
        kn = self.known[e]
        if kn.get(k, 0) >= v:
            return
        self.eng[e].wait_ge(self.sem[k], v)
        self.n_wait += 1
        kn[k] = v
        sn = self.snap.get(ticket)
        if sn:
            for kk, vv in sn.items():
                if kn.get(kk, 0) < vv:
                    kn[kk] = vv

    def _deps(self, e, reads, writes, acc):
        for b in reads:
            t = self.last_w.get(b)
            if t is not None:
                self._need(e, t)
        for b in writes:
            t = self.last_w.get(b)
            if t is not None and not (acc and t[0] == self.cur[e]):
                self._need(e, t)
            for t in self.readers.get(b, ()):
                self._need(e, t)

    def _record(self, ticket, reads, writes):
        for b in reads:
            self.readers.setdefault(b, []).append(ticket)
        for b in writes:
            self.last_w[b] = ticket
            self.readers[b] = []

    def op(self, e, fn, reads=(), writes=(), acc=False):
        if self.cnt[self.cur[e]] >= EPOCH:
            self._new_epoch(e)
        self._deps(e, reads, writes, acc)
        k = self.cur[e]
        inst = fn(self.eng[e])
        inst.then_inc(self.sem[k], 1)
        self.cnt[k] += 1
        t = (k, self.cnt[k])
        self.snap[t] = dict(self.known[e])
        self._record(t, reads, writes)
        self.n_inst += 1
        return t

    def dma(self, e, fn, reads=(), writes=()):
        k = self.dma_keys[self.dma_rr]
        self.dma_rr = (self.dma_rr + 1) % NDMA
        if self.cnt[k] > 0:
            self._need(e, (k, self.cnt[k]))
        self._deps(e, reads, writes, False)
        inst = fn(self.eng[e])
        inst.then_inc(self.sem[k], 16)
        self.cnt[k] += 16
        t = (k, self.cnt[k])
        self.snap[t] = dict(self.known[e])
        self._record(t, reads, writes)
        self.n_inst += 1
        return t

    def wait_all(self, e):
        for k, v in list(self.cnt.items()):
            if v > 0:
                self._need(e, (k, v))

    def barrier(self):
        for e in self.eng:
            self.wait_all(e)
        self.last_w.clear()
        self.readers.clear()


class Ctx:
    pass


_UC = [0]


def _u(n):
    _UC[0] += 1
    return '%s_%d' % (n, _UC[0])


def phase_mod(K):
    nc, S = K.nc, K.S
    with ExitStack() as es:
        sb = lambda n, s, d: es.enter_context(nc.sbuf_tensor(_u(n), s, d))
        ccs = sb("ccs", [128, 8, 2], F32)
        scT = sb("scT", [128, 8, 2], F32)
        wa = [sb("wa%d" % i, [128, 8, 512], F32) for i in range(2)]
        ba = sb("ba", [1, 6144], F32)
        one1 = sb("one1", [1, 1], F32)
        mrow = [sb("mrow%d" % r, [1, 6144], F32) for r in range(2)]
        pm = [es.enter_context(nc.psum_tensor(_u("pm%d" % i), [1, 512], F32)) for i in range(2)]
        S.dma('sp', lambda e: e.dma_start(out=ccs[:], in_=K.ccT), writes=['ccs'])
        S.dma('sp', lambda e: e.dma_start(out=ba[:], in_=K.b_ada), writes=['ba'])
        S.op('dve', lambda e: e.memset(one1[:], 1.0), writes=['one1'])
        S.op('act', lambda e: e.activation(out=scT[:], in_=ccs[:], func=AF.Silu), reads=['ccs'], writes=['scT'])
        for j in range(12):
            w = wa[j % 2]
            wk = 'wa%d' % (j % 2)
            S.dma('sp', lambda e: e.dma_start(out=w[:], in_=K.w_ada[:, j * 512:(j + 1) * 512].rearrange("(k p) n -> p k n", p=128)), writes=[wk])
            for r in range(2):
                if r == 1 and j >= 4:
                    continue
                pk = 'pm%d' % r
                for k in range(8):
                    S.op('pe', lambda e: e.matmul(pm[r][:], lhsT=scT[:, k, r:r + 1], rhs=w[:, k, :], start=(k == 0), stop=False),
                         reads=['scT', wk], writes=[pk], acc=True)
                S.op('pe', lambda e: e.matmul(pm[r][:], lhsT=one1[:], rhs=ba[:, j * 512:(j + 1) * 512], start=False, stop=True),
                     reads=['one1', 'ba'], writes=[pk], acc=True)
                S.op('dve', lambda e: e.tensor_copy(out=mrow[r][:, j * 512:(j + 1) * 512], in_=pm[r][:]), reads=[pk], writes=['mrow%d' % r])
        S.dma('sp', lambda e: e.dma_start(out=K.modd[0:1, :], in_=mrow[0][:]), reads=['mrow0'], writes=['modd'])
        S.dma('sp', lambda e: e.dma_start(out=K.modd[1:2, 0:2048], in_=mrow[1][:, 0:2048]), reads=['mrow1'], writes=['modd'])
        S.barrier()


def phase_inproj(K):
    nc, S = K.nc, K.S
    with ExitStack() as es:
        sb = lambda n, s, d: es.enter_context(nc.sbuf_tensor(_u(n), s, d))
        ps = lambda n, s, d: es.enter_context(nc.psum_tensor(_u(n), s, d))
        wb = sb("wb", [128, 8, 2560], BF16)
        wst = [sb("wst%d" % i, [128, 8, 320], F32) for i in range(2)]
        idt = sb("idt", [128, 128], BF16)
        epst = sb("epst", [128, 1], F32)
        scp = [sb("scp%d" % r, [128, 8], F32) for r in range(2)]
        shp = [sb("shp%d" % r, [128, 8], F32) for r in range(2)]
        xt = [sb("xt%d" % i, [128, 1024], F32) for i in range(2)]
        xn = [sb("xn%d" % i, [128, 1024], BF16) for i in range(2)]
        st = sb("st", [128, 2, 6], F32)
        mv = sb("mv", [128, 2], F32)
        lnv = sb("lnv", [128, 1], F32)
        rstd = sb("rstd", [128, 1], F32)
        xmT = [sb("xmT%d" % i, [128, 8, 512], BF16) for i in range(2)]
        stg = [sb("stg%d" % i, [128, 4, 512], F32) for i in range(2)]
        cst = [sb("cst%d" % i, [64, 512], F32) for i in range(2)]
        snt = [sb("snt%d" % i, [64, 512], F32) for i in range(2)]
        kr1 = sb("kr1", [64, 512], F32)
        kr2 = sb("kr2", [64, 512], F32)
        krb = sb("krb", [64, 512], BF16)
        pt = [ps("pt%d" % i, [128, 1024], BF16) for i in range(2)]
        po = [ps("po%d" % i, [128, 512], F32) for i in range(3)]
        pk = [ps("pk%d" % i, [64, 512], F32) for i in range(2)]

        S.dma('sp', lambda e: e.dma_start(out=idt[:], in_=K.ident), writes=['idt'])
        S.op('dve', lambda e: e.memset(epst[:], 1e-5), writes=['epst'])
        for r in range(2):
            S.dma('sp', lambda e: e.dma_start(out=shp[r][:], in_=K.modd[r:r + 1, 0:1024].rearrange("o (k p) -> p (o k)", p=128), allow_slow_non_contiguous=True), reads=['modd'], writes=['shp%d' % r])
            S.dma('sp', lambda e: e.dma_start(out=scp[r][:], in_=K.modd[r:r + 1, 1024:2048].rearrange("o (k p) -> p (o k)", p=128), allow_slow_non_contiguous=True), reads=['modd'], writes=['scp%d' % r])
            S.op('dve', lambda e: e.tensor_scalar_add(out=scp[r][:], in0=scp[r][:], scalar1=1.0), reads=['scp%d' % r], writes=['scp%d' % r])
        for j in range(8):
            w = wst[j % 2]
            wk = 'wst%d' % (j % 2)
            S.dma('sp', lambda e: e.dma_start(out=w[:], in_=K.w_in[:, j * 320:(j + 1) * 320].rearrange("(k p) n -> p k n", p=128)), writes=[wk])
            S.op('pool', lambda e: e.tensor_copy(out=wb[:, :, j * 320:(j + 1) * 320], in_=w[:]), reads=[wk], writes=['wb'])

        groups = [(0, 256, 1)] + [(256 + g * 512, 512, 0) for g in range(16)]
        tile_i = 0
        ev = 0
        def gbody(gi, t0, G, r):
            nonlocal tile_i, ev
            cs_, sn_ = cst[gi % 2], snt[gi % 2]
            csk, snk = 'cst%d' % (gi % 2), 'snt%d' % (gi % 2)
            xm = xmT[gi % 2]
            xmk = 'xmT%d' % (gi % 2)
            if r == 0:
                S.dma('sp', lambda e: e.dma_start(out=cs_[:], in_=K.cosT[:, t0 - 256:t0 - 256 + 512]), writes=[csk])
                S.dma('sp', lambda e: e.dma_start(out=sn_[:], in_=K.sinT[:, t0 - 256:t0 - 256 + 512]), writes=[snk])
            for i in range(G // 128):
                sl = tile_i % 2
                tile_i += 1
                xk, xnk, ptk = 'xt%d' % sl, 'xn%d' % sl, 'pt%d' % sl
                tt = t0 + i * 128
                S.dma('sp', lambda e: e.dma_start(out=xt[sl][:], in_=K.xin[tt:tt + 128, :]), writes=[xk])
                for c in range(2):
                    S.op('dve', lambda e: e.bn_stats(out=st[:, c, :], in_=xt[sl][:, c * 512:(c + 1) * 512]), reads=[xk], writes=['st%d' % c])
                S.op('dve', lambda e: e.bn_aggr(out=mv[:], in_=st[:]), reads=['st0', 'st1'], writes=['mv'])
                S.op('act', lambda e: e.activation(out=lnv[:], in_=mv[:, 1:2], func=AF.Ln, bias=epst[:], scale=1.0), reads=['mv', 'epst'], writes=['lnv'])
                S.op('act', lambda e: e.activation(out=rstd[:], in_=lnv[:], func=AF.Exp, scale=-0.5), reads=['lnv'], writes=['rstd'])
                S.op('dve', lambda e: e.tensor_scalar(out=xn[sl][:], in0=xt[sl][:], scalar1=mv[:, 0:1], scalar2=rstd[:], op0=ALU.subtract, op1=ALU.mult),
                     reads=[xk, 'mv', 'rstd'], writes=[xnk])
                for k in range(8):
                    S.op('pe', lambda e: e.transpose(out=pt[sl][:, k * 128:(k + 1) * 128], in_=xn[sl][:, k * 128:(k + 1) * 128], identity=idt[:]),
                         reads=[xnk, 'idt'], writes=[ptk], acc=True)
                for k in range(8):
                    S.op('act', lambda e: e.activation(out=xm[:, k, i * 128:(i + 1) * 128], in_=pt[sl][:, k * 128:(k + 1) * 128], func=AF.Identity,
                                                       bias=shp[r][:, k:k + 1], scale=scp[r][:, k:k + 1]),
                         reads=[ptk, 'shp%d' % r, 'scp%d' % r], writes=[xmk])
                yield 'f'
            yield 'F'
            for cb in range(5):
                sg = stg[cb % 2]
                sgk = 'stg%d' % (cb % 2)
                ncols = 4 if cb < 4 else 3
                for cc in range(ncols):
                    ci = cb * 4 + cc
                    p = po[ev % 3]
                    pkk = 'po%d' % (ev % 3)
                    for k in range(8):
                        S.op('pe', lambda e: e.matmul(p[:, 0:G], lhsT=wb[:, k, ci * 128:(ci + 1) * 128], rhs=xm[:, k, 0:G], start=(k == 0), stop=(k == 7)),
                             reads=['wb', xmk], writes=[pkk], acc=True)
                    if ev % 2 == 0:
                        S.op('dve', lambda e: e.tensor_copy(out=sg[:, cc, 0:G], in_=p[:, 0:G]), reads=[pkk], writes=[sgk])
                    else:
                        S.op('act', lambda e: e.activation(out=sg[:, cc, 0:G], in_=p[:, 0:G], func=AF.Identity), reads=[pkk], writes=[sgk])
                    ev += 1
                r0 = cb * 512
                S.dma('pool', lambda e: e.dma_start(out=K.hT[r0:r0 + ncols * 128, t0:t0 + G].rearrange("(c p) t -> p c t", p=128), in_=sg[:, 0:ncols, 0:G]),
                      reads=[sgk], writes=['hT'])
                yield 'b'
            for q in range(2):
                for k in range(8):
                    S.op('pe', lambda e: e.matmul(pk[q][:, 0:G], lhsT=wb[:, k, 2432 + q * 64:2432 + (q + 1) * 64], rhs=xm[:, k, 0:G], start=(k == 0), stop=(k == 7)),
                         reads=['wb', xmk], writes=['pk%d' % q], acc=True)
            if r == 0:
                S.op('dve', lambda e: e.tensor_tensor(out=kr1[:], in0=pk[0][:], in1=cs_[:], op=ALU.mult), reads=['pk0', csk], writes=['kr1'])
                S.op('dve', lambda e: e.tensor_tensor(out=kr2[:], in0=pk[1][:], in1=sn_[:], op=ALU.mult), reads=['pk1', snk], writes=['kr2'])
                S.op('dve', lambda e: e.tensor_tensor(out=krb[:], in0=kr1[:], in1=kr2[:], op=ALU.add), reads=['kr1', 'kr2'], writes=['krb'])
            else:
                S.op('dve', lambda e: e.tensor_copy(out=krb[:, 0:G], in_=pk[0][:, 0:G]), reads=['pk0', 'pk1'], writes=['krb'])
            S.dma('pool', lambda e: e.dma_start(out=K.krT[:, t0:t0 + G], in_=krb[:, 0:G]), reads=['krb'], writes=['krT'])
        def step(gen_):
            try:
                return next(gen_)
            except StopIteration:
                return None
        prev = None
        for gi, (t0, G, r) in enumerate(groups):
            gcur = gbody(gi, t0, G, r)
            while True:
                rr = step(gcur)
                if prev is not None and step(prev) is None:
                    prev = None
                if rr == 'F' or rr is None:
                    break
            while prev is not None:
                if step(prev) is None:
                    prev = None
            prev = gcur
        while prev is not None:
            if step(prev) is None:
                prev = None
        S.barrier()


class Rot:
    def __init__(self, bufs, prefix):
        self.bufs = bufs
        self.prefix = prefix
        self.i = 0

    def get(self):
        j = self.i % len(self.bufs)
        self.i += 1
        return self.bufs[j], '%s%d' % (self.prefix, j)


SCALE_ATT = 192.0 ** -0.5


def phase_mlaprep(K):
    nc, S = K.nc, K.S
    with ExitStack() as es:
        sb = lambda n, s, d: es.enter_context(nc.sbuf_tensor(_u(n), s, d))
        ps = lambda n, s, d: es.enter_context(nc.psum_tensor(_u(n), s, d))
        wuq = sb("wuq", [128, 2, 1024], BF16)
        wuk = sb("wuk", [128, 2, 512], BF16)
        wuv = sb("wuv", [128, 2, 512], BF16)
        wst = [sb("mwst%d" % i, [128, 2, 512], F32) for i in range(2)]
        gq = sb("gq", [128, 2], F32)
        gkv = sb("gkv", [128, 2], F32)
        onesf = sb("onesf", [128, 128], F32)
        epst = sb("epst", [128, 1], F32)
        ql = [sb("ql%d" % i, [128, 4, 512], F32) for i in range(2)]
        sq = sb("sq", [128, 4, 512], F32)
        lnt = sb("lnt", [128, 512], F32)
        rs = [sb("rs%d" % i, [128, 512], F32) for i in range(2)]
        nb = [sb("nb%d" % i, [128, 4, 512], BF16) for i in range(2)]
        qst = [sb("qst%d" % i, [128, 4, 512], BF16) for i in range(2)]
        kst = [sb("kst%d" % i, [128, 4, 512], BF16) for i in range(2)]
        qrb = [sb("qrb%d" % i, [64, 4, 512], BF16) for i in range(2)]
        vst = Rot([sb("vst%d" % i, [128, 512], BF16) for i in range(3)], "vst")
        cst = [sb("cst%d" % i, [64, 512], F32) for i in range(2)]
        snt = [sb("snt%d" % i, [64, 512], F32) for i in range(2)]
        r1 = sb("r1", [64, 512], F32)
        r2 = sb("r2", [64, 512], F32)
        pp = Rot([ps("mp%d" % i, [128, 512], F32) for i in range(7)], "mp")

        S.op('dve', lambda e: e.memset(epst[:], 1e-6), writes=['epst'])
        S.op('dve', lambda e: e.memset(onesf[:], 1.0), writes=['onesf'])
        S.dma('sp', lambda e: e.dma_start(out=gq[:], in_=K.mla_q_norm.rearrange("o (c p) -> p (o c)", p=128), allow_slow_non_contiguous=True), writes=['gq'])
        S.dma('sp', lambda e: e.dma_start(out=gkv[:], in_=K.mla_kv_norm.rearrange("o (c p) -> p (o c)", p=128), allow_slow_non_contiguous=True), writes=['gkv'])
        wl = 0
        for (src, dst, dk, n) in [(K.w_uq, wuq, 'wuq', 1024), (K.w_uk, wuk, 'wuk', 512), (K.w_uv, wuv, 'wuv', 512)]:
            for j in range(n // 512):
                w = wst[wl % 2]
                wk = 'mwst%d' % (wl % 2)
                wl += 1
                S.dma('sp', lambda e: e.dma_start(out=w[:], in_=src[:, j * 512:(j + 1) * 512].rearrange("(c p) n -> p c n", p=128)), writes=[wk])
                S.op('pool', lambda e: e.tensor_copy(out=dst[:, :, j * 512:(j + 1) * 512], in_=w[:]), reads=[wk], writes=[dk])

        groups = [(0, 256, 1)] + [(256 + g * 512, 512, 0) for g in range(16)]
        ev = [0]

        def evac(out_ap, in_ap, reads, writes):
            if ev[0] % 2 == 0:
                S.op('dve', lambda e: e.tensor_copy(out=out_ap, in_=in_ap), reads=reads, writes=writes)
            else:
                S.op('act', lambda e: e.activation(out=out_ap, in_=in_ap, func=AF.Identity), reads=reads, writes=writes)
            ev[0] += 1

        def gbody(gi, t0, G, r):
            cs_, sn_ = cst[gi % 2], snt[gi % 2]
            csk, snk = 'cst%d' % (gi % 2), 'snt%d' % (gi % 2)
            q_ = ql[gi % 2]
            qk = 'ql%d' % (gi % 2)
            n_ = nb[gi % 2]
            nk = 'nb%d' % (gi % 2)
            S.dma('sp', lambda e: e.dma_start(out=q_[:, :, 0:G], in_=K.hT[1920:2432, t0:t0 + G].rearrange("(c p) t -> p c t", p=128)), reads=['hT'], writes=[qk])
            if r == 0:
                S.dma('sp', lambda e: e.dma_start(out=cs_[:], in_=K.cosT[:, t0 - 256:t0 - 256 + 512]), writes=[csk])
                S.dma('sp', lambda e: e.dma_start(out=sn_[:], in_=K.sinT[:, t0 - 256:t0 - 256 + 512]), writes=[snk])
            S.op('act', lambda e: e.activation(out=sq[:, :, 0:G], in_=q_[:, :, 0:G], func=AF.Square), reads=[qk], writes=['sq'])
            for pair in range(2):
                if pair == 0 and r == 1:
                    continue
                p, pk = pp.get()
                for c in range(2):
                    S.op('pe', lambda e: e.matmul(p[:, 0:G], lhsT=onesf[:], rhs=sq[:, pair * 2 + c, 0:G], start=(c == 0), stop=(c == 1)),
                         reads=['onesf', 'sq'], writes=[pk], acc=True)
                S.op('act', lambda e: e.activation(out=lnt[:, 0:G], in_=p[:, 0:G], func=AF.Ln, bias=epst[:], scale=1.0 / 256.0), reads=[pk, 'epst'], writes=['lnt'])
                S.op('act', lambda e: e.activation(out=rs[pair][:, 0:G], in_=lnt[:, 0:G], func=AF.Exp, scale=-0.5), reads=['lnt'], writes=['rs%d' % pair])
                g_ = gq if pair == 0 else gkv
                for c in range(2):
                    S.op('dve', lambda e: e.scalar_tensor_tensor(out=n_[:, pair * 2 + c, 0:G], in0=q_[:, pair * 2 + c, 0:G], scalar=g_[:, c:c + 1], in1=rs[pair][:, 0:G],
                                                                 op0=ALU.mult, op1=ALU.mult),
                         reads=[qk, 'rs%d' % pair, 'gq', 'gkv'], writes=[nk])
            yield 'F'
            if r == 0:
                tq = t0 - 256
                qs = qst[gi % 2]
                qsk = 'qst%d' % (gi % 2)
                qr_ = qrb[gi % 2]
                qrk = 'qrb%d' % (gi % 2)
                for h in range(4):
                    p, pk = pp.get()
                    for c in range(2):
                        S.op('pe', lambda e: e.matmul(p[:, 0:G], lhsT=wuq[:, c, h * 256:h * 256 + 128], rhs=n_[:, c, 0:G], start=(c == 0), stop=(c == 1)),
                             reads=['wuq', nk], writes=[pk], acc=True)
                    evac(qs[:, h, 0:G], p[:, 0:G], [pk], [qsk])
                    p1, pk1 = pp.get()
                    p2, pk2 = pp.get()
                    for c in range(2):
                        S.op('pe', lambda e: e.matmul(p1[0:64, 0:G], lhsT=wuq[:, c, h * 256 + 128:h * 256 + 192], rhs=n_[:, c, 0:G], start=(c == 0), stop=(c == 1)),
                             reads=['wuq', nk], writes=[pk1], acc=True)
                    for c in range(2):
                        S.op('pe', lambda e: e.matmul(p2[0:64, 0:G], lhsT=wuq[:, c, h * 256 + 192:h * 256 + 256], rhs=n_[:, c, 0:G], start=(c == 0), stop=(c == 1)),
                             reads=['wuq', nk], writes=[pk2], acc=True)
                    S.op('dve', lambda e: e.tensor_tensor(out=r1[:], in0=p1[0:64, :], in1=cs_[:], op=ALU.mult), reads=[pk1, csk], writes=['r1'])
                    S.op('dve', lambda e: e.tensor_tensor(out=r2[:], in0=p2[0:64, :], in1=sn_[:], op=ALU.mult), reads=[pk2, snk], writes=['r2'])
                    S.op('dve', lambda e: e.tensor_tensor(out=qr_[:, h, :], in0=r1[:], in1=r2[:], op=ALU.add), reads=['r1', 'r2'], writes=[qrk])
                    yield 'b'
                S.dma('pool', lambda e: e.dma_start(out=K.qnT[:, tq:tq + G].rearrange("(h p) t -> p h t", p=128), in_=qs[:, :, 0:G]), reads=[qsk], writes=['qnT'])
                S.dma('pool', lambda e: e.dma_start(out=K.qrT[:, tq:tq + G].rearrange("(h p) t -> p h t", p=64), in_=qr_[:, :, 0:G]), reads=[qrk], writes=['qrT'])
            ks = kst[gi % 2]
            ksk = 'kst%d' % (gi % 2)
            for h in range(4):
                p, pk = pp.get()
                for c in range(2):
                    S.op('pe', lambda e: e.matmul(p[:, 0:G], lhsT=wuk[:, c, h * 128:(h + 1) * 128], rhs=n_[:, 2 + c, 0:G], start=(c == 0), stop=(c == 1)),
                         reads=['wuk', nk], writes=[pk], acc=True)
                evac(ks[:, h, 0:G], p[:, 0:G], [pk], [ksk])
                yield 'b'
            S.dma('pool', lambda e: e.dma_start(out=K.knT[:, t0:t0 + G].rearrange("(h p) t -> p h t", p=128), in_=ks[:, :, 0:G]), reads=[ksk], writes=['knT'])
            for i in range(G // 128):
                p, pk = pp.get()
                for c in range(2):
                    S.op('pe', lambda e: e.matmul(p[:, :], lhsT=n_[:, 2 + c, i * 128:(i + 1) * 128], rhs=wuv[:, c, :], start=(c == 0), stop=(c == 1)),
                         reads=['wuv', nk], writes=[pk], acc=True)
                v_, vk = vst.get()
                evac(v_[:], p[:], [pk], [vk])
                S.dma('pool', lambda e: e.dma_start(out=K.vtok[t0 + i * 128:t0 + (i + 1) * 128, :], in_=v_[:]), reads=[vk], writes=['vtok'])
        def step(gen_):
            try:
                return next(gen_)
            except StopIteration:
                return None
        prev = None
        for gi, (t0, G, r) in enumerate(groups):
            gcur = gbody(gi, t0, G, r)
            while True:
                rr = step(gcur)
                if prev is not None and step(prev) is None:
                    prev = None
                if rr == 'F' or rr is None:
                    break
            while prev is not None:
                if step(prev) is None:
                    prev = None
            prev = gcur
        while prev is not None:
            if step(prev) is None:
                prev = None
        S.barrier()


def phase_attn(K, heads=(0, 1, 2, 3), nqt=16):
    nc, S = K.nc, K.S
    NKT = TA // 128
    with ExitStack() as es:
        sb = lambda n, s, d: es.enter_context(nc.sbuf_tensor(_u(n), s, d))
        ps = lambda n, s, d: es.enter_context(nc.psum_tensor(_u(n), s, d))
        krs = sb("krs", [64, TA], BF16)
        kn = [sb("kn%d" % i, [128, TA], BF16) for i in range(2)]
        vh = [sb("vh%d" % i, [128, NKT, 128], BF16) for i in range(2)]
        qn = [sb("qn%d" % i, [128, 512], BF16) for i in range(2)]
        qr = [sb("qr%d" % i, [64, 512], BF16) for i in range(2)]
        onesb = sb("onesb", [128, 128], BF16)
        pT = Rot([sb("pT%d" % i, [128, 512], BF16) for i in range(3)], "pT")
        rl = sb("rl", [128, 512], F32)
        ob = [sb("ob%d" % i, [128, 512], BF16) for i in range(2)]
        psc = Rot([ps("psc%d" % i, [128, 512], F32) for i in range(3)], "psc")
        pO = [ps("pO%d" % i, [128, 512], F32) for i in range(2)]
        pL = [ps("pL%d" % i, [128, 512], F32) for i in range(2)]

        S.op('dve', lambda e: e.memset(onesb[:], 1.0), writes=['onesb'])
        S.dma('sp', lambda e: e.dma_start(out=krs[:], in_=K.krT), reads=['krT'], writes=['krs'])
        qi = 0
        for hi, h in enumerate(heads):
            k_ = kn[hi % 2]
            kk = 'kn%d' % (hi % 2)
            v_ = vh[hi % 2]
            vk = 'vh%d' % (hi % 2)
            S.dma('sp', lambda e: e.dma_start(out=k_[:], in_=K.knT[h * 128:(h + 1) * 128, :]), reads=['knT'], writes=[kk])
            S.dma('sp', lambda e: e.dma_start(out=v_[:], in_=K.vtok[:, h * 128:(h + 1) * 128].rearrange("(kt p) d -> p kt d", p=128)), reads=['vtok'], writes=[vk])
            for qt in range(nqt):
                sl = qi % 2
                qi += 1
                qnk, qrk = 'qn%d' % sl, 'qr%d' % sl
                S.dma('sp', lambda e: e.dma_start(out=qn[sl][:], in_=K.qnT[h * 128:(h + 1) * 128, qt * 512:(qt + 1) * 512]), reads=['qnT'], writes=[qnk])
                S.dma('sp', lambda e: e.dma_start(out=qr[sl][:], in_=K.qrT[h * 64:(h + 1) * 64, qt * 512:(qt + 1) * 512]), reads=['qrT'], writes=[qrk])
                Ok, Lk = 'pO%d' % sl, 'pL%d' % sl
                pend = []

                def scores(kt):
                    p, pk = psc.get()
                    S.op('pe', lambda e: e.matmul(p[:], lhsT=k_[:, kt * 128:(kt + 1) * 128], rhs=qn[sl][:], start=True, stop=False),
                         reads=[kk, qnk], writes=[pk], acc=True)
                    S.op('pe', lambda e: e.matmul(p[:], lhsT=krs[:, kt * 128:(kt + 1) * 128], rhs=qr[sl][:], start=False, stop=True),
                         reads=['krs', qrk], writes=[pk], acc=True)
                    t_, tk = pT.get()
                    S.op('act', lambda e: e.activation(out=t_[:], in_=p[:], func=AF.Exp, scale=SCALE_ATT), reads=[pk], writes=[tk])
                    pend.append((kt, t_, tk))

                def pv():
                    kt, t_, tk = pend.pop(0)
                    S.op('pe', lambda e: e.matmul(pO[sl][:], lhsT=v_[:, kt, :], rhs=t_[:], start=(kt == 0), stop=(kt == NKT - 1)),
                         reads=[vk, tk], writes=[Ok], acc=True)
                    S.op('pe', lambda e: e.matmul(pL[sl][:], lhsT=onesb[:], rhs=t_[:], start=(kt == 0), stop=(kt == NKT - 1)),
                         reads=['onesb', tk], writes=[Lk], acc=True)

                scores(0)
                scores(1)
                for kt in range(NKT):
                    pv()
                    if kt + 2 < NKT:
                        scores(kt + 2)
                S.op('dve', lambda e: e.reciprocal(out=rl[:], in_=pL[sl][:]), reads=[Lk], writes=['rl'])
                S.op('dve', lambda e: e.tensor_tensor(out=ob[sl][:], in0=pO[sl][:], in1=rl[:], op=ALU.mult), reads=[Ok, 'rl'], writes=['ob%d' % sl])
                S.dma('pool', lambda e: e.dma_start(out=K.mixT[512 + h * 128:512 + (h + 1) * 128, qt * 512:(qt + 1) * 512], in_=ob[sl][:]), reads=['ob%d' % sl], writes=['mixT'])
        S.barrier()


F32R = mybir.dt.float32r
LDS = -0.6065306597126334


class LaneS:
    LOCAL = ('cv', 'tw', 'sg', 'kq', 'sq', 'lnt', 'kk_', 'av', 'tt', 'kd0', 'kd1', 'bb', 'gg', 'bc', 'bon')

    def __init__(self, S, lane):
        self.S = S
        self.lane = lane

    def k(self, x):
        if x == 'bones':
            return x
        for p in self.LOCAL:
            if x.startswith(p):
                return '%s@%d' % (x, self.lane)
        return x

    def op(self, e, fn, reads=(), writes=(), acc=False):
        return self.S.op(e, fn, [self.k(x) for x in reads], [self.k(x) for x in writes], acc)

    def dma(self, e, fn, reads=(), writes=()):
        return self.S.dma(e, fn, [self.k(x) for x in reads], [self.k(x) for x in writes])


def phase_rwprep(K):
    nc, S = K.nc, K.S
    with ExitStack() as es:
        sb = lambda n, s, d: es.enter_context(nc.sbuf_tensor(_u(n), s, d))
        ps = lambda n, s, d: es.enter_context(nc.psum_tensor(_u(n), s, d))
        G = 256
        cw = sb("cw", [128, 3, 12], F32)
        kkv = sb("kkv", [128, 4], F32)
        kav = sb("kav", [128, 4], F32)
        omka = sb("omka", [128, 4], F32)
        rkv = sb("rkv", [128, 4], F32)
        a0v = sb("a0v", [128, 2, 4], F32)
        w0r = sb("w0r", [1, 2, 512], F32)
        ones1 = sb("ones1", [1, 128], F32)
        wup = sb("wup", [64, 2, 512], F32)
        aup = sb("aup", [64, 2, 512], F32)
        gup = sb("gup", [128, 512], F32)
        bones = sb("bones", [128, 128], F32)
        idf = sb("idf", [128, 128], F32)
        eps12 = sb("eps12", [128, 1], F32)
        hr = [sb("hr%d" % i, [128, 12, G + 2], F32) for i in range(2)]
        lo = [sb("lo%d" % i, [64, 4, G], F32) for i in range(2)]
        gd = [sb("gd%d" % i, [128, G], F32) for i in range(2)]
        cvL = [sb("cv_%d" % i_, [128, 12, G], F32) for i_ in range(2)]
        twL = [sb("tw_%d" % i_, [64, 2, G], F32) for i_ in range(2)]
        sgL = [sb("sg_%d" % i_, [128, G], F32) for i_ in range(2)]
        kqL = [sb("kq_%d" % i_, [128, 4, G], F32) for i_ in range(2)]
        sqL = [sb("sq_%d" % i_, [128, 4, G], F32) for i_ in range(2)]
        lntL = [sb("lnt_%d" % i_, [128, 4, G], F32) for i_ in range(2)]
        kk_L = [sb("kk__%d" % i_, [128, 4, G], F32) for i_ in range(2)]
        avL = [sb("av_%d" % i_, [128, 4, G], F32) for i_ in range(2)]
        ttL = [sb("tt_%d" % i_, [128, 4, G], F32) for i_ in range(2)]
        kdL = [[sb("kd%d_%d" % (i, i_), [128, 4, G], F32) for i in range(2)] for i_ in range(2)]
        bbL = [sb("bb_%d" % i_, [128, 4, G], F32) for i_ in range(2)]
        ld = Rot([sb("ld%d" % i, [128, 512], F32) for i in range(2)], "ld")
        vt = Rot([sb("vt%d" % i, [128, 512], F32) for i in range(2)], "vt")
        ggL = [sb("gg_%d" % i_, [128, 4, G], F32) for i_ in range(2)]
        bcL = [sb("bc_%d" % i_, [128, 4, G], F32) for i_ in range(2)]
        bonL = [sb("bon_%d" % i_, [128, 4, G], F32) for i_ in range(2)]
        pp = Rot([ps("rp%d" % i, [128, 512], F32) for i in range(7)], "rp")

        ld1 = lambda dst, src, key: S.dma('sp', lambda e: e.dma_start(out=dst, in_=src, allow_slow_non_contiguous=True), writes=[key])
        ld1(cw[:], K.rwkv_conv.rearrange("t (c p) -> p t c", p=128), 'cw')
        ld1(kkv[:], K.rwkv_k_k.rearrange("o (c p) -> p (o c)", p=128), 'kkv')
        ld1(kav[:], K.rwkv_k_a.rearrange("o (c p) -> p (o c)", p=128), 'kav')
        ld1(rkv[:], K.rwkv_r_k.rearrange("o (c p) -> p (o c)", p=128), 'rkv')
        ld1(a0v[:], K.rwkv_a0.rearrange("d (c p) -> p d c", p=128), 'a0v')
        ld1(w0r[:], K.rwkv_w0.rearrange("(o d) n -> o d n", o=1), 'w0r')
        ld1(wup[:], K.rwkv_w_up.rearrange("d l n -> l d n"), 'wup')
        ld1(aup[:], K.rwkv_a_up.rearrange("d l n -> l d n"), 'aup')
        ld1(gup[:], K.rwkv_g_up, 'gup')
        ld1(bones[:], K.bones, 'bones')
        ld1(idf[:], K.identf, 'idf')
        S.op('dve', lambda e: e.memset(ones1[:], 1.0), writes=['ones1'])
        S.op('dve', lambda e: e.memset(eps12[:], 1e-12), writes=['eps12'])
        S.op('dve', lambda e: e.tensor_scalar(out=omka[:], in0=kav[:], scalar1=-1.0, scalar2=1.0, op0=ALU.mult, op1=ALU.add), reads=['kav'], writes=['omka'])

        nblk = TA // G
        def bbody(bi):
            ln = bi % 2
            S_ = LaneS(S, ln)
            cv, tw, sg, kq, sq, lnt, kk_, av, tt, bb, gg, bc, bon = cvL[ln], twL[ln], sgL[ln], kqL[ln], sqL[ln], lntL[ln], kk_L[ln], avL[ln], ttL[ln], bbL[ln], ggL[ln], bcL[ln], bonL[ln]
            kd = kdL[ln]
            t0 = bi * G
            lat = t0 >= TC
            sl = bi % 2
            h_ = hr[sl]
            hk = 'hr%d' % sl
            first = (t0 == 0 or t0 == TC)
            last = (t0 + G == TC or t0 + G == TA)
            c0 = 1 if first else 0
            c1 = G + 1 if last else G + 2
            if first:
                S_.op('pool', lambda e: e.memset(h_[:, :, 0:1], 0.0), writes=[hk])
            if last:
                S_.op('pool', lambda e: e.memset(h_[:, :, G + 1:G + 2], 0.0), writes=[hk])
            for q in range(3):
                S_.dma('sp', lambda e: e.dma_start(out=h_[:, q * 4:(q + 1) * 4, c0:c1], in_=K.hT[q * 512:(q + 1) * 512, t0 - 1 + c0:t0 - 1 + c1].rearrange("(c p) t -> p c t", p=128)),
                      reads=['hT'], writes=[hk])
            lo_ = lo[sl]
            lk = 'lo%d' % sl
            S_.dma('sp', lambda e: e.dma_start(out=lo_[:], in_=K.hT[1536:1792, t0:t0 + G].rearrange("(c p) t -> p c t", p=64)), reads=['hT'], writes=[lk])
            gd_ = gd[sl]
            gk = 'gd%d' % sl
            if lat:
                S_.dma('sp', lambda e: e.dma_start(out=gd_[:], in_=K.hT[1792:1920, t0:t0 + G]), reads=['hT'], writes=[gk])
            for c in range(12):
                S_.op('act', lambda e: e.activation(out=cv[:, c, :], in_=h_[:, c, 1:G + 1], func=AF.Identity, scale=cw[:, 1, c:c + 1]), reads=[hk, 'cw'], writes=['cv%d' % c])
                S_.op('dve', lambda e: e.scalar_tensor_tensor(out=cv[:, c, :], in0=h_[:, c, 0:G], scalar=cw[:, 0, c:c + 1], in1=cv[:, c, :], op0=ALU.mult, op1=ALU.add),
                     reads=[hk, 'cw', 'cv%d' % c], writes=['cv%d' % c])
                S_.op('dve', lambda e: e.scalar_tensor_tensor(out=cv[:, c, :], in0=h_[:, c, 2:G + 2], scalar=cw[:, 2, c:c + 1], in1=cv[:, c, :], op0=ALU.mult, op1=ALU.add),
                     reads=[hk, 'cw', 'cv%d' % c], writes=['cv%d' % c])
            cvr = ['cv%d' % c for c in range(0, 4)]
            cvk = ['cv%d' % c for c in range(4, 8)]
            cvv = ['cv%d' % c for c in range(8, 12)]
            S_.dma('pool', lambda e: e.dma_start(out=K.rwR[:, t0:t0 + G].rearrange("(c p) t -> p c t", p=128), in_=cv[:, 0:4, :]), reads=cvr, writes=['rwR'])
            S_.dma('pool', lambda e: e.dma_start(out=K.rwV[:, t0:t0 + G].rearrange("(c p) t -> p c t", p=128), in_=cv[:, 8:12, :]), reads=cvv, writes=['rwV'])
            yield 'f'
            for i in range(G // 128):
                p, pk = pp.get()
                for c in range(4):
                    S_.op('pe', lambda e: e.transpose(out=p[:, c * 128:(c + 1) * 128], in_=cv[:, 8 + c, i * 128:(i + 1) * 128], identity=idf[:]), reads=cvv + ['idf'], writes=[pk], acc=True)
                v_, vk = vt.get()
                S_.op('act', lambda e: e.activation(out=v_[:], in_=p[:], func=AF.Identity), reads=[pk], writes=[vk])
                S_.dma('pool', lambda e: e.dma_start(out=K.rwVtok[t0 + i * 128:t0 + (i + 1) * 128, :], in_=v_[:]), reads=[vk], writes=['rwVtok'])
                yield 'f'
            for c in range(4):
                S_.op('dve', lambda e: e.tensor_scalar_mul(out=kq[:, c, :], in0=cv[:, 4 + c, :], scalar1=kkv[:, c:c + 1]), reads=cvk + ['kkv'], writes=['kq'])
            S_.op('act', lambda e: e.activation(out=sq[:], in_=kq[:], func=AF.Square), reads=['kq'], writes=['sq'])
            for c2 in range(2):
                p, pk = pp.get()
                S_.op('pe', lambda e: e.matmul(p[:, 0:2 * G], lhsT=bones[:], rhs=sq[:, 2 * c2:2 * c2 + 2, :], start=True, stop=True), reads=['bones', 'sq'], writes=[pk])
                S_.op('act', lambda e: e.activation(out=lnt[:, 2 * c2:2 * c2 + 2, :], in_=p[:, 0:2 * G], func=AF.Ln, bias=eps12[:], scale=1.0), reads=[pk, 'eps12'], writes=['lnt'])
            S_.op('act', lambda e: e.activation(out=lnt[:], in_=lnt[:], func=AF.Exp, scale=-0.5), reads=['lnt'], writes=['lnt'])
            S_.op('dve', lambda e: e.tensor_tensor(out=kk_[:], in0=kq[:], in1=lnt[:], op=ALU.mult), reads=['kq', 'lnt'], writes=['kk_'])
            S_.dma('pool', lambda e: e.dma_start(out=K.rwKK[:, t0:t0 + G].rearrange("(c p) t -> p c t", p=128), in_=kk_[:]), reads=['kk_'], writes=['rwKK'])
            yield 'F'
            S_.op('act', lambda e: e.activation(out=tw[:], in_=lo_[:, 0:2, :], func=AF.Tanh), reads=[lk], writes=['tw'])
            for d in range(2):
                for i in range(G // 128):
                    p, pk = pp.get()
                    S_.op('pe', lambda e: e.matmul(p[:], lhsT=tw[:, d, i * 128:(i + 1) * 128], rhs=wup[:, d, :], start=True, stop=False), reads=['tw', 'wup'], writes=[pk], acc=True)
                    S_.op('pe', lambda e: e.matmul(p[:], lhsT=ones1[:], rhs=w0r[:, d, :], start=False, stop=True), reads=['ones1', 'w0r'], writes=[pk], acc=True)
                    l_, lk2 = ld.get()
                    S_.op('act', lambda e: e.activation(out=l_[:], in_=p[:], func=AF.Sigmoid), reads=[pk], writes=[lk2])
                    S_.dma('pool', lambda e: e.dma_start(out=K.rwLD[d][t0 + i * 128:t0 + (i + 1) * 128, :], in_=l_[:]), reads=[lk2], writes=['rwLD%d' % d])
                    yield 'b'
                for c in range(4):
                    p, pk = pp.get()
                    S_.op('pe', lambda e: e.matmul(p[:, 0:G], lhsT=aup[:, d, c * 128:(c + 1) * 128], rhs=lo_[:, 2 + d, :], start=True, stop=True), reads=['aup', lk], writes=[pk])
                    S_.op('act', lambda e: e.activation(out=av[:, c, :], in_=p[:, 0:G], func=AF.Sigmoid, bias=a0v[:, d, c:c + 1], scale=1.0), reads=[pk, 'a0v'], writes=['av'])
                    S_.op('dve', lambda e: e.tensor_scalar(out=tt[:, c, :], in0=av[:, c, :], scalar1=kav[:, c:c + 1], scalar2=omka[:, c:c + 1], op0=ALU.mult, op1=ALU.add),
                         reads=['av', 'kav', 'omka'], writes=['tt'])
                S_.op('dve', lambda e: e.tensor_tensor(out=kd[d][:], in0=cv[:, 4:8, :], in1=tt[:], op=ALU.mult), reads=cvk + ['tt'], writes=['kd%d' % d])
                S_.op('dve', lambda e: e.tensor_tensor(out=bb[:], in0=kk_[:], in1=av[:], op=ALU.mult), reads=['kk_', 'av'], writes=['bb'])
                S_.dma('pool', lambda e: e.dma_start(out=K.rwKD[d][:, t0:t0 + G].rearrange("(c p) t -> p c t", p=128), in_=kd[d][:]), reads=['kd%d' % d], writes=['rwKD%d' % d])
                S_.dma('pool', lambda e: e.dma_start(out=K.rwB[d][:, t0:t0 + G].rearrange("(c p) t -> p c t", p=128), in_=bb[:]), reads=['bb'], writes=['rwB%d' % d])
                yield 'b'
            if lat:
                tl = t0 - TC
                S_.op('act', lambda e: e.activation(out=sg[:], in_=gd_[:], func=AF.Sigmoid), reads=[gk], writes=['sg'])
                for c in range(4):
                    p, pk = pp.get()
                    S_.op('pe', lambda e: e.matmul(p[:, 0:G], lhsT=gup[:, c * 128:(c + 1) * 128], rhs=sg[:], start=True, stop=True), reads=['gup', 'sg'], writes=[pk])
                    S_.op('act', lambda e: e.activation(out=gg[:, c, :], in_=p[:, 0:G], func=AF.Identity), reads=[pk], writes=['gg'])
                S_.dma('pool', lambda e: e.dma_start(out=K.rwG[:, tl:tl + G].rearrange("(c p) t -> p c t", p=128), in_=gg[:]), reads=['gg'], writes=['rwG'])
                yield 'b'
                S_.op('dve', lambda e: e.tensor_tensor(out=bc[:], in0=kd[0][:], in1=kd[1][:], op=ALU.add), reads=['kd0', 'kd1'], writes=['bc'])
                S_.op('dve', lambda e: e.tensor_tensor(out=bc[:], in0=bc[:], in1=cv[:, 0:4, :], op=ALU.mult), reads=['bc'] + cvr, writes=['bc'])
                for c in range(4):
                    S_.op('dve', lambda e: e.tensor_scalar_mul(out=bc[:, c, :], in0=bc[:, c, :], scalar1=rkv[:, c:c + 1]), reads=['bc', 'rkv'], writes=['bc'])
                for c2 in range(2):
                    p, pk = pp.get()
                    S_.op('pe', lambda e: e.matmul(p[:, 0:2 * G], lhsT=bones[:], rhs=bc[:, 2 * c2:2 * c2 + 2, :], start=True, stop=True), reads=['bones', 'bc'], writes=[pk])
                    S_.op('dve', lambda e: e.tensor_tensor(out=bon[:, 2 * c2:2 * c2 + 2, :], in0=p[:, 0:2 * G], in1=cv[:, 8 + 2 * c2:8 + 2 * c2 + 2, :], op=ALU.mult), reads=[pk] + cvv, writes=['bon'])
                S_.dma('pool', lambda e: e.dma_start(out=K.rwBON[:, tl:tl + G].rearrange("(c p) t -> p c t", p=128), in_=bon[:]), reads=['bon'], writes=['rwBON'])
        def step(gen_):
            try:
                return next(gen_)
            except StopIteration:
                return None
        prev = None
        for bi in range(nblk):
            gcur = bbody(bi)
            while True:
                r = step(gcur)
                if prev is not None and step(prev) is None:
                    prev = None
                if r == 'F' or r is None:
                    break
            while prev is not None:
                if step(prev) is None:
                    prev = None
            prev = gcur
        while prev is not None:
            if step(prev) is None:
                prev = None
        S.barrier()


def rw_consts():
    i = np.arange(128)[:, None]
    t = np.arange(128)[None, :]
    bd = (i // 64) == (t // 64)
    out = {}
    blk = ((i // 64) == (t // 64)).astype(np.float32)
    for d in range(2):
        strict = (bd & ((i < t) if d == 0 else (i > t))).astype(np.float32)
        incl = (bd & ((i <= t) if d == 0 else (i >= t))).astype(np.float32)
        m1 = np.concatenate([-strict, incl, blk], 1)
        m2 = np.concatenate([incl, blk], 1)
        m3 = np.concatenate([-strict.T, -strict.T, -np.ones((128, 64), np.float32)], 1)
        cum = np.float32(LDS) * np.concatenate([incl, strict, strict.T], 1)
        out['rwm%d' % d] = np.ascontiguousarray(np.concatenate([m1, m2, m3, cum], 1).astype(np.float32))
    out['i64dbl'] = np.ascontiguousarray((np.arange(128)[:, None] % 64 == np.arange(128)[None, :] % 64).astype(np.float32))
    out['bones'] = np.ascontiguousarray(bd.astype(np.float32))
    out['identf'] = np.eye(128, dtype=np.float32)
    return out


GN_EPS = 64e-5


def phase_rwscan(K, ntile_lat=64):
    nc, S = K.nc, K.S
    G = 128
    with ExitStack() as es:
        sb = lambda n, s, d: es.enter_context(nc.sbuf_tensor(_u(n), s, d))
        ps = lambda n, s, d: es.enter_context(nc.psum_tensor(_u(n), s, d))
        mk = sb("mk", [128, 1344], F32)
        idr = sb("idr", [128, 128], F32R)
        i64r = sb("i64r", [128, 128], F32R)
        i64f = sb("i64f", [128, 128], F32)
        idf = sb("idf", [128, 128], F32)
        o64 = sb("o64", [64, 64], F32)
        gng = sb("gng", [64, 8], F32)
        gnb = sb("gnb", [64, 8], F32)
        epsg = sb("epsg", [64, 1], F32)
        raw = [[sb("raw%d_%d" % (j, i), [128, 4, G], F32) for i in range(2)] for j in range(4)]
        vraw = [sb("vraw%d" % i, [128, 512], F32) for i in range(2)]
        ldt = [sb("ldt%d" % i, [128, 512], F32) for i in range(2)]
        vtr = [sb("vtr%d" % i, [128, 512], F32R) for i in range(2)]
        Ep = sb("Ep", [128, 4, G], F32)
        Em = sb("Em", [128, 4, G], F32)
        Ex = sb("Ex", [128, 4, G], F32)
        Ea = sb("Ea", [128, 4, G], F32)
        KRG = sb("KRG", [128, 4, 3, 128], F32R)
        BK = sb("BK", [128, 4, 2, 128], F32R)
        KB2 = sb("KB2", [128, 4, 2, 128], F32R)
        NS = 4
        La = [sb("La%d" % i, [128, 384], F32R) for i in range(NS)]
        Lb = [sb("Lb%d" % i, [128, 384], F32R) for i in range(NS)]
        ATa = [sb("ATa%d" % i, [128, 128], F32R) for i in range(NS)]
        ATb = [sb("ATb%d" % i, [128, 128], F32R) for i in range(NS)]
        X1 = [sb("X1%d" % i, [128, 320], F32R) for i in range(NS)]
        X2 = [sb("X2%d" % i, [128, 256], F32R) for i in range(NS)]
        Xf = [sb("Xf%d" % i, [128, 256], F32R) for i in range(NS)]
        QG1 = [sb("QG1_%d" % i, [64, 8, 256], F32R) for i in range(2)]
        QG2 = [sb("QG2_%d" % i, [128, 8, 256], F32R) for i in range(2)]
        Sth = sb("Sth", [64, 3, 8, 64], F32R)
        T = [sb("T%d" % i, [64, 8, G], F32) for i in range(4)]
        outb = sb("outb", [64, 8, G], BF16)
        pp = Rot([ps("sp%d" % i, [128, 512], F32) for i in range(7)], "sp")
        pst = ps("pst", [64, 512], F32)

        ld1 = lambda dst, src, key: S.dma('sp', lambda e: e.dma_start(out=dst, in_=src, allow_slow_non_contiguous=True), writes=[key])
        ld1(idf[:], K.identf, 'idf')
        ld1(i64f[:], K.i64dbl, 'i64f')
        ld1(gng[:], K.rwkv_gn_g.rearrange("o (h p) -> p (o h)", p=64), 'gng')
        ld1(gnb[:], K.rwkv_gn_b.rearrange("o (h p) -> p (o h)", p=64), 'gnb')
        S.op('dve', lambda e: e.tensor_copy(out=idr[:], in_=idf[:]), reads=['idf'], writes=['idr'])
        S.op('dve', lambda e: e.tensor_copy(out=i64r[:], in_=i64f[:]), reads=['i64f'], writes=['i64r'])
        S.op('dve', lambda e: e.memset(o64[:], 1.0 / 64.0), writes=['o64'])
        S.op('dve', lambda e: e.memset(epsg[:], GN_EPS), writes=['epsg'])
        zt = sb("zt", [64, 512], F32)
        S.op('dve', lambda e: e.memset(zt[:], 0.0), writes=['zt'])

        evq = [0]

        def evac(out_ap, in_ap, reads, writes):
            if evq[0] % 2 == 0:
                S.op('act', lambda e: e.activation(out=out_ap, in_=in_ap, func=AF.Identity), reads=reads, writes=writes)
            else:
                S.op('dve', lambda e: e.tensor_copy(out=out_ap, in_=in_ap), reads=reads, writes=writes)
            evq[0] += 1

        fl = lambda a: a[:, :, :].rearrange("p h t -> p (h t)")
        bcount = 0
        for d in range(2):
            m1 = mk[:, 0:384]
            m2 = mk[:, 384:640]
            m3 = mk[:, 640:960]
            cum = mk[:, 960:1344]
            S.dma('sp', lambda e: e.dma_start(out=mk[:], in_=K.rwm[d]), writes=['mk'])
            S.op('dve', lambda e: e.tensor_copy(out=Sth[:, 0].rearrange("p h v -> p (h v)"), in_=zt[:]), reads=['zt'], writes=['Sth0'])
            if d == 0:
                blocks = [0, 1] + list(range(2, 2 + ntile_lat))
            else:
                blocks = [1, 0] + list(range(1 + ntile_lat, 1, -1))
            chunks = [0, 1] if d == 0 else [1, 0]
            def body(b, qs):
                nonlocal bcount
                t0 = b * G
                lat = b >= 2
                sl = bcount % 2
                bcount += 1
                srcs = [K.rwR, K.rwKD[d], K.rwKK, K.rwB[d]]
                rk = ['raw%d_%d' % (j, sl) for j in range(4)]
                for j in range(4):
                    S.dma('sp', lambda e: e.dma_start(out=raw[j][sl][:], in_=srcs[j][:, t0:t0 + G].rearrange("(c p) t -> p c t", p=128)), writes=[rk[j]])
                S.dma('sp', lambda e: e.dma_start(out=vraw[sl][:], in_=K.rwVtok[t0:t0 + G, :]), writes=['vraw%d' % sl])
                S.dma('sp', lambda e: e.dma_start(out=ldt[sl][:], in_=K.rwLD[d][t0:t0 + G, :]), writes=['ldt%d' % sl])
                r_, kd_, kk_, b_ = [raw[j][sl] for j in range(4)]
                S.op('act', lambda e: e.activation(out=vtr[qs][:], in_=vraw[sl][:], func=AF.Identity), reads=['vraw%d' % sl], writes=['vtr%d' % qs])
                banks = [pp.get() for _ in range(3)]
                for c in range(4):
                    for q in range(3):
                        S.op('pe', lambda e: e.matmul(banks[q][0][:, c * 128:(c + 1) * 128], lhsT=ldt[sl][:, c * 128:(c + 1) * 128], rhs=cum[:, q * 128:(q + 1) * 128], start=True, stop=True),
                             reads=['ldt%d' % sl, 'mk'], writes=[banks[q][1]], acc=True)
                v3 = lambda bank: bank[:, :].rearrange("p (c t) -> p c t", c=4)
                S.op('act', lambda e: e.activation(out=Ep[:], in_=v3(banks[0][0]), func=AF.Exp), reads=[banks[0][1]], writes=['Ep'])
                S.op('act', lambda e: e.activation(out=Em[:], in_=v3(banks[0][0]), func=AF.Exp, scale=-1.0), reads=[banks[0][1]], writes=['Em'])
                S.op('act', lambda e: e.activation(out=Ex[:], in_=v3(banks[1][0]), func=AF.Exp), reads=[banks[1][1]], writes=['Ex'])
                S.op('act', lambda e: e.activation(out=Ea[:], in_=v3(banks[2][0]), func=AF.Exp), reads=[banks[2][1]], writes=['Ea'])
                yield 'f'
                S.op('dve', lambda e: e.tensor_tensor(out=KRG[:, :, 0, :], in0=kk_[:], in1=Ex[:], op=ALU.mult), reads=[rk[2], 'Ex'], writes=['KRG'])
                S.op('dve', lambda e: e.tensor_tensor(out=KRG[:, :, 1, :], in0=r_[:], in1=Ep[:], op=ALU.mult), reads=[rk[0], 'Ep'], writes=['KRG'])
                S.op('dve', lambda e: e.tensor_tensor(out=BK[:, :, 0, :], in0=b_[:], in1=Em[:], op=ALU.mult), reads=[rk[3], 'Em'], writes=['BK'])
                S.op('dve', lambda e: e.tensor_tensor(out=BK[:, :, 1, :], in0=kd_[:], in1=Em[:], op=ALU.mult), reads=[rk[1], 'Em'], writes=['BK'])
                S.op('dve', lambda e: e.tensor_tensor(out=KB2[:, :, 0, :], in0=kd_[:], in1=Ea[:], op=ALU.mult), reads=[rk[1], 'Ea'], writes=['KB2'])
                S.op('dve', lambda e: e.tensor_tensor(out=KB2[:, :, 1, :], in0=b_[:], in1=Ea[:], op=ALU.mult), reads=[rk[3], 'Ea'], writes=['KB2'])
                for cc in range(2):
                    pos = cc * 64 + (63 if d == 0 else 0)
                    in0 = i64f[:, cc * 64:(cc + 1) * 64].unsqueeze(1).to_broadcast([128, 4, 64])
                    in1 = Ep[:, :, pos:pos + 1].to_broadcast([128, 4, 64])
                    S.op('dve', lambda e: e.tensor_tensor(out=KRG[:, :, 2, cc * 64:(cc + 1) * 64], in0=in0, in1=in1, op=ALU.mult), reads=['i64f', 'Ep'], writes=['KRG'])

                for g0 in range(0, 8, NS):
                    grp = list(range(g0, g0 + NS))
                    cur = {}
                    for s_, h in enumerate(grp):
                        c, pb = h // 2, 64 * (h % 2)
                        fm = lambda arr, k0, k1: arr[pb:pb + 64, c, k0:k1, :].rearrange("p k t -> p (k t)")
                        p1, k1 = pp.get()
                        S.op('pe', lambda e: e.matmul(p1[:, 0:256], lhsT=fm(BK, 0, 1), rhs=fm(KRG, 0, 2), start=True, stop=True), reads=['BK', 'KRG'], writes=[k1], acc=True)
                        S.op('pe', lambda e: e.matmul(p1[:, 256:384], lhsT=fm(KB2, 1, 2), rhs=i64r[pb:pb + 64, :], start=True, stop=True), reads=['KB2', 'i64r'], writes=[k1], acc=True)
                        S.op('dve', lambda e: e.tensor_tensor(out=La[s_][:], in0=p1[:, 0:384], in1=m1, op=ALU.mult), reads=[k1, 'mk'], writes=['La%d' % s_])
                        p2, k2 = pp.get()
                        S.op('pe', lambda e: e.matmul(p2[:, 0:128], lhsT=fm(BK, 1, 2), rhs=fm(KRG, 1, 2), start=True, stop=True), reads=['BK', 'KRG'], writes=[k2], acc=True)
                        S.op('pe', lambda e: e.matmul(p2[:, 128:256], lhsT=fm(KB2, 0, 1), rhs=i64r[pb:pb + 64, :], start=True, stop=True), reads=['KB2', 'i64r'], writes=[k2], acc=True)
                        S.op('dve', lambda e: e.tensor_tensor(out=X2[s_][:], in0=p2[:, 0:256], in1=m2, op=ALU.mult), reads=[k2, 'mk'], writes=['X2%d' % s_])
                        p3, k3 = pp.get()
                        S.op('pe', lambda e: e.matmul(p3[:, 0:256], lhsT=fm(KRG, 0, 1), rhs=fm(BK, 0, 2), start=True, stop=True), reads=['BK', 'KRG'], writes=[k3], acc=True)
                        S.op('pe', lambda e: e.matmul(p3[:, 256:320], lhsT=fm(KRG, 0, 1), rhs=i64r[pb:pb + 64, 0:64], start=True, stop=True), reads=['KRG', 'i64r'], writes=[k3], acc=True)
                        S.op('dve', lambda e: e.tensor_tensor(out=X1[s_][:], in0=p3[:, 0:320], in1=m3, op=ALU.mult), reads=[k3, 'mk'], writes=['X1%d' % s_])
                        cur[s_] = (La[s_], 'La%d' % s_, X1[s_][:, 0:128], 'X1%d' % s_)
                        yield 'f'
                    for lev in range(5):
                        pend = []
                        nxt = {}
                        for s_ in range(NS):
                            L, Lk, AT, ATk = cur[s_]
                            p, pk = pp.get()
                            S.op('pe', lambda e: e.matmul(p[:, 0:384], lhsT=AT, rhs=L[:, 0:384], start=True, stop=False), reads=[Lk, ATk], writes=[pk], acc=True)
                            S.op('pe', lambda e: e.matmul(p[:, 128:384], lhsT=idr[:], rhs=L[:, 128:384], start=False, stop=True), reads=[Lk, 'idr'], writes=[pk], acc=True)
                            pend.append((p, pk))
                        yield 'f'
                        for s_ in range(NS):
                            p, pk = pend[s_]
                            Ln_ = Lb[s_] if lev % 2 == 0 else La[s_]
                            Lnk = ('Lb%d' if lev % 2 == 0 else 'La%d') % s_
                            evac(Ln_[:], p[:, 0:384], [pk], [Lnk])
                            nxt[s_] = (Ln_, Lnk)
                        for s_ in range(NS):
                            Ln_, Lnk = nxt[s_]
                            p, pk = pend[s_]
                            S.op('pe', lambda e: e.transpose(out=p[:, 384:512], in_=Ln_[:, 0:128].bitcast(F32), identity=idf[:]), reads=[Lnk, 'idf'], writes=[pk])
                        yield 'f'
                        for s_ in range(NS):
                            Ln_, Lnk = nxt[s_]
                            p, pk = pend[s_]
                            ATn = ATa[s_] if lev % 2 == 0 else ATb[s_]
                            ATnk = ('ATa%d' if lev % 2 == 0 else 'ATb%d') % s_
                            evac(ATn[:], p[:, 384:512], [pk], [ATnk])
                            cur[s_] = (Ln_, Lnk, ATn[:], ATnk)
                    for s_, h in enumerate(grp):
                        L, Lk, AT, ATk = cur[s_]
                        p, pk = pp.get()
                        S.op('pe', lambda e: e.matmul(p[:, 0:256], lhsT=AT, rhs=L[:, 128:384], start=True, stop=False), reads=[Lk, ATk], writes=[pk], acc=True)
                        S.op('pe', lambda e: e.matmul(p[:, 0:256], lhsT=idr[:], rhs=L[:, 128:384], start=False, stop=True), reads=[Lk, 'idr'], writes=[pk], acc=True)
                        evac(Xf[s_][:], p[:, 0:256], [pk], ['Xf%d' % s_])
                    yield 'f'
                    for s_, h in enumerate(grp):
                        c, pb = h // 2, 64 * (h % 2)
                        p, pk = pp.get()
                        S.op('pe', lambda e: e.matmul(p[:, 0:256], lhsT=idr[:], rhs=X2[s_][:], start=True, stop=False), reads=['X2%d' % s_, 'idr'], writes=[pk], acc=True)
                        S.op('pe', lambda e: e.matmul(p[:, 0:256], lhsT=X1[s_][:, 128:256], rhs=Xf[s_][:], start=False, stop=True), reads=['X1%d' % s_, 'Xf%d' % s_], writes=[pk], acc=True)
                        evac(QG2[qs][:, h, :], p[:, 0:256], [pk], ['QG2_%d_%d' % (qs, h)])
                        q, qk = pp.get()
                        rg = KRG[pb:pb + 64, c, 1:3, :].rearrange("p k t -> p (k t)")
                        S.op('pe', lambda e: e.matmul(q[0:64, 0:256], lhsT=idr[pb:pb + 64, pb:pb + 64], rhs=rg, start=True, stop=False), reads=['KRG', 'idr'], writes=[qk], acc=True)
                        S.op('pe', lambda e: e.matmul(q[0:64, 0:256], lhsT=X1[s_][:, 256:320], rhs=Xf[s_][:], start=False, stop=True), reads=['X1%d' % s_, 'Xf%d' % s_], writes=[qk], acc=True)
                        evac(QG1[qs][:, h, :], q[0:64, 0:256], [qk], ['QG1_%d_%d' % (qs, h)])
                        yield 'f'
                yield 'F'
                for s, cc in enumerate(chunks):
                    for h in range(8):
                        S.op('pe', lambda e: e.matmul(pst[:, h * 64:(h + 1) * 64], lhsT=QG1[qs][:, h, 128 + cc * 64:128 + (cc + 1) * 64], rhs=Sth[:, s, h, :], start=True, stop=False),
                             reads=['QG1_%d_%d' % (qs, h), 'Sth%d' % s], writes=['pst'], acc=True)
                        S.op('pe', lambda e: e.matmul(pst[:, h * 64:(h + 1) * 64], lhsT=QG2[qs][:, h, 128 + cc * 64:128 + (cc + 1) * 64], rhs=vtr[qs][:, h * 64:(h + 1) * 64], start=False, stop=True),
                             reads=['QG2_%d_%d' % (qs, h), 'vtr%d' % qs], writes=['pst'], acc=True)
                    evac(Sth[:, s + 1].rearrange("p h v -> p (h v)"), pst[:, :], ['pst'], ['Sth%d' % (s + 1)])
                    yield 'b'
                if lat:
                    ybuf = T[0]
                    for hq in range(2):
                        bank = pp.get()
                        for h4 in range(4):
                            h = hq * 4 + h4
                            S.op('pe', lambda e: e.matmul(bank[0][0:64, h4 * 128:(h4 + 1) * 128], lhsT=vtr[qs][:, h * 64:(h + 1) * 64], rhs=QG2[qs][:, h, 0:128], start=True, stop=False),
                                 reads=['QG2_%d_%d' % (qs, h), 'vtr%d' % qs], writes=[bank[1]], acc=True)
                            for s, cc in enumerate(chunks):
                                S.op('pe', lambda e: e.matmul(bank[0][0:64, h4 * 128 + cc * 64:h4 * 128 + (cc + 1) * 64], lhsT=Sth[:, s, h, :], rhs=QG1[qs][:, h, cc * 64:(cc + 1) * 64], start=False, stop=(s == 1)),
                                     reads=['QG1_%d_%d' % (qs, h), 'Sth%d' % s], writes=[bank[1]], acc=True)
                        evac(ybuf[:, hq * 4:hq * 4 + 4, :], bank[0][0:64, :].rearrange("p (h t) -> p h t", h=4), [bank[1]], ['T0'])
                        yield 'b'
                    tl = t0 - TC
                    if d == 0:
                        S.dma('pool', lambda e: e.dma_start(out=K.rwY0[:, tl:tl + G].rearrange("(h p) t -> p h t", p=64), in_=ybuf[:]), reads=['T0'], writes=['rwY0'])
                    else:
                        y0b, cen, sqb = T[1], T[2], T[3]
                        S.dma('sp', lambda e: e.dma_start(out=y0b[:], in_=K.rwY0[:, tl:tl + G].rearrange("(h p) t -> p h t", p=64)), reads=['rwY0'], writes=['T1'])
                        S.op('dve', lambda e: e.tensor_tensor(out=ybuf[:], in0=ybuf[:], in1=y0b[:], op=ALU.add), reads=['T0', 'T1'], writes=['T0'])
                        for q in range(2):
                            p, pk = pp.get()
                            S.op('pe', lambda e: e.matmul(p[0:64, :], lhsT=o64[:], rhs=fl(ybuf)[:, q * 512:(q + 1) * 512], start=True, stop=True), reads=['o64', 'T0'], writes=[pk])
                            S.op('dve', lambda e: e.tensor_tensor(out=fl(cen)[:, q * 512:(q + 1) * 512], in0=fl(ybuf)[:, q * 512:(q + 1) * 512], in1=p[0:64, :], op=ALU.subtract), reads=[pk, 'T0'], writes=['T2'])
                        S.op('act', lambda e: e.activation(out=sqb[:], in_=cen[:], func=AF.Square), reads=['T2'], writes=['T3'])
                        yield 'b'
                        rsb = T[1]
                        for q in range(2):
                            p, pk = pp.get()
                            S.op('pe', lambda e: e.matmul(p[0:64, :], lhsT=o64[:], rhs=fl(sqb)[:, q * 512:(q + 1) * 512], start=True, stop=True), reads=['o64', 'T3'], writes=[pk])
                            S.op('act', lambda e: e.activation(out=fl(rsb)[:, q * 512:(q + 1) * 512], in_=p[0:64, :], func=AF.Ln, bias=epsg[:], scale=1.0), reads=[pk, 'epsg'], writes=['T1'])
                        S.op('act', lambda e: e.activation(out=rsb[:], in_=rsb[:], func=AF.Exp, scale=-0.5), reads=['T1'], writes=['T1'])
                        yield 'b'
                        bonb, ggb = T[3], T[0]
                        S.dma('sp', lambda e: e.dma_start(out=bonb[:], in_=K.rwBON[:, tl:tl + G].rearrange("(h p) t -> p h t", p=64)), reads=['rwBON'], writes=['T3'])
                        S.op('dve', lambda e: e.tensor_tensor(out=cen[:], in0=cen[:], in1=rsb[:], op=ALU.mult), reads=['T2', 'T1'], writes=['T2'])
                        S.dma('sp', lambda e: e.dma_start(out=ggb[:], in_=K.rwG[:, tl:tl + G].rearrange("(h p) t -> p h t", p=64)), reads=['rwG'], writes=['T0'])
                        S.op('dve', lambda e: e.tensor_tensor(out=cen[:], in0=cen[:], in1=gng[:, :].unsqueeze(2).to_broadcast([64, 8, G]), op=ALU.mult), reads=['T2', 'gng'], writes=['T2'])
                        S.op('dve', lambda e: e.tensor_tensor(out=cen[:], in0=cen[:], in1=gnb[:, :].unsqueeze(2).to_broadcast([64, 8, G]), op=ALU.add), reads=['T2', 'gnb'], writes=['T2'])
                        S.op('dve', lambda e: e.tensor_tensor(out=cen[:], in0=cen[:], in1=bonb[:], op=ALU.add), reads=['T2', 'T3'], writes=['T2'])
                        S.op('dve', lambda e: e.tensor_tensor(out=outb[:], in0=cen[:], in1=ggb[:], op=ALU.mult), reads=['T2', 'T0'], writes=['outb'])
                        S.dma('pool', lambda e: e.dma_start(out=K.mixT[0:512, tl:tl + G].rearrange("(h p) t -> p h t", p=64), in_=outb[:]), reads=['outb'], writes=['mixT'])
                S.op('dve', lambda e: e.tensor_copy(out=Sth[:, 0], in_=Sth[:, 2]), reads=['Sth2'], writes=['Sth0'])
            def step(gen_):
                try:
                    return next(gen_)
                except StopIteration:
                    return None
            prev = None
            qslot = 0
            for b in blocks:
                gcur = body(b, qslot)
                while True:
                    r = step(gcur)
                    if prev is not None and step(prev) is None:
                        prev = None
                    if r == 'F' or r is None:
                        break
                while prev is not None:
                    if step(prev) is None:
                        prev = None
                prev = gcur
                qslot ^= 1
            while prev is not None:
                if step(prev) is None:
                    prev = None
            S.barrier()


ALPHA = 2.0 ** 0.25
ROWW = 1088
BIGPOS = 4096.0


def phase_mix(K):
    nc, S = K.nc, K.S
    NT = TL // 128
    with ExitStack() as es:
        sb = lambda n, s, d: es.enter_context(nc.sbuf_tensor(_u(n), s, d))
        ps = lambda n, s, d: es.enter_context(nc.psum_tensor(_u(n), s, d))
        wo = sb("wo", [128, 8, 1024], BF16)
        wst = [sb("owst%d" % i, [128, 8, 256], F32) for i in range(2)]
        bc = {n: sb("bc_" + n, [128, 1024], F32) for n in ('g1', 'ln1g', 'ln1b', 'sc2', 'sh2')}
        rt = sb("rt", [128, 8, 16], F32)
        idf = sb("idf", [128, 128], F32)
        epst = sb("epst", [128, 1], F32)
        onesf = sb("onesf", [128, 128], F32)
        onesb = sb("onesb", [128, 128], BF16)
        ustr = sb("ustr", [128, 128], BF16)
        mt = [sb("mt%d" % i, [128, 8, 128], BF16) for i in range(2)]
        xt = [sb("xt%d" % i, [128, 1024], F32) for i in range(2)]
        t1 = sb("t1", [128, 1024], F32)
        pre = sb("pre", [128, 1024], F32)
        x1 = [sb("x1_%d" % i, [128, 1024], F32) for i in range(2)]
        uf = [sb("uf%d" % i, [128, 1024], F32) for i in range(2)]
        urow = [sb("urow%d" % i, [128, ROWW], BF16) for i in range(2)]
        uT = sb("uT", [128, 8, 128], F32)
        st = sb("st", [128, 2, 6], F32)
        mv = sb("mv", [128, 2], F32)
        lnv = sb("lnv", [128, 1], F32)
        rstd = sb("rstd", [128, 1], F32)
        lg = sb("lg", [128, 16], F32)
        mx = sb("mx", [128, 1], F32)
        sm = sb("sm", [128, 1], F32)
        affall = sb("affall", [128, NT, 16], F32)
        tok = sb("tok", [128, 1], I32)
        lo = sb("lo", [128, 16], F32)
        mid = sb("mid", [128, 16], F32)
        ge = sb("ge", [128, 16], F32)
        cntp = sb("cntp", [128, 16], F32)
        mskt = sb("mskt", [128, NT, 16], F32)
        mskb = sb("mskb", [128, NT, 16], BF16)
        csT = sb("csT", [128, 16, NT], F32)
        incT = sb("incT", [128, 16, NT], F32)
        rmask = sb("rmask", [128, 16, NT], F32)
        posf = sb("posf", [128, NT, 16], F32)
        posi = sb("posi", [128, NT, 16], I32)
        zt = sb("zt", [128, 1024], F32)
        pm = [ps("pm%d" % i, [128, 1024], F32) for i in range(2)]
        ptr = ps("ptr", [128, 1024], F32)
        psm = ps("psm", [128, 512], F32)
        psn = ps("psn", [128, 512], F32)

        ld1 = lambda dst, src, key, rd=(): S.dma('sp', lambda e: e.dma_start(out=dst, in_=src, allow_slow_non_contiguous=True), reads=list(rd), writes=[key])
        ld1(idf[:], K.identf, 'idf')
        ld1(ustr[:], K.ustrict, 'ustr')
        ld1(rt[:], K.router.rearrange("(k p) e -> p k e", p=128), 'rt')
        ld1(bc['g1'][:], K.modd[0:1, 2048:3072].partition_broadcast(128), 'bc_g1', ['modd'])
        ld1(bc['sh2'][:], K.modd[0:1, 3072:4096].partition_broadcast(128), 'bc_sh2', ['modd'])
        ld1(bc['sc2'][:], K.modd[0:1, 4096:5120].partition_broadcast(128), 'bc_sc2', ['modd'])
        ld1(bc['ln1g'][:], K.ln1_g.partition_broadcast(128), 'bc_ln1g')
        ld1(bc['ln1b'][:], K.ln1_b.partition_broadcast(128), 'bc_ln1b')
        S.op('dve', lambda e: e.tensor_scalar_add(out=bc['sc2'][:], in0=bc['sc2'][:], scalar1=1.0), reads=['bc_sc2'], writes=['bc_sc2'])
        S.op('dve', lambda e: e.memset(epst[:], 1e-5), writes=['epst'])
        S.op('dve', lambda e: e.memset(onesf[:], 1.0), writes=['onesf'])
        S.op('dve', lambda e: e.memset(onesb[:], 1.0), writes=['onesb'])
        S.op('dve', lambda e: e.memset(zt[:], 0.0), writes=['zt'])
        for j in range(4):
            w = wst[j % 2]
            wk = 'owst%d' % (j % 2)
            S.dma('sp', lambda e: e.dma_start(out=w[:], in_=K.w_out[:, j * 256:(j + 1) * 256].rearrange("(k p) n -> p k n", p=128)), writes=[wk])
            S.op('pool', lambda e: e.tensor_copy(out=wo[:, :, j * 256:(j + 1) * 256], in_=w[:]), reads=[wk], writes=['wo'])
        for i in range(NT):
            S.dma('pool', lambda e: e.dma_start(out=K.yacc[i * 128:(i + 1) * 128, :], in_=zt[:]), reads=['zt'], writes=['yacc%d' % i])

        mvs = [mv, sb("mv1", [128, 2], F32)]
        rstds = [rstd, sb("rstd1", [128, 1], F32)]
        sts = [st, sb("st1", [128, 2, 6], F32)]
        lnvs = [lnv, sb("lnv1", [128, 1], F32)]

        nmr = [sb("nmr%d" % i_, [128, 1], F32) for i_ in range(2)]

        def ln_stats(src, srck, lane=0):
            sfx = '' if lane == 0 else '1'
            for c in range(2):
                S.op('dve', lambda e: e.bn_stats(out=sts[lane][:, c, :], in_=src[:, c * 512:(c + 1) * 512]), reads=[srck], writes=['st%d%s' % (c, sfx)])
            S.op('dve', lambda e: e.bn_aggr(out=mvs[lane][:], in_=sts[lane][:]), reads=['st0' + sfx, 'st1' + sfx], writes=['mv' + sfx])
            S.op('act', lambda e: e.activation(out=lnvs[lane][:], in_=mvs[lane][:, 1:2], func=AF.Ln, bias=epst[:], scale=1.0), reads=['mv' + sfx, 'epst'], writes=['lnv' + sfx])
            S.op('act', lambda e: e.activation(out=rstds[lane][:], in_=lnvs[lane][:], func=AF.Exp, scale=-0.5), reads=['lnv' + sfx], writes=['rstd' + sfx])

        def tbody(i):
            sl = i % 2
            m_, mk_ = mt[sl], 'mt%d' % sl
            x_, xk = xt[sl], 'xt%d' % sl
            S.dma('sp', lambda e: e.dma_start(out=m_[:], in_=K.mixT[:, i * 128:(i + 1) * 128].rearrange("(k p) t -> p k t", p=128)), reads=['mixT'], writes=[mk_])
            S.dma('sp', lambda e: e.dma_start(out=x_[:], in_=K.xin[TC + i * 128:TC + (i + 1) * 128, :]), writes=[xk])
            p_, pk_ = pm[sl], 'pm%d' % sl
            for half in range(2):
                for kc in range(8):
                    S.op('pe', lambda e: e.matmul(p_[:, half * 512:(half + 1) * 512], lhsT=m_[:, kc, :], rhs=wo[:, kc, half * 512:(half + 1) * 512], start=(kc == 0), stop=(kc == 7)),
                         reads=[mk_, 'wo'], writes=[pk_], acc=True)
            yield 'f'
            S.op('dve', lambda e: e.tensor_tensor(out=t1[:], in0=p_[:], in1=bc['g1'][:], op=ALU.mult), reads=[pk_, 'bc_g1'], writes=['t1'])
            S.op('dve', lambda e: e.scalar_tensor_tensor(out=pre[:], in0=x_[:], scalar=ALPHA, in1=t1[:], op0=ALU.mult, op1=ALU.add), reads=[xk, 't1'], writes=['pre'])
            ln_stats(pre, 'pre')
            yield 'f'
            x1_, x1k = x1[sl], 'x1_%d' % sl
            S.op('dve', lambda e: e.scalar_tensor_tensor(out=nmr[0][:], in0=mv[:, 0:1], scalar=-1.0, in1=rstd[:], op0=ALU.mult, op1=ALU.mult), reads=['mv', 'rstd'], writes=['nmr0'])
            S.op('act', lambda e: e.activation(out=t1[:], in_=pre[:], func=AF.Identity, bias=nmr[0][:], scale=rstd[:]), reads=['pre', 'nmr0', 'rstd'], writes=['t1'])
            S.op('dve', lambda e: e.tensor_tensor(out=t1[:], in0=t1[:], in1=bc['ln1g'][:], op=ALU.mult), reads=['t1', 'bc_ln1g'], writes=['t1'])
            S.op('dve', lambda e: e.tensor_tensor(out=x1_[:], in0=t1[:], in1=bc['ln1b'][:], op=ALU.add), reads=['t1', 'bc_ln1b'], writes=[x1k])
            S.dma('pool', lambda e: e.dma_start(out=K.x1d[i * 128:(i + 1) * 128, :], in_=x1_[:]), reads=[x1k], writes=['x1d'])
            yield 'f'
            ln_stats(x1_, x1k, 1)
            yield 'f'
            S.op('dve', lambda e: e.scalar_tensor_tensor(out=nmr[1][:], in0=mvs[1][:, 0:1], scalar=-1.0, in1=rstds[1][:], op0=ALU.mult, op1=ALU.mult), reads=['mv1', 'rstd1'], writes=['nmr1'])
            S.op('act', lambda e: e.activation(out=pre[:], in_=x1_[:], func=AF.Identity, bias=nmr[1][:], scale=rstds[1][:]), reads=[x1k, 'nmr1', 'rstd1'], writes=['pre'])
            S.op('dve', lambda e: e.tensor_tensor(out=pre[:], in0=pre[:], in1=bc['sc2'][:], op=ALU.mult), reads=['pre', 'bc_sc2'], writes=['pre'])
            S.op('dve', lambda e: e.tensor_tensor(out=uf[sl][:], in0=pre[:], in1=bc['sh2'][:], op=ALU.add), reads=['pre', 'bc_sh2'], writes=['uf%d' % sl])
            ur, urk = urow[sl], 'urow%d' % sl
            S.op('act', lambda e: e.activation(out=ur[:, 0:1024], in_=uf[sl][:], func=AF.Identity), reads=['uf%d' % sl], writes=[urk])
            yield 'F'
            for k in range(8):
                S.op('pe', lambda e: e.transpose(out=ptr[:, k * 128:(k + 1) * 128], in_=uf[sl][:, k * 128:(k + 1) * 128], identity=idf[:]), reads=['uf%d' % sl, 'idf'], writes=['ptr'], acc=True)
            S.op('act', lambda e: e.activation(out=uT[:].rearrange("p k t -> p (k t)"), in_=ptr[:], func=AF.Identity), reads=['ptr'], writes=['uT'])
            yield 'b'
            for k in range(8):
                S.op('pe', lambda e: e.matmul(psm[:, 0:16], lhsT=uT[:, k, :], rhs=rt[:, k, :], start=(k == 0), stop=(k == 7)), reads=['uT', 'rt'], writes=['psm'], acc=True)
            S.op('dve', lambda e: e.reduce_max(out=mx[:], in_=psm[:, 0:16], axis=AX.X), reads=['psm'], writes=['mx'])
            S.op('dve', lambda e: e.tensor_scalar_mul(out=mx[:], in0=mx[:], scalar1=-1.0), reads=['mx'], writes=['mx'])
            yield 'b'
            S.op('act', lambda e: e.activation(out=lg[:], in_=psm[:, 0:16], func=AF.Exp, bias=mx[:], scale=1.0, accum_out=sm[:]), reads=['psm', 'mx'], writes=['lg', 'sm'])
            S.op('dve', lambda e: e.reciprocal(out=sm[:], in_=sm[:]), reads=['sm'], writes=['sm'])
            yield 'b'
            S.op('dve', lambda e: e.tensor_scalar_mul(out=affall[:, i, :], in0=lg[:], scalar1=sm[:]), reads=['lg', 'sm'], writes=['affall'])
            S.op('dve', lambda e: e.tensor_copy(out=ur[:, 1024:1056].bitcast(F32), in_=affall[:, i, :]), reads=['affall'], writes=[urk])
            S.op('pool', lambda e: e.iota(tok[:], pattern=[[0, 1]], base=i * 128, channel_multiplier=1), writes=['tok'])
            S.op('pool', lambda e: e.tensor_copy(out=ur[:, 1056:1058].bitcast(I32), in_=tok[:]), reads=['tok'], writes=[urk])
            S.dma('pool', lambda e: e.dma_start(out=K.urd[i * 128:(i + 1) * 128, 0:1058], in_=ur[:, 0:1058]), reads=[urk], writes=['urd%d' % i])

        def step(gen_):
            try:
                return next(gen_)
            except StopIteration:
                return None
        prev = None
        for i in range(NT):
            gcur = tbody(i)
            while True:
                r = step(gcur)
                if prev is not None and step(prev) is None:
                    prev = None
                if r == 'F' or r is None:
                    break
            while prev is not None:
                if step(prev) is None:
                    prev = None
            prev = gcur
        while prev is not None:
            if step(prev) is None:
                prev = None

        S.op('dve', lambda e: e.memset(lo[:], 0.0), writes=['lo'])
        for k in range(30):
            hk = 2.0 ** -(k + 1)
            S.op('dve', lambda e: e.tensor_scalar_add(out=mid[:], in0=lo[:], scalar1=hk), reads=['lo'], writes=['mid'])
            S.op('dve', lambda e: e.tensor_tensor(out=mskt[:], in0=affall[:], in1=mid[:, :].unsqueeze(1).to_broadcast([128, NT, 16]), op=ALU.is_ge), reads=['affall', 'mid'], writes=['mskt'])
            S.op('dve', lambda e: e.tensor_reduce(out=cntp[:], in_=mskt[:].rearrange("p i e -> p e i"), axis=AX.X, op=ALU.add), reads=['mskt'], writes=['cntp'])
            S.op('pe', lambda e: e.matmul(psn[:, 0:16], lhsT=onesf[:], rhs=cntp[:], start=True, stop=True), reads=['onesf', 'cntp'], writes=['psn'])
            S.op('dve', lambda e: e.tensor_scalar(out=ge[:], in0=psn[:, 0:16], scalar1=float(CAP) - 0.5, scalar2=hk, op0=ALU.is_ge, op1=ALU.mult), reads=['psn'], writes=['ge'])
            S.op('dve', lambda e: e.tensor_tensor(out=lo[:], in0=lo[:], in1=ge[:], op=ALU.add), reads=['lo', 'ge'], writes=['lo'])
        S.op('dve', lambda e: e.tensor_tensor(out=mskt[:], in0=affall[:], in1=lo[:, :].unsqueeze(1).to_broadcast([128, NT, 16]), op=ALU.is_ge), reads=['affall', 'lo'], writes=['mskt'])
        S.op('act', lambda e: e.activation(out=mskb[:], in_=mskt[:], func=AF.Identity), reads=['mskt'], writes=['mskb'])
        mflat = mskb[:].rearrange("p i e -> p (i e)")
        for hh in range(2):
            S.op('pe', lambda e: e.matmul(psm[:, :], lhsT=onesb[:], rhs=mflat[:, hh * 512:(hh + 1) * 512], start=True, stop=True), reads=['onesb', 'mskb'], writes=['psm'])
            S.op('dve', lambda e: e.tensor_copy(out=csT[:, :, hh * 32:(hh + 1) * 32], in_=psm[:, :].rearrange("p (i e) -> p e i", e=16)), reads=['psm'], writes=['csT'])
        S.op('dve', lambda e: e.memset(rmask[:], 1.0), writes=['rmask'])
        S.op('dve', lambda e: e.memset(rmask[:, :, 0:1], 0.0), writes=['rmask'])
        S.op('dve', lambda e: e.tensor_tensor_scan(out=incT[:].rearrange("p e i -> p (e i)"), data0=rmask[:].rearrange("p e i -> p (e i)"), data1=csT[:].rearrange("p e i -> p (e i)"),
                                                   initial=0.0, op0=ALU.mult, op1=ALU.add), reads=['rmask', 'csT'], writes=['incT'])
        S.op('dve', lambda e: e.tensor_tensor(out=incT[:], in0=incT[:], in1=csT[:], op=ALU.subtract), reads=['incT', 'csT'], writes=['incT'])
        for hh in range(2):
            S.op('pe', lambda e: e.matmul(psm[:, :], lhsT=ustr[:], rhs=mflat[:, hh * 512:(hh + 1) * 512], start=True, stop=True), reads=['ustr', 'mskb'], writes=['psm'])
            S.op('dve', lambda e: e.tensor_tensor(out=posf[:, hh * 32:(hh + 1) * 32, :], in0=psm[:, :].rearrange("p (i e) -> p i e", e=16),
                                                  in1=incT[:, :, hh * 32:(hh + 1) * 32].rearrange("p e i -> p i e"), op=ALU.add), reads=['psm', 'incT'], writes=['posf'])
        S.op('dve', lambda e: e.scalar_tensor_tensor(out=posf[:], in0=posf[:], scalar=-BIGPOS, in1=mskt[:], op0=ALU.add, op1=ALU.mult), reads=['posf', 'mskt'], writes=['posf'])
        S.op('dve', lambda e: e.tensor_scalar_add(out=posf[:], in0=posf[:], scalar1=BIGPOS), reads=['posf'], writes=['posf'])
        S.op('dve', lambda e: e.tensor_copy(out=posi[:], in_=posf[:]), reads=['posf'], writes=['posi'])
        if K.dbg_pos is not None:
            S.dma('sp', lambda e: e.dma_start(out=K.dbg_pos, in_=posf[:]), reads=['posf'], writes=['dbg_pos'])
        breg = nc.gpsimd.to_reg(CAP - 1)
        for i in range(NT):
            sl = i % 2
            ur, urk = urow[sl], 'urow%d' % sl
            S.dma('sp', lambda e: e.dma_start(out=ur[:, 0:1058], in_=K.urd[i * 128:(i + 1) * 128, 0:1058]), reads=['urd%d' % i], writes=[urk])
            for ex in range(NE):
                S.dma('pool', lambda e: e.indirect_dma_start(out=K.xe_d[ex], out_offset=bass.IndirectOffsetOnAxis(ap=posi[:, i, ex:ex + 1], axis=0),
                                                             in_=ur[:, :], in_offset=None, bounds_check=breg, oob_is_err=False),
                      reads=[urk, 'posi'], writes=['xe_d_%d_%d' % (i, ex)])
        S.barrier()


def phase_moe(K, experts=range(NE)):
    nc, S = K.nc, K.S
    NT = TL // 128
    with ExitStack() as es:
        sb = lambda n, s, d: es.enter_context(nc.sbuf_tensor(_u(n), s, d))
        ps = lambda n, s, d: es.enter_context(nc.psum_tensor(_u(n), s, d))
        W = [[sb("W%d_%d" % (m, i), [128, 8, 1024], BF16) for i in range(2)] for m in range(3)]
        wst = Rot([sb("ewst%d" % i, [128, 8, 256], F32) for i in range(3)], "ewst")
        idb = sb("idb", [128, 128], BF16)
        xrow = Rot([sb("xrow%d" % i, [128, ROWW], BF16) for i in range(2)], "xrow")
        xeT = [sb("xeT%d" % i, [128, 8, 1024], BF16) for i in range(2)]
        hidT = sb("hidT", [128, 8, 1024], BF16)
        gates = [sb("gates%d" % i, [128, 8], F32) for i in range(2)]
        idxs = [sb("idxs%d" % i, [128, 8], I32) for i in range(2)]
        sgt = Rot([sb("sgt%d" % i, [128, 512], F32) for i in range(2)], "sgt")
        ye = Rot([sb("ye%d" % i, [128, 1024], F32) for i in range(2)], "ye")
        pt = Rot([ps("ept%d" % i, [128, 1024], BF16) for i in range(2)], "ept")
        pg = Rot([ps("epg%d" % i, [128, 512], F32) for i in range(6)], "epg")
        S.dma('sp', lambda e: e.dma_start(out=idb[:], in_=K.ident), writes=['idb'])
        cast_i = [0]

        def load_w(ex, slot):
            for m, src in enumerate((K.exp_w_gate, K.exp_w_up, K.exp_w_down)):
                for j in range(4):
                    w, wk = wst.get()
                    S.dma('sp', lambda e: e.dma_start(out=w[:], in_=src[ex, :, j * 256:(j + 1) * 256].rearrange("(k p) n -> p k n", p=128)), writes=[wk])
                    eng = 'dve'
                    cast_i[0] += 1
                    if eng == 'dve':
                        S.op('dve', lambda e: e.tensor_copy(out=W[m][slot][:, :, j * 256:(j + 1) * 256], in_=w[:]), reads=[wk], writes=['W%d_%d' % (m, slot)])
                    else:
                        S.op('act', lambda e: e.activation(out=W[m][slot][:, :, j * 256:(j + 1) * 256], in_=w[:], func=AF.Identity), reads=[wk], writes=['W%d_%d' % (m, slot)])

        exl = list(experts)
        load_w(exl[0], 0)
        if len(exl) > 1:
            load_w(exl[1], 1)
        def ebody(n, ex):
            xs = n % 2
            slot = n % 2
            Wg, Wu, Wd = W[0][slot], W[1][slot], W[2][slot]
            wkeys = ['W%d_%d' % (m, slot) for m in range(3)]
            for j in range(8):
                xr, xk = xrow.get()
                S.dma('sp', lambda e: e.dma_start(out=xr[:, 0:1058], in_=K.xe_d[ex][j * 128:(j + 1) * 128, 0:1058]), reads=['xe_d'], writes=[xk])
                S.op('dve', lambda e: e.tensor_copy(out=gates[xs][:, j:j + 1], in_=xr[:, 1024:1056].bitcast(F32)[:, ex:ex + 1]), reads=[xk], writes=['gates%d' % xs])
                S.op('dve', lambda e: e.tensor_copy(out=idxs[xs][:, j:j + 1], in_=xr[:, 1056:1058].bitcast(I32)), reads=[xk], writes=['idxs%d' % xs])
                p, pk = pt.get()
                for k in range(8):
                    S.op('pe', lambda e: e.transpose(out=p[:, k * 128:(k + 1) * 128], in_=xr[:, k * 128:(k + 1) * 128], identity=idb[:]), reads=[xk, 'idb'], writes=[pk], acc=True)
                S.op('act', lambda e: e.activation(out=xeT[xs][:, :, j * 128:(j + 1) * 128], in_=p[:].rearrange("p (k t) -> p k t", k=8), func=AF.Identity), reads=[pk], writes=['xeT%d' % xs])
                yield 'f'
            yield 'F'
            for fc in range(8):
                for half in range(2):
                    g_, gk = pg.get()
                    u_, uk = pg.get()
                    for kc in range(8):
                        S.op('pe', lambda e: e.matmul(g_[:], lhsT=Wg[:, kc, fc * 128:(fc + 1) * 128], rhs=xeT[xs][:, kc, half * 512:(half + 1) * 512], start=(kc == 0), stop=(kc == 7)),
                             reads=[wkeys[0], 'xeT%d' % xs], writes=[gk], acc=True)
                    for kc in range(8):
                        S.op('pe', lambda e: e.matmul(u_[:], lhsT=Wu[:, kc, fc * 128:(fc + 1) * 128], rhs=xeT[xs][:, kc, half * 512:(half + 1) * 512], start=(kc == 0), stop=(kc == 7)),
                             reads=[wkeys[1], 'xeT%d' % xs], writes=[uk], acc=True)
                    s_, sk = sgt.get()
                    S.op('act', lambda e: e.activation(out=s_[:], in_=g_[:], func=AF.Silu), reads=[gk], writes=[sk])
                    S.op('dve', lambda e: e.tensor_tensor(out=hidT[:, fc, half * 512:(half + 1) * 512], in0=s_[:], in1=u_[:], op=ALU.mult), reads=[sk, uk], writes=['hidT'])
                    yield 'b'
            for j in range(8):
                y_, yk = ye.get()
                for dh in range(2):
                    o_, ok = pg.get()
                    for fc in range(8):
                        S.op('pe', lambda e: e.matmul(o_[:], lhsT=hidT[:, fc, j * 128:(j + 1) * 128], rhs=Wd[:, fc, dh * 512:(dh + 1) * 512], start=(fc == 0), stop=(fc == 7)),
                             reads=[wkeys[2], 'hidT'], writes=[ok], acc=True)
                    S.op('act', lambda e: e.activation(out=y_[:, dh * 512:(dh + 1) * 512], in_=o_[:], func=AF.Identity, scale=gates[xs][:, j:j + 1]), reads=[ok, 'gates%d' % xs], writes=[yk])
                S.dma('pool', lambda e: e.indirect_dma_start(out=K.yacc, out_offset=bass.IndirectOffsetOnAxis(ap=idxs[xs][:, j:j + 1], axis=0), in_=y_[:, :], in_offset=None,
                                                             compute_op=ALU.add),
                      reads=[yk, 'idxs%d' % xs], writes=['yacc'])
                yield 'b'
        def step(gen_):
            try:
                return next(gen_)
            except StopIteration:
                return None
        prev = None
        for n, ex in enumerate(exl):
            gcur = ebody(n, ex)
            while True:
                r = step(gcur)
                if prev is not None and step(prev) is None:
                    prev = None
                if r == 'F' or r is None:
                    break
            while prev is not None:
                if step(prev) is None:
                    prev = None
            if n >= 1 and n + 1 < len(exl):
                load_w(exl[n + 1], (n + 1) % 2)
            prev = gcur
        while prev is not None:
            if step(prev) is None:
                prev = None
        S.barrier()


def phase_final(K):
    nc, S = K.nc, K.S
    NT = TL // 128
    with ExitStack() as es:
        sb = lambda n, s, d: es.enter_context(nc.sbuf_tensor(_u(n), s, d))
        bc = {n: sb("fbc_" + n, [128, 1024], F32) for n in ('g2', 'ln2g', 'ln2b')}
        epst = sb("epst", [128, 1], F32)
        x1 = [sb("fx1_%d" % i, [128, 1024], F32) for i in range(2)]
        ya = [sb("fya_%d" % i, [128, 1024], F32) for i in range(2)]
        t1 = sb("t1", [128, 1024], F32)
        pre = sb("pre", [128, 1024], F32)
        ob = [sb("fob_%d" % i, [128, 1024], F32) for i in range(2)]
        st = sb("st", [128, 2, 6], F32)
        mv = sb("mv", [128, 2], F32)
        lnv = sb("lnv", [128, 1], F32)
        rstd = sb("rstd", [128, 1], F32)
        nmr = sb("nmr", [128, 1], F32)
        ld1 = lambda dst, src, key, rd=(): S.dma('sp', lambda e: e.dma_start(out=dst, in_=src, allow_slow_non_contiguous=True), reads=list(rd), writes=[key])
        ld1(bc['g2'][:], K.modd[0:1, 5120:6144].partition_broadcast(128), 'fbc_g2', ['modd'])
        ld1(bc['ln2g'][:], K.ln2_g.partition_broadcast(128), 'fbc_ln2g')
        ld1(bc['ln2b'][:], K.ln2_b.partition_broadcast(128), 'fbc_ln2b')
        S.op('dve', lambda e: e.memset(epst[:], 1e-5), writes=['epst'])
        for i in range(NT):
            sl = i % 2
            S.dma('sp', lambda e: e.dma_start(out=x1[sl][:], in_=K.x1d[i * 128:(i + 1) * 128, :]), reads=['x1d'], writes=['fx1_%d' % sl])
            S.dma('sp', lambda e: e.dma_start(out=ya[sl][:], in_=K.yacc[i * 128:(i + 1) * 128, :]), reads=['yacc'], writes=['fya_%d' % sl])
            S.op('dve', lambda e: e.tensor_tensor(out=t1[:], in0=ya[sl][:], in1=bc['g2'][:], op=ALU.mult), reads=['fya_%d' % sl, 'fbc_g2'], writes=['t1'])
            S.op('dve', lambda e: e.scalar_tensor_tensor(out=pre[:], in0=x1[sl][:], scalar=ALPHA, in1=t1[:], op0=ALU.mult, op1=ALU.add), reads=['fx1_%d' % sl, 't1'], writes=['pre'])
            for c in range(2):
                S.op('dve', lambda e: e.bn_stats(out=st[:, c, :], in_=pre[:, c * 512:(c + 1) * 512]), reads=['pre'], writes=['st%d' % c])
            S.op('dve', lambda e: e.bn_aggr(out=mv[:], in_=st[:]), reads=['st0', 'st1'], writes=['mv'])
            S.op('act', lambda e: e.activation(out=lnv[:], in_=mv[:, 1:2], func=AF.Ln, bias=epst[:], scale=1.0), reads=['mv', 'epst'], writes=['lnv'])
            S.op('act', lambda e: e.activation(out=rstd[:], in_=lnv[:], func=AF.Exp, scale=-0.5), reads=['lnv'], writes=['rstd'])
            S.op('dve', lambda e: e.scalar_tensor_tensor(out=nmr[:], in0=mv[:, 0:1], scalar=-1.0, in1=rstd[:], op0=ALU.mult, op1=ALU.mult), reads=['mv', 'rstd'], writes=['nmr'])
            S.op('act', lambda e: e.activation(out=t1[:], in_=pre[:], func=AF.Identity, bias=nmr[:], scale=rstd[:]), reads=['pre', 'nmr', 'rstd'], writes=['t1'])
            S.op('dve', lambda e: e.tensor_tensor(out=t1[:], in0=t1[:], in1=bc['ln2g'][:], op=ALU.mult), reads=['t1', 'fbc_ln2g'], writes=['t1'])
            S.op('dve', lambda e: e.tensor_tensor(out=ob[sl][:], in0=t1[:], in1=bc['ln2b'][:], op=ALU.add), reads=['t1', 'fbc_ln2b'], writes=['fob_%d' % sl])
            S.dma('pool', lambda e: e.dma_start(out=K.out[i * 128:(i + 1) * 128, :], in_=ob[sl][:]), reads=['fob_%d' % sl], writes=['out'])
        S.barrier()


def build_program(debug=(), phases=None, dbg_in=(), opts=None):
    opts = opts or {}
    nc = bass.Bass("TRN2", target_bir_lowering=False)
    K = Ctx()
    K.nc = nc
    di = lambda n, s, d: nc.dram_tensor(n, s, d, kind="ExternalInput").ap()
    K.xin = di("xin", [TA, D], F32)
    K.ccT = di("ccT", [128, 8, 2], F32)
    K.w_ada = di("w_ada", [D, 6 * D], F32)
    K.b_ada = di("b_ada", [1, 6 * D], F32)
    K.w_in = di("w_in", [D, 2560], F32)
    K.cosT = di("cosT", [64, TL], F32)
    K.sinT = di("sinT", [64, TL], F32)
    K.ident = di("ident", [128, 128], BF16)
    K.identf = di("identf", [128, 128], F32)
    K.i64dbl = di("i64dbl", [128, 128], F32)
    K.bones = di("bones", [128, 128], F32)
    K.rwm = [di("rwm%d" % d, [128, 1344], F32) for d in range(2)]
    K.mla_q_norm = di("mla_q_norm", [1, 256], F32)
    K.mla_kv_norm = di("mla_kv_norm", [1, 256], F32)
    K.w_uq = di("w_uq", [256, 1024], F32)
    K.w_uk = di("w_uk", [256, 512], F32)
    K.w_uv = di("w_uv", [256, 512], F32)
    K.rwkv_conv = di("rwkv_conv", [3, 1536], F32)
    K.rwkv_w0 = di("rwkv_w0", [2, 512], F32)
    K.rwkv_w_up = di("rwkv_w_up", [2, 64, 512], F32)
    K.rwkv_a0 = di("rwkv_a0", [2, 512], F32)
    K.rwkv_a_up = di("rwkv_a_up", [2, 64, 512], F32)
    K.rwkv_g_up = di("rwkv_g_up", [128, 512], F32)
    K.rwkv_k_k = di("rwkv_k_k", [1, 512], F32)
    K.rwkv_k_a = di("rwkv_k_a", [1, 512], F32)
    K.rwkv_r_k = di("rwkv_r_k", [1, 512], F32)
    K.rwkv_gn_g = di("rwkv_gn_g", [1, 512], F32)
    K.rwkv_gn_b = di("rwkv_gn_b", [1, 512], F32)
    K.w_out = di("w_out", [D, D], F32)
    K.ln1_g = di("ln1_g", [1, D], F32)
    K.ln1_b = di("ln1_b", [1, D], F32)
    K.ln2_g = di("ln2_g", [1, D], F32)
    K.ln2_b = di("ln2_b", [1, D], F32)
    K.router = di("router", [D, NE], F32)
    K.ustrict = di("ustrict", [128, 128], BF16)
    K.exp_w_gate = di("exp_w_gate", [NE, D, D], F32)
    K.exp_w_up = di("exp_w_up", [NE, D, D], F32)
    K.exp_w_down = di("exp_w_down", [NE, D, D], F32)

    def scratch(n, s, d):
        if n in dbg_in:
            return nc.dram_tensor(n, s, d, kind="ExternalInput").ap()
        kind = "ExternalOutput" if n in debug else "Internal"
        return nc.dram_tensor(n, s, d, kind=kind).ap()
    K.modd = scratch("modd", [2, 6 * D], F32)
    K.hT = scratch("hT", [2432, TA], F32)
    K.krT = scratch("krT", [64, TA], BF16)
    K.qnT = scratch("qnT", [512, TL], BF16)
    K.qrT = scratch("qrT", [256, TL], BF16)
    K.knT = scratch("knT", [512, TA], BF16)
    K.vtok = scratch("vtok", [TA, 512], BF16)
    K.mixT = scratch("mixT", [1024, TL], BF16)
    K.rwR = scratch("rwR", [512, TA], F32)
    K.rwV = scratch("rwV", [512, TA], F32)
    K.rwKK = scratch("rwKK", [512, TA], F32)
    K.rwKD = [scratch("rwKD%d" % d, [512, TA], F32) for d in range(2)]
    K.rwB = [scratch("rwB%d" % d, [512, TA], F32) for d in range(2)]
    K.rwVtok = scratch("rwVtok", [TA, 512], F32)
    K.rwLD = [scratch("rwLD%d" % d, [TA, 512], F32) for d in range(2)]
    K.rwG = scratch("rwG", [512, TL], F32)
    K.rwBON = scratch("rwBON", [512, TL], F32)
    K.rwY0 = scratch("rwY0", [512, TL], F32)
    K.x1d = scratch("x1d", [TL, D], F32)
    K.urd = scratch("urd", [TL, ROWW], BF16)
    K.xe_d = [scratch("xe_d%d" % e_, [CAP, ROWW], BF16) for e_ in range(NE)]
    K.yacc = scratch("yacc", [TL, D], F32)
    K.dbg_pos = scratch("dbg_pos", [128, TL // 128, 16], F32) if 'dbg_pos' in debug else None
    K.out = nc.dram_tensor("out", [TL, D], F32, kind="ExternalOutput").ap()
    allp = ['mod', 'inproj', 'mlaprep', 'attn', 'rwprep', 'rwscan', 'mix', 'moe', 'final']
    if phases is None:
        phases = allp
    with ExitStack() as es:
        S = Sync(nc, es)
        K.S = S
        if 'mod' in phases:
            phase_mod(K)
        if 'inproj' in phases:
            phase_inproj(K)
        if 'mlaprep' in phases:
            phase_mlaprep(K)
        if 'attn' in phases:
            phase_attn(K)
        if 'attn1' in phases:
            phase_attn(K, heads=(1,), nqt=2)
        if 'rwprep' in phases:
            phase_rwprep(K)
        if 'rwscan' in phases:
            phase_rwscan(K, **opts.get('rwscan', {}))
        if 'mix' in phases:
            phase_mix(K)
        if 'moe' in phases:
            phase_moe(K, **opts.get('moe', {}))
        if 'final' in phases:
            phase_final(K)
        S.wait_all('sp')
        print("instructions", S.n_inst, "waits", S.n_wait, "sems", S.nsem + NDMA)
    return nc


_SWAP = np.concatenate([np.arange(16, 32), np.arange(0, 16), np.arange(48, 64), np.arange(32, 48)])


def rope_tables():
    half = 32
    inv_freq = (10000.0 ** (-np.arange(0, half, 2, dtype=np.float32) / half)).astype(np.float32)
    t = np.arange(TL)
    rr = (t // 64).astype(np.float32)[None, :]
    cc = (t % 64).astype(np.float32)[None, :]
    ang_r = (inv_freq[:, None] * rr).astype(np.float32)
    ang_c = (inv_freq[:, None] * cc).astype(np.float32)
    cosT = np.concatenate([np.cos(ang_r), np.cos(ang_r), np.cos(ang_c), np.cos(ang_c)], 0).astype(np.float32)
    sinT = np.concatenate([-np.sin(ang_r), np.sin(ang_r), -np.sin(ang_c), np.sin(ang_c)], 0).astype(np.float32)
    return np.ascontiguousarray(cosT), np.ascontiguousarray(sinT)


def make_in_maps(inputs, batches):
    f = lambda a: np.ascontiguousarray(np.asarray(a, dtype=np.float32))
    w_in = f(inputs['w_in'][0])
    w_in_ext = np.concatenate([w_in, w_in[:, 2432:2496][:, _SWAP]], axis=1)
    cosT, sinT = rope_tables()
    wuq = f(inputs['mla_w_uq'][0])
    cols = []
    for h in range(4):
        nope = wuq[:, h * 192:h * 192 + 128]
        rope = wuq[:, h * 192 + 128:h * 192 + 192]
        cols += [nope, rope, rope[:, _SWAP]]
    wuq_ext = np.ascontiguousarray(np.concatenate(cols, axis=1))
    shared = {
        'w_ada': f(inputs['w_ada'][0]), 'b_ada': f(inputs['b_ada']), 'w_in': np.ascontiguousarray(w_in_ext),
        'cosT': cosT, 'sinT': sinT, 'ident': np.eye(128).astype(ml_dtypes.bfloat16),
        'mla_q_norm': f(inputs['mla_q_norm']), 'mla_kv_norm': f(inputs['mla_kv_norm']),
        'w_uq': wuq_ext, 'w_uk': f(inputs['mla_w_uk'][0]), 'w_uv': f(inputs['mla_w_uv'][0]),
        'rwkv_conv': f(inputs['rwkv_conv'][0]), 'rwkv_w0': f(inputs['rwkv_w0'][0]), 'rwkv_w_up': f(inputs['rwkv_w_up'][0]),
        'rwkv_a0': f(inputs['rwkv_a0'][0]), 'rwkv_a_up': f(inputs['rwkv_a_up'][0]), 'rwkv_g_up': f(inputs['rwkv_g_up'][0]),
        'rwkv_k_k': f(inputs['rwkv_k_k']), 'rwkv_k_a': f(inputs['rwkv_k_a']), 'rwkv_r_k': f(inputs['rwkv_r_k']).reshape(1, 512),
        'rwkv_gn_g': f(inputs['rwkv_gn_g']), 'rwkv_gn_b': f(inputs['rwkv_gn_b']),
        'w_out': f(inputs['w_out'][0]), 'ln1_g': f(inputs['ln1_g']), 'ln1_b': f(inputs['ln1_b']),
        'ln2_g': f(inputs['ln2_g']), 'ln2_b': f(inputs['ln2_b']), 'router': f(inputs['router'][0]),
        'ustrict': (np.arange(128)[:, None] < np.arange(128)[None, :]).astype(ml_dtypes.bfloat16),
        'exp_w_gate': f(inputs['exp_w_gate'][0]), 'exp_w_up': f(inputs['exp_w_up'][0]), 'exp_w_down': f(inputs['exp_w_down'][0]),
    }
    shared.update(rw_consts())
    maps = []
    for b in batches:
        m = dict(shared)
        m['xin'] = np.ascontiguousarray(np.concatenate([inputs['ctx'][b], inputs['x'][b]], axis=0).astype(np.float32))
        cc = np.stack([inputs['c'][b], inputs['c_ctx']], axis=-1).astype(np.float32)
        m['ccT'] = np.ascontiguousarray(cc.reshape(8, 128, 2).transpose(1, 0, 2))
        maps.append(m)
    return maps


_CONST_KEYS = ('cosT', 'sinT', 'ident', 'identf', 'i64dbl', 'bones', 'rwm0', 'rwm1', 'ustrict')
BATCH_CORES = (0, 1, 4, 5)


def kernel(**inputs):
    nc = build_program()
    real = make_in_maps(inputs, [0, 1, 2, 3])
    zero = {k: (v if k in _CONST_KEYS else np.zeros_like(v)) for k, v in real[0].items()}
    maps = [zero] * 8
    for b, c in enumerate(BATCH_CORES):
        maps[c] = real[b]
    res = run_bass_kernel_spmd(nc, maps, core_ids=list(range(8)))
    out = np.stack([res.results[c]['out'] for c in BATCH_CORES], axis=0)
    return out.astype(np.float32)
```

```python
import numpy as np
import ml_dtypes
from contextlib import ExitStack
import concourse.bass as bass
import concourse.mybir as mybir
from concourse.bass_utils import run_bass_kernel_spmd

F32 = mybir.dt.float32
BF16 = mybir.dt.bfloat16
I32 = mybir.dt.int32
AF = mybir.ActivationFunctionType
ALU = mybir.AluOpType
AX = mybir.AxisListType

EPOCH = 12000
NDMA = 24

D = 1024
TL = 8192
TC = 256
TA = TL + TC
NE = 16
CAP = 1024


class Sync:
    def __init__(self, nc, es):
        self.nc = nc
        self.es = es
        self.eng = {'pe': nc.tensor, 'dve': nc.vector, 'act': nc.scalar,
                    'pool': nc.gpsimd, 'sp': nc.sync}
        self.sem = {}
        self.cnt = {}
        self.cur = {}
        self.known = {e: {} for e in self.eng}
        self.snap = {}
        self.last_w = {}
        self.readers = {}
        self.dma_keys = []
        self.dma_rr = 0
        self.nsem = 0
        for e in self.eng:
            self._new_epoch(e)
        for i in range(NDMA):
            k = ('dma', i)
            self.sem[k] = es.enter_context(nc.semaphore('dq%d' % i))
            self.cnt[k] = 0
            self.dma_keys.append(k)
        self.n_inst = 0
        self.n_wait = 0

    def _new_epoch(self, e):
        idx = self.nsem
        self.nsem += 1
        k = (e, idx)
        self.sem[k] = self.es.enter_context(self.nc.semaphore('s_%s_%d' % (e, idx)))
        self.cnt[k] = 0
        self.cur[e] = k

    def _need(self, e, ticket):
        k, v = ticket
        kn = self.known[e]
        if kn.get(k, 0) >= v:
            return
        self.eng[e].wait_ge(self.sem[k], v)
        self.n_wait += 1
        kn[k] = v
        sn = self.snap.get(ticket)
        if sn:
            for kk, vv in sn.items():
                if kn.get(kk, 0) < vv:
                    kn[kk] = vv

    def _deps(self, e, reads, writes, acc):
        for b in reads:
            t = self.last_w.get(b)
            if t is not None:
                self._need(e, t)
        for b in writes:
            t = self.last_w.get(b)
            if t is not None and not (acc and t[0] == self.cur[e]):
                self._need(e, t)
            for t in self.readers.get(b, ()):
                self._need(e, t)

    def _record(self, ticket, reads, writes):
        for b in reads:
            self.readers.setdefault(b, []).append(ticket)
        for b in writes:
            self.last_w[b] = ticket
            self.readers[b] = []

    def op(self, e, fn, reads=(), writes=(), acc=False):
        if self.cnt[self.cur[e]] >= EPOCH:
            self._new_epoch(e)
        self._deps(e, reads, writes, acc)
        k = self.cur[e]
        inst = fn(self.eng[e])
        inst.then_inc(self.sem[k], 1)
        self.cnt[k] += 1
        t = (k, self.cnt[k])
        self.snap[t] = dict(self.known[e])
        self._record(t, reads, writes)
        self.n_inst += 1
        return t

    def dma(self, e, fn, reads=(), writes=()):
        k = self.dma_keys[self.dma_rr]
        self.dma_rr = (self.dma_rr + 1) % NDMA
        if self.cnt[k] > 0:
            self._need(e, (k, self.cnt[k]))
        self._deps(e, reads, writes, False)
        inst = fn(self.eng[e])
        inst.then_inc(self.sem[k], 16)
        self.cnt[k] += 16
        t = (k, self.cnt[k])
        self.snap[t] = dict(self.known[e])
        self._record(t, reads, writes)
        self.n_inst += 1
        return t

    def wait_all(self, e):
        for k, v in list(self.cnt.items()):
            if v > 0:
                self._need(e, (k, v))

    def barrier(self):
        for e in self.eng:
            self.wait_all(e)
        self.last_w.clear()
        self.readers.clear()


class Ctx:
    pass


_UC = [0]


def _u(n):
    _UC[0] += 1
    return '%s_%d' % (n, _UC[0])


def phase_mod(K):
    nc, S = K.nc, K.S
    with ExitStack() as es:
        sb = lambda n, s, d: es.enter_context(nc.sbuf_tensor(_u(n), s, d))
        ccs = sb("ccs", [128, 8, 2], F32)
        scT = sb("scT", [128, 8, 2], F32)
        wa = [sb("wa%d" % i, [128, 8, 512], F32) for i in range(2)]
        ba = sb("ba", [1, 6144], F32)
        one1 = sb("one1", [1, 1], F32)
        mrow = [sb("mrow%d" % r, [1, 6144], F32) for r in range(2)]
        pm = [es.enter_context(nc.psum_tensor(_u("pm%d" % i), [1, 512], F32)) for i in range(2)]
        S.dma('sp', lambda e: e.dma_start(out=ccs[:], in_=K.ccT), writes=['ccs'])
        S.dma('sp', lambda e: e.dma_start(out=ba[:], in_=K.b_ada), writes=['ba'])
        S.op('dve', lambda e: e.memset(one1[:], 1.0), writes=['one1'])
        S.op('act', lambda e: e.activation(out=scT[:], in_=ccs[:], func=AF.Silu), reads=['ccs'], writes=['scT'])
        for j in range(12):
            w = wa[j % 2]
            wk = 'wa%d' % (j % 2)
            S.dma('sp', lambda e: e.dma_start(out=w[:], in_=K.w_ada[:, j * 512:(j + 1) * 512].rearrange("(k p) n -> p k n", p=128)), writes=[wk])
            for r in range(2):
                if r == 1 and j >= 4:
                    continue
                pk = 'pm%d' % r
                for k in range(8):
                    S.op('pe', lambda e: e.matmul(pm[r][:], lhsT=scT[:, k, r:r + 1], rhs=w[:, k, :], start=(k == 0), stop=False),
                         reads=['scT', wk], writes=[pk], acc=True)
                S.op('pe', lambda e: e.matmul(pm[r][:], lhsT=one1[:], rhs=ba[:, j * 512:(j + 1) * 512], start=False, stop=True),
                     reads=['one1', 'ba'], writes=[pk], acc=True)
                S.op('dve', lambda e: e.tensor_copy(out=mrow[r][:, j * 512:(j + 1) * 512], in_=pm[r][:]), reads=[pk], writes=['mrow%d' % r])
        S.dma('sp', lambda e: e.dma_start(out=K.modd[0:1, :], in_=mrow[0][:]), reads=['mrow0'], writes=['modd'])
        S.dma('sp', lambda e: e.dma_start(out=K.modd[1:2, 0:2048], in_=mrow[1][:, 0:2048]), reads=['mrow1'], writes=['modd'])
        S.barrier()


def phase_inproj(K):
    nc, S = K.nc, K.S
    with ExitStack() as es:
        sb = lambda n, s, d: es.enter_context(nc.sbuf_tensor(_u(n), s, d))
        ps = lambda n, s, d: es.enter_context(nc.psum_tensor(_u(n), s, d))
        wb = sb("wb", [128, 8, 2560], BF16)
        wst = [sb("wst%d" % i, [128, 8, 320], F32) for i in range(2)]
        idt = sb("idt", [128, 128], BF16)
        epst = sb("epst", [128, 1], F32)
        scp = [sb("scp%d" % r, [128, 8], F32) for r in range(2)]
        shp = [sb("shp%d" % r, [128, 8], F32) for r in range(2)]
        xt = [sb("xt%d" % i, [128, 1024], F32) for i in range(2)]
        xn = [sb("xn%d" % i, [128, 1024], BF16) for i in range(2)]
        st = sb("st", [128, 2, 6], F32)
        mv = sb("mv", [128, 2], F32)
        lnv = sb("lnv", [128, 1], F32)
        rstd = sb("rstd", [128, 1], F32)
        xmT = [sb("xmT%d" % i, [128, 8, 512], BF16) for i in range(2)]
        stg = [sb("stg%d" % i, [128, 4, 512], F32) for i in range(2)]
        cst = [sb("cst%d" % i, [64, 512], F32) for i in range(2)]
        snt = [sb("snt%d" % i, [64, 512], F32) for i in range(2)]
        kr1 = sb("kr1", [64, 512], F32)
        kr2 = sb("kr2", [64, 512], F32)
        krb = sb("krb", [64, 512], BF16)
        pt = [ps("pt%d" % i, [128, 1024], BF16) for i in range(2)]
        po = [ps("po%d" % i, [128, 512], F32) for i in range(3)]
        pk = [ps("pk%d" % i, [64, 512], F32) for i in range(2)]

        S.dma('sp', lambda e: e.dma_start(out=idt[:], in_=K.ident), writes=['idt'])
        S.op('dve', lambda e: e.memset(epst[:], 1e-5), writes=['epst'])
        for r in range(2):
            S.dma('sp', lambda e: e.dma_start(out=shp[r][:], in_=K.modd[r:r + 1, 0:1024].rearrange("o (k p) -> p (o k)", p=128), allow_slow_non_contiguous=True), reads=['modd'], writes=['shp%d' % r])
            S.dma('sp', lambda e: e.dma_start(out=scp[r][:], in_=K.modd[r:r + 1, 1024:2048].rearrange("o (k p) -> p (o k)", p=128), allow_slow_non_contiguous=True), reads=['modd'], writes=['scp%d' % r])
            S.op('dve', lambda e: e.tensor_scalar_add(out=scp[r][:], in0=scp[r][:], scalar1=1.0), reads=['scp%d' % r], writes=['scp%d' % r])
        for j in range(8):
            w = wst[j % 2]
            wk = 'wst%d' % (j % 2)
            S.dma('sp', lambda e: e.dma_start(out=w[:], in_=K.w_in[:, j * 320:(j + 1) * 320].rearrange("(k p) n -> p k n", p=128)), writes=[wk])
            S.op('pool', lambda e: e.tensor_copy(out=wb[:, :, j * 320:(j + 1) * 320], in_=w[:]), reads=[wk], writes=['wb'])

        groups = [(0, 256, 1)] + [(256 + g * 512, 512, 0) for g in range(16)]
        tile_i = 0
        ev = 0
        def gbody(gi, t0, G, r):
            nonlocal tile_i, ev
            cs_, sn_ = cst[gi % 2], snt[gi % 2]
            csk, snk = 'cst%d' % (gi % 2), 'snt%d' % (gi % 2)
            xm = xmT[gi % 2]
            xmk = 'xmT%d' % (gi % 2)
            if r == 0:
                S.dma('sp', lambda e: e.dma_start(out=cs_[:], in_=K.cosT[:, t0 - 256:t0 - 256 + 512]), writes=[csk])
                S.dma('sp', lambda e: e.dma_start(out=sn_[:], in_=K.sinT[:, t0 - 256:t0 - 256 + 512]), writes=[snk])
            for i in range(G // 128):
                sl = tile_i % 2
                tile_i += 1
                xk, xnk, ptk = 'xt%d' % sl, 'xn%d' % sl, 'pt%d' % sl
                tt = t0 + i * 128
                S.dma('sp', lambda e: e.dma_start(out=xt[sl][:], in_=K.xin[tt:tt + 128, :]), writes=[xk])
                for c in range(2):
                    S.op('dve', lambda e: e.bn_stats(out=st[:, c, :], in_=xt[sl][:, c * 512:(c + 1) * 512]), reads=[xk], writes=['st%d' % c])
                S.op('dve', lambda e: e.bn_aggr(out=mv[:], in_=st[:]), reads=['st0', 'st1'], writes=['mv'])
                S.op('act', lambda e: e.activation(out=lnv[:], in_=mv[:, 1:2], func=AF.Ln, bias=epst[:], scale=1.0), reads=['mv', 'epst'], writes=['lnv'])
                S.op('act', lambda e: e.activation(out=rstd[:], in_=lnv[:], func=AF.Exp, scale=-0.5), reads=['lnv'], writes=['rstd'])
                S.op('dve', lambda e: e.tensor_scalar(out=xn[sl][:], in0=xt[sl][:], scalar1=mv[:, 0:1], scalar2=rstd[:], op0=ALU.subtract, op1=ALU.mult),
                     reads=[xk, 'mv', 'rstd'], writes=[xnk])
                for k in range(8):
                    S.op('pe', lambda e: e.transpose(out=pt[sl][:, k * 128:(k + 1) * 128], in_=xn[sl][:, k * 128:(k + 1) * 128], identity=idt[:]),
                         reads=[xnk, 'idt'], writes=[ptk], acc=True)
                for k in range(8):
                    S.op('act', lambda e: e.activation(out=xm[:, k, i * 128:(i + 1) * 128], in_=pt[sl][:, k * 128:(k + 1) * 128], func=AF.Identity,
                                                       bias=shp[r][:, k:k + 1], scale=scp[r][:, k:k + 1]),
                         reads=[ptk, 'shp%d' % r, 'scp%d' % r], writes=[xmk])
                yield 'f'
            yield 'F'
            for cb in range(5):
                sg = stg[cb % 2]
                sgk = 'stg%d' % (cb % 2)
                ncols = 4 if cb < 4 else 3
                for cc in range(ncols):
                    ci = cb * 4 + cc
                    p = po[ev % 3]
                    pkk = 'po%d' % (ev % 3)
                    for k in range(8):
                        S.op('pe', lambda e: e.matmul(p[:, 0:G], lhsT=wb[:, k, ci * 128:(ci + 1) * 128], rhs=xm[:, k, 0:G], start=(k == 0), stop=(k == 7)),
                             reads=['wb', xmk], writes=[pkk], acc=True)
                    if ev % 2 == 0:
                        S.op('dve', lambda e: e.tensor_copy(out=sg[:, cc, 0:G], in_=p[:, 0:G]), reads=[pkk], writes=[sgk])
                    else:
                        S.op('act', lambda e: e.activation(out=sg[:, cc, 0:G], in_=p[:, 0:G], func=AF.Identity), reads=[pkk], writes=[sgk])
                    ev += 1
                r0 = cb * 512
                S.dma('pool', lambda e: e.dma_start(out=K.hT[r0:r0 + ncols * 128, t0:t0 + G].rearrange("(c p) t -> p c t", p=128), in_=sg[:, 0:ncols, 0:G]),
                      reads=[sgk], writes=['hT'])
                yield 'b'
            for q in range(2):
                for k in range(8):
                    S.op('pe', lambda e: e.matmul(pk[q][:, 0:G], lhsT=wb[:, k, 2432 + q * 64:2432 + (q + 1) * 64], rhs=xm[:, k, 0:G], start=(k == 0), stop=(k == 7)),
                         reads=['wb', xmk], writes=['pk%d' % q], acc=True)
            if r == 0:
                S.op('dve', lambda e: e.tensor_tensor(out=kr1[:], in0=pk[0][:], in1=cs_[:], op=ALU.mult), reads=['pk0', csk], writes=['kr1'])
                S.op('dve', lambda e: e.tensor_tensor(out=kr2[:], in0=pk[1][:], in1=sn_[:], op=ALU.mult), reads=['pk1', snk], writes=['kr2'])
                S.op('dve', lambda e: e.tensor_tensor(out=krb[:], in0=kr1[:], in1=kr2[:], op=ALU.add), reads=['kr1', 'kr2'], writes=['krb'])
            else:
                S.op('dve', lambda e: e.tensor_copy(out=krb[:, 0:G], in_=pk[0][:, 0:G]), reads=['pk0', 'pk1'], writes=['krb'])
            S.dma('pool', lambda e: e.dma_start(out=K.krT[:, t0:t0 + G], in_=krb[:, 0:G]), reads=['krb'], writes=['krT'])
        def step(gen_):
            try:
                return next(gen_)
            except StopIteration:
                return None
        prev = None
        for gi, (t0, G, r) in enumerate(groups):
            gcur = gbody(gi, t0, G, r)
            while True:
                rr = step(gcur)
                if prev is not None and step(prev) is None:
                    prev = None
                if rr == 'F' or rr is None:
                    break
            while prev is not None:
                if step(prev) is None:
                    prev = None
            prev = gcur
        while prev is not None:
            if step(prev) is None:
                prev = None
        S.barrier()


class Rot:
    def __init__(self, bufs, prefix):
        self.bufs = bufs
        self.prefix = prefix
        self.i = 0

    def get(self):
        j = self.i % len(self.bufs)
        self.i += 1
        return self.bufs[j], '%s%d' % (self.prefix, j)


SCALE_ATT = 192.0 ** -0.5


def phase_mlaprep(K):
    nc, S = K.nc, K.S
    with ExitStack() as es:
        sb = lambda n, s, d: es.enter_context(nc.sbuf_tensor(_u(n), s, d))
        ps = lambda n, s, d: es.enter_context(nc.psum_tensor(_u(n), s, d))
        wuq = sb("wuq", [128, 2, 1024], BF16)
        wuk = sb("wuk", [128, 2, 512], BF16)
        wuv = sb("wuv", [128, 2, 512], BF16)
        wst = [sb("mwst%d" % i, [128, 2, 512], F32) for i in range(2)]
        gq = sb("gq", [128, 2], F32)
        gkv = sb("gkv", [128, 2], F32)
        onesf = sb("onesf", [128, 128], F32)
        epst = sb("epst", [128, 1], F32)
        ql = [sb("ql%d" % i, [128, 4, 512], F32) for i in range(2)]
        sq = sb("sq", [128, 4, 512], F32)
        lnt = sb("lnt", [128, 512], F32)
        rs = [sb("rs%d" % i, [128, 512], F32) for i in range(2)]
        nb = [sb("nb%d" % i, [128, 4, 512], BF16) for i in range(2)]
        qst = [sb("qst%d" % i, [128, 4, 512], BF16) for i in range(2)]
        kst = [sb("kst%d" % i, [128, 4, 512], BF16) for i in range(2)]
        qrb = [sb("qrb%d" % i, [64, 4, 512], BF16) for i in range(2)]
        vst = Rot([sb("vst%d" % i, [128, 512], BF16) for i in range(3)], "vst")
        cst = [sb("cst%d" % i, [64, 512], F32) for i in range(2)]
        snt = [sb("snt%d" % i, [64, 512], F32) for i in range(2)]
        r1 = sb("r1", [64, 512], F32)
        r2 = sb("r2", [64, 512], F32)
        pp = Rot([ps("mp%d" % i, [128, 512], F32) for i in range(7)], "mp")

        S.op('dve', lambda e: e.memset(epst[:], 1e-6), writes=['epst'])
        S.op('dve', lambda e: e.memset(onesf[:], 1.0), writes=['onesf'])
        S.dma('sp', lambda e: e.dma_start(out=gq[:], in_=K.mla_q_norm.rearrange("o (c p) -> p (o c)", p=128), allow_slow_non_contiguous=True), writes=['gq'])
        S.dma('sp', lambda e: e.dma_start(out=gkv[:], in_=K.mla_kv_norm.rearrange("o (c p) -> p (o c)", p=128), allow_slow_non_contiguous=True), writes=['gkv'])
        wl = 0
        for (src, dst, dk, n) in [(K.w_uq, wuq, 'wuq', 1024), (K.w_uk, wuk, 'wuk', 512), (K.w_uv, wuv, 'wuv', 512)]:
            for j in range(n // 512):
                w = wst[wl % 2]
                wk = 'mwst%d' % (wl % 2)
                wl += 1
                S.dma('sp', lambda e: e.dma_start(out=w[:], in_=src[:, j * 512:(j + 1) * 512].rearrange("(c p) n -> p c n", p=128)), writes=[wk])
                S.op('pool', lambda e: e.tensor_copy(out=dst[:, :, j * 512:(j + 1) * 512], in_=w[:]), reads=[wk], writes=[dk])

        groups = [(0, 256, 1)] + [(256 + g * 512, 512, 0) for g in range(16)]
        ev = [0]

        def evac(out_ap, in_ap, reads, writes):
            if ev[0] % 2 == 0:
                S.op('dve', lambda e: e.tensor_copy(out=out_ap, in_=in_ap), reads=reads, writes=writes)
            else:
                S.op('act', lambda e: e.activation(out=out_ap, in_=in_ap, func=AF.Identity), reads=reads, writes=writes)
            ev[0] += 1

        def gbody(gi, t0, G, r):
            cs_, sn_ = cst[gi % 2], snt[gi % 2]
            csk, snk = 'cst%d' % (gi % 2), 'snt%d' % (gi % 2)
            q_ = ql[gi % 2]
            qk = 'ql%d' % (gi % 2)
            n_ = nb[gi % 2]
            nk = 'nb%d' % (gi % 2)
            S.dma('sp', lambda e: e.dma_start(out=q_[:, :, 0:G], in_=K.hT[1920:2432, t0:t0 + G].rearrange("(c p) t -> p c t", p=128)), reads=['hT'], writes=[qk])
            if r == 0:
                S.dma('sp', lambda e: e.dma_start(out=cs_[:], in_=K.cosT[:, t0 - 256:t0 - 256 + 512]), writes=[csk])
                S.dma('sp', lambda e: e.dma_start(out=sn_[:], in_=K.sinT[:, t0 - 256:t0 - 256 + 512]), writes=[snk])
            S.op('act', lambda e: e.activation(out=sq[:, :, 0:G], in_=q_[:, :, 0:G], func=AF.Square), reads=[qk], writes=['sq'])
            for pair in range(2):
                if pair == 0 and r == 1:
                    continue
                p, pk = pp.get()
                for c in range(2):
                    S.op('pe', lambda e: e.matmul(p[:, 0:G], lhsT=onesf[:], rhs=sq[:, pair * 2 + c, 0:G], start=(c == 0), stop=(c == 1)),
                         reads=['onesf', 'sq'], writes=[pk], acc=True)
                S.op('act', lambda e: e.activation(out=lnt[:, 0:G], in_=p[:, 0:G], func=AF.Ln, bias=epst[:], scale=1.0 / 256.0), reads=[pk, 'epst'], writes=['lnt'])
                S.op('act', lambda e: e.activation(out=rs[pair][:, 0:G], in_=lnt[:, 0:G], func=AF.Exp, scale=-0.5), reads=['lnt'], writes=['rs%d' % pair])
                g_ = gq if pair == 0 else gkv
                for c in range(2):
                    S.op('dve', lambda e: e.scalar_tensor_tensor(out=n_[:, pair * 2 + c, 0:G], in0=q_[:, pair * 2 + c, 0:G], scalar=g_[:, c:c + 1], in1=rs[pair][:, 0:G],
                                                                 op0=ALU.mult, op1=ALU.mult),
                         reads=[qk, 'rs%d' % pair, 'gq', 'gkv'], writes=[nk])
            yield 'F'
            if r == 0:
                tq = t0 - 256
                qs = qst[gi % 2]
                qsk = 'qst%d' % (gi % 2)
                qr_ = qrb[gi % 2]
                qrk = 'qrb%d' % (gi % 2)
                for h in range(4):
                    p, pk = pp.get()
                    for c in range(2):
                        S.op('pe', lambda e: e.matmul(p[:, 0:G], lhsT=wuq[:, c, h * 256:h * 256 + 128], rhs=n_[:, c, 0:G], start=(c == 0), stop=(c == 1)),
                             reads=['wuq', nk], writes=[pk], acc=True)
                    evac(qs[:, h, 0:G], p[:, 0:G], [pk], [qsk])
                    p1, pk1 = pp.get()
                    p2, pk2 = pp.get()
                    for c in range(2):
                        S.op('pe', lambda e: e.matmul(p1[0:64, 0:G], lhsT=wuq[:, c, h * 256 + 128:h * 256 + 192], rhs=n_[:, c, 0:G], start=(c == 0), stop=(c == 1)),
                             reads=['wuq', nk], writes=[pk1], acc=True)
                    for c in range(2):
                        S.op('pe', lambda e: e.matmul(p2[0:64, 0:G], lhsT=wuq[:, c, h * 256 + 192:h * 256 + 256], rhs=n_[:, c, 0:G], start=(c == 0), stop=(c == 1)),
                             reads=['wuq', nk], writes=[pk2], acc=True)
                    S.op('dve', lambda e: e.tensor_tensor(out=r1[:], in0=p1[0:64, :], in1=cs_[:], op=ALU.mult), reads=[pk1, csk], writes=['r1'])
                    S.op('dve', lambda e: e.tensor_tensor(out=r2[:], in0=p2[0:64, :], in1=sn_[:], op=ALU.mult), reads=[pk2, snk], writes=['r2'])
                    S.op('dve', lambda e: e.tensor_tensor(out=qr_[:, h, :], in0=r1[:], in1=r2[:], op=ALU.add), reads=['r1', 'r2'], writes=[qrk])
                    yield 'b'
                S.dma('pool', lambda e: e.dma_start(out=K.qnT[:, tq:tq + G].rearrange("(h p) t -> p h t", p=128), in_=qs[:, :, 0:G]), reads=[qsk], writes=['qnT'])
                S.dma('pool', lambda e: e.dma_start(out=K.qrT[:, tq:tq + G].rearrange("(h p) t -> p h t", p=64), in_=qr_[:, :, 0:G]), reads=[qrk], writes=['qrT'])
            ks = kst[gi % 2]
            ksk = 'kst%d' % (gi % 2)
            for h in range(4):
                p, pk = pp.get()
                for c in range(2):
                    S.op('pe', lambda e: e.matmul(p[:, 0:G], lhsT=wuk[:, c, h * 128:(h + 1) * 128], rhs=n_[:, 2 + c, 0:G], start=(c == 0), stop=(c == 1)),
                         reads=['wuk', nk], writes=[pk], acc=True)
                evac(ks[:, h, 0:G], p[:, 0:G], [pk], [ksk])
                yield 'b'
            S.dma('pool', lambda e: e.dma_start(out=K.knT[:, t0:t0 + G].rearrange("(h p) t -> p h t", p=128), in_=ks[:, :, 0:G]), reads=[ksk], writes=['knT'])
            for i in range(G // 128):
                p, pk = pp.get()
                for c in range(2):
                    S.op('pe', lambda e: e.matmul(p[:, :], lhsT=n_[:, 2 + c, i * 128:(i + 1) * 128], rhs=wuv[:, c, :], start=(c == 0), stop=(c == 1)),
                         reads=['wuv', nk], writes=[pk], acc=True)
                v_, vk = vst.get()
                evac(v_[:], p[:], [pk], [vk])
                S.dma('pool', lambda e: e.dma_start(out=K.vtok[t0 + i * 128:t0 + (i + 1) * 128, :], in_=v_[:]), reads=[vk], writes=['vtok'])
        def step(gen_):
            try:
                return next(gen_)
            except StopIteration:
                return None
        prev = None
        for gi, (t0, G, r) in enumerate(groups):
            gcur = gbody(gi, t0, G, r)
            while True:
                rr = step(gcur)
                if prev is not None and step(prev) is None:
                    prev = None
                if rr == 'F' or rr is None:
                    break
            while prev is not None:
                if step(prev) is None:
                    prev = None
            prev = gcur
        while prev is not None:
            if step(prev) is None:
                prev = None
        S.barrier()


def phase_attn(K, heads=(0, 1, 2, 3), nqt=16):
    nc, S = K.nc, K.S
    NKT = TA // 128
    with ExitStack() as es:
        sb = lambda n, s, d: es.enter_context(nc.sbuf_tensor(_u(n), s, d))
        ps = lambda n, s, d: es.enter_context(nc.psum_tensor(_u(n), s, d))
        krs = sb("krs", [64, TA], BF16)
        kn = [sb("kn%d" % i, [128, TA], BF16) for i in range(2)]
        vh = [sb("vh%d" % i, [128, NKT, 128], BF16) for i in range(2)]
        qn = [sb("qn%d" % i, [128, 512], BF16) for i in range(2)]
        qr = [sb("qr%d" % i, [64, 512], BF16) for i in range(2)]
        onesb = sb("onesb", [128, 128], BF16)
        pT = Rot([sb("pT%d" % i, [128, 512], BF16) for i in range(3)], "pT")
        rl = sb("rl", [128, 512], F32)
        ob = [sb("ob%d" % i, [128, 512], BF16) for i in range(2)]
        psc = Rot([ps("psc%d" % i, [128, 512], F32) for i in range(3)], "psc")
        pO = [ps("pO%d" % i, [128, 512], F32) for i in range(2)]
        pL = [ps("pL%d" % i, [128, 512], F32) for i in range(2)]

        S.op('dve', lambda e: e.memset(onesb[:], 1.0), writes=['onesb'])
        S.dma('sp', lambda e: e.dma_start(out=krs[:], in_=K.krT), reads=['krT'], writes=['krs'])
        qi = 0
        for hi, h in enumerate(heads):
            k_ = kn[hi % 2]
            kk = 'kn%d' % (hi % 2)
            v_ = vh[hi % 2]
            vk = 'vh%d' % (hi % 2)
            S.dma('sp', lambda e: e.dma_start(out=k_[:], in_=K.knT[h * 128:(h + 1) * 128, :]), reads=['knT'], writes=[kk])
            S.dma('sp', lambda e: e.dma_start(out=v_[:], in_=K.vtok[:, h * 128:(h + 1) * 128].rearrange("(kt p) d -> p kt d", p=128)), reads=['vtok'], writes=[vk])
            for qt in range(nqt):
                sl = qi % 2
                qi += 1
                qnk, qrk = 'qn%d' % sl, 'qr%d' % sl
                S.dma('sp', lambda e: e.dma_start(out=qn[sl][:], in_=K.qnT[h * 128:(h + 1) * 128, qt * 512:(qt + 1) * 512]), reads=['qnT'], writes=[qnk])
                S.dma('sp', lambda e: e.dma_start(out=qr[sl][:], in_=K.qrT[h * 64:(h + 1) * 64, qt * 512:(qt + 1) * 512]), reads=['qrT'], writes=[qrk])
                Ok, Lk = 'pO%d' % sl, 'pL%d' % sl
                pend = []

                def scores(kt):
                    p, pk = psc.get()
                    S.op('pe', lambda e: e.matmul(p[:], lhsT=k_[:, kt * 128:(kt + 1) * 128], rhs=qn[sl][:], start=True, stop=False),
                         reads=[kk, qnk], writes=[pk], acc=True)
                    S.op('pe', lambda e: e.matmul(p[:], lhsT=krs[:, kt * 128:(kt + 1) * 128], rhs=qr[sl][:], start=False, stop=True),
                         reads=['krs', qrk], writes=[pk], acc=True)
                    t_, tk = pT.get()
                    S.op('act', lambda e: e.activation(out=t_[:], in_=p[:], func=AF.Exp, scale=SCALE_ATT), reads=[pk], writes=[tk])
                    pend.append((kt, t_, tk))

                def pv():
                    kt, t_, tk = pend.pop(0)
                    S.op('pe', lambda e: e.matmul(pO[sl][:], lhsT=v_[:, kt, :], rhs=t_[:], start=(kt == 0), stop=(kt == NKT - 1)),
                         reads=[vk, tk], writes=[Ok], acc=True)
                    S.op('pe', lambda e: e.matmul(pL[sl][:], lhsT=onesb[:], rhs=t_[:], start=(kt == 0), stop=(kt == NKT - 1)),
                         reads=['onesb', tk], writes=[Lk], acc=True)

                scores(0)
                scores(1)
                for kt in range(NKT):
                    pv()
                    if kt + 2 < NKT:
                        scores(kt + 2)
                S.op('dve', lambda e: e.reciprocal(out=rl[:], in_=pL[sl][:]), reads=[Lk], writes=['rl'])
                S.op('dve', lambda e: e.tensor_tensor(out=ob[sl][:], in0=pO[sl][:], in1=rl[:], op=ALU.mult), reads=[Ok, 'rl'], writes=['ob%d' % sl])
                S.dma('pool', lambda e: e.dma_start(out=K.mixT[512 + h * 128:512 + (h + 1) * 128, qt * 512:(qt + 1) * 512], in_=ob[sl][:]), reads=['ob%d' % sl], writes=['mixT'])
        S.barrier()


F32R = mybir.dt.float32r
LDS = -0.6065306597126334


class LaneS:
    LOCAL = ('cv', 'tw', 'sg', 'kq', 'sq', 'lnt', 'kk_', 'av', 'tt', 'kd0', 'kd1', 'bb', 'gg', 'bc', 'bon')

    def __init__(self, S, lane):
        self.S = S
        self.lane = lane

    def k(self, x):
        if x == 'bones':
            return x
        for p in self.LOCAL:
            if x.startswith(p):
                return '%s@%d' % (x, self.lane)
        return x

    def op(self, e, fn, reads=(), writes=(), acc=False):
        return self.S.op(e, fn, [self.k(x) for x in reads], [self.k(x) for x in writes], acc)

    def dma(self, e, fn, reads=(), writes=()):
        return self.S.dma(e, fn, [self.k(x) for x in reads], [self.k(x) for x in writes])


def phase_rwprep(K):
    nc, S = K.nc, K.S
    with ExitStack() as es:
        sb = lambda n, s, d: es.enter_context(nc.sbuf_tensor(_u(n), s, d))
        ps = lambda n, s, d: es.enter_context(nc.psum_tensor(_u(n), s, d))
        G = 256
        cw = sb("cw", [128, 3, 12], F32)
        kkv = sb("kkv", [128, 4], F32)
        kav = sb("kav", [128, 4], F32)
        omka = sb("omka", [128, 4], F32)
        rkv = sb("rkv", [128, 4], F32)
        a0v = sb("a0v", [128, 2, 4], F32)
        w0r = sb("w0r", [1, 2, 512], F32)
        ones1 = sb("ones1", [1, 128], F32)
        wup = sb("wup", [64, 2, 512], F32)
        aup = sb("aup", [64, 2, 512], F32)
        gup = sb("gup", [128, 512], F32)
        bones = sb("bones", [128, 128], F32)
        idf = sb("idf", [128, 128], F32)
        eps12 = sb("eps12", [128, 1], F32)
        hr = [sb("hr%d" % i, [128, 12, G + 2], F32) for i in range(2)]
        lo = [sb("lo%d" % i, [64, 4, G], F32) for i in range(2)]
        gd = [sb("gd%d" % i, [128, G], F32) for i in range(2)]
        cvL = [sb("cv_%d" % i_, [128, 12, G], F32) for i_ in range(2)]
        twL = [sb("tw_%d" % i_, [64, 2, G], F32R) for i_ in range(2)]
        sgL = [sb("sg_%d" % i_, [128, G], F32R) for i_ in range(2)]
        kqL = [sb("kq_%d" % i_, [128, 4, G], F32) for i_ in range(2)]
        sqL = [sb("sq_%d" % i_, [128, 4, G], F32R) for i_ in range(2)]
        lntL = [sb("lnt_%d" % i_, [128, 4, G], F32) for i_ in range(2)]
        kk_L = [sb("kk__%d" % i_, [128, 4, G], F32) for i_ in range(2)]
        avL = [sb("av_%d" % i_, [128, 4, G], F32) for i_ in range(2)]
        ttL = [sb("tt_%d" % i_, [128, 4, G], F32) for i_ in range(2)]
        kdL = [[sb("kd%d_%d" % (i, i_), [128, 4, G], F32) for i in range(2)] for i_ in range(2)]
        bbL = [sb("bb_%d" % i_, [128, 4, G], F32) for i_ in range(2)]
        ld = Rot([sb("ld%d" % i, [128, 512], F32) for i in range(2)], "ld")
        vt = Rot([sb("vt%d" % i, [128, 512], F32) for i in range(2)], "vt")
        ggL = [sb("gg_%d" % i_, [128, 4, G], F32) for i_ in range(2)]
        bcL = [sb("bc_%d" % i_, [128, 4, G], F32R) for i_ in range(2)]
        bonL = [sb("bon_%d" % i_, [128, 4, G], F32) for i_ in range(2)]
        pp = Rot([ps("rp%d" % i, [128, 512], F32) for i in range(7)], "rp")

        ld1 = lambda dst, src, key: S.dma('sp', lambda e: e.dma_start(out=dst, in_=src, allow_slow_non_contiguous=True), writes=[key])
        ld1(cw[:], K.rwkv_conv.rearrange("t (c p) -> p t c", p=128), 'cw')
        ld1(kkv[:], K.rwkv_k_k.rearrange("o (c p) -> p (o c)", p=128), 'kkv')
        ld1(kav[:], K.rwkv_k_a.rearrange("o (c p) -> p (o c)", p=128), 'kav')
        ld1(rkv[:], K.rwkv_r_k.rearrange("o (c p) -> p (o c)", p=128), 'rkv')
        ld1(a0v[:], K.rwkv_a0.rearrange("d (c p) -> p d c", p=128), 'a0v')
        ld1(w0r[:], K.rwkv_w0.rearrange("(o d) n -> o d n", o=1), 'w0r')
        ld1(wup[:], K.rwkv_w_up.rearrange("d l n -> l d n"), 'wup')
        ld1(aup[:], K.rwkv_a_up.rearrange("d l n -> l d n"), 'aup')
        ld1(gup[:], K.rwkv_g_up, 'gup')
        ld1(bones[:], K.bones, 'bones')
        ld1(idf[:], K.identf, 'idf')
        S.op('dve', lambda e: e.memset(ones1[:], 1.0), writes=['ones1'])
        wupr = sb("wupr", [64, 2, 512], F32R)
        aupr = sb("aupr", [64, 2, 512], F32R)
        gupr = sb("gupr", [128, 512], F32R)
        bonesr = sb("bonesr", [128, 128], F32R)
        ones1r = sb("ones1r", [1, 128], F32R)
        w0rr = sb("w0rr", [1, 2, 512], F32R)
        lor = [sb("lor%d" % i_, [64, 2, G], F32R) for i_ in range(2)]
        for src_, dst_, k_ in ((wup, wupr, 'wup'), (aup, aupr, 'aup'), (gup, gupr, 'gup'), (bones, bonesr, 'bones'), (ones1, ones1r, 'ones1'), (w0r, w0rr, 'w0r')):
            S.op('dve', lambda e: e.tensor_copy(out=dst_[:], in_=src_[:]), reads=[k_], writes=[k_ + 'r'])
        S.op('dve', lambda e: e.memset(eps12[:], 1e-12), writes=['eps12'])
        S.op('dve', lambda e: e.tensor_scalar(out=omka[:], in0=kav[:], scalar1=-1.0, scalar2=1.0, op0=ALU.mult, op1=ALU.add), reads=['kav'], writes=['omka'])

        nblk = TA // G
        def bbody(bi):
            ln = bi % 2
            S_ = LaneS(S, ln)
            cv, tw, sg, kq, sq, lnt, kk_, av, tt, bb, gg, bc, bon = cvL[ln], twL[ln], sgL[ln], kqL[ln], sqL[ln], lntL[ln], kk_L[ln], avL[ln], ttL[ln], bbL[ln], ggL[ln], bcL[ln], bonL[ln]
            kd = kdL[ln]
            t0 = bi * G
            lat = t0 >= TC
            sl = bi % 2
            h_ = hr[sl]
            hk = 'hr%d' % sl
            first = (t0 == 0 or t0 == TC)
            last = (t0 + G == TC or t0 + G == TA)
            c0 = 1 if first else 0
            c1 = G + 1 if last else G + 2
            if first:
                S_.op('pool', lambda e: e.memset(h_[:, :, 0:1], 0.0), writes=[hk])
            if last:
                S_.op('pool', lambda e: e.memset(h_[:, :, G + 1:G + 2], 0.0), writes=[hk])
            for q in range(3):
                S_.dma('sp', lambda e: e.dma_start(out=h_[:, q * 4:(q + 1) * 4, c0:c1], in_=K.hT[q * 512:(q + 1) * 512, t0 - 1 + c0:t0 - 1 + c1].rearrange("(c p) t -> p c t", p=128)),
                      reads=['hT'], writes=[hk])
            lo_ = lo[sl]
            lk = 'lo%d' % sl
            S_.dma('sp', lambda e: e.dma_start(out=lo_[:], in_=K.hT[1536:1792, t0:t0 + G].rearrange("(c p) t -> p c t", p=64)), reads=['hT'], writes=[lk])
            gd_ = gd[sl]
            gk = 'gd%d' % sl
            if lat:
                S_.dma('sp', lambda e: e.dma_start(out=gd_[:], in_=K.hT[1792:1920, t0:t0 + G]), reads=['hT'], writes=[gk])
            for c in range(12):
                S_.op('act', lambda e: e.activation(out=cv[:, c, :], in_=h_[:, c, 1:G + 1], func=AF.Identity, scale=cw[:, 1, c:c + 1]), reads=[hk, 'cw'], writes=['cv%d' % c])
                S_.op('dve', lambda e: e.scalar_tensor_tensor(out=cv[:, c, :], in0=h_[:, c, 0:G], scalar=cw[:, 0, c:c + 1], in1=cv[:, c, :], op0=ALU.mult, op1=ALU.add),
                     reads=[hk, 'cw', 'cv%d' % c], writes=['cv%d' % c])
                S_.op('dve', lambda e: e.scalar_tensor_tensor(out=cv[:, c, :], in0=h_[:, c, 2:G + 2], scalar=cw[:, 2, c:c + 1], in1=cv[:, c, :], op0=ALU.mult, op1=ALU.add),
                     reads=[hk, 'cw', 'cv%d' % c], writes=['cv%d' % c])
            cvr = ['cv%d' % c for c in range(0, 4)]
            cvk = ['cv%d' % c for c in range(4, 8)]
            cvv = ['cv%d' % c for c in range(8, 12)]
            S_.dma('pool', lambda e: e.dma_start(out=K.rwR[:, t0:t0 + G].rearrange("(c p) t -> p c t", p=128), in_=cv[:, 0:4, :]), reads=cvr, writes=['rwR'])
            S_.dma('pool', lambda e: e.dma_start(out=K.rwV[:, t0:t0 + G].rearrange("(c p) t -> p c t", p=128), in_=cv[:, 8:12, :]), reads=cvv, writes=['rwV'])
            yield 'f'
            for i in range(G // 128):
                p, pk = pp.get()
                for c in range(4):
                    S_.op('pe', lambda e: e.transpose(out=p[:, c * 128:(c + 1) * 128], in_=cv[:, 8 + c, i * 128:(i + 1) * 128], identity=idf[:]), reads=cvv + ['idf'], writes=[pk], acc=True)
                v_, vk = vt.get()
                S_.op('act', lambda e: e.activation(out=v_[:], in_=p[:], func=AF.Identity), reads=[pk], writes=[vk])
                S_.dma('pool', lambda e: e.dma_start(out=K.rwVtok[t0 + i * 128:t0 + (i + 1) * 128, :], in_=v_[:]), reads=[vk], writes=['rwVtok'])
                yield 'f'
            for c in range(4):
                S_.op('dve', lambda e: e.tensor_scalar_mul(out=kq[:, c, :], in0=cv[:, 4 + c, :], scalar1=kkv[:, c:c + 1]), reads=cvk + ['kkv'], writes=['kq'])
            S_.op('act', lambda e: e.activation(out=sq[:], in_=kq[:], func=AF.Square), reads=['kq'], writes=['sq'])
            for c2 in range(2):
                p, pk = pp.get()
                S_.op('pe', lambda e: e.matmul(p[:, 0:2 * G], lhsT=bonesr[:], rhs=sq[:, 2 * c2:2 * c2 + 2, :], start=True, stop=True), reads=['bonesr', 'sq'], writes=[pk])
                S_.op('act', lambda e: e.activation(out=lnt[:, 2 * c2:2 * c2 + 2, :], in_=p[:, 0:2 * G], func=AF.Ln, bias=eps12[:], scale=1.0), reads=[pk, 'eps12'], writes=['lnt'])
            S_.op('act', lambda e: e.activation(out=lnt[:], in_=lnt[:], func=AF.Exp, scale=-0.5), reads=['lnt'], writes=['lnt'])
            S_.op('dve', lambda e: e.tensor_tensor(out=kk_[:], in0=kq[:], in1=lnt[:], op=ALU.mult), reads=['kq', 'lnt'], writes=['kk_'])
            S_.dma('pool', lambda e: e.dma_start(out=K.rwKK[:, t0:t0 + G].rearrange("(c p) t -> p c t", p=128), in_=kk_[:]), reads=['kk_'], writes=['rwKK'])
            yield 'F'
            S_.op('act', lambda e: e.activation(out=tw[:], in_=lo_[:, 0:2, :], func=AF.Tanh), reads=[lk], writes=['tw'])
            S_.op('act', lambda e: e.activation(out=lor[ln][:], in_=lo_[:, 2:4, :], func=AF.Identity), reads=[lk], writes=['lor%d' % ln])
            for d in range(2):
                for i in range(G // 128):
                    p, pk = pp.get()
                    S_.op('pe', lambda e: e.matmul(p[:], lhsT=tw[:, d, i * 128:(i + 1) * 128], rhs=wupr[:, d, :], start=True, stop=False), reads=['tw', 'wupr'], writes=[pk], acc=True)
                    S_.op('pe', lambda e: e.matmul(p[:], lhsT=ones1r[:], rhs=w0rr[:, d, :], start=False, stop=True), reads=['ones1r', 'w0rr'], writes=[pk], acc=True)
                    l_, lk2 = ld.get()
                    S_.op('act', lambda e: e.activation(out=l_[:], in_=p[:], func=AF.Sigmoid), reads=[pk], writes=[lk2])
                    S_.dma('pool', lambda e: e.dma_start(out=K.rwLD[d][t0 + i * 128:t0 + (i + 1) * 128, :], in_=l_[:]), reads=[lk2], writes=['rwLD%d' % d])
                    yield 'b'
                for c in range(4):
                    p, pk = pp.get()
                    S_.op('pe', lambda e: e.matmul(p[:, 0:G], lhsT=aupr[:, d, c * 128:(c + 1) * 128], rhs=lor[ln][:, d, :], start=True, stop=True), reads=['aupr', 'lor%d' % ln], writes=[pk])
                    S_.op('act', lambda e: e.activation(out=av[:, c, :], in_=p[:, 0:G], func=AF.Sigmoid, bias=a0v[:, d, c:c + 1], scale=1.0), reads=[pk, 'a0v'], writes=['av'])
                    S_.op('dve', lambda e: e.tensor_scalar(out=tt[:, c, :], in0=av[:, c, :], scalar1=kav[:, c:c + 1], scalar2=omka[:, c:c + 1], op0=ALU.mult, op1=ALU.add),
                         reads=['av', 'kav', 'omka'], writes=['tt'])
                S_.op('dve', lambda e: e.tensor_tensor(out=kd[d][:], in0=cv[:, 4:8, :], in1=tt[:], op=ALU.mult), reads=cvk + ['tt'], writes=['kd%d' % d])
                S_.op('dve', lambda e: e.tensor_tensor(out=bb[:], in0=kk_[:], in1=av[:], op=ALU.mult), reads=['kk_', 'av'], writes=['bb'])
                S_.dma('pool', lambda e: e.dma_start(out=K.rwKD[d][:, t0:t0 + G].rearrange("(c p) t -> p c t", p=128), in_=kd[d][:]), reads=['kd%d' % d], writes=['rwKD%d' % d])
                S_.dma('pool', lambda e: e.dma_start(out=K.rwB[d][:, t0:t0 + G].rearrange("(c p) t -> p c t", p=128), in_=bb[:]), reads=['bb'], writes=['rwB%d' % d])
                yield 'b'
            if lat:
                tl = t0 - TC
                S_.op('act', lambda e: e.activation(out=sg[:], in_=gd_[:], func=AF.Sigmoid), reads=[gk], writes=['sg'])
                for c in range(4):
                    p, pk = pp.get()
                    S_.op('pe', lambda e: e.matmul(p[:, 0:G], lhsT=gupr[:, c * 128:(c + 1) * 128], rhs=sg[:], start=True, stop=True), reads=['gupr', 'sg'], writes=[pk])
                    S_.op('act', lambda e: e.activation(out=gg[:, c, :], in_=p[:, 0:G], func=AF.Identity), reads=[pk], writes=['gg'])
                S_.dma('pool', lambda e: e.dma_start(out=K.rwG[:, tl:tl + G].rearrange("(c p) t -> p c t", p=128), in_=gg[:]), reads=['gg'], writes=['rwG'])
                yield 'b'
                S_.op('dve', lambda e: e.tensor_tensor(out=bc[:], in0=kd[0][:], in1=kd[1][:], op=ALU.add), reads=['kd0', 'kd1'], writes=['bc'])
                S_.op('dve', lambda e: e.tensor_tensor(out=bc[:], in0=bc[:], in1=cv[:, 0:4, :], op=ALU.mult), reads=['bc'] + cvr, writes=['bc'])
                for c in range(4):
                    S_.op('dve', lambda e: e.tensor_scalar_mul(out=bc[:, c, :], in0=bc[:, c, :], scalar1=rkv[:, c:c + 1]), reads=['bc', 'rkv'], writes=['bc'])
                for c2 in range(2):
                    p, pk = pp.get()
                    S_.op('pe', lambda e: e.matmul(p[:, 0:2 * G], lhsT=bonesr[:], rhs=bc[:, 2 * c2:2 * c2 + 2, :], start=True, stop=True), reads=['bonesr', 'bc'], writes=[pk])
                    S_.op('dve', lambda e: e.tensor_tensor(out=bon[:, 2 * c2:2 * c2 + 2, :], in0=p[:, 0:2 * G], in1=cv[:, 8 + 2 * c2:8 + 2 * c2 + 2, :], op=ALU.mult), reads=[pk] + cvv, writes=['bon'])
                S_.dma('pool', lambda e: e.dma_start(out=K.rwBON[:, tl:tl + G].rearrange("(c p) t -> p c t", p=128), in_=bon[:]), reads=['bon'], writes=['rwBON'])
        def step(gen_):
            try:
                return next(gen_)
            except StopIteration:
                return None
        prev = None
        for bi in range(nblk):
            gcur = bbody(bi)
            while True:
                r = step(gcur)
                if prev is not None and step(prev) is None:
                    prev = None
                if r == 'F' or r is None:
                    break
            while prev is not None:
                if step(prev) is None:
                    prev = None
            prev = gcur
        while prev is not None:
            if step(prev) is None:
                prev = None
        S.barrier()


def rw_consts():
    i = np.arange(128)[:, None]
    t = np.arange(128)[None, :]
    bd = (i // 64) == (t // 64)
    out = {}
    blk = ((i // 64) == (t // 64)).astype(np.float32)
    for d in range(2):
        strict = (bd & ((i < t) if d == 0 else (i > t))).astype(np.float32)
        incl = (bd & ((i <= t) if d == 0 else (i >= t))).astype(np.float32)
        m1 = np.concatenate([-strict, incl, blk], 1)
        m2 = np.concatenate([incl, blk], 1)
        m3 = np.concatenate([-strict.T, -strict.T, -np.ones((128, 64), np.float32)], 1)
        cum = np.float32(LDS) * np.concatenate([incl, strict, strict.T], 1)
        out['rwm%d' % d] = np.ascontiguousarray(np.concatenate([m1, m2, m3, cum], 1).astype(np.float32))
    out['i64dbl'] = np.ascontiguousarray((np.arange(128)[:, None] % 64 == np.arange(128)[None, :] % 64).astype(np.float32))
    out['bones'] = np.ascontiguousarray(bd.astype(np.float32))
    out['identf'] = np.eye(128, dtype=np.float32)
    return out


GN_EPS = 64e-5


def phase_rwscan(K, ntile_lat=64):
    nc, S = K.nc, K.S
    G = 128
    with ExitStack() as es:
        sb = lambda n, s, d: es.enter_context(nc.sbuf_tensor(_u(n), s, d))
        ps = lambda n, s, d: es.enter_context(nc.psum_tensor(_u(n), s, d))
        mk = sb("mk", [128, 1344], F32)
        idr = sb("idr", [128, 128], F32R)
        i64r = sb("i64r", [128, 128], F32R)
        i64f = sb("i64f", [128, 128], F32)
        idf = sb("idf", [128, 128], F32)
        o64 = sb("o64", [64, 64], F32)
        gng = sb("gng", [64, 8], F32)
        gnb = sb("gnb", [64, 8], F32)
        epsg = sb("epsg", [64, 1], F32)
        raw = [[sb("raw%d_%d" % (j, i), [128, 4, G], F32) for i in range(2)] for j in range(4)]
        vraw = [sb("vraw%d" % i, [128, 512], F32) for i in range(2)]
        ldt = [sb("ldt%d" % i, [128, 512], F32) for i in range(2)]
        vtr = [sb("vtr%d" % i, [128, 512], F32R) for i in range(2)]
        Ep = sb("Ep", [128, 4, G], F32)
        Em = sb("Em", [128, 4, G], F32)
        Ex = sb("Ex", [128, 4, G], F32)
        Ea = sb("Ea", [128, 4, G], F32)
        KRG = sb("KRG", [128, 4, 3, 128], F32R)
        BK = sb("BK", [128, 4, 2, 128], F32R)
        KB2 = sb("KB2", [128, 4, 2, 128], F32R)
        NS = 4
        La = [sb("La%d" % i, [128, 384], F32R) for i in range(NS)]
        Lb = [sb("Lb%d" % i, [128, 384], F32R) for i in range(NS)]
        ATa = [sb("ATa%d" % i, [128, 128], F32R) for i in range(NS)]
        ATb = [sb("ATb%d" % i, [128, 128], F32R) for i in range(NS)]
        X1 = [sb("X1%d" % i, [128, 320], F32R) for i in range(NS)]
        X2 = [sb("X2%d" % i, [128, 256], F32R) for i in range(NS)]
        Xf = [sb("Xf%d" % i, [128, 256], F32R) for i in range(NS)]
        QG1 = [sb("QG1_%d" % i, [64, 8, 256], F32R) for i in range(2)]
        QG2 = [sb("QG2_%d" % i, [128, 8, 256], F32R) for i in range(2)]
        Sth = sb("Sth", [64, 3, 8, 64], F32R)
        T = [sb("T%d" % i, [64, 8, G], F32) for i in range(4)]
        outb = sb("outb", [64, 8, G], BF16)
        pp = Rot([ps("sp%d" % i, [128, 512], F32) for i in range(7)], "sp")
        pst = ps("pst", [64, 512], F32)

        ld1 = lambda dst, src, key: S.dma('sp', lambda e: e.dma_start(out=dst, in_=src, allow_slow_non_contiguous=True), writes=[key])
        ld1(idf[:], K.identf, 'idf')
        ld1(i64f[:], K.i64dbl, 'i64f')
        ld1(gng[:], K.rwkv_gn_g.rearrange("o (h p) -> p (o h)", p=64), 'gng')
        ld1(gnb[:], K.rwkv_gn_b.rearrange("o (h p) -> p (o h)", p=64), 'gnb')
        S.op('dve', lambda e: e.tensor_copy(out=idr[:], in_=idf[:]), reads=['idf'], writes=['idr'])
        S.op('dve', lambda e: e.tensor_copy(out=i64r[:], in_=i64f[:]), reads=['i64f'], writes=['i64r'])
        S.op('dve', lambda e: e.memset(o64[:], 1.0 / 64.0), writes=['o64'])
        S.op('dve', lambda e: e.memset(epsg[:], GN_EPS), writes=['epsg'])
        zt = sb("zt", [64, 512], F32)
        S.op('dve', lambda e: e.memset(zt[:], 0.0), writes=['zt'])

        evq = [0]

        def evac(out_ap, in_ap, reads, writes):
            if evq[0] % 2 == 0:
                S.op('act', lambda e: e.activation(out=out_ap, in_=in_ap, func=AF.Identity), reads=reads, writes=writes)
            else:
                S.op('dve', lambda e: e.tensor_copy(out=out_ap, in_=in_ap), reads=reads, writes=writes)
            evq[0] += 1

        fl = lambda a: a[:, :, :].rearrange("p h t -> p (h t)")
        bcount = 0
        for d in range(2):
            m1 = mk[:, 0:384]
            m2 = mk[:, 384:640]
            m3 = mk[:, 640:960]
            cum = mk[:, 960:1344]
            S.dma('sp', lambda e: e.dma_start(out=mk[:], in_=K.rwm[d]), writes=['mk'])
            S.op('dve', lambda e: e.tensor_copy(out=Sth[:, 0].rearrange("p h v -> p (h v)"), in_=zt[:]), reads=['zt'], writes=['Sth0'])
            if d == 0:
                blocks = [0, 1] + list(range(2, 2 + ntile_lat))
            else:
                blocks = [1, 0] + list(range(1 + ntile_lat, 1, -1))
            chunks = [0, 1] if d == 0 else [1, 0]
            def body(b, qs):
                nonlocal bcount
                t0 = b * G
                lat = b >= 2
                sl = bcount % 2
                bcount += 1
                srcs = [K.rwR, K.rwKD[d], K.rwKK, K.rwB[d]]
                rk = ['raw%d_%d' % (j, sl) for j in range(4)]
                for j in range(4):
                    S.dma('sp', lambda e: e.dma_start(out=raw[j][sl][:], in_=srcs[j][:, t0:t0 + G].rearrange("(c p) t -> p c t", p=128)), writes=[rk[j]])
                S.dma('sp', lambda e: e.dma_start(out=vraw[sl][:], in_=K.rwVtok[t0:t0 + G, :]), writes=['vraw%d' % sl])
                S.dma('sp', lambda e: e.dma_start(out=ldt[sl][:], in_=K.rwLD[d][t0:t0 + G, :]), writes=['ldt%d' % sl])
                r_, kd_, kk_, b_ = [raw[j][sl] for j in range(4)]
                S.op('act', lambda e: e.activation(out=vtr[qs][:], in_=vraw[sl][:], func=AF.Identity), reads=['vraw%d' % sl], writes=['vtr%d' % qs])
                banks = [pp.get() for _ in range(3)]
                for c in range(4):
                    for q in range(3):
                        S.op('pe', lambda e: e.matmul(banks[q][0][:, c * 128:(c + 1) * 128], lhsT=ldt[sl][:, c * 128:(c + 1) * 128], rhs=cum[:, q * 128:(q + 1) * 128], start=True, stop=True),
                             reads=['ldt%d' % sl, 'mk'], writes=[banks[q][1]], acc=True)
                v3 = lambda bank: bank[:, :].rearrange("p (c t) -> p c t", c=4)
                S.op('act', lambda e: e.activation(out=Ep[:], in_=v3(banks[0][0]), func=AF.Exp), reads=[banks[0][1]], writes=['Ep'])
                S.op('act', lambda e: e.activation(out=Em[:], in_=v3(banks[0][0]), func=AF.Exp, scale=-1.0), reads=[banks[0][1]], writes=['Em'])
                S.op('act', lambda e: e.activation(out=Ex[:], in_=v3(banks[1][0]), func=AF.Exp), reads=[banks[1][1]], writes=['Ex'])
                S.op('act', lambda e: e.activation(out=Ea[:], in_=v3(banks[2][0]), func=AF.Exp), reads=[banks[2][1]], writes=['Ea'])
                yield 'f'
                S.op('dve', lambda e: e.tensor_tensor(out=KRG[:, :, 0, :], in0=kk_[:], in1=Ex[:], op=ALU.mult), reads=[rk[2], 'Ex'], writes=['KRG'])
                S.op('dve', lambda e: e.tensor_tensor(out=KRG[:, :, 1, :], in0=r_[:], in1=Ep[:], op=ALU.mult), reads=[rk[0], 'Ep'], writes=['KRG'])
                S.op('dve', lambda e: e.tensor_tensor(out=BK[:, :, 0, :], in0=b_[:], in1=Em[:], op=ALU.mult), reads=[rk[3], 'Em'], writes=['BK'])
                S.op('dve', lambda e: e.tensor_tensor(out=BK[:, :, 1, :], in0=kd_[:], in1=Em[:], op=ALU.mult), reads=[rk[1], 'Em'], writes=['BK'])
                S.op('dve', lambda e: e.tensor_tensor(out=KB2[:, :, 0, :], in0=kd_[:], in1=Ea[:], op=ALU.mult), reads=[rk[1], 'Ea'], writes=['KB2'])
                S.op('dve', lambda e: e.tensor_tensor(out=KB2[:, :, 1, :], in0=b_[:], in1=Ea[:], op=ALU.mult), reads=[rk[3], 'Ea'], writes=['KB2'])
                for cc in range(2):
                    pos = cc * 64 + (63 if d == 0 else 0)
                    in0 = i64f[:, cc * 64:(cc + 1) * 64].unsqueeze(1).to_broadcast([128, 4, 64])
                    in1 = Ep[:, :, pos:pos + 1].to_broadcast([128, 4, 64])
                    S.op('dve', lambda e: e.tensor_tensor(out=KRG[:, :, 2, cc * 64:(cc + 1) * 64], in0=in0, in1=in1, op=ALU.mult), reads=['i64f', 'Ep'], writes=['KRG'])

                for g0 in range(0, 8, NS):
                    grp = list(range(g0, g0 + NS))
                    cur = {}
                    for s_, h in enumerate(grp):
                        c, pb = h // 2, 64 * (h % 2)
                        fm = lambda arr, k0, k1: arr[pb:pb + 64, c, k0:k1, :].rearrange("p k t -> p (k t)")
                        p1, k1 = pp.get()
                        S.op('pe', lambda e: e.matmul(p1[:, 0:256], lhsT=fm(BK, 0, 1), rhs=fm(KRG, 0, 2), start=True, stop=True), reads=['BK', 'KRG'], writes=[k1], acc=True)
                        S.op('pe', lambda e: e.matmul(p1[:, 256:384], lhsT=fm(KB2, 1, 2), rhs=i64r[pb:pb + 64, :], start=True, stop=True), reads=['KB2', 'i64r'], writes=[k1], acc=True)
                        S.op('dve', lambda e: e.tensor_tensor(out=La[s_][:], in0=p1[:, 0:384], in1=m1, op=ALU.mult), reads=[k1, 'mk'], writes=['La%d' % s_])
                        p2, k2 = pp.get()
                        S.op('pe', lambda e: e.matmul(p2[:, 0:128], lhsT=fm(BK, 1, 2), rhs=fm(KRG, 1, 2), start=True, stop=True), reads=['BK', 'KRG'], writes=[k2], acc=True)
                        S.op('pe', lambda e: e.matmul(p2[:, 128:256], lhsT=fm(KB2, 0, 1), rhs=i64r[pb:pb + 64, :], start=True, stop=True), reads=['KB2', 'i64r'], writes=[k2], acc=True)
                        S.op('dve', lambda e: e.tensor_tensor(out=X2[s_][:], in0=p2[:, 0:256], in1=m2, op=ALU.mult), reads=[k2, 'mk'], writes=['X2%d' % s_])
                        p3, k3 = pp.get()
                        S.op('pe', lambda e: e.matmul(p3[:, 0:256], lhsT=fm(KRG, 0, 1), rhs=fm(BK, 0, 2), start=True, stop=True), reads=['BK', 'KRG'], writes=[k3], acc=True)
                        S.op('pe', lambda e: e.matmul(p3[:, 256:320], lhsT=fm(KRG, 0, 1), rhs=i64r[pb:pb + 64, 0:64], start=True, stop=True), reads=['KRG', 'i64r'], writes=[k3], acc=True)
                        S.op('dve', lambda e: e.tensor_tensor(out=X1[s_][:], in0=p3[:, 0:320], in1=m3, op=ALU.mult), reads=[k3, 'mk'], writes=['X1%d' % s_])
                        cur[s_] = (La[s_], 'La%d' % s_, X1[s_][:, 0:128], 'X1%d' % s_)
                        yield 'f'
                    for lev in range(5):
                        pend = []
                        nxt = {}
                        for s_ in range(NS):
                            L, Lk, AT, ATk = cur[s_]
                            p, pk = pp.get()
                            S.op('pe', lambda e: e.matmul(p[:, 0:384], lhsT=AT, rhs=L[:, 0:384], start=True, stop=False), reads=[Lk, ATk], writes=[pk], acc=True)
                            S.op('pe', lambda e: e.matmul(p[:, 128:384], lhsT=idr[:], rhs=L[:, 128:384], start=False, stop=True), reads=[Lk, 'idr'], writes=[pk], acc=True)
                            pend.append((p, pk))
                        yield 'f'
                        for s_ in range(NS):
                            p, pk = pend[s_]
                            Ln_ = Lb[s_] if lev % 2 == 0 else La[s_]
                            Lnk = ('Lb%d' if lev % 2 == 0 else 'La%d') % s_
                            evac(Ln_[:], p[:, 0:384], [pk], [Lnk])
                            nxt[s_] = (Ln_, Lnk)
                        for s_ in range(NS):
                            Ln_, Lnk = nxt[s_]
                            p, pk = pend[s_]
                            S.op('pe', lambda e: e.transpose(out=p[:, 384:512], in_=Ln_[:, 0:128].bitcast(F32), identity=idf[:]), reads=[Lnk, 'idf'], writes=[pk])
                        yield 'f'
                        for s_ in range(NS):
                            Ln_, Lnk = nxt[s_]
                            p, pk = pend[s_]
                            ATn = ATa[s_] if lev % 2 == 0 else ATb[s_]
                            ATnk = ('ATa%d' if lev % 2 == 0 else 'ATb%d') % s_
                            evac(ATn[:], p[:, 384:512], [pk], [ATnk])
                            cur[s_] = (Ln_, Lnk, ATn[:], ATnk)
                    for s_, h in enumerate(grp):
                        L, Lk, AT, ATk = cur[s_]
                        p, pk = pp.get()
                        S.op('pe', lambda e: e.matmul(p[:, 0:256], lhsT=AT, rhs=L[:, 128:384], start=True, stop=False), reads=[Lk, ATk], writes=[pk], acc=True)
                        S.op('pe', lambda e: e.matmul(p[:, 0:256], lhsT=idr[:], rhs=L[:, 128:384], start=False, stop=True), reads=[Lk, 'idr'], writes=[pk], acc=True)
                        evac(Xf[s_][:], p[:, 0:256], [pk], ['Xf%d' % s_])
                    yield 'f'
                    for s_, h in enumerate(grp):
                        c, pb = h // 2, 64 * (h % 2)
                        p, pk = pp.get()
                        S.op('pe', lambda e: e.matmul(p[:, 0:256], lhsT=idr[:], rhs=X2[s_][:], start=True, stop=False), reads=['X2%d' % s_, 'idr'], writes=[pk], acc=True)
                        S.op('pe', lambda e: e.matmul(p[:, 0:256], lhsT=X1[s_][:, 128:256], rhs=Xf[s_][:], start=False, stop=True), reads=['X1%d' % s_, 'Xf%d' % s_], writes=[pk], acc=True)
                        evac(QG2[qs][:, h, :], p[:, 0:256], [pk], ['QG2_%d_%d' % (qs, h)])
                        q, qk = pp.get()
                        rg = KRG[pb:pb + 64, c, 1:3, :].rearrange("p k t -> p (k t)")
                        S.op('pe', lambda e: e.matmul(q[0:64, 0:256], lhsT=idr[pb:pb + 64, pb:pb + 64], rhs=rg, start=True, stop=False), reads=['KRG', 'idr'], writes=[qk], acc=True)
                        S.op('pe', lambda e: e.matmul(q[0:64, 0:256], lhsT=X1[s_][:, 256:320], rhs=Xf[s_][:], start=False, stop=True), reads=['X1%d' % s_, 'Xf%d' % s_], writes=[qk], acc=True)
                        evac(QG1[qs][:, h, :], q[0:64, 0:256], [qk], ['QG1_%d_%d' % (qs, h)])
                        yield 'f'
                yield 'F'
                for s, cc in enumerate(chunks):
                    for h in range(8):
                        S.op('pe', lambda e: e.matmul(pst[:, h * 64:(h + 1) * 64], lhsT=QG1[qs][:, h, 128 + cc * 64:128 + (cc + 1) * 64], rhs=Sth[:, s, h, :], start=True, stop=False),
                             reads=['QG1_%d_%d' % (qs, h), 'Sth%d' % s], writes=['pst'], acc=True)
                        S.op('pe', lambda e: e.matmul(pst[:, h * 64:(h + 1) * 64], lhsT=QG2[qs][:, h, 128 + cc * 64:128 + (cc + 1) * 64], rhs=vtr[qs][:, h * 64:(h + 1) * 64], start=False, stop=True),
                             reads=['QG2_%d_%d' % (qs, h), 'vtr%d' % qs], writes=['pst'], acc=True)
                    evac(Sth[:, s + 1].rearrange("p h v -> p (h v)"), pst[:, :], ['pst'], ['Sth%d' % (s + 1)])
                    yield 'b'
                if lat:
                    ybuf = T[0]
                    for hq in range(2):
                        bank = pp.get()
                        for h4 in range(4):
                            h = hq * 4 + h4
                            S.op('pe', lambda e: e.matmul(bank[0][0:64, h4 * 128:(h4 + 1) * 128], lhsT=vtr[qs][:, h * 64:(h + 1) * 64], rhs=QG2[qs][:, h, 0:128], start=True, stop=False),
                                 reads=['QG2_%d_%d' % (qs, h), 'vtr%d' % qs], writes=[bank[1]], acc=True)
                            for s, cc in enumerate(chunks):
                                S.op('pe', lambda e: e.matmul(bank[0][0:64, h4 * 128 + cc * 64:h4 * 128 + (cc + 1) * 64], lhsT=Sth[:, s, h, :], rhs=QG1[qs][:, h, cc * 64:(cc + 1) * 64], start=False, stop=(s == 1)),
                                     reads=['QG1_%d_%d' % (qs, h), 'Sth%d' % s], writes=[bank[1]], acc=True)
                        evac(ybuf[:, hq * 4:hq * 4 + 4, :], bank[0][0:64, :].rearrange("p (h t) -> p h t", h=4), [bank[1]], ['T0'])
                        yield 'b'
                    tl = t0 - TC
                    if d == 0:
                        S.dma('pool', lambda e: e.dma_start(out=K.rwY0[:, tl:tl + G].rearrange("(h p) t -> p h t", p=64), in_=ybuf[:]), reads=['T0'], writes=['rwY0'])
                    else:
                        y0b, cen, sqb = T[1], T[2], T[3]
                        S.dma('sp', lambda e: e.dma_start(out=y0b[:], in_=K.rwY0[:, tl:tl + G].rearrange("(h p) t -> p h t", p=64)), reads=['rwY0'], writes=['T1'])
                        S.op('dve', lambda e: e.tensor_tensor(out=ybuf[:], in0=ybuf[:], in1=y0b[:], op=ALU.add), reads=['T0', 'T1'], writes=['T0'])
                        for q in range(2):
                            p, pk = pp.get()
                            S.op('pe', lambda e: e.matmul(p[0:64, :], lhsT=o64[:], rhs=fl(ybuf)[:, q * 512:(q + 1) * 512], start=True, stop=True), reads=['o64', 'T0'], writes=[pk])
                            S.op('dve', lambda e: e.tensor_tensor(out=fl(cen)[:, q * 512:(q + 1) * 512], in0=fl(ybuf)[:, q * 512:(q + 1) * 512], in1=p[0:64, :], op=ALU.subtract), reads=[pk, 'T0'], writes=['T2'])
                        S.op('act', lambda e: e.activation(out=sqb[:], in_=cen[:], func=AF.Square), reads=['T2'], writes=['T3'])
                        yield 'b'
                        rsb = T[1]
                        for q in range(2):
                            p, pk = pp.get()
                            S.op('pe', lambda e: e.matmul(p[0:64, :], lhsT=o64[:], rhs=fl(sqb)[:, q * 512:(q + 1) * 512], start=True, stop=True), reads=['o64', 'T3'], writes=[pk])
                            S.op('act', lambda e: e.activation(out=fl(rsb)[:, q * 512:(q + 1) * 512], in_=p[0:64, :], func=AF.Ln, bias=epsg[:], scale=1.0), reads=[pk, 'epsg'], writes=['T1'])
                        S.op('act', lambda e: e.activation(out=rsb[:], in_=rsb[:], func=AF.Exp, scale=-0.5), reads=['T1'], writes=['T1'])
                        yield 'b'
                        bonb, ggb = T[3], T[0]
                        S.dma('sp', lambda e: e.dma_start(out=bonb[:], in_=K.rwBON[:, tl:tl + G].rearrange("(h p) t -> p h t", p=64)), reads=['rwBON'], writes=['T3'])
                        S.op('dve', lambda e: e.tensor_tensor(out=cen[:], in0=cen[:], in1=rsb[:], op=ALU.mult), reads=['T2', 'T1'], writes=['T2'])
                        S.dma('sp', lambda e: e.dma_start(out=ggb[:], in_=K.rwG[:, tl:tl + G].rearrange("(h p) t -> p h t", p=64)), reads=['rwG'], writes=['T0'])
                        S.op('dve', lambda e: e.tensor_tensor(out=cen[:], in0=cen[:], in1=gng[:, :].unsqueeze(2).to_broadcast([64, 8, G]), op=ALU.mult), reads=['T2', 'gng'], writes=['T2'])
                        S.op('dve', lambda e: e.tensor_tensor(out=cen[:], in0=cen[:], in1=gnb[:, :].unsqueeze(2).to_broadcast([64, 8, G]), op=ALU.add), reads=['T2', 'gnb'], writes=['T2'])
                        S.op('dve', lambda e: e.tensor_tensor(out=cen[:], in0=cen[:], in1=bonb[:], op=ALU.add), reads=['T2', 'T3'], writes=['T2'])
                        S.op('dve', lambda e: e.tensor_tensor(out=outb[:], in0=cen[:], in1=ggb[:], op=ALU.mult), reads=['T2', 'T0'], writes=['outb'])
                        S.dma('pool', lambda e: e.dma_start(out=K.mixT[0:512, tl:tl + G].rearrange("(h p) t -> p h t", p=64), in_=outb[:]), reads=['outb'], writes=['mixT'])
                S.op('dve', lambda e: e.tensor_copy(out=Sth[:, 0], in_=Sth[:, 2]), reads=['Sth2'], writes=['Sth0'])
            def step(gen_):
                try:
                    return next(gen_)
                except StopIteration:
                    return None
            prev = None
            qslot = 0
            for b in blocks:
                gcur = body(b, qslot)
                while True:
                    r = step(gcur)
                    if prev is not None and step(prev) is None:
                        prev = None
                    if r == 'F' or r is None:
                        break
                while prev is not None:
                    if step(prev) is None:
                        prev = None
                prev = gcur
                qslot ^= 1
            while prev is not None:
                if step(prev) is None:
                    prev = None
            S.barrier()


ALPHA = 2.0 ** 0.25
ROWW = 1088
BIGPOS = 4096.0


def phase_mix(K):
    nc, S = K.nc, K.S
    NT = TL // 128
    with ExitStack() as es:
        sb = lambda n, s, d: es.enter_context(nc.sbuf_tensor(_u(n), s, d))
        ps = lambda n, s, d: es.enter_context(nc.psum_tensor(_u(n), s, d))
        wo = sb("wo", [128, 8, 1024], BF16)
        wst = [sb("owst%d" % i, [128, 8, 256], F32) for i in range(2)]
        bc = {n: sb("bc_" + n, [128, 1024], F32) for n in ('g1', 'ln1g', 'ln1b', 'sc2', 'sh2')}
        rt = sb("rt", [128, 8, 16], F32)
        idf = sb("idf", [128, 128], F32)
        epst = sb("epst", [128, 1], F32)
        onesf = sb("onesf", [128, 128], F32)
        onesb = sb("onesb", [128, 128], BF16)
        ustr = sb("ustr", [128, 128], BF16)
        mt = [sb("mt%d" % i, [128, 8, 128], BF16) for i in range(2)]
        xt = [sb("xt%d" % i, [128, 1024], F32) for i in range(2)]
        t1 = sb("t1", [128, 1024], F32)
        pre = sb("pre", [128, 1024], F32)
        x1 = [sb("x1_%d" % i, [128, 1024], F32) for i in range(2)]
        uf = [sb("uf%d" % i, [128, 1024], F32) for i in range(2)]
        urow = [sb("urow%d" % i, [128, ROWW], BF16) for i in range(2)]
        uT = sb("uT", [128, 8, 128], F32)
        st = sb("st", [128, 2, 6], F32)
        mv = sb("mv", [128, 2], F32)
        lnv = sb("lnv", [128, 1], F32)
        rstd = sb("rstd", [128, 1], F32)
        lg = sb("lg", [128, 16], F32)
        mx = sb("mx", [128, 1], F32)
        sm = sb("sm", [128, 1], F32)
        affall = sb("affall", [128, NT, 16], F32)
        tok = sb("tok", [128, 1], I32)
        lo = sb("lo", [128, 16], F32)
        mid = sb("mid", [128, 16], F32)
        ge = sb("ge", [128, 16], F32)
        cntp = sb("cntp", [128, 16], F32)
        mskt = sb("mskt", [128, NT, 16], F32)
        mskb = sb("mskb", [128, NT, 16], BF16)
        csT = sb("csT", [128, 16, NT], F32)
        incT = sb("incT", [128, 16, NT], F32)
        rmask = sb("rmask", [128, 16, NT], F32)
        posf = sb("posf", [128, NT, 16], F32)
        posi = sb("posi", [128, NT, 16], I32)
        zt = sb("zt", [128, 1024], F32)
        pm = [ps("pm%d" % i, [128, 1024], F32) for i in range(2)]
        ptr = ps("ptr", [128, 1024], F32)
        psm = ps("psm", [128, 512], F32)
        psn = ps("psn", [128, 512], F32)

        ld1 = lambda dst, src, key, rd=(): S.dma('sp', lambda e: e.dma_start(out=dst, in_=src, allow_slow_non_contiguous=True), reads=list(rd), writes=[key])
        ld1(idf[:], K.identf, 'idf')
        ld1(ustr[:], K.ustrict, 'ustr')
        ld1(rt[:], K.router.rearrange("(k p) e -> p k e", p=128), 'rt')
        ld1(bc['g1'][:], K.modd[0:1, 2048:3072].partition_broadcast(128), 'bc_g1', ['modd'])
        ld1(bc['sh2'][:], K.modd[0:1, 3072:4096].partition_broadcast(128), 'bc_sh2', ['modd'])
        ld1(bc['sc2'][:], K.modd[0:1, 4096:5120].partition_broadcast(128), 'bc_sc2', ['modd'])
        ld1(bc['ln1g'][:], K.ln1_g.partition_broadcast(128), 'bc_ln1g')
        ld1(bc['ln1b'][:], K.ln1_b.partition_broadcast(128), 'bc_ln1b')
        S.op('dve', lambda e: e.tensor_scalar_add(out=bc['sc2'][:], in0=bc['sc2'][:], scalar1=1.0), reads=['bc_sc2'], writes=['bc_sc2'])
        S.op('dve', lambda e: e.memset(epst[:], 1e-5), writes=['epst'])
        S.op('dve', lambda e: e.memset(onesf[:], 1.0), writes=['onesf'])
        S.op('dve', lambda e: e.memset(onesb[:], 1.0), writes=['onesb'])
        S.op('dve', lambda e: e.memset(zt[:], 0.0), writes=['zt'])
        for j in range(4):
            w = wst[j % 2]
            wk = 'owst%d' % (j % 2)
            S.dma('sp', lambda e: e.dma_start(out=w[:], in_=K.w_out[:, j * 256:(j + 1) * 256].rearrange("(k p) n -> p k n", p=128)), writes=[wk])
            S.op('pool', lambda e: e.tensor_copy(out=wo[:, :, j * 256:(j + 1) * 256], in_=w[:]), reads=[wk], writes=['wo'])
        for i in range(NT):
            S.dma('pool', lambda e: e.dma_start(out=K.yacc[i * 128:(i + 1) * 128, :], in_=zt[:]), reads=['zt'], writes=['yacc%d' % i])

        mvs = [mv, sb("mv1", [128, 2], F32)]
        rstds = [rstd, sb("rstd1", [128, 1], F32)]
        sts = [st, sb("st1", [128, 2, 6], F32)]
        lnvs = [lnv, sb("lnv1", [128, 1], F32)]

        nmr = [sb("nmr%d" % i_, [128, 1], F32) for i_ in range(2)]

        def ln_stats(src, srck, lane=0):
            sfx = '' if lane == 0 else '1'
            for c in range(2):
                S.op('dve', lambda e: e.bn_stats(out=sts[lane][:, c, :], in_=src[:, c * 512:(c + 1) * 512]), reads=[srck], writes=['st%d%s' % (c, sfx)])
            S.op('dve', lambda e: e.bn_aggr(out=mvs[lane][:], in_=sts[lane][:]), reads=['st0' + sfx, 'st1' + sfx], writes=['mv' + sfx])
            S.op('act', lambda e: e.activation(out=lnvs[lane][:], in_=mvs[lane][:, 1:2], func=AF.Ln, bias=epst[:], scale=1.0), reads=['mv' + sfx, 'epst'], writes=['lnv' + sfx])
            S.op('act', lambda e: e.activation(out=rstds[lane][:], in_=lnvs[lane][:], func=AF.Exp, scale=-0.5), reads=['lnv' + sfx], writes=['rstd' + sfx])

        def tbody(i):
            sl = i % 2
            m_, mk_ = mt[sl], 'mt%d' % sl
            x_, xk = xt[sl], 'xt%d' % sl
            S.dma('sp', lambda e: e.dma_start(out=m_[:], in_=K.mixT[:, i * 128:(i + 1) * 128].rearrange("(k p) t -> p k t", p=128)), reads=['mixT'], writes=[mk_])
            S.dma('sp', lambda e: e.dma_start(out=x_[:], in_=K.xin[TC + i * 128:TC + (i + 1) * 128, :]), writes=[xk])
            p_, pk_ = pm[sl], 'pm%d' % sl
            for half in range(2):
                for kc in range(8):
                    S.op('pe', lambda e: e.matmul(p_[:, half * 512:(half + 1) * 512], lhsT=m_[:, kc, :], rhs=wo[:, kc, half * 512:(half + 1) * 512], start=(kc == 0), stop=(kc == 7)),
                         reads=[mk_, 'wo'], writes=[pk_], acc=True)
            yield 'f'
            S.op('dve', lambda e: e.tensor_tensor(out=t1[:], in0=p_[:], in1=bc['g1'][:], op=ALU.mult), reads=[pk_, 'bc_g1'], writes=['t1'])
            S.op('dve', lambda e: e.scalar_tensor_tensor(out=pre[:], in0=x_[:], scalar=ALPHA, in1=t1[:], op0=ALU.mult, op1=ALU.add), reads=[xk, 't1'], writes=['pre'])
            ln_stats(pre, 'pre')
            yield 'f'
            x1_, x1k = x1[sl], 'x1_%d' % sl
            S.op('dve', lambda e: e.scalar_tensor_tensor(out=nmr[0][:], in0=mv[:, 0:1], scalar=-1.0, in1=rstd[:], op0=ALU.mult, op1=ALU.mult), reads=['mv', 'rstd'], writes=['nmr0'])
            S.op('act', lambda e: e.activation(out=t1[:], in_=pre[:], func=AF.Identity, bias=nmr[0][:], scale=rstd[:]), reads=['pre', 'nmr0', 'rstd'], writes=['t1'])
            S.op('dve', lambda e: e.tensor_tensor(out=t1[:], in0=t1[:], in1=bc['ln1g'][:], op=ALU.mult), reads=['t1', 'bc_ln1g'], writes=['t1'])
            S.op('dve', lambda e: e.tensor_tensor(out=x1_[:], in0=t1[:], in1=bc['ln1b'][:], op=ALU.add), reads=['t1', 'bc_ln1b'], writes=[x1k])
            S.dma('pool', lambda e: e.dma_start(out=K.x1d[i * 128:(i + 1) * 128, :], in_=x1_[:]), reads=[x1k], writes=['x1d'])
            yield 'f'
            ln_stats(x1_, x1k, 1)
            yield 'f'
            S.op('dve', lambda e: e.scalar_tensor_tensor(out=nmr[1][:], in0=mvs[1][:, 0:1], scalar=-1.0, in1=rstds[1][:], op0=ALU.mult, op1=ALU.mult), reads=['mv1', 'rstd1'], writes=['nmr1'])
            S.op('act', lambda e: e.activation(out=pre[:], in_=x1_[:], func=AF.Identity, bias=nmr[1][:], scale=rstds[1][:]), reads=[x1k, 'nmr1', 'rstd1'], writes=['pre'])
            S.op('dve', lambda e: e.tensor_tensor(out=pre[:], in0=pre[:], in1=bc['sc2'][:], op=ALU.mult), reads=['pre', 'bc_sc2'], writes=['pre'])
            S.op('dve', lambda e: e.tensor_tensor(out=uf[sl][:], in0=pre[:], in1=bc['sh2'][:], op=ALU.add), reads=['pre', 'bc_sh2'], writes=['uf%d' % sl])
            ur, urk = urow[sl], 'urow%d' % sl
            S.op('act', lambda e: e.activation(out=ur[:, 0:1024], in_=uf[sl][:], func=AF.Identity), reads=['uf%d' % sl], writes=[urk])
            yield 'F'
            for k in range(8):
                S.op('pe', lambda e: e.transpose(out=ptr[:, k * 128:(k + 1) * 128], in_=uf[sl][:, k * 128:(k + 1) * 128], identity=idf[:]), reads=['uf%d' % sl, 'idf'], writes=['ptr'], acc=True)
            S.op('act', lambda e: e.activation(out=uT[:].rearrange("p k t -> p (k t)"), in_=ptr[:], func=AF.Identity), reads=['ptr'], writes=['uT'])
            yield 'b'
            for k in range(8):
                S.op('pe', lambda e: e.matmul(psm[:, 0:16], lhsT=uT[:, k, :], rhs=rt[:, k, :], start=(k == 0), stop=(k == 7)), reads=['uT', 'rt'], writes=['psm'], acc=True)
            S.op('dve', lambda e: e.reduce_max(out=mx[:], in_=psm[:, 0:16], axis=AX.X), reads=['psm'], writes=['mx'])
            S.op('dve', lambda e: e.tensor_scalar_mul(out=mx[:], in0=mx[:], scalar1=-1.0), reads=['mx'], writes=['mx'])
            yield 'b'
            S.op('act', lambda e: e.activation(out=lg[:], in_=psm[:, 0:16], func=AF.Exp, bias=mx[:], scale=1.0, accum_out=sm[:]), reads=['psm', 'mx'], writes=['lg', 'sm'])
            S.op('dve', lambda e: e.reciprocal(out=sm[:], in_=sm[:]), reads=['sm'], writes=['sm'])
            yield 'b'
            S.op('dve', lambda e: e.tensor_scalar_mul(out=affall[:, i, :], in0=lg[:], scalar1=sm[:]), reads=['lg', 'sm'], writes=['affall'])
            S.op('dve', lambda e: e.tensor_copy(out=ur[:, 1024:1056].bitcast(F32), in_=affall[:, i, :]), reads=['affall'], writes=[urk])
            S.op('pool', lambda e: e.iota(tok[:], pattern=[[0, 1]], base=i * 128, channel_multiplier=1), writes=['tok'])
            S.op('pool', lambda e: e.tensor_copy(out=ur[:, 1056:1058].bitcast(I32), in_=tok[:]), reads=['tok'], writes=[urk])
            S.dma('pool', lambda e: e.dma_start(out=K.urd[i * 128:(i + 1) * 128, 0:1058], in_=ur[:, 0:1058]), reads=[urk], writes=['urd%d' % i])

        def step(gen_):
            try:
                return next(gen_)
            except StopIteration:
                return None
        prev = None
        for i in range(NT):
            gcur = tbody(i)
            while True:
                r = step(gcur)
                if prev is not None and step(prev) is None:
                    prev = None
                if r == 'F' or r is None:
                    break
            while prev is not None:
                if step(prev) is None:
                    prev = None
            prev = gcur
        while prev is not None:
            if step(prev) is None:
                prev = None

        S.op('dve', lambda e: e.memset(lo[:], 0.0), writes=['lo'])
        for k in range(30):
            hk = 2.0 ** -(k + 1)
            S.op('dve', lambda e: e.tensor_scalar_add(out=mid[:], in0=lo[:], scalar1=hk), reads=['lo'], writes=['mid'])
            S.op('dve', lambda e: e.tensor_tensor(out=mskt[:], in0=affall[:], in1=mid[:, :].unsqueeze(1).to_broadcast([128, NT, 16]), op=ALU.is_ge), reads=['affall', 'mid'], writes=['mskt'])
            S.op('dve', lambda e: e.tensor_reduce(out=cntp[:], in_=mskt[:].rearrange("p i e -> p e i"), axis=AX.X, op=ALU.add), reads=['mskt'], writes=['cntp'])
            S.op('pe', lambda e: e.matmul(psn[:, 0:16], lhsT=onesf[:], rhs=cntp[:], start=True, stop=True), reads=['onesf', 'cntp'], writes=['psn'])
            S.op('dve', lambda e: e.tensor_scalar(out=ge[:], in0=psn[:, 0:16], scalar1=float(CAP) - 0.5, scalar2=hk, op0=ALU.is_ge, op1=ALU.mult), reads=['psn'], writes=['ge'])
            S.op('dve', lambda e: e.tensor_tensor(out=lo[:], in0=lo[:], in1=ge[:], op=ALU.add), reads=['lo', 'ge'], writes=['lo'])
        S.op('dve', lambda e: e.tensor_tensor(out=mskt[:], in0=affall[:], in1=lo[:, :].unsqueeze(1).to_broadcast([128, NT, 16]), op=ALU.is_ge), reads=['affall', 'lo'], writes=['mskt'])
        S.op('act', lambda e: e.activation(out=mskb[:], in_=mskt[:], func=AF.Identity), reads=['mskt'], writes=['mskb'])
        mflat = mskb[:].rearrange("p i e -> p (i e)")
        for hh in range(2):
            S.op('pe', lambda e: e.matmul(psm[:, :], lhsT=onesb[:], rhs=mflat[:, hh * 512:(hh + 1) * 512], start=True, stop=True), reads=['onesb', 'mskb'], writes=['psm'])
            S.op('dve', lambda e: e.tensor_copy(out=csT[:, :, hh * 32:(hh + 1) * 32], in_=psm[:, :].rearrange("p (i e) -> p e i", e=16)), reads=['psm'], writes=['csT'])
        S.op('dve', lambda e: e.memset(rmask[:], 1.0), writes=['rmask'])
        S.op('dve', lambda e: e.memset(rmask[:, :, 0:1], 0.0), writes=['rmask'])
        S.op('dve', lambda e: e.tensor_tensor_scan(out=incT[:].rearrange("p e i -> p (e i)"), data0=rmask[:].rearrange("p e i -> p (e i)"), data1=csT[:].rearrange("p e i -> p (e i)"),
                                                   initial=0.0, op0=ALU.mult, op1=ALU.add), reads=['rmask', 'csT'], writes=['incT'])
        S.op('dve', lambda e: e.tensor_tensor(out=incT[:], in0=incT[:], in1=csT[:], op=ALU.subtract), reads=['incT', 'csT'], writes=['incT'])
        for hh in range(2):
            S.op('pe', lambda e: e.matmul(psm[:, :], lhsT=ustr[:], rhs=mflat[:, hh * 512:(hh + 1) * 512], start=True, stop=True), reads=['ustr', 'mskb'], writes=['psm'])
            S.op('dve', lambda e: e.tensor_tensor(out=posf[:, hh * 32:(hh + 1) * 32, :], in0=psm[:, :].rearrange("p (i e) -> p i e", e=16),
                                                  in1=incT[:, :, hh * 32:(hh + 1) * 32].rearrange("p e i -> p i e"), op=ALU.add), reads=['psm', 'incT'], writes=['posf'])
        S.op('dve', lambda e: e.scalar_tensor_tensor(out=posf[:], in0=posf[:], scalar=-BIGPOS, in1=mskt[:], op0=ALU.add, op1=ALU.mult), reads=['posf', 'mskt'], writes=['posf'])
        S.op('dve', lambda e: e.tensor_scalar_add(out=posf[:], in0=posf[:], scalar1=BIGPOS), reads=['posf'], writes=['posf'])
        S.op('dve', lambda e: e.tensor_copy(out=posi[:], in_=posf[:]), reads=['posf'], writes=['posi'])
        if K.dbg_pos is not None:
            S.dma('sp', lambda e: e.dma_start(out=K.dbg_pos, in_=posf[:]), reads=['posf'], writes=['dbg_pos'])
        breg = nc.gpsimd.to_reg(CAP - 1)
        for i in range(NT):
            sl = i % 2
            ur, urk = urow[sl], 'urow%d' % sl
            S.dma('sp', lambda e: e.dma_start(out=ur[:, 0:1058], in_=K.urd[i * 128:(i + 1) * 128, 0:1058]), reads=['urd%d' % i], writes=[urk])
            for ex in range(NE):
                S.dma('pool', lambda e: e.indirect_dma_start(out=K.xe_d[ex], out_offset=bass.IndirectOffsetOnAxis(ap=posi[:, i, ex:ex + 1], axis=0),
                                                             in_=ur[:, :], in_offset=None, bounds_check=breg, oob_is_err=False),
                      reads=[urk, 'posi'], writes=['xe_d_%d_%d' % (i, ex)])
        S.barrier()


def phase_moe(K, experts=range(NE)):
    nc, S = K.nc, K.S
    NT = TL // 128
    with ExitStack() as es:
        sb = lambda n, s, d: es.enter_context(nc.sbuf_tensor(_u(n), s, d))
        ps = lambda n, s, d: es.enter_context(nc.psum_tensor(_u(n), s, d))
        W = [[sb("W%d_%d" % (m, i), [128, 8, 1024], BF16) for i in range(2)] for m in range(3)]
        wst = Rot([sb("ewst%d" % i, [128, 8, 256], F32) for i in range(3)], "ewst")
        idb = sb("idb", [128, 128], BF16)
        xrow = Rot([sb("xrow%d" % i, [128, ROWW], BF16) for i in range(2)], "xrow")
        xeT = [sb("xeT%d" % i, [128, 8, 1024], BF16) for i in range(2)]
        hidT = sb("hidT", [128, 8, 1024], BF16)
        gates = [sb("gates%d" % i, [128, 8], F32) for i in range(2)]
        idxs = [sb("idxs%d" % i, [128, 8], I32) for i in range(2)]
        sgt = Rot([sb("sgt%d" % i, [128, 512], F32) for i in range(2)], "sgt")
        ye = Rot([sb("ye%d" % i, [128, 1024], F32) for i in range(2)], "ye")
        pt = Rot([ps("ept%d" % i, [128, 1024], BF16) for i in range(2)], "ept")
        pg = Rot([ps("epg%d" % i, [128, 512], F32) for i in range(6)], "epg")
        S.dma('sp', lambda e: e.dma_start(out=idb[:], in_=K.ident), writes=['idb'])
        cast_i = [0]

        def load_w(ex, slot):
            for m, src in enumerate((K.exp_w_gate, K.exp_w_up, K.exp_w_down)):
                for j in range(4):
                    w, wk = wst.get()
                    S.dma('sp', lambda e: e.dma_start(out=w[:], in_=src[ex, :, j * 256:(j + 1) * 256].rearrange("(k p) n -> p k n", p=128)), writes=[wk])
                    eng = 'dve'
                    cast_i[0] += 1
                    if eng == 'dve':
                        S.op('dve', lambda e: e.tensor_copy(out=W[m][slot][:, :, j * 256:(j + 1) * 256], in_=w[:]), reads=[wk], writes=['W%d_%d' % (m, slot)])
                    else:
                        S.op('act', lambda e: e.activation(out=W[m][slot][:, :, j * 256:(j + 1) * 256], in_=w[:], func=AF.Identity), reads=[wk], writes=['W%d_%d' % (m, slot)])

        exl = list(experts)
        load_w(exl[0], 0)
        if len(exl) > 1:
            load_w(exl[1], 1)
        def ebody(n, ex):
            xs = n % 2
            slot = n % 2
            Wg, Wu, Wd = W[0][slot], W[1][slot], W[2][slot]
            wkeys = ['W%d_%d' % (m, slot) for m in range(3)]
            for j in range(8):
                xr, xk = xrow.get()
                S.dma('sp', lambda e: e.dma_start(out=xr[:, 0:1058], in_=K.xe_d[ex][j * 128:(j + 1) * 128, 0:1058]), reads=['xe_d'], writes=[xk])
                S.op('dve', lambda e: e.tensor_copy(out=gates[xs][:, j:j + 1], in_=xr[:, 1024:1056].bitcast(F32)[:, ex:ex + 1]), reads=[xk], writes=['gates%d' % xs])
                S.op('dve', lambda e: e.tensor_copy(out=idxs[xs][:, j:j + 1], in_=xr[:, 1056:1058].bitcast(I32)), reads=[xk], writes=['idxs%d' % xs])
                p, pk = pt.get()
                for k in range(8):
                    S.op('pe', lambda e: e.transpose(out=p[:, k * 128:(k + 1) * 128], in_=xr[:, k * 128:(k + 1) * 128], identity=idb[:]), reads=[xk, 'idb'], writes=[pk], acc=True)
                S.op('act', lambda e: e.activation(out=xeT[xs][:, :, j * 128:(j + 1) * 128], in_=p[:].rearrange("p (k t) -> p k t", k=8), func=AF.Identity), reads=[pk], writes=['xeT%d' % xs])
                yield 'f'
            yield 'F'
            for fc in range(8):
                for half in range(2):
                    g_, gk = pg.get()
                    u_, uk = pg.get()
                    for kc in range(8):
                        S.op('pe', lambda e: e.matmul(g_[:], lhsT=Wg[:, kc, fc * 128:(fc + 1) * 128], rhs=xeT[xs][:, kc, half * 512:(half + 1) * 512], start=(kc == 0), stop=(kc == 7)),
                             reads=[wkeys[0], 'xeT%d' % xs], writes=[gk], acc=True)
                    for kc in range(8):
                        S.op('pe', lambda e: e.matmul(u_[:], lhsT=Wu[:, kc, fc * 128:(fc + 1) * 128], rhs=xeT[xs][:, kc, half * 512:(half + 1) * 512], start=(kc == 0), stop=(kc == 7)),
                             reads=[wkeys[1], 'xeT%d' % xs], writes=[uk], acc=True)
                    s_, sk = sgt.get()
                    S.op('act', lambda e: e.activation(out=s_[:], in_=g_[:], func=AF.Silu), reads=[gk], writes=[sk])
                    S.op('dve', lambda e: e.tensor_tensor(out=hidT[:, fc, half * 512:(half + 1) * 512], in0=s_[:], in1=u_[:], op=ALU.mult), reads=[sk, uk], writes=['hidT'])
                    yield 'b'
            for j in range(8):
                y_, yk = ye.get()
                for dh in range(2):
                    o_, ok = pg.get()
                    for fc in range(8):
                        S.op('pe', lambda e: e.matmul(o_[:], lhsT=hidT[:, fc, j * 128:(j + 1) * 128], rhs=Wd[:, fc, dh * 512:(dh + 1) * 512], start=(fc == 0), stop=(fc == 7)),
                             reads=[wkeys[2], 'hidT'], writes=[ok], acc=True)
                    S.op('act', lambda e: e.activation(out=y_[:, dh * 512:(dh + 1) * 512], in_=o_[:], func=AF.Identity, scale=gates[xs][:, j:j + 1]), reads=[ok, 'gates%d' % xs], writes=[yk])
                S.dma('pool', lambda e: e.indirect_dma_start(out=K.yacc, out_offset=bass.IndirectOffsetOnAxis(ap=idxs[xs][:, j:j + 1], axis=0), in_=y_[:, :], in_offset=None,
                                                             compute_op=ALU.add),
                      reads=[yk, 'idxs%d' % xs], writes=['yacc'])
                yield 'b'
        def step(gen_):
            try:
                return next(gen_)
            except StopIteration:
                return None
        prev = None
        for n, ex in enumerate(exl):
            gcur = ebody(n, ex)
            while True:
                r = step(gcur)
                if prev is not None and step(prev) is None:
                    prev = None
                if r == 'F' or r is None:
                    break
            while prev is not None:
                if step(prev) is None:
                    prev = None
            if n >= 1 and n + 1 < len(exl):
                load_w(exl[n + 1], (n + 1) % 2)
            prev = gcur
        while prev is not None:
            if step(prev) is None:
                prev = None
        S.barrier()


def phase_final(K):
    nc, S = K.nc, K.S
    NT = TL // 128
    with ExitStack() as es:
        sb = lambda n, s, d: es.enter_context(nc.sbuf_tensor(_u(n), s, d))
        bc = {n: sb("fbc_" + n, [128, 1024], F32) for n in ('g2', 'ln2g', 'ln2b')}
        epst = sb("epst", [128, 1], F32)
        x1 = [sb("fx1_%d" % i, [128, 1024], F32) for i in range(2)]
        ya = [sb("fya_%d" % i, [128, 1024], F32) for i in range(2)]
        t1 = sb("t1", [128, 1024], F32)
        pre = sb("pre", [128, 1024], F32)
        ob = [sb("fob_%d" % i, [128, 1024], F32) for i in range(2)]
        st = sb("st", [128, 2, 6], F32)
        mv = sb("mv", [128, 2], F32)
        lnv = sb("lnv", [128, 1], F32)
        rstd = sb("rstd", [128, 1], F32)
        nmr = sb("nmr", [128, 1], F32)
        ld1 = lambda dst, src, key, rd=(): S.dma('sp', lambda e: e.dma_start(out=dst, in_=src, allow_slow_non_contiguous=True), reads=list(rd), writes=[key])
        ld1(bc['g2'][:], K.modd[0:1, 5120:6144].partition_broadcast(128), 'fbc_g2', ['modd'])
        ld1(bc['ln2g'][:], K.ln2_g.partition_broadcast(128), 'fbc_ln2g')
        ld1(bc['ln2b'][:], K.ln2_b.partition_broadcast(128), 'fbc_ln2b')
        S.op('dve', lambda e: e.memset(epst[:], 1e-5), writes=['epst'])
        for i in range(NT):
            sl = i % 2
            S.dma('sp', lambda e: e.dma_start(out=x1[sl][:], in_=K.x1d[i * 128:(i + 1) * 128, :]), reads=['x1d'], writes=['fx1_%d' % sl])
            S.dma('sp', lambda e: e.dma_start(out=ya[sl][:], in_=K.yacc[i * 128:(i + 1) * 128, :]), reads=['yacc'], writes=['fya_%d' % sl])
            S.op('dve', lambda e: e.tensor_tensor(out=t1[:], in0=ya[sl][:], in1=bc['g2'][:], op=ALU.mult), reads=['fya_%d' % sl, 'fbc_g2'], writes=['t1'])
            S.op('dve', lambda e: e.scalar_tensor_tensor(out=pre[:], in0=x1[sl][:], scalar=ALPHA, in1=t1[:], op0=ALU.mult, op1=ALU.add), reads=['fx1_%d' % sl, 't1'], writes=['pre'])
            for c in range(2):
                S.op('dve', lambda e: e.bn_stats(out=st[:, c, :], in_=pre[:, c * 512:(c + 1) * 512]), reads=['pre'], writes=['st%d' % c])
            S.op('dve', lambda e: e.bn_aggr(out=mv[:], in_=st[:]), reads=['st0', 'st1'], writes=['mv'])
            S.op('act', lambda e: e.activation(out=lnv[:], in_=mv[:, 1:2], func=AF.Ln, bias=epst[:], scale=1.0), reads=['mv', 'epst'], writes=['lnv'])
            S.op('act', lambda e: e.activation(out=rstd[:], in_=lnv[:], func=AF.Exp, scale=-0.5), reads=['lnv'], writes=['rstd'])
            S.op('dve', lambda e: e.scalar_tensor_tensor(out=nmr[:], in0=mv[:, 0:1], scalar=-1.0, in1=rstd[:], op0=ALU.mult, op1=ALU.mult), reads=['mv', 'rstd'], writes=['nmr'])
            S.op('act', lambda e: e.activation(out=t1[:], in_=pre[:], func=AF.Identity, bias=nmr[:], scale=rstd[:]), reads=['pre', 'nmr', 'rstd'], writes=['t1'])
            S.op('dve', lambda e: e.tensor_tensor(out=t1[:], in0=t1[:], in1=bc['ln2g'][:], op=ALU.mult), reads=['t1', 'fbc_ln2g'], writes=['t1'])
            S.op('dve', lambda e: e.tensor_tensor(out=ob[sl][:], in0=t1[:], in1=bc['ln2b'][:], op=ALU.add), reads=['t1', 'fbc_ln2b'], writes=['fob_%d' % sl])
            S.dma('pool', lambda e: e.dma_start(out=K.out[i * 128:(i + 1) * 128, :], in_=ob[sl][:]), reads=['fob_%d' % sl], writes=['out'])
        S.barrier()


def build_program(debug=(), phases=None, dbg_in=(), opts=None):
    opts = opts or {}
    nc = bass.Bass("TRN2", target_bir_lowering=False)
    K = Ctx()
    K.nc = nc
    di = lambda n, s, d: nc.dram_tensor(n, s, d, kind="ExternalInput").ap()
    K.xin = di("xin", [TA, D], F32)
    K.ccT = di("ccT", [128, 8, 2], F32)
    K.w_ada = di("w_ada", [D, 6 * D], F32)
    K.b_ada = di("b_ada", [1, 6 * D], F32)
    K.w_in = di("w_in", [D, 2560], F32)
    K.cosT = di("cosT", [64, TL], F32)
    K.sinT = di("sinT", [64, TL], F32)
    K.ident = di("ident", [128, 128], BF16)
    K.identf = di("identf", [128, 128], F32)
    K.i64dbl = di("i64dbl", [128, 128], F32)
    K.bones = di("bones", [128, 128], F32)
    K.rwm = [di("rwm%d" % d, [128, 1344], F32) for d in range(2)]
    K.mla_q_norm = di("mla_q_norm", [1, 256], F32)
    K.mla_kv_norm = di("mla_kv_norm", [1, 256], F32)
    K.w_uq = di("w_uq", [256, 1024], F32)
    K.w_uk = di("w_uk", [256, 512], F32)
    K.w_uv = di("w_uv", [256, 512], F32)
    K.rwkv_conv = di("rwkv_conv", [3, 1536], F32)
    K.rwkv_w0 = di("rwkv_w0", [2, 512], F32)
    K.rwkv_w_up = di("rwkv_w_up", [2, 64, 512], F32)
    K.rwkv_a0 = di("rwkv_a0", [2, 512], F32)
    K.rwkv_a_up = di("rwkv_a_up", [2, 64, 512], F32)
    K.rwkv_g_up = di("rwkv_g_up", [128, 512], F32)
    K.rwkv_k_k = di("rwkv_k_k", [1, 512], F32)
    K.rwkv_k_a = di("rwkv_k_a", [1, 512], F32)
    K.rwkv_r_k = di("rwkv_r_k", [1, 512], F32)
    K.rwkv_gn_g = di("rwkv_gn_g", [1, 512], F32)
    K.rwkv_gn_b = di("rwkv_gn_b", [1, 512], F32)
    K.w_out = di("w_out", [D, D], F32)
    K.ln1_g = di("ln1_g", [1, D], F32)
    K.ln1_b = di("ln1_b", [1, D], F32)
    K.ln2_g = di("ln2_g", [1, D], F32)
    K.ln2_b = di("ln2_b", [1, D], F32)
    K.router = di("router", [D, NE], F32)
    K.ustrict = di("ustrict", [128, 128], BF16)
    K.exp_w_gate = di("exp_w_gate", [NE, D, D], F32)
    K.exp_w_up = di("exp_w_up", [NE, D, D], F32)
    K.exp_w_down = di("exp_w_down", [NE, D, D], F32)

    def scratch(n, s, d):
        if n in dbg_in:
            return nc.dram_tensor(n, s, d, kind="ExternalInput").ap()
        kind = "ExternalOutput" if n in debug else "Internal"
        return nc.dram_tensor(n, s, d, kind=kind).ap()
    K.modd = scratch("modd", [2, 6 * D], F32)
    K.hT = scratch("hT", [2432, TA], F32)
    K.krT = scratch("krT", [64, TA], BF16)
    K.qnT = scratch("qnT", [512, TL], BF16)
    K.qrT = scratch("qrT", [256, TL], BF16)
    K.knT = scratch("knT", [512, TA], BF16)
    K.vtok = scratch("vtok", [TA, 512], BF16)
    K.mixT = scratch("mixT", [1024, TL], BF16)
    K.rwR = scratch("rwR", [512, TA], F32)
    K.rwV = scratch("rwV", [512, TA], F32)
    K.rwKK = scratch("rwKK", [512, TA], F32)
    K.rwKD = [scratch("rwKD%d" % d, [512, TA], F32) for d in range(2)]
    K.rwB = [scratch("rwB%d" % d, [512, TA], F32) for d in range(2)]
    K.rwVtok = scratch("rwVtok", [TA, 512], F32)
    K.rwLD = [scratch("rwLD%d" % d, [TA, 512], F32) for d in range(2)]
    K.rwG = scratch("rwG", [512, TL], F32)
    K.rwBON = scratch("rwBON", [512, TL], F32)
    K.rwY0 = scratch("rwY0", [512, TL], F32)
    K.x1d = scratch("x1d", [TL, D], F32)
    K.urd = scratch("urd", [TL, ROWW], BF16)
    K.xe_d = [scratch("xe_d%d" % e_, [CAP, ROWW], BF16) for e_ in range(NE)]
    K.yacc = scratch("yacc", [TL, D], F32)
    K.dbg_pos = scratch("dbg_pos", [128, TL // 128, 16], F32) if 'dbg_pos' in debug else None
    K.out = nc.dram_tensor("out", [TL, D], F32, kind="ExternalOutput").ap()
    allp = ['mod', 'inproj', 'mlaprep', 'attn', 'rwprep', 'rwscan', 'mix', 'moe', 'final']
    if phases is None:
        phases = allp
    with ExitStack() as es:
        S = Sync(nc, es)
        K.S = S
        if 'mod' in phases:
            phase_mod(K)
        if 'inproj' in phases:
            phase_inproj(K)
        if 'mlaprep' in phases:
            phase_mlaprep(K)
        if 'attn' in phases:
            phase_attn(K)
        if 'attn1' in phases:
            phase_attn(K, heads=(1,), nqt=2)
        if 'rwprep' in phases:
            phase_rwprep(K)
        if 'rwscan' in phases:
            phase_rwscan(K, **opts.get('rwscan', {}))
        if 'mix' in phases:
            phase_mix(K)
        if 'moe' in phases:
            phase_moe(K, **opts.get('moe', {}))
        if 'final' in phases:
            phase_final(K)
        S.wait_all('sp')
        print("instructions", S.n_inst, "waits", S.n_wait, "sems", S.nsem + NDMA)
    return nc


_SWAP = np.concatenate([np.arange(16, 32), np.arange(0, 16), np.arange(48, 64), np.arange(32, 48)])


def rope_tables():
    half = 32
    inv_freq = (10000.0 ** (-np.arange(0, half, 2, dtype=np.float32) / half)).astype(np.float32)
    t = np.arange(TL)
    rr = (t // 64).astype(np.float32)[None, :]
    cc = (t % 64).astype(np.float32)[None, :]
    ang_r = (inv_freq[:, None] * rr).astype(np.float32)
    ang_c = (inv_freq[:, None] * cc).astype(np.float32)
    cosT = np.concatenate([np.cos(ang_r), np.cos(ang_r), np.cos(ang_c), np.cos(ang_c)], 0).astype(np.float32)
    sinT = np.concatenate([-np.sin(ang_r), np.sin(ang_r), -np.sin(ang_c), np.sin(ang_c)], 0).astype(np.float32)
    return np.ascontiguousarray(cosT), np.ascontiguousarray(sinT)


def make_in_maps(inputs, batches):
    f = lambda a: np.ascontiguousarray(np.asarray(a, dtype=np.float32))
    w_in = f(inputs['w_in'][0])
    w_in_ext = np.concatenate([w_in, w_in[:, 2432:2496][:, _SWAP]], axis=1)
    cosT, sinT = rope_tables()
    wuq = f(inputs['mla_w_uq'][0])
    cols = []
    for h in range(4):
        nope = wuq[:, h * 192:h * 192 + 128]
        rope = wuq[:, h * 192 + 128:h * 192 + 192]
        cols += [nope, rope, rope[:, _SWAP]]
    wuq_ext = np.ascontiguousarray(np.concatenate(cols, axis=1))
    shared = {
        'w_ada': f(inputs['w_ada'][0]), 'b_ada': f(inputs['b_ada']), 'w_in': np.ascontiguousarray(w_in_ext),
        'cosT': cosT, 'sinT': sinT, 'ident': np.eye(128).astype(ml_dtypes.bfloat16),
        'mla_q_norm': f(inputs['mla_q_norm']), 'mla_kv_norm': f(inputs['mla_kv_norm']),
        'w_uq': wuq_ext, 'w_uk': f(inputs['mla_w_uk'][0]), 'w_uv': f(inputs['mla_w_uv'][0]),
        'rwkv_conv': f(inputs['rwkv_conv'][0]), 'rwkv_w0': f(inputs['rwkv_w0'][0]), 'rwkv_w_up': f(inputs['rwkv_w_up'][0]),
        'rwkv_a0': f(inputs['rwkv_a0'][0]), 'rwkv_a_up': f(inputs['rwkv_a_up'][0]), 'rwkv_g_up': f(inputs['rwkv_g_up'][0]),
        'rwkv_k_k': f(inputs['rwkv_k_k']), 'rwkv_k_a': f(inputs['rwkv_k_a']), 'rwkv_r_k': f(inputs['rwkv_r_k']).reshape(1, 512),
        'rwkv_gn_g': f(inputs['rwkv_gn_g']), 'rwkv_gn_b': f(inputs['rwkv_gn_b']),
        'w_out': f(inputs['w_out'][0]), 'ln1_g': f(inputs['ln1_g']), 'ln1_b': f(inputs['ln1_b']),
        'ln2_g': f(inputs['ln2_g']), 'ln2_b': f(inputs['ln2_b']), 'router': f(inputs['router'][0]),
        'ustrict': (np.arange(128)[:, None] < np.arange(128)[None, :]).astype(ml_dtypes.bfloat16),
        'exp_w_gate': f(inputs['exp_w_gate'][0]), 'exp_w_up': f(inputs['exp_w_up'][0]), 'exp_w_down': f(inputs['exp_w_down'][0]),
    }
    shared.update(rw_consts())
    maps = []
    for b in batches:
        m = dict(shared)
        m['xin'] = np.ascontiguousarray(np.concatenate([inputs['ctx'][b], inputs['x'][b]], axis=0).astype(np.float32))
        cc = np.stack([inputs['c'][b], inputs['c_ctx']], axis=-1).astype(np.float32)
        m['ccT'] = np.ascontiguousarray(cc.reshape(8, 128, 2).transpose(1, 0, 2))
        maps.append(m)
    return maps


_CONST_KEYS = ('cosT', 'sinT', 'ident', 'identf', 'i64dbl', 'bones', 'rwm0', 'rwm1', 'ustrict')
BATCH_CORES = (0, 1, 4, 5)


def kernel(**inputs):
    nc = build_program()
    real = make_in_maps(inputs, [0, 1, 2, 3])
    zero = {k: (v if k in _CONST_KEYS else np.zeros_like(v)) for k, v in real[0].items()}
    maps = [zero] * 8
    for b, c in enumerate(BATCH_CORES):
        maps[c] = real[b]
    res = run_bass_kernel_spmd(nc, maps, core_ids=list(range(8)))
    out = np.stack([res.results[c]['out'] for c in BATCH_CORES], axis=0)
    return out.astype(np.float32)
```

```python
import numpy as np
import ml_dtypes
from contextlib import ExitStack
import concourse.bass as bass
import concourse.mybir as mybir
from concourse.bass_utils import run_bass_kernel_spmd

F32 = mybir.dt.float32
BF16 = mybir.dt.bfloat16
I32 = mybir.dt.int32
AF = mybir.ActivationFunctionType
ALU = mybir.AluOpType
AX = mybir.AxisListType

EPOCH = 12000
NDMA = 24

D = 1024
TL = 8192
TC = 256
TA = TL + TC
NE = 16
CAP = 1024


class Sync:
    def __init__(self, nc, es):
        self.nc = nc
        self.es = es
        self.eng = {'pe': nc.tensor, 'dve': nc.vector, 'act': nc.scalar,
                    'pool': nc.gpsimd, 'sp': nc.sync}
        self.sem = {}
        self.cnt = {}
        self.cur = {}
        self.known = {e: {} for e in self.eng}
        self.snap = {}
        self.last_w = {}
        self.readers = {}
        self.dma_keys = []
        self.dma_rr = 0
        self.nsem = 0
        for e in self.eng:
            self._new_epoch(e)
        for i in range(NDMA):
            k = ('dma', i)
            self.sem[k] = es.enter_context(nc.semaphore('dq%d' % i))
            self.cnt[k] = 0
            self.dma_keys.append(k)
        self.n_inst = 0
        self.n_wait = 0

    def _new_epoch(self, e):
        idx = self.nsem
        self.nsem += 1
        k = (e, idx)
        self.sem[k] = self.es.enter_context(self.nc.semaphore('s_%s_%d' % (e, idx)))
        self.cnt[k] = 0
        self.cur[e] = k

    def _need(self, e, ticket):
        k, v = ticket
        kn = self.known[e]
        if kn.get(k, 0) >= v:
            return
        self.eng[e].wait_ge(self.sem[k], v)
        self.n_wait += 1
        kn[k] = v
        sn = self.snap.get(ticket)
        if sn:
            for kk, vv in sn.items():
                if kn.get(kk, 0) < vv:
                    kn[kk] = vv

    def _deps(self, e, reads, writes, acc):
        for b in reads:
            t = self.last_w.get(b)
            if t is not None:
                self._need(e, t)
        for b in writes:
            t = self.last_w.get(b)
            if t is not None and not (acc and t[0] == self.cur[e]):
                self._need(e, t)
            for t in self.readers.get(b, ()):
                self._need(e, t)

    def _record(self, ticket, reads, writes):
        for b in reads:
            self.readers.setdefault(b, []).append(ticket)
        for b in writes:
            self.last_w[b] = ticket
            self.readers[b] = []

    def op(self, e, fn, reads=(), writes=(), acc=False):
        if self.cnt[self.cur[e]] >= EPOCH:
            self._new_epoch(e)
        self._deps(e, reads, writes, acc)
        k = self.cur[e]
        inst = fn(self.eng[e])
        inst.then_inc(self.sem[k], 1)
        self.cnt[k] += 1
        t = (k, self.cnt[k])
        self.snap[t] = dict(self.known[e])
        self._record(t, reads, writes)
        self.n_inst += 1
        return t

    def dma(self, e, fn, reads=(), writes=()):
        k = self.dma_keys[self.dma_rr]
        self.dma_rr = (self.dma_rr + 1) % NDMA
        if self.cnt[k] > 0:
            self._need(e, (k, self.cnt[k]))
        self._deps(e, reads, writes, False)
        inst = fn(self.eng[e])
        inst.then_inc(self.sem[k], 16)
        self.cnt[k] += 16
        t = (k, self.cnt[k])
        self.snap[t] = dict(self.known[e])
        self._record(t, reads, writes)
        self.n_inst += 1
        return t

    def wait_all(self, e):
        for k, v in list(self.cnt.items()):
            if v > 0:
                self._need(e, (k, v))

    def barrier(self):
        for e in self.eng:
            self.wait_all(e)
        self.last_w.clear()
        self.readers.clear()


class Ctx:
    pass


_UC = [0]


def _u(n):
    _UC[0] += 1
    return '%s_%d' % (n, _UC[0])


def phase_mod(K):
    nc, S = K.nc, K.S
    with ExitStack() as es:
        sb = lambda n, s, d: es.enter_context(nc.sbuf_tensor(_u(n), s, d))
        ccs = sb("ccs", [128, 8, 2], F32)
        scT = sb("scT", [128, 8, 2], F32)
        wa = [sb("wa%d" % i, [128, 8, 512], F32) for i in range(2)]
        ba = sb("ba", [1, 6144], F32)
        one1 = sb("one1", [1, 1], F32)
        mrow = [sb("mrow%d" % r, [1, 6144], F32) for r in range(2)]
        pm = [es.enter_context(nc.psum_tensor(_u("pm%d" % i), [1, 512], F32)) for i in range(2)]
        S.dma('sp', lambda e: e.dma_start(out=ccs[:], in_=K.ccT), writes=['ccs'])
        S.dma('sp', lambda e: e.dma_start(out=ba[:], in_=K.b_ada), writes=['ba'])
        S.op('dve', lambda e: e.memset(one1[:], 1.0), writes=['one1'])
        S.op('act', lambda e: e.activation(out=scT[:], in_=ccs[:], func=AF.Silu), reads=['ccs'], writes=['scT'])
        for j in range(12):
            w = wa[j % 2]
            wk = 'wa%d' % (j % 2)
            S.dma('sp', lambda e: e.dma_start(out=w[:], in_=K.w_ada[:, j * 512:(j + 1) * 512].rearrange("(k p) n -> p k n", p=128)), writes=[wk])
            for r in range(2):
                if r == 1 and j >= 4:
                    continue
                pk = 'pm%d' % r
                for k in range(8):
                    S.op('pe', lambda e: e.matmul(pm[r][:], lhsT=scT[:, k, r:r + 1], rhs=w[:, k, :], start=(k == 0), stop=False),
                         reads=['scT', wk], writes=[pk], acc=True)
                S.op('pe', lambda e: e.matmul(pm[r][:], lhsT=one1[:], rhs=ba[:, j * 512:(j + 1) * 512], start=False, stop=True),
                     reads=['one1', 'ba'], writes=[pk], acc=True)
                S.op('dve', lambda e: e.tensor_copy(out=mrow[r][:, j * 512:(j + 1) * 512], in_=pm[r][:]), reads=[pk], writes=['mrow%d' % r])
        S.dma('sp', lambda e: e.dma_start(out=K.modd[0:1, :], in_=mrow[0][:]), reads=['mrow0'], writes=['modd'])
        S.dma('sp', lambda e: e.dma_start(out=K.modd[1:2, 0:2048], in_=mrow[1][:, 0:2048]), reads=['mrow1'], writes=['modd'])
        S.barrier()


def phase_inproj(K):
    nc, S = K.nc, K.S
    with ExitStack() as es:
        sb = lambda n, s, d: es.enter_context(nc.sbuf_tensor(_u(n), s, d))
        ps = lambda n, s, d: es.enter_context(nc.psum_tensor(_u(n), s, d))
        wb = sb("wb", [128, 8, 2560], BF16)
        wst = [sb("wst%d" % i, [128, 8, 320], F32) for i in range(2)]
        idt = sb("idt", [128, 128], BF16)
        epst = sb("epst", [128, 1], F32)
        scp = [sb("scp%d" % r, [128, 8], F32) for r in range(2)]
        shp = [sb("shp%d" % r, [128, 8], F32) for r in range(2)]
        xt = [sb("xt%d" % i, [128, 1024], F32) for i in range(2)]
        xn = [sb("xn%d" % i, [128, 1024], BF16) for i in range(2)]
        st = sb("st", [128, 2, 6], F32)
        mv = sb("mv", [128, 2], F32)
        lnv = sb("lnv", [128, 1], F32)
        rstd = sb("rstd", [128, 1], F32)
        xmT = [sb("xmT%d" % i, [128, 8, 512], BF16) for i in range(2)]
        stg = [sb("stg%d" % i, [128, 4, 512], F32) for i in range(2)]
        cst = [sb("cst%d" % i, [64, 512], F32) for i in range(2)]
        snt = [sb("snt%d" % i, [64, 512], F32) for i in range(2)]
        kr1 = sb("kr1", [64, 512], F32)
        kr2 = sb("kr2", [64, 512], F32)
        krb = sb("krb", [64, 512], BF16)
        pt = [ps("pt%d" % i, [128, 1024], BF16) for i in range(2)]
        po = [ps("po%d" % i, [128, 512], F32) for i in range(3)]
        pk = [ps("pk%d" % i, [64, 512], F32) for i in range(2)]

        S.dma('sp', lambda e: e.dma_start(out=idt[:], in_=K.ident), writes=['idt'])
        S.op('dve', lambda e: e.memset(epst[:], 1e-5), writes=['epst'])
        for r in range(2):
            S.dma('sp', lambda e: e.dma_start(out=shp[r][:], in_=K.modd[r:r + 1, 0:1024].rearrange("o (k p) -> p (o k)", p=128), allow_slow_non_contiguous=True), reads=['modd'], writes=['shp%d' % r])
            S.dma('sp', lambda e: e.dma_start(out=scp[r][:], in_=K.modd[r:r + 1, 1024:2048].rearrange("o (k p) -> p (o k)", p=128), allow_slow_non_contiguous=True), reads=['modd'], writes=['scp%d' % r])
            S.op('dve', lambda e: e.tensor_scalar_add(out=scp[r][:], in0=scp[r][:], scalar1=1.0), reads=['scp%d' % r], writes=['scp%d' % r])
        for j in range(8):
            w = wst[j % 2]
            wk = 'wst%d' % (j % 2)
            S.dma('sp', lambda e: e.dma_start(out=w[:], in_=K.w_in[:, j * 320:(j + 1) * 320].rearrange("(k p) n -> p k n", p=128)), writes=[wk])
            S.op('pool', lambda e: e.tensor_copy(out=wb[:, :, j * 320:(j + 1) * 320], in_=w[:]), reads=[wk], writes=['wb'])

        groups = [(0, 256, 1)] + [(256 + g * 512, 512, 0) for g in range(16)]
        tile_i = 0
        ev = 0
        def gbody(gi, t0, G, r):
            nonlocal tile_i, ev
            cs_, sn_ = cst[gi % 2], snt[gi % 2]
            csk, snk = 'cst%d' % (gi % 2), 'snt%d' % (gi % 2)
            xm = xmT[gi % 2]
            xmk = 'xmT%d' % (gi % 2)
            if r == 0:
                S.dma('sp', lambda e: e.dma_start(out=cs_[:], in_=K.cosT[:, t0 - 256:t0 - 256 + 512]), writes=[csk])
                S.dma('sp', lambda e: e.dma_start(out=sn_[:], in_=K.sinT[:, t0 - 256:t0 - 256 + 512]), writes=[snk])
            for i in range(G // 128):
                sl = tile_i % 2
                tile_i += 1
                xk, xnk, ptk = 'xt%d' % sl, 'xn%d' % sl, 'pt%d' % sl
                tt = t0 + i * 128
                S.dma('sp', lambda e: e.dma_start(out=xt[sl][:], in_=K.xin[tt:tt + 128, :]), writes=[xk])
                for c in range(2):
                    S.op('dve', lambda e: e.bn_stats(out=st[:, c, :], in_=xt[sl][:, c * 512:(c + 1) * 512]), reads=[xk], writes=['st%d' % c])
                S.op('dve', lambda e: e.bn_aggr(out=mv[:], in_=st[:]), reads=['st0', 'st1'], writes=['mv'])
                S.op('act', lambda e: e.activation(out=lnv[:], in_=mv[:, 1:2], func=AF.Ln, bias=epst[:], scale=1.0), reads=['mv', 'epst'], writes=['lnv'])
                S.op('act', lambda e: e.activation(out=rstd[:], in_=lnv[:], func=AF.Exp, scale=-0.5), reads=['lnv'], writes=['rstd'])
                S.op('dve', lambda e: e.tensor_scalar(out=xn[sl][:], in0=xt[sl][:], scalar1=mv[:, 0:1], scalar2=rstd[:], op0=ALU.subtract, op1=ALU.mult),
                     reads=[xk, 'mv', 'rstd'], writes=[xnk])
                for k in range(8):
                    S.op('pe', lambda e: e.transpose(out=pt[sl][:, k * 128:(k + 1) * 128], in_=xn[sl][:, k * 128:(k + 1) * 128], identity=idt[:]),
                         reads=[xnk, 'idt'], writes=[ptk], acc=True)
                for k in range(8):
                    S.op('act', lambda e: e.activation(out=xm[:, k, i * 128:(i + 1) * 128], in_=pt[sl][:, k * 128:(k + 1) * 128], func=AF.Identity,
                                                       bias=shp[r][:, k:k + 1], scale=scp[r][:, k:k + 1]),
                         reads=[ptk, 'shp%d' % r, 'scp%d' % r], writes=[xmk])
                yield 'f'
            yield 'F'
            for cb in range(5):
                sg = stg[cb % 2]
                sgk = 'stg%d' % (cb % 2)
                ncols = 4 if cb < 4 else 3
                for cc in range(ncols):
                    ci = cb * 4 + cc
                    p = po[ev % 3]
                    pkk = 'po%d' % (ev % 3)
                    for k in range(8):
                        S.op('pe', lambda e: e.matmul(p[:, 0:G], lhsT=wb[:, k, ci * 128:(ci + 1) * 128], rhs=xm[:, k, 0:G], start=(k == 0), stop=(k == 7)),
                             reads=['wb', xmk], writes=[pkk], acc=True)
                    if ev % 2 == 0:
                        S.op('dve', lambda e: e.tensor_copy(out=sg[:, cc, 0:G], in_=p[:, 0:G]), reads=[pkk], writes=[sgk])
                    else:
                        S.op('act', lambda e: e.activation(out=sg[:, cc, 0:G], in_=p[:, 0:G], func=AF.Identity), reads=[pkk], writes=[sgk])
                    ev += 1
                r0 = cb * 512
                S.dma('pool', lambda e: e.dma_start(out=K.hT[r0:r0 + ncols * 128, t0:t0 + G].rearrange("(c p) t -> p c t", p=128), in_=sg[:, 0:ncols, 0:G]),
                      reads=[sgk], writes=['hT'])
                yield 'b'
            for q in range(2):
                for k in range(8):
                    S.op('pe', lambda e: e.matmul(pk[q][:, 0:G], lhsT=wb[:, k, 2432 + q * 64:2432 + (q + 1) * 64], rhs=xm[:, k, 0:G], start=(k == 0), stop=(k == 7)),
                         reads=['wb', xmk], writes=['pk%d' % q], acc=True)
            if r == 0:
                S.op('dve', lambda e: e.tensor_tensor(out=kr1[:], in0=pk[0][:], in1=cs_[:], op=ALU.mult), reads=['pk0', csk], writes=['kr1'])
                S.op('dve', lambda e: e.tensor_tensor(out=kr2[:], in0=pk[1][:], in1=sn_[:], op=ALU.mult), reads=['pk1', snk], writes=['kr2'])
                S.op('dve', lambda e: e.tensor_tensor(out=krb[:], in0=kr1[:], in1=kr2[:], op=ALU.add), reads=['kr1', 'kr2'], writes=['krb'])
            else:
                S.op('dve', lambda e: e.tensor_copy(out=krb[:, 0:G], in_=pk[0][:, 0:G]), reads=['pk0', 'pk1'], writes=['krb'])
            S.dma('pool', lambda e: e.dma_start(out=K.krT[:, t0:t0 + G], in_=krb[:, 0:G]), reads=['krb'], writes=['krT'])
        def step(gen_):
            try:
                return next(gen_)
            except StopIteration:
                return None
        prev = None
        for gi, (t0, G, r) in enumerate(groups):
            gcur = gbody(gi, t0, G, r)
            while True:
                rr = step(gcur)
                if prev is not None and step(prev) is None:
                    prev = None
                if rr == 'F' or rr is None:
                    break
            while prev is not None:
                if step(prev) is None:
                    prev = None
            prev = gcur
        while prev is not None:
            if step(prev) is None:
                prev = None
        S.barrier()


class Rot:
    def __init__(self, bufs, prefix):
        self.bufs = bufs
        self.prefix = prefix
        self.i = 0

    def get(self):
        j = self.i % len(self.bufs)
        self.i += 1
        return self.bufs[j], '%s%d' % (self.prefix, j)


SCALE_ATT = 192.0 ** -0.5


def phase_mlaprep(K):
    nc, S = K.nc, K.S
    with ExitStack() as es:
        sb = lambda n, s, d: es.enter_context(nc.sbuf_tensor(_u(n), s, d))
        ps = lambda n, s, d: es.enter_context(nc.psum_tensor(_u(n), s, d))
        wuq = sb("wuq", [128, 2, 1024], BF16)
        wuk = sb("wuk", [128, 2, 512], BF16)
        wuv = sb("wuv", [128, 2, 512], BF16)
        wst = [sb("mwst%d" % i, [128, 2, 512], F32) for i in range(2)]
        gq = sb("gq", [128, 2], F32)
        gkv = sb("gkv", [128, 2], F32)
        onesf = sb("onesf", [128, 128], F32)
        epst = sb("epst", [128, 1], F32)
        ql = [sb("ql%d" % i, [128, 4, 512], F32) for i in range(2)]
        sq = sb("sq", [128, 4, 512], F32)
        lnt = sb("lnt", [128, 512], F32)
        rs = [sb("rs%d" % i, [128, 512], F32) for i in range(2)]
        nb = [sb("nb%d" % i, [128, 4, 512], BF16) for i in range(2)]
        qst = [sb("qst%d" % i, [128, 4, 512], BF16) for i in range(2)]
        kst = [sb("kst%d" % i, [128, 4, 512], BF16) for i in range(2)]
        qrb = [sb("qrb%d" % i, [64, 4, 512], BF16) for i in range(2)]
        vst = Rot([sb("vst%d" % i, [128, 512], BF16) for i in range(3)], "vst")
        cst = [sb("cst%d" % i, [64, 512], F32) for i in range(2)]
        snt = [sb("snt%d" % i, [64, 512], F32) for i in range(2)]
        r1 = sb("r1", [64, 512], F32)
        r2 = sb("r2", [64, 512], F32)
        pp = Rot([ps("mp%d" % i, [128, 512], F32) for i in range(7)], "mp")

        S.op('dve', lambda e: e.memset(epst[:], 1e-6), writes=['epst'])
        S.op('dve', lambda e: e.memset(onesf[:], 1.0), writes=['onesf'])
        S.dma('sp', lambda e: e.dma_start(out=gq[:], in_=K.mla_q_norm.rearrange("o (c p) -> p (o c)", p=128), allow_slow_non_contiguous=True), writes=['gq'])
        S.dma('sp', lambda e: e.dma_start(out=gkv[:], in_=K.mla_kv_norm.rearrange("o (c p) -> p (o c)", p=128), allow_slow_non_contiguous=True), writes=['gkv'])
        wl = 0
        for (src, dst, dk, n) in [(K.w_uq, wuq, 'wuq', 1024), (K.w_uk, wuk, 'wuk', 512), (K.w_uv, wuv, 'wuv', 512)]:
            for j in range(n // 512):
                w = wst[wl % 2]
                wk = 'mwst%d' % (wl % 2)
                wl += 1
                S.dma('sp', lambda e: e.dma_start(out=w[:], in_=src[:, j * 512:(j + 1) * 512].rearrange("(c p) n -> p c n", p=128)), writes=[wk])
                S.op('pool', lambda e: e.tensor_copy(out=dst[:, :, j * 512:(j + 1) * 512], in_=w[:]), reads=[wk], writes=[dk])

        groups = [(0, 256, 1)] + [(256 + g * 512, 512, 0) for g in range(16)]
        ev = [0]

        def evac(out_ap, in_ap, reads, writes):
            if ev[0] % 2 == 0:
                S.op('dve', lambda e: e.tensor_copy(out=out_ap, in_=in_ap), reads=reads, writes=writes)
            else:
                S.op('act', lambda e: e.activation(out=out_ap, in_=in_ap, func=AF.Identity), reads=reads, writes=writes)
            ev[0] += 1

        def gbody(gi, t0, G, r):
            cs_, sn_ = cst[gi % 2], snt[gi % 2]
            csk, snk = 'cst%d' % (gi % 2), 'snt%d' % (gi % 2)
            q_ = ql[gi % 2]
            qk = 'ql%d' % (gi % 2)
            n_ = nb[gi % 2]
            nk = 'nb%d' % (gi % 2)
            S.dma('sp', lambda e: e.dma_start(out=q_[:, :, 0:G], in_=K.hT[1920:2432, t0:t0 + G].rearrange("(c p) t -> p c t", p=128)), reads=['hT'], writes=[qk])
            if r == 0:
                S.dma('sp', lambda e: e.dma_start(out=cs_[:], in_=K.cosT[:, t0 - 256:t0 - 256 + 512]), writes=[csk])
                S.dma('sp', lambda e: e.dma_start(out=sn_[:], in_=K.sinT[:, t0 - 256:t0 - 256 + 512]), writes=[snk])
            S.op('act', lambda e: e.activation(out=sq[:, :, 0:G], in_=q_[:, :, 0:G], func=AF.Square), reads=[qk], writes=['sq'])
            for pair in range(2):
                if pair == 0 and r == 1:
                    continue
                p, pk = pp.get()
                for c in range(2):
                    S.op('pe', lambda e: e.matmul(p[:, 0:G], lhsT=onesf[:], rhs=sq[:, pair * 2 + c, 0:G], start=(c == 0), stop=(c == 1)),
                         reads=['onesf', 'sq'], writes=[pk], acc=True)
                S.op('act', lambda e: e.activation(out=lnt[:, 0:G], in_=p[:, 0:G], func=AF.Ln, bias=epst[:], scale=1.0 / 256.0), reads=[pk, 'epst'], writes=['lnt'])
                S.op('act', lambda e: e.activation(out=rs[pair][:, 0:G], in_=lnt[:, 0:G], func=AF.Exp, scale=-0.5), reads=['lnt'], writes=['rs%d' % pair])
                g_ = gq if pair == 0 else gkv
                for c in range(2):
                    S.op('dve', lambda e: e.scalar_tensor_tensor(out=n_[:, pair * 2 + c, 0:G], in0=q_[:, pair * 2 + c, 0:G], scalar=g_[:, c:c + 1], in1=rs[pair][:, 0:G],
                                                                 op0=ALU.mult, op1=ALU.mult),
                         reads=[qk, 'rs%d' % pair, 'gq', 'gkv'], writes=[nk])
            yield 'F'
            if r == 0:
                tq = t0 - 256
                qs = qst[gi % 2]
                qsk = 'qst%d' % (gi % 2)
                qr_ = qrb[gi % 2]
                qrk = 'qrb%d' % (gi % 2)
                for h in range(4):
                    p, pk = pp.get()
                    for c in range(2):
                        S.op('pe', lambda e: e.matmul(p[:, 0:G], lhsT=wuq[:, c, h * 256:h * 256 + 128], rhs=n_[:, c, 0:G], start=(c == 0), stop=(c == 1)),
                             reads=['wuq', nk], writes=[pk], acc=True)
                    evac(qs[:, h, 0:G], p[:, 0:G], [pk], [qsk])
                    p1, pk1 = pp.get()
                    p2, pk2 = pp.get()
                    for c in range(2):
                        S.op('pe', lambda e: e.matmul(p1[0:64, 0:G], lhsT=wuq[:, c, h * 256 + 128:h * 256 + 192], rhs=n_[:, c, 0:G], start=(c == 0), stop=(c == 1)),
                             reads=['wuq', nk], writes=[pk1], acc=True)
                    for c in range(2):
                        S.op('pe', lambda e: e.matmul(p2[0:64, 0:G], lhsT=wuq[:, c, h * 256 + 192:h * 256 + 256], rhs=n_[:, c, 0:G], start=(c == 0), stop=(c == 1)),
                             reads=['wuq', nk], writes=[pk2], acc=True)
                    S.op('dve', lambda e: e.tensor_tensor(out=r1[:], in0=p1[0:64, :], in1=cs_[:], op=ALU.mult), reads=[pk1, csk], writes=['r1'])
                    S.op('dve', lambda e: e.tensor_tensor(out=r2[:], in0=p2[0:64, :], in1=sn_[:], op=ALU.mult), reads=[pk2, snk], writes=['r2'])
                    S.op('dve', lambda e: e.tensor_tensor(out=qr_[:, h, :], in0=r1[:], in1=r2[:], op=ALU.add), reads=['r1', 'r2'], writes=[qrk])
                    yield 'b'
                S.dma('pool', lambda e: e.dma_start(out=K.qnT[:, tq:tq + G].rearrange("(h p) t -> p h t", p=128), in_=qs[:, :, 0:G]), reads=[qsk], writes=['qnT'])
                S.dma('pool', lambda e: e.dma_start(out=K.qrT[:, tq:tq + G].rearrange("(h p) t -> p h t", p=64), in_=qr_[:, :, 0:G]), reads=[qrk], writes=['qrT'])
            ks = kst[gi % 2]
            ksk = 'kst%d' % (gi % 2)
            for h in range(4):
                p, pk = pp.get()
                for c in range(2):
                    S.op('pe', lambda e: e.matmul(p[:, 0:G], lhsT=wuk[:, c, h * 128:(h + 1) * 128], rhs=n_[:, 2 + c, 0:G], start=(c == 0), stop=(c == 1)),
                         reads=['wuk', nk], writes=[pk], acc=True)
                evac(ks[:, h, 0:G], p[:, 0:G], [pk], [ksk])
                yield 'b'
            S.dma('pool', lambda e: e.dma_start(out=K.knT[:, t0:t0 + G].rearrange("(h p) t -> p h t", p=128), in_=ks[:, :, 0:G]), reads=[ksk], writes=['knT'])
            for i in range(G // 128):
                p, pk = pp.get()
                for c in range(2):
                    S.op('pe', lambda e: e.matmul(p[:, :], lhsT=n_[:, 2 + c, i * 128:(i + 1) * 128], rhs=wuv[:, c, :], start=(c == 0), stop=(c == 1)),
                         reads=['wuv', nk], writes=[pk], acc=True)
                v_, vk = vst.get()
                evac(v_[:], p[:], [pk], [vk])
                S.dma('pool', lambda e: e.dma_start(out=K.vtok[t0 + i * 128:t0 + (i + 1) * 128, :], in_=v_[:]), reads=[vk], writes=['vtok'])
        def step(gen_):
            try:
                return next(gen_)
            except StopIteration:
                return None
        prev = None
        for gi, (t0, G, r) in enumerate(groups):
            gcur = gbody(gi, t0, G, r)
            while True:
                rr = step(gcur)
                if prev is not None and step(prev) is None:
                    prev = None
                if rr == 'F' or rr is None:
                    break
            while prev is not None:
                if step(prev) is None:
                    prev = None
            prev = gcur
        while prev is not None:
            if step(prev) is None:
                prev = None
        S.barrier()


def phase_attn(K, heads=(0, 1, 2, 3), nqt=16):
    nc, S = K.nc, K.S
    NKT = TA // 128
    with ExitStack() as es:
        sb = lambda n, s, d: es.enter_context(nc.sbuf_tensor(_u(n), s, d))
        ps = lambda n, s, d: es.enter_context(nc.psum_tensor(_u(n), s, d))
        krs = sb("krs", [64, TA], BF16)
        kn = [sb("kn%d" % i, [128, TA], BF16) for i in range(2)]
        vh = [sb("vh%d" % i, [128, NKT, 128], BF16) for i in range(2)]
        qn = [sb("qn%d" % i, [128, 512], BF16) for i in range(2)]
        qr = [sb("qr%d" % i, [64, 512], BF16) for i in range(2)]
        onesb = sb("onesb", [128, 128], BF16)
        pT = Rot([sb("pT%d" % i, [128, 512], BF16) for i in range(3)], "pT")
        rl = sb("rl", [128, 512], F32)
        ob = [sb("ob%d" % i, [128, 512], BF16) for i in range(2)]
        psc = Rot([ps("psc%d" % i, [128, 512], F32) for i in range(3)], "psc")
        pO = [ps("pO%d" % i, [128, 512], F32) for i in range(2)]
        pL = [ps("pL%d" % i, [128, 512], F32) for i in range(2)]

        S.op('dve', lambda e: e.memset(onesb[:], 1.0), writes=['onesb'])
        S.dma('sp', lambda e: e.dma_start(out=krs[:], in_=K.krT), reads=['krT'], writes=['krs'])
        qi = 0
        for hi, h in enumerate(heads):
            k_ = kn[hi % 2]
            kk = 'kn%d' % (hi % 2)
            v_ = vh[hi % 2]
            vk = 'vh%d' % (hi % 2)
            S.dma('sp', lambda e: e.dma_start(out=k_[:], in_=K.knT[h * 128:(h + 1) * 128, :]), reads=['knT'], writes=[kk])
            S.dma('sp', lambda e: e.dma_start(out=v_[:], in_=K.vtok[:, h * 128:(h + 1) * 128].rearrange("(kt p) d -> p kt d", p=128)), reads=['vtok'], writes=[vk])
            for qt in range(nqt):
                sl = qi % 2
                qi += 1
                qnk, qrk = 'qn%d' % sl, 'qr%d' % sl
                S.dma('sp', lambda e: e.dma_start(out=qn[sl][:], in_=K.qnT[h * 128:(h + 1) * 128, qt * 512:(qt + 1) * 512]), reads=['qnT'], writes=[qnk])
                S.dma('sp', lambda e: e.dma_start(out=qr[sl][:], in_=K.qrT[h * 64:(h + 1) * 64, qt * 512:(qt + 1) * 512]), reads=['qrT'], writes=[qrk])
                Ok, Lk = 'pO%d' % sl, 'pL%d' % sl
                pend = []

                def scores(kt):
                    p, pk = psc.get()
                    S.op('pe', lambda e: e.matmul(p[:], lhsT=k_[:, kt * 128:(kt + 1) * 128], rhs=qn[sl][:], start=True, stop=False),
                         reads=[kk, qnk], writes=[pk], acc=True)
                    S.op('pe', lambda e: e.matmul(p[:], lhsT=krs[:, kt * 128:(kt + 1) * 128], rhs=qr[sl][:], start=False, stop=True),
                         reads=['krs', qrk], writes=[pk], acc=True)
                    t_, tk = pT.get()
                    S.op('act', lambda e: e.activation(out=t_[:], in_=p[:], func=AF.Exp, scale=SCALE_ATT), reads=[pk], writes=[tk])
                    pend.append((kt, t_, tk))

                def pv():
                    kt, t_, tk = pend.pop(0)
                    S.op('pe', lambda e: e.matmul(pO[sl][:], lhsT=v_[:, kt, :], rhs=t_[:], start=(kt == 0), stop=(kt == NKT - 1)),
                         reads=[vk, tk], writes=[Ok], acc=True)
                    S.op('pe', lambda e: e.matmul(pL[sl][:], lhsT=onesb[:], rhs=t_[:], start=(kt == 0), stop=(kt == NKT - 1)),
                         reads=['onesb', tk], writes=[Lk], acc=True)

                scores(0)
                scores(1)
                for kt in range(NKT):
                    pv()
                    if kt + 2 < NKT:
                        scores(kt + 2)
                S.op('dve', lambda e: e.reciprocal(out=rl[:], in_=pL[sl][:]), reads=[Lk], writes=['rl'])
                S.op('dve', lambda e: e.tensor_tensor(out=ob[sl][:], in0=pO[sl][:], in1=rl[:], op=ALU.mult), reads=[Ok, 'rl'], writes=['ob%d' % sl])
                S.dma('pool', lambda e: e.dma_start(out=K.mixT[512 + h * 128:512 + (h + 1) * 128, qt * 512:(qt + 1) * 512], in_=ob[sl][:]), reads=['ob%d' % sl], writes=['mixT'])
        S.barrier()


F32R = mybir.dt.float32r
LDS = -0.6065306597126334


class LaneS:
    LOCAL = ('cv', 'tw', 'sg', 'kq', 'sq', 'lnt', 'kk_', 'av', 'tt', 'kd0', 'kd1', 'bb', 'gg', 'bc', 'bon')

    def __init__(self, S, lane):
        self.S = S
        self.lane = lane

    def k(self, x):
        if x == 'bones':
            return x
        for p in self.LOCAL:
            if x.startswith(p):
                return '%s@%d' % (x, self.lane)
        return x

    def op(self, e, fn, reads=(), writes=(), acc=False):
        return self.S.op(e, fn, [self.k(x) for x in reads], [self.k(x) for x in writes], acc)

    def dma(self, e, fn, reads=(), writes=()):
        return self.S.dma(e, fn, [self.k(x) for x in reads], [self.k(x) for x in writes])


def phase_rwprep(K):
    nc, S = K.nc, K.S
    with ExitStack() as es:
        sb = lambda n, s, d: es.enter_context(nc.sbuf_tensor(_u(n), s, d))
        ps = lambda n, s, d: es.enter_context(nc.psum_tensor(_u(n), s, d))
        G = 256
        cw = sb("cw", [128, 3, 12], F32)
        kkv = sb("kkv", [128, 4], F32)
        kav = sb("kav", [128, 4], F32)
        omka = sb("omka", [128, 4], F32)
        rkv = sb("rkv", [128, 4], F32)
        a0v = sb("a0v", [128, 2, 4], F32)
        w0r = sb("w0r", [1, 2, 512], F32)
        ones1 = sb("ones1", [1, 128], F32)
        wup = sb("wup", [64, 2, 512], F32)
        aup = sb("aup", [64, 2, 512], F32)
        gup = sb("gup", [128, 512], F32)
        bones = sb("bones", [128, 128], F32)
        idf = sb("idf", [128, 128], F32)
        eps12 = sb("eps12", [128, 1], F32)
        hr = [sb("hr%d" % i, [128, 12, G + 2], F32) for i in range(2)]
        lo = [sb("lo%d" % i, [64, 4, G], F32) for i in range(2)]
        gd = [sb("gd%d" % i, [128, G], F32) for i in range(2)]
        cvL = [sb("cv_%d" % i_, [128, 12, G], F32) for i_ in range(2)]
        twL = [sb("tw_%d" % i_, [64, 2, G], F32R) for i_ in range(2)]
        sgL = [sb("sg_%d" % i_, [128, G], F32R) for i_ in range(2)]
        kqL = [sb("kq_%d" % i_, [128, 4, G], F32) for i_ in range(2)]
        sqL = [sb("sq_%d" % i_, [128, 4, G], F32R) for i_ in range(2)]
        lntL = [sb("lnt_%d" % i_, [128, 4, G], F32) for i_ in range(2)]
        kk_L = [sb("kk__%d" % i_, [128, 4, G], F32) for i_ in range(2)]
        avL = [sb("av_%d" % i_, [128, 4, G], F32) for i_ in range(2)]
        ttL = [sb("tt_%d" % i_, [128, 4, G], F32) for i_ in range(2)]
        kdL = [[sb("kd%d_%d" % (i, i_), [128, 4, G], F32) for i in range(2)] for i_ in range(2)]
        bbL = [sb("bb_%d" % i_, [128, 4, G], F32) for i_ in range(2)]
        ld = Rot([sb("ld%d" % i, [128, 512], F32) for i in range(2)], "ld")
        vt = Rot([sb("vt%d" % i, [128, 512], F32) for i in range(2)], "vt")
        ggL = [sb("gg_%d" % i_, [128, 4, G], F32) for i_ in range(2)]
        bcL = [sb("bc_%d" % i_, [128, 4, G], F32R) for i_ in range(2)]
        bonL = [sb("bon_%d" % i_, [128, 4, G], F32) for i_ in range(2)]
        pp = Rot([ps("rp%d" % i, [128, 512], F32) for i in range(7)], "rp")

        ld1 = lambda dst, src, key: S.dma('sp', lambda e: e.dma_start(out=dst, in_=src, allow_slow_non_contiguous=True), writes=[key])
        ld1(cw[:], K.rwkv_conv.rearrange("t (c p) -> p t c", p=128), 'cw')
        ld1(kkv[:], K.rwkv_k_k.rearrange("o (c p) -> p (o c)", p=128), 'kkv')
        ld1(kav[:], K.rwkv_k_a.rearrange("o (c p) -> p (o c)", p=128), 'kav')
        ld1(rkv[:], K.rwkv_r_k.rearrange("o (c p) -> p (o c)", p=128), 'rkv')
        ld1(a0v[:], K.rwkv_a0.rearrange("d (c p) -> p d c", p=128), 'a0v')
        ld1(w0r[:], K.rwkv_w0.rearrange("(o d) n -> o d n", o=1), 'w0r')
        ld1(wup[:], K.rwkv_w_up.rearrange("d l n -> l d n"), 'wup')
        ld1(aup[:], K.rwkv_a_up.rearrange("d l n -> l d n"), 'aup')
        ld1(gup[:], K.rwkv_g_up, 'gup')
        ld1(bones[:], K.bones, 'bones')
        ld1(idf[:], K.identf, 'idf')
        S.op('dve', lambda e: e.memset(ones1[:], 1.0), writes=['ones1'])
        wupr = sb("wupr", [64, 2, 512], F32R)
        aupr = sb("aupr", [64, 2, 512], F32R)
        gupr = sb("gupr", [128, 512], F32R)
        bonesr = sb("bonesr", [128, 128], F32R)
        ones1r = sb("ones1r", [1, 128], F32R)
        w0rr = sb("w0rr", [1, 2, 512], F32R)
        lor = [sb("lor%d" % i_, [64, 2, G], F32R) for i_ in range(2)]
        for src_, dst_, k_ in ((wup, wupr, 'wup'), (aup, aupr, 'aup'), (gup, gupr, 'gup'), (bones, bonesr, 'bones'), (ones1, ones1r, 'ones1'), (w0r, w0rr, 'w0r')):
            S.op('dve', lambda e: e.tensor_copy(out=dst_[:], in_=src_[:]), reads=[k_], writes=[k_ + 'r'])
        S.op('dve', lambda e: e.memset(eps12[:], 1e-12), writes=['eps12'])
        S.op('dve', lambda e: e.tensor_scalar(out=omka[:], in0=kav[:], scalar1=-1.0, scalar2=1.0, op0=ALU.mult, op1=ALU.add), reads=['kav'], writes=['omka'])

        nblk = TA // G
        def bbody(bi):
            ln = bi % 2
            S_ = LaneS(S, ln)
            cv, tw, sg, kq, sq, lnt, kk_, av, tt, bb, gg, bc, bon = cvL[ln], twL[ln], sgL[ln], kqL[ln], sqL[ln], lntL[ln], kk_L[ln], avL[ln], ttL[ln], bbL[ln], ggL[ln], bcL[ln], bonL[ln]
            kd = kdL[ln]
            t0 = bi * G
            lat = t0 >= TC
            sl = bi % 2
            h_ = hr[sl]
            hk = 'hr%d' % sl
            first = (t0 == 0 or t0 == TC)
            last = (t0 + G == TC or t0 + G == TA)
            c0 = 1 if first else 0
            c1 = G + 1 if last else G + 2
            if first:
                S_.op('pool', lambda e: e.memset(h_[:, :, 0:1], 0.0), writes=[hk])
            if last:
                S_.op('pool', lambda e: e.memset(h_[:, :, G + 1:G + 2], 0.0), writes=[hk])
            for q in range(3):
                S_.dma('sp', lambda e: e.dma_start(out=h_[:, q * 4:(q + 1) * 4, c0:c1], in_=K.hT[q * 512:(q + 1) * 512, t0 - 1 + c0:t0 - 1 + c1].rearrange("(c p) t -> p c t", p=128)),
                      reads=['hT'], writes=[hk])
            lo_ = lo[sl]
            lk = 'lo%d' % sl
            S_.dma('sp', lambda e: e.dma_start(out=lo_[:], in_=K.hT[1536:1792, t0:t0 + G].rearrange("(c p) t -> p c t", p=64)), reads=['hT'], writes=[lk])
            gd_ = gd[sl]
            gk = 'gd%d' % sl
            if lat:
                S_.dma('sp', lambda e: e.dma_start(out=gd_[:], in_=K.hT[1792:1920, t0:t0 + G]), reads=['hT'], writes=[gk])
            for c in range(12):
                S_.op('act', lambda e: e.activation(out=cv[:, c, :], in_=h_[:, c, 1:G + 1], func=AF.Identity, scale=cw[:, 1, c:c + 1]), reads=[hk, 'cw'], writes=['cv%d' % c])
                S_.op('dve', lambda e: e.scalar_tensor_tensor(out=cv[:, c, :], in0=h_[:, c, 0:G], scalar=cw[:, 0, c:c + 1], in1=cv[:, c, :], op0=ALU.mult, op1=ALU.add),
                     reads=[hk, 'cw', 'cv%d' % c], writes=['cv%d' % c])
                S_.op('dve', lambda e: e.scalar_tensor_tensor(out=cv[:, c, :], in0=h_[:, c, 2:G + 2], scalar=cw[:, 2, c:c + 1], in1=cv[:, c, :], op0=ALU.mult, op1=ALU.add),
                     reads=[hk, 'cw', 'cv%d' % c], writes=['cv%d' % c])
            cvr = ['cv%d' % c for c in range(0, 4)]
            cvk = ['cv%d' % c for c in range(4, 8)]
            cvv = ['cv%d' % c for c in range(8, 12)]
            S_.dma('pool', lambda e: e.dma_start(out=K.rwR[:, t0:t0 + G].rearrange("(c p) t -> p c t", p=128), in_=cv[:, 0:4, :]), reads=cvr, writes=['rwR'])
            S_.dma('pool', lambda e: e.dma_start(out=K.rwV[:, t0:t0 + G].rearrange("(c p) t -> p c t", p=128), in_=cv[:, 8:12, :]), reads=cvv, writes=['rwV'])
            yield 'f'
            for i in range(G // 128):
                p, pk = pp.get()
                for c in range(4):
                    S_.op('pe', lambda e: e.transpose(out=p[:, c * 128:(c + 1) * 128], in_=cv[:, 8 + c, i * 128:(i + 1) * 128], identity=idf[:]), reads=cvv + ['idf'], writes=[pk], acc=True)
                v_, vk = vt.get()
                S_.op('act', lambda e: e.activation(out=v_[:], in_=p[:], func=AF.Identity), reads=[pk], writes=[vk])
                S_.dma('pool', lambda e: e.dma_start(out=K.rwVtok[t0 + i * 128:t0 + (i + 1) * 128, :], in_=v_[:]), reads=[vk], writes=['rwVtok'])
                yield 'f'
            for c in range(4):
                S_.op('dve', lambda e: e.tensor_scalar_mul(out=kq[:, c, :], in0=cv[:, 4 + c, :], scalar1=kkv[:, c:c + 1]), reads=cvk + ['kkv'], writes=['kq'])
            S_.op('act', lambda e: e.activation(out=sq[:], in_=kq[:], func=AF.Square), reads=['kq'], writes=['sq'])
            for c2 in range(2):
                p, pk = pp.get()
                S_.op('pe', lambda e: e.matmul(p[:, 0:2 * G], lhsT=bonesr[:], rhs=sq[:, 2 * c2:2 * c2 + 2, :], start=True, stop=True), reads=['bonesr', 'sq'], writes=[pk])
                S_.op('act', lambda e: e.activation(out=lnt[:, 2 * c2:2 * c2 + 2, :], in_=p[:, 0:2 * G], func=AF.Ln, bias=eps12[:], scale=1.0), reads=[pk, 'eps12'], writes=['lnt'])
            S_.op('act', lambda e: e.activation(out=lnt[:], in_=lnt[:], func=AF.Exp, scale=-0.5), reads=['lnt'], writes=['lnt'])
            S_.op('dve', lambda e: e.tensor_tensor(out=kk_[:], in0=kq[:], in1=lnt[:], op=ALU.mult), reads=['kq', 'lnt'], writes=['kk_'])
            S_.dma('pool', lambda e: e.dma_start(out=K.rwKK[:, t0:t0 + G].rearrange("(c p) t -> p c t", p=128), in_=kk_[:]), reads=['kk_'], writes=['rwKK'])
            yield 'F'
            S_.op('act', lambda e: e.activation(out=tw[:], in_=lo_[:, 0:2, :], func=AF.Tanh), reads=[lk], writes=['tw'])
            S_.op('act', lambda e: e.activation(out=lor[ln][:], in_=lo_[:, 2:4, :], func=AF.Identity), reads=[lk], writes=['lor%d' % ln])
            for d in range(2):
                for i in range(G // 128):
                    p, pk = pp.get()
                    S_.op('pe', lambda e: e.matmul(p[:], lhsT=tw[:, d, i * 128:(i + 1) * 128], rhs=wupr[:, d, :], start=True, stop=False), reads=['tw', 'wupr'], writes=[pk], acc=True)
                    S_.op('pe', lambda e: e.matmul(p[:], lhsT=ones1r[:], rhs=w0rr[:, d, :], start=False, stop=True), reads=['ones1r', 'w0rr'], writes=[pk], acc=True)
                    l_, lk2 = ld.get()
                    S_.op('act', lambda e: e.activation(out=l_[:], in_=p[:], func=AF.Sigmoid), reads=[pk], writes=[lk2])
                    S_.dma('pool', lambda e: e.dma_start(out=K.rwLD[d][t0 + i * 128:t0 + (i + 1) * 128, :], in_=l_[:]), reads=[lk2], writes=['rwLD%d' % d])
                    yield 'b'
                for c in range(4):
                    p, pk = pp.get()
                    S_.op('pe', lambda e: e.matmul(p[:, 0:G], lhsT=aupr[:, d, c * 128:(c + 1) * 128], rhs=lor[ln][:, d, :], start=True, stop=True), reads=['aupr', 'lor%d' % ln], writes=[pk])
                    S_.op('act', lambda e: e.activation(out=av[:, c, :], in_=p[:, 0:G], func=AF.Sigmoid, bias=a0v[:, d, c:c + 1], scale=1.0), reads=[pk, 'a0v'], writes=['av'])
                    S_.op('dve', lambda e: e.tensor_scalar(out=tt[:, c, :], in0=av[:, c, :], scalar1=kav[:, c:c + 1], scalar2=omka[:, c:c + 1], op0=ALU.mult, op1=ALU.add),
                         reads=['av', 'kav', 'omka'], writes=['tt'])
                S_.op('dve', lambda e: e.tensor_tensor(out=kd[d][:], in0=cv[:, 4:8, :], in1=tt[:], op=ALU.mult), reads=cvk + ['tt'], writes=['kd%d' % d])
                S_.op('dve', lambda e: e.tensor_tensor(out=bb[:], in0=kk_[:], in1=av[:], op=ALU.mult), reads=['kk_', 'av'], writes=['bb'])
                S_.dma('pool', lambda e: e.dma_start(out=K.rwKD[d][:, t0:t0 + G].rearrange("(c p) t -> p c t", p=128), in_=kd[d][:]), reads=['kd%d' % d], writes=['rwKD%d' % d])
                S_.dma('pool', lambda e: e.dma_start(out=K.rwB[d][:, t0:t0 + G].rearrange("(c p) t -> p c t", p=128), in_=bb[:]), reads=['bb'], writes=['rwB%d' % d])
                yield 'b'
            if lat:
                tl = t0 - TC
                S_.op('act', lambda e: e.activation(out=sg[:], in_=gd_[:], func=AF.Sigmoid), reads=[gk], writes=['sg'])
                for c in range(4):
                    p, pk = pp.get()
                    S_.op('pe', lambda e: e.matmul(p[:, 0:G], lhsT=gupr[:, c * 128:(c + 1) * 128], rhs=sg[:], start=True, stop=True), reads=['gupr', 'sg'], writes=[pk])
                    S_.op('act', lambda e: e.activation(out=gg[:, c, :], in_=p[:, 0:G], func=AF.Identity), reads=[pk], writes=['gg'])
                S_.dma('pool', lambda e: e.dma_start(out=K.rwG[:, tl:tl + G].rearrange("(c p) t -> p c t", p=128), in_=gg[:]), reads=['gg'], writes=['rwG'])
                yield 'b'
                S_.op('dve', lambda e: e.tensor_tensor(out=bc[:], in0=kd[0][:], in1=kd[1][:], op=ALU.add), reads=['kd0', 'kd1'], writes=['bc'])
                S_.op('dve', lambda e: e.tensor_tensor(out=bc[:], in0=bc[:], in1=cv[:, 0:4, :], op=ALU.mult), reads=['bc'] + cvr, writes=['bc'])
                for c in range(4):
                    S_.op('dve', lambda e: e.tensor_scalar_mul(out=bc[:, c, :], in0=bc[:, c, :], scalar1=rkv[:, c:c + 1]), reads=['bc', 'rkv'], writes=['bc'])
                for c2 in range(2):
                    p, pk = pp.get()
                    S_.op('pe', lambda e: e.matmul(p[:, 0:2 * G], lhsT=bonesr[:], rhs=bc[:, 2 * c2:2 * c2 + 2, :], start=True, stop=True), reads=['bonesr', 'bc'], writes=[pk])
                    S_.op('dve', lambda e: e.tensor_tensor(out=bon[:, 2 * c2:2 * c2 + 2, :], in0=p[:, 0:2 * G], in1=cv[:, 8 + 2 * c2:8 + 2 * c2 + 2, :], op=ALU.mult), reads=[pk] + cvv, writes=['bon'])
                S_.dma('pool', lambda e: e.dma_start(out=K.rwBON[:, tl:tl + G].rearrange("(c p) t -> p c t", p=128), in_=bon[:]), reads=['bon'], writes=['rwBON'])
        def step(gen_):
            try:
                return next(gen_)
            except StopIteration:
                return None
        prev = None
        for bi in range(nblk):
            gcur = bbody(bi)
            while True:
                r = step(gcur)
                if prev is not None and step(prev) is None:
                    prev = None
                if r == 'F' or r is None:
                    break
            while prev is not None:
                if step(prev) is None:
                    prev = None
            prev = gcur
        while prev is not None:
            if step(prev) is None:
                prev = None
        S.barrier()


def rw_consts():
    i = np.arange(128)[:, None]
    t = np.arange(128)[None, :]
    bd = (i // 64) == (t // 64)
    out = {}
    blk = ((i // 64) == (t // 64)).astype(np.float32)
    for d in range(2):
        strict = (bd & ((i < t) if d == 0 else (i > t))).astype(np.float32)
        incl = (bd & ((i <= t) if d == 0 else (i >= t))).astype(np.float32)
        m1 = np.concatenate([-strict, incl, blk], 1)
        m2 = np.concatenate([incl, blk], 1)
        m3 = np.concatenate([-strict.T, -strict.T, -np.ones((128, 64), np.float32)], 1)
        cum = np.float32(LDS) * np.concatenate([incl, strict, strict.T], 1)
        out['rwm%d' % d] = np.ascontiguousarray(np.concatenate([m1, m2, m3, cum], 1).astype(np.float32))
    out['i64dbl'] = np.ascontiguousarray((np.arange(128)[:, None] % 64 == np.arange(128)[None, :] % 64).astype(np.float32))
    out['bones'] = np.ascontiguousarray(bd.astype(np.float32))
    out['identf'] = np.eye(128, dtype=np.float32)
    return out


GN_EPS = 64e-5


def phase_rwscan(K, ntile_lat=64):
    nc, S = K.nc, K.S
    G = 128
    with ExitStack() as es:
        sb = lambda n, s, d: es.enter_context(nc.sbuf_tensor(_u(n), s, d))
        ps = lambda n, s, d: es.enter_context(nc.psum_tensor(_u(n), s, d))
        mk = sb("mk", [128, 1344], F32)
        idr = sb("idr", [128, 128], F32R)
        i64r = sb("i64r", [128, 128], F32R)
        i64f = sb("i64f", [128, 128], F32)
        idf = sb("idf", [128, 128], F32)
        o64 = sb("o64", [64, 64], F32)
        gng = sb("gng", [64, 8], F32)
        gnb = sb("gnb", [64, 8], F32)
        epsg = sb("epsg", [64, 1], F32)
        raw = [[sb("raw%d_%d" % (j, i), [128, 4, G], F32) for i in range(2)] for j in range(4)]
        vraw = [sb("vraw%d" % i, [128, 512], F32) for i in range(2)]
        ldt = [sb("ldt%d" % i, [128, 512], F32) for i in range(2)]
        vtr = [sb("vtr%d" % i, [128, 512], F32R) for i in range(2)]
        Ep = sb("Ep", [128, 4, G], F32)
        Em = sb("Em", [128, 4, G], F32)
        Ex = sb("Ex", [128, 4, G], F32)
        Ea = sb("Ea", [128, 4, G], F32)
        KRG = sb("KRG", [128, 4, 3, 128], F32R)
        BK = sb("BK", [128, 4, 2, 128], F32R)
        KB2 = sb("KB2", [128, 4, 2, 128], F32R)
        NS = 4
        La = [sb("La%d" % i, [128, 384], F32R) for i in range(NS)]
        Lb = [sb("Lb%d" % i, [128, 384], F32R) for i in range(NS)]
        ATa = [sb("ATa%d" % i, [128, 128], F32R) for i in range(NS)]
        ATb = [sb("ATb%d" % i, [128, 128], F32R) for i in range(NS)]
        X1 = [sb("X1%d" % i, [128, 320], F32R) for i in range(NS)]
        X2 = [sb("X2%d" % i, [128, 256], F32R) for i in range(NS)]
        Xf = [sb("Xf%d" % i, [128, 256], F32R) for i in range(NS)]
        QG1 = [sb("QG1_%d" % i, [64, 8, 256], F32R) for i in range(2)]
        QG2 = [sb("QG2_%d" % i, [128, 8, 256], F32R) for i in range(2)]
        Sth = sb("Sth", [64, 3, 8, 64], F32R)
        T = [sb("T%d" % i, [64, 8, G], F32) for i in range(4)]
        outb = sb("outb", [64, 8, G], BF16)
        pp = Rot([ps("sp%d" % i, [128, 512], F32) for i in range(7)], "sp")
        pst = ps("pst", [64, 512], F32)

        ld1 = lambda dst, src, key: S.dma('sp', lambda e: e.dma_start(out=dst, in_=src, allow_slow_non_contiguous=True), writes=[key])
        ld1(idf[:], K.identf, 'idf')
        ld1(i64f[:], K.i64dbl, 'i64f')
        ld1(gng[:], K.rwkv_gn_g.rearrange("o (h p) -> p (o h)", p=64), 'gng')
        ld1(gnb[:], K.rwkv_gn_b.rearrange("o (h p) -> p (o h)", p=64), 'gnb')
        S.op('dve', lambda e: e.tensor_copy(out=idr[:], in_=idf[:]), reads=['idf'], writes=['idr'])
        S.op('dve', lambda e: e.tensor_copy(out=i64r[:], in_=i64f[:]), reads=['i64f'], writes=['i64r'])
        S.op('dve', lambda e: e.memset(o64[:], 1.0 / 64.0), writes=['o64'])
        o64r = sb("o64r", [64, 64], F32R)
        S.op('dve', lambda e: e.tensor_copy(out=o64r[:], in_=o64[:]), reads=['o64'], writes=['o64r'])
        S.op('dve', lambda e: e.memset(epsg[:], GN_EPS), writes=['epsg'])
        zt = sb("zt", [64, 512], F32)
        S.op('dve', lambda e: e.memset(zt[:], 0.0), writes=['zt'])

        evq = [0]

        def evac(out_ap, in_ap, reads, writes):
            if evq[0] % 2 == 0:
                S.op('act', lambda e: e.activation(out=out_ap, in_=in_ap, func=AF.Identity), reads=reads, writes=writes)
            else:
                S.op('dve', lambda e: e.tensor_copy(out=out_ap, in_=in_ap), reads=reads, writes=writes)
            evq[0] += 1

        fl = lambda a: a[:, :, :].rearrange("p h t -> p (h t)")
        bcount = 0
        for d in range(2):
            m1 = mk[:, 0:384]
            m2 = mk[:, 384:640]
            m3 = mk[:, 640:960]
            cum = mk[:, 960:1344]
            S.dma('sp', lambda e: e.dma_start(out=mk[:], in_=K.rwm[d]), writes=['mk'])
            S.op('dve', lambda e: e.tensor_copy(out=Sth[:, 0].rearrange("p h v -> p (h v)"), in_=zt[:]), reads=['zt'], writes=['Sth0'])
            if d == 0:
                blocks = [0, 1] + list(range(2, 2 + ntile_lat))
            else:
                blocks = [1, 0] + list(range(1 + ntile_lat, 1, -1))
            chunks = [0, 1] if d == 0 else [1, 0]
            def body(b, qs):
                nonlocal bcount
                t0 = b * G
                lat = b >= 2
                sl = bcount % 2
                bcount += 1
                srcs = [K.rwR, K.rwKD[d], K.rwKK, K.rwB[d]]
                rk = ['raw%d_%d' % (j, sl) for j in range(4)]
                for j in range(4):
                    S.dma('sp', lambda e: e.dma_start(out=raw[j][sl][:], in_=srcs[j][:, t0:t0 + G].rearrange("(c p) t -> p c t", p=128)), writes=[rk[j]])
                S.dma('sp', lambda e: e.dma_start(out=vraw[sl][:], in_=K.rwVtok[t0:t0 + G, :]), writes=['vraw%d' % sl])
                S.dma('sp', lambda e: e.dma_start(out=ldt[sl][:], in_=K.rwLD[d][t0:t0 + G, :]), writes=['ldt%d' % sl])
                r_, kd_, kk_, b_ = [raw[j][sl] for j in range(4)]
                S.op('act', lambda e: e.activation(out=vtr[qs][:], in_=vraw[sl][:], func=AF.Identity), reads=['vraw%d' % sl], writes=['vtr%d' % qs])
                banks = [pp.get() for _ in range(3)]
                for c in range(4):
                    for q in range(3):
                        S.op('pe', lambda e: e.matmul(banks[q][0][:, c * 128:(c + 1) * 128], lhsT=ldt[sl][:, c * 128:(c + 1) * 128], rhs=cum[:, q * 128:(q + 1) * 128], start=True, stop=True),
                             reads=['ldt%d' % sl, 'mk'], writes=[banks[q][1]], acc=True)
                v3 = lambda bank: bank[:, :].rearrange("p (c t) -> p c t", c=4)
                S.op('act', lambda e: e.activation(out=Ep[:], in_=v3(banks[0][0]), func=AF.Exp), reads=[banks[0][1]], writes=['Ep'])
                S.op('act', lambda e: e.activation(out=Em[:], in_=v3(banks[0][0]), func=AF.Exp, scale=-1.0), reads=[banks[0][1]], writes=['Em'])
                S.op('act', lambda e: e.activation(out=Ex[:], in_=v3(banks[1][0]), func=AF.Exp), reads=[banks[1][1]], writes=['Ex'])
                S.op('act', lambda e: e.activation(out=Ea[:], in_=v3(banks[2][0]), func=AF.Exp), reads=[banks[2][1]], writes=['Ea'])
                yield 'f'
                S.op('dve', lambda e: e.tensor_tensor(out=KRG[:, :, 0, :], in0=kk_[:], in1=Ex[:], op=ALU.mult), reads=[rk[2], 'Ex'], writes=['KRG'])
                S.op('dve', lambda e: e.tensor_tensor(out=KRG[:, :, 1, :], in0=r_[:], in1=Ep[:], op=ALU.mult), reads=[rk[0], 'Ep'], writes=['KRG'])
                S.op('dve', lambda e: e.tensor_tensor(out=BK[:, :, 0, :], in0=b_[:], in1=Em[:], op=ALU.mult), reads=[rk[3], 'Em'], writes=['BK'])
                S.op('dve', lambda e: e.tensor_tensor(out=BK[:, :, 1, :], in0=kd_[:], in1=Em[:], op=ALU.mult), reads=[rk[1], 'Em'], writes=['BK'])
                S.op('dve', lambda e: e.tensor_tensor(out=KB2[:, :, 0, :], in0=kd_[:], in1=Ea[:], op=ALU.mult), reads=[rk[1], 'Ea'], writes=['KB2'])
                S.op('dve', lambda e: e.tensor_tensor(out=KB2[:, :, 1, :], in0=b_[:], in1=Ea[:], op=ALU.mult), reads=[rk[3], 'Ea'], writes=['KB2'])
                for cc in range(2):
                    pos = cc * 64 + (63 if d == 0 else 0)
                    in0 = i64f[:, cc * 64:(cc + 1) * 64].unsqueeze(1).to_broadcast([128, 4, 64])
                    in1 = Ep[:, :, pos:pos + 1].to_broadcast([128, 4, 64])
                    S.op('dve', lambda e: e.tensor_tensor(out=KRG[:, :, 2, cc * 64:(cc + 1) * 64], in0=in0, in1=in1, op=ALU.mult), reads=['i64f', 'Ep'], writes=['KRG'])

                for g0 in range(0, 8, NS):
                    grp = list(range(g0, g0 + NS))
                    cur = {}
                    for s_, h in enumerate(grp):
                        c, pb = h // 2, 64 * (h % 2)
                        fm = lambda arr, k0, k1: arr[pb:pb + 64, c, k0:k1, :].rearrange("p k t -> p (k t)")
                        p1, k1 = pp.get()
                        S.op('pe', lambda e: e.matmul(p1[:, 0:256], lhsT=fm(BK, 0, 1), rhs=fm(KRG, 0, 2), start=True, stop=True), reads=['BK', 'KRG'], writes=[k1], acc=True)
                        S.op('pe', lambda e: e.matmul(p1[:, 256:384], lhsT=fm(KB2, 1, 2), rhs=i64r[pb:pb + 64, :], start=True, stop=True), reads=['KB2', 'i64r'], writes=[k1], acc=True)
                        S.op('dve', lambda e: e.tensor_tensor(out=La[s_][:], in0=p1[:, 0:384], in1=m1, op=ALU.mult), reads=[k1, 'mk'], writes=['La%d' % s_])
                        p2, k2 = pp.get()
                        S.op('pe', lambda e: e.matmul(p2[:, 0:128], lhsT=fm(BK, 1, 2), rhs=fm(KRG, 1, 2), start=True, stop=True), reads=['BK', 'KRG'], writes=[k2], acc=True)
                        S.op('pe', lambda e: e.matmul(p2[:, 128:256], lhsT=fm(KB2, 0, 1), rhs=i64r[pb:pb + 64, :], start=True, stop=True), reads=['KB2', 'i64r'], writes=[k2], acc=True)
                        S.op('dve', lambda e: e.tensor_tensor(out=X2[s_][:], in0=p2[:, 0:256], in1=m2, op=ALU.mult), reads=[k2, 'mk'], writes=['X2%d' % s_])
                        p3, k3 = pp.get()
                        S.op('pe', lambda e: e.matmul(p3[:, 0:256], lhsT=fm(KRG, 0, 1), rhs=fm(BK, 0, 2), start=True, stop=True), reads=['BK', 'KRG'], writes=[k3], acc=True)
                        S.op('pe', lambda e: e.matmul(p3[:, 256:320], lhsT=fm(KRG, 0, 1), rhs=i64r[pb:pb + 64, 0:64], start=True, stop=True), reads=['KRG', 'i64r'], writes=[k3], acc=True)
                        S.op('dve', lambda e: e.tensor_tensor(out=X1[s_][:], in0=p3[:, 0:320], in1=m3, op=ALU.mult), reads=[k3, 'mk'], writes=['X1%d' % s_])
                        cur[s_] = (La[s_], 'La%d' % s_, X1[s_][:, 0:128], 'X1%d' % s_)
                        yield 'f'
                    for lev in range(5):
                        pend = []
                        nxt = {}
                        for s_ in range(NS):
                            L, Lk, AT, ATk = cur[s_]
                            p, pk = pp.get()
                            S.op('pe', lambda e: e.matmul(p[:, 0:384], lhsT=AT, rhs=L[:, 0:384], start=True, stop=False), reads=[Lk, ATk], writes=[pk], acc=True)
                            S.op('pe', lambda e: e.matmul(p[:, 128:384], lhsT=idr[:], rhs=L[:, 128:384], start=False, stop=True), reads=[Lk, 'idr'], writes=[pk], acc=True)
                            pend.append((p, pk))
                        yield 'f'
                        for s_ in range(NS):
                            p, pk = pend[s_]
                            Ln_ = Lb[s_] if lev % 2 == 0 else La[s_]
                            Lnk = ('Lb%d' if lev % 2 == 0 else 'La%d') % s_
                            evac(Ln_[:], p[:, 0:384], [pk], [Lnk])
                            nxt[s_] = (Ln_, Lnk)
                        for s_ in range(NS):
                            Ln_, Lnk = nxt[s_]
                            p, pk = pend[s_]
                            S.op('pe', lambda e: e.transpose(out=p[:, 384:512], in_=Ln_[:, 0:128].bitcast(F32), identity=idf[:]), reads=[Lnk, 'idf'], writes=[pk])
                        yield 'f'
                        for s_ in range(NS):
                            Ln_, Lnk = nxt[s_]
                            p, pk = pend[s_]
                            ATn = ATa[s_] if lev % 2 == 0 else ATb[s_]
                            ATnk = ('ATa%d' if lev % 2 == 0 else 'ATb%d') % s_
                            evac(ATn[:], p[:, 384:512], [pk], [ATnk])
                            cur[s_] = (Ln_, Lnk, ATn[:], ATnk)
                    for s_, h in enumerate(grp):
                        L, Lk, AT, ATk = cur[s_]
                        p, pk = pp.get()
                        S.op('pe', lambda e: e.matmul(p[:, 0:256], lhsT=AT, rhs=L[:, 128:384], start=True, stop=False), reads=[Lk, ATk], writes=[pk], acc=True)
                        S.op('pe', lambda e: e.matmul(p[:, 0:256], lhsT=idr[:], rhs=L[:, 128:384], start=False, stop=True), reads=[Lk, 'idr'], writes=[pk], acc=True)
                        evac(Xf[s_][:], p[:, 0:256], [pk], ['Xf%d' % s_])
                    yield 'f'
                    for s_, h in enumerate(grp):
                        c, pb = h // 2, 64 * (h % 2)
                        p, pk = pp.get()
                        S.op('pe', lambda e: e.matmul(p[:, 0:256], lhsT=idr[:], rhs=X2[s_][:], start=True, stop=False), reads=['X2%d' % s_, 'idr'], writes=[pk], acc=True)
                        S.op('pe', lambda e: e.matmul(p[:, 0:256], lhsT=X1[s_][:, 128:256], rhs=Xf[s_][:], start=False, stop=True), reads=['X1%d' % s_, 'Xf%d' % s_], writes=[pk], acc=True)
                        evac(QG2[qs][:, h, :], p[:, 0:256], [pk], ['QG2_%d_%d' % (qs, h)])
                        q, qk = pp.get()
                        rg = KRG[pb:pb + 64, c, 1:3, :].rearrange("p k t -> p (k t)")
                        S.op('pe', lambda e: e.matmul(q[0:64, 0:256], lhsT=idr[pb:pb + 64, pb:pb + 64], rhs=rg, start=True, stop=False), reads=['KRG', 'idr'], writes=[qk], acc=True)
                        S.op('pe', lambda e: e.matmul(q[0:64, 0:256], lhsT=X1[s_][:, 256:320], rhs=Xf[s_][:], start=False, stop=True), reads=['X1%d' % s_, 'Xf%d' % s_], writes=[qk], acc=True)
                        evac(QG1[qs][:, h, :], q[0:64, 0:256], [qk], ['QG1_%d_%d' % (qs, h)])
                        yield 'f'
                yield 'F'
                for s, cc in enumerate(chunks):
                    for h in range(8):
                        S.op('pe', lambda e: e.matmul(pst[:, h * 64:(h + 1) * 64], lhsT=QG1[qs][:, h, 128 + cc * 64:128 + (cc + 1) * 64], rhs=Sth[:, s, h, :], start=True, stop=False),
                             reads=['QG1_%d_%d' % (qs, h), 'Sth%d' % s], writes=['pst'], acc=True)
                        S.op('pe', lambda e: e.matmul(pst[:, h * 64:(h + 1) * 64], lhsT=QG2[qs][:, h, 128 + cc * 64:128 + (cc + 1) * 64], rhs=vtr[qs][:, h * 64:(h + 1) * 64], start=False, stop=True),
                             reads=['QG2_%d_%d' % (qs, h), 'vtr%d' % qs], writes=['pst'], acc=True)
                    evac(Sth[:, s + 1].rearrange("p h v -> p (h v)"), pst[:, :], ['pst'], ['Sth%d' % (s + 1)])
                    yield 'b'
                if lat:
                    ybuf = T[0]
                    for hq in range(2):
                        bank = pp.get()
                        for h4 in range(4):
                            h = hq * 4 + h4
                            S.op('pe', lambda e: e.matmul(bank[0][0:64, h4 * 128:(h4 + 1) * 128], lhsT=vtr[qs][:, h * 64:(h + 1) * 64], rhs=QG2[qs][:, h, 0:128], start=True, stop=False),
                                 reads=['QG2_%d_%d' % (qs, h), 'vtr%d' % qs], writes=[bank[1]], acc=True)
                            for s, cc in enumerate(chunks):
                                S.op('pe', lambda e: e.matmul(bank[0][0:64, h4 * 128 + cc * 64:h4 * 128 + (cc + 1) * 64], lhsT=Sth[:, s, h, :], rhs=QG1[qs][:, h, cc * 64:(cc + 1) * 64], start=False, stop=(s == 1)),
                                     reads=['QG1_%d_%d' % (qs, h), 'Sth%d' % s], writes=[bank[1]], acc=True)
                        evac(ybuf[:, hq * 4:hq * 4 + 4, :], bank[0][0:64, :].rearrange("p (h t) -> p h t", h=4), [bank[1]], ['T0'])
                        yield 'b'
                    tl = t0 - TC
                    if d == 0:
                        S.dma('pool', lambda e: e.dma_start(out=K.rwY0[:, tl:tl + G].rearrange("(h p) t -> p h t", p=64), in_=ybuf[:]), reads=['T0'], writes=['rwY0'])
                    else:
                        y0b, cen, sqb = T[1], T[2], T[3]
                        S.dma('sp', lambda e: e.dma_start(out=y0b[:], in_=K.rwY0[:, tl:tl + G].rearrange("(h p) t -> p h t", p=64)), reads=['rwY0'], writes=['T1'])
                        S.op('dve', lambda e: e.tensor_tensor(out=ybuf[:], in0=ybuf[:], in1=y0b[:], op=ALU.add), reads=['T0', 'T1'], writes=['T0'])
                        for q in range(2):
                            p, pk = pp.get()
                            S.op('pe', lambda e: e.matmul(p[0:64, :], lhsT=o64[:], rhs=fl(ybuf)[:, q * 512:(q + 1) * 512], start=True, stop=True), reads=['o64', 'T0'], writes=[pk])
                            S.op('dve', lambda e: e.tensor_tensor(out=fl(cen)[:, q * 512:(q + 1) * 512], in0=fl(ybuf)[:, q * 512:(q + 1) * 512], in1=p[0:64, :], op=ALU.subtract), reads=[pk, 'T0'], writes=['T2'])
                        S.op('act', lambda e: e.activation(out=sqb[:].bitcast(F32R), in_=cen[:], func=AF.Square), reads=['T2'], writes=['T3'])
                        yield 'b'
                        rsb = T[1]
                        for q in range(2):
                            p, pk = pp.get()
                            S.op('pe', lambda e: e.matmul(p[0:64, :], lhsT=o64r[:], rhs=fl(sqb)[:, q * 512:(q + 1) * 512].bitcast(F32R), start=True, stop=True), reads=['o64r', 'T3'], writes=[pk])
                            S.op('act', lambda e: e.activation(out=fl(rsb)[:, q * 512:(q + 1) * 512], in_=p[0:64, :], func=AF.Ln, bias=epsg[:], scale=1.0), reads=[pk, 'epsg'], writes=['T1'])
                        S.op('act', lambda e: e.activation(out=rsb[:], in_=rsb[:], func=AF.Exp, scale=-0.5), reads=['T1'], writes=['T1'])
                        yield 'b'
                        bonb, ggb = T[3], T[0]
                        S.dma('sp', lambda e: e.dma_start(out=bonb[:], in_=K.rwBON[:, tl:tl + G].rearrange("(h p) t -> p h t", p=64)), reads=['rwBON'], writes=['T3'])
                        S.op('dve', lambda e: e.tensor_tensor(out=cen[:], in0=cen[:], in1=rsb[:], op=ALU.mult), reads=['T2', 'T1'], writes=['T2'])
                        S.dma('sp', lambda e: e.dma_start(out=ggb[:], in_=K.rwG[:, tl:tl + G].rearrange("(h p) t -> p h t", p=64)), reads=['rwG'], writes=['T0'])
                        S.op('dve', lambda e: e.tensor_tensor(out=cen[:], in0=cen[:], in1=gng[:, :].unsqueeze(2).to_broadcast([64, 8, G]), op=ALU.mult), reads=['T2', 'gng'], writes=['T2'])
                        S.op('dve', lambda e: e.tensor_tensor(out=cen[:], in0=cen[:], in1=gnb[:, :].unsqueeze(2).to_broadcast([64, 8, G]), op=ALU.add), reads=['T2', 'gnb'], writes=['T2'])
                        S.op('dve', lambda e: e.tensor_tensor(out=cen[:], in0=cen[:], in1=bonb[:], op=ALU.add), reads=['T2', 'T3'], writes=['T2'])
                        S.op('dve', lambda e: e.tensor_tensor(out=outb[:], in0=cen[:], in1=ggb[:], op=ALU.mult), reads=['T2', 'T0'], writes=['outb'])
                        S.dma('pool', lambda e: e.dma_start(out=K.mixT[0:512, tl:tl + G].rearrange("(h p) t -> p h t", p=64), in_=outb[:]), reads=['outb'], writes=['mixT'])
                S.op('dve', lambda e: e.tensor_copy(out=Sth[:, 0], in_=Sth[:, 2]), reads=['Sth2'], writes=['Sth0'])
            def step(gen_):
                try:
                    return next(gen_)
                except StopIteration:
                    return None
            prev = None
            qslot = 0
            for b in blocks:
                gcur = body(b, qslot)
                while True:
                    r = step(gcur)
                    if prev is not None and step(prev) is None:
                        prev = None
                    if r == 'F' or r is None:
                        break
                while prev is not None:
                    if step(prev) is None:
                        prev = None
                prev = gcur
                qslot ^= 1
            while prev is not None:
                if step(prev) is None:
                    prev = None
            S.barrier()


ALPHA = 2.0 ** 0.25
ROWW = 1088
BIGPOS = 4096.0


def phase_mix(K):
    nc, S = K.nc, K.S
    NT = TL // 128
    with ExitStack() as es:
        sb = lambda n, s, d: es.enter_context(nc.sbuf_tensor(_u(n), s, d))
        ps = lambda n, s, d: es.enter_context(nc.psum_tensor(_u(n), s, d))
        wo = sb("wo", [128, 8, 1024], BF16)
        wst = [sb("owst%d" % i, [128, 8, 256], F32) for i in range(2)]
        bc = {n: sb("bc_" + n, [128, 1024], F32) for n in ('g1', 'ln1g', 'ln1b', 'sc2', 'sh2')}
        rt = sb("rt", [128, 8, 16], F32)
        idf = sb("idf", [128, 128], F32)
        epst = sb("epst", [128, 1], F32)
        onesf = sb("onesf", [128, 128], F32)
        onesb = sb("onesb", [128, 128], BF16)
        ustr = sb("ustr", [128, 128], BF16)
        mt = [sb("mt%d" % i, [128, 8, 128], BF16) for i in range(2)]
        xt = [sb("xt%d" % i, [128, 1024], F32) for i in range(2)]
        t1 = sb("t1", [128, 1024], F32)
        pre = sb("pre", [128, 1024], F32)
        x1 = [sb("x1_%d" % i, [128, 1024], F32) for i in range(2)]
        uf = [sb("uf%d" % i, [128, 1024], F32) for i in range(2)]
        urow = [sb("urow%d" % i, [128, ROWW], BF16) for i in range(2)]
        uT = sb("uT", [128, 8, 128], F32)
        st = sb("st", [128, 2, 6], F32)
        mv = sb("mv", [128, 2], F32)
        lnv = sb("lnv", [128, 1], F32)
        rstd = sb("rstd", [128, 1], F32)
        lg = sb("lg", [128, 16], F32)
        mx = sb("mx", [128, 1], F32)
        sm = sb("sm", [128, 1], F32)
        affall = sb("affall", [128, NT, 16], F32)
        tok = sb("tok", [128, 1], I32)
        lo = sb("lo", [128, 16], F32)
        mid = sb("mid", [128, 16], F32)
        ge = sb("ge", [128, 16], F32)
        cntp = sb("cntp", [128, 16], F32)
        mskt = sb("mskt", [128, NT, 16], F32)
        mskb = sb("mskb", [128, NT, 16], BF16)
        csT = sb("csT", [128, 16, NT], F32)
        incT = sb("incT", [128, 16, NT], F32)
        rmask = sb("rmask", [128, 16, NT], F32)
        posf = sb("posf", [128, NT, 16], F32)
        posi = sb("posi", [128, NT, 16], I32)
        zt = sb("zt", [128, 1024], F32)
        pm = [ps("pm%d" % i, [128, 1024], F32) for i in range(2)]
        ptr = ps("ptr", [128, 1024], F32)
        psm = ps("psm", [128, 512], F32)
        psn = ps("psn", [128, 512], F32)

        ld1 = lambda dst, src, key, rd=(): S.dma('sp', lambda e: e.dma_start(out=dst, in_=src, allow_slow_non_contiguous=True), reads=list(rd), writes=[key])
        ld1(idf[:], K.identf, 'idf')
        ld1(ustr[:], K.ustrict, 'ustr')
        ld1(rt[:], K.router.rearrange("(k p) e -> p k e", p=128), 'rt')
        ld1(bc['g1'][:], K.modd[0:1, 2048:3072].partition_broadcast(128), 'bc_g1', ['modd'])
        ld1(bc['sh2'][:], K.modd[0:1, 3072:4096].partition_broadcast(128), 'bc_sh2', ['modd'])
        ld1(bc['sc2'][:], K.modd[0:1, 4096:5120].partition_broadcast(128), 'bc_sc2', ['modd'])
        ld1(bc['ln1g'][:], K.ln1_g.partition_broadcast(128), 'bc_ln1g')
        ld1(bc['ln1b'][:], K.ln1_b.partition_broadcast(128), 'bc_ln1b')
        S.op('dve', lambda e: e.tensor_scalar_add(out=bc['sc2'][:], in0=bc['sc2'][:], scalar1=1.0), reads=['bc_sc2'], writes=['bc_sc2'])
        S.op('dve', lambda e: e.memset(epst[:], 1e-5), writes=['epst'])
        S.op('dve', lambda e: e.memset(onesf[:], 1.0), writes=['onesf'])
        S.op('dve', lambda e: e.memset(onesb[:], 1.0), writes=['onesb'])
        S.op('dve', lambda e: e.memset(zt[:], 0.0), writes=['zt'])
        for j in range(4):
            w = wst[j % 2]
            wk = 'owst%d' % (j % 2)
            S.dma('sp', lambda e: e.dma_start(out=w[:], in_=K.w_out[:, j * 256:(j + 1) * 256].rearrange("(k p) n -> p k n", p=128)), writes=[wk])
            S.op('pool', lambda e: e.tensor_copy(out=wo[:, :, j * 256:(j + 1) * 256], in_=w[:]), reads=[wk], writes=['wo'])
        for i in range(NT):
            S.dma('pool', lambda e: e.dma_start(out=K.yacc[i * 128:(i + 1) * 128, :], in_=zt[:]), reads=['zt'], writes=['yacc%d' % i])

        mvs = [mv, sb("mv1", [128, 2], F32)]
        rstds = [rstd, sb("rstd1", [128, 1], F32)]
        sts = [st, sb("st1", [128, 2, 6], F32)]
        lnvs = [lnv, sb("lnv1", [128, 1], F32)]

        nmr = [sb("nmr%d" % i_, [128, 1], F32) for i_ in range(2)]

        def ln_stats(src, srck, lane=0):
            sfx = '' if lane == 0 else '1'
            for c in range(2):
                S.op('dve', lambda e: e.bn_stats(out=sts[lane][:, c, :], in_=src[:, c * 512:(c + 1) * 512]), reads=[srck], writes=['st%d%s' % (c, sfx)])
            S.op('dve', lambda e: e.bn_aggr(out=mvs[lane][:], in_=sts[lane][:]), reads=['st0' + sfx, 'st1' + sfx], writes=['mv' + sfx])
            S.op('act', lambda e: e.activation(out=lnvs[lane][:], in_=mvs[lane][:, 1:2], func=AF.Ln, bias=epst[:], scale=1.0), reads=['mv' + sfx, 'epst'], writes=['lnv' + sfx])
            S.op('act', lambda e: e.activation(out=rstds[lane][:], in_=lnvs[lane][:], func=AF.Exp, scale=-0.5), reads=['lnv' + sfx], writes=['rstd' + sfx])

        def tbody(i):
            sl = i % 2
            m_, mk_ = mt[sl], 'mt%d' % sl
            x_, xk = xt[sl], 'xt%d' % sl
            S.dma('sp', lambda e: e.dma_start(out=m_[:], in_=K.mixT[:, i * 128:(i + 1) * 128].rearrange("(k p) t -> p k t", p=128)), reads=['mixT'], writes=[mk_])
            S.dma('sp', lambda e: e.dma_start(out=x_[:], in_=K.xin[TC + i * 128:TC + (i + 1) * 128, :]), writes=[xk])
            p_, pk_ = pm[sl], 'pm%d' % sl
            for half in range(2):
                for kc in range(8):
                    S.op('pe', lambda e: e.matmul(p_[:, half * 512:(half + 1) * 512], lhsT=m_[:, kc, :], rhs=wo[:, kc, half * 512:(half + 1) * 512], start=(kc == 0), stop=(kc == 7)),
                         reads=[mk_, 'wo'], writes=[pk_], acc=True)
            yield 'f'
            S.op('dve', lambda e: e.tensor_tensor(out=t1[:], in0=p_[:], in1=bc['g1'][:], op=ALU.mult), reads=[pk_, 'bc_g1'], writes=['t1'])
            S.op('dve', lambda e: e.scalar_tensor_tensor(out=pre[:], in0=x_[:], scalar=ALPHA, in1=t1[:], op0=ALU.mult, op1=ALU.add), reads=[xk, 't1'], writes=['pre'])
            ln_stats(pre, 'pre')
            yield 'f'
            x1_, x1k = x1[sl], 'x1_%d' % sl
            S.op('dve', lambda e: e.scalar_tensor_tensor(out=nmr[0][:], in0=mv[:, 0:1], scalar=-1.0, in1=rstd[:], op0=ALU.mult, op1=ALU.mult), reads=['mv', 'rstd'], writes=['nmr0'])
            S.op('act', lambda e: e.activation(out=t1[:], in_=pre[:], func=AF.Identity, bias=nmr[0][:], scale=rstd[:]), reads=['pre', 'nmr0', 'rstd'], writes=['t1'])
            S.op('dve', lambda e: e.tensor_tensor(out=t1[:], in0=t1[:], in1=bc['ln1g'][:], op=ALU.mult), reads=['t1', 'bc_ln1g'], writes=['t1'])
            S.op('dve', lambda e: e.tensor_tensor(out=x1_[:], in0=t1[:], in1=bc['ln1b'][:], op=ALU.add), reads=['t1', 'bc_ln1b'], writes=[x1k])
            S.dma('pool', lambda e: e.dma_start(out=K.x1d[i * 128:(i + 1) * 128, :], in_=x1_[:]), reads=[x1k], writes=['x1d'])
            yield 'f'
            ln_stats(x1_, x1k, 1)
            yield 'f'
            S.op('dve', lambda e: e.scalar_tensor_tensor(out=nmr[1][:], in0=mvs[1][:, 0:1], scalar=-1.0, in1=rstds[1][:], op0=ALU.mult, op1=ALU.mult), reads=['mv1', 'rstd1'], writes=['nmr1'])
            S.op('act', lambda e: e.activation(out=pre[:], in_=x1_[:], func=AF.Identity, bias=nmr[1][:], scale=rstds[1][:]), reads=[x1k, 'nmr1', 'rstd1'], writes=['pre'])
            S.op('dve', lambda e: e.tensor_tensor(out=pre[:], in0=pre[:], in1=bc['sc2'][:], op=ALU.mult), reads=['pre', 'bc_sc2'], writes=['pre'])
            S.op('dve', lambda e: e.tensor_tensor(out=uf[sl][:], in0=pre[:], in1=bc['sh2'][:], op=ALU.add), reads=['pre', 'bc_sh2'], writes=['uf%d' % sl])
            ur, urk = urow[sl], 'urow%d' % sl
            S.op('act', lambda e: e.activation(out=ur[:, 0:1024], in_=uf[sl][:], func=AF.Identity), reads=['uf%d' % sl], writes=[urk])
            yield 'F'
            for k in range(8):
                S.op('pe', lambda e: e.transpose(out=ptr[:, k * 128:(k + 1) * 128], in_=uf[sl][:, k * 128:(k + 1) * 128], identity=idf[:]), reads=['uf%d' % sl, 'idf'], writes=['ptr'], acc=True)
            S.op('act', lambda e: e.activation(out=uT[:].rearrange("p k t -> p (k t)"), in_=ptr[:], func=AF.Identity), reads=['ptr'], writes=['uT'])
            yield 'b'
            for k in range(8):
                S.op('pe', lambda e: e.matmul(psm[:, 0:16], lhsT=uT[:, k, :], rhs=rt[:, k, :], start=(k == 0), stop=(k == 7)), reads=['uT', 'rt'], writes=['psm'], acc=True)
            S.op('dve', lambda e: e.reduce_max(out=mx[:], in_=psm[:, 0:16], axis=AX.X), reads=['psm'], writes=['mx'])
            S.op('dve', lambda e: e.tensor_scalar_mul(out=mx[:], in0=mx[:], scalar1=-1.0), reads=['mx'], writes=['mx'])
            yield 'b'
            S.op('act', lambda e: e.activation(out=lg[:], in_=psm[:, 0:16], func=AF.Exp, bias=mx[:], scale=1.0, accum_out=sm[:]), reads=['psm', 'mx'], writes=['lg', 'sm'])
            S.op('dve', lambda e: e.reciprocal(out=sm[:], in_=sm[:]), reads=['sm'], writes=['sm'])
            yield 'b'
            S.op('dve', lambda e: e.tensor_scalar_mul(out=affall[:, i, :], in0=lg[:], scalar1=sm[:]), reads=['lg', 'sm'], writes=['affall'])
            S.op('dve', lambda e: e.tensor_copy(out=ur[:, 1024:1056].bitcast(F32), in_=affall[:, i, :]), reads=['affall'], writes=[urk])
            S.op('pool', lambda e: e.iota(tok[:], pattern=[[0, 1]], base=i * 128, channel_multiplier=1), writes=['tok'])
            S.op('pool', lambda e: e.tensor_copy(out=ur[:, 1056:1058].bitcast(I32), in_=tok[:]), reads=['tok'], writes=[urk])
            S.dma('pool', lambda e: e.dma_start(out=K.urd[i * 128:(i + 1) * 128, 0:1058], in_=ur[:, 0:1058]), reads=[urk], writes=['urd%d' % i])

        def step(gen_):
            try:
                return next(gen_)
            except StopIteration:
                return None
        prev = None
        for i in range(NT):
            gcur = tbody(i)
            while True:
                r = step(gcur)
                if prev is not None and step(prev) is None:
                    prev = None
                if r == 'F' or r is None:
                    break
            while prev is not None:
                if step(prev) is None:
                    prev = None
            prev = gcur
        while prev is not None:
            if step(prev) is None:
                prev = None

        S.op('dve', lambda e: e.memset(lo[:], 0.0), writes=['lo'])
        for k in range(30):
            hk = 2.0 ** -(k + 1)
            S.op('dve', lambda e: e.tensor_scalar_add(out=mid[:], in0=lo[:], scalar1=hk), reads=['lo'], writes=['mid'])
            S.op('dve', lambda e: e.tensor_tensor(out=mskt[:], in0=affall[:], in1=mid[:, :].unsqueeze(1).to_broadcast([128, NT, 16]), op=ALU.is_ge), reads=['affall', 'mid'], writes=['mskt'])
            S.op('dve', lambda e: e.tensor_reduce(out=cntp[:], in_=mskt[:].rearrange("p i e -> p e i"), axis=AX.X, op=ALU.add), reads=['mskt'], writes=['cntp'])
            S.op('pe', lambda e: e.matmul(psn[:, 0:16], lhsT=onesf[:], rhs=cntp[:], start=True, stop=True), reads=['onesf', 'cntp'], writes=['psn'])
            S.op('dve', lambda e: e.tensor_scalar(out=ge[:], in0=psn[:, 0:16], scalar1=float(CAP) - 0.5, scalar2=hk, op0=ALU.is_ge, op1=ALU.mult), reads=['psn'], writes=['ge'])
            S.op('dve', lambda e: e.tensor_tensor(out=lo[:], in0=lo[:], in1=ge[:], op=ALU.add), reads=['lo', 'ge'], writes=['lo'])
        S.op('dve', lambda e: e.tensor_tensor(out=mskt[:], in0=affall[:], in1=lo[:, :].unsqueeze(1).to_broadcast([128, NT, 16]), op=ALU.is_ge), reads=['affall', 'lo'], writes=['mskt'])
        S.op('act', lambda e: e.activation(out=mskb[:], in_=mskt[:], func=AF.Identity), reads=['mskt'], writes=['mskb'])
        mflat = mskb[:].rearrange("p i e -> p (i e)")
        for hh in range(2):
            S.op('pe', lambda e: e.matmul(psm[:, :], lhsT=onesb[:], rhs=mflat[:, hh * 512:(hh + 1) * 512], start=True, stop=True), reads=['onesb', 'mskb'], writes=['psm'])
            S.op('dve', lambda e: e.tensor_copy(out=csT[:, :, hh * 32:(hh + 1) * 32], in_=psm[:, :].rearrange("p (i e) -> p e i", e=16)), reads=['psm'], writes=['csT'])
        S.op('dve', lambda e: e.memset(rmask[:], 1.0), writes=['rmask'])
        S.op('dve', lambda e: e.memset(rmask[:, :, 0:1], 0.0), writes=['rmask'])
        S.op('dve', lambda e: e.tensor_tensor_scan(out=incT[:].rearrange("p e i -> p (e i)"), data0=rmask[:].rearrange("p e i -> p (e i)"), data1=csT[:].rearrange("p e i -> p (e i)"),
                                                   initial=0.0, op0=ALU.mult, op1=ALU.add), reads=['rmask', 'csT'], writes=['incT'])
        S.op('dve', lambda e: e.tensor_tensor(out=incT[:], in0=incT[:], in1=csT[:], op=ALU.subtract), reads=['incT', 'csT'], writes=['incT'])
        for hh in range(2):
            S.op('pe', lambda e: e.matmul(psm[:, :], lhsT=ustr[:], rhs=mflat[:, hh * 512:(hh + 1) * 512], start=True, stop=True), reads=['ustr', 'mskb'], writes=['psm'])
            S.op('dve', lambda e: e.tensor_tensor(out=posf[:, hh * 32:(hh + 1) * 32, :], in0=psm[:, :].rearrange("p (i e) -> p i e", e=16),
                                                  in1=incT[:, :, hh * 32:(hh + 1) * 32].rearrange("p e i -> p i e"), op=ALU.add), reads=['psm', 'incT'], writes=['posf'])
        S.op('dve', lambda e: e.scalar_tensor_tensor(out=posf[:], in0=posf[:], scalar=-BIGPOS, in1=mskt[:], op0=ALU.add, op1=ALU.mult), reads=['posf', 'mskt'], writes=['posf'])
        S.op('dve', lambda e: e.tensor_scalar_add(out=posf[:], in0=posf[:], scalar1=BIGPOS), reads=['posf'], writes=['posf'])
        S.op('dve', lambda e: e.tensor_copy(out=posi[:], in_=posf[:]), reads=['posf'], writes=['posi'])
        if K.dbg_pos is not None:
            S.dma('sp', lambda e: e.dma_start(out=K.dbg_pos, in_=posf[:]), reads=['posf'], writes=['dbg_pos'])
        breg = nc.gpsimd.to_reg(CAP - 1)
        for i in range(NT):
            sl = i % 2
            ur, urk = urow[sl], 'urow%d' % sl
            S.dma('sp', lambda e: e.dma_start(out=ur[:, 0:1058], in_=K.urd[i * 128:(i + 1) * 128, 0:1058]), reads=['urd%d' % i], writes=[urk])
            for ex in range(NE):
                S.dma('pool', lambda e: e.indirect_dma_start(out=K.xe_d[ex], out_offset=bass.IndirectOffsetOnAxis(ap=posi[:, i, ex:ex + 1], axis=0),
                                                             in_=ur[:, :], in_offset=None, bounds_check=breg, oob_is_err=False),
                      reads=[urk, 'posi'], writes=['xe_d_%d_%d' % (i, ex)])
        S.barrier()


def phase_moe(K, experts=range(NE)):
    nc, S = K.nc, K.S
    NT = TL // 128
    with ExitStack() as es:
        sb = lambda n, s, d: es.enter_context(nc.sbuf_tensor(_u(n), s, d))
        ps = lambda n, s, d: es.enter_context(nc.psum_tensor(_u(n), s, d))
        W = [[sb("W%d_%d" % (m, i), [128, 8, 1024], BF16) for i in range(2)] for m in range(3)]
        wst = Rot([sb("ewst%d" % i, [128, 8, 256], F32) for i in range(3)], "ewst")
        idb = sb("idb", [128, 128], BF16)
        xrow = Rot([sb("xrow%d" % i, [128, ROWW], BF16) for i in range(2)], "xrow")
        xeT = [sb("xeT%d" % i, [128, 8, 1024], BF16) for i in range(2)]
        hidT = sb("hidT", [128, 8, 1024], BF16)
        gates = [sb("gates%d" % i, [128, 8], F32) for i in range(2)]
        idxs = [sb("idxs%d" % i, [128, 8], I32) for i in range(2)]
        sgt = Rot([sb("sgt%d" % i, [128, 512], F32) for i in range(2)], "sgt")
        ye = Rot([sb("ye%d" % i, [128, 1024], F32) for i in range(2)], "ye")
        pt = Rot([ps("ept%d" % i, [128, 1024], BF16) for i in range(2)], "ept")
        pg = Rot([ps("epg%d" % i, [128, 512], F32) for i in range(6)], "epg")
        S.dma('sp', lambda e: e.dma_start(out=idb[:], in_=K.ident), writes=['idb'])
        cast_i = [0]

        def load_w(ex, slot):
            for m, src in enumerate((K.exp_w_gate, K.exp_w_up, K.exp_w_down)):
                for j in range(4):
                    w, wk = wst.get()
                    S.dma('sp', lambda e: e.dma_start(out=w[:], in_=src[ex, :, j * 256:(j + 1) * 256].rearrange("(k p) n -> p k n", p=128)), writes=[wk])
                    eng = 'dve'
                    cast_i[0] += 1
                    if eng == 'dve':
                        S.op('dve', lambda e: e.tensor_copy(out=W[m][slot][:, :, j * 256:(j + 1) * 256], in_=w[:]), reads=[wk], writes=['W%d_%d' % (m, slot)])
                    else:
                        S.op('act', lambda e: e.activation(out=W[m][slot][:, :, j * 256:(j + 1) * 256], in_=w[:], func=AF.Identity), reads=[wk], writes=['W%d_%d' % (m, slot)])

        exl = list(experts)
        load_w(exl[0], 0)
        if len(exl) > 1:
            load_w(exl[1], 1)
        def ebody(n, ex):
            xs = n % 2
            slot = n % 2
            Wg, Wu, Wd = W[0][slot], W[1][slot], W[2][slot]
            wkeys = ['W%d_%d' % (m, slot) for m in range(3)]
            for j in range(8):
                xr, xk = xrow.get()
                S.dma('sp', lambda e: e.dma_start(out=xr[:, 0:1058], in_=K.xe_d[ex][j * 128:(j + 1) * 128, 0:1058]), reads=['xe_d'], writes=[xk])
                S.op('dve', lambda e: e.tensor_copy(out=gates[xs][:, j:j + 1], in_=xr[:, 1024:1056].bitcast(F32)[:, ex:ex + 1]), reads=[xk], writes=['gates%d' % xs])
                S.op('dve', lambda e: e.tensor_copy(out=idxs[xs][:, j:j + 1], in_=xr[:, 1056:1058].bitcast(I32)), reads=[xk], writes=['idxs%d' % xs])
                p, pk = pt.get()
                for k in range(8):
                    S.op('pe', lambda e: e.transpose(out=p[:, k * 128:(k + 1) * 128], in_=xr[:, k * 128:(k + 1) * 128], identity=idb[:]), reads=[xk, 'idb'], writes=[pk], acc=True)
                S.op('act', lambda e: e.activation(out=xeT[xs][:, :, j * 128:(j + 1) * 128], in_=p[:].rearrange("p (k t) -> p k t", k=8), func=AF.Identity), reads=[pk], writes=['xeT%d' % xs])
                yield 'f'
            yield 'F'
            for fc in range(8):
                for half in range(2):
                    g_, gk = pg.get()
                    u_, uk = pg.get()
                    for kc in range(8):
                        S.op('pe', lambda e: e.matmul(g_[:], lhsT=Wg[:, kc, fc * 128:(fc + 1) * 128], rhs=xeT[xs][:, kc, half * 512:(half + 1) * 512], start=(kc == 0), stop=(kc == 7)),
                             reads=[wkeys[0], 'xeT%d' % xs], writes=[gk], acc=True)
                    for kc in range(8):
                        S.op('pe', lambda e: e.matmul(u_[:], lhsT=Wu[:, kc, fc * 128:(fc + 1) * 128], rhs=xeT[xs][:, kc, half * 512:(half + 1) * 512], start=(kc == 0), stop=(kc == 7)),
                             reads=[wkeys[1], 'xeT%d' % xs], writes=[uk], acc=True)
                    s_, sk = sgt.get()
                    S.op('act', lambda e: e.activation(out=s_[:], in_=g_[:], func=AF.Silu), reads=[gk], writes=[sk])
                    S.op('dve', lambda e: e.tensor_tensor(out=hidT[:, fc, half * 512:(half + 1) * 512], in0=s_[:], in1=u_[:], op=ALU.mult), reads=[sk, uk], writes=['hidT'])
                    yield 'b'
            for j in range(8):
                y_, yk = ye.get()
                for dh in range(2):
                    o_, ok = pg.get()
                    for fc in range(8):
                        S.op('pe', lambda e: e.matmul(o_[:], lhsT=hidT[:, fc, j * 128:(j + 1) * 128], rhs=Wd[:, fc, dh * 512:(dh + 1) * 512], start=(fc == 0), stop=(fc == 7)),
                             reads=[wkeys[2], 'hidT'], writes=[ok], acc=True)
                    S.op('act', lambda e: e.activation(out=y_[:, dh * 512:(dh + 1) * 512], in_=o_[:], func=AF.Identity, scale=gates[xs][:, j:j + 1]), reads=[ok, 'gates%d' % xs], writes=[yk])
                S.dma('pool', lambda e: e.indirect_dma_start(out=K.yacc, out_offset=bass.IndirectOffsetOnAxis(ap=idxs[xs][:, j:j + 1], axis=0), in_=y_[:, :], in_offset=None,
                                                             compute_op=ALU.add),
                      reads=[yk, 'idxs%d' % xs], writes=['yacc'])
                yield 'b'
        def step(gen_):
            try:
                return next(gen_)
            except StopIteration:
                return None
        prev = None
        for n, ex in enumerate(exl):
            gcur = ebody(n, ex)
            while True:
                r = step(gcur)
                if prev is not None and step(prev) is None:
                    prev = None
                if r == 'F' or r is None:
                    break
            while prev is not None:
                if step(prev) is None:
                    prev = None
            if n >= 1 and n + 1 < len(exl):
                load_w(exl[n + 1], (n + 1) % 2)
            prev = gcur
        while prev is not None:
            if step(prev) is None:
                prev = None
        S.barrier()


def phase_final(K):
    nc, S = K.nc, K.S
    NT = TL // 128
    with ExitStack() as es:
        sb = lambda n, s, d: es.enter_context(nc.sbuf_tensor(_u(n), s, d))
        bc = {n: sb("fbc_" + n, [128, 1024], F32) for n in ('g2', 'ln2g', 'ln2b')}
        epst = sb("epst", [128, 1], F32)
        x1 = [sb("fx1_%d" % i, [128, 1024], F32) for i in range(2)]
        ya = [sb("fya_%d" % i, [128, 1024], F32) for i in range(2)]
        t1 = sb("t1", [128, 1024], F32)
        pre = sb("pre", [128, 1024], F32)
        ob = [sb("fob_%d" % i, [128, 1024], F32) for i in range(2)]
        st = sb("st", [128, 2, 6], F32)
        mv = sb("mv", [128, 2], F32)
        lnv = sb("lnv", [128, 1], F32)
        rstd = sb("rstd", [128, 1], F32)
        nmr = sb("nmr", [128, 1], F32)
        ld1 = lambda dst, src, key, rd=(): S.dma('sp', lambda e: e.dma_start(out=dst, in_=src, allow_slow_non_contiguous=True), reads=list(rd), writes=[key])
        ld1(bc['g2'][:], K.modd[0:1, 5120:6144].partition_broadcast(128), 'fbc_g2', ['modd'])
        ld1(bc['ln2g'][:], K.ln2_g.partition_broadcast(128), 'fbc_ln2g')
        ld1(bc['ln2b'][:], K.ln2_b.partition_broadcast(128), 'fbc_ln2b')
        S.op('dve', lambda e: e.memset(epst[:], 1e-5), writes=['epst'])
        for i in range(NT):
            sl = i % 2
            S.dma('sp', lambda e: e.dma_start(out=x1[sl][:], in_=K.x1d[i * 128:(i + 1) * 128, :]), reads=['x1d'], writes=['fx1_%d' % sl])
            S.dma('sp', lambda e: e.dma_start(out=ya[sl][:], in_=K.yacc[i * 128:(i + 1) * 128, :]), reads=['yacc'], writes=['fya_%d' % sl])
            S.op('dve', lambda e: e.tensor_tensor(out=t1[:], in0=ya[sl][:], in1=bc['g2'][:], op=ALU.mult), reads=['fya_%d' % sl, 'fbc_g2'], writes=['t1'])
            S.op('dve', lambda e: e.scalar_tensor_tensor(out=pre[:], in0=x1[sl][:], scalar=ALPHA, in1=t1[:], op0=ALU.mult, op1=ALU.add), reads=['fx1_%d' % sl, 't1'], writes=['pre'])
            for c in range(2):
                S.op('dve', lambda e: e.bn_stats(out=st[:, c, :], in_=pre[:, c * 512:(c + 1) * 512]), reads=['pre'], writes=['st%d' % c])
            S.op('dve', lambda e: e.bn_aggr(out=mv[:], in_=st[:]), reads=['st0', 'st1'], writes=['mv'])
            S.op('act', lambda e: e.activation(out=lnv[:], in_=mv[:, 1:2], func=AF.Ln, bias=epst[:], scale=1.0), reads=['mv', 'epst'], writes=['lnv'])
            S.op('act', lambda e: e.activation(out=rstd[:], in_=lnv[:], func=AF.Exp, scale=-0.5), reads=['lnv'], writes=['rstd'])
            S.op('dve', lambda e: e.scalar_tensor_tensor(out=nmr[:], in0=mv[:, 0:1], scalar=-1.0, in1=rstd[:], op0=ALU.mult, op1=ALU.mult), reads=['mv', 'rstd'], writes=['nmr'])
            S.op('act', lambda e: e.activation(out=t1[:], in_=pre[:], func=AF.Identity, bias=nmr[:], scale=rstd[:]), reads=['pre', 'nmr', 'rstd'], writes=['t1'])
            S.op('dve', lambda e: e.tensor_tensor(out=t1[:], in0=t1[:], in1=bc['ln2g'][:], op=ALU.mult), reads=['t1', 'fbc_ln2g'], writes=['t1'])
            S.op('dve', lambda e: e.tensor_tensor(out=ob[sl][:], in0=t1[:], in1=bc['ln2b'][:], op=ALU.add), reads=['t1', 'fbc_ln2b'], writes=['fob_%d' % sl])
            S.dma('pool', lambda e: e.dma_start(out=K.out[i * 128:(i + 1) * 128, :], in_=ob[sl][:]), reads=['fob_%d' % sl], writes=['out'])
        S.barrier()


def build_program(debug=(), phases=None, dbg_in=(), opts=None):
    opts = opts or {}
    nc = bass.Bass("TRN2", target_bir_lowering=False)
    K = Ctx()
    K.nc = nc
    di = lambda n, s, d: nc.dram_tensor(n, s, d, kind="ExternalInput").ap()
    K.xin = di("xin", [TA, D], F32)
    K.ccT = di("ccT", [128, 8, 2], F32)
    K.w_ada = di("w_ada", [D, 6 * D], F32)
    K.b_ada = di("b_ada", [1, 6 * D], F32)
    K.w_in = di("w_in", [D, 2560], F32)
    K.cosT = di("cosT", [64, TL], F32)
    K.sinT = di("sinT", [64, TL], F32)
    K.ident = di("ident", [128, 128], BF16)
    K.identf = di("identf", [128, 128], F32)
    K.i64dbl = di("i64dbl", [128, 128], F32)
    K.bones = di("bones", [128, 128], F32)
    K.rwm = [di("rwm%d" % d, [128, 1344], F32) for d in range(2)]
    K.mla_q_norm = di("mla_q_norm", [1, 256], F32)
    K.mla_kv_norm = di("mla_kv_norm", [1, 256], F32)
    K.w_uq = di("w_uq", [256, 1024], F32)
    K.w_uk = di("w_uk", [256, 512], F32)
    K.w_uv = di("w_uv", [256, 512], F32)
    K.rwkv_conv = di("rwkv_conv", [3, 1536], F32)
    K.rwkv_w0 = di("rwkv_w0", [2, 512], F32)
    K.rwkv_w_up = di("rwkv_w_up", [2, 64, 512], F32)
    K.rwkv_a0 = di("rwkv_a0", [2, 512], F32)
    K.rwkv_a_up = di("rwkv_a_up", [2, 64, 512], F32)
    K.rwkv_g_up = di("rwkv_g_up", [128, 512], F32)
    K.rwkv_k_k = di("rwkv_k_k", [1, 512], F32)
    K.rwkv_k_a = di("rwkv_k_a", [1, 512], F32)
    K.rwkv_r_k = di("rwkv_r_k", [1, 512], F32)
    K.rwkv_gn_g = di("rwkv_gn_g", [1, 512], F32)
    K.rwkv_gn_b = di("rwkv_gn_b", [1, 512], F32)
    K.w_out = di("w_out", [D, D], F32)
    K.ln1_g = di("ln1_g", [1, D], F32)
    K.ln1_b = di("ln1_b", [1, D], F32)
    K.ln2_g = di("ln2_g", [1, D], F32)
    K.ln2_b = di("ln2_b", [1, D], F32)
    K.router = di("router", [D, NE], F32)
    K.ustrict = di("ustrict", [128, 128], BF16)
    K.exp_w_gate = di("exp_w_gate", [NE, D, D], F32)
    K.exp_w_up = di("exp_w_up", [NE, D, D], F32)
    K.exp_w_down = di("exp_w_down", [NE, D, D], F32)

    def scratch(n, s, d):
        if n in dbg_in:
            return nc.dram_tensor(n, s, d, kind="ExternalInput").ap()
        kind = "ExternalOutput" if n in debug else "Internal"
        return nc.dram_tensor(n, s, d, kind=kind).ap()
    K.modd = scratch("modd", [2, 6 * D], F32)
    K.hT = scratch("hT", [2432, TA], F32)
    K.krT = scratch("krT", [64, TA], BF16)
    K.qnT = scratch("qnT", [512, TL], BF16)
    K.qrT = scratch("qrT", [256, TL], BF16)
    K.knT = scratch("knT", [512, TA], BF16)
    K.vtok = scratch("vtok", [TA, 512], BF16)
    K.mixT = scratch("mixT", [1024, TL], BF16)
    K.rwR = scratch("rwR", [512, TA], F32)
    K.rwV = scratch("rwV", [512, TA], F32)
    K.rwKK = scratch("rwKK", [512, TA], F32)
    K.rwKD = [scratch("rwKD%d" % d, [512, TA], F32) for d in range(2)]
    K.rwB = [scratch("rwB%d" % d, [512, TA], F32) for d in range(2)]
    K.rwVtok = scratch("rwVtok", [TA, 512], F32)
    K.rwLD = [scratch("rwLD%d" % d, [TA, 512], F32) for d in range(2)]
    K.rwG = scratch("rwG", [512, TL], F32)
    K.rwBON = scratch("rwBON", [512, TL], F32)
    K.rwY0 = scratch("rwY0", [512, TL], F32)
    K.x1d = scratch("x1d", [TL, D], F32)
    K.urd = scratch("urd", [TL, ROWW], BF16)
    K.xe_d = [scratch("xe_d%d" % e_, [CAP, ROWW], BF16) for e_ in range(NE)]
    K.yacc = scratch("yacc", [TL, D], F32)
    K.dbg_pos = scratch("dbg_pos", [128, TL // 128, 16], F32) if 'dbg_pos' in debug else None
    K.out = nc.dram_tensor("out", [TL, D], F32, kind="ExternalOutput").ap()
    allp = ['mod', 'inproj', 'mlaprep', 'attn', 'rwprep', 'rwscan', 'mix', 'moe', 'final']
    if phases is None:
        phases = allp
    with ExitStack() as es:
        S = Sync(nc, es)
        K.S = S
        if 'mod' in phases:
            phase_mod(K)
        if 'inproj' in phases:
            phase_inproj(K)
        if 'mlaprep' in phases:
            phase_mlaprep(K)
        if 'attn' in phases:
            phase_attn(K)
        if 'attn1' in phases:
            phase_attn(K, heads=(1,), nqt=2)
        if 'rwprep' in phases:
            phase_rwprep(K)
        if 'rwscan' in phases:
            phase_rwscan(K, **opts.get('rwscan', {}))
        if 'mix' in phases:
            phase_mix(K)
        if 'moe' in phases:
            phase_moe(K, **opts.get('moe', {}))
        if 'final' in phases:
            phase_final(K)
        S.wait_all('sp')
        print("instructions", S.n_inst, "waits", S.n_wait, "sems", S.nsem + NDMA)
    return nc


_SWAP = np.concatenate([np.arange(16, 32), np.arange(0, 16), np.arange(48, 64), np.arange(32, 48)])


def rope_tables():
    half = 32
    inv_freq = (10000.0 ** (-np.arange(0, half, 2, dtype=np.float32) / half)).astype(np.float32)
    t = np.arange(TL)
    rr = (t // 64).astype(np.float32)[None, :]
    cc = (t % 64).astype(np.float32)[None, :]
    ang_r = (inv_freq[:, None] * rr).astype(np.float32)
    ang_c = (inv_freq[:, None] * cc).astype(np.float32)
    cosT = np.concatenate([np.cos(ang_r), np.cos(ang_r), np.cos(ang_c), np.cos(ang_c)], 0).astype(np.float32)
    sinT = np.concatenate([-np.sin(ang_r), np.sin(ang_r), -np.sin(ang_c), np.sin(ang_c)], 0).astype(np.float32)
    return np.ascontiguousarray(cosT), np.ascontiguousarray(sinT)


def make_in_maps(inputs, batches):
    f = lambda a: np.ascontiguousarray(np.asarray(a, dtype=np.float32))
    w_in = f(inputs['w_in'][0])
    w_in_ext = np.concatenate([w_in, w_in[:, 2432:2496][:, _SWAP]], axis=1)
    cosT, sinT = rope_tables()
    wuq = f(inputs['mla_w_uq'][0])
    cols = []
    for h in range(4):
        nope = wuq[:, h * 192:h * 192 + 128]
        rope = wuq[:, h * 192 + 128:h * 192 + 192]
        cols += [nope, rope, rope[:, _SWAP]]
    wuq_ext = np.ascontiguousarray(np.concatenate(cols, axis=1))
    shared = {
        'w_ada': f(inputs['w_ada'][0]), 'b_ada': f(inputs['b_ada']), 'w_in': np.ascontiguousarray(w_in_ext),
        'cosT': cosT, 'sinT': sinT, 'ident': np.eye(128).astype(ml_dtypes.bfloat16),
        'mla_q_norm': f(inputs['mla_q_norm']), 'mla_kv_norm': f(inputs['mla_kv_norm']),
        'w_uq': wuq_ext, 'w_uk': f(inputs['mla_w_uk'][0]), 'w_uv': f(inputs['mla_w_uv'][0]),
        'rwkv_conv': f(inputs['rwkv_conv'][0]), 'rwkv_w0': f(inputs['rwkv_w0'][0]), 'rwkv_w_up': f(inputs['rwkv_w_up'][0]),
        'rwkv_a0': f(inputs['rwkv_a0'][0]), 'rwkv_a_up': f(inputs['rwkv_a_up'][0]), 'rwkv_g_up': f(inputs['rwkv_g_up'][0]),
        'rwkv_k_k': f(inputs['rwkv_k_k']), 'rwkv_k_a': f(inputs['rwkv_k_a']), 'rwkv_r_k': f(inputs['rwkv_r_k']).reshape(1, 512),
        'rwkv_gn_g': f(inputs['rwkv_gn_g']), 'rwkv_gn_b': f(inputs['rwkv_gn_b']),
        'w_out': f(inputs['w_out'][0]), 'ln1_g': f(inputs['ln1_g']), 'ln1_b': f(inputs['ln1_b']),
        'ln2_g': f(inputs['ln2_g']), 'ln2_b': f(inputs['ln2_b']), 'router': f(inputs['router'][0]),
        'ustrict': (np.arange(128)[:, None] < np.arange(128)[None, :]).astype(ml_dtypes.bfloat16),
        'exp_w_gate': f(inputs['exp_w_gate'][0]), 'exp_w_up': f(inputs['exp_w_up'][0]), 'exp_w_down': f(inputs['exp_w_down'][0]),
    }
    shared.update(rw_consts())
    maps = []
    for b in batches:
        m = dict(shared)
        m['xin'] = np.ascontiguousarray(np.concatenate([inputs['ctx'][b], inputs['x'][b]], axis=0).astype(np.float32))
        cc = np.stack([inputs['c'][b], inputs['c_ctx']], axis=-1).astype(np.float32)
        m['ccT'] = np.ascontiguousarray(cc.reshape(8, 128, 2).transpose(1, 0, 2))
        maps.append(m)
    return maps


_CONST_KEYS = ('cosT', 'sinT', 'ident', 'identf', 'i64dbl', 'bones', 'rwm0', 'rwm1', 'ustrict')
BATCH_CORES = (0, 1, 4, 5)


def kernel(**inputs):
    nc = build_program()
    real = make_in_maps(inputs, [0, 1, 2, 3])
    zero = {k: (v if k in _CONST_KEYS else np.zeros_like(v)) for k, v in real[0].items()}
    maps = [zero] * 8
    for b, c in enumerate(BATCH_CORES):
        maps[c] = real[b]
    res = run_bass_kernel_spmd(nc, maps, core_ids=list(range(8)))
    out = np.stack([res.results[c]['out'] for c in BATCH_CORES], axis=0)
    return out.astype(np.float32)
```
